# Optimizing a Trainium2 kernel written in Bass

```python
import math
import jax
import jax.numpy as jnp
from jax import lax
import numpy as np

D_MODEL = 1024
BATCH = 4
SEQ = 4096
DEPTH = 2

N_EVEN = (DEPTH + 1) // 2
N_ODD = DEPTH // 2

CONV_CH = D_MODEL // 2
CONV_WIDTH = 31
ATT_HEADS = 8
HEAD_DIM = 64
ATT_WIDTH = ATT_HEADS * HEAD_DIM
DILATED = ((128, 1), (512, 4), (2048, 16))
IN_COLS = 2 * CONV_CH + 3 * ATT_WIDTH
MIX_WIDTH = CONV_CH + ATT_WIDTH
SSM_GROUP = 16
SSM_GROUPS = D_MODEL // SSM_GROUP
SSM_STATE = 64
N_EXPERTS = 16
EXPERT_HIDDEN = D_MODEL
CAPACITY_FACTOR = 2
EPS = 1e-6
NEG_INF = -1e30

kernel_name = 'hybrid_conv_dilattn_s5_ecmoe'

F32 = jnp.float32


def rmsnorm(x, g):
    xf = x.astype(F32)
    y = xf * lax.rsqrt(jnp.mean(xf * xf, axis=-1, keepdims=True) + EPS)
    return (y * g.astype(F32)).astype(x.dtype)


def layernorm(x, g, b):
    mu = jnp.mean(x, axis=-1, keepdims=True)
    var = jnp.mean(jnp.square(x - mu), axis=-1, keepdims=True)
    return (x - mu) * lax.rsqrt(var + EPS) * g + b


def conformer_conv(a_val, a_gate, conv_w, conv_b, ln_g, ln_b):
    h = a_val.astype(F32) * jax.nn.sigmoid(a_gate.astype(F32))
    h = lax.conv_general_dilated(
        h, conv_w.astype(F32)[:, None, :], window_strides=(1,),
        padding=[(CONV_WIDTH // 2, CONV_WIDTH // 2)],
        dimension_numbers=('NWC', 'WIO', 'NWC'),
        feature_group_count=CONV_CH) + conv_b.astype(F32)
    h = layernorm(h, ln_g.astype(F32), ln_b.astype(F32))
    return jax.nn.silu(h)


def dilated_band_attention(q, k, v, slopes, window, dilation):
    B, H, S, Dh = q.shape
    r = window // (2 * dilation)
    L = S // dilation
    nb = -(-L // r)
    Lp = nb * r

    def to_classes(t):
        t = t.reshape(B, H, L, dilation, Dh).transpose(0, 1, 3, 2, 4)
        return jnp.pad(t, ((0, 0), (0, 0), (0, 0), (0, Lp - L), (0, 0)))

    def key_blocks(t):
        t = jnp.pad(to_classes(t), ((0, 0), (0, 0), (0, 0), (r, r), (0, 0)))
        return t.reshape(B, H, dilation, nb + 2, r, Dh)

    qb = to_classes(q).reshape(B, H, dilation, nb, r, Dh)
    kb = key_blocks(k)
    vb = key_blocks(v)

    blk = jnp.arange(nb)[:, None, None, None]
    a_q = jnp.arange(r)[None, :, None, None]
    off = jnp.arange(3)[None, None, :, None]
    a_k = jnp.arange(r)[None, None, None, :]
    qi = blk * r + a_q
    kj = (blk + off - 1) * r + a_k
    rel = kj - qi
    valid = (jnp.abs(rel) <= r) & (kj >= 0) & (kj < L)
    dist = (dilation * jnp.abs(rel)).astype(F32)
    bias = -slopes[:, None, None, None, None, None] * dist

    scale = 1.0 / math.sqrt(Dh)
    s = jnp.stack([jnp.einsum('bhcnqd,bhcnkd->bhcnqk', qb, kb[:, :, :, o:o + nb])
                   for o in range(3)], axis=-2)
    s = jnp.where(valid, s * scale + bias, NEG_INF)
    m = jnp.max(s, axis=(-2, -1), keepdims=True)
    p = jnp.exp(s - m)
    z = jnp.sum(p, axis=(-2, -1))
    o_acc = sum(jnp.einsum('bhcnqk,bhcnkd->bhcnqd', p[..., o, :], vb[:, :, :, o:o + nb])
                for o in range(3))
    out = o_acc / z[..., None]
    lse = m[..., 0, 0] + jnp.log(z)
    out = out.reshape(B, H, dilation, Lp, Dh)[:, :, :, :L]
    out = out.transpose(0, 1, 3, 2, 4).reshape(B, H, S, Dh)
    lse = lse.reshape(B, H, dilation, Lp)[:, :, :, :L].transpose(0, 1, 3, 2).reshape(B, H, S)
    return out, lse


def even_mixer(h, w_in, conv_w, conv_b, ln_g, ln_b, q_g, k_g, w_out):
    B, S, _ = h.shape
    proj = jnp.einsum('bsd,de->bse', h, w_in)
    a_val, a_gate, q, k, v = jnp.split(
        proj, [CONV_CH, 2 * CONV_CH, 2 * CONV_CH + ATT_WIDTH, 2 * CONV_CH + 2 * ATT_WIDTH], axis=-1)
    conv_out = conformer_conv(a_val, a_gate, conv_w, conv_b, ln_g, ln_b)

    def heads(t):
        return t.reshape(B, S, ATT_HEADS, HEAD_DIM).transpose(0, 2, 1, 3).astype(F32)

    qh = rmsnorm(heads(q), q_g)
    kh = rmsnorm(heads(k), k_g)
    vh = heads(v)
    slopes = 2.0 ** (-8.0 * jnp.arange(1, ATT_HEADS + 1, dtype=F32) / ATT_HEADS)
    results = [dilated_band_attention(qh, kh, vh, slopes, w, d) for (w, d) in DILATED]
    outs = jnp.stack([res[0] for res in results])
    lses = jnp.stack([res[1] for res in results])
    wts = jax.nn.softmax(lses, axis=0)
    att = jnp.sum(wts[..., None] * outs, axis=0)
    att = att.transpose(0, 2, 1, 3).reshape(B, S, ATT_WIDTH)
    mixed = jnp.concatenate([conv_out, att], axis=-1).astype(h.dtype)
    return jnp.einsum('bsc,cd->bsd', mixed, w_out)


def s5_scan(u, lam_re, lam_im, log_dt, b_re, b_im, c_re, c_im):
    lam_re = lam_re.astype(F32)
    lam_im = lam_im.astype(F32)
    dt = jnp.exp(log_dt.astype(F32))[:, None]
    mag = jnp.exp(lam_re * dt)
    ang = lam_im * dt
    lb_re = mag * jnp.cos(ang)
    lb_im = mag * jnp.sin(ang)
    den = lam_re * lam_re + lam_im * lam_im
    nr = lb_re - 1.0
    f_re = (nr * lam_re + lb_im * lam_im) / den
    f_im = (lb_im * lam_re - nr * lam_im) / den
    b_re = b_re.astype(F32)
    b_im = b_im.astype(F32)
    bb_re = f_re[..., None] * b_re - f_im[..., None] * b_im
    bb_im = f_re[..., None] * b_im + f_im[..., None] * b_re
    bu_re = jnp.einsum('bsgc,gpc->bsgp', u, bb_re)
    bu_im = jnp.einsum('bsgc,gpc->bsgp', u, bb_im)
    a_re = jnp.broadcast_to(lb_re, bu_re.shape)
    a_im = jnp.broadcast_to(lb_im, bu_im.shape)

    def combine(e1, e2):
        a1r, a1i, b1r, b1i = e1
        a2r, a2i, b2r, b2i = e2
        return (a2r * a1r - a2i * a1i,
                a2r * a1i + a2i * a1r,
                a2r * b1r - a2i * b1i + b2r,
                a2r * b1i + a2i * b1r + b2i)

    _, _, h_re, h_im = lax.associative_scan(combine, (a_re, a_im, bu_re, bu_im), axis=1)
    return (jnp.einsum('bsgp,gcp->bsgc', h_re, c_re.astype(F32))
            - jnp.einsum('bsgp,gcp->bsgc', h_im, c_im.astype(F32)))


def s5_mixer(h, lam_re, lam_im, log_dt, b_re, b_im, c_re, c_im, d_skip, w_glu):
    B, S, D = h.shape
    hf = h.astype(F32)
    u = hf.reshape(B, S, SSM_GROUPS, SSM_GROUP)
    y_f = s5_scan(u, lam_re[0], lam_im[0], log_dt[0], b_re[0], b_im[0], c_re[0], c_im[0])
    y_b = jnp.flip(s5_scan(jnp.flip(u, axis=1), lam_re[1], lam_im[1], log_dt[1],
                           b_re[1], b_im[1], c_re[1], c_im[1]), axis=1)
    y = (y_f + y_b).reshape(B, S, D) + d_skip.astype(F32) * hf
    z = jax.nn.gelu(y)
    val, gate = jnp.split(jnp.einsum('bsd,de->bse', z, w_glu.astype(F32)), 2, axis=-1)
    return (val * jax.nn.sigmoid(gate)).astype(h.dtype)


def expert_choice_ffn(h, w_router, b_router, w_gate, w_up, w_down):
    B, S, D = h.shape
    cap = CAPACITY_FACTOR * S // N_EXPERTS
    logits = jnp.einsum('bsd,de->bse', h.astype(F32), w_router.astype(F32)) + b_router.astype(F32)
    aff = jax.nn.softmax(logits, axis=-1)
    gate, idx = lax.top_k(jnp.swapaxes(aff, 1, 2), cap)
    xin = jax.vmap(lambda hb, ib: hb[ib])(h, idx)
    g = jnp.einsum('becd,edf->becf', xin, w_gate)
    u = jnp.einsum('becd,edf->becf', xin, w_up)
    out = jnp.einsum('becf,efd->becd', jax.nn.silu(g) * u, w_down)
    out = out * gate[..., None].astype(out.dtype)

    def scatter(ob, ib):
        return jnp.zeros((S, D), ob.dtype).at[ib.reshape(-1)].add(ob.reshape(-1, D))

    return jax.vmap(scatter)(out, idx).astype(h.dtype)


def setup_inputs(seed: int = 0) -> dict:
    key = jax.random.key(seed)
    ks = iter(jax.random.split(key, 40))

    def nrm(shape, scale):
        return scale * jax.random.normal(next(ks), shape, F32)

    def gain(shape):
        return 1.0 + 0.05 * jax.random.normal(next(ks), shape, F32)

    P = SSM_STATE
    G = SSM_GROUPS
    lam_im_base = math.pi * jnp.arange(P, dtype=F32)
    return {
        'x': jax.random.normal(next(ks), (BATCH, SEQ, D_MODEL), F32),
        'mix_norm_even': gain((N_EVEN, D_MODEL)),
        'w_in': nrm((N_EVEN, D_MODEL, IN_COLS), D_MODEL ** -0.5),
        'conv_w': nrm((N_EVEN, CONV_WIDTH, CONV_CH), CONV_WIDTH ** -0.5),
        'conv_b': nrm((N_EVEN, CONV_CH), 0.01),
        'conv_ln_g': gain((N_EVEN, CONV_CH)),
        'conv_ln_b': nrm((N_EVEN, CONV_CH), 0.01),
        'q_norm': gain((N_EVEN, HEAD_DIM)),
        'k_norm': gain((N_EVEN, HEAD_DIM)),
        'w_out': nrm((N_EVEN, MIX_WIDTH, D_MODEL), MIX_WIDTH ** -0.5),
        'mix_norm_odd': gain((N_ODD, D_MODEL)),
        'ssm_lam_re': -0.5 + nrm((N_ODD, 2, G, P), 0.01),
        'ssm_lam_im': lam_im_base + nrm((N_ODD, 2, G, P), 0.01),
        'ssm_log_dt': jax.random.uniform(next(ks), (N_ODD, 2, G), F32,
                                         math.log(1e-3), math.log(1e-1)),
        'ssm_b_re': nrm((N_ODD, 2, G, P, SSM_GROUP), (2 * SSM_GROUP) ** -0.5),
        'ssm_b_im': nrm((N_ODD, 2, G, P, SSM_GROUP), (2 * SSM_GROUP) ** -0.5),
        'ssm_c_re': nrm((N_ODD, 2, G, SSM_GROUP, P), P ** -0.5),
        'ssm_c_im': nrm((N_ODD, 2, G, SSM_GROUP, P), P ** -0.5),
        'ssm_d': nrm((N_ODD, D_MODEL), 0.5),
        'w_glu': nrm((N_ODD, D_MODEL, 2 * D_MODEL), D_MODEL ** -0.5),
        'ffn_norm': gain((DEPTH, D_MODEL)),
        'w_router': nrm((DEPTH, D_MODEL, N_EXPERTS), D_MODEL ** -0.5),
        'b_router': nrm((DEPTH, N_EXPERTS), 0.01),
        'w_e_gate': nrm((DEPTH, N_EXPERTS, D_MODEL, EXPERT_HIDDEN), D_MODEL ** -0.5),
        'w_e_up': nrm((DEPTH, N_EXPERTS, D_MODEL, EXPERT_HIDDEN), D_MODEL ** -0.5),
        'w_e_down': nrm((DEPTH, N_EXPERTS, EXPERT_HIDDEN, D_MODEL), EXPERT_HIDDEN ** -0.5),
    }


def reference(x, mix_norm_even, w_in, conv_w, conv_b, conv_ln_g, conv_ln_b, q_norm, k_norm,
              w_out, mix_norm_odd, ssm_lam_re, ssm_lam_im, ssm_log_dt, ssm_b_re, ssm_b_im,
              ssm_c_re, ssm_c_im, ssm_d, w_glu, ffn_norm, w_router, b_router,
              w_e_gate, w_e_up, w_e_down):
    for layer in range(DEPTH):
        i = layer // 2
        if layer % 2 == 0:
            x = x + even_mixer(rmsnorm(x, mix_norm_even[i]), w_in[i], conv_w[i], conv_b[i],
                               conv_ln_g[i], conv_ln_b[i], q_norm[i], k_norm[i], w_out[i])
        else:
            x = x + s5_mixer(rmsnorm(x, mix_norm_odd[i]), ssm_lam_re[i], ssm_lam_im[i],
                             ssm_log_dt[i], ssm_b_re[i], ssm_b_im[i], ssm_c_re[i], ssm_c_im[i],
                             ssm_d[i], w_glu[i])
        x = x + expert_choice_ffn(rmsnorm(x, ffn_norm[layer]), w_router[layer], b_router[layer],
                                  w_e_gate[layer], w_e_up[layer], w_e_down[layer])
    return x
```

```python
import numpy as np
import contextlib
import concourse.bass as bass
import concourse.mybir as mybir
from concourse.bass_utils import run_bass_kernel_spmd

F32 = mybir.dt.float32
BF16 = mybir.dt.bfloat16
ALU = mybir.AluOpType
AF = mybir.ActivationFunctionType
AX = mybir.AxisListType

NCORES = 8
D = 1024
SEQ = 4096
OWN = 2048
WIN = 3072
EPS = 1e-6
DEBUG = False


class Buf:
    __slots__ = ("name", "w", "r")

    def __init__(self, name=""):
        self.name = name
        self.w = None
        self.r = {}


class Stream:
    def __init__(self, P, name, eng):
        self.P = P
        self.name = name
        self.eng = eng
        self.sem = P.newsem("c_" + name)
        self.cnt = 0
        self.seen = {}
        self.nslots = 8
        self.dsems = None
        self.dn = 0
        self.selfsync = name != "pe"

    def _wait(self, tok):
        key, sem, val = tok
        if self.seen.get(key, 0) < val:
            self.eng.wait_ge(sem, val)
            self.seen[key] = val

    def _deps(self, reads, writes, dma):
        toks = []
        for b in reads:
            if b.w is not None:
                toks.append(b.w)
        for b in writes:
            if b.w is not None:
                toks.append(b.w)
            toks.extend(b.r.values())
        for t in toks:
            if dma or t[0] is not self or self.selfsync:
                self._wait(t)

    def _mark(self, tok, reads, writes):
        for b in reads:
            b.r[tok[0]] = tok
        for b in writes:
            b.w = tok
            b.r = {}

    def op(self, fn, reads=(), writes=()):
        if self.P.stopped:
            return None
        self._deps(reads, writes, False)
        ins = fn(self.eng)
        self.cnt += 1
        ins.then_inc(self.sem, 1)
        self._mark((self, self.sem, self.cnt), reads, writes)
        return ins

    def dma(self, out, in_, reads=(), writes=(), **kw):
        if self.P.stopped:
            return None
        if self.dsems is None:
            self.dsems = [self.P.newsem("d%d_%s" % (i, self.name)) for i in range(self.nslots)]
        j = self.dn
        slot = j % self.nslots
        key = (self, slot)
        prev = 16 * (j // self.nslots)
        if prev > 0:
            self._wait((key, self.dsems[slot], prev))
        self._deps(reads, writes, True)
        ins = self.eng.dma_start(out=out, in_=in_, **kw)
        ins.then_inc(self.dsems[slot], 16)
        self.dn += 1
        self._mark((key, self.dsems[slot], prev + 16), reads, writes)
        return ins


class Prog:
    def __init__(self):
        self.nc = bass.Bass("TRN2", target_bir_lowering=False)
        self.stack = contextlib.ExitStack()
        self._n = 0
        self.stopped = False
        self.cur = None
        self.ccsem = None
        self.ccn = 0
        self.ncores = NCORES
        nc = self.nc
        self.pe = Stream(self, "pe", nc.tensor)
        self.act = Stream(self, "act", nc.scalar)
        self.dve = Stream(self, "dve", nc.vector)
        self.pool = Stream(self, "pool", nc.gpsimd)
        self.sp = Stream(self, "sp", nc.sync)
        self.streams = [self.pe, self.act, self.dve, self.pool, self.sp]

    def newsem(self, name):
        return self.stack.enter_context(self.nc.semaphore(name))

    def inp(self, name, shape, dt=F32):
        return self.nc.dram_tensor(name, list(shape), dt, kind="ExternalInput").ap()

    def outp(self, name, shape, dt=F32):
        return self.nc.dram_tensor(name, list(shape), dt, kind="ExternalOutput").ap()

    def sb(self, shape, dt=F32, name=None, stack=None):
        self._n += 1
        name = "s%d_%s" % (self._n, name or "t")
        return (stack or self.cur or self.stack).enter_context(self.nc.sbuf_tensor(name, list(shape), dt))

    def ps(self, shape, dt=F32, name=None, stack=None):
        self._n += 1
        name = "p%d_%s" % (self._n, name or "t")
        return (stack or self.cur or self.stack).enter_context(self.nc.psum_tensor(name, list(shape), dt))

    def barrier(self):
        toks = []
        for s in self.streams:
            if s.cnt:
                toks.append((s, s.sem, s.cnt))
            if s.dsems is not None:
                for slot in range(s.nslots):
                    n = (s.dn - slot + s.nslots - 1) // s.nslots
                    if n > 0:
                        toks.append(((s, slot), s.dsems[slot], 16 * n))
        for s in self.streams:
            for t in toks:
                if t[0] is not s:
                    s._wait(t)

    def dram(self, name, shape, dt=F32):
        return self.nc.dram_tensor(name, list(shape), dt)

    def all_gather_pair(self, src, dst):
        if self.ccsem is None:
            self.ccsem = self.newsem("ccsem")
            self.ccdummy = self.sb([128, 1], F32, name="ccdummy", stack=self.stack)
            self.bccd = Buf()
        self.barrier()
        groups = [[2 * i, 2 * i + 1] for i in range(self.ncores // 2)]
        self.ccn += 1
        n = self.ccn

        def fn(e):
            e.collective_compute("AllGather", ALU.bypass, replica_groups=groups,
                                 ins=[src.ap().opt()], outs=[dst.ap().opt()]).then_inc(self.ccsem, 1)
            e.wait_ge(self.ccsem, n)
            return e.memset(self.ccdummy[:], 0.0)
        self.pool.op(fn, [], [self.bccd])
        self.barrier()

    def finish(self):
        self.barrier()
        self.stack.close()
        return self.nc


def bcast_rows(ap, nparts):
    n = ap.shape[-1]
    return bass.AP(ap.tensor, ap.offset, [[0, nparts], [1, n]])


class Ring:
    def __init__(self, P, n, shape, dt, stack, psum=False):
        alloc = P.ps if psum else P.sb
        self.t = [alloc(shape, dt, stack=stack) for _ in range(n)]
        self.b = [Buf() for _ in range(n)]
        self.i = -1

    def next(self):
        self.i = (self.i + 1) % len(self.t)
        return self.t[self.i], self.b[self.i]


def act_fn(out, in_, func, **kw):
    return lambda e: e.activation(out=out, in_=in_, func=func, **kw)


def mm_fn(out, lhsT, rhs, start, stop):
    return lambda e: e.matmul(out, lhsT, rhs, start=start, stop=stop)


def tt_fn(out, in0, in1, op):
    return lambda e: e.tensor_tensor(out=out, in0=in0, in1=in1, op=op)


def ts_fn(out, in0, s1, s2, op0, op1=None):
    if op1 is None:
        return lambda e: e.tensor_scalar(out=out, in0=in0, scalar1=s1, scalar2=None, op0=op0)
    return lambda e: e.tensor_scalar(out=out, in0=in0, scalar1=s1, scalar2=s2, op0=op0, op1=op1)


def stt_fn(out, in0, scalar, in1, op0, op1):
    return lambda e: e.scalar_tensor_tensor(out=out, in0=in0, scalar=scalar, in1=in1, op0=op0, op1=op1)


ATT_CFG = (1, 4, 16)


def vtile_list():
    tiles = []
    for ci, d in enumerate(ATT_CFG):
        nb = OWN // (128 * d)
        for c in range(d):
            for j in range(nb + 1):
                i0 = 0 if j == 0 else 128 * j - 64
                tiles.append((ci, d, c, j, c + d * i0))
    return tiles


def rms_rstd(P, x_ap, xb, n, ring_junk, ring_small, pbuf_extra=()):
    junk, bj = ring_junk.next()
    ss, bs = ring_small.next()
    P.act.op(act_fn(junk[:, 0:n], x_ap, AF.Square, accum_out=ss[:, 0:1]), [xb], [bj, bs])
    sd, bd = ring_small.next()
    P.act.op(act_fn(sd[:, 0:1], ss[:, 0:1], AF.Sqrt, scale=1.0 / n, bias=P.eps_t[:, 0:1]), [bs, P.beps], [bd])
    rs, br = ring_small.next()
    P.dve.op(lambda e: e.reciprocal(out=rs[:, 0:1], in_=sd[:, 0:1]), [bd], [br])
    return rs, br


def load_consts(P, cst_in):
    cst = P.sb([128, 4, 128], F32, name="cst")
    P.bcst = Buf("cst")
    P.sp.dma(out=cst[:], in_=cst_in, writes=[P.bcst])
    P.identf = cst[:, 0, :]
    P.ones512 = cst[:, 1, :]
    P.ones64 = cst[:, 2, :]
    P.cst = cst
    idb = P.sb([128, 128], BF16, name="identb")
    P.bidb = Buf("identb")
    P.dve.op(lambda e: e.tensor_copy(out=idb[:], in_=cst[:, 0, :]), [P.bcst], [P.bidb])
    P.identb = idb
    eps_t = P.sb([128, 1], F32, name="eps")
    P.beps = Buf("eps")
    P.dve.op(lambda e: e.memset(eps_t[:], EPS), [], [P.beps])
    P.eps_t = eps_t


def router_tile(P, x1t, bx1, gfb, bgfb, wr, bwr, brb, bbrb, rings, aff_out, affT_out, tt, hf_out=None):
    rs, br = rms_rstd(P, x1t[:], bx1, D, rings["junk"], rings["small"])
    hf, bhf = rings["hf"].next()
    P.dve.op(stt_fn(hf[:], x1t[:], rs[:, 0:1], gfb[:], ALU.mult, ALU.mult), [bx1, br, bgfb], [bhf])
    ptf, bptf = rings["ptf"].next()
    for k in range(8):
        P.pe.op(lambda e, k=k: e.transpose(ptf[:, k, :], hf[:, k * 128:(k + 1) * 128], P.identf),
                [bhf, P.bcst], [bptf])
    hfT, bhfT = rings["hfT"].next()
    P.act.op(act_fn(hfT[:], ptf[:], AF.Copy), [bptf], [bhfT])
    plog, bplog = rings["plog"].next()
    for k in range(8):
        P.pe.op(mm_fn(plog[:, 0:16], hfT[:, k, :], wr[:, k, :], k == 0, k == 7), [bhfT, bwr], [bplog])
    lg, blg = rings["sm16"].next()
    P.dve.op(tt_fn(lg[:], plog[:, 0:16], brb[:], ALU.add), [bplog, bbrb], [blg])
    mx, bmx = rings["small"].next()
    P.dve.op(lambda e: e.reduce_max(out=mx[:, 0:1], in_=lg[:], axis=AX.X, negate=True), [blg], [bmx])
    ex, bex = rings["sm16"].next()
    sm, bsm = rings["small"].next()
    P.act.op(act_fn(ex[:], lg[:], AF.Exp, bias=mx[:, 0:1], scale=1.0, accum_out=sm[:, 0:1]), [blg, bmx], [bex, bsm])
    rsm, brsm = rings["small"].next()
    P.dve.op(lambda e: e.reciprocal(out=rsm[:, 0:1], in_=sm[:, 0:1]), [bsm], [brsm])
    af, baf = rings["sm16"].next()
    P.dve.op(ts_fn(af[:], ex[:], rsm[:, 0:1], None, ALU.mult), [bex, brsm], [baf])
    P.sp.dma(out=aff_out[tt * 128:(tt + 1) * 128, :], in_=af[:], reads=[baf])
    pat, bpat = rings["pat"].next()
    P.pe.op(lambda e: e.transpose(pat[0:16, 0:128], af[:], P.identf), [baf, P.bcst], [bpat])
    aT, baT = rings["aT"].next()
    P.act.op(act_fn(aT[:], pat[0:16, 0:128], AF.Copy), [bpat], [baT])
    P.sp.dma(out=affT_out[:, tt * 128:(tt + 1) * 128], in_=aT[:], reads=[baT])
    return hf, bhf


def emit_phaseA(P, io):
    xw, g0, w_in, cvec, qk_g, w_out = io["xw"], io["g0"], io["w_in"], io["cvec"], io["qk_g"], io["w_out"]
    gf, w_r, b_r, eb = io["gf"], io["w_r"], io["b_r"], io["eb"]
    x1_out, aff_out, affT_out = io["x1_out"], io["aff_out"], io["affT_out"]
    dbg = None
    with contextlib.ExitStack() as ph:
        P.cur = ph
        _phaseA_body(P, xw, g0, w_in, cvec, qk_g, w_out, gf, w_r, b_r, eb, x1_out, aff_out, affT_out)
        P.barrier()
    P.cur = None


def _phaseA_body(P, xw, g0, w_in, cvec, qk_g, w_out, gf, w_r, b_r, eb, x1_out, aff_out, affT_out):
    w_in_v = w_in.rearrange("(k p) e -> p k e", p=128)
    w_out_v = w_out.rearrange("(k p) e -> p k e", p=128)
    w_r_v = w_r.rearrange("(k p) e -> p k e", p=128)

    hT = P.sb([128, 8, WIN], BF16, name="hT")
    bhT = [Buf("hT%d" % i) for i in range(WIN // 128)]
    mixT = P.sb([128, 8, OWN], BF16, name="mixT")
    bmix = [Buf("mix%d" % i) for i in range(8)]
    gb = P.sb([128, D], F32, name="gb")
    bgb = Buf("gb")
    P.sp.dma(out=gb[:], in_=bcast_rows(g0, 128), writes=[bgb])
    cv = P.sb([128, 4, 34], F32, name="cv")
    bcv = Buf("cv")
    P.sp.dma(out=cv[:], in_=cvec, writes=[bcv])
    qkg = P.sb([128, 2], F32, name="qkg")
    bqkg = Buf("qkg")
    P.sp.dma(out=qkg[:], in_=qk_g, writes=[bqkg])

    with contextlib.ExitStack() as st:
        xr = Ring(P, 2, [128, D], F32, st)
        junk = Ring(P, 1, [128, D], F32, st)
        small = Ring(P, 8, [128, 1], F32, st)
        hnr = Ring(P, 2, [128, D], BF16, st)
        ptr = Ring(P, 2, [128, 8, 128], BF16, st, psum=True)
        for tt in range(WIN // 128):
            xt, bx = xr.next()
            P.sp.dma(out=xt[:], in_=xw[tt * 128:(tt + 1) * 128, :], writes=[bx])
            rs, br = rms_rstd(P, xt[:], bx, D, junk, small)
            hn, bhn = hnr.next()
            P.dve.op(stt_fn(hn[:], xt[:], rs[:, 0:1], gb[:], ALU.mult, ALU.mult), [bx, br, bgb], [bhn])
            pt, bpt = ptr.next()
            for k in range(8):
                P.pe.op(lambda e, k=k: e.transpose(pt[:, k, :], hn[:, k * 128:(k + 1) * 128], P.identb[:]),
                        [bhn, P.bidb], [bpt])
            P.act.op(act_fn(hT[:, :, tt * 128:(tt + 1) * 128], pt[:], AF.Copy), [bpt], [bhT[tt]])
        P.barrier()

    def hT_bufs(t0, n):
        return bhT[t0 // 128:(t0 + n + 127) // 128]

    NCV = OWN + 128
    with contextlib.ExitStack() as st:
        hglu = P.sb([128, 4, 15 + NCV], BF16, stack=st)
        bhg = [Buf() for _ in range(4)]
        diag = P.sb([128, 4, 31, 128], BF16, stack=st)
        bdiag = [Buf() for _ in range(4)]
        wcv = Ring(P, 2, [128, 8, 256], BF16, st)
        pmr = Ring(P, 2, [128, 512], F32, st, psum=True)
        pgr = Ring(P, 2, [128, 512], F32, st, psum=True)
        pst = Ring(P, 2, [128, 512], F32, st, psum=True)
        sgr = Ring(P, 2, [128, 512], F32, st)
        vbuf = [Ring(P, 2, [128, 512], F32, st) for _ in range(4)]
        sqr = [Ring(P, 2, [128, 512], F32, st) for _ in range(4)]
        tmp = Ring(P, 10, [128, 512], F32, st)
        zr = Ring(P, 2, [128, 512], F32, st)
        for cc in range(4):
            P.dve.op(lambda e, cc=cc: e.memset(hglu[:, cc, 0:15], 0.0), [], [bhg[cc]])
            for k in range(31):
                P.pool.op(ts_fn(diag[:, cc, k, :], P.identf, cv[:, cc, k:k + 1], None, ALU.mult),
                          [P.bcst, bcv], [bdiag[cc]])
        ntiles = [(i * 512, 512) for i in range(4)] + [(2048, 128)]
        for cc in range(4):
            w, bw = wcv.next()
            P.pool.dma(out=w[:, :, 0:128], in_=w_in_v[:, :, cc * 128:(cc + 1) * 128], writes=[bw])
            P.pool.dma(out=w[:, :, 128:256], in_=w_in_v[:, :, 512 + cc * 128:512 + (cc + 1) * 128], writes=[bw])
            for (t0, n) in ntiles:
                pm, bpm = pmr.next()
                pg, bpg = pgr.next()
                for k in range(8):
                    P.pe.op(mm_fn(pm[:, 0:n], w[:, k, 0:128], hT[:, k, t0:t0 + n], k == 0, k == 7),
                            [bw] + hT_bufs(t0, n), [bpm])
                for k in range(8):
                    P.pe.op(mm_fn(pg[:, 0:n], w[:, k, 128:256], hT[:, k, t0:t0 + n], k == 0, k == 7),
                            [bw] + hT_bufs(t0, n), [bpg])
                sg, bsg = sgr.next()
                P.act.op(act_fn(sg[:, 0:n], pg[:, 0:n], AF.Sigmoid), [bpg], [bsg])
                P.dve.op(tt_fn(hglu[:, cc, 15 + t0:15 + t0 + n], pm[:, 0:n], sg[:, 0:n], ALU.mult),
                         [bpm, bsg], [bhg[cc]])
        for nt in range(4):
            t0 = nt * 512
            vb = []
            for cc in range(4):
                pm, bpm = pmr.next()
                for k in range(31):
                    P.pe.op(mm_fn(pm[:], diag[:, cc, k, :], hglu[:, cc, t0 + k:t0 + k + 512], k == 0, k == 30),
                            [bdiag[cc], bhg[cc]], [bpm])
                v, bv = vbuf[cc].next()
                sq, bsq = sqr[cc].next()
                P.act.op(act_fn(v[:], pm[:], AF.Identity, bias=cv[:, cc, 31:32], scale=1.0), [bpm, bcv], [bv])
                P.act.op(act_fn(sq[:], pm[:], AF.Square, bias=cv[:, cc, 31:32], scale=1.0), [bpm, bcv], [bsq])
                vb.append((v, bv, sq, bsq))
            pmean, bpmean = pst.next()
            pex2, bpex2 = pst.next()
            for cc in range(4):
                P.pe.op(mm_fn(pmean[:], P.ones512, vb[cc][0][:], cc == 0, cc == 3), [P.bcst, vb[cc][1]], [bpmean])
            for cc in range(4):
                P.pe.op(mm_fn(pex2[:], P.ones512, vb[cc][2][:], cc == 0, cc == 3), [P.bcst, vb[cc][3]], [bpex2])
            mean, bmean = tmp.next()
            P.act.op(act_fn(mean[:], pmean[:], AF.Copy), [bpmean], [bmean])
            m2, bm2 = tmp.next()
            P.dve.op(tt_fn(m2[:], mean[:], mean[:], ALU.mult), [bmean], [bm2])
            var, bvar = tmp.next()
            P.dve.op(tt_fn(var[:], pex2[:], m2[:], ALU.subtract), [bpex2, bm2], [bvar])
            sd, bsd = tmp.next()
            P.act.op(act_fn(sd[:], var[:], AF.Sqrt, bias=P.eps_t[:, 0:1], scale=1.0), [bvar, P.beps], [bsd])
            rstd, brstd = tmp.next()
            P.dve.op(lambda e: e.reciprocal(out=rstd[:], in_=sd[:]), [bsd], [brstd])
            for cc in range(4):
                v, bv, sq, bsq = vb[cc]
                z, bz = zr.next()
                P.dve.op(tt_fn(z[:], v[:], mean[:], ALU.subtract), [bv, bmean], [bz])
                P.dve.op(tt_fn(z[:], z[:], rstd[:], ALU.mult), [bz, brstd], [bz])
                P.act.op(act_fn(mixT[:, cc, t0:t0 + 512], z[:], AF.Silu, scale=cv[:, cc, 32:33],
                                bias=cv[:, cc, 33:34]), [bz, bcv], [bmix[cc]])
        P.barrier()

    tiles = vtile_list()
    NT = len(tiles)
    tindex = {(ci, c, j): i for i, (ci, d, c, j, s) in enumerate(tiles)}
    with contextlib.ExitStack() as st:
        wq = Ring(P, 2, [128, 8, 384], BF16, st)
        ebr = Ring(P, 2, [128, 6, 384], F32, st)
        qT = P.sb([128, OWN], BF16, stack=st)
        bqT = Buf()
        kT = P.sb([128, WIN], BF16, stack=st)
        bkT = Buf()
        Vt = P.sb([128, NT, 256], BF16, stack=st)
        bVt = [Buf() for _ in range(NT)]
        bones = Buf()
        acc = P.sb([128, 2, OWN], F32, stack=st)
        bacc = [Buf(), Buf()]
        pmr = Ring(P, 2, [128, 512], F32, st, psum=True)
        pst = Ring(P, 1, [128, 512], F32, st, psum=True)
        psr = Ring(P, 3, [128, 512], F32, st, psum=True)
        por = Ring(P, 2, [128, 512], F32, st, psum=True)
        qfr = Ring(P, 2, [128, 512], F32, st)
        sqr = Ring(P, 2, [128, 512], F32, st)
        tmp = Ring(P, 4, [128, 512], F32, st)
        pex = Ring(P, 3, [128, 256], F32, st)
        pTr = Ring(P, 4, [128, 256], BF16, st)
        rzr = Ring(P, 1, [64, OWN], F32, st)
        Vt4 = Vt[:].rearrange("p t (s c) -> p t s c", s=4)
        P.pool.op(lambda e: e.memset(Vt4[:, :, 1, :], 1.0), [], [bones])
        P.pool.op(lambda e: e.memset(Vt4[:, :, 3, :], 1.0), [], [bones])
        ps_slot = [0]
        po_slot = [0]
        pv_slot = [0]
        psb = [[Buf(), Buf()], [Buf(), Buf()]]
        pob = [[Buf() for _ in range(4)] for _ in range(2)]
        pvb = [Buf() for _ in range(4)]
        for hp in range(4):
            w, bw = wq.next()
            for i, base in enumerate((1024, 1536, 2048)):
                P.pool.dma(out=w[:, :, i * 128:(i + 1) * 128],
                           in_=w_in_v[:, :, base + hp * 128:base + (hp + 1) * 128], writes=[bw])
            ebt, bebt = ebr.next()
            P.sp.dma(out=ebt[:], in_=eb[hp], writes=[bebt])
            for which, ntok, dst, bdst, gcol in ((0, OWN, qT, bqT, 0), (1, WIN, kT, bkT, 1)):
                for nt in range(ntok // 512):
                    t0 = nt * 512
                    pm, bpm = pmr.next()
                    for k in range(8):
                        P.pe.op(mm_fn(pm[:], w[:, k, which * 128:(which + 1) * 128], hT[:, k, t0:t0 + 512],
                                      k == 0, k == 7), [bw] + hT_bufs(t0, 512), [bpm])
                    qf, bqf = qfr.next()
                    sq, bsq = sqr.next()
                    P.act.op(act_fn(qf[:], pm[:], AF.Copy), [bpm], [bqf])
                    P.act.op(act_fn(sq[:], pm[:], AF.Square), [bpm], [bsq])
                    pms, bpms = pst.next()
                    P.pe.op(mm_fn(pms[:], P.ones64, sq[:], True, True), [P.bcst, bsq], [bpms])
                    sd, bsd = tmp.next()
                    P.act.op(act_fn(sd[:], pms[:], AF.Sqrt, bias=P.eps_t[:, 0:1], scale=1.0), [bpms, P.beps], [bsd])
                    rstd, brstd = tmp.next()
                    P.dve.op(lambda e, rstd=rstd, sd=sd: e.reciprocal(out=rstd[:], in_=sd[:]), [bsd], [brstd])
                    P.dve.op(tt_fn(qf[:], qf[:], rstd[:], ALU.mult), [bqf, brstd], [bqf])
                    P.dve.op(ts_fn(dst[:, t0:t0 + 512], qf[:], qkg[:, gcol:gcol + 1],
                                   0.125 if which == 0 else 1.0, ALU.mult, ALU.mult), [bqf, bqkg], [bdst])
            for ti, (ci, d, c, j, s0) in enumerate(tiles):
                pv, bpv = pmr.next()
                for k in range(8):
                    P.pe.op(mm_fn(pv[:, 0:128], hT[:, k, s0:s0 + 127 * d + 1:d], w[:, k, 256:384], k == 0, k == 7),
                            [bw] + bhT, [bpv])
                P.act.op(act_fn(Vt4[:, ti, 0:4:2, :], pv[:, 0:128].rearrange("p (s c) -> p s c", s=2), AF.Copy),
                         [bpv, bones], [bVt[ti]])
            for hh in range(2):
                r0 = 64 * hh
                for ci, d in enumerate(ATT_CFG):
                    nb = OWN // (128 * d)
                    for c in range(d):
                        pts = []
                        for j in range(nb + 1):
                            i0 = 0 if j == 0 else 128 * j - 64
                            ks = c + d * i0
                            jlo, jhi = max(j - 1, 0), min(j, nb - 1)
                            nq = 128 * (jhi - jlo + 1)
                            q0 = c + d * 128 * jlo
                            pst_, pbuf = psr.next()
                            sl = pst_[:, 0:nq]
                            P.pe.op(mm_fn(sl, kT[r0:r0 + 64, ks:ks + 127 * d + 1:d],
                                          qT[r0:r0 + 64, q0:q0 + (nq - 1) * d + 1:d], True, True), [bkT, bqT], [pbuf])
                            pe_, bpe = pex.next()
                            P.act.op(act_fn(pe_[:, 0:nq], sl, AF.Exp), [pbuf], [bpe])
                            if j == 0:
                                ebs = ebt[:, ci * 2 + hh, 256:384]
                            elif j == nb:
                                ebs = ebt[:, ci * 2 + hh, 0:128]
                            else:
                                ebs = ebt[:, ci * 2 + hh, 0:256]
                            pT, bpT = pTr.next()
                            P.dve.op(tt_fn(pT[:, 0:nq], pe_[:, 0:nq], ebs, ALU.mult), [bpe, bebt], [bpT])
                            pts.append((pT, bpT, nq))
                            if j >= 1:
                                jb = j - 1
                                pTa, bpTa, nqa = pts[jb]
                                a_off = 0 if jb == 0 else 128
                                po_, pobuf = por.next()
                                osl = po_[:, 0:128]
                                ta = tindex[(ci, c, jb)]
                                tb = tindex[(ci, c, j)]
                                P.pe.op(mm_fn(osl, Vt[:, ta, 128 * hh:128 * hh + 128], pTa[:, a_off:a_off + 128],
                                              True, False), [bVt[ta], bpTa], [pobuf])
                                P.pe.op(mm_fn(osl, Vt[:, tb, 128 * hh:128 * hh + 128], pT[:, 0:128],
                                              False, True), [bVt[tb], bpT], [pobuf])
                                a0 = c + d * 128 * jb
                                dsl = acc[:, hh, a0:a0 + 127 * d + 1:d]
                                if ci == 0:
                                    P.dve.op(lambda e, dsl=dsl, osl=osl: e.tensor_copy(out=dsl, in_=osl),
                                             [pobuf], [bacc[hh]])
                                else:
                                    P.dve.op(tt_fn(dsl, dsl, osl, ALU.add), [pobuf, bacc[hh]], [bacc[hh]])
                rz, brz = rzr.next()
                P.dve.op(lambda e, rz=rz, hh=hh: e.reciprocal(out=rz[:], in_=acc[64:128, hh, :]), [bacc[hh]], [brz])
                P.dve.op(tt_fn(mixT[r0:r0 + 64, 4 + hp, :], acc[0:64, hh, :], rz[:], ALU.mult),
                         [bacc[hh], brz], [bmix[4 + hp]])
        P.barrier()

    with contextlib.ExitStack() as st:
        wo = P.sb([128, 8, D], BF16, stack=st)
        bwo = Buf()
        for k in range(8):
            P.pool.dma(out=wo[:, k, :], in_=w_out_v[:, k, :], writes=[bwo])
        gfb = P.sb([128, D], F32, stack=st)
        bgfb = Buf()
        P.sp.dma(out=gfb[:], in_=bcast_rows(gf, 128), writes=[bgfb])
        wr = P.sb([128, 8, 16], F32, stack=st)
        bwr = Buf()
        P.sp.dma(out=wr[:], in_=w_r_v, writes=[bwr])
        brb = P.sb([128, 16], F32, stack=st)
        bbrb = Buf()
        P.sp.dma(out=brb[:], in_=bcast_rows(b_r, 128), writes=[bbrb])
        xr = Ring(P, 2, [128, D], F32, st)
        x1r = Ring(P, 2, [128, D], F32, st)
        pmr = Ring(P, 2, [128, 512], F32, st, psum=True)
        rings = {
            "junk": Ring(P, 1, [128, D], F32, st),
            "small": Ring(P, 12, [128, 1], F32, st),
            "hf": Ring(P, 2, [128, D], F32, st),
            "ptf": Ring(P, 1, [128, 8, 128], F32, st, psum=True),
            "hfT": Ring(P, 2, [128, 8, 128], F32, st),
            "plog": Ring(P, 2, [128, 512], F32, st, psum=True),
            "sm16": Ring(P, 6, [128, 16], F32, st),
            "pat": Ring(P, 1, [128, 512], F32, st, psum=True),
            "aT": Ring(P, 2, [16, 128], F32, st),
        }
        for tt in range(OWN // 128):
            xt, bx = xr.next()
            P.sp.dma(out=xt[:], in_=xw[tt * 128:(tt + 1) * 128, :], writes=[bx])
            x1t, bx1 = x1r.next()
            for half in range(2):
                pm, bpm = pmr.next()
                for k in range(8):
                    P.pe.op(mm_fn(pm[:], mixT[:, k, tt * 128:(tt + 1) * 128], wo[:, k, half * 512:(half + 1) * 512],
                                  k == 0, k == 7), [bmix[k], bwo], [bpm])
                P.dve.op(tt_fn(x1t[:, half * 512:(half + 1) * 512], pm[:], xt[:, half * 512:(half + 1) * 512],
                               ALU.add), [bpm, bx], [bx1])
            P.sp.dma(out=x1_out[tt * 128:(tt + 1) * 128, :], in_=x1t[:], reads=[bx1])
            router_tile(P, x1t, bx1, gfb, bgfb, wr, bwr, brb, bbrb, rings, aff_out, affT_out, tt)
    return


def host_consts():
    cst = np.zeros((128, 4, 128), np.float32)
    cst[:, 0, :] = np.eye(128, dtype=np.float32)
    cst[:, 1, :] = 1.0 / 512.0
    blk = np.zeros((128, 128), np.float32)
    blk[:64, :64] = 1.0 / 64.0
    blk[64:, 64:] = 1.0 / 64.0
    cst[:, 2, :] = blk
    cst[:, 3, :] = 1.0
    return cst


def host_eb():
    p = np.arange(128)[:, None].astype(np.float64)
    f = np.arange(128)[None, :].astype(np.float64)
    eb = np.zeros((4, 128, 6, 384), np.float32)
    for head in range(8):
        slope = 2.0 ** (-(head + 1))
        hp, hh = head // 2, head % 2
        for ci, d in enumerate(ATT_CFG):
            B = np.where(p <= f, np.exp(-slope * d * np.abs(p + 64 - f)), 0.0)
            A = np.where(p >= f, np.exp(-slope * d * np.abs(p - 64 - f)), 0.0)
            A0 = np.where((p <= 63) & (p >= f - 64), np.exp(-slope * d * np.abs(p - f)), 0.0)
            eb[hp, :, ci * 2 + hh, 0:128] = B
            eb[hp, :, ci * 2 + hh, 128:256] = A
            eb[hp, :, ci * 2 + hh, 256:384] = A0
    return eb


def local_view(xb, h, n):
    if h == 0:
        return np.ascontiguousarray(xb[:n])
    return np.ascontiguousarray(xb[::-1][:n])


_CACHE = {}


def get_prog(name, builder):
    if name not in _CACHE:
        _CACHE[name] = builder()
    return _CACHE[name]


def run_phaseA(x, mix_norm_even, w_in, conv_w, conv_b, conv_ln_g, conv_ln_b, q_norm, k_norm, w_out,
               ffn_norm0, w_router0, b_router0):
    nc = get_prog("A", build_phaseA)
    cst = host_consts()
    eb = host_eb()
    in_maps = []
    for c in range(NCORES):
        b, h = c // 2, c % 2
        cw = conv_w[0] if h == 0 else conv_w[0][::-1]
        cvec = np.zeros((128, 4, 34), np.float32)
        cvec[:, :, 0:31] = cw.T.reshape(4, 128, 31).transpose(1, 0, 2)
        cvec[:, :, 31] = conv_b[0].reshape(4, 128).T
        cvec[:, :, 32] = conv_ln_g[0].reshape(4, 128).T
        cvec[:, :, 33] = conv_ln_b[0].reshape(4, 128).T
        qk = np.stack([np.tile(q_norm[0], 2), np.tile(k_norm[0], 2)], axis=1).astype(np.float32)
        in_maps.append({
            "xw": local_view(x[b], h, WIN),
            "g0": np.ascontiguousarray(mix_norm_even[0][None, :]),
            "w_in": np.ascontiguousarray(w_in[0]),
            "cvec": cvec,
            "qk_g": np.ascontiguousarray(qk),
            "w_out": np.ascontiguousarray(w_out[0]),
            "gf": np.ascontiguousarray(ffn_norm0[None, :]),
            "w_r": np.ascontiguousarray(w_router0),
            "b_r": np.ascontiguousarray(b_router0[None, :]),
            "eb": eb,
            "cst": cst,
        })
    res = run_bass_kernel_spmd(nc, in_maps, core_ids=list(range(NCORES)))
    return res.results


NEXP = 16
CAP = 512


def emit_phaseB(P, io):
    with contextlib.ExitStack() as ph:
        P.cur = ph
        _phaseB_body(P, io["x1"], io["affT_seq"], io["aff_own"], io["gf"], io["wg"], io["wu"], io["wd"],
                     io["x2_out"], io.get("gn"), io.get("hn_out"))
        P.barrier()
    P.cur = None


def _phaseB_body(P, x1, affT_seq, aff_own, gf, wg, wu, wd, x2_out, gn, hn_out):
    if gn is not None:
        gnb = P.sb([128, D], F32, name="gnb")
        bgnb = Buf()
        P.sp.dma(out=gnb[:], in_=bcast_rows(gn, 128), writes=[bgnb])
    gfb = P.sb([128, D], F32, name="gfb")
    bgfb = Buf()
    P.sp.dma(out=gfb[:], in_=bcast_rows(gf, 128), writes=[bgfb])
    gate = P.sb([128, OWN // 128, NEXP], F32, name="gate")
    bgate = Buf()
    pthr = P.ps([128, 512], F32, name="pthr")
    bpthr = Buf()

    with contextlib.ExitStack() as st:
        aT = P.sb([NEXP, SEQ], F32, stack=st)
        baT = Buf()
        P.sp.dma(out=aT[:].rearrange("e (r t) -> e r t", r=2), in_=affT_seq, writes=[baT])
        junk = P.sb([NEXP, SEQ], F32, stack=st)
        bjunk = Buf()
        lo = P.sb([NEXP, 1], F32, stack=st)
        blo = Buf()
        mid = P.sb([NEXP, 1], F32, stack=st)
        bmid = Buf()
        cnt = P.sb([NEXP, 1], F32, stack=st)
        bcnt = Buf()
        ge = P.sb([NEXP, 1], F32, stack=st)
        bge = Buf()
        P.dve.op(lambda e: e.memset(lo[:], 0.0), [], [blo])
        for it in range(36):
            wk = 2.0 ** (-(it + 1))
            P.dve.op(ts_fn(mid[:], lo[:], wk, None, ALU.add), [blo], [bmid])
            P.dve.op(lambda e: e.tensor_scalar(out=junk[:], in0=aT[:], scalar1=mid[:, 0:1], scalar2=0.0,
                                               op0=ALU.is_ge, op1=ALU.add, accum_out=cnt[:, 0:1]),
                     [baT, bmid], [bjunk, bcnt])
            P.dve.op(ts_fn(ge[:], cnt[:], CAP - 0.5, None, ALU.is_ge), [bcnt], [bge])
            P.dve.op(stt_fn(lo[:], ge[:], wk, lo[:], ALU.mult, ALU.add), [bge, blo], [blo])
        dthr = P.sb([NEXP, NEXP], F32, stack=st)
        bdthr = Buf()
        P.dve.op(ts_fn(dthr[:], P.cst[0:NEXP, 0, 0:NEXP], lo[:, 0:1], None, ALU.mult), [P.bcst, blo], [bdthr])
        P.pe.op(mm_fn(pthr[:, 0:NEXP], P.cst[0:NEXP, 3, :], dthr[:], True, True), [P.bcst, bdthr], [bpthr])
        thrb = P.sb([128, NEXP], F32, stack=st)
        bthrb = Buf()
        P.act.op(act_fn(thrb[:], pthr[:, 0:NEXP], AF.Copy), [bpthr], [bthrb])
        ao = P.sb([128, OWN // 128, NEXP], F32, stack=st)
        bao = Buf()
        P.sp.dma(out=ao[:], in_=aff_own.rearrange("(t p) e -> p t e", p=128), writes=[bao])
        msk = P.sb([128, OWN // 128, NEXP], F32, stack=st)
        bmsk = Buf()
        thr_bc = bass.AP(thrb.tensor if hasattr(thrb, "tensor") else thrb[:].tensor, thrb[:].offset,
                         [list(thrb[:].ap[0]), [0, OWN // 128], [1, NEXP]])
        P.dve.op(tt_fn(msk[:], ao[:], thr_bc, ALU.is_ge), [bao, bthrb], [bmsk])
        P.dve.op(tt_fn(gate[:], msk[:], ao[:], ALU.mult), [bmsk, bao], [bgate])
        P.barrier()

    HT = OWN // 2
    with contextlib.ExitStack() as st:
        acc = P.sb([128, HT // 128, D], F32, stack=st)
        bacc = [Buf() for _ in range(HT // 128)]
        hT = P.sb([128, 8, HT], BF16, stack=st)
        bhT = Buf()
        junk = Ring(P, 1, [128, D], F32, st)
        small = Ring(P, 8, [128, 1], F32, st)
        hnr = Ring(P, 2, [128, D], BF16, st)
        hor = Ring(P, 2, [128, D], F32, st)
        ptr = Ring(P, 1, [128, 8, 128], BF16, st, psum=True)
        wgr = Ring(P, 2, [128, 8, D], BF16, st)
        wur = Ring(P, 2, [128, 8, D], BF16, st)
        wdr = Ring(P, 2, [128, 8, D], BF16, st)
        atr = Ring(P, 2, [128, 8, 512], BF16, st)
        sgr = Ring(P, 2, [128, 512], F32, st)
        pgr = Ring(P, 2, [128, 512], F32, st, psum=True)
        pur = Ring(P, 2, [128, 512], F32, st, psum=True)
        pyr = Ring(P, 2, [128, 512], F32, st, psum=True)
        for half in range(2):
            for t in range(HT // 128):
                tg = half * (HT // 128) + t
                P.sp.dma(out=acc[:, t, :], in_=x1[tg * 128:(tg + 1) * 128, :], writes=[bacc[t]])
                rs, br = rms_rstd(P, acc[:, t, :], bacc[t], D, junk, small)
                hn, bhn = hnr.next()
                P.dve.op(stt_fn(hn[:], acc[:, t, :], rs[:, 0:1], gfb[:], ALU.mult, ALU.mult),
                         [bacc[t], br, bgfb], [bhn])
                pt, bpt = ptr.next()
                for k in range(8):
                    P.pe.op(lambda e, k=k, pt=pt, hn=hn: e.transpose(pt[:, k, :], hn[:, k * 128:(k + 1) * 128],
                                                                      P.identb[:]), [bhn, P.bidb], [bpt])
                P.act.op(act_fn(hT[:, :, t * 128:(t + 1) * 128], pt[:], AF.Copy), [bpt], [bhT])
            for e in range(NEXP):
                wts = []
                for src, ring in ((wg, wgr), (wu, wur), (wd, wdr)):
                    w, bw = ring.next()
                    sv = src[e].rearrange("(k p) f -> p k f", p=128)
                    P.pool.dma(out=w[:, 0:4, :], in_=sv[:, 0:4, :], writes=[bw])
                    P.pool.dma(out=w[:, 4:8, :], in_=sv[:, 4:8, :], writes=[bw])
                    wts.append((w, bw))
                (Wg, bWg), (Wu, bWu), (Wd, bWd) = wts
                for q in range(HT // 512):
                    AT, bAT = atr.next()
                    for fc in range(8):
                        pg, bpg = pgr.next()
                        pu, bpu = pur.next()
                        for k in range(8):
                            P.pe.op(mm_fn(pg[:], Wg[:, k, fc * 128:(fc + 1) * 128], hT[:, k, q * 512:(q + 1) * 512],
                                          k == 0, k == 7), [bWg, bhT], [bpg])
                        for k in range(8):
                            P.pe.op(mm_fn(pu[:], Wu[:, k, fc * 128:(fc + 1) * 128], hT[:, k, q * 512:(q + 1) * 512],
                                          k == 0, k == 7), [bWu, bhT], [bpu])
                        sg, bsg = sgr.next()
                        P.act.op(act_fn(sg[:], pg[:], AF.Silu), [bpg], [bsg])
                        P.dve.op(tt_fn(AT[:, fc, :], sg[:], pu[:], ALU.mult), [bsg, bpu], [bAT])
                    for tt in range(4):
                        t = q * 4 + tt
                        tg = half * (HT // 128) + t
                        for hc in range(2):
                            py, bpy = pyr.next()
                            for fc in range(8):
                                P.pe.op(mm_fn(py[:], AT[:, fc, tt * 128:(tt + 1) * 128],
                                              Wd[:, fc, hc * 512:(hc + 1) * 512], fc == 0, fc == 7), [bAT, bWd], [bpy])
                            asl = acc[:, t, hc * 512:(hc + 1) * 512]
                            P.dve.op(stt_fn(asl, py[:], gate[:, tg, e:e + 1], asl, ALU.mult, ALU.add),
                                     [bpy, bgate, bacc[t]], [bacc[t]])
            for t in range(HT // 128):
                tg = half * (HT // 128) + t
                P.sp.dma(out=x2_out[tg * 128:(tg + 1) * 128, :], in_=acc[:, t, :], reads=[bacc[t]])
                if gn is not None:
                    rs, br = rms_rstd(P, acc[:, t, :], bacc[t], D, junk, small)
                    ho, bho = hor.next()
                    P.dve.op(stt_fn(ho[:], acc[:, t, :], rs[:, 0:1], gnb[:], ALU.mult, ALU.mult),
                             [bacc[t], br, bgnb], [bho])
                    P.sp.dma(out=hn_out[tg * 128:(tg + 1) * 128, :], in_=ho[:], reads=[bho])


def run_phaseB(x1_cores, affT_cores, aff_cores, gf, wg, wu, wd, gn):
    nc = get_prog("B", build_phaseB)
    cst = host_consts()
    in_maps = []
    for c in range(NCORES):
        b = c // 2
        affT_seq = np.ascontiguousarray(np.concatenate([affT_cores[2 * b], affT_cores[2 * b + 1]], axis=1))
        in_maps.append({
            "x1": x1_cores[c], "affT_seq": affT_seq, "aff_own": aff_cores[c],
            "gf": np.ascontiguousarray(gf[None, :]), "wg": wg, "wu": wu, "wd": wd, "cst": cst,
            "gn": np.ascontiguousarray(gn[None, :]),
        })
    res = run_bass_kernel_spmd(nc, in_maps, core_ids=list(range(NCORES)))
    return [r["x2"] for r in res.results], [r["hn"] for r in res.results]


NG = 32
NK = SEQ // 8
NWIN = NK // 8
HALF_PI = 1.5707963267948966


class _Stop(Exception):
    pass


def build_phaseC(stop_after=99):
    P = Prog()
    try:
        _phaseC_body(P, stop_after)
    except _Stop:
        P.barrier()
    return P.finish()


def _phaseC_body(P, stop_after):
    lamr_i = P.inp("lamr", [128, NG])
    lami_i = P.inp("lami", [128, NG])
    ldt_i = P.inp("ldt", [128, NG])
    B_i = P.inp("Bri", [128, 2, NG, 16])
    C_i = P.inp("Cri", [128, 2, NG, 16])
    dcol_i = P.inp("dcol", [128, NG])
    Unat = P.inp("Unat", [NG, 128, NK])
    Uw = P.inp("Uw", [NWIN, 128, NG, 8])
    Uwr = P.inp("Uwr", [NWIN, 128, NG, 8])
    masks_i = P.inp("masks", [128, 2, 128])
    cst_in = P.inp("cst", [128, 4, 128])
    zp = P.outp("zp", [NG, 128, NK])
    load_consts(P, cst_in)
    dve, act, pe, pool, sp = P.dve, P.act, P.pe, P.pool, P.sp

    M = P.sb([128, NG, 128], BF16, name="M")
    bM = Buf()
    Wz = P.sb([128, NG, 4, 128], BF16, name="Wz")
    bWz = Buf()
    Rr = P.sb([128, NG, 128], BF16, name="Rr")
    Ri = P.sb([128, NG, 128], BF16, name="Ri")
    bR = Buf()
    A8 = P.sb([128, 2, 2, NG], F32, name="A8")
    bA8 = Buf()
    dcol = P.sb([128, NG], F32, name="dcol")
    bdcol = Buf()
    sp.dma(out=dcol[:], in_=dcol_i, writes=[bdcol])

    def small(st, name=None):
        return P.sb([128, NG], F32, stack=st), Buf()

    with contextlib.ExitStack() as st:
        lamr, blamr = small(st)
        lami, blami = small(st)
        ldt, bldt = small(st)
        sp.dma(out=lamr[:], in_=lamr_i, writes=[blamr])
        sp.dma(out=lami[:], in_=lami_i, writes=[blami])
        sp.dma(out=ldt[:], in_=ldt_i, writes=[bldt])
        Bt = P.sb([128, 2, NG, 16], F32, stack=st)
        bBt = Buf()
        Ct = P.sb([128, 2, NG, 16], F32, stack=st)
        bCt = Buf()
        sp.dma(out=Bt[:], in_=B_i, writes=[bBt])
        sp.dma(out=Ct[:], in_=C_i, writes=[bCt])
        mk = P.sb([128, 2, 128], F32, stack=st)
        bmk = Buf()
        sp.dma(out=mk[:], in_=masks_i, writes=[bmk])
        hpi = P.sb([128, 1], F32, stack=st)
        bhpi = Buf()
        dve.op(lambda e: e.memset(hpi[:], HALF_PI), [], [bhpi])

        def T2(a, ba, b, bb, op):
            o, bo = small(st)
            dve.op(tt_fn(o[:], a[:], b[:], op), [ba, bb], [bo])
            return o, bo

        dt, bdt = small(st)
        act.op(act_fn(dt[:], ldt[:], AF.Exp), [bldt], [bdt])
        a_, ba_ = T2(lamr, blamr, dt, bdt, ALU.mult)
        ang, bang = T2(lami, blami, dt, bdt, ALU.mult)
        mag, bmag = small(st)
        act.op(act_fn(mag[:], a_[:], AF.Exp, scale=1.0 / 16), [ba_], [bmag])
        s16, bs16 = small(st)
        act.op(act_fn(s16[:], ang[:], AF.Sin, scale=1.0 / 16), [bang], [bs16])
        c16, bc16 = small(st)
        act.op(act_fn(c16[:], ang[:], AF.Sin, scale=1.0 / 16, bias=hpi[:, 0:1]), [bang, bhpi], [bc16])
        re, bre = T2(mag, bmag, c16, bc16, ALU.mult)
        im, bim = T2(mag, bmag, s16, bs16, ALU.mult)
        for _ in range(4):
            r2, br2 = T2(re, bre, re, bre, ALU.mult)
            i2, bi2 = T2(im, bim, im, bim, ALU.mult)
            nre, bnre = T2(r2, br2, i2, bi2, ALU.subtract)
            nim, bnim = small(st)
            dve.op(stt_fn(nim[:], re[:], 2.0, im[:], ALU.mult, ALU.mult), [bre, bim], [bnim])
            re, bre, im, bim = nre, bnre, nim, bnim
        pw = P.sb([128, 9, 2, NG], F32, stack=st)
        bpw = Buf()
        ipw = P.sb([128, 9, 2, NG], F32, stack=st)
        bipw = Buf()
        dve.op(lambda e: e.memset(pw[:, 0, 0, :], 1.0), [], [bpw])
        dve.op(lambda e: e.memset(pw[:, 0, 1, :], 0.0), [], [bpw])
        dve.op(lambda e: e.memset(ipw[:, 0, 0, :], 1.0), [], [bipw])
        dve.op(lambda e: e.memset(ipw[:, 0, 1, :], 0.0), [], [bipw])
        dve.op(lambda e: e.tensor_copy(out=pw[:, 1, 0, :], in_=re[:]), [bre], [bpw])
        dve.op(lambda e: e.tensor_copy(out=pw[:, 1, 1, :], in_=im[:]), [bim], [bpw])
        t1, bt1 = small(st)
        t2, bt2 = small(st)
        for n in range(1, 8):
            dve.op(tt_fn(t1[:], pw[:, n, 0, :], re[:], ALU.mult), [bpw, bre], [bt1])
            dve.op(tt_fn(t2[:], pw[:, n, 1, :], im[:], ALU.mult), [bpw, bim], [bt2])
            dve.op(tt_fn(pw[:, n + 1, 0, :], t1[:], t2[:], ALU.subtract), [bt1, bt2], [bpw])
            dve.op(tt_fn(t1[:], pw[:, n, 0, :], im[:], ALU.mult), [bpw, bim], [bt1])
            dve.op(tt_fn(t2[:], pw[:, n, 1, :], re[:], ALU.mult), [bpw, bre], [bt2])
            dve.op(tt_fn(pw[:, n + 1, 1, :], t1[:], t2[:], ALU.add), [bt1, bt2], [bpw])
        en, ben = small(st)
        for n in range(1, 9):
            act.op(act_fn(en[:], a_[:], AF.Exp, scale=-2.0 * n), [ba_], [ben])
            dve.op(tt_fn(ipw[:, n, 0, :], pw[:, n, 0, :], en[:], ALU.mult), [bpw, ben], [bipw])
            dve.op(stt_fn(ipw[:, n, 1, :], pw[:, n, 1, :], -1.0, en[:], ALU.mult, ALU.mult), [bpw, ben], [bipw])
        dve.op(lambda e: e.tensor_copy(out=A8[:, 0, 0, :], in_=pw[:, 8, 0, :]), [bpw], [bA8])
        dve.op(lambda e: e.tensor_copy(out=A8[:, 0, 1, :], in_=pw[:, 8, 0, :]), [bpw], [bA8])
        dve.op(ts_fn(A8[:, 1, 0, :], pw[:, 8, 1, :], -1.0, None, ALU.mult), [bpw], [bA8])
        dve.op(lambda e: e.tensor_copy(out=A8[:, 1, 1, :], in_=pw[:, 8, 1, :]), [bpw], [bA8])
        lr2, blr2 = T2(lamr, blamr, lamr, blamr, ALU.mult)
        li2, bli2 = T2(lami, blami, lami, blami, ALU.mult)
        den, bden = T2(lr2, blr2, li2, bli2, ALU.add)
        rden, brden = small(st)
        dve.op(lambda e: e.reciprocal(out=rden[:], in_=den[:]), [bden], [brden])
        nr, bnr = small(st)
        dve.op(ts_fn(nr[:], re[:], -1.0, None, ALU.add), [bre], [bnr])
        u1, bu1 = T2(nr, bnr, lamr, blamr, ALU.mult)
        u2, bu2 = T2(im, bim, lami, blami, ALU.mult)
        u3, bu3 = T2(u1, bu1, u2, bu2, ALU.add)
        fre, bfre = T2(u3, bu3, rden, brden, ALU.mult)
        u4, bu4 = T2(im, bim, lamr, blamr, ALU.mult)
        u5, bu5 = T2(nr, bnr, lami, blami, ALU.mult)
        u6, bu6 = T2(u4, bu4, u5, bu5, ALU.subtract)
        fim, bfim = T2(u6, bu6, rden, brden, ALU.mult)

        def bc16_(t):
            a = t[:]
            return bass.AP(a.tensor, a.offset, [list(a.ap[0]), [1, NG], [0, 16]])

        big = lambda: (P.sb([128, NG, 16], F32, stack=st), Buf())
        bbr, bbbr = big()
        bbi, bbbi = big()
        g1, bg1 = big()
        g2, bg2 = big()
        dve.op(tt_fn(g1[:], Bt[:, 0, :, :], bc16_(fre), ALU.mult), [bBt, bfre], [bg1])
        dve.op(tt_fn(g2[:], Bt[:, 1, :, :], bc16_(fim), ALU.mult), [bBt, bfim], [bg2])
        dve.op(tt_fn(bbr[:], g1[:], g2[:], ALU.subtract), [bg1, bg2], [bbbr])
        dve.op(tt_fn(g1[:], Bt[:, 1, :, :], bc16_(fre), ALU.mult), [bBt, bfre], [bg1])
        dve.op(tt_fn(g2[:], Bt[:, 0, :, :], bc16_(fim), ALU.mult), [bBt, bfim], [bg2])
        dve.op(tt_fn(bbi[:], g1[:], g2[:], ALU.add), [bg1, bg2], [bbbi])

        def table(top, bot):
            t = P.sb([128, 8, 2, NG], F32, stack=st)
            bt = Buf()
            for i in range(8):
                (ta, tn), (ba, bn) = top[i], bot[i]
                pool.op(lambda e, i=i, ta=ta, tn=tn: e.tensor_copy(out=t[0:64, i, :, :], in_=ta[0:64, tn, :, :]),
                        [bpw, bipw], [bt])
                pool.op(lambda e, i=i, ba=ba, bn=bn: e.tensor_copy(out=t[64:128, i, :, :], in_=ba[64:128, bn, :, :]),
                        [bpw, bipw], [bt])
            return t, bt

        QX, bQX = table([(ipw, s) for s in range(8)], [(pw, s) for s in range(8)])
        PY, bPY = table([(pw, t) for t in range(8)], [(ipw, t) for t in range(8)])
        QW, bQW = table([(pw, 7 - s) for s in range(8)], [(pw, s) for s in range(8)])
        PR, bPR = table([(pw, j + 1) for j in range(8)], [(pw, 8 - j) for j in range(8)])

        def tb(t, i, ri):
            a = t[:, i, ri, :]
            return bass.AP(a.tensor, a.offset, [list(a.ap[0]), [1, NG], [0, 16]])

        def cprod(dst_re, dst_im, bdst, xr, xi, bx, tab, btab, neg_im=False, eng=None):
            eng = eng or dve
            bx = list(bx) if isinstance(bx, (list, tuple)) else [bx]
            for i in range(8):
                eng.op(tt_fn(g1[:], xr[:], tb(tab, i, 0), ALU.mult), bx + [btab], [bg1])
                eng.op(tt_fn(g2[:], xi[:], tb(tab, i, 1), ALU.mult), bx + [btab], [bg2])
                eng.op(tt_fn(dst_re[:, :, i, :], g1[:], g2[:], ALU.subtract), [bg1, bg2], [bdst])
                eng.op(tt_fn(g1[:], xr[:], tb(tab, i, 1), ALU.mult), bx + [btab], [bg1])
                eng.op(tt_fn(g2[:], xi[:], tb(tab, i, 0), ALU.mult), bx + [btab], [bg2])
                if neg_im:
                    eng.op(stt_fn(dst_im[:, :, i, :], g1[:], -1.0, g2[:], ALU.mult, ALU.subtract), [bg1, bg2], [bdst])
                else:
                    eng.op(tt_fn(dst_im[:, :, i, :], g1[:], g2[:], ALU.add), [bg1, bg2], [bdst])

        bbx = Buf()
        with contextlib.ExitStack() as st2:
            Xr = P.sb([128, NG, 8, 16], F32, stack=st2)
            Xi = P.sb([128, NG, 8, 16], F32, stack=st2)
            Yr = P.sb([128, NG, 8, 16], F32, stack=st2)
            Yn = P.sb([128, NG, 8, 16], F32, stack=st2)
            bX, bY = Buf(), Buf()
            bBB = Buf()
            cprod(Xr, Xi, bX, bbr, bbi, [bbbr, bbbi], QX, bQX)
            cprod(Yr, Yn, bY, Ct[:, 0, :, :], Ct[:, 1, :, :], bCt, PY, bPY, neg_im=True)
            psF = Ring(P, 2, [128, 512], F32, st2, psum=True)
            psB = Ring(P, 2, [128, 512], F32, st2, psum=True)
            tmr = Ring(P, 2, [128, 128], F32, st2)
            Xr3 = Xr[:].rearrange("p g s c -> p g (s c)")
            Xi3 = Xi[:].rearrange("p g s c -> p g (s c)")
            Yr3 = Yr[:].rearrange("p g s c -> p g (s c)")
            Yn3 = Yn[:].rearrange("p g s c -> p g (s c)")
            for g in range(NG):
                pf, bpf = psF.next()
                pb_, bpb = psB.next()
                pe.op(mm_fn(pf[:, 0:128], Xr3[0:64, g, :], Yr3[0:64, g, :], True, False), [bX, bY], [bpf])
                pe.op(mm_fn(pf[:, 0:128], Xi3[0:64, g, :], Yn3[0:64, g, :], False, True), [bX, bY], [bpf])
                pe.op(mm_fn(pb_[:, 0:128], Xr3[64:128, g, :], Yr3[64:128, g, :], True, False), [bX, bY], [bpb])
                pe.op(mm_fn(pb_[:, 0:128], Xi3[64:128, g, :], Yn3[64:128, g, :], False, True), [bX, bY], [bpb])
                tm, btm = tmr.next()
                dve.op(tt_fn(tm[:], pf[:, 0:128], mk[:, 0, :], ALU.mult), [bpf, bmk], [btm])
                tm2, btm2 = tmr.next()
                dve.op(tt_fn(tm2[:], pb_[:, 0:128], mk[:, 1, :], ALU.mult), [bpb, bmk], [btm2])
                dve.op(tt_fn(M[:, g, :], tm[:], tm2[:], ALU.add), [btm, btm2], [bM])
            P.barrier()
        if stop_after <= 1:
            P.stopped = True
        with contextlib.ExitStack() as st2:
            Wr = P.sb([128, NG, 8, 16], F32, stack=st2)
            Wi = P.sb([128, NG, 8, 16], F32, stack=st2)
            bW = Buf()
            cprod(Wr, Wi, bW, bbr, bbi, [bbbr, bbbi], QW, bQW)
            pool.op(lambda e: e.memset(Wz[:], 0.0), [], [bWz])
            ptw = Ring(P, 2, [128, 512], F32, st2, psum=True)
            W3 = (Wr[:].rearrange("p g s c -> p g (s c)"), Wi[:].rearrange("p g s c -> p g (s c)"))
            for g in range(NG):
                for ri in range(2):
                    pt, bpt = ptw.next()
                    pe.op(lambda e, pt=pt, g=g, ri=ri: e.transpose(pt[:, 0:128], W3[ri][:, g, :], P.identf),
                          [bW, P.bcst], [bpt])
                    act.op(act_fn(Wz[:, g, 2 * ri, 0:64], pt[:, 0:64], AF.Copy), [bpt], [bWz])
                    act.op(act_fn(Wz[:, g, 2 * ri + 1, 64:128], pt[:, 64:128], AF.Copy), [bpt], [bWz])
            P.barrier()
        if stop_after <= 2:
            P.stopped = True
        Rr4 = Rr[:].rearrange("p g (j c) -> p g j c", j=8)
        Ri4 = Ri[:].rearrange("p g (j c) -> p g j c", j=8)
        cprod(Rr4, Ri4, bR, Ct[:, 0, :, :], Ct[:, 1, :, :], bCt, PR, bPR, neg_im=True)
        P.barrier()

    with contextlib.ExitStack() as st:
        hist = P.sb([128, 2, NG, NK], BF16, stack=st)
        bhist = Buf()
        uwr = Ring(P, 3, [128, NG, 8], BF16, st)
        uwrr = Ring(P, 3, [128, NG, 8], BF16, st)
        pvr = Ring(P, 3, [128, 2, NG, 8], F32, st, psum=True)
        S4r = Ring(P, 4, [128, 3, NG], F32, st)
        p1 = P.sb([128, 2, NG], F32, stack=st)
        p2 = P.sb([128, 2, NG], F32, stack=st)
        bp1, bp2 = Buf(), Buf()
        S4, bS = S4r.next()
        dve.op(lambda e: e.memset(S4[:], 0.0), [], [bS])
        for w in range(NWIN):
            uw, buw = uwr.next()
            uwb, buwb = uwrr.next()
            pool.dma(out=uw[:], in_=Uw[w], writes=[buw])
            pool.dma(out=uwb[:], in_=Uwr[w], writes=[buwb])
            pv, bpv = pvr.next()
            for g in range(NG):
                for ri in range(2):
                    pe.op(mm_fn(pv[:, ri, g, :], Wz[:, g, 2 * ri, :], uw[:, g, :], True, False), [bWz, buw], [bpv])
                    pe.op(mm_fn(pv[:, ri, g, :], Wz[:, g, 2 * ri + 1, :], uwb[:, g, :], False, True), [bWz, buwb], [bpv])
            for jj in range(8):
                j = w * 8 + jj
                act.op(act_fn(hist[0:64, :, :, j], S4[0:64, 0:2, :], AF.Copy), [bS], [bhist])
                act.op(act_fn(hist[64:128, :, :, NK - 1 - j], S4[64:128, 0:2, :], AF.Copy), [bS], [bhist])
                Sn, bSn = S4r.next()
                dve.op(tt_fn(p1[:], A8[:, 0, :, :], S4[:, 0:2, :], ALU.mult), [bA8, bS], [bp1])
                dve.op(tt_fn(p2[:], A8[:, 1, :, :], S4[:, 1:3, :], ALU.mult), [bA8, bS], [bp2])
                dve.op(tt_fn(p1[:], p1[:], p2[:], ALU.add), [bp1, bp2], [bp1])
                dve.op(tt_fn(Sn[:, 0:2, :], p1[:], pv[:, :, :, jj], ALU.add), [bp1, bpv], [bSn])
                dve.op(lambda e, Sn=Sn: e.tensor_copy(out=Sn[:, 2, :], in_=Sn[:, 0, :]), [bSn], [bSn])
                S4, bS = Sn, bSn
        P.barrier()
        if stop_after <= 4:
            P.stopped = True
        ubr = Ring(P, 2, [128, NK], BF16, st)
        ufr = Ring(P, 2, [128, NK], F32, st)
        pyr = Ring(P, 2, [128, 512], F32, st, psum=True)
        yr = Ring(P, 2, [128, NK], F32, st)
        tr = Ring(P, 4, [128, NK], F32, st)
        for g in range(NG):
            ub, bub = ubr.next()
            uf, buf_ = ufr.next()
            pool.dma(out=ub[:], in_=Unat[g], writes=[bub])
            sp.dma(out=uf[:], in_=Unat[g], writes=[buf_])
            py, bpy = pyr.next()
            pe.op(mm_fn(py[:], M[:, g, :], ub[:], True, False), [bM, bub], [bpy])
            pe.op(mm_fn(py[:], Rr[:, g, :], hist[:, 0, g, :], False, False), [bR, bhist], [bpy])
            pe.op(mm_fn(py[:], Ri[:, g, :], hist[:, 1, g, :], False, True), [bR, bhist], [bpy])
            y, by = yr.next()
            dve.op(stt_fn(y[:], uf[:], dcol[:, g:g + 1], py[:], ALU.mult, ALU.add), [buf_, bdcol, bpy], [by])
            a1, ba1 = tr.next()
            dve.op(tt_fn(a1[:], y[:], y[:], ALU.mult), [by], [ba1])
            dve.op(ts_fn(a1[:], a1[:], 0.044715, 1.0, ALU.mult, ALU.add), [ba1], [ba1])
            dve.op(tt_fn(a1[:], a1[:], y[:], ALU.mult), [ba1, by], [ba1])
            a2, ba2 = tr.next()
            act.op(act_fn(a2[:], a1[:], AF.Sigmoid, scale=1.5957691216057308), [ba1], [ba2])
            dve.op(tt_fn(a2[:], a2[:], y[:], ALU.mult), [ba2, by], [ba2])
            sp.dma(out=zp[g], in_=a2[:], reads=[ba2])
    return


def host_masks():
    s = np.arange(128)[:, None] // 16
    t = np.arange(128)[None, :] // 16
    m = np.zeros((128, 2, 128), np.float32)
    m[:, 0, :] = (t >= s)
    m[:, 1, :] = (s >= t)
    return m


def phaseC_inputs(hn1_seq, gh, lam_re, lam_im, log_dt, b_re, b_im, c_re, c_im, d_skip):
    G0 = gh * NG
    sl = slice(G0, G0 + NG)

    def dpg(a):
        return np.ascontiguousarray(a.transpose(0, 2, 1).reshape(128, NG))

    lamr = dpg(lam_re[:, sl, :])
    lami = dpg(lam_im[:, sl, :])
    ldt = dpg(np.broadcast_to(log_dt[:, sl, None], (2, NG, 64)))
    Bri = np.stack([b_re[:, sl].transpose(0, 2, 1, 3).reshape(128, NG, 16),
                    b_im[:, sl].transpose(0, 2, 1, 3).reshape(128, NG, 16)], axis=1)
    Cri = np.stack([c_re[:, sl].transpose(0, 3, 1, 2).reshape(128, NG, 16),
                    c_im[:, sl].transpose(0, 3, 1, 2).reshape(128, NG, 16)], axis=1)
    dg = d_skip[G0 * 16:(G0 + NG) * 16].reshape(NG, 16)
    dcol = np.ascontiguousarray(np.broadcast_to(dg.T[None, :, :], (8, 16, NG)).reshape(128, NG))
    u = hn1_seq[:, G0 * 16:(G0 + NG) * 16].reshape(NK, 8, NG, 16)
    Unat = np.ascontiguousarray(u.transpose(2, 1, 3, 0).reshape(NG, 128, NK))
    Uw = np.ascontiguousarray(Unat.reshape(NG, 128, NWIN, 8).transpose(2, 1, 0, 3))
    Uwr = np.ascontiguousarray(Unat[:, :, ::-1].reshape(NG, 128, NWIN, 8).transpose(2, 1, 0, 3))
    return {"lamr": lamr, "lami": lami, "ldt": ldt, "Bri": np.ascontiguousarray(Bri),
            "Cri": np.ascontiguousarray(Cri), "dcol": dcol, "Unat": Unat, "Uw": Uw, "Uwr": Uwr,
            "masks": host_masks(), "cst": host_consts()}


def phaseC_unpack(zp):
    return np.ascontiguousarray(zp.reshape(NG, 8, 16, NK).transpose(3, 1, 0, 2).reshape(SEQ, NG * 16))


def emit_phaseD(P, io):
    with contextlib.ExitStack() as ph:
        P.cur = ph
        _phaseD_body(P, io["zs"], io["hn"], io["dvec"], io["x2"], io["w_glu"], io["gf"], io["w_r"], io["b_r"],
                     io["x3_out"], io["aff_out"], io["affT_out"])
        P.barrier()
    P.cur = None


def _phaseD_body(P, zt, hn_in, dvec, x2, w_glu, gf, w_r, b_r, x3_out, aff_out, affT_out):
    dvb = P.sb([128, D], F32, name="dvb")
    bdvb = Buf()
    P.sp.dma(out=dvb[:], in_=bcast_rows(dvec, 128), writes=[bdvb])
    st = P.cur
    wgl = P.sb([128, 8, 2 * D], BF16, name="wgl")
    bwgl = Buf()
    wv = w_glu.rearrange("(k p) e -> p k e", p=128)
    for k in range(8):
        P.pool.dma(out=wgl[:, k, :], in_=wv[:, k, :], writes=[bwgl])
    gfb = P.sb([128, D], F32, name="gfb")
    bgfb = Buf()
    P.sp.dma(out=gfb[:], in_=bcast_rows(gf, 128), writes=[bgfb])
    wr = P.sb([128, 8, 16], F32, name="wr")
    bwr = Buf()
    P.sp.dma(out=wr[:], in_=w_r.rearrange("(k p) e -> p k e", p=128), writes=[bwr])
    brb = P.sb([128, 16], F32, name="brb")
    bbrb = Buf()
    P.sp.dma(out=brb[:], in_=bcast_rows(b_r, 128), writes=[bbrb])
    zr = Ring(P, 2, [128, D], F32, st)
    zbr = Ring(P, 2, [128, D], BF16, st)
    hnr2 = Ring(P, 2, [128, D], F32, st)
    xr = Ring(P, 2, [128, D], F32, st)
    x3r = Ring(P, 2, [128, D], F32, st)
    ptr = Ring(P, 1, [128, 8, 128], BF16, st, psum=True)
    zTr = Ring(P, 2, [128, 8, 128], BF16, st)
    pvr = Ring(P, 1, [128, 512], F32, st, psum=True)
    pgr = Ring(P, 1, [128, 512], F32, st, psum=True)
    sgr = Ring(P, 2, [128, 512], F32, st)
    rings = {
        "junk": Ring(P, 1, [128, D], F32, st),
        "small": Ring(P, 12, [128, 1], F32, st),
        "hf": Ring(P, 2, [128, D], F32, st),
        "ptf": Ring(P, 1, [128, 8, 128], F32, st, psum=True),
        "hfT": Ring(P, 2, [128, 8, 128], F32, st),
        "plog": Ring(P, 1, [128, 512], F32, st, psum=True),
        "sm16": Ring(P, 6, [128, 16], F32, st),
        "pat": Ring(P, 1, [128, 512], F32, st, psum=True),
        "aT": Ring(P, 2, [16, 128], F32, st),
    }
    for tt in range(OWN // 128):
        z_, bz = zr.next()
        P.sp.dma(out=z_[:], in_=zt[tt * 128:(tt + 1) * 128, :], writes=[bz])
        xt, bx = xr.next()
        P.sp.dma(out=xt[:], in_=x2[tt * 128:(tt + 1) * 128, :], writes=[bx])
        hn_, bhn_ = hnr2.next()
        P.sp.dma(out=hn_[:], in_=hn_in[tt * 128:(tt + 1) * 128, :], writes=[bhn_])
        P.dve.op(tt_fn(hn_[:], hn_[:], dvb[:], ALU.mult), [bhn_, bdvb], [bhn_])
        P.dve.op(tt_fn(z_[:], z_[:], hn_[:], ALU.add), [bz, bhn_], [bz])
        P.dve.op(tt_fn(hn_[:], z_[:], z_[:], ALU.mult), [bz], [bhn_])
        P.dve.op(ts_fn(hn_[:], hn_[:], 0.044715, 1.0, ALU.mult, ALU.add), [bhn_], [bhn_])
        P.dve.op(tt_fn(hn_[:], hn_[:], z_[:], ALU.mult), [bhn_, bz], [bhn_])
        P.act.op(act_fn(hn_[:], hn_[:], AF.Sigmoid, scale=1.5957691216057308), [bhn_], [bhn_])
        zb, bzb = zbr.next()
        P.dve.op(tt_fn(zb[:], hn_[:], z_[:], ALU.mult), [bhn_, bz], [bzb])
        pt, bpt = ptr.next()
        for k in range(8):
            P.pe.op(lambda e, k=k, pt=pt, zb=zb: e.transpose(pt[:, k, :], zb[:, k * 128:(k + 1) * 128], P.identb[:]),
                    [bzb, P.bidb], [bpt])
        zT, bzT = zTr.next()
        P.act.op(act_fn(zT[:], pt[:], AF.Copy), [bpt], [bzT])
        x3t, bx3 = x3r.next()
        for half in range(2):
            pv, bpv = pvr.next()
            pg, bpg = pgr.next()
            for k in range(8):
                P.pe.op(mm_fn(pv[:], zT[:, k, :], wgl[:, k, half * 512:(half + 1) * 512], k == 0, k == 7),
                        [bzT, bwgl], [bpv])
            for k in range(8):
                P.pe.op(mm_fn(pg[:], zT[:, k, :], wgl[:, k, D + half * 512:D + (half + 1) * 512], k == 0, k == 7),
                        [bzT, bwgl], [bpg])
            sg, bsg = sgr.next()
            P.act.op(act_fn(sg[:], pg[:], AF.Sigmoid), [bpg], [bsg])
            P.dve.op(tt_fn(sg[:], sg[:], pv[:], ALU.mult), [bsg, bpv], [bsg])
            P.dve.op(tt_fn(x3t[:, half * 512:(half + 1) * 512], sg[:], xt[:, half * 512:(half + 1) * 512], ALU.add),
                     [bsg, bx], [bx3])
        P.sp.dma(out=x3_out[tt * 128:(tt + 1) * 128, :], in_=x3t[:], reads=[bx3])
        router_tile(P, x3t, bx3, gfb, bgfb, wr, bwr, brb, bbrb, rings, aff_out, affT_out, tt)
    return


def to_local(full_seq, h):
    return local_view(full_seq, h, OWN)


def from_local(parts):
    out = []
    for b in range(NCORES // 2):
        a0 = parts[2 * b]
        a1 = parts[2 * b + 1][::-1]
        out.append(np.concatenate([a0, a1], axis=0))
    return np.stack(out)


G32 = 32
NKL = OWN // 8
NWL = NKL // 8


def emit_phaseC2(P, io):
    with contextlib.ExitStack() as ph:
        P.cur = ph
        _phaseC2_body(P, io)
        P.barrier()
    P.cur = None


def _phaseC2_body(P, io):
    dve, act, pe, pool, sp = P.dve, P.act, P.pe, P.pool, P.sp
    s5p, s5B, s5C = io["s5p"], io["s5B"], io["s5C"]
    HN, ZS = io["hn"], io["zs_out"]
    SAo, SAall = io["sa_own"], io["sa_all"]

    MS, RS = io["ms"], io["rs"]
    bM = Buf()
    bR = Buf()
    U = P.sb([128, 64, NKL], BF16, name="U")
    bU = Buf()
    A8 = [P.sb([128, 2, 2, G32], F32, name="A8_%d" % d) for d in range(2)]
    bA8 = Buf()
    mk = P.sb([128, 2, 128], F32, name="mk")
    bmk = Buf()
    sp.dma(out=mk[:], in_=io["masks"], writes=[bmk])
    flg = P.sb([128, 2], F32, name="flg")
    bflg = Buf()
    sp.dma(out=flg[:], in_=io["flags"], writes=[bflg])
    hpi = P.sb([128, 1], F32, name="hpi")
    bhpi = Buf()
    dve.op(lambda e: e.memset(hpi[:], HALF_PI), [], [bhpi])
    g1 = P.sb([128, G32, 16], F32, name="g1")
    g2 = P.sb([128, G32, 16], F32, name="g2")
    bg1, bg2 = Buf(), Buf()
    pw_t = [P.sb([128, 9, 2, G32], F32, name="pw%d" % d) for d in range(2)]
    ipw_t = [P.sb([128, 9, 2, G32], F32, name="ipw%d" % d) for d in range(2)]
    bbr_t = [P.sb([128, G32, 16], F32, name="bbr%d" % d) for d in range(2)]
    bbi_t = [P.sb([128, G32, 16], F32, name="bbi%d" % d) for d in range(2)]
    with contextlib.ExitStack() as stp:
        M = P.sb([128, 64, 128], BF16, name="M", stack=stp)
        Rt = [[P.sb([128, G32, 128], BF16, name="R%d%d" % (d, r), stack=stp) for r in range(2)] for d in range(2)]
        par = P.sb([128, 2, 3, G32], F32, name="par", stack=stp)
        bpar = Buf()
        sp.dma(out=par[:], in_=s5p, writes=[bpar])
        Bt = P.sb([128, 2, 2, G32, 16], F32, name="Bt", stack=stp)
        bBt = Buf()
        sp.dma(out=Bt[:], in_=s5B, writes=[bBt])
        Ct = P.sb([128, 2, 2, G32, 16], F32, name="Ct", stack=stp)
        bCt = Buf()
        sp.dma(out=Ct[:], in_=s5C, writes=[bCt])

        def small():
            return P.sb([128, G32], F32, stack=stp), Buf()

        def T2(a, ba, b, bb, op):
            o, bo = small()
            dve.op(tt_fn(o[:], a[:], b[:], op), [ba, bb], [bo])
            return o, bo

        def bc(a):
            return bass.AP(a.tensor, a.offset, [list(a.ap[0]), [1, G32], [0, 16]])

        pws, ipws, bbs = [], [], []
        for d in range(2):
            lamr, lami, ldt = par[:, d, 0, :], par[:, d, 1, :], par[:, d, 2, :]
            dt, bdt = small()
            act.op(act_fn(dt[:], ldt, AF.Exp), [bpar], [bdt])
            a_, ba_ = small()
            dve.op(tt_fn(a_[:], lamr, dt[:], ALU.mult), [bpar, bdt], [ba_])
            ang, bang = small()
            dve.op(tt_fn(ang[:], lami, dt[:], ALU.mult), [bpar, bdt], [bang])
            mag, bmag = small()
            act.op(act_fn(mag[:], a_[:], AF.Exp, scale=1.0 / 16), [ba_], [bmag])
            s16, bs16 = small()
            act.op(act_fn(s16[:], ang[:], AF.Sin, scale=1.0 / 16), [bang], [bs16])
            c16, bc16 = small()
            act.op(act_fn(c16[:], ang[:], AF.Sin, scale=1.0 / 16, bias=hpi[:, 0:1]), [bang, bhpi], [bc16])
            re, bre = T2(mag, bmag, c16, bc16, ALU.mult)
            im, bim = T2(mag, bmag, s16, bs16, ALU.mult)
            for _ in range(4):
                r2, br2 = T2(re, bre, re, bre, ALU.mult)
                i2, bi2 = T2(im, bim, im, bim, ALU.mult)
                nre, bnre = T2(r2, br2, i2, bi2, ALU.subtract)
                nim, bnim = small()
                dve.op(stt_fn(nim[:], re[:], 2.0, im[:], ALU.mult, ALU.mult), [bre, bim], [bnim])
                re, bre, im, bim = nre, bnre, nim, bnim
            pw = pw_t[d]
            ipw = ipw_t[d]
            bpw, bipw = Buf(), Buf()
            dve.op(lambda e, pw=pw: e.memset(pw[:, 0, 0, :], 1.0), [], [bpw])
            dve.op(lambda e, pw=pw: e.memset(pw[:, 0, 1, :], 0.0), [], [bpw])
            dve.op(lambda e, ipw=ipw: e.memset(ipw[:, 0, 0, :], 1.0), [], [bipw])
            dve.op(lambda e, ipw=ipw: e.memset(ipw[:, 0, 1, :], 0.0), [], [bipw])
            dve.op(lambda e, pw=pw, re=re: e.tensor_copy(out=pw[:, 1, 0, :], in_=re[:]), [bre], [bpw])
            dve.op(lambda e, pw=pw, im=im: e.tensor_copy(out=pw[:, 1, 1, :], in_=im[:]), [bim], [bpw])
            t1, bt1 = small()
            t2, bt2 = small()
            for n in range(1, 8):
                dve.op(tt_fn(t1[:], pw[:, n, 0, :], re[:], ALU.mult), [bpw, bre], [bt1])
                dve.op(tt_fn(t2[:], pw[:, n, 1, :], im[:], ALU.mult), [bpw, bim], [bt2])
                dve.op(tt_fn(pw[:, n + 1, 0, :], t1[:], t2[:], ALU.subtract), [bt1, bt2], [bpw])
                dve.op(tt_fn(t1[:], pw[:, n, 0, :], im[:], ALU.mult), [bpw, bim], [bt1])
                dve.op(tt_fn(t2[:], pw[:, n, 1, :], re[:], ALU.mult), [bpw, bre], [bt2])
                dve.op(tt_fn(pw[:, n + 1, 1, :], t1[:], t2[:], ALU.add), [bt1, bt2], [bpw])
            en, ben = small()
            for n in range(1, 9):
                act.op(act_fn(en[:], a_[:], AF.Exp, scale=-2.0 * n), [ba_], [ben])
                dve.op(tt_fn(ipw[:, n, 0, :], pw[:, n, 0, :], en[:], ALU.mult), [bpw, ben], [bipw])
                dve.op(stt_fn(ipw[:, n, 1, :], pw[:, n, 1, :], -1.0, en[:], ALU.mult, ALU.mult), [bpw, ben], [bipw])
            a8 = A8[d]
            dve.op(lambda e, a8=a8, pw=pw: e.tensor_copy(out=a8[:, 0, 0, :], in_=pw[:, 8, 0, :]), [bpw], [bA8])
            dve.op(lambda e, a8=a8, pw=pw: e.tensor_copy(out=a8[:, 0, 1, :], in_=pw[:, 8, 0, :]), [bpw], [bA8])
            dve.op(ts_fn(a8[:, 1, 0, :], pw[:, 8, 1, :], -1.0, None, ALU.mult), [bpw], [bA8])
            dve.op(lambda e, a8=a8, pw=pw: e.tensor_copy(out=a8[:, 1, 1, :], in_=pw[:, 8, 1, :]), [bpw], [bA8])
            lr2, blr2 = small()
            dve.op(tt_fn(lr2[:], lamr, lamr, ALU.mult), [bpar], [blr2])
            li2, bli2 = small()
            dve.op(tt_fn(li2[:], lami, lami, ALU.mult), [bpar], [bli2])
            den, bden = T2(lr2, blr2, li2, bli2, ALU.add)
            rden, brden = small()
            dve.op(lambda e, rden=rden, den=den: e.reciprocal(out=rden[:], in_=den[:]), [bden], [brden])
            nr, bnr = small()
            dve.op(ts_fn(nr[:], re[:], -1.0, None, ALU.add), [bre], [bnr])
            u1, bu1 = small()
            dve.op(tt_fn(u1[:], nr[:], lamr, ALU.mult), [bnr, bpar], [bu1])
            u2, bu2 = small()
            dve.op(tt_fn(u2[:], im[:], lami, ALU.mult), [bim, bpar], [bu2])
            u3, bu3 = T2(u1, bu1, u2, bu2, ALU.add)
            fre, bfre = T2(u3, bu3, rden, brden, ALU.mult)
            u4, bu4 = small()
            dve.op(tt_fn(u4[:], im[:], lamr, ALU.mult), [bim, bpar], [bu4])
            u5, bu5 = small()
            dve.op(tt_fn(u5[:], nr[:], lami, ALU.mult), [bnr, bpar], [bu5])
            u6, bu6 = T2(u4, bu4, u5, bu5, ALU.subtract)
            fim, bfim = T2(u6, bu6, rden, brden, ALU.mult)
            bbr = bbr_t[d]
            bbi = bbi_t[d]
            bbb = Buf()
            dve.op(tt_fn(g1[:], Bt[:, d, 0, :, :], bc(fre[:]), ALU.mult), [bBt, bfre], [bg1])
            dve.op(tt_fn(g2[:], Bt[:, d, 1, :, :], bc(fim[:]), ALU.mult), [bBt, bfim], [bg2])
            dve.op(tt_fn(bbr[:], g1[:], g2[:], ALU.subtract), [bg1, bg2], [bbb])
            dve.op(tt_fn(g1[:], Bt[:, d, 1, :, :], bc(fre[:]), ALU.mult), [bBt, bfre], [bg1])
            dve.op(tt_fn(g2[:], Bt[:, d, 0, :, :], bc(fim[:]), ALU.mult), [bBt, bfim], [bg2])
            dve.op(tt_fn(bbi[:], g1[:], g2[:], ALU.add), [bg1, bg2], [bbb])
            pws.append((pw, bpw))
            ipws.append((ipw, bipw))
            bbs.append((bbr, bbi, bbb))

        def cprod(dst_re, dst_im, bdst, xr, xi, bx, tab, btab, idx, neg_im=False):
            bx = list(bx) if isinstance(bx, (list, tuple)) else [bx]
            for i in range(8):
                tr, ti = bc(tab[:, idx(i), 0, :]), bc(tab[:, idx(i), 1, :])
                dve.op(tt_fn(g1[:], xr, tr, ALU.mult), bx + [btab], [bg1])
                dve.op(tt_fn(g2[:], xi, ti, ALU.mult), bx + [btab], [bg2])
                dve.op(tt_fn(dst_re[:, :, i, :], g1[:], g2[:], ALU.subtract), [bg1, bg2], [bdst])
                dve.op(tt_fn(g1[:], xr, ti, ALU.mult), bx + [btab], [bg1])
                dve.op(tt_fn(g2[:], xi, tr, ALU.mult), bx + [btab], [bg2])
                if neg_im:
                    dve.op(stt_fn(dst_im[:, :, i, :], g1[:], -1.0, g2[:], ALU.mult, ALU.subtract), [bg1, bg2], [bdst])
                else:
                    dve.op(tt_fn(dst_im[:, :, i, :], g1[:], g2[:], ALU.add), [bg1, bg2], [bdst])

        v4 = lambda t: t[:].rearrange("p g (j c) -> p g j c", j=8)
        f3 = lambda t: t[:].rearrange("p g s c -> p g (s c)")

        with contextlib.ExitStack() as st2:
            XY = [[P.sb([128, G32, 8, 16], BF16, stack=st2) for _ in range(4)] for _ in range(2)]
            bXY = Buf()
            (pwA, bpwA), (ipwA, bipwA) = pws[0], ipws[0]
            (pwB, bpwB), (ipwB, bipwB) = pws[1], ipws[1]
            cprod(XY[0][0], XY[0][1], bXY, bbs[0][0][:], bbs[0][1][:], bbs[0][2], ipwA, bipwA, lambda s: s)
            cprod(XY[0][2], XY[0][3], bXY, Ct[:, 0, 0, :, :], Ct[:, 0, 1, :, :], bCt, pwA, bpwA, lambda t: t, neg_im=True)
            cprod(XY[1][0], XY[1][1], bXY, bbs[1][0][:], bbs[1][1][:], bbs[1][2], pwB, bpwB, lambda s: s)
            cprod(XY[1][2], XY[1][3], bXY, Ct[:, 1, 0, :, :], Ct[:, 1, 1, :, :], bCt, ipwB, bipwB, lambda t: t, neg_im=True)
            cprod(v4(Rt[0][0]), v4(Rt[0][1]), bR, Ct[:, 0, 0, :, :], Ct[:, 0, 1, :, :], bCt, pwA, bpwA,
                  lambda j: j + 1, neg_im=True)
            cprod(v4(Rt[1][0]), v4(Rt[1][1]), bR, Ct[:, 1, 0, :, :], Ct[:, 1, 1, :, :], bCt, pwB, bpwB,
                  lambda j: 8 - j, neg_im=True)
            pk = [[Ring(P, 1, [128, 512], F32, st2, psum=True) for _ in range(2)] for _ in range(2)]
            tmr = Ring(P, 4, [128, 128], F32, st2)
            for g in range(G32):
                pp = [[None, None], [None, None]]
                for d in range(2):
                    Xr, Xi, Yr, Yn = [f3(t) for t in XY[d]]
                    for hf in range(2):
                        r0 = 64 * hf
                        pt, bpt = pk[d][hf].next()
                        pe.op(mm_fn(pt[:, 0:128], Xr[r0:r0 + 64, g, :], Yr[r0:r0 + 64, g, :], True, False), [bXY], [bpt])
                        pe.op(mm_fn(pt[:, 0:128], Xi[r0:r0 + 64, g, :], Yn[r0:r0 + 64, g, :], False, True), [bXY], [bpt])
                        pp[d][hf] = (pt, bpt)
                for hf in range(2):
                    tm, btm = tmr.next()
                    dve.op(tt_fn(tm[:], pp[0][hf][0][:, 0:128], mk[:, 0, :], ALU.mult), [pp[0][hf][1], bmk], [btm])
                    tm2, btm2 = tmr.next()
                    dve.op(tt_fn(tm2[:], pp[1][hf][0][:, 0:128], mk[:, 1, :], ALU.mult), [pp[1][hf][1], bmk], [btm2])
                    dve.op(tt_fn(M[:, hf * G32 + g, :], tm[:], tm2[:], ALU.add), [btm, btm2], [bM])
            sp.dma(out=MS.ap(), in_=M[:].rearrange("p g c -> p (g c)"), reads=[bM])
            for d in range(2):
                for r in range(2):
                    sp.dma(out=RS.ap()[:, (2 * d + r) * G32 * 128:(2 * d + r + 1) * G32 * 128],
                           in_=Rt[d][r][:].rearrange("p g c -> p (g c)"), reads=[bR])
            P.barrier()

    with contextlib.ExitStack() as st2:
        Tb = Ring(P, 2, [128, 8, D], BF16, st2)
        Tb2 = Ring(P, 1, [128, 64, 128], BF16, st2)
        ptu = Ring(P, 2, [128, 8, 128], BF16, st2, psum=True)
        for kb in range(NKL // 128):
            tb, btb = Tb.next()
            src = HN[kb * 1024:(kb + 1) * 1024, :].rearrange("(p s) d -> p s d", s=8)
            pool.dma(out=tb[:], in_=src, writes=[btb])
            tb2, btb2 = Tb2.next()
            dve.op(lambda e, tb=tb, tb2=tb2: e.tensor_copy(
                out=tb2[:].rearrange("p g (s c) -> p s g c", s=8),
                in_=tb[:].rearrange("p s (g c) -> p s g c", c=16)), [btb], [btb2])
            for g0 in range(0, 64, 8):
                pt, bpt = ptu.next()
                for gi in range(8):
                    g = g0 + gi
                    pe.op(lambda e, pt=pt, gi=gi, g=g, tb2=tb2: e.transpose(pt[:, gi, :], tb2[:, g, :],
                                                                           P.identb[:]), [btb2, P.bidb], [bpt])
                act.op(act_fn(U[:, g0:g0 + 8, kb * 128:(kb + 1) * 128], pt[:], AF.Copy), [bpt], [bU])
        P.barrier()

    with contextlib.ExitStack() as sth:
        hist = [P.sb([128, 2, G32, NKL], BF16, stack=sth) for _ in range(2)]
        bhist = Buf()
        with contextlib.ExitStack() as st3:
            Wz = P.sb([128, G32, 4, 128], BF16, stack=st3)
            bWz = Buf()
            WT = [P.sb([128, G32, 8, 16], BF16, stack=st3) for _ in range(2)]
            bWT = Buf()
            ptw = Ring(P, 2, [128, 4, 128], BF16, st3, psum=True)
            pvr = Ring(P, 3, [128, 2, G32, 8], F32, st3, psum=True)
            S4r = Ring(P, 4, [128, 3, G32], F32, st3)
            p1 = P.sb([128, 2, G32], F32, stack=st3)
            p2 = P.sb([128, 2, G32], F32, stack=st3)
            bp1, bp2 = Buf(), Buf()
            gx = [P.sb([128, 2 * G32], F32, stack=st3) for _ in range(2)]
            bgx = Buf()
            for d in range(2):
                pw, bpw = pws[d]
                cprod(WT[0], WT[1], bWT, bbs[d][0][:], bbs[d][1][:], bbs[d][2], pw, bpw,
                      (lambda s: 7 - s) if d == 0 else (lambda s: s))
                pool.op(lambda e: e.memset(Wz[:], 0.0), [], [bWz])
                W3 = (f3(WT[0]), f3(WT[1]))
                for g in range(G32):
                    pt, bpt = ptw.next()
                    for ri in range(2):
                        pe.op(lambda e, pt=pt, g=g, ri=ri: e.transpose(pt[:, ri, :], W3[ri][:, g, :], P.identb[:]),
                              [bWT, P.bidb], [bpt])
                    act.op(act_fn(Wz[:, g, 0:4:2, 0:64], pt[:, 0:2, 0:64], AF.Copy), [bpt], [bWz])
                    act.op(act_fn(Wz[:, g, 1:4:2, 64:128], pt[:, 0:2, 64:128], AF.Copy), [bpt], [bWz])
                S4, bS = S4r.next()
                if d == 0:
                    dve.op(lambda e, S4=S4: e.memset(S4[:], 0.0), [], [bS])
                else:
                    sp.dma(out=gx[0][:], in_=SAall.ap()[0:128, :], writes=[bgx])
                    sp.dma(out=gx[1][:], in_=SAall.ap()[128:256, :], writes=[bgx])
                    dve.op(ts_fn(gx[0][:], gx[0][:], flg[:, 0:1], None, ALU.mult), [bgx, bflg], [bgx])
                    S2v = S4[:, 0:2, :].rearrange("p r g -> p (r g)")
                    dve.op(stt_fn(S2v, gx[1][:], flg[:, 1:2], gx[0][:], ALU.mult, ALU.add), [bgx, bflg], [bS])
                    dve.op(lambda e, S4=S4: e.tensor_copy(out=S4[:, 2, :], in_=S4[:, 0, :]), [bS], [bS])
                a8 = A8[d]
                for wi in range(NWL):
                    w = wi if d == 0 else NWL - 1 - wi
                    pv, bpv = pvr.next()
                    for g in range(G32):
                        for ri in range(2):
                            pe.op(mm_fn(pv[:, ri, g, :], Wz[:, g, 2 * ri, :], U[:, g, 8 * w:8 * w + 8], True, False),
                                  [bWz, bU], [bpv])
                            pe.op(mm_fn(pv[:, ri, g, :], Wz[:, g, 2 * ri + 1, :], U[:, G32 + g, 8 * w:8 * w + 8],
                                        False, True), [bWz, bU], [bpv])
                    for ji in range(8):
                        jj = ji if d == 0 else 7 - ji
                        k = 8 * w + jj
                        act.op(act_fn(hist[d][:, :, :, k], S4[:, 0:2, :], AF.Copy), [bS], [bhist])
                        Sn, bSn = S4r.next()
                        dve.op(tt_fn(p1[:], a8[:, 0, :, :], S4[:, 0:2, :], ALU.mult), [bA8, bS], [bp1])
                        dve.op(tt_fn(p2[:], a8[:, 1, :, :], S4[:, 1:3, :], ALU.mult), [bA8, bS], [bp2])
                        dve.op(tt_fn(p1[:], p1[:], p2[:], ALU.add), [bp1, bp2], [bp1])
                        dve.op(tt_fn(Sn[:, 0:2, :], p1[:], pv[:, :, :, jj], ALU.add), [bp1, bpv], [bSn])
                        dve.op(lambda e, Sn=Sn: e.tensor_copy(out=Sn[:, 2, :], in_=Sn[:, 0, :]), [bSn], [bSn])
                        S4, bS = Sn, bSn
                if d == 0:
                    sp.dma(out=SAo.ap(), in_=S4[:, 0:2, :].rearrange("p r g -> p (r g)"), reads=[bS])
                    P.all_gather_pair(SAo, SAall)
            P.barrier()
        with contextlib.ExitStack() as st4:
            Z = P.sb([128, 8, D // 2], F32, stack=st4)
            bZ = Buf()
            M = P.sb([128, 64, 128], BF16, stack=st4)
            Rt = [[P.sb([128, G32, 128], BF16, stack=st4) for r in range(2)] for d in range(2)]
            bM, bR = Buf(), Buf()
            sp.dma(out=M[:].rearrange("p g c -> p (g c)"), in_=MS.ap(), writes=[bM])
            for d in range(2):
                for r in range(2):
                    sp.dma(out=Rt[d][r][:].rearrange("p g c -> p (g c)"),
                           in_=RS.ap()[:, (2 * d + r) * G32 * 128:(2 * d + r + 1) * G32 * 128], writes=[bR])
            p1r = Ring(P, 2, [128, 512], F32, st4, psum=True)
            p2r = [Ring(P, 1, [128, 512], F32, st4, psum=True) for _ in range(2)]
            pTr = Ring(P, 2, [128, 4, 128], F32, st4, psum=True)
            c1r = Ring(P, 2, [128, 512], F32, st4)
            ygr = Ring(P, 2, [128, 512], F32, st4)
            for kb in range(NKL // 128):
                ks = slice(kb * 128, (kb + 1) * 128)
                for hf in range(2):
                    r0 = 64 * hf
                    for q in range(G32 // 4):
                        P1, bP1 = p1r.next()
                        P2, bP2 = p2r[hf].next()
                        for gi in range(4):
                            g32 = 4 * q + gi
                            g = hf * G32 + g32
                            cs = slice(gi * 128, (gi + 1) * 128)
                            pe.op(mm_fn(P1[:, cs], M[:, g, :], U[:, g, ks], True, True), [bM, bU], [bP1])
                            seq = [(Rt[0][0], hist[0], 0), (Rt[0][1], hist[0], 1), (Rt[1][0], hist[1], 0),
                                   (Rt[1][1], hist[1], 1)]
                            for n, (Rm, hs, ri) in enumerate(seq):
                                pe.op(mm_fn(P2[:, cs], Rm[r0:r0 + 64, g32, :], hs[r0:r0 + 64, ri, g32, ks],
                                            n == 0, n == 3), [bR, bhist], [bP2])
                        c1, bc1 = c1r.next()
                        act.op(act_fn(c1[:], P1[:], AF.Copy), [bP1], [bc1])
                        yg, byg = ygr.next()
                        dve.op(tt_fn(yg[:], P2[:], c1[:], ALU.add), [bP2, bc1], [byg])
                        pT, bpT = pTr.next()
                        for gi in range(4):
                            pe.op(lambda e, pT=pT, gi=gi, yg=yg: e.transpose(pT[:, gi, :], yg[:, gi * 128:(gi + 1) * 128],
                                                                             P.identf), [byg, P.bcst], [bpT])
                        c0 = 16 * (4 * q)
                        zdst = Z[:, :, c0:c0 + 64].rearrange("p t (g c) -> p t g c", g=4)
                        zsrc = pT[:].rearrange("p g (t c) -> p t g c", t=8)
                        act.op(act_fn(zdst, zsrc, AF.Copy), [bpT], [bZ])
                    dst = ZS[kb * 1024:(kb + 1) * 1024, hf * 512:(hf + 1) * 512].rearrange("(p t) d -> p t d", t=8)
                    sp.dma(out=dst, in_=Z[:], reads=[bZ])


def build_fused(ncores=NCORES):
    P = Prog()
    P.ncores = ncores
    xw = P.inp("xw", [WIN, D])
    g0 = P.inp("g0", [1, D])
    w_in = P.inp("w_in", [D, 2560])
    cvec = P.inp("cvec", [128, 4, 34])
    qk_g = P.inp("qk_g", [128, 2])
    w_out = P.inp("w_out", [D, D])
    eb = P.inp("eb", [4, 128, 6, 384])
    cst_in = P.inp("cst", [128, 4, 128])
    gf = P.inp("gf", [2, D])
    w_r = P.inp("w_r", [2, D, 16])
    b_r = P.inp("b_r", [2, 16])
    wg = P.inp("wg", [2, NEXP, D, D])
    wu = P.inp("wu", [2, NEXP, D, D])
    wd = P.inp("wd", [2, NEXP, D, D])
    gn = P.inp("gn", [1, D])
    s5p = P.inp("s5p", [128, 2, 3, G32])
    s5B = P.inp("s5B", [128, 2, 2, G32, 16])
    s5C = P.inp("s5C", [128, 2, 2, G32, 16])
    dvec = P.inp("dvec", [1, D])
    masks = P.inp("masks", [128, 2, 128])
    flags = P.inp("flags", [128, 2])
    w_glu = P.inp("w_glu", [D, 2 * D])
    out = P.outp("out", [OWN, D])
    X1 = P.dram("X1", [OWN, D])
    X2 = P.dram("X2", [OWN, D])
    X3 = P.dram("X3", [OWN, D])
    HN = P.dram("HN", [OWN, D])
    ZS = P.dram("ZS", [OWN, D])
    AFF = P.dram("AFF", [OWN, NEXP])
    ATo = P.dram("ATo", [NEXP, OWN])
    ATall = P.dram("ATall", [2 * NEXP, OWN])
    SAo = P.dram("SAo", [128, 2 * G32])
    SAall = P.dram("SAall", [256, 2 * G32])
    MS = P.dram("MS", [128, 64 * 128], BF16)
    RS = P.dram("RS", [128, 4 * G32 * 128], BF16)
    load_consts(P, cst_in)
    emit_phaseA(P, dict(xw=xw, g0=g0, w_in=w_in, cvec=cvec, qk_g=qk_g, w_out=w_out, gf=gf[0:1, :], w_r=w_r[0],
                        b_r=b_r[0:1, :], eb=eb, x1_out=X1.ap(), aff_out=AFF.ap(), affT_out=ATo.ap()))
    P.all_gather_pair(ATo, ATall)
    at_view = ATall.ap().rearrange("(r e) t -> e r t", r=2)
    emit_phaseB(P, dict(x1=X1.ap(), affT_seq=at_view, aff_own=AFF.ap(), gf=gf[0:1, :], wg=wg[0], wu=wu[0], wd=wd[0],
                        x2_out=X2.ap(), gn=gn, hn_out=HN.ap()))
    emit_phaseC2(P, dict(s5p=s5p, s5B=s5B, s5C=s5C, masks=masks, flags=flags, hn=HN.ap(), zs_out=ZS.ap(),
                         sa_own=SAo, sa_all=SAall, ms=MS, rs=RS))
    emit_phaseD(P, dict(zs=ZS.ap(), hn=HN.ap(), dvec=dvec, x2=X2.ap(), w_glu=w_glu, gf=gf[1:2, :], w_r=w_r[1],
                        b_r=b_r[1:2, :], x3_out=X3.ap(), aff_out=AFF.ap(), affT_out=ATo.ap()))
    P.all_gather_pair(ATo, ATall)
    emit_phaseB(P, dict(x1=X3.ap(), affT_seq=at_view, aff_own=AFF.ap(), gf=gf[1:2, :], wg=wg[1], wu=wu[1], wd=wd[1],
                        x2_out=out))
    return P.finish()


def fused_inputs(c, x, mix_norm_even, w_in, conv_w, conv_b, conv_ln_g, conv_ln_b, q_norm, k_norm,
                 w_out, mix_norm_odd, ssm_lam_re, ssm_lam_im, ssm_log_dt, ssm_b_re, ssm_b_im,
                 ssm_c_re, ssm_c_im, ssm_d, w_glu, ffn_norm, w_router, b_router,
                 w_e_gate, w_e_up, w_e_down, shared):
    b, h = c // 2, c % 2
    cw = conv_w[0] if h == 0 else conv_w[0][::-1]
    cvec = np.zeros((128, 4, 34), np.float32)
    cvec[:, :, 0:31] = cw.T.reshape(4, 128, 31).transpose(1, 0, 2)
    cvec[:, :, 31] = conv_b[0].reshape(4, 128).T
    cvec[:, :, 32] = conv_ln_g[0].reshape(4, 128).T
    cvec[:, :, 33] = conv_ln_b[0].reshape(4, 128).T
    qk = np.stack([np.tile(q_norm[0], 2), np.tile(k_norm[0], 2)], axis=1).astype(np.float32)
    order = [0, 1] if h == 0 else [1, 0]

    def gp(a):
        a = a[order]
        return a.reshape(2, 2, G32, 64).transpose(1, 3, 0, 2).reshape(128, 2, G32)

    ldt = np.broadcast_to(ssm_log_dt[0][:, :, None], (2, 64, 64))
    s5p = np.ascontiguousarray(np.stack([gp(ssm_lam_re[0]), gp(ssm_lam_im[0]), gp(ldt)], axis=2))

    def gB(a):
        a = a[order]
        return a.reshape(2, 2, G32, 64, 16).transpose(1, 3, 0, 2, 4).reshape(128, 2, G32, 16)

    def gC(a):
        a = a[order]
        return a.reshape(2, 2, G32, 16, 64).transpose(1, 4, 0, 2, 3).reshape(128, 2, G32, 16)

    s5B = np.ascontiguousarray(np.stack([gB(ssm_b_re[0]), gB(ssm_b_im[0])], axis=2))
    s5C = np.ascontiguousarray(np.stack([gC(ssm_c_re[0]), gC(ssm_c_im[0])], axis=2))
    flags = np.zeros((128, 2), np.float32)
    flags[:, 1 - h] = 1.0
    m = dict(shared)
    m.update({
        "xw": local_view(x[b], h, WIN), "cvec": cvec, "qk_g": np.ascontiguousarray(qk),
        "s5p": s5p, "s5B": s5B, "s5C": s5C, "flags": flags,
    })
    return m


def kernel(x, mix_norm_even, w_in, conv_w, conv_b, conv_ln_g, conv_ln_b, q_norm, k_norm,
           w_out, mix_norm_odd, ssm_lam_re, ssm_lam_im, ssm_log_dt, ssm_b_re, ssm_b_im,
           ssm_c_re, ssm_c_im, ssm_d, w_glu, ffn_norm, w_router, b_router,
           w_e_gate, w_e_up, w_e_down):
    f = lambda a: np.ascontiguousarray(np.asarray(a, dtype=np.float32))
    args = [f(a) for a in (x, mix_norm_even, w_in, conv_w, conv_b, conv_ln_g, conv_ln_b, q_norm, k_norm,
                           w_out, mix_norm_odd, ssm_lam_re, ssm_lam_im, ssm_log_dt, ssm_b_re, ssm_b_im,
                           ssm_c_re, ssm_c_im, ssm_d, w_glu, ffn_norm, w_router, b_router,
                           w_e_gate, w_e_up, w_e_down)]
    (x, mix_norm_even, w_in, conv_w, conv_b, conv_ln_g, conv_ln_b, q_norm, k_norm,
     w_out, mix_norm_odd, ssm_lam_re, ssm_lam_im, ssm_log_dt, ssm_b_re, ssm_b_im,
     ssm_c_re, ssm_c_im, ssm_d, w_glu, ffn_norm, w_router, b_router, w_e_gate, w_e_up, w_e_down) = args
    shared = {
        "g0": np.ascontiguousarray(mix_norm_even[0][None, :]), "w_in": w_in[0], "w_out": w_out[0],
        "eb": host_eb(), "cst": host_consts(), "gf": ffn_norm, "w_r": w_router, "b_r": b_router,
        "wg": w_e_gate, "wu": w_e_up, "wd": w_e_down, "gn": np.ascontiguousarray(mix_norm_odd[0][None, :]),
        "dvec": np.ascontiguousarray(ssm_d[0][None, :]), "masks": host_masks(), "w_glu": w_glu[0],
    }
    nc = get_prog("F", build_fused)
    in_maps = [fused_inputs(c, *args, shared) for c in range(NCORES)]
    res = run_bass_kernel_spmd(nc, in_maps, core_ids=list(range(NCORES)))
    return from_local([r["out"] for r in res.results]).astype(np.float32)
```

```python
import numpy as np
import contextlib
import concourse.bass as bass
import concourse.mybir as mybir
from concourse.bass_utils import run_bass_kernel_spmd

F32 = mybir.dt.float32
BF16 = mybir.dt.bfloat16
ALU = mybir.AluOpType
AF = mybir.ActivationFunctionType
AX = mybir.AxisListType

NCORES = 8
D = 1024
SEQ = 4096
OWN = 2048
WIN = 3072
EPS = 1e-6
DEBUG = False
SPARSE = True


class Buf:
    __slots__ = ("name", "w", "r")

    def __init__(self, name=""):
        self.name = name
        self.w = None
        self.r = {}


class Stream:
    def __init__(self, P, name, eng):
        self.P = P
        self.name = name
        self.eng = eng
        self.sem = P.newsem("c_" + name)
        self.cnt = 0
        self.seen = {}
        self.nslots = 8
        self.dsems = None
        self.dn = 0
        self.selfsync = name != "pe"

    def _wait(self, tok):
        key, sem, val = tok
        if self.seen.get(key, 0) < val:
            self.eng.wait_ge(sem, val)
            self.seen[key] = val

    def _deps(self, reads, writes, dma):
        toks = []
        for b in reads:
            if b.w is not None:
                toks.append(b.w)
        for b in writes:
            if b.w is not None:
                toks.append(b.w)
            toks.extend(b.r.values())
        for t in toks:
            if dma or t[0] is not self or self.selfsync:
                self._wait(t)

    def _mark(self, tok, reads, writes):
        for b in reads:
            b.r[tok[0]] = tok
        for b in writes:
            b.w = tok
            b.r = {}

    def op(self, fn, reads=(), writes=()):
        if self.P.stopped:
            return None
        self._deps(reads, writes, False)
        ins = fn(self.eng)
        self.cnt += 1
        ins.then_inc(self.sem, 1)
        self._mark((self, self.sem, self.cnt), reads, writes)
        return ins

    def dma(self, out, in_, reads=(), writes=(), **kw):
        if self.P.stopped:
            return None
        if self.dsems is None:
            self.dsems = [self.P.newsem("d%d_%s" % (i, self.name)) for i in range(self.nslots)]
        j = self.dn
        slot = j % self.nslots
        key = (self, slot)
        prev = 16 * (j // self.nslots)
        if prev > 0:
            self._wait((key, self.dsems[slot], prev))
        self._deps(reads, writes, True)
        ins = self.eng.dma_start(out=out, in_=in_, **kw)
        ins.then_inc(self.dsems[slot], 16)
        self.dn += 1
        self._mark((key, self.dsems[slot], prev + 16), reads, writes)
        return ins


class Prog:
    def __init__(self):
        self.nc = bass.Bass("TRN2", target_bir_lowering=False)
        self.stack = contextlib.ExitStack()
        self._n = 0
        self.stopped = False
        self.cur = None
        self.ccsem = None
        self.ccn = 0
        self.ncores = NCORES
        self.bcreg = None
        nc = self.nc
        self.pe = Stream(self, "pe", nc.tensor)
        self.act = Stream(self, "act", nc.scalar)
        self.dve = Stream(self, "dve", nc.vector)
        self.pool = Stream(self, "pool", nc.gpsimd)
        self.sp = Stream(self, "sp", nc.sync)
        self.streams = [self.pe, self.act, self.dve, self.pool, self.sp]

    def newsem(self, name):
        return self.stack.enter_context(self.nc.semaphore(name))

    def inp(self, name, shape, dt=F32):
        return self.nc.dram_tensor(name, list(shape), dt, kind="ExternalInput").ap()

    def outp(self, name, shape, dt=F32):
        return self.nc.dram_tensor(name, list(shape), dt, kind="ExternalOutput").ap()

    def sb(self, shape, dt=F32, name=None, stack=None):
        self._n += 1
        name = "s%d_%s" % (self._n, name or "t")
        return (stack or self.cur or self.stack).enter_context(self.nc.sbuf_tensor(name, list(shape), dt))

    def ps(self, shape, dt=F32, name=None, stack=None):
        self._n += 1
        name = "p%d_%s" % (self._n, name or "t")
        return (stack or self.cur or self.stack).enter_context(self.nc.psum_tensor(name, list(shape), dt))

    def barrier(self):
        toks = []
        for s in self.streams:
            if s.cnt:
                toks.append((s, s.sem, s.cnt))
            if s.dsems is not None:
                for slot in range(s.nslots):
                    n = (s.dn - slot + s.nslots - 1) // s.nslots
                    if n > 0:
                        toks.append(((s, slot), s.dsems[slot], 16 * n))
        for s in self.streams:
            for t in toks:
                if t[0] is not s:
                    s._wait(t)

    def dram(self, name, shape, dt=F32):
        return self.nc.dram_tensor(name, list(shape), dt)

    def all_gather_pair(self, src, dst):
        if self.ccsem is None:
            self.ccsem = self.newsem("ccsem")
            self.ccdummy = self.sb([128, 1], F32, name="ccdummy", stack=self.stack)
            self.bccd = Buf()
        self.barrier()
        groups = [[2 * i, 2 * i + 1] for i in range(self.ncores // 2)]
        self.ccn += 1
        n = self.ccn

        def fn(e):
            e.collective_compute("AllGather", ALU.bypass, replica_groups=groups,
                                 ins=[src.ap().opt()], outs=[dst.ap().opt()]).then_inc(self.ccsem, 1)
            e.wait_ge(self.ccsem, n)
            return e.memset(self.ccdummy[:], 0.0)
        self.pool.op(fn, [], [self.bccd])
        self.barrier()

    def finish(self):
        self.barrier()
        self.stack.close()
        return self.nc


def bcast_rows(ap, nparts):
    n = ap.shape[-1]
    return bass.AP(ap.tensor, ap.offset, [[0, nparts], [1, n]])


class Ring:
    def __init__(self, P, n, shape, dt, stack, psum=False):
        alloc = P.ps if psum else P.sb
        self.t = [alloc(shape, dt, stack=stack) for _ in range(n)]
        self.b = [Buf() for _ in range(n)]
        self.i = -1

    def next(self):
        self.i = (self.i + 1) % len(self.t)
        return self.t[self.i], self.b[self.i]


def act_fn(out, in_, func, **kw):
    return lambda e: e.activation(out=out, in_=in_, func=func, **kw)


def mm_fn(out, lhsT, rhs, start, stop):
    return lambda e: e.matmul(out, lhsT, rhs, start=start, stop=stop)


def tt_fn(out, in0, in1, op):
    return lambda e: e.tensor_tensor(out=out, in0=in0, in1=in1, op=op)


def ts_fn(out, in0, s1, s2, op0, op1=None):
    if op1 is None:
        return lambda e: e.tensor_scalar(out=out, in0=in0, scalar1=s1, scalar2=None, op0=op0)
    return lambda e: e.tensor_scalar(out=out, in0=in0, scalar1=s1, scalar2=s2, op0=op0, op1=op1)


def stt_fn(out, in0, scalar, in1, op0, op1):
    return lambda e: e.scalar_tensor_tensor(out=out, in0=in0, scalar=scalar, in1=in1, op0=op0, op1=op1)


ATT_CFG = (1, 4, 16)


def vtile_list():
    tiles = []
    for ci, d in enumerate(ATT_CFG):
        nb = OWN // (128 * d)
        for c in range(d):
            for j in range(nb + 1):
                i0 = 0 if j == 0 else 128 * j - 64
                tiles.append((ci, d, c, j, c + d * i0))
    return tiles


def rms_rstd(P, x_ap, xb, n, ring_junk, ring_small, pbuf_extra=()):
    junk, bj = ring_junk.next()
    ss, bs = ring_small.next()
    P.act.op(act_fn(junk[:, 0:n], x_ap, AF.Square, accum_out=ss[:, 0:1]), [xb], [bj, bs])
    sd, bd = ring_small.next()
    P.act.op(act_fn(sd[:, 0:1], ss[:, 0:1], AF.Sqrt, scale=1.0 / n, bias=P.eps_t[:, 0:1]), [bs, P.beps], [bd])
    rs, br = ring_small.next()
    P.dve.op(lambda e: e.reciprocal(out=rs[:, 0:1], in_=sd[:, 0:1]), [bd], [br])
    return rs, br


def load_consts(P, cst_in):
    cst = P.sb([128, 6, 128], F32, name="cst")
    P.bcst = Buf("cst")
    P.sp.dma(out=cst[:], in_=cst_in, writes=[P.bcst])
    P.identf = cst[:, 0, :]
    P.ones512 = cst[:, 1, :]
    P.ones64 = cst[:, 2, :]
    P.cst = cst
    idb = P.sb([128, 128], BF16, name="identb")
    P.bidb = Buf("identb")
    P.dve.op(lambda e: e.tensor_copy(out=idb[:], in_=cst[:, 0, :]), [P.bcst], [P.bidb])
    P.identb = idb
    eps_t = P.sb([128, 1], F32, name="eps")
    P.beps = Buf("eps")
    P.dve.op(lambda e: e.memset(eps_t[:], EPS), [], [P.beps])
    P.eps_t = eps_t


def router_tile(P, x1t, bx1, gfb, bgfb, wr, bwr, brb, bbrb, rings, aff_out, affT_out, tt, hf_out=None):
    rs, br = rms_rstd(P, x1t[:], bx1, D, rings["junk"], rings["small"])
    hf, bhf = rings["hf"].next()
    P.dve.op(stt_fn(hf[:], x1t[:], rs[:, 0:1], gfb[:], ALU.mult, ALU.mult), [bx1, br, bgfb], [bhf])
    ptf, bptf = rings["ptf"].next()
    for k in range(8):
        P.pe.op(lambda e, k=k: e.transpose(ptf[:, k, :], hf[:, k * 128:(k + 1) * 128], P.identf),
                [bhf, P.bcst], [bptf])
    hfT, bhfT = rings["hfT"].next()
    P.act.op(act_fn(hfT[:], ptf[:], AF.Copy), [bptf], [bhfT])
    plog, bplog = rings["plog"].next()
    for k in range(8):
        P.pe.op(mm_fn(plog[:, 0:16], hfT[:, k, :], wr[:, k, :], k == 0, k == 7), [bhfT, bwr], [bplog])
    lg, blg = rings["sm16"].next()
    P.dve.op(tt_fn(lg[:], plog[:, 0:16], brb[:], ALU.add), [bplog, bbrb], [blg])
    mx, bmx = rings["small"].next()
    P.dve.op(lambda e: e.reduce_max(out=mx[:, 0:1], in_=lg[:], axis=AX.X, negate=True), [blg], [bmx])
    ex, bex = rings["sm16"].next()
    sm, bsm = rings["small"].next()
    P.act.op(act_fn(ex[:], lg[:], AF.Exp, bias=mx[:, 0:1], scale=1.0, accum_out=sm[:, 0:1]), [blg, bmx], [bex, bsm])
    rsm, brsm = rings["small"].next()
    P.dve.op(lambda e: e.reciprocal(out=rsm[:, 0:1], in_=sm[:, 0:1]), [bsm], [brsm])
    af, baf = rings["sm16"].next()
    P.dve.op(ts_fn(af[:], ex[:], rsm[:, 0:1], None, ALU.mult), [bex, brsm], [baf])
    P.sp.dma(out=aff_out[tt * 128:(tt + 1) * 128, :], in_=af[:], reads=[baf])
    pat, bpat = rings["pat"].next()
    P.pe.op(lambda e: e.transpose(pat[0:16, 0:128], af[:], P.identf), [baf, P.bcst], [bpat])
    aT, baT = rings["aT"].next()
    P.act.op(act_fn(aT[:], pat[0:16, 0:128], AF.Copy), [bpat], [baT])
    P.sp.dma(out=affT_out[:, tt * 128:(tt + 1) * 128], in_=aT[:], reads=[baT])
    return hf, bhf


def emit_phaseA(P, io):
    xw, g0, w_in, cvec, qk_g, w_out = io["xw"], io["g0"], io["w_in"], io["cvec"], io["qk_g"], io["w_out"]
    gf, w_r, b_r, eb = io["gf"], io["w_r"], io["b_r"], io["eb"]
    x1_out, aff_out, affT_out = io["x1_out"], io["aff_out"], io["affT_out"]
    dbg = None
    with contextlib.ExitStack() as ph:
        P.cur = ph
        _phaseA_body(P, xw, g0, w_in, cvec, qk_g, w_out, gf, w_r, b_r, eb, x1_out, aff_out, affT_out)
        P.barrier()
    P.cur = None


def _phaseA_body(P, xw, g0, w_in, cvec, qk_g, w_out, gf, w_r, b_r, eb, x1_out, aff_out, affT_out):
    w_in_v = w_in.rearrange("(k p) e -> p k e", p=128)
    w_out_v = w_out.rearrange("(k p) e -> p k e", p=128)
    w_r_v = w_r.rearrange("(k p) e -> p k e", p=128)

    hT = P.sb([128, 8, WIN], BF16, name="hT")
    bhT = [Buf("hT%d" % i) for i in range(WIN // 128)]
    mixT = P.sb([128, 8, OWN], BF16, name="mixT")
    bmix = [Buf("mix%d" % i) for i in range(8)]
    gb = P.sb([128, D], F32, name="gb")
    bgb = Buf("gb")
    P.sp.dma(out=gb[:], in_=bcast_rows(g0, 128), writes=[bgb])
    cv = P.sb([128, 4, 34], F32, name="cv")
    bcv = Buf("cv")
    P.sp.dma(out=cv[:], in_=cvec, writes=[bcv])
    qkg = P.sb([128, 2], F32, name="qkg")
    bqkg = Buf("qkg")
    P.sp.dma(out=qkg[:], in_=qk_g, writes=[bqkg])

    with contextlib.ExitStack() as st:
        xr = Ring(P, 2, [128, D], F32, st)
        junk = Ring(P, 1, [128, D], F32, st)
        small = Ring(P, 8, [128, 1], F32, st)
        hnr = Ring(P, 2, [128, D], BF16, st)
        ptr = Ring(P, 2, [128, 8, 128], BF16, st, psum=True)
        for tt in range(WIN // 128):
            xt, bx = xr.next()
            P.sp.dma(out=xt[:], in_=xw[tt * 128:(tt + 1) * 128, :], writes=[bx])
            rs, br = rms_rstd(P, xt[:], bx, D, junk, small)
            hn, bhn = hnr.next()
            P.dve.op(stt_fn(hn[:], xt[:], rs[:, 0:1], gb[:], ALU.mult, ALU.mult), [bx, br, bgb], [bhn])
            pt, bpt = ptr.next()
            for k in range(8):
                P.pe.op(lambda e, k=k: e.transpose(pt[:, k, :], hn[:, k * 128:(k + 1) * 128], P.identb[:]),
                        [bhn, P.bidb], [bpt])
            P.act.op(act_fn(hT[:, :, tt * 128:(tt + 1) * 128], pt[:], AF.Copy), [bpt], [bhT[tt]])
        P.barrier()

    def hT_bufs(t0, n):
        return bhT[t0 // 128:(t0 + n + 127) // 128]

    NCV = OWN + 128
    with contextlib.ExitStack() as st:
        hglu = P.sb([128, 4, 15 + NCV], BF16, stack=st)
        bhg = [Buf() for _ in range(4)]
        diag = P.sb([128, 4, 31, 128], BF16, stack=st)
        bdiag = [Buf() for _ in range(4)]
        wcv = Ring(P, 2, [128, 8, 256], BF16, st)
        pmr = Ring(P, 2, [128, 512], F32, st, psum=True)
        pgr = Ring(P, 2, [128, 512], F32, st, psum=True)
        pst = Ring(P, 2, [128, 512], F32, st, psum=True)
        sgr = Ring(P, 2, [128, 512], F32, st)
        vbuf = [Ring(P, 2, [128, 512], F32, st) for _ in range(4)]
        sqr = [Ring(P, 2, [128, 512], F32, st) for _ in range(4)]
        tmp = Ring(P, 10, [128, 512], F32, st)
        zr = Ring(P, 2, [128, 512], F32, st)
        for cc in range(4):
            P.dve.op(lambda e, cc=cc: e.memset(hglu[:, cc, 0:15], 0.0), [], [bhg[cc]])
            for k in range(31):
                P.pool.op(ts_fn(diag[:, cc, k, :], P.identf, cv[:, cc, k:k + 1], None, ALU.mult),
                          [P.bcst, bcv], [bdiag[cc]])
        ntiles = [(i * 512, 512) for i in range(4)] + [(2048, 128)]
        for cc in range(4):
            w, bw = wcv.next()
            P.pool.dma(out=w[:, :, 0:128], in_=w_in_v[:, :, cc * 128:(cc + 1) * 128], writes=[bw])
            P.pool.dma(out=w[:, :, 128:256], in_=w_in_v[:, :, 512 + cc * 128:512 + (cc + 1) * 128], writes=[bw])
            for (t0, n) in ntiles:
                pm, bpm = pmr.next()
                pg, bpg = pgr.next()
                for k in range(8):
                    P.pe.op(mm_fn(pm[:, 0:n], w[:, k, 0:128], hT[:, k, t0:t0 + n], k == 0, k == 7),
                            [bw] + hT_bufs(t0, n), [bpm])
                for k in range(8):
                    P.pe.op(mm_fn(pg[:, 0:n], w[:, k, 128:256], hT[:, k, t0:t0 + n], k == 0, k == 7),
                            [bw] + hT_bufs(t0, n), [bpg])
                sg, bsg = sgr.next()
                P.act.op(act_fn(sg[:, 0:n], pg[:, 0:n], AF.Sigmoid), [bpg], [bsg])
                P.dve.op(tt_fn(hglu[:, cc, 15 + t0:15 + t0 + n], pm[:, 0:n], sg[:, 0:n], ALU.mult),
                         [bpm, bsg], [bhg[cc]])
        for nt in range(4):
            t0 = nt * 512
            vb = []
            for cc in range(4):
                pm, bpm = pmr.next()
                for k in range(31):
                    P.pe.op(mm_fn(pm[:], diag[:, cc, k, :], hglu[:, cc, t0 + k:t0 + k + 512], k == 0, k == 30),
                            [bdiag[cc], bhg[cc]], [bpm])
                v, bv = vbuf[cc].next()
                sq, bsq = sqr[cc].next()
                P.act.op(act_fn(v[:], pm[:], AF.Identity, bias=cv[:, cc, 31:32], scale=1.0), [bpm, bcv], [bv])
                P.act.op(act_fn(sq[:], pm[:], AF.Square, bias=cv[:, cc, 31:32], scale=1.0), [bpm, bcv], [bsq])
                vb.append((v, bv, sq, bsq))
            pmean, bpmean = pst.next()
            pex2, bpex2 = pst.next()
            for cc in range(4):
                P.pe.op(mm_fn(pmean[:], P.ones512, vb[cc][0][:], cc == 0, cc == 3), [P.bcst, vb[cc][1]], [bpmean])
            for cc in range(4):
                P.pe.op(mm_fn(pex2[:], P.ones512, vb[cc][2][:], cc == 0, cc == 3), [P.bcst, vb[cc][3]], [bpex2])
            mean, bmean = tmp.next()
            P.act.op(act_fn(mean[:], pmean[:], AF.Copy), [bpmean], [bmean])
            m2, bm2 = tmp.next()
            P.dve.op(tt_fn(m2[:], mean[:], mean[:], ALU.mult), [bmean], [bm2])
            var, bvar = tmp.next()
            P.dve.op(tt_fn(var[:], pex2[:], m2[:], ALU.subtract), [bpex2, bm2], [bvar])
            sd, bsd = tmp.next()
            P.act.op(act_fn(sd[:], var[:], AF.Sqrt, bias=P.eps_t[:, 0:1], scale=1.0), [bvar, P.beps], [bsd])
            rstd, brstd = tmp.next()
            P.dve.op(lambda e: e.reciprocal(out=rstd[:], in_=sd[:]), [bsd], [brstd])
            for cc in range(4):
                v, bv, sq, bsq = vb[cc]
                z, bz = zr.next()
                P.dve.op(tt_fn(z[:], v[:], mean[:], ALU.subtract), [bv, bmean], [bz])
                P.dve.op(tt_fn(z[:], z[:], rstd[:], ALU.mult), [bz, brstd], [bz])
                P.act.op(act_fn(mixT[:, cc, t0:t0 + 512], z[:], AF.Silu, scale=cv[:, cc, 32:33],
                                bias=cv[:, cc, 33:34]), [bz, bcv], [bmix[cc]])
        P.barrier()

    tiles = vtile_list()
    NT = len(tiles)
    tindex = {(ci, c, j): i for i, (ci, d, c, j, s) in enumerate(tiles)}
    with contextlib.ExitStack() as st:
        wq = Ring(P, 2, [128, 8, 384], BF16, st)
        ebr = Ring(P, 2, [128, 6, 384], F32, st)
        qT = P.sb([128, OWN], BF16, stack=st)
        bqT = Buf()
        kT = P.sb([128, WIN], BF16, stack=st)
        bkT = Buf()
        Vt = P.sb([128, NT, 256], BF16, stack=st)
        bVt = [Buf() for _ in range(NT)]
        bones = Buf()
        acc = P.sb([128, 2, OWN], F32, stack=st)
        bacc = [Buf(), Buf()]
        pmr = Ring(P, 2, [128, 512], F32, st, psum=True)
        pst = Ring(P, 1, [128, 512], F32, st, psum=True)
        psr = Ring(P, 3, [128, 512], F32, st, psum=True)
        por = Ring(P, 2, [128, 512], F32, st, psum=True)
        qfr = Ring(P, 2, [128, 512], F32, st)
        sqr = Ring(P, 2, [128, 512], F32, st)
        tmp = Ring(P, 4, [128, 512], F32, st)
        pex = Ring(P, 3, [128, 256], F32, st)
        pTr = Ring(P, 4, [128, 256], BF16, st)
        rzr = Ring(P, 1, [64, OWN], F32, st)
        Vt4 = Vt[:].rearrange("p t (s c) -> p t s c", s=4)
        P.pool.op(lambda e: e.memset(Vt4[:, :, 1, :], 1.0), [], [bones])
        P.pool.op(lambda e: e.memset(Vt4[:, :, 3, :], 1.0), [], [bones])
        ps_slot = [0]
        po_slot = [0]
        pv_slot = [0]
        psb = [[Buf(), Buf()], [Buf(), Buf()]]
        pob = [[Buf() for _ in range(4)] for _ in range(2)]
        pvb = [Buf() for _ in range(4)]
        for hp in range(4):
            w, bw = wq.next()
            for i, base in enumerate((1024, 1536, 2048)):
                P.pool.dma(out=w[:, :, i * 128:(i + 1) * 128],
                           in_=w_in_v[:, :, base + hp * 128:base + (hp + 1) * 128], writes=[bw])
            ebt, bebt = ebr.next()
            P.sp.dma(out=ebt[:], in_=eb[hp], writes=[bebt])
            for which, ntok, dst, bdst, gcol in ((0, OWN, qT, bqT, 0), (1, WIN, kT, bkT, 1)):
                for nt in range(ntok // 512):
                    t0 = nt * 512
                    pm, bpm = pmr.next()
                    for k in range(8):
                        P.pe.op(mm_fn(pm[:], w[:, k, which * 128:(which + 1) * 128], hT[:, k, t0:t0 + 512],
                                      k == 0, k == 7), [bw] + hT_bufs(t0, 512), [bpm])
                    qf, bqf = qfr.next()
                    sq, bsq = sqr.next()
                    P.act.op(act_fn(qf[:], pm[:], AF.Copy), [bpm], [bqf])
                    P.act.op(act_fn(sq[:], pm[:], AF.Square), [bpm], [bsq])
                    pms, bpms = pst.next()
                    P.pe.op(mm_fn(pms[:], P.ones64, sq[:], True, True), [P.bcst, bsq], [bpms])
                    sd, bsd = tmp.next()
                    P.act.op(act_fn(sd[:], pms[:], AF.Sqrt, bias=P.eps_t[:, 0:1], scale=1.0), [bpms, P.beps], [bsd])
                    rstd, brstd = tmp.next()
                    P.dve.op(lambda e, rstd=rstd, sd=sd: e.reciprocal(out=rstd[:], in_=sd[:]), [bsd], [brstd])
                    P.dve.op(tt_fn(qf[:], qf[:], rstd[:], ALU.mult), [bqf, brstd], [bqf])
                    P.dve.op(ts_fn(dst[:, t0:t0 + 512], qf[:], qkg[:, gcol:gcol + 1],
                                   0.125 if which == 0 else 1.0, ALU.mult, ALU.mult), [bqf, bqkg], [bdst])
            for ti, (ci, d, c, j, s0) in enumerate(tiles):
                pv, bpv = pmr.next()
                for k in range(8):
                    P.pe.op(mm_fn(pv[:, 0:128], hT[:, k, s0:s0 + 127 * d + 1:d], w[:, k, 256:384], k == 0, k == 7),
                            [bw] + bhT, [bpv])
                P.act.op(act_fn(Vt4[:, ti, 0:4:2, :], pv[:, 0:128].rearrange("p (s c) -> p s c", s=2), AF.Copy),
                         [bpv, bones], [bVt[ti]])
            for hh in range(2):
                r0 = 64 * hh
                for ci, d in enumerate(ATT_CFG):
                    nb = OWN // (128 * d)
                    for c in range(d):
                        pts = []
                        for j in range(nb + 1):
                            i0 = 0 if j == 0 else 128 * j - 64
                            ks = c + d * i0
                            jlo, jhi = max(j - 1, 0), min(j, nb - 1)
                            nq = 128 * (jhi - jlo + 1)
                            q0 = c + d * 128 * jlo
                            pst_, pbuf = psr.next()
                            sl = pst_[:, 0:nq]
                            P.pe.op(mm_fn(sl, kT[r0:r0 + 64, ks:ks + 127 * d + 1:d],
                                          qT[r0:r0 + 64, q0:q0 + (nq - 1) * d + 1:d], True, True), [bkT, bqT], [pbuf])
                            pe_, bpe = pex.next()
                            P.act.op(act_fn(pe_[:, 0:nq], sl, AF.Exp), [pbuf], [bpe])
                            if j == 0:
                                ebs = ebt[:, ci * 2 + hh, 256:384]
                            elif j == nb:
                                ebs = ebt[:, ci * 2 + hh, 0:128]
                            else:
                                ebs = ebt[:, ci * 2 + hh, 0:256]
                            pT, bpT = pTr.next()
                            P.dve.op(tt_fn(pT[:, 0:nq], pe_[:, 0:nq], ebs, ALU.mult), [bpe, bebt], [bpT])
                            pts.append((pT, bpT, nq))
                            if j >= 1:
                                jb = j - 1
                                pTa, bpTa, nqa = pts[jb]
                                a_off = 0 if jb == 0 else 128
                                po_, pobuf = por.next()
                                osl = po_[:, 0:128]
                                ta = tindex[(ci, c, jb)]
                                tb = tindex[(ci, c, j)]
                                P.pe.op(mm_fn(osl, Vt[:, ta, 128 * hh:128 * hh + 128], pTa[:, a_off:a_off + 128],
                                              True, False), [bVt[ta], bpTa], [pobuf])
                                P.pe.op(mm_fn(osl, Vt[:, tb, 128 * hh:128 * hh + 128], pT[:, 0:128],
                                              False, True), [bVt[tb], bpT], [pobuf])
                                a0 = c + d * 128 * jb
                                dsl = acc[:, hh, a0:a0 + 127 * d + 1:d]
                                if ci == 0:
                                    P.dve.op(lambda e, dsl=dsl, osl=osl: e.tensor_copy(out=dsl, in_=osl),
                                             [pobuf], [bacc[hh]])
                                else:
                                    P.dve.op(tt_fn(dsl, dsl, osl, ALU.add), [pobuf, bacc[hh]], [bacc[hh]])
                rz, brz = rzr.next()
                P.dve.op(lambda e, rz=rz, hh=hh: e.reciprocal(out=rz[:], in_=acc[64:128, hh, :]), [bacc[hh]], [brz])
                P.dve.op(tt_fn(mixT[r0:r0 + 64, 4 + hp, :], acc[0:64, hh, :], rz[:], ALU.mult),
                         [bacc[hh], brz], [bmix[4 + hp]])
        P.barrier()

    with contextlib.ExitStack() as st:
        wo = P.sb([128, 8, D], BF16, stack=st)
        bwo = Buf()
        for k in range(8):
            P.pool.dma(out=wo[:, k, :], in_=w_out_v[:, k, :], writes=[bwo])
        gfb = P.sb([128, D], F32, stack=st)
        bgfb = Buf()
        P.sp.dma(out=gfb[:], in_=bcast_rows(gf, 128), writes=[bgfb])
        wr = P.sb([128, 8, 16], F32, stack=st)
        bwr = Buf()
        P.sp.dma(out=wr[:], in_=w_r_v, writes=[bwr])
        brb = P.sb([128, 16], F32, stack=st)
        bbrb = Buf()
        P.sp.dma(out=brb[:], in_=bcast_rows(b_r, 128), writes=[bbrb])
        xr = Ring(P, 2, [128, D], F32, st)
        x1r = Ring(P, 2, [128, D], F32, st)
        pmr = Ring(P, 2, [128, 512], F32, st, psum=True)
        rings = {
            "junk": Ring(P, 1, [128, D], F32, st),
            "small": Ring(P, 12, [128, 1], F32, st),
            "hf": Ring(P, 2, [128, D], F32, st),
            "ptf": Ring(P, 1, [128, 8, 128], F32, st, psum=True),
            "hfT": Ring(P, 2, [128, 8, 128], F32, st),
            "plog": Ring(P, 2, [128, 512], F32, st, psum=True),
            "sm16": Ring(P, 6, [128, 16], F32, st),
            "pat": Ring(P, 1, [128, 512], F32, st, psum=True),
            "aT": Ring(P, 2, [16, 128], F32, st),
        }
        for tt in range(OWN // 128):
            xt, bx = xr.next()
            P.sp.dma(out=xt[:], in_=xw[tt * 128:(tt + 1) * 128, :], writes=[bx])
            x1t, bx1 = x1r.next()
            for half in range(2):
                pm, bpm = pmr.next()
                for k in range(8):
                    P.pe.op(mm_fn(pm[:], mixT[:, k, tt * 128:(tt + 1) * 128], wo[:, k, half * 512:(half + 1) * 512],
                                  k == 0, k == 7), [bmix[k], bwo], [bpm])
                P.dve.op(tt_fn(x1t[:, half * 512:(half + 1) * 512], pm[:], xt[:, half * 512:(half + 1) * 512],
                               ALU.add), [bpm, bx], [bx1])
            P.sp.dma(out=x1_out[tt * 128:(tt + 1) * 128, :], in_=x1t[:], reads=[bx1])
            router_tile(P, x1t, bx1, gfb, bgfb, wr, bwr, brb, bbrb, rings, aff_out, affT_out, tt)
    return


def host_consts():
    cst = np.zeros((128, 6, 128), np.float32)
    cst[:, 0, :] = np.eye(128, dtype=np.float32)
    cst[:, 1, :] = 1.0 / 512.0
    blk = np.zeros((128, 128), np.float32)
    blk[:64, :64] = 1.0 / 64.0
    blk[64:, 64:] = 1.0 / 64.0
    cst[:, 2, :] = blk
    cst[:, 3, :] = 1.0
    cst[:, 4, :] = np.triu(np.ones((128, 128), np.float32), 1)
    cst[:, 5, 0:16] = 512.0 * np.arange(16, dtype=np.float32)[None, :]
    return cst


def host_eb():
    p = np.arange(128)[:, None].astype(np.float64)
    f = np.arange(128)[None, :].astype(np.float64)
    eb = np.zeros((4, 128, 6, 384), np.float32)
    for head in range(8):
        slope = 2.0 ** (-(head + 1))
        hp, hh = head // 2, head % 2
        for ci, d in enumerate(ATT_CFG):
            B = np.where(p <= f, np.exp(-slope * d * np.abs(p + 64 - f)), 0.0)
            A = np.where(p >= f, np.exp(-slope * d * np.abs(p - 64 - f)), 0.0)
            A0 = np.where((p <= 63) & (p >= f - 64), np.exp(-slope * d * np.abs(p - f)), 0.0)
            eb[hp, :, ci * 2 + hh, 0:128] = B
            eb[hp, :, ci * 2 + hh, 128:256] = A
            eb[hp, :, ci * 2 + hh, 256:384] = A0
    return eb


def local_view(xb, h, n):
    if h == 0:
        return np.ascontiguousarray(xb[:n])
    return np.ascontiguousarray(xb[::-1][:n])


_CACHE = {}


def get_prog(name, builder):
    if name not in _CACHE:
        _CACHE[name] = builder()
    return _CACHE[name]


def run_phaseA(x, mix_norm_even, w_in, conv_w, conv_b, conv_ln_g, conv_ln_b, q_norm, k_norm, w_out,
               ffn_norm0, w_router0, b_router0):
    nc = get_prog("A", build_phaseA)
    cst = host_consts()
    eb = host_eb()
    in_maps = []
    for c in range(NCORES):
        b, h = c // 2, c % 2
        cw = conv_w[0] if h == 0 else conv_w[0][::-1]
        cvec = np.zeros((128, 4, 34), np.float32)
        cvec[:, :, 0:31] = cw.T.reshape(4, 128, 31).transpose(1, 0, 2)
        cvec[:, :, 31] = conv_b[0].reshape(4, 128).T
        cvec[:, :, 32] = conv_ln_g[0].reshape(4, 128).T
        cvec[:, :, 33] = conv_ln_b[0].reshape(4, 128).T
        qk = np.stack([np.tile(q_norm[0], 2), np.tile(k_norm[0], 2)], axis=1).astype(np.float32)
        in_maps.append({
            "xw": local_view(x[b], h, WIN),
            "g0": np.ascontiguousarray(mix_norm_even[0][None, :]),
            "w_in": np.ascontiguousarray(w_in[0]),
            "cvec": cvec,
            "qk_g": np.ascontiguousarray(qk),
            "w_out": np.ascontiguousarray(w_out[0]),
            "gf": np.ascontiguousarray(ffn_norm0[None, :]),
            "w_r": np.ascontiguousarray(w_router0),
            "b_r": np.ascontiguousarray(b_router0[None, :]),
            "eb": eb,
            "cst": cst,
        })
    res = run_bass_kernel_spmd(nc, in_maps, core_ids=list(range(NCORES)))
    return res.results


NEXP = 16
CAP = 512


def emit_phaseB(P, io):
    with contextlib.ExitStack() as ph:
        P.cur = ph
        _phaseB_body(P, io["x1"], io["affT_seq"], io["aff_own"], io["gf"], io["wg"], io["wu"], io["wd"],
                     io["x2_out"], io.get("gn"), io.get("hn_out"))
        P.barrier()
    P.cur = None


def _phaseB_body(P, x1, affT_seq, aff_own, gf, wg, wu, wd, x2_out, gn, hn_out):
    if gn is not None:
        gnb = P.sb([128, D], F32, name="gnb")
        bgnb = Buf()
        P.sp.dma(out=gnb[:], in_=bcast_rows(gn, 128), writes=[bgnb])
    gfb = P.sb([128, D], F32, name="gfb")
    bgfb = Buf()
    P.sp.dma(out=gfb[:], in_=bcast_rows(gf, 128), writes=[bgfb])
    gate = P.sb([128, OWN // 128, NEXP], F32, name="gate")
    bgate = Buf()
    pthr = P.ps([128, 512], F32, name="pthr")
    bpthr = Buf()

    with contextlib.ExitStack() as st:
        aT = P.sb([NEXP, SEQ], F32, stack=st)
        baT = Buf()
        P.sp.dma(out=aT[:].rearrange("e (r t) -> e r t", r=2), in_=affT_seq, writes=[baT])
        junk = P.sb([NEXP, SEQ], F32, stack=st)
        bjunk = Buf()
        lo = P.sb([NEXP, 1], F32, stack=st)
        blo = Buf()
        mid = P.sb([NEXP, 1], F32, stack=st)
        bmid = Buf()
        cnt = P.sb([NEXP, 1], F32, stack=st)
        bcnt = Buf()
        ge = P.sb([NEXP, 1], F32, stack=st)
        bge = Buf()
        P.dve.op(lambda e: e.memset(lo[:], 0.0), [], [blo])
        for it in range(36):
            wk = 2.0 ** (-(it + 1))
            P.dve.op(ts_fn(mid[:], lo[:], wk, None, ALU.add), [blo], [bmid])
            P.dve.op(lambda e: e.tensor_scalar(out=junk[:], in0=aT[:], scalar1=mid[:, 0:1], scalar2=0.0,
                                               op0=ALU.is_ge, op1=ALU.add, accum_out=cnt[:, 0:1]),
                     [baT, bmid], [bjunk, bcnt])
            P.dve.op(ts_fn(ge[:], cnt[:], CAP - 0.5, None, ALU.is_ge), [bcnt], [bge])
            P.dve.op(stt_fn(lo[:], ge[:], wk, lo[:], ALU.mult, ALU.add), [bge, blo], [blo])
        dthr = P.sb([NEXP, NEXP], F32, stack=st)
        bdthr = Buf()
        P.dve.op(ts_fn(dthr[:], P.cst[0:NEXP, 0, 0:NEXP], lo[:, 0:1], None, ALU.mult), [P.bcst, blo], [bdthr])
        P.pe.op(mm_fn(pthr[:, 0:NEXP], P.cst[0:NEXP, 3, :], dthr[:], True, True), [P.bcst, bdthr], [bpthr])
        thrb = P.sb([128, NEXP], F32, stack=st)
        bthrb = Buf()
        P.act.op(act_fn(thrb[:], pthr[:, 0:NEXP], AF.Copy), [bpthr], [bthrb])
        ao = P.sb([128, OWN // 128, NEXP], F32, stack=st)
        bao = Buf()
        P.sp.dma(out=ao[:], in_=aff_own.rearrange("(t p) e -> p t e", p=128), writes=[bao])
        msk = P.sb([128, OWN // 128, NEXP], F32, stack=st)
        bmsk = Buf()
        thr_bc = bass.AP(thrb.tensor if hasattr(thrb, "tensor") else thrb[:].tensor, thrb[:].offset,
                         [list(thrb[:].ap[0]), [0, OWN // 128], [1, NEXP]])
        P.dve.op(tt_fn(msk[:], ao[:], thr_bc, ALU.is_ge), [bao, bthrb], [bmsk])
        P.dve.op(tt_fn(gate[:], msk[:], ao[:], ALU.mult), [bmsk, bao], [bgate])
        P.barrier()

    HT = OWN // 2
    with contextlib.ExitStack() as st:
        acc = P.sb([128, HT // 128, D], F32, stack=st)
        bacc = [Buf() for _ in range(HT // 128)]
        hT = P.sb([128, 8, HT], BF16, stack=st)
        bhT = Buf()
        junk = Ring(P, 1, [128, D], F32, st)
        small = Ring(P, 8, [128, 1], F32, st)
        hnr = Ring(P, 2, [128, D], BF16, st)
        hor = Ring(P, 2, [128, D], F32, st)
        ptr = Ring(P, 1, [128, 8, 128], BF16, st, psum=True)
        wgr = Ring(P, 2, [128, 8, D], BF16, st)
        wur = Ring(P, 2, [128, 8, D], BF16, st)
        wdr = Ring(P, 2, [128, 8, D], BF16, st)
        atr = Ring(P, 2, [128, 8, 512], BF16, st)
        sgr = Ring(P, 2, [128, 512], F32, st)
        pgr = Ring(P, 2, [128, 512], F32, st, psum=True)
        pur = Ring(P, 2, [128, 512], F32, st, psum=True)
        pyr = Ring(P, 2, [128, 512], F32, st, psum=True)
        for half in range(2):
            for t in range(HT // 128):
                tg = half * (HT // 128) + t
                P.sp.dma(out=acc[:, t, :], in_=x1[tg * 128:(tg + 1) * 128, :], writes=[bacc[t]])
                rs, br = rms_rstd(P, acc[:, t, :], bacc[t], D, junk, small)
                hn, bhn = hnr.next()
                P.dve.op(stt_fn(hn[:], acc[:, t, :], rs[:, 0:1], gfb[:], ALU.mult, ALU.mult),
                         [bacc[t], br, bgfb], [bhn])
                pt, bpt = ptr.next()
                for k in range(8):
                    P.pe.op(lambda e, k=k, pt=pt, hn=hn: e.transpose(pt[:, k, :], hn[:, k * 128:(k + 1) * 128],
                                                                      P.identb[:]), [bhn, P.bidb], [bpt])
                P.act.op(act_fn(hT[:, :, t * 128:(t + 1) * 128], pt[:], AF.Copy), [bpt], [bhT])
            for e in range(NEXP):
                wts = []
                for src, ring in ((wg, wgr), (wu, wur), (wd, wdr)):
                    w, bw = ring.next()
                    sv = src[e].rearrange("(k p) f -> p k f", p=128)
                    P.pool.dma(out=w[:, 0:4, :], in_=sv[:, 0:4, :], writes=[bw])
                    P.pool.dma(out=w[:, 4:8, :], in_=sv[:, 4:8, :], writes=[bw])
                    wts.append((w, bw))
                (Wg, bWg), (Wu, bWu), (Wd, bWd) = wts
                for q in range(HT // 512):
                    AT, bAT = atr.next()
                    for fc in range(8):
                        pg, bpg = pgr.next()
                        pu, bpu = pur.next()
                        for k in range(8):
                            P.pe.op(mm_fn(pg[:], Wg[:, k, fc * 128:(fc + 1) * 128], hT[:, k, q * 512:(q + 1) * 512],
                                          k == 0, k == 7), [bWg, bhT], [bpg])
                        for k in range(8):
                            P.pe.op(mm_fn(pu[:], Wu[:, k, fc * 128:(fc + 1) * 128], hT[:, k, q * 512:(q + 1) * 512],
                                          k == 0, k == 7), [bWu, bhT], [bpu])
                        sg, bsg = sgr.next()
                        P.act.op(act_fn(sg[:], pg[:], AF.Silu), [bpg], [bsg])
                        P.dve.op(tt_fn(AT[:, fc, :], sg[:], pu[:], ALU.mult), [bsg, bpu], [bAT])
                    for tt in range(4):
                        t = q * 4 + tt
                        tg = half * (HT // 128) + t
                        for hc in range(2):
                            py, bpy = pyr.next()
                            for fc in range(8):
                                P.pe.op(mm_fn(py[:], AT[:, fc, tt * 128:(tt + 1) * 128],
                                              Wd[:, fc, hc * 512:(hc + 1) * 512], fc == 0, fc == 7), [bAT, bWd], [bpy])
                            asl = acc[:, t, hc * 512:(hc + 1) * 512]
                            P.dve.op(stt_fn(asl, py[:], gate[:, tg, e:e + 1], asl, ALU.mult, ALU.add),
                                     [bpy, bgate, bacc[t]], [bacc[t]])
            for t in range(HT // 128):
                tg = half * (HT // 128) + t
                P.sp.dma(out=x2_out[tg * 128:(tg + 1) * 128, :], in_=acc[:, t, :], reads=[bacc[t]])
                if gn is not None:
                    rs, br = rms_rstd(P, acc[:, t, :], bacc[t], D, junk, small)
                    ho, bho = hor.next()
                    P.dve.op(stt_fn(ho[:], acc[:, t, :], rs[:, 0:1], gnb[:], ALU.mult, ALU.mult),
                             [bacc[t], br, bgnb], [bho])
                    P.sp.dma(out=hn_out[tg * 128:(tg + 1) * 128, :], in_=ho[:], reads=[bho])


def run_phaseB(x1_cores, affT_cores, aff_cores, gf, wg, wu, wd, gn):
    nc = get_prog("B", build_phaseB)
    cst = host_consts()
    in_maps = []
    for c in range(NCORES):
        b = c // 2
        affT_seq = np.ascontiguousarray(np.concatenate([affT_cores[2 * b], affT_cores[2 * b + 1]], axis=1))
        in_maps.append({
            "x1": x1_cores[c], "affT_seq": affT_seq, "aff_own": aff_cores[c],
            "gf": np.ascontiguousarray(gf[None, :]), "wg": wg, "wu": wu, "wd": wd, "cst": cst,
            "gn": np.ascontiguousarray(gn[None, :]),
        })
    res = run_bass_kernel_spmd(nc, in_maps, core_ids=list(range(NCORES)))
    return [r["x2"] for r in res.results], [r["hn"] for r in res.results]


NG = 32
NK = SEQ // 8
NWIN = NK // 8
HALF_PI = 1.5707963267948966


class _Stop(Exception):
    pass


def build_phaseC(stop_after=99):
    P = Prog()
    try:
        _phaseC_body(P, stop_after)
    except _Stop:
        P.barrier()
    return P.finish()


def _phaseC_body(P, stop_after):
    lamr_i = P.inp("lamr", [128, NG])
    lami_i = P.inp("lami", [128, NG])
    ldt_i = P.inp("ldt", [128, NG])
    B_i = P.inp("Bri", [128, 2, NG, 16])
    C_i = P.inp("Cri", [128, 2, NG, 16])
    dcol_i = P.inp("dcol", [128, NG])
    Unat = P.inp("Unat", [NG, 128, NK])
    Uw = P.inp("Uw", [NWIN, 128, NG, 8])
    Uwr = P.inp("Uwr", [NWIN, 128, NG, 8])
    masks_i = P.inp("masks", [128, 2, 128])
    cst_in = P.inp("cst", [128, 6, 128])
    zp = P.outp("zp", [NG, 128, NK])
    load_consts(P, cst_in)
    dve, act, pe, pool, sp = P.dve, P.act, P.pe, P.pool, P.sp

    M = P.sb([128, NG, 128], BF16, name="M")
    bM = Buf()
    Wz = P.sb([128, NG, 4, 128], BF16, name="Wz")
    bWz = Buf()
    Rr = P.sb([128, NG, 128], BF16, name="Rr")
    Ri = P.sb([128, NG, 128], BF16, name="Ri")
    bR = Buf()
    A8 = P.sb([128, 2, 2, NG], F32, name="A8")
    bA8 = Buf()
    dcol = P.sb([128, NG], F32, name="dcol")
    bdcol = Buf()
    sp.dma(out=dcol[:], in_=dcol_i, writes=[bdcol])

    def small(st, name=None):
        return P.sb([128, NG], F32, stack=st), Buf()

    with contextlib.ExitStack() as st:
        lamr, blamr = small(st)
        lami, blami = small(st)
        ldt, bldt = small(st)
        sp.dma(out=lamr[:], in_=lamr_i, writes=[blamr])
        sp.dma(out=lami[:], in_=lami_i, writes=[blami])
        sp.dma(out=ldt[:], in_=ldt_i, writes=[bldt])
        Bt = P.sb([128, 2, NG, 16], F32, stack=st)
        bBt = Buf()
        Ct = P.sb([128, 2, NG, 16], F32, stack=st)
        bCt = Buf()
        sp.dma(out=Bt[:], in_=B_i, writes=[bBt])
        sp.dma(out=Ct[:], in_=C_i, writes=[bCt])
        mk = P.sb([128, 2, 128], F32, stack=st)
        bmk = Buf()
        sp.dma(out=mk[:], in_=masks_i, writes=[bmk])
        hpi = P.sb([128, 1], F32, stack=st)
        bhpi = Buf()
        dve.op(lambda e: e.memset(hpi[:], HALF_PI), [], [bhpi])

        def T2(a, ba, b, bb, op):
            o, bo = small(st)
            dve.op(tt_fn(o[:], a[:], b[:], op), [ba, bb], [bo])
            return o, bo

        dt, bdt = small(st)
        act.op(act_fn(dt[:], ldt[:], AF.Exp), [bldt], [bdt])
        a_, ba_ = T2(lamr, blamr, dt, bdt, ALU.mult)
        ang, bang = T2(lami, blami, dt, bdt, ALU.mult)
        mag, bmag = small(st)
        act.op(act_fn(mag[:], a_[:], AF.Exp, scale=1.0 / 16), [ba_], [bmag])
        s16, bs16 = small(st)
        act.op(act_fn(s16[:], ang[:], AF.Sin, scale=1.0 / 16), [bang], [bs16])
        c16, bc16 = small(st)
        act.op(act_fn(c16[:], ang[:], AF.Sin, scale=1.0 / 16, bias=hpi[:, 0:1]), [bang, bhpi], [bc16])
        re, bre = T2(mag, bmag, c16, bc16, ALU.mult)
        im, bim = T2(mag, bmag, s16, bs16, ALU.mult)
        for _ in range(4):
            r2, br2 = T2(re, bre, re, bre, ALU.mult)
            i2, bi2 = T2(im, bim, im, bim, ALU.mult)
            nre, bnre = T2(r2, br2, i2, bi2, ALU.subtract)
            nim, bnim = small(st)
            dve.op(stt_fn(nim[:], re[:], 2.0, im[:], ALU.mult, ALU.mult), [bre, bim], [bnim])
            re, bre, im, bim = nre, bnre, nim, bnim
        pw = P.sb([128, 9, 2, NG], F32, stack=st)
        bpw = Buf()
        ipw = P.sb([128, 9, 2, NG], F32, stack=st)
        bipw = Buf()
        dve.op(lambda e: e.memset(pw[:, 0, 0, :], 1.0), [], [bpw])
        dve.op(lambda e: e.memset(pw[:, 0, 1, :], 0.0), [], [bpw])
        dve.op(lambda e: e.memset(ipw[:, 0, 0, :], 1.0), [], [bipw])
        dve.op(lambda e: e.memset(ipw[:, 0, 1, :], 0.0), [], [bipw])
        dve.op(lambda e: e.tensor_copy(out=pw[:, 1, 0, :], in_=re[:]), [bre], [bpw])
        dve.op(lambda e: e.tensor_copy(out=pw[:, 1, 1, :], in_=im[:]), [bim], [bpw])
        t1, bt1 = small(st)
        t2, bt2 = small(st)
        for n in range(1, 8):
            dve.op(tt_fn(t1[:], pw[:, n, 0, :], re[:], ALU.mult), [bpw, bre], [bt1])
            dve.op(tt_fn(t2[:], pw[:, n, 1, :], im[:], ALU.mult), [bpw, bim], [bt2])
            dve.op(tt_fn(pw[:, n + 1, 0, :], t1[:], t2[:], ALU.subtract), [bt1, bt2], [bpw])
            dve.op(tt_fn(t1[:], pw[:, n, 0, :], im[:], ALU.mult), [bpw, bim], [bt1])
            dve.op(tt_fn(t2[:], pw[:, n, 1, :], re[:], ALU.mult), [bpw, bre], [bt2])
            dve.op(tt_fn(pw[:, n + 1, 1, :], t1[:], t2[:], ALU.add), [bt1, bt2], [bpw])
        en, ben = small(st)
        for n in range(1, 9):
            act.op(act_fn(en[:], a_[:], AF.Exp, scale=-2.0 * n), [ba_], [ben])
            dve.op(tt_fn(ipw[:, n, 0, :], pw[:, n, 0, :], en[:], ALU.mult), [bpw, ben], [bipw])
            dve.op(stt_fn(ipw[:, n, 1, :], pw[:, n, 1, :], -1.0, en[:], ALU.mult, ALU.mult), [bpw, ben], [bipw])
        dve.op(lambda e: e.tensor_copy(out=A8[:, 0, 0, :], in_=pw[:, 8, 0, :]), [bpw], [bA8])
        dve.op(lambda e: e.tensor_copy(out=A8[:, 0, 1, :], in_=pw[:, 8, 0, :]), [bpw], [bA8])
        dve.op(ts_fn(A8[:, 1, 0, :], pw[:, 8, 1, :], -1.0, None, ALU.mult), [bpw], [bA8])
        dve.op(lambda e: e.tensor_copy(out=A8[:, 1, 1, :], in_=pw[:, 8, 1, :]), [bpw], [bA8])
        lr2, blr2 = T2(lamr, blamr, lamr, blamr, ALU.mult)
        li2, bli2 = T2(lami, blami, lami, blami, ALU.mult)
        den, bden = T2(lr2, blr2, li2, bli2, ALU.add)
        rden, brden = small(st)
        dve.op(lambda e: e.reciprocal(out=rden[:], in_=den[:]), [bden], [brden])
        nr, bnr = small(st)
        dve.op(ts_fn(nr[:], re[:], -1.0, None, ALU.add), [bre], [bnr])
        u1, bu1 = T2(nr, bnr, lamr, blamr, ALU.mult)
        u2, bu2 = T2(im, bim, lami, blami, ALU.mult)
        u3, bu3 = T2(u1, bu1, u2, bu2, ALU.add)
        fre, bfre = T2(u3, bu3, rden, brden, ALU.mult)
        u4, bu4 = T2(im, bim, lamr, blamr, ALU.mult)
        u5, bu5 = T2(nr, bnr, lami, blami, ALU.mult)
        u6, bu6 = T2(u4, bu4, u5, bu5, ALU.subtract)
        fim, bfim = T2(u6, bu6, rden, brden, ALU.mult)

        def bc16_(t):
            a = t[:]
            return bass.AP(a.tensor, a.offset, [list(a.ap[0]), [1, NG], [0, 16]])

        big = lambda: (P.sb([128, NG, 16], F32, stack=st), Buf())
        bbr, bbbr = big()
        bbi, bbbi = big()
        g1, bg1 = big()
        g2, bg2 = big()
        dve.op(tt_fn(g1[:], Bt[:, 0, :, :], bc16_(fre), ALU.mult), [bBt, bfre], [bg1])
        dve.op(tt_fn(g2[:], Bt[:, 1, :, :], bc16_(fim), ALU.mult), [bBt, bfim], [bg2])
        dve.op(tt_fn(bbr[:], g1[:], g2[:], ALU.subtract), [bg1, bg2], [bbbr])
        dve.op(tt_fn(g1[:], Bt[:, 1, :, :], bc16_(fre), ALU.mult), [bBt, bfre], [bg1])
        dve.op(tt_fn(g2[:], Bt[:, 0, :, :], bc16_(fim), ALU.mult), [bBt, bfim], [bg2])
        dve.op(tt_fn(bbi[:], g1[:], g2[:], ALU.add), [bg1, bg2], [bbbi])

        def table(top, bot):
            t = P.sb([128, 8, 2, NG], F32, stack=st)
            bt = Buf()
            for i in range(8):
                (ta, tn), (ba, bn) = top[i], bot[i]
                pool.op(lambda e, i=i, ta=ta, tn=tn: e.tensor_copy(out=t[0:64, i, :, :], in_=ta[0:64, tn, :, :]),
                        [bpw, bipw], [bt])
                pool.op(lambda e, i=i, ba=ba, bn=bn: e.tensor_copy(out=t[64:128, i, :, :], in_=ba[64:128, bn, :, :]),
                        [bpw, bipw], [bt])
            return t, bt

        QX, bQX = table([(ipw, s) for s in range(8)], [(pw, s) for s in range(8)])
        PY, bPY = table([(pw, t) for t in range(8)], [(ipw, t) for t in range(8)])
        QW, bQW = table([(pw, 7 - s) for s in range(8)], [(pw, s) for s in range(8)])
        PR, bPR = table([(pw, j + 1) for j in range(8)], [(pw, 8 - j) for j in range(8)])

        def tb(t, i, ri):
            a = t[:, i, ri, :]
            return bass.AP(a.tensor, a.offset, [list(a.ap[0]), [1, NG], [0, 16]])

        def cprod(dst_re, dst_im, bdst, xr, xi, bx, tab, btab, neg_im=False, eng=None):
            eng = eng or dve
            bx = list(bx) if isinstance(bx, (list, tuple)) else [bx]
            for i in range(8):
                eng.op(tt_fn(g1[:], xr[:], tb(tab, i, 0), ALU.mult), bx + [btab], [bg1])
                eng.op(tt_fn(g2[:], xi[:], tb(tab, i, 1), ALU.mult), bx + [btab], [bg2])
                eng.op(tt_fn(dst_re[:, :, i, :], g1[:], g2[:], ALU.subtract), [bg1, bg2], [bdst])
                eng.op(tt_fn(g1[:], xr[:], tb(tab, i, 1), ALU.mult), bx + [btab], [bg1])
                eng.op(tt_fn(g2[:], xi[:], tb(tab, i, 0), ALU.mult), bx + [btab], [bg2])
                if neg_im:
                    eng.op(stt_fn(dst_im[:, :, i, :], g1[:], -1.0, g2[:], ALU.mult, ALU.subtract), [bg1, bg2], [bdst])
                else:
                    eng.op(tt_fn(dst_im[:, :, i, :], g1[:], g2[:], ALU.add), [bg1, bg2], [bdst])

        bbx = Buf()
        with contextlib.ExitStack() as st2:
            Xr = P.sb([128, NG, 8, 16], F32, stack=st2)
            Xi = P.sb([128, NG, 8, 16], F32, stack=st2)
            Yr = P.sb([128, NG, 8, 16], F32, stack=st2)
            Yn = P.sb([128, NG, 8, 16], F32, stack=st2)
            bX, bY = Buf(), Buf()
            bBB = Buf()
            cprod(Xr, Xi, bX, bbr, bbi, [bbbr, bbbi], QX, bQX)
            cprod(Yr, Yn, bY, Ct[:, 0, :, :], Ct[:, 1, :, :], bCt, PY, bPY, neg_im=True)
            psF = Ring(P, 2, [128, 512], F32, st2, psum=True)
            psB = Ring(P, 2, [128, 512], F32, st2, psum=True)
            tmr = Ring(P, 2, [128, 128], F32, st2)
            Xr3 = Xr[:].rearrange("p g s c -> p g (s c)")
            Xi3 = Xi[:].rearrange("p g s c -> p g (s c)")
            Yr3 = Yr[:].rearrange("p g s c -> p g (s c)")
            Yn3 = Yn[:].rearrange("p g s c -> p g (s c)")
            for g in range(NG):
                pf, bpf = psF.next()
                pb_, bpb = psB.next()
                pe.op(mm_fn(pf[:, 0:128], Xr3[0:64, g, :], Yr3[0:64, g, :], True, False), [bX, bY], [bpf])
                pe.op(mm_fn(pf[:, 0:128], Xi3[0:64, g, :], Yn3[0:64, g, :], False, True), [bX, bY], [bpf])
                pe.op(mm_fn(pb_[:, 0:128], Xr3[64:128, g, :], Yr3[64:128, g, :], True, False), [bX, bY], [bpb])
                pe.op(mm_fn(pb_[:, 0:128], Xi3[64:128, g, :], Yn3[64:128, g, :], False, True), [bX, bY], [bpb])
                tm, btm = tmr.next()
                dve.op(tt_fn(tm[:], pf[:, 0:128], mk[:, 0, :], ALU.mult), [bpf, bmk], [btm])
                tm2, btm2 = tmr.next()
                dve.op(tt_fn(tm2[:], pb_[:, 0:128], mk[:, 1, :], ALU.mult), [bpb, bmk], [btm2])
                dve.op(tt_fn(M[:, g, :], tm[:], tm2[:], ALU.add), [btm, btm2], [bM])
            P.barrier()
        if stop_after <= 1:
            P.stopped = True
        with contextlib.ExitStack() as st2:
            Wr = P.sb([128, NG, 8, 16], F32, stack=st2)
            Wi = P.sb([128, NG, 8, 16], F32, stack=st2)
            bW = Buf()
            cprod(Wr, Wi, bW, bbr, bbi, [bbbr, bbbi], QW, bQW)
            pool.op(lambda e: e.memset(Wz[:], 0.0), [], [bWz])
            ptw = Ring(P, 2, [128, 512], F32, st2, psum=True)
            W3 = (Wr[:].rearrange("p g s c -> p g (s c)"), Wi[:].rearrange("p g s c -> p g (s c)"))
            for g in range(NG):
                for ri in range(2):
                    pt, bpt = ptw.next()
                    pe.op(lambda e, pt=pt, g=g, ri=ri: e.transpose(pt[:, 0:128], W3[ri][:, g, :], P.identf),
                          [bW, P.bcst], [bpt])
                    act.op(act_fn(Wz[:, g, 2 * ri, 0:64], pt[:, 0:64], AF.Copy), [bpt], [bWz])
                    act.op(act_fn(Wz[:, g, 2 * ri + 1, 64:128], pt[:, 64:128], AF.Copy), [bpt], [bWz])
            P.barrier()
        if stop_after <= 2:
            P.stopped = True
        Rr4 = Rr[:].rearrange("p g (j c) -> p g j c", j=8)
        Ri4 = Ri[:].rearrange("p g (j c) -> p g j c", j=8)
        cprod(Rr4, Ri4, bR, Ct[:, 0, :, :], Ct[:, 1, :, :], bCt, PR, bPR, neg_im=True)
        P.barrier()

    with contextlib.ExitStack() as st:
        hist = P.sb([128, 2, NG, NK], BF16, stack=st)
        bhist = Buf()
        uwr = Ring(P, 3, [128, NG, 8], BF16, st)
        uwrr = Ring(P, 3, [128, NG, 8], BF16, st)
        pvr = Ring(P, 3, [128, 2, NG, 8], F32, st, psum=True)
        S4r = Ring(P, 4, [128, 3, NG], F32, st)
        p1 = P.sb([128, 2, NG], F32, stack=st)
        p2 = P.sb([128, 2, NG], F32, stack=st)
        bp1, bp2 = Buf(), Buf()
        S4, bS = S4r.next()
        dve.op(lambda e: e.memset(S4[:], 0.0), [], [bS])
        for w in range(NWIN):
            uw, buw = uwr.next()
            uwb, buwb = uwrr.next()
            pool.dma(out=uw[:], in_=Uw[w], writes=[buw])
            pool.dma(out=uwb[:], in_=Uwr[w], writes=[buwb])
            pv, bpv = pvr.next()
            for g in range(NG):
                for ri in range(2):
                    pe.op(mm_fn(pv[:, ri, g, :], Wz[:, g, 2 * ri, :], uw[:, g, :], True, False), [bWz, buw], [bpv])
                    pe.op(mm_fn(pv[:, ri, g, :], Wz[:, g, 2 * ri + 1, :], uwb[:, g, :], False, True), [bWz, buwb], [bpv])
            for jj in range(8):
                j = w * 8 + jj
                act.op(act_fn(hist[0:64, :, :, j], S4[0:64, 0:2, :], AF.Copy), [bS], [bhist])
                act.op(act_fn(hist[64:128, :, :, NK - 1 - j], S4[64:128, 0:2, :], AF.Copy), [bS], [bhist])
                Sn, bSn = S4r.next()
                dve.op(tt_fn(p1[:], A8[:, 0, :, :], S4[:, 0:2, :], ALU.mult), [bA8, bS], [bp1])
                dve.op(tt_fn(p2[:], A8[:, 1, :, :], S4[:, 1:3, :], ALU.mult), [bA8, bS], [bp2])
                dve.op(tt_fn(p1[:], p1[:], p2[:], ALU.add), [bp1, bp2], [bp1])
                dve.op(tt_fn(Sn[:, 0:2, :], p1[:], pv[:, :, :, jj], ALU.add), [bp1, bpv], [bSn])
                dve.op(lambda e, Sn=Sn: e.tensor_copy(out=Sn[:, 2, :], in_=Sn[:, 0, :]), [bSn], [bSn])
                S4, bS = Sn, bSn
        P.barrier()
        if stop_after <= 4:
            P.stopped = True
        ubr = Ring(P, 2, [128, NK], BF16, st)
        ufr = Ring(P, 2, [128, NK], F32, st)
        pyr = Ring(P, 2, [128, 512], F32, st, psum=True)
        yr = Ring(P, 2, [128, NK], F32, st)
        tr = Ring(P, 4, [128, NK], F32, st)
        for g in range(NG):
            ub, bub = ubr.next()
            uf, buf_ = ufr.next()
            pool.dma(out=ub[:], in_=Unat[g], writes=[bub])
            sp.dma(out=uf[:], in_=Unat[g], writes=[buf_])
            py, bpy = pyr.next()
            pe.op(mm_fn(py[:], M[:, g, :], ub[:], True, False), [bM, bub], [bpy])
            pe.op(mm_fn(py[:], Rr[:, g, :], hist[:, 0, g, :], False, False), [bR, bhist], [bpy])
            pe.op(mm_fn(py[:], Ri[:, g, :], hist[:, 1, g, :], False, True), [bR, bhist], [bpy])
            y, by = yr.next()
            dve.op(stt_fn(y[:], uf[:], dcol[:, g:g + 1], py[:], ALU.mult, ALU.add), [buf_, bdcol, bpy], [by])
            a1, ba1 = tr.next()
            dve.op(tt_fn(a1[:], y[:], y[:], ALU.mult), [by], [ba1])
            dve.op(ts_fn(a1[:], a1[:], 0.044715, 1.0, ALU.mult, ALU.add), [ba1], [ba1])
            dve.op(tt_fn(a1[:], a1[:], y[:], ALU.mult), [ba1, by], [ba1])
            a2, ba2 = tr.next()
            act.op(act_fn(a2[:], a1[:], AF.Sigmoid, scale=1.5957691216057308), [ba1], [ba2])
            dve.op(tt_fn(a2[:], a2[:], y[:], ALU.mult), [ba2, by], [ba2])
            sp.dma(out=zp[g], in_=a2[:], reads=[ba2])
    return


def host_masks():
    s = np.arange(128)[:, None] // 16
    t = np.arange(128)[None, :] // 16
    m = np.zeros((128, 2, 128), np.float32)
    m[:, 0, :] = (t >= s)
    m[:, 1, :] = (s >= t)
    return m


def phaseC_inputs(hn1_seq, gh, lam_re, lam_im, log_dt, b_re, b_im, c_re, c_im, d_skip):
    G0 = gh * NG
    sl = slice(G0, G0 + NG)

    def dpg(a):
        return np.ascontiguousarray(a.transpose(0, 2, 1).reshape(128, NG))

    lamr = dpg(lam_re[:, sl, :])
    lami = dpg(lam_im[:, sl, :])
    ldt = dpg(np.broadcast_to(log_dt[:, sl, None], (2, NG, 64)))
    Bri = np.stack([b_re[:, sl].transpose(0, 2, 1, 3).reshape(128, NG, 16),
                    b_im[:, sl].transpose(0, 2, 1, 3).reshape(128, NG, 16)], axis=1)
    Cri = np.stack([c_re[:, sl].transpose(0, 3, 1, 2).reshape(128, NG, 16),
                    c_im[:, sl].transpose(0, 3, 1, 2).reshape(128, NG, 16)], axis=1)
    dg = d_skip[G0 * 16:(G0 + NG) * 16].reshape(NG, 16)
    dcol = np.ascontiguousarray(np.broadcast_to(dg.T[None, :, :], (8, 16, NG)).reshape(128, NG))
    u = hn1_seq[:, G0 * 16:(G0 + NG) * 16].reshape(NK, 8, NG, 16)
    Unat = np.ascontiguousarray(u.transpose(2, 1, 3, 0).reshape(NG, 128, NK))
    Uw = np.ascontiguousarray(Unat.reshape(NG, 128, NWIN, 8).transpose(2, 1, 0, 3))
    Uwr = np.ascontiguousarray(Unat[:, :, ::-1].reshape(NG, 128, NWIN, 8).transpose(2, 1, 0, 3))
    return {"lamr": lamr, "lami": lami, "ldt": ldt, "Bri": np.ascontiguousarray(Bri),
            "Cri": np.ascontiguousarray(Cri), "dcol": dcol, "Unat": Unat, "Uw": Uw, "Uwr": Uwr,
            "masks": host_masks(), "cst": host_consts()}


def phaseC_unpack(zp):
    return np.ascontiguousarray(zp.reshape(NG, 8, 16, NK).transpose(3, 1, 0, 2).reshape(SEQ, NG * 16))


def emit_phaseD(P, io):
    with contextlib.ExitStack() as ph:
        P.cur = ph
        _phaseD_body(P, io["zs"], io["hn"], io["dvec"], io["x2"], io["w_glu"], io["gf"], io["w_r"], io["b_r"],
                     io["x3_out"], io["aff_out"], io["affT_out"])
        P.barrier()
    P.cur = None


def _phaseD_body(P, zt, hn_in, dvec, x2, w_glu, gf, w_r, b_r, x3_out, aff_out, affT_out):
    dvb = P.sb([128, D], F32, name="dvb")
    bdvb = Buf()
    P.sp.dma(out=dvb[:], in_=bcast_rows(dvec, 128), writes=[bdvb])
    st = P.cur
    wgl = P.sb([128, 8, 2 * D], BF16, name="wgl")
    bwgl = Buf()
    wv = w_glu.rearrange("(k p) e -> p k e", p=128)
    for k in range(8):
        P.pool.dma(out=wgl[:, k, :], in_=wv[:, k, :], writes=[bwgl])
    gfb = P.sb([128, D], F32, name="gfb")
    bgfb = Buf()
    P.sp.dma(out=gfb[:], in_=bcast_rows(gf, 128), writes=[bgfb])
    wr = P.sb([128, 8, 16], F32, name="wr")
    bwr = Buf()
    P.sp.dma(out=wr[:], in_=w_r.rearrange("(k p) e -> p k e", p=128), writes=[bwr])
    brb = P.sb([128, 16], F32, name="brb")
    bbrb = Buf()
    P.sp.dma(out=brb[:], in_=bcast_rows(b_r, 128), writes=[bbrb])
    zr = Ring(P, 2, [128, D], F32, st)
    zbr = Ring(P, 2, [128, D], BF16, st)
    hnr2 = Ring(P, 2, [128, D], F32, st)
    xr = Ring(P, 2, [128, D], F32, st)
    x3r = Ring(P, 2, [128, D], F32, st)
    ptr = Ring(P, 1, [128, 8, 128], BF16, st, psum=True)
    zTr = Ring(P, 2, [128, 8, 128], BF16, st)
    pvr = Ring(P, 1, [128, 512], F32, st, psum=True)
    pgr = Ring(P, 1, [128, 512], F32, st, psum=True)
    sgr = Ring(P, 2, [128, 512], F32, st)
    rings = {
        "junk": Ring(P, 1, [128, D], F32, st),
        "small": Ring(P, 12, [128, 1], F32, st),
        "hf": Ring(P, 2, [128, D], F32, st),
        "ptf": Ring(P, 1, [128, 8, 128], F32, st, psum=True),
        "hfT": Ring(P, 2, [128, 8, 128], F32, st),
        "plog": Ring(P, 1, [128, 512], F32, st, psum=True),
        "sm16": Ring(P, 6, [128, 16], F32, st),
        "pat": Ring(P, 1, [128, 512], F32, st, psum=True),
        "aT": Ring(P, 2, [16, 128], F32, st),
    }
    for tt in range(OWN // 128):
        z_, bz = zr.next()
        P.sp.dma(out=z_[:], in_=zt[tt * 128:(tt + 1) * 128, :], writes=[bz])
        xt, bx = xr.next()
        P.sp.dma(out=xt[:], in_=x2[tt * 128:(tt + 1) * 128, :], writes=[bx])
        hn_, bhn_ = hnr2.next()
        P.sp.dma(out=hn_[:], in_=hn_in[tt * 128:(tt + 1) * 128, :], writes=[bhn_])
        P.dve.op(tt_fn(hn_[:], hn_[:], dvb[:], ALU.mult), [bhn_, bdvb], [bhn_])
        P.dve.op(tt_fn(z_[:], z_[:], hn_[:], ALU.add), [bz, bhn_], [bz])
        P.dve.op(tt_fn(hn_[:], z_[:], z_[:], ALU.mult), [bz], [bhn_])
        P.dve.op(ts_fn(hn_[:], hn_[:], 0.044715, 1.0, ALU.mult, ALU.add), [bhn_], [bhn_])
        P.dve.op(tt_fn(hn_[:], hn_[:], z_[:], ALU.mult), [bhn_, bz], [bhn_])
        P.act.op(act_fn(hn_[:], hn_[:], AF.Sigmoid, scale=1.5957691216057308), [bhn_], [bhn_])
        zb, bzb = zbr.next()
        P.dve.op(tt_fn(zb[:], hn_[:], z_[:], ALU.mult), [bhn_, bz], [bzb])
        pt, bpt = ptr.next()
        for k in range(8):
            P.pe.op(lambda e, k=k, pt=pt, zb=zb: e.transpose(pt[:, k, :], zb[:, k * 128:(k + 1) * 128], P.identb[:]),
                    [bzb, P.bidb], [bpt])
        zT, bzT = zTr.next()
        P.act.op(act_fn(zT[:], pt[:], AF.Copy), [bpt], [bzT])
        x3t, bx3 = x3r.next()
        for half in range(2):
            pv, bpv = pvr.next()
            pg, bpg = pgr.next()
            for k in range(8):
                P.pe.op(mm_fn(pv[:], zT[:, k, :], wgl[:, k, half * 512:(half + 1) * 512], k == 0, k == 7),
                        [bzT, bwgl], [bpv])
            for k in range(8):
                P.pe.op(mm_fn(pg[:], zT[:, k, :], wgl[:, k, D + half * 512:D + (half + 1) * 512], k == 0, k == 7),
                        [bzT, bwgl], [bpg])
            sg, bsg = sgr.next()
            P.act.op(act_fn(sg[:], pg[:], AF.Sigmoid), [bpg], [bsg])
            P.dve.op(tt_fn(sg[:], sg[:], pv[:], ALU.mult), [bsg, bpv], [bsg])
            P.dve.op(tt_fn(x3t[:, half * 512:(half + 1) * 512], sg[:], xt[:, half * 512:(half + 1) * 512], ALU.add),
                     [bsg, bx], [bx3])
        P.sp.dma(out=x3_out[tt * 128:(tt + 1) * 128, :], in_=x3t[:], reads=[bx3])
        router_tile(P, x3t, bx3, gfb, bgfb, wr, bwr, brb, bbrb, rings, aff_out, affT_out, tt)
    return


def to_local(full_seq, h):
    return local_view(full_seq, h, OWN)


def from_local(parts):
    out = []
    for b in range(NCORES // 2):
        a0 = parts[2 * b]
        a1 = parts[2 * b + 1][::-1]
        out.append(np.concatenate([a0, a1], axis=0))
    return np.stack(out)


G32 = 32
NKL = OWN // 8
NWL = NKL // 8


def emit_phaseC2(P, io):
    with contextlib.ExitStack() as ph:
        P.cur = ph
        _phaseC2_body(P, io)
        P.barrier()
    P.cur = None


def _phaseC2_body(P, io):
    dve, act, pe, pool, sp = P.dve, P.act, P.pe, P.pool, P.sp
    s5p, s5B, s5C = io["s5p"], io["s5B"], io["s5C"]
    HN, ZS = io["hn"], io["zs_out"]
    SAo, SAall = io["sa_own"], io["sa_all"]

    MS, RS = io["ms"], io["rs"]
    bM = Buf()
    bR = Buf()
    U = P.sb([128, 64, NKL], BF16, name="U")
    bU = Buf()
    A8 = [P.sb([128, 2, 2, G32], F32, name="A8_%d" % d) for d in range(2)]
    bA8 = Buf()
    mk = P.sb([128, 2, 128], F32, name="mk")
    bmk = Buf()
    sp.dma(out=mk[:], in_=io["masks"], writes=[bmk])
    flg = P.sb([128, 2], F32, name="flg")
    bflg = Buf()
    sp.dma(out=flg[:], in_=io["flags"], writes=[bflg])
    hpi = P.sb([128, 1], F32, name="hpi")
    bhpi = Buf()
    dve.op(lambda e: e.memset(hpi[:], HALF_PI), [], [bhpi])
    g1 = P.sb([128, G32, 16], F32, name="g1")
    g2 = P.sb([128, G32, 16], F32, name="g2")
    bg1, bg2 = Buf(), Buf()
    pw_t = [P.sb([128, 9, 2, G32], F32, name="pw%d" % d) for d in range(2)]
    ipw_t = [P.sb([128, 9, 2, G32], F32, name="ipw%d" % d) for d in range(2)]
    bbr_t = [P.sb([128, G32, 16], F32, name="bbr%d" % d) for d in range(2)]
    bbi_t = [P.sb([128, G32, 16], F32, name="bbi%d" % d) for d in range(2)]
    with contextlib.ExitStack() as stp:
        M = P.sb([128, 64, 128], BF16, name="M", stack=stp)
        Rt = [[P.sb([128, G32, 128], BF16, name="R%d%d" % (d, r), stack=stp) for r in range(2)] for d in range(2)]
        par = P.sb([128, 2, 3, G32], F32, name="par", stack=stp)
        bpar = Buf()
        sp.dma(out=par[:], in_=s5p, writes=[bpar])
        Bt = P.sb([128, 2, 2, G32, 16], F32, name="Bt", stack=stp)
        bBt = Buf()
        sp.dma(out=Bt[:], in_=s5B, writes=[bBt])
        Ct = P.sb([128, 2, 2, G32, 16], F32, name="Ct", stack=stp)
        bCt = Buf()
        sp.dma(out=Ct[:], in_=s5C, writes=[bCt])

        def small():
            return P.sb([128, G32], F32, stack=stp), Buf()

        def T2(a, ba, b, bb, op):
            o, bo = small()
            dve.op(tt_fn(o[:], a[:], b[:], op), [ba, bb], [bo])
            return o, bo

        def bc(a):
            return bass.AP(a.tensor, a.offset, [list(a.ap[0]), [1, G32], [0, 16]])

        pws, ipws, bbs = [], [], []
        for d in range(2):
            lamr, lami, ldt = par[:, d, 0, :], par[:, d, 1, :], par[:, d, 2, :]
            dt, bdt = small()
            act.op(act_fn(dt[:], ldt, AF.Exp), [bpar], [bdt])
            a_, ba_ = small()
            dve.op(tt_fn(a_[:], lamr, dt[:], ALU.mult), [bpar, bdt], [ba_])
            ang, bang = small()
            dve.op(tt_fn(ang[:], lami, dt[:], ALU.mult), [bpar, bdt], [bang])
            mag, bmag = small()
            act.op(act_fn(mag[:], a_[:], AF.Exp, scale=1.0 / 16), [ba_], [bmag])
            s16, bs16 = small()
            act.op(act_fn(s16[:], ang[:], AF.Sin, scale=1.0 / 16), [bang], [bs16])
            c16, bc16 = small()
            act.op(act_fn(c16[:], ang[:], AF.Sin, scale=1.0 / 16, bias=hpi[:, 0:1]), [bang, bhpi], [bc16])
            re, bre = T2(mag, bmag, c16, bc16, ALU.mult)
            im, bim = T2(mag, bmag, s16, bs16, ALU.mult)
            for _ in range(4):
                r2, br2 = T2(re, bre, re, bre, ALU.mult)
                i2, bi2 = T2(im, bim, im, bim, ALU.mult)
                nre, bnre = T2(r2, br2, i2, bi2, ALU.subtract)
                nim, bnim = small()
                dve.op(stt_fn(nim[:], re[:], 2.0, im[:], ALU.mult, ALU.mult), [bre, bim], [bnim])
                re, bre, im, bim = nre, bnre, nim, bnim
            pw = pw_t[d]
            ipw = ipw_t[d]
            bpw, bipw = Buf(), Buf()
            dve.op(lambda e, pw=pw: e.memset(pw[:, 0, 0, :], 1.0), [], [bpw])
            dve.op(lambda e, pw=pw: e.memset(pw[:, 0, 1, :], 0.0), [], [bpw])
            dve.op(lambda e, ipw=ipw: e.memset(ipw[:, 0, 0, :], 1.0), [], [bipw])
            dve.op(lambda e, ipw=ipw: e.memset(ipw[:, 0, 1, :], 0.0), [], [bipw])
            dve.op(lambda e, pw=pw, re=re: e.tensor_copy(out=pw[:, 1, 0, :], in_=re[:]), [bre], [bpw])
            dve.op(lambda e, pw=pw, im=im: e.tensor_copy(out=pw[:, 1, 1, :], in_=im[:]), [bim], [bpw])
            t1, bt1 = small()
            t2, bt2 = small()
            for n in range(1, 8):
                dve.op(tt_fn(t1[:], pw[:, n, 0, :], re[:], ALU.mult), [bpw, bre], [bt1])
                dve.op(tt_fn(t2[:], pw[:, n, 1, :], im[:], ALU.mult), [bpw, bim], [bt2])
                dve.op(tt_fn(pw[:, n + 1, 0, :], t1[:], t2[:], ALU.subtract), [bt1, bt2], [bpw])
                dve.op(tt_fn(t1[:], pw[:, n, 0, :], im[:], ALU.mult), [bpw, bim], [bt1])
                dve.op(tt_fn(t2[:], pw[:, n, 1, :], re[:], ALU.mult), [bpw, bre], [bt2])
                dve.op(tt_fn(pw[:, n + 1, 1, :], t1[:], t2[:], ALU.add), [bt1, bt2], [bpw])
            en, ben = small()
            for n in range(1, 9):
                act.op(act_fn(en[:], a_[:], AF.Exp, scale=-2.0 * n), [ba_], [ben])
                dve.op(tt_fn(ipw[:, n, 0, :], pw[:, n, 0, :], en[:], ALU.mult), [bpw, ben], [bipw])
                dve.op(stt_fn(ipw[:, n, 1, :], pw[:, n, 1, :], -1.0, en[:], ALU.mult, ALU.mult), [bpw, ben], [bipw])
            a8 = A8[d]
            dve.op(lambda e, a8=a8, pw=pw: e.tensor_copy(out=a8[:, 0, 0, :], in_=pw[:, 8, 0, :]), [bpw], [bA8])
            dve.op(lambda e, a8=a8, pw=pw: e.tensor_copy(out=a8[:, 0, 1, :], in_=pw[:, 8, 0, :]), [bpw], [bA8])
            dve.op(ts_fn(a8[:, 1, 0, :], pw[:, 8, 1, :], -1.0, None, ALU.mult), [bpw], [bA8])
            dve.op(lambda e, a8=a8, pw=pw: e.tensor_copy(out=a8[:, 1, 1, :], in_=pw[:, 8, 1, :]), [bpw], [bA8])
            lr2, blr2 = small()
            dve.op(tt_fn(lr2[:], lamr, lamr, ALU.mult), [bpar], [blr2])
            li2, bli2 = small()
            dve.op(tt_fn(li2[:], lami, lami, ALU.mult), [bpar], [bli2])
            den, bden = T2(lr2, blr2, li2, bli2, ALU.add)
            rden, brden = small()
            dve.op(lambda e, rden=rden, den=den: e.reciprocal(out=rden[:], in_=den[:]), [bden], [brden])
            nr, bnr = small()
            dve.op(ts_fn(nr[:], re[:], -1.0, None, ALU.add), [bre], [bnr])
            u1, bu1 = small()
            dve.op(tt_fn(u1[:], nr[:], lamr, ALU.mult), [bnr, bpar], [bu1])
            u2, bu2 = small()
            dve.op(tt_fn(u2[:], im[:], lami, ALU.mult), [bim, bpar], [bu2])
            u3, bu3 = T2(u1, bu1, u2, bu2, ALU.add)
            fre, bfre = T2(u3, bu3, rden, brden, ALU.mult)
            u4, bu4 = small()
            dve.op(tt_fn(u4[:], im[:], lamr, ALU.mult), [bim, bpar], [bu4])
            u5, bu5 = small()
            dve.op(tt_fn(u5[:], nr[:], lami, ALU.mult), [bnr, bpar], [bu5])
            u6, bu6 = T2(u4, bu4, u5, bu5, ALU.subtract)
            fim, bfim = T2(u6, bu6, rden, brden, ALU.mult)
            bbr = bbr_t[d]
            bbi = bbi_t[d]
            bbb = Buf()
            dve.op(tt_fn(g1[:], Bt[:, d, 0, :, :], bc(fre[:]), ALU.mult), [bBt, bfre], [bg1])
            dve.op(tt_fn(g2[:], Bt[:, d, 1, :, :], bc(fim[:]), ALU.mult), [bBt, bfim], [bg2])
            dve.op(tt_fn(bbr[:], g1[:], g2[:], ALU.subtract), [bg1, bg2], [bbb])
            dve.op(tt_fn(g1[:], Bt[:, d, 1, :, :], bc(fre[:]), ALU.mult), [bBt, bfre], [bg1])
            dve.op(tt_fn(g2[:], Bt[:, d, 0, :, :], bc(fim[:]), ALU.mult), [bBt, bfim], [bg2])
            dve.op(tt_fn(bbi[:], g1[:], g2[:], ALU.add), [bg1, bg2], [bbb])
            pws.append((pw, bpw))
            ipws.append((ipw, bipw))
            bbs.append((bbr, bbi, bbb))

        def cprod(dst_re, dst_im, bdst, xr, xi, bx, tab, btab, idx, neg_im=False):
            bx = list(bx) if isinstance(bx, (list, tuple)) else [bx]
            for i in range(8):
                tr, ti = bc(tab[:, idx(i), 0, :]), bc(tab[:, idx(i), 1, :])
                dve.op(tt_fn(g1[:], xr, tr, ALU.mult), bx + [btab], [bg1])
                dve.op(tt_fn(g2[:], xi, ti, ALU.mult), bx + [btab], [bg2])
                dve.op(tt_fn(dst_re[:, :, i, :], g1[:], g2[:], ALU.subtract), [bg1, bg2], [bdst])
                dve.op(tt_fn(g1[:], xr, ti, ALU.mult), bx + [btab], [bg1])
                dve.op(tt_fn(g2[:], xi, tr, ALU.mult), bx + [btab], [bg2])
                if neg_im:
                    dve.op(stt_fn(dst_im[:, :, i, :], g1[:], -1.0, g2[:], ALU.mult, ALU.subtract), [bg1, bg2], [bdst])
                else:
                    dve.op(tt_fn(dst_im[:, :, i, :], g1[:], g2[:], ALU.add), [bg1, bg2], [bdst])

        v4 = lambda t: t[:].rearrange("p g (j c) -> p g j c", j=8)
        f3 = lambda t: t[:].rearrange("p g s c -> p g (s c)")

        with contextlib.ExitStack() as st2:
            XY = [[P.sb([128, G32, 8, 16], BF16, stack=st2) for _ in range(4)] for _ in range(2)]
            bXY = Buf()
            (pwA, bpwA), (ipwA, bipwA) = pws[0], ipws[0]
            (pwB, bpwB), (ipwB, bipwB) = pws[1], ipws[1]
            cprod(XY[0][0], XY[0][1], bXY, bbs[0][0][:], bbs[0][1][:], bbs[0][2], ipwA, bipwA, lambda s: s)
            cprod(XY[0][2], XY[0][3], bXY, Ct[:, 0, 0, :, :], Ct[:, 0, 1, :, :], bCt, pwA, bpwA, lambda t: t, neg_im=True)
            cprod(XY[1][0], XY[1][1], bXY, bbs[1][0][:], bbs[1][1][:], bbs[1][2], pwB, bpwB, lambda s: s)
            cprod(XY[1][2], XY[1][3], bXY, Ct[:, 1, 0, :, :], Ct[:, 1, 1, :, :], bCt, ipwB, bipwB, lambda t: t, neg_im=True)
            cprod(v4(Rt[0][0]), v4(Rt[0][1]), bR, Ct[:, 0, 0, :, :], Ct[:, 0, 1, :, :], bCt, pwA, bpwA,
                  lambda j: j + 1, neg_im=True)
            cprod(v4(Rt[1][0]), v4(Rt[1][1]), bR, Ct[:, 1, 0, :, :], Ct[:, 1, 1, :, :], bCt, pwB, bpwB,
                  lambda j: 8 - j, neg_im=True)
            pk = [[Ring(P, 1, [128, 512], F32, st2, psum=True) for _ in range(2)] for _ in range(2)]
            tmr = Ring(P, 4, [128, 128], F32, st2)
            for g in range(G32):
                pp = [[None, None], [None, None]]
                for d in range(2):
                    Xr, Xi, Yr, Yn = [f3(t) for t in XY[d]]
                    for hf in range(2):
                        r0 = 64 * hf
                        pt, bpt = pk[d][hf].next()
                        pe.op(mm_fn(pt[:, 0:128], Xr[r0:r0 + 64, g, :], Yr[r0:r0 + 64, g, :], True, False), [bXY], [bpt])
                        pe.op(mm_fn(pt[:, 0:128], Xi[r0:r0 + 64, g, :], Yn[r0:r0 + 64, g, :], False, True), [bXY], [bpt])
                        pp[d][hf] = (pt, bpt)
                for hf in range(2):
                    tm, btm = tmr.next()
                    dve.op(tt_fn(tm[:], pp[0][hf][0][:, 0:128], mk[:, 0, :], ALU.mult), [pp[0][hf][1], bmk], [btm])
                    tm2, btm2 = tmr.next()
                    dve.op(tt_fn(tm2[:], pp[1][hf][0][:, 0:128], mk[:, 1, :], ALU.mult), [pp[1][hf][1], bmk], [btm2])
                    dve.op(tt_fn(M[:, hf * G32 + g, :], tm[:], tm2[:], ALU.add), [btm, btm2], [bM])
            sp.dma(out=MS.ap(), in_=M[:].rearrange("p g c -> p (g c)"), reads=[bM])
            for d in range(2):
                for r in range(2):
                    sp.dma(out=RS.ap()[:, (2 * d + r) * G32 * 128:(2 * d + r + 1) * G32 * 128],
                           in_=Rt[d][r][:].rearrange("p g c -> p (g c)"), reads=[bR])
            P.barrier()

    with contextlib.ExitStack() as st2:
        Tb = Ring(P, 2, [128, 8, D], BF16, st2)
        Tb2 = Ring(P, 1, [128, 64, 128], BF16, st2)
        ptu = Ring(P, 2, [128, 8, 128], BF16, st2, psum=True)
        for kb in range(NKL // 128):
            tb, btb = Tb.next()
            src = HN[kb * 1024:(kb + 1) * 1024, :].rearrange("(p s) d -> p s d", s=8)
            pool.dma(out=tb[:], in_=src, writes=[btb])
            tb2, btb2 = Tb2.next()
            dve.op(lambda e, tb=tb, tb2=tb2: e.tensor_copy(
                out=tb2[:].rearrange("p g (s c) -> p s g c", s=8),
                in_=tb[:].rearrange("p s (g c) -> p s g c", c=16)), [btb], [btb2])
            for g0 in range(0, 64, 8):
                pt, bpt = ptu.next()
                for gi in range(8):
                    g = g0 + gi
                    pe.op(lambda e, pt=pt, gi=gi, g=g, tb2=tb2: e.transpose(pt[:, gi, :], tb2[:, g, :],
                                                                           P.identb[:]), [btb2, P.bidb], [bpt])
                act.op(act_fn(U[:, g0:g0 + 8, kb * 128:(kb + 1) * 128], pt[:], AF.Copy), [bpt], [bU])
        P.barrier()

    with contextlib.ExitStack() as sth:
        hist = [P.sb([128, 2, G32, NKL], BF16, stack=sth) for _ in range(2)]
        bhist = Buf()
        with contextlib.ExitStack() as st3:
            Wz = P.sb([128, G32, 4, 128], BF16, stack=st3)
            bWz = Buf()
            WT = [P.sb([128, G32, 8, 16], BF16, stack=st3) for _ in range(2)]
            bWT = Buf()
            ptw = Ring(P, 2, [128, 4, 128], BF16, st3, psum=True)
            pvr = Ring(P, 3, [128, 2, G32, 8], F32, st3, psum=True)
            S4r = Ring(P, 4, [128, 3, G32], F32, st3)
            p1 = P.sb([128, 2, G32], F32, stack=st3)
            p2 = P.sb([128, 2, G32], F32, stack=st3)
            bp1, bp2 = Buf(), Buf()
            gx = [P.sb([128, 2 * G32], F32, stack=st3) for _ in range(2)]
            bgx = Buf()
            for d in range(2):
                pw, bpw = pws[d]
                cprod(WT[0], WT[1], bWT, bbs[d][0][:], bbs[d][1][:], bbs[d][2], pw, bpw,
                      (lambda s: 7 - s) if d == 0 else (lambda s: s))
                pool.op(lambda e: e.memset(Wz[:], 0.0), [], [bWz])
                W3 = (f3(WT[0]), f3(WT[1]))
                for g in range(G32):
                    pt, bpt = ptw.next()
                    for ri in range(2):
                        pe.op(lambda e, pt=pt, g=g, ri=ri: e.transpose(pt[:, ri, :], W3[ri][:, g, :], P.identb[:]),
                              [bWT, P.bidb], [bpt])
                    act.op(act_fn(Wz[:, g, 0:4:2, 0:64], pt[:, 0:2, 0:64], AF.Copy), [bpt], [bWz])
                    act.op(act_fn(Wz[:, g, 1:4:2, 64:128], pt[:, 0:2, 64:128], AF.Copy), [bpt], [bWz])
                S4, bS = S4r.next()
                if d == 0:
                    dve.op(lambda e, S4=S4: e.memset(S4[:], 0.0), [], [bS])
                else:
                    sp.dma(out=gx[0][:], in_=SAall.ap()[0:128, :], writes=[bgx])
                    sp.dma(out=gx[1][:], in_=SAall.ap()[128:256, :], writes=[bgx])
                    dve.op(ts_fn(gx[0][:], gx[0][:], flg[:, 0:1], None, ALU.mult), [bgx, bflg], [bgx])
                    S2v = S4[:, 0:2, :].rearrange("p r g -> p (r g)")
                    dve.op(stt_fn(S2v, gx[1][:], flg[:, 1:2], gx[0][:], ALU.mult, ALU.add), [bgx, bflg], [bS])
                    dve.op(lambda e, S4=S4: e.tensor_copy(out=S4[:, 2, :], in_=S4[:, 0, :]), [bS], [bS])
                a8 = A8[d]
                for wi in range(NWL):
                    w = wi if d == 0 else NWL - 1 - wi
                    pv, bpv = pvr.next()
                    for g in range(G32):
                        for ri in range(2):
                            pe.op(mm_fn(pv[:, ri, g, :], Wz[:, g, 2 * ri, :], U[:, g, 8 * w:8 * w + 8], True, False),
                                  [bWz, bU], [bpv])
                            pe.op(mm_fn(pv[:, ri, g, :], Wz[:, g, 2 * ri + 1, :], U[:, G32 + g, 8 * w:8 * w + 8],
                                        False, True), [bWz, bU], [bpv])
                    for ji in range(8):
                        jj = ji if d == 0 else 7 - ji
                        k = 8 * w + jj
                        act.op(act_fn(hist[d][:, :, :, k], S4[:, 0:2, :], AF.Copy), [bS], [bhist])
                        Sn, bSn = S4r.next()
                        dve.op(tt_fn(p1[:], a8[:, 0, :, :], S4[:, 0:2, :], ALU.mult), [bA8, bS], [bp1])
                        dve.op(tt_fn(p2[:], a8[:, 1, :, :], S4[:, 1:3, :], ALU.mult), [bA8, bS], [bp2])
                        dve.op(tt_fn(p1[:], p1[:], p2[:], ALU.add), [bp1, bp2], [bp1])
                        dve.op(tt_fn(Sn[:, 0:2, :], p1[:], pv[:, :, :, jj], ALU.add), [bp1, bpv], [bSn])
                        dve.op(lambda e, Sn=Sn: e.tensor_copy(out=Sn[:, 2, :], in_=Sn[:, 0, :]), [bSn], [bSn])
                        S4, bS = Sn, bSn
                if d == 0:
                    sp.dma(out=SAo.ap(), in_=S4[:, 0:2, :].rearrange("p r g -> p (r g)"), reads=[bS])
                    P.all_gather_pair(SAo, SAall)
            P.barrier()
        with contextlib.ExitStack() as st4:
            Z = P.sb([128, 8, D // 2], F32, stack=st4)
            bZ = Buf()
            M = P.sb([128, 64, 128], BF16, stack=st4)
            Rt = [[P.sb([128, G32, 128], BF16, stack=st4) for r in range(2)] for d in range(2)]
            bM, bR = Buf(), Buf()
            sp.dma(out=M[:].rearrange("p g c -> p (g c)"), in_=MS.ap(), writes=[bM])
            for d in range(2):
                for r in range(2):
                    sp.dma(out=Rt[d][r][:].rearrange("p g c -> p (g c)"),
                           in_=RS.ap()[:, (2 * d + r) * G32 * 128:(2 * d + r + 1) * G32 * 128], writes=[bR])
            p1r = Ring(P, 2, [128, 512], F32, st4, psum=True)
            p2r = [Ring(P, 1, [128, 512], F32, st4, psum=True) for _ in range(2)]
            pTr = Ring(P, 2, [128, 4, 128], F32, st4, psum=True)
            c1r = Ring(P, 2, [128, 512], F32, st4)
            ygr = Ring(P, 2, [128, 512], F32, st4)
            for kb in range(NKL // 128):
                ks = slice(kb * 128, (kb + 1) * 128)
                for hf in range(2):
                    r0 = 64 * hf
                    for q in range(G32 // 4):
                        P1, bP1 = p1r.next()
                        P2, bP2 = p2r[hf].next()
                        for gi in range(4):
                            g32 = 4 * q + gi
                            g = hf * G32 + g32
                            cs = slice(gi * 128, (gi + 1) * 128)
                            pe.op(mm_fn(P1[:, cs], M[:, g, :], U[:, g, ks], True, True), [bM, bU], [bP1])
                            seq = [(Rt[0][0], hist[0], 0), (Rt[0][1], hist[0], 1), (Rt[1][0], hist[1], 0),
                                   (Rt[1][1], hist[1], 1)]
                            for n, (Rm, hs, ri) in enumerate(seq):
                                pe.op(mm_fn(P2[:, cs], Rm[r0:r0 + 64, g32, :], hs[r0:r0 + 64, ri, g32, ks],
                                            n == 0, n == 3), [bR, bhist], [bP2])
                        c1, bc1 = c1r.next()
                        act.op(act_fn(c1[:], P1[:], AF.Copy), [bP1], [bc1])
                        yg, byg = ygr.next()
                        dve.op(tt_fn(yg[:], P2[:], c1[:], ALU.add), [bP2, bc1], [byg])
                        pT, bpT = pTr.next()
                        for gi in range(4):
                            pe.op(lambda e, pT=pT, gi=gi, yg=yg: e.transpose(pT[:, gi, :], yg[:, gi * 128:(gi + 1) * 128],
                                                                             P.identf), [byg, P.bcst], [bpT])
                        c0 = 16 * (4 * q)
                        zdst = Z[:, :, c0:c0 + 64].rearrange("p t (g c) -> p t g c", g=4)
                        zsrc = pT[:].rearrange("p g (t c) -> p t g c", t=8)
                        act.op(act_fn(zdst, zsrc, AF.Copy), [bpT], [bZ])
                    dst = ZS[kb * 1024:(kb + 1) * 1024, hf * 512:(hf + 1) * 512].rearrange("(p t) d -> p t d", t=8)
                    sp.dma(out=dst, in_=Z[:], reads=[bZ])


def build_fused(ncores=NCORES):
    P = Prog()
    P.ncores = ncores
    xw = P.inp("xw", [WIN, D])
    g0 = P.inp("g0", [1, D])
    w_in = P.inp("w_in", [D, 2560])
    cvec = P.inp("cvec", [128, 4, 34])
    qk_g = P.inp("qk_g", [128, 2])
    w_out = P.inp("w_out", [D, D])
    eb = P.inp("eb", [4, 128, 6, 384])
    cst_in = P.inp("cst", [128, 6, 128])
    gf = P.inp("gf", [2, D])
    w_r = P.inp("w_r", [2, D, 16])
    b_r = P.inp("b_r", [2, 16])
    wg = P.inp("wg", [2, NEXP, D, D])
    wu = P.inp("wu", [2, NEXP, D, D])
    wd = P.inp("wd", [2, NEXP, D, D])
    gn = P.inp("gn", [1, D])
    s5p = P.inp("s5p", [128, 2, 3, G32])
    s5B = P.inp("s5B", [128, 2, 2, G32, 16])
    s5C = P.inp("s5C", [128, 2, 2, G32, 16])
    dvec = P.inp("dvec", [1, D])
    masks = P.inp("masks", [128, 2, 128])
    flags = P.inp("flags", [128, 2])
    w_glu = P.inp("w_glu", [D, 2 * D])
    out = P.outp("out", [OWN, D])
    X1 = P.dram("X1", [OWN, D])
    X2 = P.dram("X2", [OWN, D])
    X3 = P.dram("X3", [OWN, D])
    HN = P.dram("HN", [OWN, D])
    ZS = P.dram("ZS", [OWN, D])
    AFF = P.dram("AFF", [OWN, NEXP])
    ATo = P.dram("ATo", [NEXP, OWN])
    ATall = P.dram("ATall", [2 * NEXP, OWN])
    SAo = P.dram("SAo", [128, 2 * G32])
    SAall = P.dram("SAall", [256, 2 * G32])
    MS = P.dram("MS", [128, 64 * 128], BF16)
    RS = P.dram("RS", [128, 4 * G32 * 128], BF16)
    Xc = P.dram("Xc", [NEXP * CAP, D], BF16)
    Yc = P.dram("Yc", [NEXP * CAP, D], BF16)
    load_consts(P, cst_in)
    if SPARSE:
        with contextlib.ExitStack() as zs:
            zt = P.sb([128, 8 * D], BF16, name="zeros", stack=zs)
            bzt = Buf()
            P.dve.op(lambda e: e.memset(zt[:], 0.0), [], [bzt])
            for i in range(NEXP * CAP // 1024):
                P.sp.dma(out=Xc.ap()[i * 1024:(i + 1) * 1024, :].rearrange("(p s) d -> p (s d)", p=128), in_=zt[:],
                         reads=[bzt])
                P.sp.dma(out=Yc.ap()[i * 1024:(i + 1) * 1024, :].rearrange("(p s) d -> p (s d)", p=128), in_=zt[:],
                         reads=[bzt])
            P.barrier()
    emit_phaseA(P, dict(xw=xw, g0=g0, w_in=w_in, cvec=cvec, qk_g=qk_g, w_out=w_out, gf=gf[0:1, :], w_r=w_r[0],
                        b_r=b_r[0:1, :], eb=eb, x1_out=X1.ap(), aff_out=AFF.ap(), affT_out=ATo.ap()))
    P.all_gather_pair(ATo, ATall)
    at_view = ATall.ap().rearrange("(r e) t -> e r t", r=2)
    emitB = emit_phaseB_sparse if SPARSE else emit_phaseB
    emitB(P, dict(x1=X1.ap(), affT_seq=at_view, aff_own=AFF.ap(), gf=gf[0:1, :], wg=wg[0], wu=wu[0], wd=wd[0],
                  x2_out=X2.ap(), gn=gn, hn_out=HN.ap(), xc=Xc.ap(), yc=Yc.ap()))
    emit_phaseC2(P, dict(s5p=s5p, s5B=s5B, s5C=s5C, masks=masks, flags=flags, hn=HN.ap(), zs_out=ZS.ap(),
                         sa_own=SAo, sa_all=SAall, ms=MS, rs=RS))
    emit_phaseD(P, dict(zs=ZS.ap(), hn=HN.ap(), dvec=dvec, x2=X2.ap(), w_glu=w_glu, gf=gf[1:2, :], w_r=w_r[1],
                        b_r=b_r[1:2, :], x3_out=X3.ap(), aff_out=AFF.ap(), affT_out=ATo.ap()))
    P.all_gather_pair(ATo, ATall)
    emitB(P, dict(x1=X3.ap(), affT_seq=at_view, aff_own=AFF.ap(), gf=gf[1:2, :], wg=wg[1], wu=wu[1], wd=wd[1],
                  x2_out=out, xc=Xc.ap(), yc=Yc.ap()))
    return P.finish()


def fused_inputs(c, x, mix_norm_even, w_in, conv_w, conv_b, conv_ln_g, conv_ln_b, q_norm, k_norm,
                 w_out, mix_norm_odd, ssm_lam_re, ssm_lam_im, ssm_log_dt, ssm_b_re, ssm_b_im,
                 ssm_c_re, ssm_c_im, ssm_d, w_glu, ffn_norm, w_router, b_router,
                 w_e_gate, w_e_up, w_e_down, shared):
    b, h = c // 2, c % 2
    cw = conv_w[0] if h == 0 else conv_w[0][::-1]
    cvec = np.zeros((128, 4, 34), np.float32)
    cvec[:, :, 0:31] = cw.T.reshape(4, 128, 31).transpose(1, 0, 2)
    cvec[:, :, 31] = conv_b[0].reshape(4, 128).T
    cvec[:, :, 32] = conv_ln_g[0].reshape(4, 128).T
    cvec[:, :, 33] = conv_ln_b[0].reshape(4, 128).T
    qk = np.stack([np.tile(q_norm[0], 2), np.tile(k_norm[0], 2)], axis=1).astype(np.float32)
    order = [0, 1] if h == 0 else [1, 0]

    def gp(a):
        a = a[order]
        return a.reshape(2, 2, G32, 64).transpose(1, 3, 0, 2).reshape(128, 2, G32)

    ldt = np.broadcast_to(ssm_log_dt[0][:, :, None], (2, 64, 64))
    s5p = np.ascontiguousarray(np.stack([gp(ssm_lam_re[0]), gp(ssm_lam_im[0]), gp(ldt)], axis=2))

    def gB(a):
        a = a[order]
        return a.reshape(2, 2, G32, 64, 16).transpose(1, 3, 0, 2, 4).reshape(128, 2, G32, 16)

    def gC(a):
        a = a[order]
        return a.reshape(2, 2, G32, 16, 64).transpose(1, 4, 0, 2, 3).reshape(128, 2, G32, 16)

    s5B = np.ascontiguousarray(np.stack([gB(ssm_b_re[0]), gB(ssm_b_im[0])], axis=2))
    s5C = np.ascontiguousarray(np.stack([gC(ssm_c_re[0]), gC(ssm_c_im[0])], axis=2))
    flags = np.zeros((128, 2), np.float32)
    flags[:, 1 - h] = 1.0
    m = dict(shared)
    m.update({
        "xw": local_view(x[b], h, WIN), "cvec": cvec, "qk_g": np.ascontiguousarray(qk),
        "s5p": s5p, "s5B": s5B, "s5C": s5C, "flags": flags,
    })
    return m


def kernel(x, mix_norm_even, w_in, conv_w, conv_b, conv_ln_g, conv_ln_b, q_norm, k_norm,
           w_out, mix_norm_odd, ssm_lam_re, ssm_lam_im, ssm_log_dt, ssm_b_re, ssm_b_im,
           ssm_c_re, ssm_c_im, ssm_d, w_glu, ffn_norm, w_router, b_router,
           w_e_gate, w_e_up, w_e_down):
    f = lambda a: np.ascontiguousarray(np.asarray(a, dtype=np.float32))
    args = [f(a) for a in (x, mix_norm_even, w_in, conv_w, conv_b, conv_ln_g, conv_ln_b, q_norm, k_norm,
                           w_out, mix_norm_odd, ssm_lam_re, ssm_lam_im, ssm_log_dt, ssm_b_re, ssm_b_im,
                           ssm_c_re, ssm_c_im, ssm_d, w_glu, ffn_norm, w_router, b_router,
                           w_e_gate, w_e_up, w_e_down)]
    (x, mix_norm_even, w_in, conv_w, conv_b, conv_ln_g, conv_ln_b, q_norm, k_norm,
     w_out, mix_norm_odd, ssm_lam_re, ssm_lam_im, ssm_log_dt, ssm_b_re, ssm_b_im,
     ssm_c_re, ssm_c_im, ssm_d, w_glu, ffn_norm, w_router, b_router, w_e_gate, w_e_up, w_e_down) = args
    shared = {
        "g0": np.ascontiguousarray(mix_norm_even[0][None, :]), "w_in": w_in[0], "w_out": w_out[0],
        "eb": host_eb(), "cst": host_consts(), "gf": ffn_norm, "w_r": w_router, "b_r": b_router,
        "wg": w_e_gate, "wu": w_e_up, "wd": w_e_down, "gn": np.ascontiguousarray(mix_norm_odd[0][None, :]),
        "dvec": np.ascontiguousarray(ssm_d[0][None, :]), "masks": host_masks(), "w_glu": w_glu[0],
    }
    nc = get_prog("F", build_fused)
    in_maps = [fused_inputs(c, *args, shared) for c in range(NCORES)]
    res = run_bass_kernel_spmd(nc, in_maps, core_ids=list(range(NCORES)))
    return from_local([r["out"] for r in res.results]).astype(np.float32)


BIGIDX = float(1 << 20)
I32 = mybir.dt.int32


def emit_phaseB_sparse(P, io):
    with contextlib.ExitStack() as ph:
        P.cur = ph
        _phaseB_sparse_body(P, io["x1"], io["affT_seq"], io["aff_own"], io["gf"], io["wg"], io["wu"], io["wd"],
                            io["x2_out"], io.get("gn"), io.get("hn_out"), io["xc"], io["yc"])
        P.barrier()
    P.cur = None


def _phaseB_sparse_body(P, x1, affT_seq, aff_own, gf, wg, wu, wd, x2_out, gn, hn_out, Xc, Yc):
    dve, act, pe, pool, sp = P.dve, P.act, P.pe, P.pool, P.sp
    NT = OWN // 128
    if gn is not None:
        gnb = P.sb([128, D], F32, name="gnb")
        bgnb = Buf()
        sp.dma(out=gnb[:], in_=bcast_rows(gn, 128), writes=[bgnb])
    gfb = P.sb([128, D], F32, name="gfb")
    bgfb = Buf()
    sp.dma(out=gfb[:], in_=bcast_rows(gf, 128), writes=[bgfb])
    gate = P.sb([128, NT, NEXP], F32, name="gate")
    bgate = Buf()
    idx = P.sb([128, NT, NEXP], I32, name="idx")
    bidx = Buf()
    if P.bcreg is None:
        P.bcreg = P.nc.gpsimd.alloc_register("bcreg")
        P.nc.gpsimd.reg_mov(P.bcreg, NEXP * CAP - 1)

    with contextlib.ExitStack() as st:
        pthr = P.ps([128, 512], F32, stack=st)
        bpthr = Buf()
        aT = P.sb([NEXP, SEQ], F32, stack=st)
        baT = Buf()
        sp.dma(out=aT[:].rearrange("e (r t) -> e r t", r=2), in_=affT_seq, writes=[baT])
        junk = P.sb([NEXP, SEQ], F32, stack=st)
        bjunk = Buf()
        lo = P.sb([NEXP, 1], F32, stack=st)
        blo = Buf()
        mid = P.sb([NEXP, 1], F32, stack=st)
        bmid = Buf()
        cnt = P.sb([NEXP, 1], F32, stack=st)
        bcnt = Buf()
        ge = P.sb([NEXP, 1], F32, stack=st)
        bge = Buf()
        dve.op(lambda e: e.memset(lo[:], 0.0), [], [blo])
        for it in range(36):
            wk = 2.0 ** (-(it + 1))
            dve.op(ts_fn(mid[:], lo[:], wk, None, ALU.add), [blo], [bmid])
            dve.op(lambda e: e.tensor_scalar(out=junk[:], in0=aT[:], scalar1=mid[:, 0:1], scalar2=0.0,
                                             op0=ALU.is_ge, op1=ALU.add, accum_out=cnt[:, 0:1]),
                   [baT, bmid], [bjunk, bcnt])
            dve.op(ts_fn(ge[:], cnt[:], CAP - 0.5, None, ALU.is_ge), [bcnt], [bge])
            dve.op(stt_fn(lo[:], ge[:], wk, lo[:], ALU.mult, ALU.add), [bge, blo], [blo])
        dthr = P.sb([NEXP, NEXP], F32, stack=st)
        bdthr = Buf()
        dve.op(ts_fn(dthr[:], P.cst[0:NEXP, 0, 0:NEXP], lo[:, 0:1], None, ALU.mult), [P.bcst, blo], [bdthr])
        pe.op(mm_fn(pthr[:, 0:NEXP], P.cst[0:NEXP, 3, :], dthr[:], True, True), [P.bcst, bdthr], [bpthr])
        thrb = P.sb([128, NEXP], F32, stack=st)
        bthrb = Buf()
        act.op(act_fn(thrb[:], pthr[:, 0:NEXP], AF.Copy), [bpthr], [bthrb])
        ao = P.sb([128, NT, NEXP], F32, stack=st)
        bao = Buf()
        sp.dma(out=ao[:], in_=aff_own.rearrange("(t p) e -> p t e", p=128), writes=[bao])
        msk = P.sb([128, NT, NEXP], F32, stack=st)
        bmsk = Buf()

        def bct(t):
            a = t[:]
            return bass.AP(a.tensor, a.offset, [list(a.ap[0]), [0, NT], [1, NEXP]])

        dve.op(tt_fn(msk[:], ao[:], bct(thrb), ALU.is_ge), [bao, bthrb], [bmsk])
        ppos = P.ps([128, 512], F32, stack=st)
        bppos = Buf()
        pcnt = P.ps([128, 512], F32, stack=st)
        bpcnt = Buf()
        m2 = msk[:].rearrange("p t e -> p (t e)")
        pe.op(mm_fn(ppos[:, 0:NT * NEXP], P.cst[:, 4, :], m2, True, True), [P.bcst, bmsk], [bppos])
        pe.op(mm_fn(pcnt[:, 0:NT * NEXP], P.cst[:, 3, :], m2, True, True), [P.bcst, bmsk], [bpcnt])
        csb = P.sb([128, NT, NEXP], F32, stack=st)
        bcsb = Buf()
        act.op(act_fn(csb[:].rearrange("p t e -> p (t e)"), pcnt[:, 0:NT * NEXP], AF.Copy), [bpcnt], [bcsb])
        off = P.sb([128, NT, NEXP], F32, stack=st)
        boff = Buf()
        dve.op(lambda e: e.memset(off[:, 0, :], 0.0), [], [boff])
        for i in range(1, NT):
            dve.op(tt_fn(off[:, i, :], off[:, i - 1, :], csb[:, i - 1, :], ALU.add), [boff, bcsb], [boff])
        pos = P.sb([128, NT, NEXP], F32, stack=st)
        bpos = Buf()
        dve.op(tt_fn(pos[:].rearrange("p t e -> p (t e)"), ppos[:, 0:NT * NEXP],
                     off[:].rearrange("p t e -> p (t e)"), ALU.add), [bppos, boff], [bpos])
        ok = P.sb([128, NT, NEXP], F32, stack=st)
        bok = Buf()
        dve.op(ts_fn(ok[:], pos[:], CAP - 0.5, None, ALU.is_lt), [bpos], [bok])
        dve.op(tt_fn(msk[:], msk[:], ok[:], ALU.mult), [bmsk, bok], [bmsk])
        dve.op(tt_fn(gate[:], msk[:], ao[:], ALU.mult), [bmsk, bao], [bgate])
        ebase = P.cst[:, 5, 0:NEXP]
        eb_bc = bass.AP(ebase.tensor, ebase.offset, [list(ebase.ap[0]), [0, NT], [1, NEXP]])
        dve.op(tt_fn(pos[:], pos[:], eb_bc, ALU.add), [bpos, P.bcst], [bpos])
        dve.op(ts_fn(pos[:], pos[:], -BIGIDX, None, ALU.add), [bpos], [bpos])
        dve.op(tt_fn(pos[:], pos[:], msk[:], ALU.mult), [bpos, bmsk], [bpos])
        dve.op(ts_fn(pos[:], pos[:], BIGIDX, None, ALU.add), [bpos], [bpos])
        dve.op(lambda e: e.tensor_copy(out=idx[:], in_=pos[:]), [bpos], [bidx])
        P.barrier()

    def indirect(out, out_off, in_, in_off, reads, writes):
        s_ = pool
        if s_.dsems is None:
            s_.dsems = [P.newsem("d%d_%s" % (i, s_.name)) for i in range(s_.nslots)]
        j = s_.dn
        slot = j % s_.nslots
        key = (s_, slot)
        prev = 16 * (j // s_.nslots)
        if prev > 0:
            s_._wait((key, s_.dsems[slot], prev))
        s_._deps(reads, writes, True)
        ins = s_.eng.indirect_dma_start(out=out, out_offset=out_off, in_=in_, in_offset=in_off,
                                        bounds_check=P.bcreg, oob_is_err=False)
        ins.then_inc(s_.dsems[slot], 16)
        s_.dn += 1
        s_._mark((key, s_.dsems[slot], prev + 16), reads, writes)

    with contextlib.ExitStack() as st:
        acc = P.sb([128, NT, D], F32, stack=st)
        bacc = [Buf() for _ in range(NT)]
        bXc = Buf()
        bYc = [Buf() for _ in range(NEXP)]
        small = Ring(P, 8, [128, 1], F32, st)
        with contextlib.ExitStack() as st1:
            junk = Ring(P, 1, [128, D], F32, st1)
            hnr = Ring(P, 3, [128, D], BF16, st1)
            for t in range(NT):
                sp.dma(out=acc[:, t, :], in_=x1[t * 128:(t + 1) * 128, :], writes=[bacc[t]])
                rs, br = rms_rstd(P, acc[:, t, :], bacc[t], D, junk, small)
                hn, bhn = hnr.next()
                dve.op(stt_fn(hn[:], acc[:, t, :], rs[:, 0:1], gfb[:], ALU.mult, ALU.mult), [bacc[t], br, bgfb], [bhn])
                for e in range(NEXP):
                    indirect(Xc[:, :], bass.IndirectOffsetOnAxis(ap=idx[:, t, e:e + 1], axis=0), hn[:, :], None,
                             [bhn, bidx], [bXc])
            P.barrier()
        wgr = Ring(P, 2, [128, 8, D], BF16, st)
        wur = Ring(P, 2, [128, 8, D], BF16, st)
        wdr = Ring(P, 1, [128, 8, D], BF16, st)
        xsr = Ring(P, 1, [128, 4, D], BF16, st)
        xTr = Ring(P, 1, [128, 8, CAP], BF16, st)
        atr = Ring(P, 1, [128, 8, CAP], BF16, st)
        ysr = Ring(P, 1, [128, 4, D], BF16, st)
        sgr = Ring(P, 1, [128, 512], F32, st)
        ybr = Ring(P, 3, [128, D], BF16, st)
        ptr = Ring(P, 2, [128, 8, 128], BF16, st, psum=True)
        pgr = Ring(P, 2, [128, 512], F32, st, psum=True)
        pur = Ring(P, 2, [128, 512], F32, st, psum=True)
        pyr = Ring(P, 2, [128, 512], F32, st, psum=True)
        for yb_, byb_ in zip(ybr.t, ybr.b):
            dve.op(lambda e, yb_=yb_: e.memset(yb_[:], 0.0), [], [byb_])

        def load_w(e, which=((0, 1, 2))):
            wts = []
            for src, ring in [((wg, wgr), (wu, wur), (wd, wdr))[i_] for i_ in which]:
                w, bw = ring.next()
                sv = src[e].rearrange("(k p) f -> p k f", p=128)
                pool.dma(out=w[:, 0:4, :], in_=sv[:, 0:4, :], writes=[bw])
                pool.dma(out=w[:, 4:8, :], in_=sv[:, 4:8, :], writes=[bw])
                wts.append((w, bw))
            return wts

        def gather_back(e):
            for t in range(NT):
                yb, byb = ybr.next()
                indirect(yb[:, :], None, Yc[:, :], bass.IndirectOffsetOnAxis(ap=idx[:, t, e:e + 1], axis=0),
                         [bYc[e], bidx], [byb])
                dve.op(stt_fn(acc[:, t, :], yb[:], gate[:, t, e:e + 1], acc[:, t, :], ALU.mult, ALU.add),
                       [byb, bgate, bacc[t]], [bacc[t]])

        nxt = load_w(0, (0, 1))
        for e in range(NEXP):
            (Wg, bWg), (Wu, bWu) = nxt
            ((Wd, bWd),) = load_w(e, (2,))
            if e + 1 < NEXP:
                nxt = load_w(e + 1, (0, 1))
            xs, bxs = xsr.next()
            sp.dma(out=xs[:], in_=Xc[e * CAP:(e + 1) * CAP, :].rearrange("(s p) d -> p s d", p=128),
                   reads=[bXc], writes=[bxs])
            xT, bxT = xTr.next()
            for sl in range(4):
                pt, bpt = ptr.next()
                for k in range(8):
                    pe.op(lambda en, pt=pt, k=k, xs=xs, sl=sl: en.transpose(pt[:, k, :], xs[:, sl, k * 128:(k + 1) * 128],
                                                                           P.identb[:]), [bxs, P.bidb], [bpt])
                act.op(act_fn(xT[:, :, sl * 128:(sl + 1) * 128], pt[:], AF.Copy), [bpt], [bxT])
            AT, bAT = atr.next()
            for fc in range(8):
                pg, bpg = pgr.next()
                pu, bpu = pur.next()
                for k in range(8):
                    pe.op(mm_fn(pg[:], Wg[:, k, fc * 128:(fc + 1) * 128], xT[:, k, :], k == 0, k == 7), [bWg, bxT], [bpg])
                for k in range(8):
                    pe.op(mm_fn(pu[:], Wu[:, k, fc * 128:(fc + 1) * 128], xT[:, k, :], k == 0, k == 7), [bWu, bxT], [bpu])
                sg, bsg = sgr.next()
                act.op(act_fn(sg[:], pg[:], AF.Silu), [bpg], [bsg])
                dve.op(tt_fn(AT[:, fc, :], sg[:], pu[:], ALU.mult), [bsg, bpu], [bAT])
            ys, bys = ysr.next()
            for sl in range(4):
                for hc in range(2):
                    py, bpy = pyr.next()
                    for fc in range(8):
                        pe.op(mm_fn(py[:], AT[:, fc, sl * 128:(sl + 1) * 128], Wd[:, fc, hc * 512:(hc + 1) * 512],
                                    fc == 0, fc == 7), [bAT, bWd], [bpy])
                    act.op(act_fn(ys[:, sl, hc * 512:(hc + 1) * 512], py[:], AF.Copy), [bpy], [bys])
            sp.dma(out=Yc[e * CAP:(e + 1) * CAP, :].rearrange("(s p) d -> p s d", p=128), in_=ys[:],
                   reads=[bys], writes=[bYc[e]])
            if e >= 1:
                gather_back(e - 1)
        gather_back(NEXP - 1)
        hor = Ring(P, 1, [128, D], F32, st)
        junk = hor
        for t in range(NT):
            sp.dma(out=x2_out[t * 128:(t + 1) * 128, :], in_=acc[:, t, :], reads=[bacc[t]])
            if gn is not None:
                rs, br = rms_rstd(P, acc[:, t, :], bacc[t], D, junk, small)
                ho, bho = hor.next()
                dve.op(stt_fn(ho[:], acc[:, t, :], rs[:, 0:1], gnb[:], ALU.mult, ALU.mult),
                       [bacc[t], br, bgnb], [bho])
                sp.dma(out=hn_out[t * 128:(t + 1) * 128, :], in_=ho[:], reads=[bho])
```

```python
import numpy as np
import contextlib
import concourse.bass as bass
import concourse.mybir as mybir
from concourse.bass_utils import run_bass_kernel_spmd

F32 = mybir.dt.float32
BF16 = mybir.dt.bfloat16
ALU = mybir.AluOpType
AF = mybir.ActivationFunctionType
AX = mybir.AxisListType

NCORES = 8
D = 1024
SEQ = 4096
OWN = 2048
WIN = 3072
EPS = 1e-6
DEBUG = False
SPARSE = True


class Buf:
    __slots__ = ("name", "w", "r")

    def __init__(self, name=""):
        self.name = name
        self.w = None
        self.r = {}


class Stream:
    def __init__(self, P, name, eng):
        self.P = P
        self.name = name
        self.eng = eng
        self.sem = P.newsem("c_" + name)
        self.cnt = 0
        self.seen = {}
        self.nslots = 8
        self.dsems = None
        self.dn = 0
        self.selfsync = name != "pe"

    def _wait(self, tok):
        key, sem, val = tok
        if self.seen.get(key, 0) < val:
            self.eng.wait_ge(sem, val)
            self.seen[key] = val

    def _deps(self, reads, writes, dma):
        toks = []
        for b in reads:
            if b.w is not None:
                toks.append(b.w)
        for b in writes:
            if b.w is not None:
                toks.append(b.w)
            toks.extend(b.r.values())
        for t in toks:
            if dma or t[0] is not self or self.selfsync:
                self._wait(t)

    def _mark(self, tok, reads, writes):
        for b in reads:
            b.r[tok[0]] = tok
        for b in writes:
            b.w = tok
            b.r = {}

    def op(self, fn, reads=(), writes=()):
        if self.P.stopped:
            return None
        self._deps(reads, writes, False)
        ins = fn(self.eng)
        self.cnt += 1
        ins.then_inc(self.sem, 1)
        self._mark((self, self.sem, self.cnt), reads, writes)
        return ins

    def dma(self, out, in_, reads=(), writes=(), **kw):
        if self.P.stopped:
            return None
        if self.dsems is None:
            self.dsems = [self.P.newsem("d%d_%s" % (i, self.name)) for i in range(self.nslots)]
        j = self.dn
        slot = j % self.nslots
        key = (self, slot)
        prev = 16 * (j // self.nslots)
        if prev > 0:
            self._wait((key, self.dsems[slot], prev))
        self._deps(reads, writes, True)
        ins = self.eng.dma_start(out=out, in_=in_, **kw)
        ins.then_inc(self.dsems[slot], 16)
        self.dn += 1
        self._mark((key, self.dsems[slot], prev + 16), reads, writes)
        return ins


class Prog:
    def __init__(self):
        self.nc = bass.Bass("TRN2", target_bir_lowering=False)
        self.stack = contextlib.ExitStack()
        self._n = 0
        self.stopped = False
        self.cur = None
        self.ccsem = None
        self.ccn = 0
        self.ncores = NCORES
        self.bcreg = None
        nc = self.nc
        self.pe = Stream(self, "pe", nc.tensor)
        self.act = Stream(self, "act", nc.scalar)
        self.dve = Stream(self, "dve", nc.vector)
        self.pool = Stream(self, "pool", nc.gpsimd)
        self.sp = Stream(self, "sp", nc.sync)
        self.streams = [self.pe, self.act, self.dve, self.pool, self.sp]

    def newsem(self, name):
        return self.stack.enter_context(self.nc.semaphore(name))

    def inp(self, name, shape, dt=F32):
        return self.nc.dram_tensor(name, list(shape), dt, kind="ExternalInput").ap()

    def outp(self, name, shape, dt=F32):
        return self.nc.dram_tensor(name, list(shape), dt, kind="ExternalOutput").ap()

    def sb(self, shape, dt=F32, name=None, stack=None):
        self._n += 1
        name = "s%d_%s" % (self._n, name or "t")
        return (stack or self.cur or self.stack).enter_context(self.nc.sbuf_tensor(name, list(shape), dt))

    def ps(self, shape, dt=F32, name=None, stack=None):
        self._n += 1
        name = "p%d_%s" % (self._n, name or "t")
        return (stack or self.cur or self.stack).enter_context(self.nc.psum_tensor(name, list(shape), dt))

    def barrier(self):
        toks = []
        for s in self.streams:
            if s.cnt:
                toks.append((s, s.sem, s.cnt))
            if s.dsems is not None:
                for slot in range(s.nslots):
                    n = (s.dn - slot + s.nslots - 1) // s.nslots
                    if n > 0:
                        toks.append(((s, slot), s.dsems[slot], 16 * n))
        for s in self.streams:
            for t in toks:
                if t[0] is not s:
                    s._wait(t)

    def dram(self, name, shape, dt=F32):
        return self.nc.dram_tensor(name, list(shape), dt)

    def all_gather_pair(self, src, dst):
        if self.ccsem is None:
            self.ccsem = self.newsem("ccsem")
            self.ccdummy = self.sb([128, 1], F32, name="ccdummy", stack=self.stack)
            self.bccd = Buf()
        self.barrier()
        groups = [[2 * i, 2 * i + 1] for i in range(self.ncores // 2)]
        self.ccn += 1
        n = self.ccn

        def fn(e):
            e.collective_compute("AllGather", ALU.bypass, replica_groups=groups,
                                 ins=[src.ap().opt()], outs=[dst.ap().opt()]).then_inc(self.ccsem, 1)
            e.wait_ge(self.ccsem, n)
            return e.memset(self.ccdummy[:], 0.0)
        self.pool.op(fn, [], [self.bccd])
        self.barrier()

    def finish(self):
        self.barrier()
        self.stack.close()
        return self.nc


def bcast_rows(ap, nparts):
    n = ap.shape[-1]
    return bass.AP(ap.tensor, ap.offset, [[0, nparts], [1, n]])


class Ring:
    def __init__(self, P, n, shape, dt, stack, psum=False):
        alloc = P.ps if psum else P.sb
        self.t = [alloc(shape, dt, stack=stack) for _ in range(n)]
        self.b = [Buf() for _ in range(n)]
        self.i = -1

    def next(self):
        self.i = (self.i + 1) % len(self.t)
        return self.t[self.i], self.b[self.i]


def act_fn(out, in_, func, **kw):
    return lambda e: e.activation(out=out, in_=in_, func=func, **kw)


def mm_fn(out, lhsT, rhs, start, stop):
    return lambda e: e.matmul(out, lhsT, rhs, start=start, stop=stop)


def tt_fn(out, in0, in1, op):
    return lambda e: e.tensor_tensor(out=out, in0=in0, in1=in1, op=op)


def ts_fn(out, in0, s1, s2, op0, op1=None):
    if op1 is None:
        return lambda e: e.tensor_scalar(out=out, in0=in0, scalar1=s1, scalar2=None, op0=op0)
    return lambda e: e.tensor_scalar(out=out, in0=in0, scalar1=s1, scalar2=s2, op0=op0, op1=op1)


def stt_fn(out, in0, scalar, in1, op0, op1):
    return lambda e: e.scalar_tensor_tensor(out=out, in0=in0, scalar=scalar, in1=in1, op0=op0, op1=op1)


ATT_CFG = (1, 4, 16)


def vtile_list():
    tiles = []
    for ci, d in enumerate(ATT_CFG):
        nb = OWN // (128 * d)
        for c in range(d):
            for j in range(nb + 1):
                i0 = 0 if j == 0 else 128 * j - 64
                tiles.append((ci, d, c, j, c + d * i0))
    return tiles


def rms_rstd(P, x_ap, xb, n, ring_junk, ring_small, pbuf_extra=()):
    junk, bj = ring_junk.next()
    ss, bs = ring_small.next()
    P.act.op(act_fn(junk[:, 0:n], x_ap, AF.Square, accum_out=ss[:, 0:1]), [xb], [bj, bs])
    sd, bd = ring_small.next()
    P.act.op(act_fn(sd[:, 0:1], ss[:, 0:1], AF.Sqrt, scale=1.0 / n, bias=P.eps_t[:, 0:1]), [bs, P.beps], [bd])
    rs, br = ring_small.next()
    P.dve.op(lambda e: e.reciprocal(out=rs[:, 0:1], in_=sd[:, 0:1]), [bd], [br])
    return rs, br


def load_consts(P, cst_in):
    cst = P.sb([128, 6, 128], F32, name="cst")
    P.bcst = Buf("cst")
    P.sp.dma(out=cst[:], in_=cst_in, writes=[P.bcst])
    P.identf = cst[:, 0, :]
    P.ones512 = cst[:, 1, :]
    P.ones64 = cst[:, 2, :]
    P.cst = cst
    idb = P.sb([128, 128], BF16, name="identb")
    P.bidb = Buf("identb")
    P.dve.op(lambda e: e.tensor_copy(out=idb[:], in_=cst[:, 0, :]), [P.bcst], [P.bidb])
    P.identb = idb
    eps_t = P.sb([128, 1], F32, name="eps")
    P.beps = Buf("eps")
    P.dve.op(lambda e: e.memset(eps_t[:], EPS), [], [P.beps])
    P.eps_t = eps_t


def router_tile(P, x1t, bx1, gfb, bgfb, wr, bwr, brb, bbrb, rings, aff_out, affT_out, tt, hf_out=None):
    rs, br = rms_rstd(P, x1t[:], bx1, D, rings["junk"], rings["small"])
    hf, bhf = rings["hf"].next()
    P.dve.op(stt_fn(hf[:], x1t[:], rs[:, 0:1], gfb[:], ALU.mult, ALU.mult), [bx1, br, bgfb], [bhf])
    ptf, bptf = rings["ptf"].next()
    for k in range(8):
        P.pe.op(lambda e, k=k: e.transpose(ptf[:, k, :], hf[:, k * 128:(k + 1) * 128], P.identf),
                [bhf, P.bcst], [bptf])
    hfT, bhfT = rings["hfT"].next()
    P.act.op(act_fn(hfT[:], ptf[:], AF.Copy), [bptf], [bhfT])
    plog, bplog = rings["plog"].next()
    for k in range(8):
        P.pe.op(mm_fn(plog[:, 0:16], hfT[:, k, :], wr[:, k, :], k == 0, k == 7), [bhfT, bwr], [bplog])
    lg, blg = rings["sm16"].next()
    P.dve.op(tt_fn(lg[:], plog[:, 0:16], brb[:], ALU.add), [bplog, bbrb], [blg])
    mx, bmx = rings["small"].next()
    P.dve.op(lambda e: e.reduce_max(out=mx[:, 0:1], in_=lg[:], axis=AX.X, negate=True), [blg], [bmx])
    ex, bex = rings["sm16"].next()
    sm, bsm = rings["small"].next()
    P.act.op(act_fn(ex[:], lg[:], AF.Exp, bias=mx[:, 0:1], scale=1.0, accum_out=sm[:, 0:1]), [blg, bmx], [bex, bsm])
    rsm, brsm = rings["small"].next()
    P.dve.op(lambda e: e.reciprocal(out=rsm[:, 0:1], in_=sm[:, 0:1]), [bsm], [brsm])
    af, baf = rings["sm16"].next()
    P.dve.op(ts_fn(af[:], ex[:], rsm[:, 0:1], None, ALU.mult), [bex, brsm], [baf])
    P.sp.dma(out=aff_out[tt * 128:(tt + 1) * 128, :], in_=af[:], reads=[baf])
    pat, bpat = rings["pat"].next()
    P.pe.op(lambda e: e.transpose(pat[0:16, 0:128], af[:], P.identf), [baf, P.bcst], [bpat])
    aT, baT = rings["aT"].next()
    P.act.op(act_fn(aT[:], pat[0:16, 0:128], AF.Copy), [bpat], [baT])
    P.sp.dma(out=affT_out[:, tt * 128:(tt + 1) * 128], in_=aT[:], reads=[baT])
    return hf, bhf


def emit_phaseA(P, io):
    xw, g0, w_in, cvec, qk_g, w_out = io["xw"], io["g0"], io["w_in"], io["cvec"], io["qk_g"], io["w_out"]
    gf, w_r, b_r, eb = io["gf"], io["w_r"], io["b_r"], io["eb"]
    x1_out, aff_out, affT_out = io["x1_out"], io["aff_out"], io["affT_out"]
    dbg = None
    with contextlib.ExitStack() as ph:
        P.cur = ph
        _phaseA_body(P, xw, g0, w_in, cvec, qk_g, w_out, gf, w_r, b_r, eb, x1_out, aff_out, affT_out)
        P.barrier()
    P.cur = None


def _phaseA_body(P, xw, g0, w_in, cvec, qk_g, w_out, gf, w_r, b_r, eb, x1_out, aff_out, affT_out):
    w_in_v = w_in.rearrange("(k p) e -> p k e", p=128)
    w_out_v = w_out.rearrange("(k p) e -> p k e", p=128)
    w_r_v = w_r.rearrange("(k p) e -> p k e", p=128)

    hT = P.sb([128, 8, WIN], BF16, name="hT")
    bhT = [Buf("hT%d" % i) for i in range(WIN // 128)]
    mixT = P.sb([128, 8, OWN], BF16, name="mixT")
    bmix = [Buf("mix%d" % i) for i in range(8)]
    gb = P.sb([128, D], F32, name="gb")
    bgb = Buf("gb")
    P.sp.dma(out=gb[:], in_=bcast_rows(g0, 128), writes=[bgb])
    cv = P.sb([128, 4, 34], F32, name="cv")
    bcv = Buf("cv")
    P.sp.dma(out=cv[:], in_=cvec, writes=[bcv])
    qkg = P.sb([128, 2], F32, name="qkg")
    bqkg = Buf("qkg")
    P.sp.dma(out=qkg[:], in_=qk_g, writes=[bqkg])

    with contextlib.ExitStack() as st:
        xr = Ring(P, 2, [128, D], F32, st)
        junk = Ring(P, 1, [128, D], F32, st)
        small = Ring(P, 8, [128, 1], F32, st)
        hnr = Ring(P, 2, [128, D], BF16, st)
        ptr = Ring(P, 2, [128, 8, 128], BF16, st, psum=True)
        for tt in range(WIN // 128):
            xt, bx = xr.next()
            P.sp.dma(out=xt[:], in_=xw[tt * 128:(tt + 1) * 128, :], writes=[bx])
            rs, br = rms_rstd(P, xt[:], bx, D, junk, small)
            hn, bhn = hnr.next()
            P.dve.op(stt_fn(hn[:], xt[:], rs[:, 0:1], gb[:], ALU.mult, ALU.mult), [bx, br, bgb], [bhn])
            pt, bpt = ptr.next()
            for k in range(8):
                P.pe.op(lambda e, k=k: e.transpose(pt[:, k, :], hn[:, k * 128:(k + 1) * 128], P.identb[:]),
                        [bhn, P.bidb], [bpt])
            P.act.op(act_fn(hT[:, :, tt * 128:(tt + 1) * 128], pt[:], AF.Copy), [bpt], [bhT[tt]])
        P.barrier()

    def hT_bufs(t0, n):
        return bhT[t0 // 128:(t0 + n + 127) // 128]

    NCV = OWN + 128
    with contextlib.ExitStack() as st:
        hglu = P.sb([128, 4, 15 + NCV], BF16, stack=st)
        bhg = [Buf() for _ in range(4)]
        diag = P.sb([128, 4, 31, 128], BF16, stack=st)
        bdiag = [Buf() for _ in range(4)]
        wcv = Ring(P, 2, [128, 8, 256], BF16, st)
        pmr = Ring(P, 2, [128, 512], F32, st, psum=True)
        pgr = Ring(P, 2, [128, 512], F32, st, psum=True)
        pst = Ring(P, 2, [128, 512], F32, st, psum=True)
        sgr = Ring(P, 2, [128, 512], F32, st)
        vbuf = [Ring(P, 2, [128, 512], F32, st) for _ in range(4)]
        sqr = [Ring(P, 2, [128, 512], F32, st) for _ in range(4)]
        tmp = Ring(P, 10, [128, 512], F32, st)
        zr = Ring(P, 2, [128, 512], F32, st)
        for cc in range(4):
            P.dve.op(lambda e, cc=cc: e.memset(hglu[:, cc, 0:15], 0.0), [], [bhg[cc]])
            for k in range(31):
                P.pool.op(ts_fn(diag[:, cc, k, :], P.identf, cv[:, cc, k:k + 1], None, ALU.mult),
                          [P.bcst, bcv], [bdiag[cc]])
        ntiles = [(i * 512, 512) for i in range(4)] + [(2048, 128)]
        for cc in range(4):
            w, bw = wcv.next()
            P.pool.dma(out=w[:, :, 0:128], in_=w_in_v[:, :, cc * 128:(cc + 1) * 128], writes=[bw])
            P.pool.dma(out=w[:, :, 128:256], in_=w_in_v[:, :, 512 + cc * 128:512 + (cc + 1) * 128], writes=[bw])
            for (t0, n) in ntiles:
                pm, bpm = pmr.next()
                pg, bpg = pgr.next()
                for k in range(8):
                    P.pe.op(mm_fn(pm[:, 0:n], w[:, k, 0:128], hT[:, k, t0:t0 + n], k == 0, k == 7),
                            [bw] + hT_bufs(t0, n), [bpm])
                for k in range(8):
                    P.pe.op(mm_fn(pg[:, 0:n], w[:, k, 128:256], hT[:, k, t0:t0 + n], k == 0, k == 7),
                            [bw] + hT_bufs(t0, n), [bpg])
                sg, bsg = sgr.next()
                P.act.op(act_fn(sg[:, 0:n], pg[:, 0:n], AF.Sigmoid), [bpg], [bsg])
                P.dve.op(tt_fn(hglu[:, cc, 15 + t0:15 + t0 + n], pm[:, 0:n], sg[:, 0:n], ALU.mult),
                         [bpm, bsg], [bhg[cc]])
        for nt in range(4):
            t0 = nt * 512
            vb = []
            for cc in range(4):
                pm, bpm = pmr.next()
                for k in range(31):
                    P.pe.op(mm_fn(pm[:], diag[:, cc, k, :], hglu[:, cc, t0 + k:t0 + k + 512], k == 0, k == 30),
                            [bdiag[cc], bhg[cc]], [bpm])
                v, bv = vbuf[cc].next()
                sq, bsq = sqr[cc].next()
                P.act.op(act_fn(v[:], pm[:], AF.Identity, bias=cv[:, cc, 31:32], scale=1.0), [bpm, bcv], [bv])
                P.act.op(act_fn(sq[:], pm[:], AF.Square, bias=cv[:, cc, 31:32], scale=1.0), [bpm, bcv], [bsq])
                vb.append((v, bv, sq, bsq))
            pmean, bpmean = pst.next()
            pex2, bpex2 = pst.next()
            for cc in range(4):
                P.pe.op(mm_fn(pmean[:], P.ones512, vb[cc][0][:], cc == 0, cc == 3), [P.bcst, vb[cc][1]], [bpmean])
            for cc in range(4):
                P.pe.op(mm_fn(pex2[:], P.ones512, vb[cc][2][:], cc == 0, cc == 3), [P.bcst, vb[cc][3]], [bpex2])
            mean, bmean = tmp.next()
            P.act.op(act_fn(mean[:], pmean[:], AF.Copy), [bpmean], [bmean])
            m2, bm2 = tmp.next()
            P.dve.op(tt_fn(m2[:], mean[:], mean[:], ALU.mult), [bmean], [bm2])
            var, bvar = tmp.next()
            P.dve.op(tt_fn(var[:], pex2[:], m2[:], ALU.subtract), [bpex2, bm2], [bvar])
            sd, bsd = tmp.next()
            P.act.op(act_fn(sd[:], var[:], AF.Sqrt, bias=P.eps_t[:, 0:1], scale=1.0), [bvar, P.beps], [bsd])
            rstd, brstd = tmp.next()
            P.dve.op(lambda e: e.reciprocal(out=rstd[:], in_=sd[:]), [bsd], [brstd])
            for cc in range(4):
                v, bv, sq, bsq = vb[cc]
                z, bz = zr.next()
                P.dve.op(tt_fn(z[:], v[:], mean[:], ALU.subtract), [bv, bmean], [bz])
                P.dve.op(tt_fn(z[:], z[:], rstd[:], ALU.mult), [bz, brstd], [bz])
                P.act.op(act_fn(mixT[:, cc, t0:t0 + 512], z[:], AF.Silu, scale=cv[:, cc, 32:33],
                                bias=cv[:, cc, 33:34]), [bz, bcv], [bmix[cc]])
        P.barrier()

    tiles = vtile_list()
    NT = len(tiles)
    tindex = {(ci, c, j): i for i, (ci, d, c, j, s) in enumerate(tiles)}
    with contextlib.ExitStack() as st:
        wq = Ring(P, 2, [128, 8, 384], BF16, st)
        ebr = Ring(P, 2, [128, 6, 384], F32, st)
        qT = P.sb([128, OWN], BF16, stack=st)
        bqT = Buf()
        kT = P.sb([128, WIN], BF16, stack=st)
        bkT = Buf()
        Vt = P.sb([128, NT, 256], BF16, stack=st)
        bVt = [Buf() for _ in range(NT)]
        bones = Buf()
        acc = P.sb([128, 2, OWN], F32, stack=st)
        bacc = [Buf(), Buf()]
        pmr = Ring(P, 2, [128, 512], F32, st, psum=True)
        pst = Ring(P, 1, [128, 512], F32, st, psum=True)
        psr = Ring(P, 3, [128, 512], F32, st, psum=True)
        por = Ring(P, 2, [128, 512], F32, st, psum=True)
        qfr = Ring(P, 2, [128, 512], F32, st)
        sqr = Ring(P, 2, [128, 512], F32, st)
        tmp = Ring(P, 4, [128, 512], F32, st)
        pex = Ring(P, 3, [128, 256], F32, st)
        pTr = Ring(P, 4, [128, 256], BF16, st)
        rzr = Ring(P, 1, [64, OWN], F32, st)
        Vt4 = Vt[:].rearrange("p t (s c) -> p t s c", s=4)
        P.pool.op(lambda e: e.memset(Vt4[:, :, 1, :], 1.0), [], [bones])
        P.pool.op(lambda e: e.memset(Vt4[:, :, 3, :], 1.0), [], [bones])
        ps_slot = [0]
        po_slot = [0]
        pv_slot = [0]
        psb = [[Buf(), Buf()], [Buf(), Buf()]]
        pob = [[Buf() for _ in range(4)] for _ in range(2)]
        pvb = [Buf() for _ in range(4)]
        for hp in range(4):
            w, bw = wq.next()
            for i, base in enumerate((1024, 1536, 2048)):
                P.pool.dma(out=w[:, :, i * 128:(i + 1) * 128],
                           in_=w_in_v[:, :, base + hp * 128:base + (hp + 1) * 128], writes=[bw])
            ebt, bebt = ebr.next()
            P.sp.dma(out=ebt[:], in_=eb[hp], writes=[bebt])
            for which, ntok, dst, bdst, gcol in ((0, OWN, qT, bqT, 0), (1, WIN, kT, bkT, 1)):
                for nt in range(ntok // 512):
                    t0 = nt * 512
                    pm, bpm = pmr.next()
                    for k in range(8):
                        P.pe.op(mm_fn(pm[:], w[:, k, which * 128:(which + 1) * 128], hT[:, k, t0:t0 + 512],
                                      k == 0, k == 7), [bw] + hT_bufs(t0, 512), [bpm])
                    qf, bqf = qfr.next()
                    sq, bsq = sqr.next()
                    P.act.op(act_fn(qf[:], pm[:], AF.Copy), [bpm], [bqf])
                    P.act.op(act_fn(sq[:], pm[:], AF.Square), [bpm], [bsq])
                    pms, bpms = pst.next()
                    P.pe.op(mm_fn(pms[:], P.ones64, sq[:], True, True), [P.bcst, bsq], [bpms])
                    sd, bsd = tmp.next()
                    P.act.op(act_fn(sd[:], pms[:], AF.Sqrt, bias=P.eps_t[:, 0:1], scale=1.0), [bpms, P.beps], [bsd])
                    rstd, brstd = tmp.next()
                    P.dve.op(lambda e, rstd=rstd, sd=sd: e.reciprocal(out=rstd[:], in_=sd[:]), [bsd], [brstd])
                    P.dve.op(tt_fn(qf[:], qf[:], rstd[:], ALU.mult), [bqf, brstd], [bqf])
                    P.dve.op(ts_fn(dst[:, t0:t0 + 512], qf[:], qkg[:, gcol:gcol + 1],
                                   0.125 if which == 0 else 1.0, ALU.mult, ALU.mult), [bqf, bqkg], [bdst])
            for ti, (ci, d, c, j, s0) in enumerate(tiles):
                pv, bpv = pmr.next()
                for k in range(8):
                    P.pe.op(mm_fn(pv[:, 0:128], hT[:, k, s0:s0 + 127 * d + 1:d], w[:, k, 256:384], k == 0, k == 7),
                            [bw] + bhT, [bpv])
                P.act.op(act_fn(Vt4[:, ti, 0:4:2, :], pv[:, 0:128].rearrange("p (s c) -> p s c", s=2), AF.Copy),
                         [bpv, bones], [bVt[ti]])
            for hh in range(2):
                r0 = 64 * hh
                for ci, d in enumerate(ATT_CFG):
                    nb = OWN // (128 * d)
                    for c in range(d):
                        pts = []
                        for j in range(nb + 1):
                            i0 = 0 if j == 0 else 128 * j - 64
                            ks = c + d * i0
                            jlo, jhi = max(j - 1, 0), min(j, nb - 1)
                            nq = 128 * (jhi - jlo + 1)
                            q0 = c + d * 128 * jlo
                            pst_, pbuf = psr.next()
                            sl = pst_[:, 0:nq]
                            P.pe.op(mm_fn(sl, kT[r0:r0 + 64, ks:ks + 127 * d + 1:d],
                                          qT[r0:r0 + 64, q0:q0 + (nq - 1) * d + 1:d], True, True), [bkT, bqT], [pbuf])
                            pe_, bpe = pex.next()
                            P.act.op(act_fn(pe_[:, 0:nq], sl, AF.Exp), [pbuf], [bpe])
                            if j == 0:
                                ebs = ebt[:, ci * 2 + hh, 256:384]
                            elif j == nb:
                                ebs = ebt[:, ci * 2 + hh, 0:128]
                            else:
                                ebs = ebt[:, ci * 2 + hh, 0:256]
                            pT, bpT = pTr.next()
                            P.dve.op(tt_fn(pT[:, 0:nq], pe_[:, 0:nq], ebs, ALU.mult), [bpe, bebt], [bpT])
                            pts.append((pT, bpT, nq))
                            if j >= 1:
                                jb = j - 1
                                pTa, bpTa, nqa = pts[jb]
                                a_off = 0 if jb == 0 else 128
                                po_, pobuf = por.next()
                                osl = po_[:, 0:128]
                                ta = tindex[(ci, c, jb)]
                                tb = tindex[(ci, c, j)]
                                P.pe.op(mm_fn(osl, Vt[:, ta, 128 * hh:128 * hh + 128], pTa[:, a_off:a_off + 128],
                                              True, False), [bVt[ta], bpTa], [pobuf])
                                P.pe.op(mm_fn(osl, Vt[:, tb, 128 * hh:128 * hh + 128], pT[:, 0:128],
                                              False, True), [bVt[tb], bpT], [pobuf])
                                a0 = c + d * 128 * jb
                                dsl = acc[:, hh, a0:a0 + 127 * d + 1:d]
                                if ci == 0:
                                    P.dve.op(lambda e, dsl=dsl, osl=osl: e.tensor_copy(out=dsl, in_=osl),
                                             [pobuf], [bacc[hh]])
                                else:
                                    P.dve.op(tt_fn(dsl, dsl, osl, ALU.add), [pobuf, bacc[hh]], [bacc[hh]])
                rz, brz = rzr.next()
                P.dve.op(lambda e, rz=rz, hh=hh: e.reciprocal(out=rz[:], in_=acc[64:128, hh, :]), [bacc[hh]], [brz])
                P.dve.op(tt_fn(mixT[r0:r0 + 64, 4 + hp, :], acc[0:64, hh, :], rz[:], ALU.mult),
                         [bacc[hh], brz], [bmix[4 + hp]])
        P.barrier()

    with contextlib.ExitStack() as st:
        wo = P.sb([128, 8, D], BF16, stack=st)
        bwo = Buf()
        for k in range(8):
            P.pool.dma(out=wo[:, k, :], in_=w_out_v[:, k, :], writes=[bwo])
        gfb = P.sb([128, D], F32, stack=st)
        bgfb = Buf()
        P.sp.dma(out=gfb[:], in_=bcast_rows(gf, 128), writes=[bgfb])
        wr = P.sb([128, 8, 16], F32, stack=st)
        bwr = Buf()
        P.sp.dma(out=wr[:], in_=w_r_v, writes=[bwr])
        brb = P.sb([128, 16], F32, stack=st)
        bbrb = Buf()
        P.sp.dma(out=brb[:], in_=bcast_rows(b_r, 128), writes=[bbrb])
        xr = Ring(P, 2, [128, D], F32, st)
        x1r = Ring(P, 2, [128, D], F32, st)
        pmr = Ring(P, 2, [128, 512], F32, st, psum=True)
        rings = {
            "junk": Ring(P, 1, [128, D], F32, st),
            "small": Ring(P, 12, [128, 1], F32, st),
            "hf": Ring(P, 2, [128, D], F32, st),
            "ptf": Ring(P, 1, [128, 8, 128], F32, st, psum=True),
            "hfT": Ring(P, 2, [128, 8, 128], F32, st),
            "plog": Ring(P, 2, [128, 512], F32, st, psum=True),
            "sm16": Ring(P, 6, [128, 16], F32, st),
            "pat": Ring(P, 1, [128, 512], F32, st, psum=True),
            "aT": Ring(P, 2, [16, 128], F32, st),
        }
        for tt in range(OWN // 128):
            xt, bx = xr.next()
            P.sp.dma(out=xt[:], in_=xw[tt * 128:(tt + 1) * 128, :], writes=[bx])
            x1t, bx1 = x1r.next()
            for half in range(2):
                pm, bpm = pmr.next()
                for k in range(8):
                    P.pe.op(mm_fn(pm[:], mixT[:, k, tt * 128:(tt + 1) * 128], wo[:, k, half * 512:(half + 1) * 512],
                                  k == 0, k == 7), [bmix[k], bwo], [bpm])
                P.dve.op(tt_fn(x1t[:, half * 512:(half + 1) * 512], pm[:], xt[:, half * 512:(half + 1) * 512],
                               ALU.add), [bpm, bx], [bx1])
            P.sp.dma(out=x1_out[tt * 128:(tt + 1) * 128, :], in_=x1t[:], reads=[bx1])
            router_tile(P, x1t, bx1, gfb, bgfb, wr, bwr, brb, bbrb, rings, aff_out, affT_out, tt)
    return


def host_consts():
    cst = np.zeros((128, 6, 128), np.float32)
    cst[:, 0, :] = np.eye(128, dtype=np.float32)
    cst[:, 1, :] = 1.0 / 512.0
    blk = np.zeros((128, 128), np.float32)
    blk[:64, :64] = 1.0 / 64.0
    blk[64:, 64:] = 1.0 / 64.0
    cst[:, 2, :] = blk
    cst[:, 3, :] = 1.0
    cst[:, 4, :] = np.triu(np.ones((128, 128), np.float32), 1)
    cst[:, 5, 0:16] = 512.0 * np.arange(16, dtype=np.float32)[None, :]
    return cst


def host_eb():
    p = np.arange(128)[:, None].astype(np.float64)
    f = np.arange(128)[None, :].astype(np.float64)
    eb = np.zeros((4, 128, 6, 384), np.float32)
    for head in range(8):
        slope = 2.0 ** (-(head + 1))
        hp, hh = head // 2, head % 2
        for ci, d in enumerate(ATT_CFG):
            B = np.where(p <= f, np.exp(-slope * d * np.abs(p + 64 - f)), 0.0)
            A = np.where(p >= f, np.exp(-slope * d * np.abs(p - 64 - f)), 0.0)
            A0 = np.where((p <= 63) & (p >= f - 64), np.exp(-slope * d * np.abs(p - f)), 0.0)
            eb[hp, :, ci * 2 + hh, 0:128] = B
            eb[hp, :, ci * 2 + hh, 128:256] = A
            eb[hp, :, ci * 2 + hh, 256:384] = A0
    return eb


def local_view(xb, h, n):
    if h == 0:
        return np.ascontiguousarray(xb[:n])
    return np.ascontiguousarray(xb[::-1][:n])


_CACHE = {}


def get_prog(name, builder):
    if name not in _CACHE:
        _CACHE[name] = builder()
    return _CACHE[name]


def run_phaseA(x, mix_norm_even, w_in, conv_w, conv_b, conv_ln_g, conv_ln_b, q_norm, k_norm, w_out,
               ffn_norm0, w_router0, b_router0):
    nc = get_prog("A", build_phaseA)
    cst = host_consts()
    eb = host_eb()
    in_maps = []
    for c in range(NCORES):
        b, h = c // 2, c % 2
        cw = conv_w[0] if h == 0 else conv_w[0][::-1]
        cvec = np.zeros((128, 4, 34), np.float32)
        cvec[:, :, 0:31] = cw.T.reshape(4, 128, 31).transpose(1, 0, 2)
        cvec[:, :, 31] = conv_b[0].reshape(4, 128).T
        cvec[:, :, 32] = conv_ln_g[0].reshape(4, 128).T
        cvec[:, :, 33] = conv_ln_b[0].reshape(4, 128).T
        qk = np.stack([np.tile(q_norm[0], 2), np.tile(k_norm[0], 2)], axis=1).astype(np.float32)
        in_maps.append({
            "xw": local_view(x[b], h, WIN),
            "g0": np.ascontiguousarray(mix_norm_even[0][None, :]),
            "w_in": np.ascontiguousarray(w_in[0]),
            "cvec": cvec,
            "qk_g": np.ascontiguousarray(qk),
            "w_out": np.ascontiguousarray(w_out[0]),
            "gf": np.ascontiguousarray(ffn_norm0[None, :]),
            "w_r": np.ascontiguousarray(w_router0),
            "b_r": np.ascontiguousarray(b_router0[None, :]),
            "eb": eb,
            "cst": cst,
        })
    res = run_bass_kernel_spmd(nc, in_maps, core_ids=list(range(NCORES)))
    return res.results


NEXP = 16
CAP = 512


def emit_phaseB(P, io):
    with contextlib.ExitStack() as ph:
        P.cur = ph
        _phaseB_body(P, io["x1"], io["affT_seq"], io["aff_own"], io["gf"], io["wg"], io["wu"], io["wd"],
                     io["x2_out"], io.get("gn"), io.get("hn_out"))
        P.barrier()
    P.cur = None


def _phaseB_body(P, x1, affT_seq, aff_own, gf, wg, wu, wd, x2_out, gn, hn_out):
    if gn is not None:
        gnb = P.sb([128, D], F32, name="gnb")
        bgnb = Buf()
        P.sp.dma(out=gnb[:], in_=bcast_rows(gn, 128), writes=[bgnb])
    gfb = P.sb([128, D], F32, name="gfb")
    bgfb = Buf()
    P.sp.dma(out=gfb[:], in_=bcast_rows(gf, 128), writes=[bgfb])
    gate = P.sb([128, OWN // 128, NEXP], F32, name="gate")
    bgate = Buf()
    pthr = P.ps([128, 512], F32, name="pthr")
    bpthr = Buf()

    with contextlib.ExitStack() as st:
        aT = P.sb([NEXP, SEQ], F32, stack=st)
        baT = Buf()
        P.sp.dma(out=aT[:].rearrange("e (r t) -> e r t", r=2), in_=affT_seq, writes=[baT])
        junk = P.sb([NEXP, SEQ], F32, stack=st)
        bjunk = Buf()
        lo = P.sb([NEXP, 1], F32, stack=st)
        blo = Buf()
        mid = P.sb([NEXP, 1], F32, stack=st)
        bmid = Buf()
        cnt = P.sb([NEXP, 1], F32, stack=st)
        bcnt = Buf()
        ge = P.sb([NEXP, 1], F32, stack=st)
        bge = Buf()
        P.dve.op(lambda e: e.memset(lo[:], 0.0), [], [blo])
        for it in range(36):
            wk = 2.0 ** (-(it + 1))
            P.dve.op(ts_fn(mid[:], lo[:], wk, None, ALU.add), [blo], [bmid])
            P.dve.op(lambda e: e.tensor_scalar(out=junk[:], in0=aT[:], scalar1=mid[:, 0:1], scalar2=0.0,
                                               op0=ALU.is_ge, op1=ALU.add, accum_out=cnt[:, 0:1]),
                     [baT, bmid], [bjunk, bcnt])
            P.dve.op(ts_fn(ge[:], cnt[:], CAP - 0.5, None, ALU.is_ge), [bcnt], [bge])
            P.dve.op(stt_fn(lo[:], ge[:], wk, lo[:], ALU.mult, ALU.add), [bge, blo], [blo])
        dthr = P.sb([NEXP, NEXP], F32, stack=st)
        bdthr = Buf()
        P.dve.op(ts_fn(dthr[:], P.cst[0:NEXP, 0, 0:NEXP], lo[:, 0:1], None, ALU.mult), [P.bcst, blo], [bdthr])
        P.pe.op(mm_fn(pthr[:, 0:NEXP], P.cst[0:NEXP, 3, :], dthr[:], True, True), [P.bcst, bdthr], [bpthr])
        thrb = P.sb([128, NEXP], F32, stack=st)
        bthrb = Buf()
        P.act.op(act_fn(thrb[:], pthr[:, 0:NEXP], AF.Copy), [bpthr], [bthrb])
        ao = P.sb([128, OWN // 128, NEXP], F32, stack=st)
        bao = Buf()
        P.sp.dma(out=ao[:], in_=aff_own.rearrange("(t p) e -> p t e", p=128), writes=[bao])
        msk = P.sb([128, OWN // 128, NEXP], F32, stack=st)
        bmsk = Buf()
        thr_bc = bass.AP(thrb.tensor if hasattr(thrb, "tensor") else thrb[:].tensor, thrb[:].offset,
                         [list(thrb[:].ap[0]), [0, OWN // 128], [1, NEXP]])
        P.dve.op(tt_fn(msk[:], ao[:], thr_bc, ALU.is_ge), [bao, bthrb], [bmsk])
        P.dve.op(tt_fn(gate[:], msk[:], ao[:], ALU.mult), [bmsk, bao], [bgate])
        P.barrier()

    HT = OWN // 2
    with contextlib.ExitStack() as st:
        acc = P.sb([128, HT // 128, D], F32, stack=st)
        bacc = [Buf() for _ in range(HT // 128)]
        hT = P.sb([128, 8, HT], BF16, stack=st)
        bhT = Buf()
        junk = Ring(P, 1, [128, D], F32, st)
        small = Ring(P, 8, [128, 1], F32, st)
        hnr = Ring(P, 2, [128, D], BF16, st)
        hor = Ring(P, 2, [128, D], F32, st)
        ptr = Ring(P, 1, [128, 8, 128], BF16, st, psum=True)
        wgr = Ring(P, 2, [128, 8, D], BF16, st)
        wur = Ring(P, 2, [128, 8, D], BF16, st)
        wdr = Ring(P, 2, [128, 8, D], BF16, st)
        atr = Ring(P, 2, [128, 8, 512], BF16, st)
        sgr = Ring(P, 2, [128, 512], F32, st)
        pgr = Ring(P, 2, [128, 512], F32, st, psum=True)
        pur = Ring(P, 2, [128, 512], F32, st, psum=True)
        pyr = Ring(P, 2, [128, 512], F32, st, psum=True)
        for half in range(2):
            for t in range(HT // 128):
                tg = half * (HT // 128) + t
                P.sp.dma(out=acc[:, t, :], in_=x1[tg * 128:(tg + 1) * 128, :], writes=[bacc[t]])
                rs, br = rms_rstd(P, acc[:, t, :], bacc[t], D, junk, small)
                hn, bhn = hnr.next()
                P.dve.op(stt_fn(hn[:], acc[:, t, :], rs[:, 0:1], gfb[:], ALU.mult, ALU.mult),
                         [bacc[t], br, bgfb], [bhn])
                pt, bpt = ptr.next()
                for k in range(8):
                    P.pe.op(lambda e, k=k, pt=pt, hn=hn: e.transpose(pt[:, k, :], hn[:, k * 128:(k + 1) * 128],
                                                                      P.identb[:]), [bhn, P.bidb], [bpt])
                P.act.op(act_fn(hT[:, :, t * 128:(t + 1) * 128], pt[:], AF.Copy), [bpt], [bhT])
            for e in range(NEXP):
                wts = []
                for src, ring in ((wg, wgr), (wu, wur), (wd, wdr)):
                    w, bw = ring.next()
                    sv = src[e].rearrange("(k p) f -> p k f", p=128)
                    P.pool.dma(out=w[:, 0:4, :], in_=sv[:, 0:4, :], writes=[bw])
                    P.pool.dma(out=w[:, 4:8, :], in_=sv[:, 4:8, :], writes=[bw])
                    wts.append((w, bw))
                (Wg, bWg), (Wu, bWu), (Wd, bWd) = wts
                for q in range(HT // 512):
                    AT, bAT = atr.next()
                    for fc in range(8):
                        pg, bpg = pgr.next()
                        pu, bpu = pur.next()
                        for k in range(8):
                            P.pe.op(mm_fn(pg[:], Wg[:, k, fc * 128:(fc + 1) * 128], hT[:, k, q * 512:(q + 1) * 512],
                                          k == 0, k == 7), [bWg, bhT], [bpg])
                        for k in range(8):
                            P.pe.op(mm_fn(pu[:], Wu[:, k, fc * 128:(fc + 1) * 128], hT[:, k, q * 512:(q + 1) * 512],
                                          k == 0, k == 7), [bWu, bhT], [bpu])
                        sg, bsg = sgr.next()
                        P.act.op(act_fn(sg[:], pg[:], AF.Silu), [bpg], [bsg])
                        P.dve.op(tt_fn(AT[:, fc, :], sg[:], pu[:], ALU.mult), [bsg, bpu], [bAT])
                    for tt in range(4):
                        t = q * 4 + tt
                        tg = half * (HT // 128) + t
                        for hc in range(2):
                            py, bpy = pyr.next()
                            for fc in range(8):
                                P.pe.op(mm_fn(py[:], AT[:, fc, tt * 128:(tt + 1) * 128],
                                              Wd[:, fc, hc * 512:(hc + 1) * 512], fc == 0, fc == 7), [bAT, bWd], [bpy])
                            asl = acc[:, t, hc * 512:(hc + 1) * 512]
                            P.dve.op(stt_fn(asl, py[:], gate[:, tg, e:e + 1], asl, ALU.mult, ALU.add),
                                     [bpy, bgate, bacc[t]], [bacc[t]])
            for t in range(HT // 128):
                tg = half * (HT // 128) + t
                P.sp.dma(out=x2_out[tg * 128:(tg + 1) * 128, :], in_=acc[:, t, :], reads=[bacc[t]])
                if gn is not None:
                    rs, br = rms_rstd(P, acc[:, t, :], bacc[t], D, junk, small)
                    ho, bho = hor.next()
                    P.dve.op(stt_fn(ho[:], acc[:, t, :], rs[:, 0:1], gnb[:], ALU.mult, ALU.mult),
                             [bacc[t], br, bgnb], [bho])
                    P.sp.dma(out=hn_out[tg * 128:(tg + 1) * 128, :], in_=ho[:], reads=[bho])


def run_phaseB(x1_cores, affT_cores, aff_cores, gf, wg, wu, wd, gn):
    nc = get_prog("B", build_phaseB)
    cst = host_consts()
    in_maps = []
    for c in range(NCORES):
        b = c // 2
        affT_seq = np.ascontiguousarray(np.concatenate([affT_cores[2 * b], affT_cores[2 * b + 1]], axis=1))
        in_maps.append({
            "x1": x1_cores[c], "affT_seq": affT_seq, "aff_own": aff_cores[c],
            "gf": np.ascontiguousarray(gf[None, :]), "wg": wg, "wu": wu, "wd": wd, "cst": cst,
            "gn": np.ascontiguousarray(gn[None, :]),
        })
    res = run_bass_kernel_spmd(nc, in_maps, core_ids=list(range(NCORES)))
    return [r["x2"] for r in res.results], [r["hn"] for r in res.results]


NG = 32
NK = SEQ // 8
NWIN = NK // 8
HALF_PI = 1.5707963267948966


class _Stop(Exception):
    pass


def build_phaseC(stop_after=99):
    P = Prog()
    try:
        _phaseC_body(P, stop_after)
    except _Stop:
        P.barrier()
    return P.finish()


def _phaseC_body(P, stop_after):
    lamr_i = P.inp("lamr", [128, NG])
    lami_i = P.inp("lami", [128, NG])
    ldt_i = P.inp("ldt", [128, NG])
    B_i = P.inp("Bri", [128, 2, NG, 16])
    C_i = P.inp("Cri", [128, 2, NG, 16])
    dcol_i = P.inp("dcol", [128, NG])
    Unat = P.inp("Unat", [NG, 128, NK])
    Uw = P.inp("Uw", [NWIN, 128, NG, 8])
    Uwr = P.inp("Uwr", [NWIN, 128, NG, 8])
    masks_i = P.inp("masks", [128, 2, 128])
    cst_in = P.inp("cst", [128, 6, 128])
    zp = P.outp("zp", [NG, 128, NK])
    load_consts(P, cst_in)
    dve, act, pe, pool, sp = P.dve, P.act, P.pe, P.pool, P.sp

    M = P.sb([128, NG, 128], BF16, name="M")
    bM = Buf()
    Wz = P.sb([128, NG, 4, 128], BF16, name="Wz")
    bWz = Buf()
    Rr = P.sb([128, NG, 128], BF16, name="Rr")
    Ri = P.sb([128, NG, 128], BF16, name="Ri")
    bR = Buf()
    A8 = P.sb([128, 2, 2, NG], F32, name="A8")
    bA8 = Buf()
    dcol = P.sb([128, NG], F32, name="dcol")
    bdcol = Buf()
    sp.dma(out=dcol[:], in_=dcol_i, writes=[bdcol])

    def small(st, name=None):
        return P.sb([128, NG], F32, stack=st), Buf()

    with contextlib.ExitStack() as st:
        lamr, blamr = small(st)
        lami, blami = small(st)
        ldt, bldt = small(st)
        sp.dma(out=lamr[:], in_=lamr_i, writes=[blamr])
        sp.dma(out=lami[:], in_=lami_i, writes=[blami])
        sp.dma(out=ldt[:], in_=ldt_i, writes=[bldt])
        Bt = P.sb([128, 2, NG, 16], F32, stack=st)
        bBt = Buf()
        Ct = P.sb([128, 2, NG, 16], F32, stack=st)
        bCt = Buf()
        sp.dma(out=Bt[:], in_=B_i, writes=[bBt])
        sp.dma(out=Ct[:], in_=C_i, writes=[bCt])
        mk = P.sb([128, 2, 128], F32, stack=st)
        bmk = Buf()
        sp.dma(out=mk[:], in_=masks_i, writes=[bmk])
        hpi = P.sb([128, 1], F32, stack=st)
        bhpi = Buf()
        dve.op(lambda e: e.memset(hpi[:], HALF_PI), [], [bhpi])

        def T2(a, ba, b, bb, op):
            o, bo = small(st)
            dve.op(tt_fn(o[:], a[:], b[:], op), [ba, bb], [bo])
            return o, bo

        dt, bdt = small(st)
        act.op(act_fn(dt[:], ldt[:], AF.Exp), [bldt], [bdt])
        a_, ba_ = T2(lamr, blamr, dt, bdt, ALU.mult)
        ang, bang = T2(lami, blami, dt, bdt, ALU.mult)
        mag, bmag = small(st)
        act.op(act_fn(mag[:], a_[:], AF.Exp, scale=1.0 / 16), [ba_], [bmag])
        s16, bs16 = small(st)
        act.op(act_fn(s16[:], ang[:], AF.Sin, scale=1.0 / 16), [bang], [bs16])
        c16, bc16 = small(st)
        act.op(act_fn(c16[:], ang[:], AF.Sin, scale=1.0 / 16, bias=hpi[:, 0:1]), [bang, bhpi], [bc16])
        re, bre = T2(mag, bmag, c16, bc16, ALU.mult)
        im, bim = T2(mag, bmag, s16, bs16, ALU.mult)
        for _ in range(4):
            r2, br2 = T2(re, bre, re, bre, ALU.mult)
            i2, bi2 = T2(im, bim, im, bim, ALU.mult)
            nre, bnre = T2(r2, br2, i2, bi2, ALU.subtract)
            nim, bnim = small(st)
            dve.op(stt_fn(nim[:], re[:], 2.0, im[:], ALU.mult, ALU.mult), [bre, bim], [bnim])
            re, bre, im, bim = nre, bnre, nim, bnim
        pw = P.sb([128, 9, 2, NG], F32, stack=st)
        bpw = Buf()
        ipw = P.sb([128, 9, 2, NG], F32, stack=st)
        bipw = Buf()
        dve.op(lambda e: e.memset(pw[:, 0, 0, :], 1.0), [], [bpw])
        dve.op(lambda e: e.memset(pw[:, 0, 1, :], 0.0), [], [bpw])
        dve.op(lambda e: e.memset(ipw[:, 0, 0, :], 1.0), [], [bipw])
        dve.op(lambda e: e.memset(ipw[:, 0, 1, :], 0.0), [], [bipw])
        dve.op(lambda e: e.tensor_copy(out=pw[:, 1, 0, :], in_=re[:]), [bre], [bpw])
        dve.op(lambda e: e.tensor_copy(out=pw[:, 1, 1, :], in_=im[:]), [bim], [bpw])
        t1, bt1 = small(st)
        t2, bt2 = small(st)
        for n in range(1, 8):
            dve.op(tt_fn(t1[:], pw[:, n, 0, :], re[:], ALU.mult), [bpw, bre], [bt1])
            dve.op(tt_fn(t2[:], pw[:, n, 1, :], im[:], ALU.mult), [bpw, bim], [bt2])
            dve.op(tt_fn(pw[:, n + 1, 0, :], t1[:], t2[:], ALU.subtract), [bt1, bt2], [bpw])
            dve.op(tt_fn(t1[:], pw[:, n, 0, :], im[:], ALU.mult), [bpw, bim], [bt1])
            dve.op(tt_fn(t2[:], pw[:, n, 1, :], re[:], ALU.mult), [bpw, bre], [bt2])
            dve.op(tt_fn(pw[:, n + 1, 1, :], t1[:], t2[:], ALU.add), [bt1, bt2], [bpw])
        en, ben = small(st)
        for n in range(1, 9):
            act.op(act_fn(en[:], a_[:], AF.Exp, scale=-2.0 * n), [ba_], [ben])
            dve.op(tt_fn(ipw[:, n, 0, :], pw[:, n, 0, :], en[:], ALU.mult), [bpw, ben], [bipw])
            dve.op(stt_fn(ipw[:, n, 1, :], pw[:, n, 1, :], -1.0, en[:], ALU.mult, ALU.mult), [bpw, ben], [bipw])
        dve.op(lambda e: e.tensor_copy(out=A8[:, 0, 0, :], in_=pw[:, 8, 0, :]), [bpw], [bA8])
        dve.op(lambda e: e.tensor_copy(out=A8[:, 0, 1, :], in_=pw[:, 8, 0, :]), [bpw], [bA8])
        dve.op(ts_fn(A8[:, 1, 0, :], pw[:, 8, 1, :], -1.0, None, ALU.mult), [bpw], [bA8])
        dve.op(lambda e: e.tensor_copy(out=A8[:, 1, 1, :], in_=pw[:, 8, 1, :]), [bpw], [bA8])
        lr2, blr2 = T2(lamr, blamr, lamr, blamr, ALU.mult)
        li2, bli2 = T2(lami, blami, lami, blami, ALU.mult)
        den, bden = T2(lr2, blr2, li2, bli2, ALU.add)
        rden, brden = small(st)
        dve.op(lambda e: e.reciprocal(out=rden[:], in_=den[:]), [bden], [brden])
        nr, bnr = small(st)
        dve.op(ts_fn(nr[:], re[:], -1.0, None, ALU.add), [bre], [bnr])
        u1, bu1 = T2(nr, bnr, lamr, blamr, ALU.mult)
        u2, bu2 = T2(im, bim, lami, blami, ALU.mult)
        u3, bu3 = T2(u1, bu1, u2, bu2, ALU.add)
        fre, bfre = T2(u3, bu3, rden, brden, ALU.mult)
        u4, bu4 = T2(im, bim, lamr, blamr, ALU.mult)
        u5, bu5 = T2(nr, bnr, lami, blami, ALU.mult)
        u6, bu6 = T2(u4, bu4, u5, bu5, ALU.subtract)
        fim, bfim = T2(u6, bu6, rden, brden, ALU.mult)

        def bc16_(t):
            a = t[:]
            return bass.AP(a.tensor, a.offset, [list(a.ap[0]), [1, NG], [0, 16]])

        big = lambda: (P.sb([128, NG, 16], F32, stack=st), Buf())
        bbr, bbbr = big()
        bbi, bbbi = big()
        g1, bg1 = big()
        g2, bg2 = big()
        dve.op(tt_fn(g1[:], Bt[:, 0, :, :], bc16_(fre), ALU.mult), [bBt, bfre], [bg1])
        dve.op(tt_fn(g2[:], Bt[:, 1, :, :], bc16_(fim), ALU.mult), [bBt, bfim], [bg2])
        dve.op(tt_fn(bbr[:], g1[:], g2[:], ALU.subtract), [bg1, bg2], [bbbr])
        dve.op(tt_fn(g1[:], Bt[:, 1, :, :], bc16_(fre), ALU.mult), [bBt, bfre], [bg1])
        dve.op(tt_fn(g2[:], Bt[:, 0, :, :], bc16_(fim), ALU.mult), [bBt, bfim], [bg2])
        dve.op(tt_fn(bbi[:], g1[:], g2[:], ALU.add), [bg1, bg2], [bbbi])

        def table(top, bot):
            t = P.sb([128, 8, 2, NG], F32, stack=st)
            bt = Buf()
            for i in range(8):
                (ta, tn), (ba, bn) = top[i], bot[i]
                pool.op(lambda e, i=i, ta=ta, tn=tn: e.tensor_copy(out=t[0:64, i, :, :], in_=ta[0:64, tn, :, :]),
                        [bpw, bipw], [bt])
                pool.op(lambda e, i=i, ba=ba, bn=bn: e.tensor_copy(out=t[64:128, i, :, :], in_=ba[64:128, bn, :, :]),
                        [bpw, bipw], [bt])
            return t, bt

        QX, bQX = table([(ipw, s) for s in range(8)], [(pw, s) for s in range(8)])
        PY, bPY = table([(pw, t) for t in range(8)], [(ipw, t) for t in range(8)])
        QW, bQW = table([(pw, 7 - s) for s in range(8)], [(pw, s) for s in range(8)])
        PR, bPR = table([(pw, j + 1) for j in range(8)], [(pw, 8 - j) for j in range(8)])

        def tb(t, i, ri):
            a = t[:, i, ri, :]
            return bass.AP(a.tensor, a.offset, [list(a.ap[0]), [1, NG], [0, 16]])

        def cprod(dst_re, dst_im, bdst, xr, xi, bx, tab, btab, neg_im=False, eng=None):
            eng = eng or dve
            bx = list(bx) if isinstance(bx, (list, tuple)) else [bx]
            for i in range(8):
                eng.op(tt_fn(g1[:], xr[:], tb(tab, i, 0), ALU.mult), bx + [btab], [bg1])
                eng.op(tt_fn(g2[:], xi[:], tb(tab, i, 1), ALU.mult), bx + [btab], [bg2])
                eng.op(tt_fn(dst_re[:, :, i, :], g1[:], g2[:], ALU.subtract), [bg1, bg2], [bdst])
                eng.op(tt_fn(g1[:], xr[:], tb(tab, i, 1), ALU.mult), bx + [btab], [bg1])
                eng.op(tt_fn(g2[:], xi[:], tb(tab, i, 0), ALU.mult), bx + [btab], [bg2])
                if neg_im:
                    eng.op(stt_fn(dst_im[:, :, i, :], g1[:], -1.0, g2[:], ALU.mult, ALU.subtract), [bg1, bg2], [bdst])
                else:
                    eng.op(tt_fn(dst_im[:, :, i, :], g1[:], g2[:], ALU.add), [bg1, bg2], [bdst])

        bbx = Buf()
        with contextlib.ExitStack() as st2:
            Xr = P.sb([128, NG, 8, 16], F32, stack=st2)
            Xi = P.sb([128, NG, 8, 16], F32, stack=st2)
            Yr = P.sb([128, NG, 8, 16], F32, stack=st2)
            Yn = P.sb([128, NG, 8, 16], F32, stack=st2)
            bX, bY = Buf(), Buf()
            bBB = Buf()
            cprod(Xr, Xi, bX, bbr, bbi, [bbbr, bbbi], QX, bQX)
            cprod(Yr, Yn, bY, Ct[:, 0, :, :], Ct[:, 1, :, :], bCt, PY, bPY, neg_im=True)
            psF = Ring(P, 2, [128, 512], F32, st2, psum=True)
            psB = Ring(P, 2, [128, 512], F32, st2, psum=True)
            tmr = Ring(P, 2, [128, 128], F32, st2)
            Xr3 = Xr[:].rearrange("p g s c -> p g (s c)")
            Xi3 = Xi[:].rearrange("p g s c -> p g (s c)")
            Yr3 = Yr[:].rearrange("p g s c -> p g (s c)")
            Yn3 = Yn[:].rearrange("p g s c -> p g (s c)")
            for g in range(NG):
                pf, bpf = psF.next()
                pb_, bpb = psB.next()
                pe.op(mm_fn(pf[:, 0:128], Xr3[0:64, g, :], Yr3[0:64, g, :], True, False), [bX, bY], [bpf])
                pe.op(mm_fn(pf[:, 0:128], Xi3[0:64, g, :], Yn3[0:64, g, :], False, True), [bX, bY], [bpf])
                pe.op(mm_fn(pb_[:, 0:128], Xr3[64:128, g, :], Yr3[64:128, g, :], True, False), [bX, bY], [bpb])
                pe.op(mm_fn(pb_[:, 0:128], Xi3[64:128, g, :], Yn3[64:128, g, :], False, True), [bX, bY], [bpb])
                tm, btm = tmr.next()
                dve.op(tt_fn(tm[:], pf[:, 0:128], mk[:, 0, :], ALU.mult), [bpf, bmk], [btm])
                tm2, btm2 = tmr.next()
                dve.op(tt_fn(tm2[:], pb_[:, 0:128], mk[:, 1, :], ALU.mult), [bpb, bmk], [btm2])
                dve.op(tt_fn(M[:, g, :], tm[:], tm2[:], ALU.add), [btm, btm2], [bM])
            P.barrier()
        if stop_after <= 1:
            P.stopped = True
        with contextlib.ExitStack() as st2:
            Wr = P.sb([128, NG, 8, 16], F32, stack=st2)
            Wi = P.sb([128, NG, 8, 16], F32, stack=st2)
            bW = Buf()
            cprod(Wr, Wi, bW, bbr, bbi, [bbbr, bbbi], QW, bQW)
            pool.op(lambda e: e.memset(Wz[:], 0.0), [], [bWz])
            ptw = Ring(P, 2, [128, 512], F32, st2, psum=True)
            W3 = (Wr[:].rearrange("p g s c -> p g (s c)"), Wi[:].rearrange("p g s c -> p g (s c)"))
            for g in range(NG):
                for ri in range(2):
                    pt, bpt = ptw.next()
                    pe.op(lambda e, pt=pt, g=g, ri=ri: e.transpose(pt[:, 0:128], W3[ri][:, g, :], P.identf),
                          [bW, P.bcst], [bpt])
                    act.op(act_fn(Wz[:, g, 2 * ri, 0:64], pt[:, 0:64], AF.Copy), [bpt], [bWz])
                    act.op(act_fn(Wz[:, g, 2 * ri + 1, 64:128], pt[:, 64:128], AF.Copy), [bpt], [bWz])
            P.barrier()
        if stop_after <= 2:
            P.stopped = True
        Rr4 = Rr[:].rearrange("p g (j c) -> p g j c", j=8)
        Ri4 = Ri[:].rearrange("p g (j c) -> p g j c", j=8)
        cprod(Rr4, Ri4, bR, Ct[:, 0, :, :], Ct[:, 1, :, :], bCt, PR, bPR, neg_im=True)
        P.barrier()

    with contextlib.ExitStack() as st:
        hist = P.sb([128, 2, NG, NK], BF16, stack=st)
        bhist = Buf()
        uwr = Ring(P, 3, [128, NG, 8], BF16, st)
        uwrr = Ring(P, 3, [128, NG, 8], BF16, st)
        pvr = Ring(P, 3, [128, 2, NG, 8], F32, st, psum=True)
        S4r = Ring(P, 4, [128, 3, NG], F32, st)
        p1 = P.sb([128, 2, NG], F32, stack=st)
        p2 = P.sb([128, 2, NG], F32, stack=st)
        bp1, bp2 = Buf(), Buf()
        S4, bS = S4r.next()
        dve.op(lambda e: e.memset(S4[:], 0.0), [], [bS])
        for w in range(NWIN):
            uw, buw = uwr.next()
            uwb, buwb = uwrr.next()
            pool.dma(out=uw[:], in_=Uw[w], writes=[buw])
            pool.dma(out=uwb[:], in_=Uwr[w], writes=[buwb])
            pv, bpv = pvr.next()
            for g in range(NG):
                for ri in range(2):
                    pe.op(mm_fn(pv[:, ri, g, :], Wz[:, g, 2 * ri, :], uw[:, g, :], True, False), [bWz, buw], [bpv])
                    pe.op(mm_fn(pv[:, ri, g, :], Wz[:, g, 2 * ri + 1, :], uwb[:, g, :], False, True), [bWz, buwb], [bpv])
            for jj in range(8):
                j = w * 8 + jj
                act.op(act_fn(hist[0:64, :, :, j], S4[0:64, 0:2, :], AF.Copy), [bS], [bhist])
                act.op(act_fn(hist[64:128, :, :, NK - 1 - j], S4[64:128, 0:2, :], AF.Copy), [bS], [bhist])
                Sn, bSn = S4r.next()
                dve.op(tt_fn(p1[:], A8[:, 0, :, :], S4[:, 0:2, :], ALU.mult), [bA8, bS], [bp1])
                dve.op(tt_fn(p2[:], A8[:, 1, :, :], S4[:, 1:3, :], ALU.mult), [bA8, bS], [bp2])
                dve.op(tt_fn(p1[:], p1[:], p2[:], ALU.add), [bp1, bp2], [bp1])
                dve.op(tt_fn(Sn[:, 0:2, :], p1[:], pv[:, :, :, jj], ALU.add), [bp1, bpv], [bSn])
                dve.op(lambda e, Sn=Sn: e.tensor_copy(out=Sn[:, 2, :], in_=Sn[:, 0, :]), [bSn], [bSn])
                S4, bS = Sn, bSn
        P.barrier()
        if stop_after <= 4:
            P.stopped = True
        ubr = Ring(P, 2, [128, NK], BF16, st)
        ufr = Ring(P, 2, [128, NK], F32, st)
        pyr = Ring(P, 2, [128, 512], F32, st, psum=True)
        yr = Ring(P, 2, [128, NK], F32, st)
        tr = Ring(P, 4, [128, NK], F32, st)
        for g in range(NG):
            ub, bub = ubr.next()
            uf, buf_ = ufr.next()
            pool.dma(out=ub[:], in_=Unat[g], writes=[bub])
            sp.dma(out=uf[:], in_=Unat[g], writes=[buf_])
            py, bpy = pyr.next()
            pe.op(mm_fn(py[:], M[:, g, :], ub[:], True, False), [bM, bub], [bpy])
            pe.op(mm_fn(py[:], Rr[:, g, :], hist[:, 0, g, :], False, False), [bR, bhist], [bpy])
            pe.op(mm_fn(py[:], Ri[:, g, :], hist[:, 1, g, :], False, True), [bR, bhist], [bpy])
            y, by = yr.next()
            dve.op(stt_fn(y[:], uf[:], dcol[:, g:g + 1], py[:], ALU.mult, ALU.add), [buf_, bdcol, bpy], [by])
            a1, ba1 = tr.next()
            dve.op(tt_fn(a1[:], y[:], y[:], ALU.mult), [by], [ba1])
            dve.op(ts_fn(a1[:], a1[:], 0.044715, 1.0, ALU.mult, ALU.add), [ba1], [ba1])
            dve.op(tt_fn(a1[:], a1[:], y[:], ALU.mult), [ba1, by], [ba1])
            a2, ba2 = tr.next()
            act.op(act_fn(a2[:], a1[:], AF.Sigmoid, scale=1.5957691216057308), [ba1], [ba2])
            dve.op(tt_fn(a2[:], a2[:], y[:], ALU.mult), [ba2, by], [ba2])
            sp.dma(out=zp[g], in_=a2[:], reads=[ba2])
    return


def host_masks():
    s = np.arange(128)[:, None] // 16
    t = np.arange(128)[None, :] // 16
    m = np.zeros((128, 2, 128), np.float32)
    m[:, 0, :] = (t >= s)
    m[:, 1, :] = (s >= t)
    return m


def phaseC_inputs(hn1_seq, gh, lam_re, lam_im, log_dt, b_re, b_im, c_re, c_im, d_skip):
    G0 = gh * NG
    sl = slice(G0, G0 + NG)

    def dpg(a):
        return np.ascontiguousarray(a.transpose(0, 2, 1).reshape(128, NG))

    lamr = dpg(lam_re[:, sl, :])
    lami = dpg(lam_im[:, sl, :])
    ldt = dpg(np.broadcast_to(log_dt[:, sl, None], (2, NG, 64)))
    Bri = np.stack([b_re[:, sl].transpose(0, 2, 1, 3).reshape(128, NG, 16),
                    b_im[:, sl].transpose(0, 2, 1, 3).reshape(128, NG, 16)], axis=1)
    Cri = np.stack([c_re[:, sl].transpose(0, 3, 1, 2).reshape(128, NG, 16),
                    c_im[:, sl].transpose(0, 3, 1, 2).reshape(128, NG, 16)], axis=1)
    dg = d_skip[G0 * 16:(G0 + NG) * 16].reshape(NG, 16)
    dcol = np.ascontiguousarray(np.broadcast_to(dg.T[None, :, :], (8, 16, NG)).reshape(128, NG))
    u = hn1_seq[:, G0 * 16:(G0 + NG) * 16].reshape(NK, 8, NG, 16)
    Unat = np.ascontiguousarray(u.transpose(2, 1, 3, 0).reshape(NG, 128, NK))
    Uw = np.ascontiguousarray(Unat.reshape(NG, 128, NWIN, 8).transpose(2, 1, 0, 3))
    Uwr = np.ascontiguousarray(Unat[:, :, ::-1].reshape(NG, 128, NWIN, 8).transpose(2, 1, 0, 3))
    return {"lamr": lamr, "lami": lami, "ldt": ldt, "Bri": np.ascontiguousarray(Bri),
            "Cri": np.ascontiguousarray(Cri), "dcol": dcol, "Unat": Unat, "Uw": Uw, "Uwr": Uwr,
            "masks": host_masks(), "cst": host_consts()}


def phaseC_unpack(zp):
    return np.ascontiguousarray(zp.reshape(NG, 8, 16, NK).transpose(3, 1, 0, 2).reshape(SEQ, NG * 16))


def emit_phaseD(P, io):
    with contextlib.ExitStack() as ph:
        P.cur = ph
        _phaseD_body(P, io["zs"], io["hn"], io["dvec"], io["x2"], io["w_glu"], io["gf"], io["w_r"], io["b_r"],
                     io["x3_out"], io["aff_out"], io["affT_out"])
        P.barrier()
    P.cur = None


def _phaseD_body(P, zt, hn_in, dvec, x2, w_glu, gf, w_r, b_r, x3_out, aff_out, affT_out):
    dvb = P.sb([128, D], F32, name="dvb")
    bdvb = Buf()
    P.sp.dma(out=dvb[:], in_=bcast_rows(dvec, 128), writes=[bdvb])
    st = P.cur
    wgl = P.sb([128, 8, 2 * D], BF16, name="wgl")
    bwgl = Buf()
    wv = w_glu.rearrange("(k p) e -> p k e", p=128)
    for k in range(8):
        P.pool.dma(out=wgl[:, k, :], in_=wv[:, k, :], writes=[bwgl])
    gfb = P.sb([128, D], F32, name="gfb")
    bgfb = Buf()
    P.sp.dma(out=gfb[:], in_=bcast_rows(gf, 128), writes=[bgfb])
    wr = P.sb([128, 8, 16], F32, name="wr")
    bwr = Buf()
    P.sp.dma(out=wr[:], in_=w_r.rearrange("(k p) e -> p k e", p=128), writes=[bwr])
    brb = P.sb([128, 16], F32, name="brb")
    bbrb = Buf()
    P.sp.dma(out=brb[:], in_=bcast_rows(b_r, 128), writes=[bbrb])
    zr = Ring(P, 2, [128, D], F32, st)
    zbr = Ring(P, 2, [128, D], BF16, st)
    hnr2 = Ring(P, 2, [128, D], F32, st)
    xr = Ring(P, 2, [128, D], F32, st)
    x3r = Ring(P, 2, [128, D], F32, st)
    ptr = Ring(P, 1, [128, 8, 128], BF16, st, psum=True)
    zTr = Ring(P, 2, [128, 8, 128], BF16, st)
    pvr = Ring(P, 1, [128, 512], F32, st, psum=True)
    pgr = Ring(P, 1, [128, 512], F32, st, psum=True)
    sgr = Ring(P, 2, [128, 512], F32, st)
    rings = {
        "junk": Ring(P, 1, [128, D], F32, st),
        "small": Ring(P, 12, [128, 1], F32, st),
        "hf": Ring(P, 2, [128, D], F32, st),
        "ptf": Ring(P, 1, [128, 8, 128], F32, st, psum=True),
        "hfT": Ring(P, 2, [128, 8, 128], F32, st),
        "plog": Ring(P, 1, [128, 512], F32, st, psum=True),
        "sm16": Ring(P, 6, [128, 16], F32, st),
        "pat": Ring(P, 1, [128, 512], F32, st, psum=True),
        "aT": Ring(P, 2, [16, 128], F32, st),
    }
    for tt in range(OWN // 128):
        z_, bz = zr.next()
        P.sp.dma(out=z_[:], in_=zt[tt * 128:(tt + 1) * 128, :], writes=[bz])
        xt, bx = xr.next()
        P.sp.dma(out=xt[:], in_=x2[tt * 128:(tt + 1) * 128, :], writes=[bx])
        hn_, bhn_ = hnr2.next()
        P.sp.dma(out=hn_[:], in_=hn_in[tt * 128:(tt + 1) * 128, :], writes=[bhn_])
        P.dve.op(tt_fn(hn_[:], hn_[:], dvb[:], ALU.mult), [bhn_, bdvb], [bhn_])
        P.dve.op(tt_fn(z_[:], z_[:], hn_[:], ALU.add), [bz, bhn_], [bz])
        P.dve.op(tt_fn(hn_[:], z_[:], z_[:], ALU.mult), [bz], [bhn_])
        P.dve.op(ts_fn(hn_[:], hn_[:], 0.044715, 1.0, ALU.mult, ALU.add), [bhn_], [bhn_])
        P.dve.op(tt_fn(hn_[:], hn_[:], z_[:], ALU.mult), [bhn_, bz], [bhn_])
        P.act.op(act_fn(hn_[:], hn_[:], AF.Sigmoid, scale=1.5957691216057308), [bhn_], [bhn_])
        zb, bzb = zbr.next()
        P.dve.op(tt_fn(zb[:], hn_[:], z_[:], ALU.mult), [bhn_, bz], [bzb])
        pt, bpt = ptr.next()
        for k in range(8):
            P.pe.op(lambda e, k=k, pt=pt, zb=zb: e.transpose(pt[:, k, :], zb[:, k * 128:(k + 1) * 128], P.identb[:]),
                    [bzb, P.bidb], [bpt])
        zT, bzT = zTr.next()
        P.act.op(act_fn(zT[:], pt[:], AF.Copy), [bpt], [bzT])
        x3t, bx3 = x3r.next()
        for half in range(2):
            pv, bpv = pvr.next()
            pg, bpg = pgr.next()
            for k in range(8):
                P.pe.op(mm_fn(pv[:], zT[:, k, :], wgl[:, k, half * 512:(half + 1) * 512], k == 0, k == 7),
                        [bzT, bwgl], [bpv])
            for k in range(8):
                P.pe.op(mm_fn(pg[:], zT[:, k, :], wgl[:, k, D + half * 512:D + (half + 1) * 512], k == 0, k == 7),
                        [bzT, bwgl], [bpg])
            sg, bsg = sgr.next()
            P.act.op(act_fn(sg[:], pg[:], AF.Sigmoid), [bpg], [bsg])
            P.dve.op(tt_fn(sg[:], sg[:], pv[:], ALU.mult), [bsg, bpv], [bsg])
            P.dve.op(tt_fn(x3t[:, half * 512:(half + 1) * 512], sg[:], xt[:, half * 512:(half + 1) * 512], ALU.add),
                     [bsg, bx], [bx3])
        P.sp.dma(out=x3_out[tt * 128:(tt + 1) * 128, :], in_=x3t[:], reads=[bx3])
        router_tile(P, x3t, bx3, gfb, bgfb, wr, bwr, brb, bbrb, rings, aff_out, affT_out, tt)
    return


def to_local(full_seq, h):
    return local_view(full_seq, h, OWN)


def from_local(parts):
    out = []
    for b in range(NCORES // 2):
        a0 = parts[2 * b]
        a1 = parts[2 * b + 1][::-1]
        out.append(np.concatenate([a0, a1], axis=0))
    return np.stack(out)


G32 = 32
NKL = OWN // 8
NWL = NKL // 8


def emit_phaseC2(P, io):
    with contextlib.ExitStack() as ph:
        P.cur = ph
        _phaseC2_body(P, io)
        P.barrier()
    P.cur = None


def _phaseC2_body(P, io):
    dve, act, pe, pool, sp = P.dve, P.act, P.pe, P.pool, P.sp
    s5p, s5B, s5C = io["s5p"], io["s5B"], io["s5C"]
    HN, ZS = io["hn"], io["zs_out"]
    SAo, SAall = io["sa_own"], io["sa_all"]

    MS, RS = io["ms"], io["rs"]
    bM = Buf()
    bR = Buf()
    U = P.sb([128, 64, NKL], BF16, name="U")
    bU = Buf()
    A8 = [P.sb([128, 2, 2, G32], F32, name="A8_%d" % d) for d in range(2)]
    bA8 = Buf()
    mk = P.sb([128, 2, 128], F32, name="mk")
    bmk = Buf()
    sp.dma(out=mk[:], in_=io["masks"], writes=[bmk])
    flg = P.sb([128, 2], F32, name="flg")
    bflg = Buf()
    sp.dma(out=flg[:], in_=io["flags"], writes=[bflg])
    hpi = P.sb([128, 1], F32, name="hpi")
    bhpi = Buf()
    dve.op(lambda e: e.memset(hpi[:], HALF_PI), [], [bhpi])
    g1 = P.sb([128, G32, 16], F32, name="g1")
    g2 = P.sb([128, G32, 16], F32, name="g2")
    bg1, bg2 = Buf(), Buf()
    pw_t = [P.sb([128, 9, 2, G32], F32, name="pw%d" % d) for d in range(2)]
    ipw_t = [P.sb([128, 9, 2, G32], F32, name="ipw%d" % d) for d in range(2)]
    bbr_t = [P.sb([128, G32, 16], F32, name="bbr%d" % d) for d in range(2)]
    bbi_t = [P.sb([128, G32, 16], F32, name="bbi%d" % d) for d in range(2)]
    with contextlib.ExitStack() as stp:
        M = P.sb([128, 64, 128], BF16, name="M", stack=stp)
        Rt = [[P.sb([128, G32, 128], BF16, name="R%d%d" % (d, r), stack=stp) for r in range(2)] for d in range(2)]
        par = P.sb([128, 2, 3, G32], F32, name="par", stack=stp)
        bpar = Buf()
        sp.dma(out=par[:], in_=s5p, writes=[bpar])
        Bt = P.sb([128, 2, 2, G32, 16], F32, name="Bt", stack=stp)
        bBt = Buf()
        sp.dma(out=Bt[:], in_=s5B, writes=[bBt])
        Ct = P.sb([128, 2, 2, G32, 16], F32, name="Ct", stack=stp)
        bCt = Buf()
        sp.dma(out=Ct[:], in_=s5C, writes=[bCt])

        def small():
            return P.sb([128, G32], F32, stack=stp), Buf()

        def T2(a, ba, b, bb, op):
            o, bo = small()
            dve.op(tt_fn(o[:], a[:], b[:], op), [ba, bb], [bo])
            return o, bo

        def bc(a):
            return bass.AP(a.tensor, a.offset, [list(a.ap[0]), [1, G32], [0, 16]])

        pws, ipws, bbs = [], [], []
        for d in range(2):
            lamr, lami, ldt = par[:, d, 0, :], par[:, d, 1, :], par[:, d, 2, :]
            dt, bdt = small()
            act.op(act_fn(dt[:], ldt, AF.Exp), [bpar], [bdt])
            a_, ba_ = small()
            dve.op(tt_fn(a_[:], lamr, dt[:], ALU.mult), [bpar, bdt], [ba_])
            ang, bang = small()
            dve.op(tt_fn(ang[:], lami, dt[:], ALU.mult), [bpar, bdt], [bang])
            mag, bmag = small()
            act.op(act_fn(mag[:], a_[:], AF.Exp, scale=1.0 / 16), [ba_], [bmag])
            s16, bs16 = small()
            act.op(act_fn(s16[:], ang[:], AF.Sin, scale=1.0 / 16), [bang], [bs16])
            c16, bc16 = small()
            act.op(act_fn(c16[:], ang[:], AF.Sin, scale=1.0 / 16, bias=hpi[:, 0:1]), [bang, bhpi], [bc16])
            re, bre = T2(mag, bmag, c16, bc16, ALU.mult)
            im, bim = T2(mag, bmag, s16, bs16, ALU.mult)
            for _ in range(4):
                r2, br2 = T2(re, bre, re, bre, ALU.mult)
                i2, bi2 = T2(im, bim, im, bim, ALU.mult)
                nre, bnre = T2(r2, br2, i2, bi2, ALU.subtract)
                nim, bnim = small()
                dve.op(stt_fn(nim[:], re[:], 2.0, im[:], ALU.mult, ALU.mult), [bre, bim], [bnim])
                re, bre, im, bim = nre, bnre, nim, bnim
            pw = pw_t[d]
            ipw = ipw_t[d]
            bpw, bipw = Buf(), Buf()
            dve.op(lambda e, pw=pw: e.memset(pw[:, 0, 0, :], 1.0), [], [bpw])
            dve.op(lambda e, pw=pw: e.memset(pw[:, 0, 1, :], 0.0), [], [bpw])
            dve.op(lambda e, ipw=ipw: e.memset(ipw[:, 0, 0, :], 1.0), [], [bipw])
            dve.op(lambda e, ipw=ipw: e.memset(ipw[:, 0, 1, :], 0.0), [], [bipw])
            dve.op(lambda e, pw=pw, re=re: e.tensor_copy(out=pw[:, 1, 0, :], in_=re[:]), [bre], [bpw])
            dve.op(lambda e, pw=pw, im=im: e.tensor_copy(out=pw[:, 1, 1, :], in_=im[:]), [bim], [bpw])
            t1, bt1 = small()
            t2, bt2 = small()
            for n in range(1, 8):
                dve.op(tt_fn(t1[:], pw[:, n, 0, :], re[:], ALU.mult), [bpw, bre], [bt1])
                dve.op(tt_fn(t2[:], pw[:, n, 1, :], im[:], ALU.mult), [bpw, bim], [bt2])
                dve.op(tt_fn(pw[:, n + 1, 0, :], t1[:], t2[:], ALU.subtract), [bt1, bt2], [bpw])
                dve.op(tt_fn(t1[:], pw[:, n, 0, :], im[:], ALU.mult), [bpw, bim], [bt1])
                dve.op(tt_fn(t2[:], pw[:, n, 1, :], re[:], ALU.mult), [bpw, bre], [bt2])
                dve.op(tt_fn(pw[:, n + 1, 1, :], t1[:], t2[:], ALU.add), [bt1, bt2], [bpw])
            en, ben = small()
            for n in range(1, 9):
                act.op(act_fn(en[:], a_[:], AF.Exp, scale=-2.0 * n), [ba_], [ben])
                dve.op(tt_fn(ipw[:, n, 0, :], pw[:, n, 0, :], en[:], ALU.mult), [bpw, ben], [bipw])
                dve.op(stt_fn(ipw[:, n, 1, :], pw[:, n, 1, :], -1.0, en[:], ALU.mult, ALU.mult), [bpw, ben], [bipw])
            a8 = A8[d]
            dve.op(lambda e, a8=a8, pw=pw: e.tensor_copy(out=a8[:, 0, 0, :], in_=pw[:, 8, 0, :]), [bpw], [bA8])
            dve.op(lambda e, a8=a8, pw=pw: e.tensor_copy(out=a8[:, 0, 1, :], in_=pw[:, 8, 0, :]), [bpw], [bA8])
            dve.op(ts_fn(a8[:, 1, 0, :], pw[:, 8, 1, :], -1.0, None, ALU.mult), [bpw], [bA8])
            dve.op(lambda e, a8=a8, pw=pw: e.tensor_copy(out=a8[:, 1, 1, :], in_=pw[:, 8, 1, :]), [bpw], [bA8])
            lr2, blr2 = small()
            dve.op(tt_fn(lr2[:], lamr, lamr, ALU.mult), [bpar], [blr2])
            li2, bli2 = small()
            dve.op(tt_fn(li2[:], lami, lami, ALU.mult), [bpar], [bli2])
            den, bden = T2(lr2, blr2, li2, bli2, ALU.add)
            rden, brden = small()
            dve.op(lambda e, rden=rden, den=den: e.reciprocal(out=rden[:], in_=den[:]), [bden], [brden])
            nr, bnr = small()
            dve.op(ts_fn(nr[:], re[:], -1.0, None, ALU.add), [bre], [bnr])
            u1, bu1 = small()
            dve.op(tt_fn(u1[:], nr[:], lamr, ALU.mult), [bnr, bpar], [bu1])
            u2, bu2 = small()
            dve.op(tt_fn(u2[:], im[:], lami, ALU.mult), [bim, bpar], [bu2])
            u3, bu3 = T2(u1, bu1, u2, bu2, ALU.add)
            fre, bfre = T2(u3, bu3, rden, brden, ALU.mult)
            u4, bu4 = small()
            dve.op(tt_fn(u4[:], im[:], lamr, ALU.mult), [bim, bpar], [bu4])
            u5, bu5 = small()
            dve.op(tt_fn(u5[:], nr[:], lami, ALU.mult), [bnr, bpar], [bu5])
            u6, bu6 = T2(u4, bu4, u5, bu5, ALU.subtract)
            fim, bfim = T2(u6, bu6, rden, brden, ALU.mult)
            bbr = bbr_t[d]
            bbi = bbi_t[d]
            bbb = Buf()
            dve.op(tt_fn(g1[:], Bt[:, d, 0, :, :], bc(fre[:]), ALU.mult), [bBt, bfre], [bg1])
            dve.op(tt_fn(g2[:], Bt[:, d, 1, :, :], bc(fim[:]), ALU.mult), [bBt, bfim], [bg2])
            dve.op(tt_fn(bbr[:], g1[:], g2[:], ALU.subtract), [bg1, bg2], [bbb])
            dve.op(tt_fn(g1[:], Bt[:, d, 1, :, :], bc(fre[:]), ALU.mult), [bBt, bfre], [bg1])
            dve.op(tt_fn(g2[:], Bt[:, d, 0, :, :], bc(fim[:]), ALU.mult), [bBt, bfim], [bg2])
            dve.op(tt_fn(bbi[:], g1[:], g2[:], ALU.add), [bg1, bg2], [bbb])
            pws.append((pw, bpw))
            ipws.append((ipw, bipw))
            bbs.append((bbr, bbi, bbb))

        def cprod(dst_re, dst_im, bdst, xr, xi, bx, tab, btab, idx, neg_im=False):
            bx = list(bx) if isinstance(bx, (list, tuple)) else [bx]
            for i in range(8):
                tr, ti = bc(tab[:, idx(i), 0, :]), bc(tab[:, idx(i), 1, :])
                dve.op(tt_fn(g1[:], xr, tr, ALU.mult), bx + [btab], [bg1])
                dve.op(tt_fn(g2[:], xi, ti, ALU.mult), bx + [btab], [bg2])
                dve.op(tt_fn(dst_re[:, :, i, :], g1[:], g2[:], ALU.subtract), [bg1, bg2], [bdst])
                dve.op(tt_fn(g1[:], xr, ti, ALU.mult), bx + [btab], [bg1])
                dve.op(tt_fn(g2[:], xi, tr, ALU.mult), bx + [btab], [bg2])
                if neg_im:
                    dve.op(stt_fn(dst_im[:, :, i, :], g1[:], -1.0, g2[:], ALU.mult, ALU.subtract), [bg1, bg2], [bdst])
                else:
                    dve.op(tt_fn(dst_im[:, :, i, :], g1[:], g2[:], ALU.add), [bg1, bg2], [bdst])

        v4 = lambda t: t[:].rearrange("p g (j c) -> p g j c", j=8)
        f3 = lambda t: t[:].rearrange("p g s c -> p g (s c)")

        with contextlib.ExitStack() as st2:
            XY = [[P.sb([128, G32, 8, 16], BF16, stack=st2) for _ in range(4)] for _ in range(2)]
            bXY = Buf()
            (pwA, bpwA), (ipwA, bipwA) = pws[0], ipws[0]
            (pwB, bpwB), (ipwB, bipwB) = pws[1], ipws[1]
            cprod(XY[0][0], XY[0][1], bXY, bbs[0][0][:], bbs[0][1][:], bbs[0][2], ipwA, bipwA, lambda s: s)
            cprod(XY[0][2], XY[0][3], bXY, Ct[:, 0, 0, :, :], Ct[:, 0, 1, :, :], bCt, pwA, bpwA, lambda t: t, neg_im=True)
            cprod(XY[1][0], XY[1][1], bXY, bbs[1][0][:], bbs[1][1][:], bbs[1][2], pwB, bpwB, lambda s: s)
            cprod(XY[1][2], XY[1][3], bXY, Ct[:, 1, 0, :, :], Ct[:, 1, 1, :, :], bCt, ipwB, bipwB, lambda t: t, neg_im=True)
            cprod(v4(Rt[0][0]), v4(Rt[0][1]), bR, Ct[:, 0, 0, :, :], Ct[:, 0, 1, :, :], bCt, pwA, bpwA,
                  lambda j: j + 1, neg_im=True)
            cprod(v4(Rt[1][0]), v4(Rt[1][1]), bR, Ct[:, 1, 0, :, :], Ct[:, 1, 1, :, :], bCt, pwB, bpwB,
                  lambda j: 8 - j, neg_im=True)
            pk = [[Ring(P, 1, [128, 512], F32, st2, psum=True) for _ in range(2)] for _ in range(2)]
            tmr = Ring(P, 4, [128, 128], F32, st2)
            for g in range(G32):
                pp = [[None, None], [None, None]]
                for d in range(2):
                    Xr, Xi, Yr, Yn = [f3(t) for t in XY[d]]
                    for hf in range(2):
                        r0 = 64 * hf
                        pt, bpt = pk[d][hf].next()
                        pe.op(mm_fn(pt[:, 0:128], Xr[r0:r0 + 64, g, :], Yr[r0:r0 + 64, g, :], True, False), [bXY], [bpt])
                        pe.op(mm_fn(pt[:, 0:128], Xi[r0:r0 + 64, g, :], Yn[r0:r0 + 64, g, :], False, True), [bXY], [bpt])
                        pp[d][hf] = (pt, bpt)
                for hf in range(2):
                    tm, btm = tmr.next()
                    dve.op(tt_fn(tm[:], pp[0][hf][0][:, 0:128], mk[:, 0, :], ALU.mult), [pp[0][hf][1], bmk], [btm])
                    tm2, btm2 = tmr.next()
                    dve.op(tt_fn(tm2[:], pp[1][hf][0][:, 0:128], mk[:, 1, :], ALU.mult), [pp[1][hf][1], bmk], [btm2])
                    dve.op(tt_fn(M[:, hf * G32 + g, :], tm[:], tm2[:], ALU.add), [btm, btm2], [bM])
            sp.dma(out=MS.ap(), in_=M[:].rearrange("p g c -> p (g c)"), reads=[bM])
            for d in range(2):
                for r in range(2):
                    sp.dma(out=RS.ap()[:, (2 * d + r) * G32 * 128:(2 * d + r + 1) * G32 * 128],
                           in_=Rt[d][r][:].rearrange("p g c -> p (g c)"), reads=[bR])
            P.barrier()

    with contextlib.ExitStack() as st2:
        Tb = Ring(P, 2, [128, 8, D], BF16, st2)
        Tb2 = Ring(P, 1, [128, 64, 128], BF16, st2)
        ptu = Ring(P, 2, [128, 8, 128], BF16, st2, psum=True)
        for kb in range(NKL // 128):
            tb, btb = Tb.next()
            src = HN[kb * 1024:(kb + 1) * 1024, :].rearrange("(p s) d -> p s d", s=8)
            pool.dma(out=tb[:], in_=src, writes=[btb])
            tb2, btb2 = Tb2.next()
            dve.op(lambda e, tb=tb, tb2=tb2: e.tensor_copy(
                out=tb2[:].rearrange("p g (s c) -> p s g c", s=8),
                in_=tb[:].rearrange("p s (g c) -> p s g c", c=16)), [btb], [btb2])
            for g0 in range(0, 64, 8):
                pt, bpt = ptu.next()
                for gi in range(8):
                    g = g0 + gi
                    pe.op(lambda e, pt=pt, gi=gi, g=g, tb2=tb2: e.transpose(pt[:, gi, :], tb2[:, g, :],
                                                                           P.identb[:]), [btb2, P.bidb], [bpt])
                act.op(act_fn(U[:, g0:g0 + 8, kb * 128:(kb + 1) * 128], pt[:], AF.Copy), [bpt], [bU])
        P.barrier()

    with contextlib.ExitStack() as sth:
        hist = [P.sb([128, 2, G32, NKL], BF16, stack=sth) for _ in range(2)]
        bhist = Buf()
        with contextlib.ExitStack() as st3:
            Wz = P.sb([128, G32, 4, 128], BF16, stack=st3)
            bWz = Buf()
            WT = [P.sb([128, G32, 8, 16], BF16, stack=st3) for _ in range(2)]
            bWT = Buf()
            ptw = Ring(P, 2, [128, 4, 128], BF16, st3, psum=True)
            pvr = Ring(P, 3, [128, 2, G32, 8], F32, st3, psum=True)
            S4r = Ring(P, 4, [128, 3, G32], F32, st3)
            p1 = P.sb([128, 2, G32], F32, stack=st3)
            p2 = P.sb([128, 2, G32], F32, stack=st3)
            bp1, bp2 = Buf(), Buf()
            gx = [P.sb([128, 2 * G32], F32, stack=st3) for _ in range(2)]
            bgx = Buf()
            for d in range(2):
                pw, bpw = pws[d]
                cprod(WT[0], WT[1], bWT, bbs[d][0][:], bbs[d][1][:], bbs[d][2], pw, bpw,
                      (lambda s: 7 - s) if d == 0 else (lambda s: s))
                pool.op(lambda e: e.memset(Wz[:], 0.0), [], [bWz])
                W3 = (f3(WT[0]), f3(WT[1]))
                for g in range(G32):
                    pt, bpt = ptw.next()
                    for ri in range(2):
                        pe.op(lambda e, pt=pt, g=g, ri=ri: e.transpose(pt[:, ri, :], W3[ri][:, g, :], P.identb[:]),
                              [bWT, P.bidb], [bpt])
                    act.op(act_fn(Wz[:, g, 0:4:2, 0:64], pt[:, 0:2, 0:64], AF.Copy), [bpt], [bWz])
                    act.op(act_fn(Wz[:, g, 1:4:2, 64:128], pt[:, 0:2, 64:128], AF.Copy), [bpt], [bWz])
                S4, bS = S4r.next()
                if d == 0:
                    dve.op(lambda e, S4=S4: e.memset(S4[:], 0.0), [], [bS])
                else:
                    sp.dma(out=gx[0][:], in_=SAall.ap()[0:128, :], writes=[bgx])
                    sp.dma(out=gx[1][:], in_=SAall.ap()[128:256, :], writes=[bgx])
                    dve.op(ts_fn(gx[0][:], gx[0][:], flg[:, 0:1], None, ALU.mult), [bgx, bflg], [bgx])
                    S2v = S4[:, 0:2, :].rearrange("p r g -> p (r g)")
                    dve.op(stt_fn(S2v, gx[1][:], flg[:, 1:2], gx[0][:], ALU.mult, ALU.add), [bgx, bflg], [bS])
                    dve.op(lambda e, S4=S4: e.tensor_copy(out=S4[:, 2, :], in_=S4[:, 0, :]), [bS], [bS])
                a8 = A8[d]
                for wi in range(NWL):
                    w = wi if d == 0 else NWL - 1 - wi
                    pv, bpv = pvr.next()
                    for g in range(G32):
                        for ri in range(2):
                            pe.op(mm_fn(pv[:, ri, g, :], Wz[:, g, 2 * ri, :], U[:, g, 8 * w:8 * w + 8], True, False),
                                  [bWz, bU], [bpv])
                            pe.op(mm_fn(pv[:, ri, g, :], Wz[:, g, 2 * ri + 1, :], U[:, G32 + g, 8 * w:8 * w + 8],
                                        False, True), [bWz, bU], [bpv])
                    for ji in range(8):
                        jj = ji if d == 0 else 7 - ji
                        k = 8 * w + jj
                        act.op(act_fn(hist[d][:, :, :, k], S4[:, 0:2, :], AF.Copy), [bS], [bhist])
                        Sn, bSn = S4r.next()
                        dve.op(tt_fn(p1[:], a8[:, 0, :, :], S4[:, 0:2, :], ALU.mult), [bA8, bS], [bp1])
                        dve.op(tt_fn(p2[:], a8[:, 1, :, :], S4[:, 1:3, :], ALU.mult), [bA8, bS], [bp2])
                        dve.op(tt_fn(p1[:], p1[:], p2[:], ALU.add), [bp1, bp2], [bp1])
                        dve.op(tt_fn(Sn[:, 0:2, :], p1[:], pv[:, :, :, jj], ALU.add), [bp1, bpv], [bSn])
                        dve.op(lambda e, Sn=Sn: e.tensor_copy(out=Sn[:, 2, :], in_=Sn[:, 0, :]), [bSn], [bSn])
                        S4, bS = Sn, bSn
                if d == 0:
                    sp.dma(out=SAo.ap(), in_=S4[:, 0:2, :].rearrange("p r g -> p (r g)"), reads=[bS])
                    P.all_gather_pair(SAo, SAall)
            P.barrier()
        with contextlib.ExitStack() as st4:
            Z = P.sb([128, 8, D // 2], F32, stack=st4)
            bZ = Buf()
            M = P.sb([128, 64, 128], BF16, stack=st4)
            Rt = [[P.sb([128, G32, 128], BF16, stack=st4) for r in range(2)] for d in range(2)]
            bM, bR = Buf(), Buf()
            sp.dma(out=M[:].rearrange("p g c -> p (g c)"), in_=MS.ap(), writes=[bM])
            for d in range(2):
                for r in range(2):
                    sp.dma(out=Rt[d][r][:].rearrange("p g c -> p (g c)"),
                           in_=RS.ap()[:, (2 * d + r) * G32 * 128:(2 * d + r + 1) * G32 * 128], writes=[bR])
            p1r = Ring(P, 2, [128, 512], F32, st4, psum=True)
            p2r = [Ring(P, 1, [128, 512], F32, st4, psum=True) for _ in range(2)]
            pTr = Ring(P, 2, [128, 4, 128], F32, st4, psum=True)
            c1r = Ring(P, 2, [128, 512], F32, st4)
            ygr = Ring(P, 2, [128, 512], F32, st4)
            for kb in range(NKL // 128):
                ks = slice(kb * 128, (kb + 1) * 128)
                for hf in range(2):
                    r0 = 64 * hf
                    for q in range(G32 // 4):
                        P1, bP1 = p1r.next()
                        P2, bP2 = p2r[hf].next()
                        for gi in range(4):
                            g32 = 4 * q + gi
                            g = hf * G32 + g32
                            cs = slice(gi * 128, (gi + 1) * 128)
                            pe.op(mm_fn(P1[:, cs], M[:, g, :], U[:, g, ks], True, True), [bM, bU], [bP1])
                            seq = [(Rt[0][0], hist[0], 0), (Rt[0][1], hist[0], 1), (Rt[1][0], hist[1], 0),
                                   (Rt[1][1], hist[1], 1)]
                            for n, (Rm, hs, ri) in enumerate(seq):
                                pe.op(mm_fn(P2[:, cs], Rm[r0:r0 + 64, g32, :], hs[r0:r0 + 64, ri, g32, ks],
                                            n == 0, n == 3), [bR, bhist], [bP2])
                        c1, bc1 = c1r.next()
                        act.op(act_fn(c1[:], P1[:], AF.Copy), [bP1], [bc1])
                        yg, byg = ygr.next()
                        dve.op(tt_fn(yg[:], P2[:], c1[:], ALU.add), [bP2, bc1], [byg])
                        pT, bpT = pTr.next()
                        for gi in range(4):
                            pe.op(lambda e, pT=pT, gi=gi, yg=yg: e.transpose(pT[:, gi, :], yg[:, gi * 128:(gi + 1) * 128],
                                                                             P.identf), [byg, P.bcst], [bpT])
                        c0 = 16 * (4 * q)
                        zdst = Z[:, :, c0:c0 + 64].rearrange("p t (g c) -> p t g c", g=4)
                        zsrc = pT[:].rearrange("p g (t c) -> p t g c", t=8)
                        act.op(act_fn(zdst, zsrc, AF.Copy), [bpT], [bZ])
                    dst = ZS[kb * 1024:(kb + 1) * 1024, hf * 512:(hf + 1) * 512].rearrange("(p t) d -> p t d", t=8)
                    sp.dma(out=dst, in_=Z[:], reads=[bZ])


def build_fused(ncores=NCORES):
    P = Prog()
    P.ncores = ncores
    xw = P.inp("xw", [WIN, D])
    g0 = P.inp("g0", [1, D])
    w_in = P.inp("w_in", [D, 2560])
    cvec = P.inp("cvec", [128, 4, 34])
    qk_g = P.inp("qk_g", [128, 2])
    w_out = P.inp("w_out", [D, D])
    eb = P.inp("eb", [4, 128, 6, 384])
    cst_in = P.inp("cst", [128, 6, 128])
    gf = P.inp("gf", [2, D])
    w_r = P.inp("w_r", [2, D, 16])
    b_r = P.inp("b_r", [2, 16])
    wg = P.inp("wg", [2, NEXP, D, D])
    wu = P.inp("wu", [2, NEXP, D, D])
    wd = P.inp("wd", [2, NEXP, D, D])
    gn = P.inp("gn", [1, D])
    s5p = P.inp("s5p", [128, 2, 3, G32])
    s5B = P.inp("s5B", [128, 2, 2, G32, 16])
    s5C = P.inp("s5C", [128, 2, 2, G32, 16])
    dvec = P.inp("dvec", [1, D])
    masks = P.inp("masks", [128, 2, 128])
    flags = P.inp("flags", [128, 2])
    w_glu = P.inp("w_glu", [D, 2 * D])
    out = P.outp("out", [OWN, D])
    X1 = P.dram("X1", [OWN, D])
    X2 = P.dram("X2", [OWN, D])
    X3 = P.dram("X3", [OWN, D])
    HN = P.dram("HN", [OWN, D])
    ZS = P.dram("ZS", [OWN, D])
    AFF = P.dram("AFF", [OWN, NEXP])
    ATo = P.dram("ATo", [NEXP, OWN])
    ATall = P.dram("ATall", [2 * NEXP, OWN])
    SAo = P.dram("SAo", [128, 2 * G32])
    SAall = P.dram("SAall", [256, 2 * G32])
    MS = P.dram("MS", [128, 64 * 128], BF16)
    RS = P.dram("RS", [128, 4 * G32 * 128], BF16)
    Xc = P.dram("Xc", [NEXP * CAP, D], BF16)
    Yc = P.dram("Yc", [NEXP * CAP, D], BF16)
    load_consts(P, cst_in)
    if SPARSE:
        with contextlib.ExitStack() as zs:
            zt = P.sb([128, 8 * D], BF16, name="zeros", stack=zs)
            bzt = Buf()
            P.dve.op(lambda e: e.memset(zt[:], 0.0), [], [bzt])
            for i in range(NEXP * CAP // 1024):
                P.sp.dma(out=Xc.ap()[i * 1024:(i + 1) * 1024, :].rearrange("(p s) d -> p (s d)", p=128), in_=zt[:],
                         reads=[bzt])
                P.sp.dma(out=Yc.ap()[i * 1024:(i + 1) * 1024, :].rearrange("(p s) d -> p (s d)", p=128), in_=zt[:],
                         reads=[bzt])
            P.barrier()
    emit_phaseA(P, dict(xw=xw, g0=g0, w_in=w_in, cvec=cvec, qk_g=qk_g, w_out=w_out, gf=gf[0:1, :], w_r=w_r[0],
                        b_r=b_r[0:1, :], eb=eb, x1_out=X1.ap(), aff_out=AFF.ap(), affT_out=ATo.ap()))
    P.all_gather_pair(ATo, ATall)
    at_view = ATall.ap().rearrange("(r e) t -> e r t", r=2)
    emitB = emit_phaseB_sparse if SPARSE else emit_phaseB
    emitB(P, dict(x1=X1.ap(), affT_seq=at_view, aff_own=AFF.ap(), gf=gf[0:1, :], wg=wg[0], wu=wu[0], wd=wd[0],
                  x2_out=X2.ap(), gn=gn, hn_out=HN.ap(), xc=Xc.ap(), yc=Yc.ap()))
    emit_phaseC2(P, dict(s5p=s5p, s5B=s5B, s5C=s5C, masks=masks, flags=flags, hn=HN.ap(), zs_out=ZS.ap(),
                         sa_own=SAo, sa_all=SAall, ms=MS, rs=RS))
    emit_phaseD(P, dict(zs=ZS.ap(), hn=HN.ap(), dvec=dvec, x2=X2.ap(), w_glu=w_glu, gf=gf[1:2, :], w_r=w_r[1],
                        b_r=b_r[1:2, :], x3_out=X3.ap(), aff_out=AFF.ap(), affT_out=ATo.ap()))
    P.all_gather_pair(ATo, ATall)
    emitB(P, dict(x1=X3.ap(), affT_seq=at_view, aff_own=AFF.ap(), gf=gf[1:2, :], wg=wg[1], wu=wu[1], wd=wd[1],
                  x2_out=out, xc=Xc.ap(), yc=Yc.ap()))
    return P.finish()


def fused_inputs(c, x, mix_norm_even, w_in, conv_w, conv_b, conv_ln_g, conv_ln_b, q_norm, k_norm,
                 w_out, mix_norm_odd, ssm_lam_re, ssm_lam_im, ssm_log_dt, ssm_b_re, ssm_b_im,
                 ssm_c_re, ssm_c_im, ssm_d, w_glu, ffn_norm, w_router, b_router,
                 w_e_gate, w_e_up, w_e_down, shared):
    b, h = c // 2, c % 2
    cw = conv_w[0] if h == 0 else conv_w[0][::-1]
    cvec = np.zeros((128, 4, 34), np.float32)
    cvec[:, :, 0:31] = cw.T.reshape(4, 128, 31).transpose(1, 0, 2)
    cvec[:, :, 31] = conv_b[0].reshape(4, 128).T
    cvec[:, :, 32] = conv_ln_g[0].reshape(4, 128).T
    cvec[:, :, 33] = conv_ln_b[0].reshape(4, 128).T
    qk = np.stack([np.tile(q_norm[0], 2), np.tile(k_norm[0], 2)], axis=1).astype(np.float32)
    order = [0, 1] if h == 0 else [1, 0]

    def gp(a):
        a = a[order]
        return a.reshape(2, 2, G32, 64).transpose(1, 3, 0, 2).reshape(128, 2, G32)

    ldt = np.broadcast_to(ssm_log_dt[0][:, :, None], (2, 64, 64))
    s5p = np.ascontiguousarray(np.stack([gp(ssm_lam_re[0]), gp(ssm_lam_im[0]), gp(ldt)], axis=2))

    def gB(a):
        a = a[order]
        return a.reshape(2, 2, G32, 64, 16).transpose(1, 3, 0, 2, 4).reshape(128, 2, G32, 16)

    def gC(a):
        a = a[order]
        return a.reshape(2, 2, G32, 16, 64).transpose(1, 4, 0, 2, 3).reshape(128, 2, G32, 16)

    s5B = np.ascontiguousarray(np.stack([gB(ssm_b_re[0]), gB(ssm_b_im[0])], axis=2))
    s5C = np.ascontiguousarray(np.stack([gC(ssm_c_re[0]), gC(ssm_c_im[0])], axis=2))
    flags = np.zeros((128, 2), np.float32)
    flags[:, 1 - h] = 1.0
    m = dict(shared)
    m.update({
        "xw": local_view(x[b], h, WIN), "cvec": cvec, "qk_g": np.ascontiguousarray(qk),
        "s5p": s5p, "s5B": s5B, "s5C": s5C, "flags": flags,
    })
    return m


def kernel(x, mix_norm_even, w_in, conv_w, conv_b, conv_ln_g, conv_ln_b, q_norm, k_norm,
           w_out, mix_norm_odd, ssm_lam_re, ssm_lam_im, ssm_log_dt, ssm_b_re, ssm_b_im,
           ssm_c_re, ssm_c_im, ssm_d, w_glu, ffn_norm, w_router, b_router,
           w_e_gate, w_e_up, w_e_down):
    f = lambda a: np.ascontiguousarray(np.asarray(a, dtype=np.float32))
    args = [f(a) for a in (x, mix_norm_even, w_in, conv_w, conv_b, conv_ln_g, conv_ln_b, q_norm, k_norm,
                           w_out, mix_norm_odd, ssm_lam_re, ssm_lam_im, ssm_log_dt, ssm_b_re, ssm_b_im,
                           ssm_c_re, ssm_c_im, ssm_d, w_glu, ffn_norm, w_router, b_router,
                           w_e_gate, w_e_up, w_e_down)]
    (x, mix_norm_even, w_in, conv_w, conv_b, conv_ln_g, conv_ln_b, q_norm, k_norm,
     w_out, mix_norm_odd, ssm_lam_re, ssm_lam_im, ssm_log_dt, ssm_b_re, ssm_b_im,
     ssm_c_re, ssm_c_im, ssm_d, w_glu, ffn_norm, w_router, b_router, w_e_gate, w_e_up, w_e_down) = args
    shared = {
        "g0": np.ascontiguousarray(mix_norm_even[0][None, :]), "w_in": w_in[0], "w_out": w_out[0],
        "eb": host_eb(), "cst": host_consts(), "gf": ffn_norm, "w_r": w_router, "b_r": b_router,
        "wg": w_e_gate, "wu": w_e_up, "wd": w_e_down, "gn": np.ascontiguousarray(mix_norm_odd[0][None, :]),
        "dvec": np.ascontiguousarray(ssm_d[0][None, :]), "masks": host_masks(), "w_glu": w_glu[0],
    }
    nc = get_prog("F", build_fused)
    in_maps = [fused_inputs(c, *args, shared) for c in range(NCORES)]
    res = run_bass_kernel_spmd(nc, in_maps, core_ids=list(range(NCORES)))
    return from_local([r["out"] for r in res.results]).astype(np.float32)


BIGIDX = float(1 << 20)
I32 = mybir.dt.int32


def emit_phaseB_sparse(P, io):
    with contextlib.ExitStack() as ph:
        P.cur = ph
        _phaseB_sparse_body(P, io["x1"], io["affT_seq"], io["aff_own"], io["gf"], io["wg"], io["wu"], io["wd"],
                            io["x2_out"], io.get("gn"), io.get("hn_out"), io["xc"], io["yc"])
        P.barrier()
    P.cur = None


def _phaseB_sparse_body(P, x1, affT_seq, aff_own, gf, wg, wu, wd, x2_out, gn, hn_out, Xc, Yc):
    dve, act, pe, pool, sp = P.dve, P.act, P.pe, P.pool, P.sp
    NT = OWN // 128
    if gn is not None:
        gnb = P.sb([128, D], F32, name="gnb")
        bgnb = Buf()
        sp.dma(out=gnb[:], in_=bcast_rows(gn, 128), writes=[bgnb])
    gfb = P.sb([128, D], F32, name="gfb")
    bgfb = Buf()
    sp.dma(out=gfb[:], in_=bcast_rows(gf, 128), writes=[bgfb])
    gate = P.sb([128, NT, NEXP], F32, name="gate")
    bgate = Buf()
    idx = P.sb([128, NT, NEXP], I32, name="idx")
    bidx = Buf()
    if P.bcreg is None:
        P.bcreg = P.nc.gpsimd.alloc_register("bcreg")
        P.nc.gpsimd.reg_mov(P.bcreg, NEXP * CAP - 1)

    with contextlib.ExitStack() as st:
        pthr = P.ps([128, 512], F32, stack=st)
        bpthr = Buf()
        aT = P.sb([NEXP, SEQ], F32, stack=st)
        baT = Buf()
        sp.dma(out=aT[:].rearrange("e (r t) -> e r t", r=2), in_=affT_seq, writes=[baT])
        junk = P.sb([NEXP, SEQ], F32, stack=st)
        bjunk = Buf()
        lo = P.sb([NEXP, 1], F32, stack=st)
        blo = Buf()
        mid = P.sb([NEXP, 1], F32, stack=st)
        bmid = Buf()
        cnt = P.sb([NEXP, 1], F32, stack=st)
        bcnt = Buf()
        ge = P.sb([NEXP, 1], F32, stack=st)
        bge = Buf()
        dve.op(lambda e: e.memset(lo[:], 0.0), [], [blo])
        for it in range(30):
            wk = 2.0 ** (-(it + 1))
            dve.op(ts_fn(mid[:], lo[:], wk, None, ALU.add), [blo], [bmid])
            dve.op(lambda e: e.tensor_scalar(out=junk[:], in0=aT[:], scalar1=mid[:, 0:1], scalar2=0.0,
                                             op0=ALU.is_ge, op1=ALU.add, accum_out=cnt[:, 0:1]),
                   [baT, bmid], [bjunk, bcnt])
            dve.op(ts_fn(ge[:], cnt[:], CAP - 0.5, None, ALU.is_ge), [bcnt], [bge])
            dve.op(stt_fn(lo[:], ge[:], wk, lo[:], ALU.mult, ALU.add), [bge, blo], [blo])
        dthr = P.sb([NEXP, NEXP], F32, stack=st)
        bdthr = Buf()
        dve.op(ts_fn(dthr[:], P.cst[0:NEXP, 0, 0:NEXP], lo[:, 0:1], None, ALU.mult), [P.bcst, blo], [bdthr])
        pe.op(mm_fn(pthr[:, 0:NEXP], P.cst[0:NEXP, 3, :], dthr[:], True, True), [P.bcst, bdthr], [bpthr])
        thrb = P.sb([128, NEXP], F32, stack=st)
        bthrb = Buf()
        act.op(act_fn(thrb[:], pthr[:, 0:NEXP], AF.Copy), [bpthr], [bthrb])
        ao = P.sb([128, NT, NEXP], F32, stack=st)
        bao = Buf()
        sp.dma(out=ao[:], in_=aff_own.rearrange("(t p) e -> p t e", p=128), writes=[bao])
        msk = P.sb([128, NT, NEXP], F32, stack=st)
        bmsk = Buf()

        def bct(t):
            a = t[:]
            return bass.AP(a.tensor, a.offset, [list(a.ap[0]), [0, NT], [1, NEXP]])

        dve.op(tt_fn(msk[:], ao[:], bct(thrb), ALU.is_ge), [bao, bthrb], [bmsk])
        ppos = P.ps([128, 512], F32, stack=st)
        bppos = Buf()
        pcnt = P.ps([128, 512], F32, stack=st)
        bpcnt = Buf()
        m2 = msk[:].rearrange("p t e -> p (t e)")
        pe.op(mm_fn(ppos[:, 0:NT * NEXP], P.cst[:, 4, :], m2, True, True), [P.bcst, bmsk], [bppos])
        pe.op(mm_fn(pcnt[:, 0:NT * NEXP], P.cst[:, 3, :], m2, True, True), [P.bcst, bmsk], [bpcnt])
        csb = P.sb([128, NT, NEXP], F32, stack=st)
        bcsb = Buf()
        act.op(act_fn(csb[:].rearrange("p t e -> p (t e)"), pcnt[:, 0:NT * NEXP], AF.Copy), [bpcnt], [bcsb])
        off = P.sb([128, NT, NEXP], F32, stack=st)
        boff = Buf()
        dve.op(lambda e: e.memset(off[:, 0, :], 0.0), [], [boff])
        for i in range(1, NT):
            dve.op(tt_fn(off[:, i, :], off[:, i - 1, :], csb[:, i - 1, :], ALU.add), [boff, bcsb], [boff])
        pos = P.sb([128, NT, NEXP], F32, stack=st)
        bpos = Buf()
        dve.op(tt_fn(pos[:].rearrange("p t e -> p (t e)"), ppos[:, 0:NT * NEXP],
                     off[:].rearrange("p t e -> p (t e)"), ALU.add), [bppos, boff], [bpos])
        ok = P.sb([128, NT, NEXP], F32, stack=st)
        bok = Buf()
        dve.op(ts_fn(ok[:], pos[:], CAP - 0.5, None, ALU.is_lt), [bpos], [bok])
        dve.op(tt_fn(msk[:], msk[:], ok[:], ALU.mult), [bmsk, bok], [bmsk])
        dve.op(tt_fn(gate[:], msk[:], ao[:], ALU.mult), [bmsk, bao], [bgate])
        ebase = P.cst[:, 5, 0:NEXP]
        eb_bc = bass.AP(ebase.tensor, ebase.offset, [list(ebase.ap[0]), [0, NT], [1, NEXP]])
        dve.op(tt_fn(pos[:], pos[:], eb_bc, ALU.add), [bpos, P.bcst], [bpos])
        dve.op(ts_fn(pos[:], pos[:], -BIGIDX, None, ALU.add), [bpos], [bpos])
        dve.op(tt_fn(pos[:], pos[:], msk[:], ALU.mult), [bpos, bmsk], [bpos])
        dve.op(ts_fn(pos[:], pos[:], BIGIDX, None, ALU.add), [bpos], [bpos])
        dve.op(lambda e: e.tensor_copy(out=idx[:], in_=pos[:]), [bpos], [bidx])
        P.barrier()

    def indirect(out, out_off, in_, in_off, reads, writes):
        s_ = pool
        if s_.dsems is None:
            s_.dsems = [P.newsem("d%d_%s" % (i, s_.name)) for i in range(s_.nslots)]
        j = s_.dn
        slot = j % s_.nslots
        key = (s_, slot)
        prev = 16 * (j // s_.nslots)
        if prev > 0:
            s_._wait((key, s_.dsems[slot], prev))
        s_._deps(reads, writes, True)
        ins = s_.eng.indirect_dma_start(out=out, out_offset=out_off, in_=in_, in_offset=in_off,
                                        bounds_check=P.bcreg, oob_is_err=False)
        ins.then_inc(s_.dsems[slot], 16)
        s_.dn += 1
        s_._mark((key, s_.dsems[slot], prev + 16), reads, writes)

    with contextlib.ExitStack() as st:
        acc = P.sb([128, NT, D], F32, stack=st)
        bacc = [Buf() for _ in range(NT)]
        bXc = [Buf() for _ in range(NEXP)]
        bYc = [Buf() for _ in range(NEXP)]
        small = Ring(P, 8, [128, 1], F32, st)
        with contextlib.ExitStack() as st1:
            junk = Ring(P, 1, [128, D], F32, st1)
            hnr = Ring(P, 3, [128, D], BF16, st1)
            for t in range(NT):
                sp.dma(out=acc[:, t, :], in_=x1[t * 128:(t + 1) * 128, :], writes=[bacc[t]])
                rs, br = rms_rstd(P, acc[:, t, :], bacc[t], D, junk, small)
                hn, bhn = hnr.next()
                dve.op(stt_fn(hn[:], acc[:, t, :], rs[:, 0:1], gfb[:], ALU.mult, ALU.mult), [bacc[t], br, bgfb], [bhn])
                for e in range(NEXP):
                    indirect(Xc[:, :], bass.IndirectOffsetOnAxis(ap=idx[:, t, e:e + 1], axis=0), hn[:, :], None,
                             [bhn, bidx], [bXc[e]])
            P.barrier()
        wgr = Ring(P, 2, [128, 8, D], BF16, st)
        wur = Ring(P, 2, [128, 8, D], BF16, st)
        wdr = Ring(P, 1, [128, 8, D], BF16, st)
        xsr = Ring(P, 1, [128, 4, D], BF16, st)
        xTr = Ring(P, 1, [128, 8, CAP], BF16, st)
        atr = Ring(P, 1, [128, 8, CAP], BF16, st)
        ysr = Ring(P, 1, [128, 4, D], BF16, st)
        sgr = Ring(P, 1, [128, 512], F32, st)
        ybr = Ring(P, 3, [128, D], BF16, st)
        ptr = Ring(P, 2, [128, 8, 128], BF16, st, psum=True)
        pgr = Ring(P, 2, [128, 512], F32, st, psum=True)
        pur = Ring(P, 2, [128, 512], F32, st, psum=True)
        pyr = Ring(P, 2, [128, 512], F32, st, psum=True)
        for yb_, byb_ in zip(ybr.t, ybr.b):
            dve.op(lambda e, yb_=yb_: e.memset(yb_[:], 0.0), [], [byb_])

        def load_w(e, which=((0, 1, 2))):
            wts = []
            for src, ring in [((wg, wgr), (wu, wur), (wd, wdr))[i_] for i_ in which]:
                w, bw = ring.next()
                sv = src[e].rearrange("(k p) f -> p k f", p=128)
                pool.dma(out=w[:, 0:4, :], in_=sv[:, 0:4, :], writes=[bw])
                pool.dma(out=w[:, 4:8, :], in_=sv[:, 4:8, :], writes=[bw])
                wts.append((w, bw))
            return wts

        def gather_back(e):
            for t in range(NT):
                yb, byb = ybr.next()
                indirect(yb[:, :], None, Yc[:, :], bass.IndirectOffsetOnAxis(ap=idx[:, t, e:e + 1], axis=0),
                         [bYc[e], bidx], [byb])
                dve.op(stt_fn(acc[:, t, :], yb[:], gate[:, t, e:e + 1], acc[:, t, :], ALU.mult, ALU.add),
                       [byb, bgate, bacc[t]], [bacc[t]])

        nxt = load_w(0, (0, 1))
        for e in range(NEXP):
            (Wg, bWg), (Wu, bWu) = nxt
            ((Wd, bWd),) = load_w(e, (2,))
            if e + 1 < NEXP:
                nxt = load_w(e + 1, (0, 1))
            xs, bxs = xsr.next()
            sp.dma(out=xs[:], in_=Xc[e * CAP:(e + 1) * CAP, :].rearrange("(s p) d -> p s d", p=128),
                   reads=[bXc[e]], writes=[bxs])
            xT, bxT = xTr.next()
            for sl in range(4):
                pt, bpt = ptr.next()
                for k in range(8):
                    pe.op(lambda en, pt=pt, k=k, xs=xs, sl=sl: en.transpose(pt[:, k, :], xs[:, sl, k * 128:(k + 1) * 128],
                                                                           P.identb[:]), [bxs, P.bidb], [bpt])
                act.op(act_fn(xT[:, :, sl * 128:(sl + 1) * 128], pt[:], AF.Copy), [bpt], [bxT])
            AT, bAT = atr.next()
            for fc in range(8):
                pg, bpg = pgr.next()
                pu, bpu = pur.next()
                for k in range(8):
                    pe.op(mm_fn(pg[:], Wg[:, k, fc * 128:(fc + 1) * 128], xT[:, k, :], k == 0, k == 7), [bWg, bxT], [bpg])
                for k in range(8):
                    pe.op(mm_fn(pu[:], Wu[:, k, fc * 128:(fc + 1) * 128], xT[:, k, :], k == 0, k == 7), [bWu, bxT], [bpu])
                sg, bsg = sgr.next()
                act.op(act_fn(sg[:], pg[:], AF.Silu), [bpg], [bsg])
                dve.op(tt_fn(AT[:, fc, :], sg[:], pu[:], ALU.mult), [bsg, bpu], [bAT])
            ys, bys = ysr.next()
            for sl in range(4):
                for hc in range(2):
                    py, bpy = pyr.next()
                    for fc in range(8):
                        pe.op(mm_fn(py[:], AT[:, fc, sl * 128:(sl + 1) * 128], Wd[:, fc, hc * 512:(hc + 1) * 512],
                                    fc == 0, fc == 7), [bAT, bWd], [bpy])
                    act.op(act_fn(ys[:, sl, hc * 512:(hc + 1) * 512], py[:], AF.Copy), [bpy], [bys])
            sp.dma(out=Yc[e * CAP:(e + 1) * CAP, :].rearrange("(s p) d -> p s d", p=128), in_=ys[:],
                   reads=[bys], writes=[bYc[e]])
            if e >= 1:
                gather_back(e - 1)
        gather_back(NEXP - 1)
        hor = Ring(P, 1, [128, D], F32, st)
        junk = hor
        for t in range(NT):
            sp.dma(out=x2_out[t * 128:(t + 1) * 128, :], in_=acc[:, t, :], reads=[bacc[t]])
            if gn is not None:
                rs, br = rms_rstd(P, acc[:, t, :], bacc[t], D, junk, small)
                ho, bho = hor.next()
                dve.op(stt_fn(ho[:], acc[:, t, :], rs[:, 0:1], gnb[:], ALU.mult, ALU.mult),
                       [bacc[t], br, bgnb], [bho])
                sp.dma(out=hn_out[t * 128:(t + 1) * 128, :], in_=ho[:], reads=[bho])
```

```python
import numpy as np
import ml_dtypes
import contextlib
import concourse.bass as bass
import concourse.mybir as mybir
from concourse.bass_utils import run_bass_kernel_spmd

F32 = mybir.dt.float32
BF16 = mybir.dt.bfloat16
ALU = mybir.AluOpType
AF = mybir.ActivationFunctionType
AX = mybir.AxisListType

NCORES = 8
D = 1024
SEQ = 4096
OWN = 2048
WIN = 3072
EPS = 1e-6
DEBUG = False
SPARSE = True
SCAN_SELFSYNC = False


class Buf:
    __slots__ = ("name", "w", "r")

    def __init__(self, name=""):
        self.name = name
        self.w = None
        self.r = {}


class Stream:
    def __init__(self, P, name, eng):
        self.P = P
        self.name = name
        self.eng = eng
        self.sem = P.newsem("c_" + name)
        self.cnt = 0
        self.seen = {}
        self.nslots = 8
        self.dsems = None
        self.dn = 0
        self.selfsync = name != "pe"

    def _wait(self, tok):
        key, sem, val = tok
        if self.seen.get(key, 0) < val:
            self.eng.wait_ge(sem, val)
            self.seen[key] = val

    def _deps(self, reads, writes, dma):
        toks = []
        for b in reads:
            if b.w is not None:
                toks.append(b.w)
        for b in writes:
            if b.w is not None:
                toks.append(b.w)
            toks.extend(b.r.values())
        for t in toks:
            if dma or t[0] is not self or self.selfsync:
                self._wait(t)

    def _mark(self, tok, reads, writes):
        for b in reads:
            b.r[tok[0]] = tok
        for b in writes:
            b.w = tok
            b.r = {}

    def op(self, fn, reads=(), writes=()):
        if self.P.stopped:
            return None
        self._deps(reads, writes, False)
        ins = fn(self.eng)
        self.cnt += 1
        ins.then_inc(self.sem, 1)
        self._mark((self, self.sem, self.cnt), reads, writes)
        return ins

    def dma(self, out, in_, reads=(), writes=(), **kw):
        if self.P.stopped:
            return None
        if self.dsems is None:
            self.dsems = [self.P.newsem("d%d_%s" % (i, self.name)) for i in range(self.nslots)]
        j = self.dn
        slot = j % self.nslots
        key = (self, slot)
        prev = 16 * (j // self.nslots)
        if prev > 0:
            self._wait((key, self.dsems[slot], prev))
        self._deps(reads, writes, True)
        ins = self.eng.dma_start(out=out, in_=in_, **kw)
        ins.then_inc(self.dsems[slot], 16)
        self.dn += 1
        self._mark((key, self.dsems[slot], prev + 16), reads, writes)
        return ins


class Prog:
    def __init__(self):
        self.nc = bass.Bass("TRN2", target_bir_lowering=False)
        self.stack = contextlib.ExitStack()
        self._n = 0
        self.stopped = False
        self.cur = None
        self.ccsem = None
        self.ccn = 0
        self.ncores = NCORES
        self.bcreg = None
        nc = self.nc
        self.pe = Stream(self, "pe", nc.tensor)
        self.act = Stream(self, "act", nc.scalar)
        self.dve = Stream(self, "dve", nc.vector)
        self.pool = Stream(self, "pool", nc.gpsimd)
        self.sp = Stream(self, "sp", nc.sync)
        self.streams = [self.pe, self.act, self.dve, self.pool, self.sp]

    def newsem(self, name):
        return self.stack.enter_context(self.nc.semaphore(name))

    def inp(self, name, shape, dt=F32):
        return self.nc.dram_tensor(name, list(shape), dt, kind="ExternalInput").ap()

    def outp(self, name, shape, dt=F32):
        return self.nc.dram_tensor(name, list(shape), dt, kind="ExternalOutput").ap()

    def sb(self, shape, dt=F32, name=None, stack=None):
        self._n += 1
        name = "s%d_%s" % (self._n, name or "t")
        return (stack or self.cur or self.stack).enter_context(self.nc.sbuf_tensor(name, list(shape), dt))

    def ps(self, shape, dt=F32, name=None, stack=None):
        self._n += 1
        name = "p%d_%s" % (self._n, name or "t")
        return (stack or self.cur or self.stack).enter_context(self.nc.psum_tensor(name, list(shape), dt))

    def barrier(self):
        toks = []
        for s in self.streams:
            if s.cnt:
                toks.append((s, s.sem, s.cnt))
            if s.dsems is not None:
                for slot in range(s.nslots):
                    n = (s.dn - slot + s.nslots - 1) // s.nslots
                    if n > 0:
                        toks.append(((s, slot), s.dsems[slot], 16 * n))
        for s in self.streams:
            for t in toks:
                if t[0] is not s:
                    s._wait(t)

    def dram(self, name, shape, dt=F32):
        return self.nc.dram_tensor(name, list(shape), dt)

    def all_gather_pair(self, src, dst):
        if self.ccsem is None:
            self.ccsem = self.newsem("ccsem")
            self.ccdummy = self.sb([128, 1], F32, name="ccdummy", stack=self.stack)
            self.bccd = Buf()
        self.barrier()
        groups = [[2 * i, 2 * i + 1] for i in range(self.ncores // 2)]
        self.ccn += 1
        n = self.ccn

        def fn(e):
            e.collective_compute("AllGather", ALU.bypass, replica_groups=groups,
                                 ins=[src.ap().opt()], outs=[dst.ap().opt()]).then_inc(self.ccsem, 1)
            e.wait_ge(self.ccsem, n)
            return e.memset(self.ccdummy[:], 0.0)
        self.pool.op(fn, [], [self.bccd])
        self.barrier()

    def finish(self):
        self.barrier()
        self.stack.close()
        return self.nc


def bcast_rows(ap, nparts):
    n = ap.shape[-1]
    return bass.AP(ap.tensor, ap.offset, [[0, nparts], [1, n]])


class Ring:
    def __init__(self, P, n, shape, dt, stack, psum=False):
        alloc = P.ps if psum else P.sb
        self.t = [alloc(shape, dt, stack=stack) for _ in range(n)]
        self.b = [Buf() for _ in range(n)]
        self.i = -1

    def next(self):
        self.i = (self.i + 1) % len(self.t)
        return self.t[self.i], self.b[self.i]


def act_fn(out, in_, func, **kw):
    return lambda e: e.activation(out=out, in_=in_, func=func, **kw)


def mm_fn(out, lhsT, rhs, start, stop):
    return lambda e: e.matmul(out, lhsT, rhs, start=start, stop=stop)


def tt_fn(out, in0, in1, op):
    return lambda e: e.tensor_tensor(out=out, in0=in0, in1=in1, op=op)


def ts_fn(out, in0, s1, s2, op0, op1=None):
    if op1 is None:
        return lambda e: e.tensor_scalar(out=out, in0=in0, scalar1=s1, scalar2=None, op0=op0)
    return lambda e: e.tensor_scalar(out=out, in0=in0, scalar1=s1, scalar2=s2, op0=op0, op1=op1)


def stt_fn(out, in0, scalar, in1, op0, op1):
    return lambda e: e.scalar_tensor_tensor(out=out, in0=in0, scalar=scalar, in1=in1, op0=op0, op1=op1)


ATT_CFG = (1, 4, 16)


def vtile_list():
    tiles = []
    for ci, d in enumerate(ATT_CFG):
        nb = OWN // (128 * d)
        for c in range(d):
            for j in range(nb + 1):
                i0 = 0 if j == 0 else 128 * j - 64
                tiles.append((ci, d, c, j, c + d * i0))
    return tiles


def rms_rstd(P, x_ap, xb, n, ring_junk, ring_small, pbuf_extra=()):
    junk, bj = ring_junk.next()
    ss, bs = ring_small.next()
    P.act.op(act_fn(junk[:, 0:n], x_ap, AF.Square, accum_out=ss[:, 0:1]), [xb], [bj, bs])
    sd, bd = ring_small.next()
    P.act.op(act_fn(sd[:, 0:1], ss[:, 0:1], AF.Sqrt, scale=1.0 / n, bias=P.eps_t[:, 0:1]), [bs, P.beps], [bd])
    rs, br = ring_small.next()
    P.dve.op(lambda e: e.reciprocal(out=rs[:, 0:1], in_=sd[:, 0:1]), [bd], [br])
    return rs, br


def load_consts(P, cst_in):
    cst = P.sb([128, 6, 128], F32, name="cst")
    P.bcst = Buf("cst")
    P.sp.dma(out=cst[:], in_=cst_in, writes=[P.bcst])
    P.identf = cst[:, 0, :]
    P.ones512 = cst[:, 1, :]
    P.ones64 = cst[:, 2, :]
    P.cst = cst
    idb = P.sb([128, 128], BF16, name="identb")
    P.bidb = Buf("identb")
    P.dve.op(lambda e: e.tensor_copy(out=idb[:], in_=cst[:, 0, :]), [P.bcst], [P.bidb])
    P.identb = idb
    eps_t = P.sb([128, 1], F32, name="eps")
    P.beps = Buf("eps")
    P.dve.op(lambda e: e.memset(eps_t[:], EPS), [], [P.beps])
    P.eps_t = eps_t


def router_tile(P, x1t, bx1, gfb, bgfb, wr, bwr, brb, bbrb, rings, aff_out, affT_out, tt, hf_out=None):
    rs, br = rms_rstd(P, x1t[:], bx1, D, rings["junk"], rings["small"])
    hf, bhf = rings["hf"].next()
    P.dve.op(stt_fn(hf[:], x1t[:], rs[:, 0:1], gfb[:], ALU.mult, ALU.mult), [bx1, br, bgfb], [bhf])
    ptf, bptf = rings["ptf"].next()
    for k in range(8):
        P.pe.op(lambda e, k=k: e.transpose(ptf[:, k, :], hf[:, k * 128:(k + 1) * 128], P.identf),
                [bhf, P.bcst], [bptf])
    hfT, bhfT = rings["hfT"].next()
    P.act.op(act_fn(hfT[:], ptf[:], AF.Copy), [bptf], [bhfT])
    plog, bplog = rings["plog"].next()
    for k in range(8):
        P.pe.op(mm_fn(plog[:, 0:16], hfT[:, k, :], wr[:, k, :], k == 0, k == 7), [bhfT, bwr], [bplog])
    lg, blg = rings["sm16"].next()
    P.dve.op(tt_fn(lg[:], plog[:, 0:16], brb[:], ALU.add), [bplog, bbrb], [blg])
    mx, bmx = rings["small"].next()
    P.dve.op(lambda e: e.reduce_max(out=mx[:, 0:1], in_=lg[:], axis=AX.X, negate=True), [blg], [bmx])
    ex, bex = rings["sm16"].next()
    sm, bsm = rings["small"].next()
    P.act.op(act_fn(ex[:], lg[:], AF.Exp, bias=mx[:, 0:1], scale=1.0, accum_out=sm[:, 0:1]), [blg, bmx], [bex, bsm])
    rsm, brsm = rings["small"].next()
    P.dve.op(lambda e: e.reciprocal(out=rsm[:, 0:1], in_=sm[:, 0:1]), [bsm], [brsm])
    af, baf = rings["sm16"].next()
    P.dve.op(ts_fn(af[:], ex[:], rsm[:, 0:1], None, ALU.mult), [bex, brsm], [baf])
    P.sp.dma(out=aff_out[tt * 128:(tt + 1) * 128, :], in_=af[:], reads=[baf])
    pat, bpat = rings["pat"].next()
    P.pe.op(lambda e: e.transpose(pat[0:16, 0:128], af[:], P.identf), [baf, P.bcst], [bpat])
    aT, baT = rings["aT"].next()
    P.act.op(act_fn(aT[:], pat[0:16, 0:128], AF.Copy), [bpat], [baT])
    P.sp.dma(out=affT_out[:, tt * 128:(tt + 1) * 128], in_=aT[:], reads=[baT])
    return hf, bhf


def emit_phaseA(P, io):
    xw, g0, w_in, cvec, qk_g, w_out = io["xw"], io["g0"], io["w_in"], io["cvec"], io["qk_g"], io["w_out"]
    gf, w_r, b_r, eb = io["gf"], io["w_r"], io["b_r"], io["eb"]
    x1_out, aff_out, affT_out = io["x1_out"], io["aff_out"], io["affT_out"]
    dbg = None
    with contextlib.ExitStack() as ph:
        P.cur = ph
        _phaseA_body(P, xw, g0, w_in, cvec, qk_g, w_out, gf, w_r, b_r, eb, x1_out, aff_out, affT_out)
        P.barrier()
    P.cur = None


def _phaseA_body(P, xw, g0, w_in, cvec, qk_g, w_out, gf, w_r, b_r, eb, x1_out, aff_out, affT_out):
    w_in_v = w_in.rearrange("(k p) e -> p k e", p=128)
    w_out_v = w_out.rearrange("(k p) e -> p k e", p=128)
    w_r_v = w_r.rearrange("(k p) e -> p k e", p=128)

    hT = P.sb([128, 8, WIN], BF16, name="hT")
    bhT = [Buf("hT%d" % i) for i in range(WIN // 128)]
    mixT = P.sb([128, 8, OWN], BF16, name="mixT")
    bmix = [Buf("mix%d" % i) for i in range(8)]
    gb = P.sb([128, D], F32, name="gb")
    bgb = Buf("gb")
    P.sp.dma(out=gb[:], in_=bcast_rows(g0, 128), writes=[bgb])
    cv = P.sb([128, 4, 34], F32, name="cv")
    bcv = Buf("cv")
    P.sp.dma(out=cv[:], in_=cvec, writes=[bcv])
    qkg = P.sb([128, 2], F32, name="qkg")
    bqkg = Buf("qkg")
    P.sp.dma(out=qkg[:], in_=qk_g, writes=[bqkg])

    with contextlib.ExitStack() as st:
        xr = Ring(P, 2, [128, D], F32, st)
        junk = Ring(P, 1, [128, D], F32, st)
        small = Ring(P, 8, [128, 1], F32, st)
        hnr = Ring(P, 2, [128, D], BF16, st)
        ptr = Ring(P, 2, [128, 8, 128], BF16, st, psum=True)
        for tt in range(WIN // 128):
            xt, bx = xr.next()
            P.sp.dma(out=xt[:], in_=xw[tt * 128:(tt + 1) * 128, :], writes=[bx])
            rs, br = rms_rstd(P, xt[:], bx, D, junk, small)
            hn, bhn = hnr.next()
            P.dve.op(stt_fn(hn[:], xt[:], rs[:, 0:1], gb[:], ALU.mult, ALU.mult), [bx, br, bgb], [bhn])
            pt, bpt = ptr.next()
            for k in range(8):
                P.pe.op(lambda e, k=k: e.transpose(pt[:, k, :], hn[:, k * 128:(k + 1) * 128], P.identb[:]),
                        [bhn, P.bidb], [bpt])
            P.act.op(act_fn(hT[:, :, tt * 128:(tt + 1) * 128], pt[:], AF.Copy), [bpt], [bhT[tt]])
        P.barrier()

    def hT_bufs(t0, n):
        return bhT[t0 // 128:(t0 + n + 127) // 128]

    NCV = OWN + 128
    with contextlib.ExitStack() as st:
        hglu = P.sb([128, 4, 15 + NCV], BF16, stack=st)
        bhg = [Buf() for _ in range(4)]
        diag = P.sb([128, 4, 31, 128], BF16, stack=st)
        bdiag = [Buf() for _ in range(4)]
        wcv = Ring(P, 2, [128, 8, 256], BF16, st)
        pmr = Ring(P, 2, [128, 512], F32, st, psum=True)
        pgr = Ring(P, 2, [128, 512], F32, st, psum=True)
        pst = Ring(P, 2, [128, 512], F32, st, psum=True)
        sgr = Ring(P, 2, [128, 512], F32, st)
        vbuf = [Ring(P, 2, [128, 512], F32, st) for _ in range(4)]
        sqr = [Ring(P, 2, [128, 512], F32, st) for _ in range(4)]
        tmp = Ring(P, 10, [128, 512], F32, st)
        zr = Ring(P, 2, [128, 512], F32, st)
        for cc in range(4):
            P.dve.op(lambda e, cc=cc: e.memset(hglu[:, cc, 0:15], 0.0), [], [bhg[cc]])
            for k in range(31):
                P.pool.op(ts_fn(diag[:, cc, k, :], P.identf, cv[:, cc, k:k + 1], None, ALU.mult),
                          [P.bcst, bcv], [bdiag[cc]])
        ntiles = [(i * 512, 512) for i in range(4)] + [(2048, 128)]
        for cc in range(4):
            w, bw = wcv.next()
            P.pool.dma(out=w[:, :, 0:128], in_=w_in_v[:, :, cc * 128:(cc + 1) * 128], writes=[bw])
            P.pool.dma(out=w[:, :, 128:256], in_=w_in_v[:, :, 512 + cc * 128:512 + (cc + 1) * 128], writes=[bw])
            for (t0, n) in ntiles:
                pm, bpm = pmr.next()
                pg, bpg = pgr.next()
                for k in range(8):
                    P.pe.op(mm_fn(pm[:, 0:n], w[:, k, 0:128], hT[:, k, t0:t0 + n], k == 0, k == 7),
                            [bw] + hT_bufs(t0, n), [bpm])
                for k in range(8):
                    P.pe.op(mm_fn(pg[:, 0:n], w[:, k, 128:256], hT[:, k, t0:t0 + n], k == 0, k == 7),
                            [bw] + hT_bufs(t0, n), [bpg])
                sg, bsg = sgr.next()
                P.act.op(act_fn(sg[:, 0:n], pg[:, 0:n], AF.Sigmoid), [bpg], [bsg])
                P.dve.op(tt_fn(hglu[:, cc, 15 + t0:15 + t0 + n], pm[:, 0:n], sg[:, 0:n], ALU.mult),
                         [bpm, bsg], [bhg[cc]])
        for nt in range(4):
            t0 = nt * 512
            vb = []
            for cc in range(4):
                pm, bpm = pmr.next()
                for k in range(31):
                    P.pe.op(mm_fn(pm[:], diag[:, cc, k, :], hglu[:, cc, t0 + k:t0 + k + 512], k == 0, k == 30),
                            [bdiag[cc], bhg[cc]], [bpm])
                v, bv = vbuf[cc].next()
                sq, bsq = sqr[cc].next()
                P.act.op(act_fn(v[:], pm[:], AF.Identity, bias=cv[:, cc, 31:32], scale=1.0), [bpm, bcv], [bv])
                P.act.op(act_fn(sq[:], pm[:], AF.Square, bias=cv[:, cc, 31:32], scale=1.0), [bpm, bcv], [bsq])
                vb.append((v, bv, sq, bsq))
            pmean, bpmean = pst.next()
            pex2, bpex2 = pst.next()
            for cc in range(4):
                P.pe.op(mm_fn(pmean[:], P.ones512, vb[cc][0][:], cc == 0, cc == 3), [P.bcst, vb[cc][1]], [bpmean])
            for cc in range(4):
                P.pe.op(mm_fn(pex2[:], P.ones512, vb[cc][2][:], cc == 0, cc == 3), [P.bcst, vb[cc][3]], [bpex2])
            mean, bmean = tmp.next()
            P.act.op(act_fn(mean[:], pmean[:], AF.Copy), [bpmean], [bmean])
            m2, bm2 = tmp.next()
            P.dve.op(tt_fn(m2[:], mean[:], mean[:], ALU.mult), [bmean], [bm2])
            var, bvar = tmp.next()
            P.dve.op(tt_fn(var[:], pex2[:], m2[:], ALU.subtract), [bpex2, bm2], [bvar])
            sd, bsd = tmp.next()
            P.act.op(act_fn(sd[:], var[:], AF.Sqrt, bias=P.eps_t[:, 0:1], scale=1.0), [bvar, P.beps], [bsd])
            rstd, brstd = tmp.next()
            P.dve.op(lambda e: e.reciprocal(out=rstd[:], in_=sd[:]), [bsd], [brstd])
            for cc in range(4):
                v, bv, sq, bsq = vb[cc]
                z, bz = zr.next()
                P.dve.op(tt_fn(z[:], v[:], mean[:], ALU.subtract), [bv, bmean], [bz])
                P.dve.op(tt_fn(z[:], z[:], rstd[:], ALU.mult), [bz, brstd], [bz])
                P.act.op(act_fn(mixT[:, cc, t0:t0 + 512], z[:], AF.Silu, scale=cv[:, cc, 32:33],
                                bias=cv[:, cc, 33:34]), [bz, bcv], [bmix[cc]])
        P.barrier()

    tiles = vtile_list()
    NT = len(tiles)
    tindex = {(ci, c, j): i for i, (ci, d, c, j, s) in enumerate(tiles)}
    with contextlib.ExitStack() as st:
        wq = Ring(P, 2, [128, 8, 384], BF16, st)
        ebr = Ring(P, 1, [128, 6, 384], F32, st)
        qT = P.sb([128, OWN], BF16, stack=st)
        bqT = Buf()
        kT = P.sb([128, WIN], BF16, stack=st)
        bkT = Buf()
        vT = P.sb([128, WIN], BF16, stack=st)
        bvT = Buf()
        Vt = P.sb([128, NT, 256], BF16, stack=st)
        bVt = [Buf() for _ in range(NT)]
        bones = Buf()
        acc = P.sb([128, 2, OWN], F32, stack=st)
        bacc = [Buf(), Buf()]
        pmr = Ring(P, 2, [128, 512], F32, st, psum=True)
        pst = Ring(P, 1, [128, 512], F32, st, psum=True)
        psr = Ring(P, 2, [128, 512], F32, st, psum=True)
        ptvr = Ring(P, 1, [128, 1024], BF16, st, psum=True)
        por = Ring(P, 2, [128, 512], F32, st, psum=True)
        qfr = Ring(P, 2, [128, 512], F32, st)
        sqr = Ring(P, 2, [128, 512], F32, st)
        tmp = Ring(P, 4, [128, 512], F32, st)
        pex = Ring(P, 3, [128, 256], F32, st)
        pTr = Ring(P, 4, [128, 256], BF16, st)
        rzr = Ring(P, 1, [64, OWN], F32, st)
        Vt4 = Vt[:].rearrange("p t (s c) -> p t s c", s=4)
        P.pool.op(lambda e: e.memset(Vt4[:, :, 1, :], 1.0), [], [bones])
        P.pool.op(lambda e: e.memset(Vt4[:, :, 3, :], 1.0), [], [bones])
        ps_slot = [0]
        po_slot = [0]
        pv_slot = [0]
        psb = [[Buf(), Buf()], [Buf(), Buf()]]
        pob = [[Buf() for _ in range(4)] for _ in range(2)]
        pvb = [Buf() for _ in range(4)]
        for hp in range(4):
            w, bw = wq.next()
            for i, base in enumerate((1024, 1536, 2048)):
                P.pool.dma(out=w[:, :, i * 128:(i + 1) * 128],
                           in_=w_in_v[:, :, base + hp * 128:base + (hp + 1) * 128], writes=[bw])
            ebt, bebt = ebr.next()
            P.sp.dma(out=ebt[:], in_=eb[hp], writes=[bebt])
            for which, ntok, dst, bdst, gcol in ((0, OWN, qT, bqT, 0), (1, WIN, kT, bkT, 1)):
                for nt in range(ntok // 512):
                    t0 = nt * 512
                    pm, bpm = pmr.next()
                    for k in range(8):
                        P.pe.op(mm_fn(pm[:], w[:, k, which * 128:(which + 1) * 128], hT[:, k, t0:t0 + 512],
                                      k == 0, k == 7), [bw] + hT_bufs(t0, 512), [bpm])
                    qf, bqf = qfr.next()
                    sq, bsq = sqr.next()
                    P.act.op(act_fn(qf[:], pm[:], AF.Copy), [bpm], [bqf])
                    P.act.op(act_fn(sq[:], pm[:], AF.Square), [bpm], [bsq])
                    pms, bpms = pst.next()
                    P.pe.op(mm_fn(pms[:], P.ones64, sq[:], True, True), [P.bcst, bsq], [bpms])
                    sd, bsd = tmp.next()
                    P.act.op(act_fn(sd[:], pms[:], AF.Sqrt, bias=P.eps_t[:, 0:1], scale=1.0), [bpms, P.beps], [bsd])
                    rstd, brstd = tmp.next()
                    P.dve.op(lambda e, rstd=rstd, sd=sd: e.reciprocal(out=rstd[:], in_=sd[:]), [bsd], [brstd])
                    P.dve.op(tt_fn(qf[:], qf[:], rstd[:], ALU.mult), [bqf, brstd], [bqf])
                    P.dve.op(ts_fn(dst[:, t0:t0 + 512], qf[:], qkg[:, gcol:gcol + 1],
                                   0.125 if which == 0 else 1.0, ALU.mult, ALU.mult), [bqf, bqkg], [bdst])
            for nt in range(WIN // 512):
                t0 = nt * 512
                pm, bpm = pmr.next()
                for k in range(8):
                    P.pe.op(mm_fn(pm[:], w[:, k, 256:384], hT[:, k, t0:t0 + 512], k == 0, k == 7),
                            [bw] + hT_bufs(t0, 512), [bpm])
                P.act.op(act_fn(vT[:, t0:t0 + 512], pm[:], AF.Copy), [bpm], [bvT])
            for ti, (ci, d, c, j, s0) in enumerate(tiles):
                pv, bpv = ptvr.next()
                P.pe.op(lambda e, pv=pv, s0=s0, d=d: e.transpose(pv[:, 0:128], vT[:, s0:s0 + 127 * d + 1:d], P.identb[:]),
                        [bvT, P.bidb], [bpv])
                P.act.op(act_fn(Vt4[:, ti, 0:4:2, :], pv[:, 0:128].rearrange("p (s c) -> p s c", s=2), AF.Copy),
                         [bpv, bones], [bVt[ti]])
            for hh in range(2):
                r0 = 64 * hh
                for ci, d in enumerate(ATT_CFG):
                    nb = OWN // (128 * d)
                    for c in range(d):
                        pts = []
                        for j in range(nb + 1):
                            i0 = 0 if j == 0 else 128 * j - 64
                            ks = c + d * i0
                            jlo, jhi = max(j - 1, 0), min(j, nb - 1)
                            nq = 128 * (jhi - jlo + 1)
                            q0 = c + d * 128 * jlo
                            pst_, pbuf = psr.next()
                            sl = pst_[:, 0:nq]
                            P.pe.op(mm_fn(sl, kT[r0:r0 + 64, ks:ks + 127 * d + 1:d],
                                          qT[r0:r0 + 64, q0:q0 + (nq - 1) * d + 1:d], True, True), [bkT, bqT], [pbuf])
                            pe_, bpe = pex.next()
                            P.act.op(act_fn(pe_[:, 0:nq], sl, AF.Exp), [pbuf], [bpe])
                            if j == 0:
                                ebs = ebt[:, ci * 2 + hh, 256:384]
                            elif j == nb:
                                ebs = ebt[:, ci * 2 + hh, 0:128]
                            else:
                                ebs = ebt[:, ci * 2 + hh, 0:256]
                            pT, bpT = pTr.next()
                            P.dve.op(tt_fn(pT[:, 0:nq], pe_[:, 0:nq], ebs, ALU.mult), [bpe, bebt], [bpT])
                            pts.append((pT, bpT, nq))
                            if j >= 1:
                                jb = j - 1
                                pTa, bpTa, nqa = pts[jb]
                                a_off = 0 if jb == 0 else 128
                                po_, pobuf = por.next()
                                osl = po_[:, 0:128]
                                ta = tindex[(ci, c, jb)]
                                tb = tindex[(ci, c, j)]
                                P.pe.op(mm_fn(osl, Vt[:, ta, 128 * hh:128 * hh + 128], pTa[:, a_off:a_off + 128],
                                              True, False), [bVt[ta], bpTa], [pobuf])
                                P.pe.op(mm_fn(osl, Vt[:, tb, 128 * hh:128 * hh + 128], pT[:, 0:128],
                                              False, True), [bVt[tb], bpT], [pobuf])
                                a0 = c + d * 128 * jb
                                dsl = acc[:, hh, a0:a0 + 127 * d + 1:d]
                                if ci == 0:
                                    P.dve.op(lambda e, dsl=dsl, osl=osl: e.tensor_copy(out=dsl, in_=osl),
                                             [pobuf], [bacc[hh]])
                                else:
                                    P.dve.op(tt_fn(dsl, dsl, osl, ALU.add), [pobuf, bacc[hh]], [bacc[hh]])
                rz, brz = rzr.next()
                P.dve.op(lambda e, rz=rz, hh=hh: e.reciprocal(out=rz[:], in_=acc[64:128, hh, :]), [bacc[hh]], [brz])
                P.dve.op(tt_fn(mixT[r0:r0 + 64, 4 + hp, :], acc[0:64, hh, :], rz[:], ALU.mult),
                         [bacc[hh], brz], [bmix[4 + hp]])
        P.barrier()

    with contextlib.ExitStack() as st:
        wo = P.sb([128, 8, D], BF16, stack=st)
        bwo = Buf()
        for k in range(8):
            P.pool.dma(out=wo[:, k, :], in_=w_out_v[:, k, :], writes=[bwo])
        gfb = P.sb([128, D], F32, stack=st)
        bgfb = Buf()
        P.sp.dma(out=gfb[:], in_=bcast_rows(gf, 128), writes=[bgfb])
        wr = P.sb([128, 8, 16], F32, stack=st)
        bwr = Buf()
        P.sp.dma(out=wr[:], in_=w_r_v, writes=[bwr])
        brb = P.sb([128, 16], F32, stack=st)
        bbrb = Buf()
        P.sp.dma(out=brb[:], in_=bcast_rows(b_r, 128), writes=[bbrb])
        xr = Ring(P, 2, [128, D], F32, st)
        x1r = Ring(P, 2, [128, D], F32, st)
        pmr = Ring(P, 2, [128, 512], F32, st, psum=True)
        rings = {
            "junk": Ring(P, 1, [128, D], F32, st),
            "small": Ring(P, 12, [128, 1], F32, st),
            "hf": Ring(P, 2, [128, D], F32, st),
            "ptf": Ring(P, 1, [128, 8, 128], F32, st, psum=True),
            "hfT": Ring(P, 2, [128, 8, 128], F32, st),
            "plog": Ring(P, 2, [128, 512], F32, st, psum=True),
            "sm16": Ring(P, 6, [128, 16], F32, st),
            "pat": Ring(P, 1, [128, 512], F32, st, psum=True),
            "aT": Ring(P, 2, [16, 128], F32, st),
        }
        for tt in range(OWN // 128):
            xt, bx = xr.next()
            P.sp.dma(out=xt[:], in_=xw[tt * 128:(tt + 1) * 128, :], writes=[bx])
            x1t, bx1 = x1r.next()
            for half in range(2):
                pm, bpm = pmr.next()
                for k in range(8):
                    P.pe.op(mm_fn(pm[:], mixT[:, k, tt * 128:(tt + 1) * 128], wo[:, k, half * 512:(half + 1) * 512],
                                  k == 0, k == 7), [bmix[k], bwo], [bpm])
                P.dve.op(tt_fn(x1t[:, half * 512:(half + 1) * 512], pm[:], xt[:, half * 512:(half + 1) * 512],
                               ALU.add), [bpm, bx], [bx1])
            P.sp.dma(out=x1_out[tt * 128:(tt + 1) * 128, :], in_=x1t[:], reads=[bx1])
            router_tile(P, x1t, bx1, gfb, bgfb, wr, bwr, brb, bbrb, rings, aff_out, affT_out, tt)
    return


def host_consts():
    cst = np.zeros((128, 6, 128), np.float32)
    cst[:, 0, :] = np.eye(128, dtype=np.float32)
    cst[:, 1, :] = 1.0 / 512.0
    blk = np.zeros((128, 128), np.float32)
    blk[:64, :64] = 1.0 / 64.0
    blk[64:, 64:] = 1.0 / 64.0
    cst[:, 2, :] = blk
    cst[:, 3, :] = 1.0
    cst[:, 4, :] = np.triu(np.ones((128, 128), np.float32), 1)
    cst[:, 5, 0:16] = 512.0 * np.arange(16, dtype=np.float32)[None, :]
    return cst


def host_eb():
    p = np.arange(128)[:, None].astype(np.float64)
    f = np.arange(128)[None, :].astype(np.float64)
    eb = np.zeros((4, 128, 6, 384), np.float32)
    for head in range(8):
        slope = 2.0 ** (-(head + 1))
        hp, hh = head // 2, head % 2
        for ci, d in enumerate(ATT_CFG):
            B = np.where(p <= f, np.exp(-slope * d * np.abs(p + 64 - f)), 0.0)
            A = np.where(p >= f, np.exp(-slope * d * np.abs(p - 64 - f)), 0.0)
            A0 = np.where((p <= 63) & (p >= f - 64), np.exp(-slope * d * np.abs(p - f)), 0.0)
            eb[hp, :, ci * 2 + hh, 0:128] = B
            eb[hp, :, ci * 2 + hh, 128:256] = A
            eb[hp, :, ci * 2 + hh, 256:384] = A0
    return eb


def local_view(xb, h, n):
    if h == 0:
        return np.ascontiguousarray(xb[:n])
    return np.ascontiguousarray(xb[::-1][:n])


_CACHE = {}


def get_prog(name, builder):
    if name not in _CACHE:
        _CACHE[name] = builder()
    return _CACHE[name]


def run_phaseA(x, mix_norm_even, w_in, conv_w, conv_b, conv_ln_g, conv_ln_b, q_norm, k_norm, w_out,
               ffn_norm0, w_router0, b_router0):
    nc = get_prog("A", build_phaseA)
    cst = host_consts()
    eb = host_eb()
    in_maps = []
    for c in range(NCORES):
        b, h = c // 2, c % 2
        cw = conv_w[0] if h == 0 else conv_w[0][::-1]
        cvec = np.zeros((128, 4, 34), np.float32)
        cvec[:, :, 0:31] = cw.T.reshape(4, 128, 31).transpose(1, 0, 2)
        cvec[:, :, 31] = conv_b[0].reshape(4, 128).T
        cvec[:, :, 32] = conv_ln_g[0].reshape(4, 128).T
        cvec[:, :, 33] = conv_ln_b[0].reshape(4, 128).T
        qk = np.stack([np.tile(q_norm[0], 2), np.tile(k_norm[0], 2)], axis=1).astype(np.float32)
        in_maps.append({
            "xw": local_view(x[b], h, WIN),
            "g0": np.ascontiguousarray(mix_norm_even[0][None, :]),
            "w_in": np.ascontiguousarray(w_in[0]),
            "cvec": cvec,
            "qk_g": np.ascontiguousarray(qk),
            "w_out": np.ascontiguousarray(w_out[0]),
            "gf": np.ascontiguousarray(ffn_norm0[None, :]),
            "w_r": np.ascontiguousarray(w_router0),
            "b_r": np.ascontiguousarray(b_router0[None, :]),
            "eb": eb,
            "cst": cst,
        })
    res = run_bass_kernel_spmd(nc, in_maps, core_ids=list(range(NCORES)))
    return res.results


NEXP = 16
CAP = 512


def emit_phaseB(P, io):
    with contextlib.ExitStack() as ph:
        P.cur = ph
        _phaseB_body(P, io["x1"], io["affT_seq"], io["aff_own"], io["gf"], io["wg"], io["wu"], io["wd"],
                     io["x2_out"], io.get("gn"), io.get("hn_out"))
        P.barrier()
    P.cur = None


def _phaseB_body(P, x1, affT_seq, aff_own, gf, wg, wu, wd, x2_out, gn, hn_out):
    if gn is not None:
        gnb = P.sb([128, D], F32, name="gnb")
        bgnb = Buf()
        P.sp.dma(out=gnb[:], in_=bcast_rows(gn, 128), writes=[bgnb])
    gfb = P.sb([128, D], F32, name="gfb")
    bgfb = Buf()
    P.sp.dma(out=gfb[:], in_=bcast_rows(gf, 128), writes=[bgfb])
    gate = P.sb([128, OWN // 128, NEXP], F32, name="gate")
    bgate = Buf()
    pthr = P.ps([128, 512], F32, name="pthr")
    bpthr = Buf()

    with contextlib.ExitStack() as st:
        aT = P.sb([NEXP, SEQ], F32, stack=st)
        baT = Buf()
        P.sp.dma(out=aT[:].rearrange("e (r t) -> e r t", r=2), in_=affT_seq, writes=[baT])
        junk = P.sb([NEXP, SEQ], F32, stack=st)
        bjunk = Buf()
        lo = P.sb([NEXP, 1], F32, stack=st)
        blo = Buf()
        mid = P.sb([NEXP, 1], F32, stack=st)
        bmid = Buf()
        cnt = P.sb([NEXP, 1], F32, stack=st)
        bcnt = Buf()
        ge = P.sb([NEXP, 1], F32, stack=st)
        bge = Buf()
        P.dve.op(lambda e: e.memset(lo[:], 0.0), [], [blo])
        for it in range(36):
            wk = 2.0 ** (-(it + 1))
            P.dve.op(ts_fn(mid[:], lo[:], wk, None, ALU.add), [blo], [bmid])
            P.dve.op(lambda e: e.tensor_scalar(out=junk[:], in0=aT[:], scalar1=mid[:, 0:1], scalar2=0.0,
                                               op0=ALU.is_ge, op1=ALU.add, accum_out=cnt[:, 0:1]),
                     [baT, bmid], [bjunk, bcnt])
            P.dve.op(ts_fn(ge[:], cnt[:], CAP - 0.5, None, ALU.is_ge), [bcnt], [bge])
            P.dve.op(stt_fn(lo[:], ge[:], wk, lo[:], ALU.mult, ALU.add), [bge, blo], [blo])
        dthr = P.sb([NEXP, NEXP], F32, stack=st)
        bdthr = Buf()
        P.dve.op(ts_fn(dthr[:], P.cst[0:NEXP, 0, 0:NEXP], lo[:, 0:1], None, ALU.mult), [P.bcst, blo], [bdthr])
        P.pe.op(mm_fn(pthr[:, 0:NEXP], P.cst[0:NEXP, 3, :], dthr[:], True, True), [P.bcst, bdthr], [bpthr])
        thrb = P.sb([128, NEXP], F32, stack=st)
        bthrb = Buf()
        P.act.op(act_fn(thrb[:], pthr[:, 0:NEXP], AF.Copy), [bpthr], [bthrb])
        ao = P.sb([128, OWN // 128, NEXP], F32, stack=st)
        bao = Buf()
        P.sp.dma(out=ao[:], in_=aff_own.rearrange("(t p) e -> p t e", p=128), writes=[bao])
        msk = P.sb([128, OWN // 128, NEXP], F32, stack=st)
        bmsk = Buf()
        thr_bc = bass.AP(thrb.tensor if hasattr(thrb, "tensor") else thrb[:].tensor, thrb[:].offset,
                         [list(thrb[:].ap[0]), [0, OWN // 128], [1, NEXP]])
        P.dve.op(tt_fn(msk[:], ao[:], thr_bc, ALU.is_ge), [bao, bthrb], [bmsk])
        P.dve.op(tt_fn(gate[:], msk[:], ao[:], ALU.mult), [bmsk, bao], [bgate])
        P.barrier()

    HT = OWN // 2
    with contextlib.ExitStack() as st:
        acc = P.sb([128, HT // 128, D], F32, stack=st)
        bacc = [Buf() for _ in range(HT // 128)]
        hT = P.sb([128, 8, HT], BF16, stack=st)
        bhT = Buf()
        junk = Ring(P, 1, [128, D], F32, st)
        small = Ring(P, 8, [128, 1], F32, st)
        hnr = Ring(P, 2, [128, D], BF16, st)
        hor = Ring(P, 2, [128, D], F32, st)
        ptr = Ring(P, 1, [128, 8, 128], BF16, st, psum=True)
        wgr = Ring(P, 2, [128, 8, D], BF16, st)
        wur = Ring(P, 2, [128, 8, D], BF16, st)
        wdr = Ring(P, 2, [128, 8, D], BF16, st)
        atr = Ring(P, 2, [128, 8, 512], BF16, st)
        sgr = Ring(P, 2, [128, 512], F32, st)
        pgr = Ring(P, 2, [128, 512], F32, st, psum=True)
        pur = Ring(P, 2, [128, 512], F32, st, psum=True)
        pyr = Ring(P, 2, [128, 512], F32, st, psum=True)
        for half in range(2):
            for t in range(HT // 128):
                tg = half * (HT // 128) + t
                P.sp.dma(out=acc[:, t, :], in_=x1[tg * 128:(tg + 1) * 128, :], writes=[bacc[t]])
                rs, br = rms_rstd(P, acc[:, t, :], bacc[t], D, junk, small)
                hn, bhn = hnr.next()
                P.dve.op(stt_fn(hn[:], acc[:, t, :], rs[:, 0:1], gfb[:], ALU.mult, ALU.mult),
                         [bacc[t], br, bgfb], [bhn])
                pt, bpt = ptr.next()
                for k in range(8):
                    P.pe.op(lambda e, k=k, pt=pt, hn=hn: e.transpose(pt[:, k, :], hn[:, k * 128:(k + 1) * 128],
                                                                      P.identb[:]), [bhn, P.bidb], [bpt])
                P.act.op(act_fn(hT[:, :, t * 128:(t + 1) * 128], pt[:], AF.Copy), [bpt], [bhT])
            for e in range(NEXP):
                wts = []
                for src, ring in ((wg, wgr), (wu, wur), (wd, wdr)):
                    w, bw = ring.next()
                    sv = src[e].rearrange("(k p) f -> p k f", p=128)
                    P.pool.dma(out=w[:, 0:4, :], in_=sv[:, 0:4, :], writes=[bw])
                    P.pool.dma(out=w[:, 4:8, :], in_=sv[:, 4:8, :], writes=[bw])
                    wts.append((w, bw))
                (Wg, bWg), (Wu, bWu), (Wd, bWd) = wts
                for q in range(HT // 512):
                    AT, bAT = atr.next()
                    for fc in range(8):
                        pg, bpg = pgr.next()
                        pu, bpu = pur.next()
                        for k in range(8):
                            P.pe.op(mm_fn(pg[:], Wg[:, k, fc * 128:(fc + 1) * 128], hT[:, k, q * 512:(q + 1) * 512],
                                          k == 0, k == 7), [bWg, bhT], [bpg])
                        for k in range(8):
                            P.pe.op(mm_fn(pu[:], Wu[:, k, fc * 128:(fc + 1) * 128], hT[:, k, q * 512:(q + 1) * 512],
                                          k == 0, k == 7), [bWu, bhT], [bpu])
                        sg, bsg = sgr.next()
                        P.act.op(act_fn(sg[:], pg[:], AF.Silu), [bpg], [bsg])
                        P.dve.op(tt_fn(AT[:, fc, :], sg[:], pu[:], ALU.mult), [bsg, bpu], [bAT])
                    for tt in range(4):
                        t = q * 4 + tt
                        tg = half * (HT // 128) + t
                        for hc in range(2):
                            py, bpy = pyr.next()
                            for fc in range(8):
                                P.pe.op(mm_fn(py[:], AT[:, fc, tt * 128:(tt + 1) * 128],
                                              Wd[:, fc, hc * 512:(hc + 1) * 512], fc == 0, fc == 7), [bAT, bWd], [bpy])
                            asl = acc[:, t, hc * 512:(hc + 1) * 512]
                            P.dve.op(stt_fn(asl, py[:], gate[:, tg, e:e + 1], asl, ALU.mult, ALU.add),
                                     [bpy, bgate, bacc[t]], [bacc[t]])
            for t in range(HT // 128):
                tg = half * (HT // 128) + t
                P.sp.dma(out=x2_out[tg * 128:(tg + 1) * 128, :], in_=acc[:, t, :], reads=[bacc[t]])
                if gn is not None:
                    rs, br = rms_rstd(P, acc[:, t, :], bacc[t], D, junk, small)
                    ho, bho = hor.next()
                    P.dve.op(stt_fn(ho[:], acc[:, t, :], rs[:, 0:1], gnb[:], ALU.mult, ALU.mult),
                             [bacc[t], br, bgnb], [bho])
                    P.sp.dma(out=hn_out[tg * 128:(tg + 1) * 128, :], in_=ho[:], reads=[bho])


def run_phaseB(x1_cores, affT_cores, aff_cores, gf, wg, wu, wd, gn):
    nc = get_prog("B", build_phaseB)
    cst = host_consts()
    in_maps = []
    for c in range(NCORES):
        b = c // 2
        affT_seq = np.ascontiguousarray(np.concatenate([affT_cores[2 * b], affT_cores[2 * b + 1]], axis=1))
        in_maps.append({
            "x1": x1_cores[c], "affT_seq": affT_seq, "aff_own": aff_cores[c],
            "gf": np.ascontiguousarray(gf[None, :]), "wg": wg, "wu": wu, "wd": wd, "cst": cst,
            "gn": np.ascontiguousarray(gn[None, :]),
        })
    res = run_bass_kernel_spmd(nc, in_maps, core_ids=list(range(NCORES)))
    return [r["x2"] for r in res.results], [r["hn"] for r in res.results]


NG = 32
NK = SEQ // 8
NWIN = NK // 8
HALF_PI = 1.5707963267948966


class _Stop(Exception):
    pass


def build_phaseC(stop_after=99):
    P = Prog()
    try:
        _phaseC_body(P, stop_after)
    except _Stop:
        P.barrier()
    return P.finish()


def _phaseC_body(P, stop_after):
    lamr_i = P.inp("lamr", [128, NG])
    lami_i = P.inp("lami", [128, NG])
    ldt_i = P.inp("ldt", [128, NG])
    B_i = P.inp("Bri", [128, 2, NG, 16])
    C_i = P.inp("Cri", [128, 2, NG, 16])
    dcol_i = P.inp("dcol", [128, NG])
    Unat = P.inp("Unat", [NG, 128, NK])
    Uw = P.inp("Uw", [NWIN, 128, NG, 8])
    Uwr = P.inp("Uwr", [NWIN, 128, NG, 8])
    masks_i = P.inp("masks", [128, 2, 128])
    cst_in = P.inp("cst", [128, 6, 128])
    zp = P.outp("zp", [NG, 128, NK])
    load_consts(P, cst_in)
    dve, act, pe, pool, sp = P.dve, P.act, P.pe, P.pool, P.sp

    M = P.sb([128, NG, 128], BF16, name="M")
    bM = Buf()
    Wz = P.sb([128, NG, 4, 128], BF16, name="Wz")
    bWz = Buf()
    Rr = P.sb([128, NG, 128], BF16, name="Rr")
    Ri = P.sb([128, NG, 128], BF16, name="Ri")
    bR = Buf()
    A8 = P.sb([128, 2, 2, NG], F32, name="A8")
    bA8 = Buf()
    dcol = P.sb([128, NG], F32, name="dcol")
    bdcol = Buf()
    sp.dma(out=dcol[:], in_=dcol_i, writes=[bdcol])

    def small(st, name=None):
        return P.sb([128, NG], F32, stack=st), Buf()

    with contextlib.ExitStack() as st:
        lamr, blamr = small(st)
        lami, blami = small(st)
        ldt, bldt = small(st)
        sp.dma(out=lamr[:], in_=lamr_i, writes=[blamr])
        sp.dma(out=lami[:], in_=lami_i, writes=[blami])
        sp.dma(out=ldt[:], in_=ldt_i, writes=[bldt])
        Bt = P.sb([128, 2, NG, 16], F32, stack=st)
        bBt = Buf()
        Ct = P.sb([128, 2, NG, 16], F32, stack=st)
        bCt = Buf()
        sp.dma(out=Bt[:], in_=B_i, writes=[bBt])
        sp.dma(out=Ct[:], in_=C_i, writes=[bCt])
        mk = P.sb([128, 2, 128], F32, stack=st)
        bmk = Buf()
        sp.dma(out=mk[:], in_=masks_i, writes=[bmk])
        hpi = P.sb([128, 1], F32, stack=st)
        bhpi = Buf()
        dve.op(lambda e: e.memset(hpi[:], HALF_PI), [], [bhpi])

        def T2(a, ba, b, bb, op):
            o, bo = small(st)
            dve.op(tt_fn(o[:], a[:], b[:], op), [ba, bb], [bo])
            return o, bo

        dt, bdt = small(st)
        act.op(act_fn(dt[:], ldt[:], AF.Exp), [bldt], [bdt])
        a_, ba_ = T2(lamr, blamr, dt, bdt, ALU.mult)
        ang, bang = T2(lami, blami, dt, bdt, ALU.mult)
        mag, bmag = small(st)
        act.op(act_fn(mag[:], a_[:], AF.Exp, scale=1.0 / 16), [ba_], [bmag])
        s16, bs16 = small(st)
        act.op(act_fn(s16[:], ang[:], AF.Sin, scale=1.0 / 16), [bang], [bs16])
        c16, bc16 = small(st)
        act.op(act_fn(c16[:], ang[:], AF.Sin, scale=1.0 / 16, bias=hpi[:, 0:1]), [bang, bhpi], [bc16])
        re, bre = T2(mag, bmag, c16, bc16, ALU.mult)
        im, bim = T2(mag, bmag, s16, bs16, ALU.mult)
        for _ in range(4):
            r2, br2 = T2(re, bre, re, bre, ALU.mult)
            i2, bi2 = T2(im, bim, im, bim, ALU.mult)
            nre, bnre = T2(r2, br2, i2, bi2, ALU.subtract)
            nim, bnim = small(st)
            dve.op(stt_fn(nim[:], re[:], 2.0, im[:], ALU.mult, ALU.mult), [bre, bim], [bnim])
            re, bre, im, bim = nre, bnre, nim, bnim
        pw = P.sb([128, 9, 2, NG], F32, stack=st)
        bpw = Buf()
        ipw = P.sb([128, 9, 2, NG], F32, stack=st)
        bipw = Buf()
        dve.op(lambda e: e.memset(pw[:, 0, 0, :], 1.0), [], [bpw])
        dve.op(lambda e: e.memset(pw[:, 0, 1, :], 0.0), [], [bpw])
        dve.op(lambda e: e.memset(ipw[:, 0, 0, :], 1.0), [], [bipw])
        dve.op(lambda e: e.memset(ipw[:, 0, 1, :], 0.0), [], [bipw])
        dve.op(lambda e: e.tensor_copy(out=pw[:, 1, 0, :], in_=re[:]), [bre], [bpw])
        dve.op(lambda e: e.tensor_copy(out=pw[:, 1, 1, :], in_=im[:]), [bim], [bpw])
        t1, bt1 = small(st)
        t2, bt2 = small(st)
        for n in range(1, 8):
            dve.op(tt_fn(t1[:], pw[:, n, 0, :], re[:], ALU.mult), [bpw, bre], [bt1])
            dve.op(tt_fn(t2[:], pw[:, n, 1, :], im[:], ALU.mult), [bpw, bim], [bt2])
            dve.op(tt_fn(pw[:, n + 1, 0, :], t1[:], t2[:], ALU.subtract), [bt1, bt2], [bpw])
            dve.op(tt_fn(t1[:], pw[:, n, 0, :], im[:], ALU.mult), [bpw, bim], [bt1])
            dve.op(tt_fn(t2[:], pw[:, n, 1, :], re[:], ALU.mult), [bpw, bre], [bt2])
            dve.op(tt_fn(pw[:, n + 1, 1, :], t1[:], t2[:], ALU.add), [bt1, bt2], [bpw])
        en, ben = small(st)
        for n in range(1, 9):
            act.op(act_fn(en[:], a_[:], AF.Exp, scale=-2.0 * n), [ba_], [ben])
            dve.op(tt_fn(ipw[:, n, 0, :], pw[:, n, 0, :], en[:], ALU.mult), [bpw, ben], [bipw])
            dve.op(stt_fn(ipw[:, n, 1, :], pw[:, n, 1, :], -1.0, en[:], ALU.mult, ALU.mult), [bpw, ben], [bipw])
        dve.op(lambda e: e.tensor_copy(out=A8[:, 0, 0, :], in_=pw[:, 8, 0, :]), [bpw], [bA8])
        dve.op(lambda e: e.tensor_copy(out=A8[:, 0, 1, :], in_=pw[:, 8, 0, :]), [bpw], [bA8])
        dve.op(ts_fn(A8[:, 1, 0, :], pw[:, 8, 1, :], -1.0, None, ALU.mult), [bpw], [bA8])
        dve.op(lambda e: e.tensor_copy(out=A8[:, 1, 1, :], in_=pw[:, 8, 1, :]), [bpw], [bA8])
        lr2, blr2 = T2(lamr, blamr, lamr, blamr, ALU.mult)
        li2, bli2 = T2(lami, blami, lami, blami, ALU.mult)
        den, bden = T2(lr2, blr2, li2, bli2, ALU.add)
        rden, brden = small(st)
        dve.op(lambda e: e.reciprocal(out=rden[:], in_=den[:]), [bden], [brden])
        nr, bnr = small(st)
        dve.op(ts_fn(nr[:], re[:], -1.0, None, ALU.add), [bre], [bnr])
        u1, bu1 = T2(nr, bnr, lamr, blamr, ALU.mult)
        u2, bu2 = T2(im, bim, lami, blami, ALU.mult)
        u3, bu3 = T2(u1, bu1, u2, bu2, ALU.add)
        fre, bfre = T2(u3, bu3, rden, brden, ALU.mult)
        u4, bu4 = T2(im, bim, lamr, blamr, ALU.mult)
        u5, bu5 = T2(nr, bnr, lami, blami, ALU.mult)
        u6, bu6 = T2(u4, bu4, u5, bu5, ALU.subtract)
        fim, bfim = T2(u6, bu6, rden, brden, ALU.mult)

        def bc16_(t):
            a = t[:]
            return bass.AP(a.tensor, a.offset, [list(a.ap[0]), [1, NG], [0, 16]])

        big = lambda: (P.sb([128, NG, 16], F32, stack=st), Buf())
        bbr, bbbr = big()
        bbi, bbbi = big()
        g1, bg1 = big()
        g2, bg2 = big()
        dve.op(tt_fn(g1[:], Bt[:, 0, :, :], bc16_(fre), ALU.mult), [bBt, bfre], [bg1])
        dve.op(tt_fn(g2[:], Bt[:, 1, :, :], bc16_(fim), ALU.mult), [bBt, bfim], [bg2])
        dve.op(tt_fn(bbr[:], g1[:], g2[:], ALU.subtract), [bg1, bg2], [bbbr])
        dve.op(tt_fn(g1[:], Bt[:, 1, :, :], bc16_(fre), ALU.mult), [bBt, bfre], [bg1])
        dve.op(tt_fn(g2[:], Bt[:, 0, :, :], bc16_(fim), ALU.mult), [bBt, bfim], [bg2])
        dve.op(tt_fn(bbi[:], g1[:], g2[:], ALU.add), [bg1, bg2], [bbbi])

        def table(top, bot):
            t = P.sb([128, 8, 2, NG], F32, stack=st)
            bt = Buf()
            for i in range(8):
                (ta, tn), (ba, bn) = top[i], bot[i]
                pool.op(lambda e, i=i, ta=ta, tn=tn: e.tensor_copy(out=t[0:64, i, :, :], in_=ta[0:64, tn, :, :]),
                        [bpw, bipw], [bt])
                pool.op(lambda e, i=i, ba=ba, bn=bn: e.tensor_copy(out=t[64:128, i, :, :], in_=ba[64:128, bn, :, :]),
                        [bpw, bipw], [bt])
            return t, bt

        QX, bQX = table([(ipw, s) for s in range(8)], [(pw, s) for s in range(8)])
        PY, bPY = table([(pw, t) for t in range(8)], [(ipw, t) for t in range(8)])
        QW, bQW = table([(pw, 7 - s) for s in range(8)], [(pw, s) for s in range(8)])
        PR, bPR = table([(pw, j + 1) for j in range(8)], [(pw, 8 - j) for j in range(8)])

        def tb(t, i, ri):
            a = t[:, i, ri, :]
            return bass.AP(a.tensor, a.offset, [list(a.ap[0]), [1, NG], [0, 16]])

        def cprod(dst_re, dst_im, bdst, xr, xi, bx, tab, btab, neg_im=False, eng=None):
            eng = eng or dve
            bx = list(bx) if isinstance(bx, (list, tuple)) else [bx]
            for i in range(8):
                eng.op(tt_fn(g1[:], xr[:], tb(tab, i, 0), ALU.mult), bx + [btab], [bg1])
                eng.op(tt_fn(g2[:], xi[:], tb(tab, i, 1), ALU.mult), bx + [btab], [bg2])
                eng.op(tt_fn(dst_re[:, :, i, :], g1[:], g2[:], ALU.subtract), [bg1, bg2], [bdst])
                eng.op(tt_fn(g1[:], xr[:], tb(tab, i, 1), ALU.mult), bx + [btab], [bg1])
                eng.op(tt_fn(g2[:], xi[:], tb(tab, i, 0), ALU.mult), bx + [btab], [bg2])
                if neg_im:
                    eng.op(stt_fn(dst_im[:, :, i, :], g1[:], -1.0, g2[:], ALU.mult, ALU.subtract), [bg1, bg2], [bdst])
                else:
                    eng.op(tt_fn(dst_im[:, :, i, :], g1[:], g2[:], ALU.add), [bg1, bg2], [bdst])

        bbx = Buf()
        with contextlib.ExitStack() as st2:
            Xr = P.sb([128, NG, 8, 16], F32, stack=st2)
            Xi = P.sb([128, NG, 8, 16], F32, stack=st2)
            Yr = P.sb([128, NG, 8, 16], F32, stack=st2)
            Yn = P.sb([128, NG, 8, 16], F32, stack=st2)
            bX, bY = Buf(), Buf()
            bBB = Buf()
            cprod(Xr, Xi, bX, bbr, bbi, [bbbr, bbbi], QX, bQX)
            cprod(Yr, Yn, bY, Ct[:, 0, :, :], Ct[:, 1, :, :], bCt, PY, bPY, neg_im=True)
            psF = Ring(P, 2, [128, 512], F32, st2, psum=True)
            psB = Ring(P, 2, [128, 512], F32, st2, psum=True)
            tmr = Ring(P, 2, [128, 128], F32, st2)
            Xr3 = Xr[:].rearrange("p g s c -> p g (s c)")
            Xi3 = Xi[:].rearrange("p g s c -> p g (s c)")
            Yr3 = Yr[:].rearrange("p g s c -> p g (s c)")
            Yn3 = Yn[:].rearrange("p g s c -> p g (s c)")
            for g in range(NG):
                pf, bpf = psF.next()
                pb_, bpb = psB.next()
                pe.op(mm_fn(pf[:, 0:128], Xr3[0:64, g, :], Yr3[0:64, g, :], True, False), [bX, bY], [bpf])
                pe.op(mm_fn(pf[:, 0:128], Xi3[0:64, g, :], Yn3[0:64, g, :], False, True), [bX, bY], [bpf])
                pe.op(mm_fn(pb_[:, 0:128], Xr3[64:128, g, :], Yr3[64:128, g, :], True, False), [bX, bY], [bpb])
                pe.op(mm_fn(pb_[:, 0:128], Xi3[64:128, g, :], Yn3[64:128, g, :], False, True), [bX, bY], [bpb])
                tm, btm = tmr.next()
                dve.op(tt_fn(tm[:], pf[:, 0:128], mk[:, 0, :], ALU.mult), [bpf, bmk], [btm])
                tm2, btm2 = tmr.next()
                dve.op(tt_fn(tm2[:], pb_[:, 0:128], mk[:, 1, :], ALU.mult), [bpb, bmk], [btm2])
                dve.op(tt_fn(M[:, g, :], tm[:], tm2[:], ALU.add), [btm, btm2], [bM])
            P.barrier()
        if stop_after <= 1:
            P.stopped = True
        with contextlib.ExitStack() as st2:
            Wr = P.sb([128, NG, 8, 16], F32, stack=st2)
            Wi = P.sb([128, NG, 8, 16], F32, stack=st2)
            bW = Buf()
            cprod(Wr, Wi, bW, bbr, bbi, [bbbr, bbbi], QW, bQW)
            pool.op(lambda e: e.memset(Wz[:], 0.0), [], [bWz])
            ptw = Ring(P, 2, [128, 512], F32, st2, psum=True)
            W3 = (Wr[:].rearrange("p g s c -> p g (s c)"), Wi[:].rearrange("p g s c -> p g (s c)"))
            for g in range(NG):
                for ri in range(2):
                    pt, bpt = ptw.next()
                    pe.op(lambda e, pt=pt, g=g, ri=ri: e.transpose(pt[:, 0:128], W3[ri][:, g, :], P.identf),
                          [bW, P.bcst], [bpt])
                    act.op(act_fn(Wz[:, g, 2 * ri, 0:64], pt[:, 0:64], AF.Copy), [bpt], [bWz])
                    act.op(act_fn(Wz[:, g, 2 * ri + 1, 64:128], pt[:, 64:128], AF.Copy), [bpt], [bWz])
            P.barrier()
        if stop_after <= 2:
            P.stopped = True
        Rr4 = Rr[:].rearrange("p g (j c) -> p g j c", j=8)
        Ri4 = Ri[:].rearrange("p g (j c) -> p g j c", j=8)
        cprod(Rr4, Ri4, bR, Ct[:, 0, :, :], Ct[:, 1, :, :], bCt, PR, bPR, neg_im=True)
        P.barrier()

    with contextlib.ExitStack() as st:
        hist = P.sb([128, 2, NG, NK], BF16, stack=st)
        bhist = Buf()
        uwr = Ring(P, 3, [128, NG, 8], BF16, st)
        uwrr = Ring(P, 3, [128, NG, 8], BF16, st)
        pvr = Ring(P, 3, [128, 2, NG, 8], F32, st, psum=True)
        S4r = Ring(P, 4, [128, 3, NG], F32, st)
        p1 = P.sb([128, 2, NG], F32, stack=st)
        p2 = P.sb([128, 2, NG], F32, stack=st)
        bp1, bp2 = Buf(), Buf()
        S4, bS = S4r.next()
        dve.op(lambda e: e.memset(S4[:], 0.0), [], [bS])
        for w in range(NWIN):
            uw, buw = uwr.next()
            uwb, buwb = uwrr.next()
            pool.dma(out=uw[:], in_=Uw[w], writes=[buw])
            pool.dma(out=uwb[:], in_=Uwr[w], writes=[buwb])
            pv, bpv = pvr.next()
            for g in range(NG):
                for ri in range(2):
                    pe.op(mm_fn(pv[:, ri, g, :], Wz[:, g, 2 * ri, :], uw[:, g, :], True, False), [bWz, buw], [bpv])
                    pe.op(mm_fn(pv[:, ri, g, :], Wz[:, g, 2 * ri + 1, :], uwb[:, g, :], False, True), [bWz, buwb], [bpv])
            for jj in range(8):
                j = w * 8 + jj
                act.op(act_fn(hist[0:64, :, :, j], S4[0:64, 0:2, :], AF.Copy), [bS], [bhist])
                act.op(act_fn(hist[64:128, :, :, NK - 1 - j], S4[64:128, 0:2, :], AF.Copy), [bS], [bhist])
                Sn, bSn = S4r.next()
                dve.op(tt_fn(p1[:], A8[:, 0, :, :], S4[:, 0:2, :], ALU.mult), [bA8, bS], [bp1])
                dve.op(tt_fn(p2[:], A8[:, 1, :, :], S4[:, 1:3, :], ALU.mult), [bA8, bS], [bp2])
                dve.op(tt_fn(p1[:], p1[:], p2[:], ALU.add), [bp1, bp2], [bp1])
                dve.op(tt_fn(Sn[:, 0:2, :], p1[:], pv[:, :, :, jj], ALU.add), [bp1, bpv], [bSn])
                dve.op(lambda e, Sn=Sn: e.tensor_copy(out=Sn[:, 2, :], in_=Sn[:, 0, :]), [bSn], [bSn])
                S4, bS = Sn, bSn
        P.barrier()
        if stop_after <= 4:
            P.stopped = True
        ubr = Ring(P, 2, [128, NK], BF16, st)
        ufr = Ring(P, 2, [128, NK], F32, st)
        pyr = Ring(P, 2, [128, 512], F32, st, psum=True)
        yr = Ring(P, 2, [128, NK], F32, st)
        tr = Ring(P, 4, [128, NK], F32, st)
        for g in range(NG):
            ub, bub = ubr.next()
            uf, buf_ = ufr.next()
            pool.dma(out=ub[:], in_=Unat[g], writes=[bub])
            sp.dma(out=uf[:], in_=Unat[g], writes=[buf_])
            py, bpy = pyr.next()
            pe.op(mm_fn(py[:], M[:, g, :], ub[:], True, False), [bM, bub], [bpy])
            pe.op(mm_fn(py[:], Rr[:, g, :], hist[:, 0, g, :], False, False), [bR, bhist], [bpy])
            pe.op(mm_fn(py[:], Ri[:, g, :], hist[:, 1, g, :], False, True), [bR, bhist], [bpy])
            y, by = yr.next()
            dve.op(stt_fn(y[:], uf[:], dcol[:, g:g + 1], py[:], ALU.mult, ALU.add), [buf_, bdcol, bpy], [by])
            a1, ba1 = tr.next()
            dve.op(tt_fn(a1[:], y[:], y[:], ALU.mult), [by], [ba1])
            dve.op(ts_fn(a1[:], a1[:], 0.044715, 1.0, ALU.mult, ALU.add), [ba1], [ba1])
            dve.op(tt_fn(a1[:], a1[:], y[:], ALU.mult), [ba1, by], [ba1])
            a2, ba2 = tr.next()
            act.op(act_fn(a2[:], a1[:], AF.Sigmoid, scale=1.5957691216057308), [ba1], [ba2])
            dve.op(tt_fn(a2[:], a2[:], y[:], ALU.mult), [ba2, by], [ba2])
            sp.dma(out=zp[g], in_=a2[:], reads=[ba2])
    return


def host_masks():
    s = np.arange(128)[:, None] // 16
    t = np.arange(128)[None, :] // 16
    m = np.zeros((128, 2, 128), np.float32)
    m[:, 0, :] = (t >= s)
    m[:, 1, :] = (s >= t)
    return m


def phaseC_inputs(hn1_seq, gh, lam_re, lam_im, log_dt, b_re, b_im, c_re, c_im, d_skip):
    G0 = gh * NG
    sl = slice(G0, G0 + NG)

    def dpg(a):
        return np.ascontiguousarray(a.transpose(0, 2, 1).reshape(128, NG))

    lamr = dpg(lam_re[:, sl, :])
    lami = dpg(lam_im[:, sl, :])
    ldt = dpg(np.broadcast_to(log_dt[:, sl, None], (2, NG, 64)))
    Bri = np.stack([b_re[:, sl].transpose(0, 2, 1, 3).reshape(128, NG, 16),
                    b_im[:, sl].transpose(0, 2, 1, 3).reshape(128, NG, 16)], axis=1)
    Cri = np.stack([c_re[:, sl].transpose(0, 3, 1, 2).reshape(128, NG, 16),
                    c_im[:, sl].transpose(0, 3, 1, 2).reshape(128, NG, 16)], axis=1)
    dg = d_skip[G0 * 16:(G0 + NG) * 16].reshape(NG, 16)
    dcol = np.ascontiguousarray(np.broadcast_to(dg.T[None, :, :], (8, 16, NG)).reshape(128, NG))
    u = hn1_seq[:, G0 * 16:(G0 + NG) * 16].reshape(NK, 8, NG, 16)
    Unat = np.ascontiguousarray(u.transpose(2, 1, 3, 0).reshape(NG, 128, NK))
    Uw = np.ascontiguousarray(Unat.reshape(NG, 128, NWIN, 8).transpose(2, 1, 0, 3))
    Uwr = np.ascontiguousarray(Unat[:, :, ::-1].reshape(NG, 128, NWIN, 8).transpose(2, 1, 0, 3))
    return {"lamr": lamr, "lami": lami, "ldt": ldt, "Bri": np.ascontiguousarray(Bri),
            "Cri": np.ascontiguousarray(Cri), "dcol": dcol, "Unat": Unat, "Uw": Uw, "Uwr": Uwr,
            "masks": host_masks(), "cst": host_consts()}


def phaseC_unpack(zp):
    return np.ascontiguousarray(zp.reshape(NG, 8, 16, NK).transpose(3, 1, 0, 2).reshape(SEQ, NG * 16))


def emit_phaseD(P, io):
    with contextlib.ExitStack() as ph:
        P.cur = ph
        _phaseD_body(P, io["zs"], io["hn"], io["dvec"], io["x2"], io["w_glu"], io["gf"], io["w_r"], io["b_r"],
                     io["x3_out"], io["aff_out"], io["affT_out"])
        P.barrier()
    P.cur = None


def _phaseD_body(P, zt, hn_in, dvec, x2, w_glu, gf, w_r, b_r, x3_out, aff_out, affT_out):
    dvb = P.sb([128, D], F32, name="dvb")
    bdvb = Buf()
    P.sp.dma(out=dvb[:], in_=bcast_rows(dvec, 128), writes=[bdvb])
    st = P.cur
    wgl = P.sb([128, 8, 2 * D], BF16, name="wgl")
    bwgl = Buf()
    wv = w_glu.rearrange("(k p) e -> p k e", p=128)
    for k in range(8):
        P.pool.dma(out=wgl[:, k, :], in_=wv[:, k, :], writes=[bwgl])
    gfb = P.sb([128, D], F32, name="gfb")
    bgfb = Buf()
    P.sp.dma(out=gfb[:], in_=bcast_rows(gf, 128), writes=[bgfb])
    wr = P.sb([128, 8, 16], F32, name="wr")
    bwr = Buf()
    P.sp.dma(out=wr[:], in_=w_r.rearrange("(k p) e -> p k e", p=128), writes=[bwr])
    brb = P.sb([128, 16], F32, name="brb")
    bbrb = Buf()
    P.sp.dma(out=brb[:], in_=bcast_rows(b_r, 128), writes=[bbrb])
    zr = Ring(P, 2, [128, D], F32, st)
    zbr = Ring(P, 2, [128, D], BF16, st)
    hnr2 = Ring(P, 2, [128, D], F32, st)
    xr = Ring(P, 2, [128, D], F32, st)
    x3r = Ring(P, 2, [128, D], F32, st)
    ptr = Ring(P, 1, [128, 8, 128], BF16, st, psum=True)
    zTr = Ring(P, 2, [128, 8, 128], BF16, st)
    pvr = Ring(P, 1, [128, 512], F32, st, psum=True)
    pgr = Ring(P, 1, [128, 512], F32, st, psum=True)
    sgr = Ring(P, 2, [128, 512], F32, st)
    rings = {
        "junk": Ring(P, 1, [128, D], F32, st),
        "small": Ring(P, 12, [128, 1], F32, st),
        "hf": Ring(P, 2, [128, D], F32, st),
        "ptf": Ring(P, 1, [128, 8, 128], F32, st, psum=True),
        "hfT": Ring(P, 2, [128, 8, 128], F32, st),
        "plog": Ring(P, 1, [128, 512], F32, st, psum=True),
        "sm16": Ring(P, 6, [128, 16], F32, st),
        "pat": Ring(P, 1, [128, 512], F32, st, psum=True),
        "aT": Ring(P, 2, [16, 128], F32, st),
    }
    for tt in range(OWN // 128):
        z_, bz = zr.next()
        P.sp.dma(out=z_[:], in_=zt[tt * 128:(tt + 1) * 128, :], writes=[bz])
        xt, bx = xr.next()
        P.sp.dma(out=xt[:], in_=x2[tt * 128:(tt + 1) * 128, :], writes=[bx])
        hn_, bhn_ = hnr2.next()
        P.sp.dma(out=hn_[:], in_=hn_in[tt * 128:(tt + 1) * 128, :], writes=[bhn_])
        P.dve.op(tt_fn(hn_[:], hn_[:], dvb[:], ALU.mult), [bhn_, bdvb], [bhn_])
        P.dve.op(tt_fn(z_[:], z_[:], hn_[:], ALU.add), [bz, bhn_], [bz])
        P.dve.op(tt_fn(hn_[:], z_[:], z_[:], ALU.mult), [bz], [bhn_])
        P.dve.op(ts_fn(hn_[:], hn_[:], 0.044715, 1.0, ALU.mult, ALU.add), [bhn_], [bhn_])
        P.dve.op(tt_fn(hn_[:], hn_[:], z_[:], ALU.mult), [bhn_, bz], [bhn_])
        P.act.op(act_fn(hn_[:], hn_[:], AF.Sigmoid, scale=1.5957691216057308), [bhn_], [bhn_])
        zb, bzb = zbr.next()
        P.dve.op(tt_fn(zb[:], hn_[:], z_[:], ALU.mult), [bhn_, bz], [bzb])
        pt, bpt = ptr.next()
        for k in range(8):
            P.pe.op(lambda e, k=k, pt=pt, zb=zb: e.transpose(pt[:, k, :], zb[:, k * 128:(k + 1) * 128], P.identb[:]),
                    [bzb, P.bidb], [bpt])
        zT, bzT = zTr.next()
        P.act.op(act_fn(zT[:], pt[:], AF.Copy), [bpt], [bzT])
        x3t, bx3 = x3r.next()
        for half in range(2):
            pv, bpv = pvr.next()
            pg, bpg = pgr.next()
            for k in range(8):
                P.pe.op(mm_fn(pv[:], zT[:, k, :], wgl[:, k, half * 512:(half + 1) * 512], k == 0, k == 7),
                        [bzT, bwgl], [bpv])
            for k in range(8):
                P.pe.op(mm_fn(pg[:], zT[:, k, :], wgl[:, k, D + half * 512:D + (half + 1) * 512], k == 0, k == 7),
                        [bzT, bwgl], [bpg])
            sg, bsg = sgr.next()
            P.act.op(act_fn(sg[:], pg[:], AF.Sigmoid), [bpg], [bsg])
            P.dve.op(tt_fn(sg[:], sg[:], pv[:], ALU.mult), [bsg, bpv], [bsg])
            P.dve.op(tt_fn(x3t[:, half * 512:(half + 1) * 512], sg[:], xt[:, half * 512:(half + 1) * 512], ALU.add),
                     [bsg, bx], [bx3])
        P.sp.dma(out=x3_out[tt * 128:(tt + 1) * 128, :], in_=x3t[:], reads=[bx3])
        router_tile(P, x3t, bx3, gfb, bgfb, wr, bwr, brb, bbrb, rings, aff_out, affT_out, tt)
    return


def to_local(full_seq, h):
    return local_view(full_seq, h, OWN)


def from_local(parts):
    out = []
    for b in range(NCORES // 2):
        a0 = parts[2 * b]
        a1 = parts[2 * b + 1][::-1]
        out.append(np.concatenate([a0, a1], axis=0))
    return np.stack(out)


G32 = 32
NKL = OWN // 8
NWL = NKL // 8


def emit_phaseC2(P, io):
    with contextlib.ExitStack() as ph:
        P.cur = ph
        _phaseC2_body(P, io)
        P.barrier()
    P.cur = None


def _phaseC2_body(P, io):
    dve, act, pe, pool, sp = P.dve, P.act, P.pe, P.pool, P.sp
    s5p, s5B, s5C = io["s5p"], io["s5B"], io["s5C"]
    HN, ZS = io["hn"], io["zs_out"]
    SAo, SAall = io["sa_own"], io["sa_all"]

    MS, RS = io["ms"], io["rs"]
    bM = Buf()
    bR = Buf()
    U = P.sb([128, 64, NKL], BF16, name="U")
    bU = Buf()
    A8 = [P.sb([128, 2, 2, G32], F32, name="A8_%d" % d) for d in range(2)]
    bA8 = Buf()
    mk = P.sb([128, 2, 128], F32, name="mk")
    bmk = Buf()
    sp.dma(out=mk[:], in_=io["masks"], writes=[bmk])
    flg = P.sb([128, 2], F32, name="flg")
    bflg = Buf()
    sp.dma(out=flg[:], in_=io["flags"], writes=[bflg])
    hpi = P.sb([128, 1], F32, name="hpi")
    bhpi = Buf()
    dve.op(lambda e: e.memset(hpi[:], HALF_PI), [], [bhpi])
    g1 = P.sb([128, G32, 16], F32, name="g1")
    g2 = P.sb([128, G32, 16], F32, name="g2")
    bg1, bg2 = Buf(), Buf()
    pw_t = [P.sb([128, 9, 2, G32], F32, name="pw%d" % d) for d in range(2)]
    ipw_t = [P.sb([128, 9, 2, G32], F32, name="ipw%d" % d) for d in range(2)]
    bbr_t = [P.sb([128, G32, 16], F32, name="bbr%d" % d) for d in range(2)]
    bbi_t = [P.sb([128, G32, 16], F32, name="bbi%d" % d) for d in range(2)]
    with contextlib.ExitStack() as stp:
        M = P.sb([128, 64, 128], BF16, name="M", stack=stp)
        Rt = [[P.sb([128, G32, 128], BF16, name="R%d%d" % (d, r), stack=stp) for r in range(2)] for d in range(2)]
        par = P.sb([128, 2, 3, G32], F32, name="par", stack=stp)
        bpar = Buf()
        sp.dma(out=par[:], in_=s5p, writes=[bpar])
        Bt = P.sb([128, 2, 2, G32, 16], F32, name="Bt", stack=stp)
        bBt = Buf()
        sp.dma(out=Bt[:], in_=s5B, writes=[bBt])
        Ct = P.sb([128, 2, 2, G32, 16], F32, name="Ct", stack=stp)
        bCt = Buf()
        sp.dma(out=Ct[:], in_=s5C, writes=[bCt])

        def small():
            return P.sb([128, G32], F32, stack=stp), Buf()

        def T2(a, ba, b, bb, op):
            o, bo = small()
            dve.op(tt_fn(o[:], a[:], b[:], op), [ba, bb], [bo])
            return o, bo

        def bc(a):
            return bass.AP(a.tensor, a.offset, [list(a.ap[0]), [1, G32], [0, 16]])

        pws, ipws, bbs = [], [], []
        for d in range(2):
            lamr, lami, ldt = par[:, d, 0, :], par[:, d, 1, :], par[:, d, 2, :]
            dt, bdt = small()
            act.op(act_fn(dt[:], ldt, AF.Exp), [bpar], [bdt])
            a_, ba_ = small()
            dve.op(tt_fn(a_[:], lamr, dt[:], ALU.mult), [bpar, bdt], [ba_])
            ang, bang = small()
            dve.op(tt_fn(ang[:], lami, dt[:], ALU.mult), [bpar, bdt], [bang])
            mag, bmag = small()
            act.op(act_fn(mag[:], a_[:], AF.Exp, scale=1.0 / 16), [ba_], [bmag])
            s16, bs16 = small()
            act.op(act_fn(s16[:], ang[:], AF.Sin, scale=1.0 / 16), [bang], [bs16])
            c16, bc16 = small()
            act.op(act_fn(c16[:], ang[:], AF.Sin, scale=1.0 / 16, bias=hpi[:, 0:1]), [bang, bhpi], [bc16])
            re, bre = T2(mag, bmag, c16, bc16, ALU.mult)
            im, bim = T2(mag, bmag, s16, bs16, ALU.mult)
            for _ in range(4):
                r2, br2 = T2(re, bre, re, bre, ALU.mult)
                i2, bi2 = T2(im, bim, im, bim, ALU.mult)
                nre, bnre = T2(r2, br2, i2, bi2, ALU.subtract)
                nim, bnim = small()
                dve.op(stt_fn(nim[:], re[:], 2.0, im[:], ALU.mult, ALU.mult), [bre, bim], [bnim])
                re, bre, im, bim = nre, bnre, nim, bnim
            pw = pw_t[d]
            ipw = ipw_t[d]
            bpw, bipw = Buf(), Buf()
            dve.op(lambda e, pw=pw: e.memset(pw[:, 0, 0, :], 1.0), [], [bpw])
            dve.op(lambda e, pw=pw: e.memset(pw[:, 0, 1, :], 0.0), [], [bpw])
            dve.op(lambda e, ipw=ipw: e.memset(ipw[:, 0, 0, :], 1.0), [], [bipw])
            dve.op(lambda e, ipw=ipw: e.memset(ipw[:, 0, 1, :], 0.0), [], [bipw])
            dve.op(lambda e, pw=pw, re=re: e.tensor_copy(out=pw[:, 1, 0, :], in_=re[:]), [bre], [bpw])
            dve.op(lambda e, pw=pw, im=im: e.tensor_copy(out=pw[:, 1, 1, :], in_=im[:]), [bim], [bpw])
            t1, bt1 = small()
            t2, bt2 = small()
            for n in range(1, 8):
                dve.op(tt_fn(t1[:], pw[:, n, 0, :], re[:], ALU.mult), [bpw, bre], [bt1])
                dve.op(tt_fn(t2[:], pw[:, n, 1, :], im[:], ALU.mult), [bpw, bim], [bt2])
                dve.op(tt_fn(pw[:, n + 1, 0, :], t1[:], t2[:], ALU.subtract), [bt1, bt2], [bpw])
                dve.op(tt_fn(t1[:], pw[:, n, 0, :], im[:], ALU.mult), [bpw, bim], [bt1])
                dve.op(tt_fn(t2[:], pw[:, n, 1, :], re[:], ALU.mult), [bpw, bre], [bt2])
                dve.op(tt_fn(pw[:, n + 1, 1, :], t1[:], t2[:], ALU.add), [bt1, bt2], [bpw])
            en, ben = small()
            for n in range(1, 9):
                act.op(act_fn(en[:], a_[:], AF.Exp, scale=-2.0 * n), [ba_], [ben])
                dve.op(tt_fn(ipw[:, n, 0, :], pw[:, n, 0, :], en[:], ALU.mult), [bpw, ben], [bipw])
                dve.op(stt_fn(ipw[:, n, 1, :], pw[:, n, 1, :], -1.0, en[:], ALU.mult, ALU.mult), [bpw, ben], [bipw])
            a8 = A8[d]
            dve.op(lambda e, a8=a8, pw=pw: e.tensor_copy(out=a8[:, 0, 0, :], in_=pw[:, 8, 0, :]), [bpw], [bA8])
            dve.op(lambda e, a8=a8, pw=pw: e.tensor_copy(out=a8[:, 0, 1, :], in_=pw[:, 8, 0, :]), [bpw], [bA8])
            dve.op(ts_fn(a8[:, 1, 0, :], pw[:, 8, 1, :], -1.0, None, ALU.mult), [bpw], [bA8])
            dve.op(lambda e, a8=a8, pw=pw: e.tensor_copy(out=a8[:, 1, 1, :], in_=pw[:, 8, 1, :]), [bpw], [bA8])
            lr2, blr2 = small()
            dve.op(tt_fn(lr2[:], lamr, lamr, ALU.mult), [bpar], [blr2])
            li2, bli2 = small()
            dve.op(tt_fn(li2[:], lami, lami, ALU.mult), [bpar], [bli2])
            den, bden = T2(lr2, blr2, li2, bli2, ALU.add)
            rden, brden = small()
            dve.op(lambda e, rden=rden, den=den: e.reciprocal(out=rden[:], in_=den[:]), [bden], [brden])
            nr, bnr = small()
            dve.op(ts_fn(nr[:], re[:], -1.0, None, ALU.add), [bre], [bnr])
            u1, bu1 = small()
            dve.op(tt_fn(u1[:], nr[:], lamr, ALU.mult), [bnr, bpar], [bu1])
            u2, bu2 = small()
            dve.op(tt_fn(u2[:], im[:], lami, ALU.mult), [bim, bpar], [bu2])
            u3, bu3 = T2(u1, bu1, u2, bu2, ALU.add)
            fre, bfre = T2(u3, bu3, rden, brden, ALU.mult)
            u4, bu4 = small()
            dve.op(tt_fn(u4[:], im[:], lamr, ALU.mult), [bim, bpar], [bu4])
            u5, bu5 = small()
            dve.op(tt_fn(u5[:], nr[:], lami, ALU.mult), [bnr, bpar], [bu5])
            u6, bu6 = T2(u4, bu4, u5, bu5, ALU.subtract)
            fim, bfim = T2(u6, bu6, rden, brden, ALU.mult)
            bbr = bbr_t[d]
            bbi = bbi_t[d]
            bbb = Buf()
            dve.op(tt_fn(g1[:], Bt[:, d, 0, :, :], bc(fre[:]), ALU.mult), [bBt, bfre], [bg1])
            dve.op(tt_fn(g2[:], Bt[:, d, 1, :, :], bc(fim[:]), ALU.mult), [bBt, bfim], [bg2])
            dve.op(tt_fn(bbr[:], g1[:], g2[:], ALU.subtract), [bg1, bg2], [bbb])
            dve.op(tt_fn(g1[:], Bt[:, d, 1, :, :], bc(fre[:]), ALU.mult), [bBt, bfre], [bg1])
            dve.op(tt_fn(g2[:], Bt[:, d, 0, :, :], bc(fim[:]), ALU.mult), [bBt, bfim], [bg2])
            dve.op(tt_fn(bbi[:], g1[:], g2[:], ALU.add), [bg1, bg2], [bbb])
            pws.append((pw, bpw))
            ipws.append((ipw, bipw))
            bbs.append((bbr, bbi, bbb))

        def cprod(dst_re, dst_im, bdst, xr, xi, bx, tab, btab, idx, neg_im=False):
            bx = list(bx) if isinstance(bx, (list, tuple)) else [bx]
            for i in range(8):
                tr, ti = bc(tab[:, idx(i), 0, :]), bc(tab[:, idx(i), 1, :])
                dve.op(tt_fn(g1[:], xr, tr, ALU.mult), bx + [btab], [bg1])
                dve.op(tt_fn(g2[:], xi, ti, ALU.mult), bx + [btab], [bg2])
                dve.op(tt_fn(dst_re[:, :, i, :], g1[:], g2[:], ALU.subtract), [bg1, bg2], [bdst])
                dve.op(tt_fn(g1[:], xr, ti, ALU.mult), bx + [btab], [bg1])
                dve.op(tt_fn(g2[:], xi, tr, ALU.mult), bx + [btab], [bg2])
                if neg_im:
                    dve.op(stt_fn(dst_im[:, :, i, :], g1[:], -1.0, g2[:], ALU.mult, ALU.subtract), [bg1, bg2], [bdst])
                else:
                    dve.op(tt_fn(dst_im[:, :, i, :], g1[:], g2[:], ALU.add), [bg1, bg2], [bdst])

        v4 = lambda t: t[:].rearrange("p g (j c) -> p g j c", j=8)
        f3 = lambda t: t[:].rearrange("p g s c -> p g (s c)")

        with contextlib.ExitStack() as st2:
            XY = [[P.sb([128, G32, 8, 16], BF16, stack=st2) for _ in range(4)] for _ in range(2)]
            bXY = Buf()
            (pwA, bpwA), (ipwA, bipwA) = pws[0], ipws[0]
            (pwB, bpwB), (ipwB, bipwB) = pws[1], ipws[1]
            cprod(XY[0][0], XY[0][1], bXY, bbs[0][0][:], bbs[0][1][:], bbs[0][2], ipwA, bipwA, lambda s: s)
            cprod(XY[0][2], XY[0][3], bXY, Ct[:, 0, 0, :, :], Ct[:, 0, 1, :, :], bCt, pwA, bpwA, lambda t: t, neg_im=True)
            cprod(XY[1][0], XY[1][1], bXY, bbs[1][0][:], bbs[1][1][:], bbs[1][2], pwB, bpwB, lambda s: s)
            cprod(XY[1][2], XY[1][3], bXY, Ct[:, 1, 0, :, :], Ct[:, 1, 1, :, :], bCt, ipwB, bipwB, lambda t: t, neg_im=True)
            cprod(v4(Rt[0][0]), v4(Rt[0][1]), bR, Ct[:, 0, 0, :, :], Ct[:, 0, 1, :, :], bCt, pwA, bpwA,
                  lambda j: j + 1, neg_im=True)
            cprod(v4(Rt[1][0]), v4(Rt[1][1]), bR, Ct[:, 1, 0, :, :], Ct[:, 1, 1, :, :], bCt, pwB, bpwB,
                  lambda j: 8 - j, neg_im=True)
            pk = [[Ring(P, 1, [128, 512], F32, st2, psum=True) for _ in range(2)] for _ in range(2)]
            tmr = Ring(P, 4, [128, 128], F32, st2)
            for g in range(G32):
                pp = [[None, None], [None, None]]
                for d in range(2):
                    Xr, Xi, Yr, Yn = [f3(t) for t in XY[d]]
                    for hf in range(2):
                        r0 = 64 * hf
                        pt, bpt = pk[d][hf].next()
                        pe.op(mm_fn(pt[:, 0:128], Xr[r0:r0 + 64, g, :], Yr[r0:r0 + 64, g, :], True, False), [bXY], [bpt])
                        pe.op(mm_fn(pt[:, 0:128], Xi[r0:r0 + 64, g, :], Yn[r0:r0 + 64, g, :], False, True), [bXY], [bpt])
                        pp[d][hf] = (pt, bpt)
                for hf in range(2):
                    tm, btm = tmr.next()
                    dve.op(tt_fn(tm[:], pp[0][hf][0][:, 0:128], mk[:, 0, :], ALU.mult), [pp[0][hf][1], bmk], [btm])
                    tm2, btm2 = tmr.next()
                    dve.op(tt_fn(tm2[:], pp[1][hf][0][:, 0:128], mk[:, 1, :], ALU.mult), [pp[1][hf][1], bmk], [btm2])
                    dve.op(tt_fn(M[:, hf * G32 + g, :], tm[:], tm2[:], ALU.add), [btm, btm2], [bM])
            sp.dma(out=MS.ap(), in_=M[:].rearrange("p g c -> p (g c)"), reads=[bM])
            for d in range(2):
                for r in range(2):
                    sp.dma(out=RS.ap()[:, (2 * d + r) * G32 * 128:(2 * d + r + 1) * G32 * 128],
                           in_=Rt[d][r][:].rearrange("p g c -> p (g c)"), reads=[bR])
            P.barrier()

    with contextlib.ExitStack() as st2:
        Tb = Ring(P, 2, [128, 8, D], BF16, st2)
        Tb2 = Ring(P, 1, [128, 64, 128], BF16, st2)
        ptu = Ring(P, 2, [128, 8, 128], BF16, st2, psum=True)
        for kb in range(NKL // 128):
            tb, btb = Tb.next()
            src = HN[kb * 1024:(kb + 1) * 1024, :].rearrange("(p s) d -> p s d", s=8)
            pool.dma(out=tb[:], in_=src, writes=[btb])
            tb2, btb2 = Tb2.next()
            dve.op(lambda e, tb=tb, tb2=tb2: e.tensor_copy(
                out=tb2[:].rearrange("p g (s c) -> p s g c", s=8),
                in_=tb[:].rearrange("p s (g c) -> p s g c", c=16)), [btb], [btb2])
            for g0 in range(0, 64, 8):
                pt, bpt = ptu.next()
                for gi in range(8):
                    g = g0 + gi
                    pe.op(lambda e, pt=pt, gi=gi, g=g, tb2=tb2: e.transpose(pt[:, gi, :], tb2[:, g, :],
                                                                           P.identb[:]), [btb2, P.bidb], [bpt])
                act.op(act_fn(U[:, g0:g0 + 8, kb * 128:(kb + 1) * 128], pt[:], AF.Copy), [bpt], [bU])
        P.barrier()

    with contextlib.ExitStack() as sth:
        hist = [P.sb([128, 2, G32, NKL], BF16, stack=sth) for _ in range(2)]
        bhist = Buf()
        with contextlib.ExitStack() as st3:
            Wz = P.sb([128, G32, 4, 128], BF16, stack=st3)
            bWz = Buf()
            WT = [P.sb([128, G32, 8, 16], BF16, stack=st3) for _ in range(2)]
            bWT = Buf()
            ptw = Ring(P, 2, [128, 4, 128], BF16, st3, psum=True)
            pvr = Ring(P, 3, [128, 2, G32, 8], F32, st3, psum=True)
            S4r = Ring(P, 4, [128, 3, G32], F32, st3)
            p1 = P.sb([128, 2, G32], F32, stack=st3)
            p2 = P.sb([128, 2, G32], F32, stack=st3)
            bp1, bp2 = Buf(), Buf()
            gx = [P.sb([128, 2 * G32], F32, stack=st3) for _ in range(2)]
            bgx = Buf()
            for d in range(2):
                pw, bpw = pws[d]
                cprod(WT[0], WT[1], bWT, bbs[d][0][:], bbs[d][1][:], bbs[d][2], pw, bpw,
                      (lambda s: 7 - s) if d == 0 else (lambda s: s))
                pool.op(lambda e: e.memset(Wz[:], 0.0), [], [bWz])
                W3 = (f3(WT[0]), f3(WT[1]))
                for g in range(G32):
                    pt, bpt = ptw.next()
                    for ri in range(2):
                        pe.op(lambda e, pt=pt, g=g, ri=ri: e.transpose(pt[:, ri, :], W3[ri][:, g, :], P.identb[:]),
                              [bWT, P.bidb], [bpt])
                    act.op(act_fn(Wz[:, g, 0:4:2, 0:64], pt[:, 0:2, 0:64], AF.Copy), [bpt], [bWz])
                    act.op(act_fn(Wz[:, g, 1:4:2, 64:128], pt[:, 0:2, 64:128], AF.Copy), [bpt], [bWz])
                S4, bS = S4r.next()
                if d == 0:
                    dve.op(lambda e, S4=S4: e.memset(S4[:], 0.0), [], [bS])
                else:
                    sp.dma(out=gx[0][:], in_=SAall.ap()[0:128, :], writes=[bgx])
                    sp.dma(out=gx[1][:], in_=SAall.ap()[128:256, :], writes=[bgx])
                    dve.op(ts_fn(gx[0][:], gx[0][:], flg[:, 0:1], None, ALU.mult), [bgx, bflg], [bgx])
                    S2v = S4[:, 0:2, :].rearrange("p r g -> p (r g)")
                    dve.op(stt_fn(S2v, gx[1][:], flg[:, 1:2], gx[0][:], ALU.mult, ALU.add), [bgx, bflg], [bS])
                    dve.op(lambda e, S4=S4: e.tensor_copy(out=S4[:, 2, :], in_=S4[:, 0, :]), [bS], [bS])
                a8 = A8[d]
                for wi in range(NWL):
                    w = wi if d == 0 else NWL - 1 - wi
                    pv, bpv = pvr.next()
                    for g in range(G32):
                        for ri in range(2):
                            pe.op(mm_fn(pv[:, ri, g, :], Wz[:, g, 2 * ri, :], U[:, g, 8 * w:8 * w + 8], True, False),
                                  [bWz, bU], [bpv])
                            pe.op(mm_fn(pv[:, ri, g, :], Wz[:, g, 2 * ri + 1, :], U[:, G32 + g, 8 * w:8 * w + 8],
                                        False, True), [bWz, bU], [bpv])
                    dve.selfsync = SCAN_SELFSYNC
                    for ji in range(8):
                        jj = ji if d == 0 else 7 - ji
                        k = 8 * w + jj
                        act.op(act_fn(hist[d][:, :, :, k], S4[:, 0:2, :], AF.Copy), [bS], [bhist])
                        Sn, bSn = S4r.next()
                        dve.op(tt_fn(p1[:], a8[:, 0, :, :], S4[:, 0:2, :], ALU.mult), [bA8, bS], [bp1])
                        dve.op(tt_fn(p2[:], a8[:, 1, :, :], S4[:, 1:3, :], ALU.mult), [bA8, bS], [bp2])
                        dve.op(tt_fn(p1[:], p1[:], p2[:], ALU.add), [bp1, bp2], [bp1])
                        dve.op(tt_fn(Sn[:, 0:2, :], p1[:], pv[:, :, :, jj], ALU.add), [bp1, bpv], [bSn])
                        dve.op(lambda e, Sn=Sn: e.tensor_copy(out=Sn[:, 2, :], in_=Sn[:, 0, :]), [bSn], [bSn])
                        S4, bS = Sn, bSn
                    dve.selfsync = True
                if d == 0:
                    sp.dma(out=SAo.ap(), in_=S4[:, 0:2, :].rearrange("p r g -> p (r g)"), reads=[bS])
                    P.all_gather_pair(SAo, SAall)
            P.barrier()
        with contextlib.ExitStack() as st4:
            Z = P.sb([128, 8, D // 2], F32, stack=st4)
            bZ = Buf()
            M = P.sb([128, 64, 128], BF16, stack=st4)
            Rt = [[P.sb([128, G32, 128], BF16, stack=st4) for r in range(2)] for d in range(2)]
            bM, bR = Buf(), Buf()
            sp.dma(out=M[:].rearrange("p g c -> p (g c)"), in_=MS.ap(), writes=[bM])
            for d in range(2):
                for r in range(2):
                    sp.dma(out=Rt[d][r][:].rearrange("p g c -> p (g c)"),
                           in_=RS.ap()[:, (2 * d + r) * G32 * 128:(2 * d + r + 1) * G32 * 128], writes=[bR])
            p1r = Ring(P, 2, [128, 512], F32, st4, psum=True)
            p2r = [Ring(P, 1, [128, 512], F32, st4, psum=True) for _ in range(2)]
            pTr = Ring(P, 2, [128, 4, 128], F32, st4, psum=True)
            c1r = Ring(P, 2, [128, 512], F32, st4)
            ygr = Ring(P, 2, [128, 512], F32, st4)
            for kb in range(NKL // 128):
                ks = slice(kb * 128, (kb + 1) * 128)
                for hf in range(2):
                    r0 = 64 * hf
                    for q in range(G32 // 4):
                        P1, bP1 = p1r.next()
                        P2, bP2 = p2r[hf].next()
                        for gi in range(4):
                            g32 = 4 * q + gi
                            g = hf * G32 + g32
                            cs = slice(gi * 128, (gi + 1) * 128)
                            pe.op(mm_fn(P1[:, cs], M[:, g, :], U[:, g, ks], True, True), [bM, bU], [bP1])
                            seq = [(Rt[0][0], hist[0], 0), (Rt[0][1], hist[0], 1), (Rt[1][0], hist[1], 0),
                                   (Rt[1][1], hist[1], 1)]
                            for n, (Rm, hs, ri) in enumerate(seq):
                                pe.op(mm_fn(P2[:, cs], Rm[r0:r0 + 64, g32, :], hs[r0:r0 + 64, ri, g32, ks],
                                            n == 0, n == 3), [bR, bhist], [bP2])
                        c1, bc1 = c1r.next()
                        act.op(act_fn(c1[:], P1[:], AF.Copy), [bP1], [bc1])
                        yg, byg = ygr.next()
                        dve.op(tt_fn(yg[:], P2[:], c1[:], ALU.add), [bP2, bc1], [byg])
                        pT, bpT = pTr.next()
                        for gi in range(4):
                            pe.op(lambda e, pT=pT, gi=gi, yg=yg: e.transpose(pT[:, gi, :], yg[:, gi * 128:(gi + 1) * 128],
                                                                             P.identf), [byg, P.bcst], [bpT])
                        c0 = 16 * (4 * q)
                        zdst = Z[:, :, c0:c0 + 64].rearrange("p t (g c) -> p t g c", g=4)
                        zsrc = pT[:].rearrange("p g (t c) -> p t g c", t=8)
                        act.op(act_fn(zdst, zsrc, AF.Copy), [bpT], [bZ])
                    dst = ZS[kb * 1024:(kb + 1) * 1024, hf * 512:(hf + 1) * 512].rearrange("(p t) d -> p t d", t=8)
                    sp.dma(out=dst, in_=Z[:], reads=[bZ])


def build_fused(ncores=NCORES):
    P = Prog()
    P.ncores = ncores
    xw = P.inp("xw", [WIN, D])
    g0 = P.inp("g0", [1, D])
    w_in = P.inp("w_in", [D, 2560])
    cvec = P.inp("cvec", [128, 4, 34])
    qk_g = P.inp("qk_g", [128, 2])
    w_out = P.inp("w_out", [D, D])
    eb = P.inp("eb", [4, 128, 6, 384])
    cst_in = P.inp("cst", [128, 6, 128])
    gf = P.inp("gf", [2, D])
    w_r = P.inp("w_r", [2, D, 16])
    b_r = P.inp("b_r", [2, 16])
    wg = P.inp("wg", [2, NEXP, D, D])
    wu = P.inp("wu", [2, NEXP, D, D])
    wd = P.inp("wd", [2, NEXP, D, D])
    gn = P.inp("gn", [1, D])
    s5p = P.inp("s5p", [128, 2, 3, G32])
    s5B = P.inp("s5B", [128, 2, 2, G32, 16])
    s5C = P.inp("s5C", [128, 2, 2, G32, 16])
    dvec = P.inp("dvec", [1, D])
    masks = P.inp("masks", [128, 2, 128])
    flags = P.inp("flags", [128, 2])
    w_glu = P.inp("w_glu", [D, 2 * D])
    zeros_in = P.inp("zeros", [1024, D], BF16)
    out = P.outp("out", [OWN, D])
    X1 = P.dram("X1", [OWN, D])
    X2 = P.dram("X2", [OWN, D])
    X3 = P.dram("X3", [OWN, D])
    HN = P.dram("HN", [OWN, D])
    ZS = P.dram("ZS", [OWN, D])
    AFF = P.dram("AFF", [OWN, NEXP])
    ATo = P.dram("ATo", [NEXP, OWN])
    ATall = P.dram("ATall", [2 * NEXP, OWN])
    SAo = P.dram("SAo", [128, 2 * G32])
    SAall = P.dram("SAall", [256, 2 * G32])
    MS = P.dram("MS", [128, 64 * 128], BF16)
    RS = P.dram("RS", [128, 4 * G32 * 128], BF16)
    Xc = P.dram("Xc", [NEXP * CAP, D], BF16)
    Yc = P.dram("Yc", [NEXP * CAP, D], BF16)
    load_consts(P, cst_in)
    if SPARSE:
        for i in range(NEXP * CAP // 1024):
            P.sp.dma(out=Xc.ap()[i * 1024:(i + 1) * 1024, :], in_=zeros_in)
    emit_phaseA(P, dict(xw=xw, g0=g0, w_in=w_in, cvec=cvec, qk_g=qk_g, w_out=w_out, gf=gf[0:1, :], w_r=w_r[0],
                        b_r=b_r[0:1, :], eb=eb, x1_out=X1.ap(), aff_out=AFF.ap(), affT_out=ATo.ap()))
    P.all_gather_pair(ATo, ATall)
    at_view = ATall.ap().rearrange("(r e) t -> e r t", r=2)
    emitB = emit_phaseB_sparse if SPARSE else emit_phaseB
    emitB(P, dict(x1=X1.ap(), affT_seq=at_view, aff_own=AFF.ap(), gf=gf[0:1, :], wg=wg[0], wu=wu[0], wd=wd[0],
                  x2_out=X2.ap(), gn=gn, hn_out=HN.ap(), xc=Xc.ap(), yc=Yc.ap()))
    emit_phaseC2(P, dict(s5p=s5p, s5B=s5B, s5C=s5C, masks=masks, flags=flags, hn=HN.ap(), zs_out=ZS.ap(),
                         sa_own=SAo, sa_all=SAall, ms=MS, rs=RS))
    emit_phaseD(P, dict(zs=ZS.ap(), hn=HN.ap(), dvec=dvec, x2=X2.ap(), w_glu=w_glu, gf=gf[1:2, :], w_r=w_r[1],
                        b_r=b_r[1:2, :], x3_out=X3.ap(), aff_out=AFF.ap(), affT_out=ATo.ap()))
    P.all_gather_pair(ATo, ATall)
    emitB(P, dict(x1=X3.ap(), affT_seq=at_view, aff_own=AFF.ap(), gf=gf[1:2, :], wg=wg[1], wu=wu[1], wd=wd[1],
                  x2_out=out, xc=Xc.ap(), yc=Yc.ap()))
    return P.finish()


def fused_inputs(c, x, mix_norm_even, w_in, conv_w, conv_b, conv_ln_g, conv_ln_b, q_norm, k_norm,
                 w_out, mix_norm_odd, ssm_lam_re, ssm_lam_im, ssm_log_dt, ssm_b_re, ssm_b_im,
                 ssm_c_re, ssm_c_im, ssm_d, w_glu, ffn_norm, w_router, b_router,
                 w_e_gate, w_e_up, w_e_down, shared):
    b, h = c // 2, c % 2
    cw = conv_w[0] if h == 0 else conv_w[0][::-1]
    cvec = np.zeros((128, 4, 34), np.float32)
    cvec[:, :, 0:31] = cw.T.reshape(4, 128, 31).transpose(1, 0, 2)
    cvec[:, :, 31] = conv_b[0].reshape(4, 128).T
    cvec[:, :, 32] = conv_ln_g[0].reshape(4, 128).T
    cvec[:, :, 33] = conv_ln_b[0].reshape(4, 128).T
    qk = np.stack([np.tile(q_norm[0], 2), np.tile(k_norm[0], 2)], axis=1).astype(np.float32)
    order = [0, 1] if h == 0 else [1, 0]

    def gp(a):
        a = a[order]
        return a.reshape(2, 2, G32, 64).transpose(1, 3, 0, 2).reshape(128, 2, G32)

    ldt = np.broadcast_to(ssm_log_dt[0][:, :, None], (2, 64, 64))
    s5p = np.ascontiguousarray(np.stack([gp(ssm_lam_re[0]), gp(ssm_lam_im[0]), gp(ldt)], axis=2))

    def gB(a):
        a = a[order]
        return a.reshape(2, 2, G32, 64, 16).transpose(1, 3, 0, 2, 4).reshape(128, 2, G32, 16)

    def gC(a):
        a = a[order]
        return a.reshape(2, 2, G32, 16, 64).transpose(1, 4, 0, 2, 3).reshape(128, 2, G32, 16)

    s5B = np.ascontiguousarray(np.stack([gB(ssm_b_re[0]), gB(ssm_b_im[0])], axis=2))
    s5C = np.ascontiguousarray(np.stack([gC(ssm_c_re[0]), gC(ssm_c_im[0])], axis=2))
    flags = np.zeros((128, 2), np.float32)
    flags[:, 1 - h] = 1.0
    m = dict(shared)
    m.update({
        "xw": local_view(x[b], h, WIN), "cvec": cvec, "qk_g": np.ascontiguousarray(qk),
        "s5p": s5p, "s5B": s5B, "s5C": s5C, "flags": flags,
    })
    return m


def kernel(x, mix_norm_even, w_in, conv_w, conv_b, conv_ln_g, conv_ln_b, q_norm, k_norm,
           w_out, mix_norm_odd, ssm_lam_re, ssm_lam_im, ssm_log_dt, ssm_b_re, ssm_b_im,
           ssm_c_re, ssm_c_im, ssm_d, w_glu, ffn_norm, w_router, b_router,
           w_e_gate, w_e_up, w_e_down):
    f = lambda a: np.ascontiguousarray(np.asarray(a, dtype=np.float32))
    args = [f(a) for a in (x, mix_norm_even, w_in, conv_w, conv_b, conv_ln_g, conv_ln_b, q_norm, k_norm,
                           w_out, mix_norm_odd, ssm_lam_re, ssm_lam_im, ssm_log_dt, ssm_b_re, ssm_b_im,
                           ssm_c_re, ssm_c_im, ssm_d, w_glu, ffn_norm, w_router, b_router,
                           w_e_gate, w_e_up, w_e_down)]
    (x, mix_norm_even, w_in, conv_w, conv_b, conv_ln_g, conv_ln_b, q_norm, k_norm,
     w_out, mix_norm_odd, ssm_lam_re, ssm_lam_im, ssm_log_dt, ssm_b_re, ssm_b_im,
     ssm_c_re, ssm_c_im, ssm_d, w_glu, ffn_norm, w_router, b_router, w_e_gate, w_e_up, w_e_down) = args
    shared = {
        "g0": np.ascontiguousarray(mix_norm_even[0][None, :]), "w_in": w_in[0], "w_out": w_out[0],
        "eb": host_eb(), "cst": host_consts(), "gf": ffn_norm, "w_r": w_router, "b_r": b_router,
        "wg": w_e_gate, "wu": w_e_up, "wd": w_e_down, "gn": np.ascontiguousarray(mix_norm_odd[0][None, :]),
        "dvec": np.ascontiguousarray(ssm_d[0][None, :]), "masks": host_masks(), "w_glu": w_glu[0],
        "zeros": np.zeros((1024, D), ml_dtypes.bfloat16),
    }
    nc = get_prog("F", build_fused)
    in_maps = [fused_inputs(c, *args, shared) for c in range(NCORES)]
    res = run_bass_kernel_spmd(nc, in_maps, core_ids=list(range(NCORES)))
    return from_local([r["out"] for r in res.results]).astype(np.float32)


BIGIDX = float(1 << 20)
I32 = mybir.dt.int32


def emit_phaseB_sparse(P, io):
    with contextlib.ExitStack() as ph:
        P.cur = ph
        _phaseB_sparse_body(P, io["x1"], io["affT_seq"], io["aff_own"], io["gf"], io["wg"], io["wu"], io["wd"],
                            io["x2_out"], io.get("gn"), io.get("hn_out"), io["xc"], io["yc"])
        P.barrier()
    P.cur = None


def _phaseB_sparse_body(P, x1, affT_seq, aff_own, gf, wg, wu, wd, x2_out, gn, hn_out, Xc, Yc):
    dve, act, pe, pool, sp = P.dve, P.act, P.pe, P.pool, P.sp
    NT = OWN // 128
    if gn is not None:
        gnb = P.sb([128, D], F32, name="gnb")
        bgnb = Buf()
        sp.dma(out=gnb[:], in_=bcast_rows(gn, 128), writes=[bgnb])
    gfb = P.sb([128, D], F32, name="gfb")
    bgfb = Buf()
    sp.dma(out=gfb[:], in_=bcast_rows(gf, 128), writes=[bgfb])
    gate = P.sb([128, NT, NEXP], F32, name="gate")
    bgate = Buf()
    idx = P.sb([128, NT, NEXP], I32, name="idx")
    bidx = Buf()
    if P.bcreg is None:
        P.bcreg = P.nc.gpsimd.alloc_register("bcreg")
        P.nc.gpsimd.reg_mov(P.bcreg, NEXP * CAP - 1)

    with contextlib.ExitStack() as st:
        pthr = P.ps([128, 512], F32, stack=st)
        bpthr = Buf()
        aT = P.sb([NEXP, SEQ], F32, stack=st)
        baT = Buf()
        sp.dma(out=aT[:].rearrange("e (r t) -> e r t", r=2), in_=affT_seq, writes=[baT])
        junk = P.sb([NEXP, SEQ], F32, stack=st)
        bjunk = Buf()
        lo = P.sb([NEXP, 1], F32, stack=st)
        blo = Buf()
        mid = P.sb([NEXP, 1], F32, stack=st)
        bmid = Buf()
        cnt = P.sb([NEXP, 1], F32, stack=st)
        bcnt = Buf()
        ge = P.sb([NEXP, 1], F32, stack=st)
        bge = Buf()
        dve.op(lambda e: e.memset(lo[:], 0.0), [], [blo])
        for it in range(30):
            wk = 2.0 ** (-(it + 1))
            dve.op(ts_fn(mid[:], lo[:], wk, None, ALU.add), [blo], [bmid])
            dve.op(lambda e: e.tensor_scalar(out=junk[:], in0=aT[:], scalar1=mid[:, 0:1], scalar2=0.0,
                                             op0=ALU.is_ge, op1=ALU.add, accum_out=cnt[:, 0:1]),
                   [baT, bmid], [bjunk, bcnt])
            dve.op(ts_fn(ge[:], cnt[:], CAP - 0.5, None, ALU.is_ge), [bcnt], [bge])
            dve.op(stt_fn(lo[:], ge[:], wk, lo[:], ALU.mult, ALU.add), [bge, blo], [blo])
        dthr = P.sb([NEXP, NEXP], F32, stack=st)
        bdthr = Buf()
        dve.op(ts_fn(dthr[:], P.cst[0:NEXP, 0, 0:NEXP], lo[:, 0:1], None, ALU.mult), [P.bcst, blo], [bdthr])
        pe.op(mm_fn(pthr[:, 0:NEXP], P.cst[0:NEXP, 3, :], dthr[:], True, True), [P.bcst, bdthr], [bpthr])
        thrb = P.sb([128, NEXP], F32, stack=st)
        bthrb = Buf()
        act.op(act_fn(thrb[:], pthr[:, 0:NEXP], AF.Copy), [bpthr], [bthrb])
        ao = P.sb([128, NT, NEXP], F32, stack=st)
        bao = Buf()
        sp.dma(out=ao[:], in_=aff_own.rearrange("(t p) e -> p t e", p=128), writes=[bao])
        msk = P.sb([128, NT, NEXP], F32, stack=st)
        bmsk = Buf()

        def bct(t):
            a = t[:]
            return bass.AP(a.tensor, a.offset, [list(a.ap[0]), [0, NT], [1, NEXP]])

        dve.op(tt_fn(msk[:], ao[:], bct(thrb), ALU.is_ge), [bao, bthrb], [bmsk])
        ppos = P.ps([128, 512], F32, stack=st)
        bppos = Buf()
        pcnt = P.ps([128, 512], F32, stack=st)
        bpcnt = Buf()
        m2 = msk[:].rearrange("p t e -> p (t e)")
        pe.op(mm_fn(ppos[:, 0:NT * NEXP], P.cst[:, 4, :], m2, True, True), [P.bcst, bmsk], [bppos])
        pe.op(mm_fn(pcnt[:, 0:NT * NEXP], P.cst[:, 3, :], m2, True, True), [P.bcst, bmsk], [bpcnt])
        csb = P.sb([128, NT, NEXP], F32, stack=st)
        bcsb = Buf()
        act.op(act_fn(csb[:].rearrange("p t e -> p (t e)"), pcnt[:, 0:NT * NEXP], AF.Copy), [bpcnt], [bcsb])
        off = P.sb([128, NT, NEXP], F32, stack=st)
        boff = Buf()
        dve.op(lambda e: e.memset(off[:, 0, :], 0.0), [], [boff])
        for i in range(1, NT):
            dve.op(tt_fn(off[:, i, :], off[:, i - 1, :], csb[:, i - 1, :], ALU.add), [boff, bcsb], [boff])
        pos = P.sb([128, NT, NEXP], F32, stack=st)
        bpos = Buf()
        dve.op(tt_fn(pos[:].rearrange("p t e -> p (t e)"), ppos[:, 0:NT * NEXP],
                     off[:].rearrange("p t e -> p (t e)"), ALU.add), [bppos, boff], [bpos])
        ok = P.sb([128, NT, NEXP], F32, stack=st)
        bok = Buf()
        dve.op(ts_fn(ok[:], pos[:], CAP - 0.5, None, ALU.is_lt), [bpos], [bok])
        dve.op(tt_fn(msk[:], msk[:], ok[:], ALU.mult), [bmsk, bok], [bmsk])
        dve.op(tt_fn(gate[:], msk[:], ao[:], ALU.mult), [bmsk, bao], [bgate])
        ebase = P.cst[:, 5, 0:NEXP]
        eb_bc = bass.AP(ebase.tensor, ebase.offset, [list(ebase.ap[0]), [0, NT], [1, NEXP]])
        dve.op(tt_fn(pos[:], pos[:], eb_bc, ALU.add), [bpos, P.bcst], [bpos])
        dve.op(ts_fn(pos[:], pos[:], -BIGIDX, None, ALU.add), [bpos], [bpos])
        dve.op(tt_fn(pos[:], pos[:], msk[:], ALU.mult), [bpos, bmsk], [bpos])
        dve.op(ts_fn(pos[:], pos[:], BIGIDX, None, ALU.add), [bpos], [bpos])
        dve.op(lambda e: e.tensor_copy(out=idx[:], in_=pos[:]), [bpos], [bidx])
        P.barrier()

    def indirect(out, out_off, in_, in_off, reads, writes):
        s_ = pool
        if s_.dsems is None:
            s_.dsems = [P.newsem("d%d_%s" % (i, s_.name)) for i in range(s_.nslots)]
        j = s_.dn
        slot = j % s_.nslots
        key = (s_, slot)
        prev = 16 * (j // s_.nslots)
        if prev > 0:
            s_._wait((key, s_.dsems[slot], prev))
        s_._deps(reads, writes, True)
        ins = s_.eng.indirect_dma_start(out=out, out_offset=out_off, in_=in_, in_offset=in_off,
                                        bounds_check=P.bcreg, oob_is_err=False)
        ins.then_inc(s_.dsems[slot], 16)
        s_.dn += 1
        s_._mark((key, s_.dsems[slot], prev + 16), reads, writes)

    with contextlib.ExitStack() as st:
        acc = P.sb([128, NT, D], F32, stack=st)
        bacc = [Buf() for _ in range(NT)]
        bXc = [Buf() for _ in range(NEXP)]
        bYc = [Buf() for _ in range(NEXP)]
        small = Ring(P, 8, [128, 1], F32, st)
        with contextlib.ExitStack() as st1:
            junk = Ring(P, 1, [128, D], F32, st1)
            hnr = Ring(P, 3, [128, D], BF16, st1)
            for t in range(NT):
                sp.dma(out=acc[:, t, :], in_=x1[t * 128:(t + 1) * 128, :], writes=[bacc[t]])
                rs, br = rms_rstd(P, acc[:, t, :], bacc[t], D, junk, small)
                hn, bhn = hnr.next()
                dve.op(stt_fn(hn[:], acc[:, t, :], rs[:, 0:1], gfb[:], ALU.mult, ALU.mult), [bacc[t], br, bgfb], [bhn])
                for e in range(NEXP):
                    indirect(Xc[:, :], bass.IndirectOffsetOnAxis(ap=idx[:, t, e:e + 1], axis=0), hn[:, :], None,
                             [bhn, bidx], [bXc[e]])
            P.barrier()
        wgr = Ring(P, 2, [128, 8, D], BF16, st)
        wur = Ring(P, 2, [128, 8, D], BF16, st)
        wdr = Ring(P, 1, [128, 8, D], BF16, st)
        xsr = Ring(P, 1, [128, 4, D], BF16, st)
        xTr = Ring(P, 1, [128, 8, CAP], BF16, st)
        atr = Ring(P, 1, [128, 8, CAP], BF16, st)
        ysr = Ring(P, 1, [128, 4, D], BF16, st)
        sgr = Ring(P, 1, [128, 512], F32, st)
        ybr = Ring(P, 3, [128, D], BF16, st)
        ptr = Ring(P, 2, [128, 8, 128], BF16, st, psum=True)
        pgr = Ring(P, 2, [128, 512], F32, st, psum=True)
        pur = Ring(P, 2, [128, 512], F32, st, psum=True)
        pyr = Ring(P, 2, [128, 512], F32, st, psum=True)
        for yb_, byb_ in zip(ybr.t, ybr.b):
            dve.op(lambda e, yb_=yb_: e.memset(yb_[:], 0.0), [], [byb_])

        def load_w(e, which=((0, 1, 2))):
            wts = []
            for src, ring in [((wg, wgr), (wu, wur), (wd, wdr))[i_] for i_ in which]:
                w, bw = ring.next()
                sv = src[e].rearrange("(k p) f -> p k f", p=128)
                pool.dma(out=w[:, 0:4, :], in_=sv[:, 0:4, :], writes=[bw])
                pool.dma(out=w[:, 4:8, :], in_=sv[:, 4:8, :], writes=[bw])
                wts.append((w, bw))
            return wts

        def gather_back(e):
            for t in range(NT):
                yb, byb = ybr.next()
                indirect(yb[:, :], None, Yc[:, :], bass.IndirectOffsetOnAxis(ap=idx[:, t, e:e + 1], axis=0),
                         [bYc[e], bidx], [byb])
                dve.op(stt_fn(acc[:, t, :], yb[:], gate[:, t, e:e + 1], acc[:, t, :], ALU.mult, ALU.add),
                       [byb, bgate, bacc[t]], [bacc[t]])

        nxt = load_w(0, (0, 1))
        for e in range(NEXP):
            (Wg, bWg), (Wu, bWu) = nxt
            ((Wd, bWd),) = load_w(e, (2,))
            if e + 1 < NEXP:
                nxt = load_w(e + 1, (0, 1))
            xs, bxs = xsr.next()
            sp.dma(out=xs[:], in_=Xc[e * CAP:(e + 1) * CAP, :].rearrange("(s p) d -> p s d", p=128),
                   reads=[bXc[e]], writes=[bxs])
            xT, bxT = xTr.next()
            for sl in range(4):
                pt, bpt = ptr.next()
                for k in range(8):
                    pe.op(lambda en, pt=pt, k=k, xs=xs, sl=sl: en.transpose(pt[:, k, :], xs[:, sl, k * 128:(k + 1) * 128],
                                                                           P.identb[:]), [bxs, P.bidb], [bpt])
                act.op(act_fn(xT[:, :, sl * 128:(sl + 1) * 128], pt[:], AF.Copy), [bpt], [bxT])
            AT, bAT = atr.next()
            for fc in range(8):
                pg, bpg = pgr.next()
                pu, bpu = pur.next()
                for k in range(8):
                    pe.op(mm_fn(pg[:], Wg[:, k, fc * 128:(fc + 1) * 128], xT[:, k, :], k == 0, k == 7), [bWg, bxT], [bpg])
                for k in range(8):
                    pe.op(mm_fn(pu[:], Wu[:, k, fc * 128:(fc + 1) * 128], xT[:, k, :], k == 0, k == 7), [bWu, bxT], [bpu])
                sg, bsg = sgr.next()
                act.op(act_fn(sg[:], pg[:], AF.Silu), [bpg], [bsg])
                dve.op(tt_fn(AT[:, fc, :], sg[:], pu[:], ALU.mult), [bsg, bpu], [bAT])
            ys, bys = ysr.next()
            for sl in range(4):
                for hc in range(2):
                    py, bpy = pyr.next()
                    for fc in range(8):
                        pe.op(mm_fn(py[:], AT[:, fc, sl * 128:(sl + 1) * 128], Wd[:, fc, hc * 512:(hc + 1) * 512],
                                    fc == 0, fc == 7), [bAT, bWd], [bpy])
                    act.op(act_fn(ys[:, sl, hc * 512:(hc + 1) * 512], py[:], AF.Copy), [bpy], [bys])
            sp.dma(out=Yc[e * CAP:(e + 1) * CAP, :].rearrange("(s p) d -> p s d", p=128), in_=ys[:],
                   reads=[bys], writes=[bYc[e]])
            if e >= 1:
                gather_back(e - 1)
        gather_back(NEXP - 1)
        hor = Ring(P, 1, [128, D], F32, st)
        junk = hor
        for t in range(NT):
            sp.dma(out=x2_out[t * 128:(t + 1) * 128, :], in_=acc[:, t, :], reads=[bacc[t]])
            if gn is not None:
                rs, br = rms_rstd(P, acc[:, t, :], bacc[t], D, junk, small)
                ho, bho = hor.next()
                dve.op(stt_fn(ho[:], acc[:, t, :], rs[:, 0:1], gnb[:], ALU.mult, ALU.mult),
                       [bacc[t], br, bgnb], [bho])
                sp.dma(out=hn_out[t * 128:(t + 1) * 128, :], in_=ho[:], reads=[bho])
```

```python
import numpy as np
import ml_dtypes
import contextlib
import concourse.bass as bass
import concourse.mybir as mybir
from concourse.bass_utils import run_bass_kernel_spmd

F32 = mybir.dt.float32
BF16 = mybir.dt.bfloat16
ALU = mybir.AluOpType
AF = mybir.ActivationFunctionType
AX = mybir.AxisListType

NCORES = 8
D = 1024
SEQ = 4096
OWN = 2048
WIN = 3072
EPS = 1e-6
DEBUG = False
SPARSE = True
SCAN_SELFSYNC = False


class Buf:
    __slots__ = ("name", "w", "r")

    def __init__(self, name=""):
        self.name = name
        self.w = None
        self.r = {}


class Stream:
    def __init__(self, P, name, eng):
        self.P = P
        self.name = name
        self.eng = eng
        self.sem = P.newsem("c_" + name)
        self.cnt = 0
        self.seen = {}
        self.nslots = 8
        self.dsems = None
        self.dn = 0
        self.selfsync = name != "pe"

    def _wait(self, tok):
        key, sem, val = tok
        if self.seen.get(key, 0) < val:
            self.eng.wait_ge(sem, val)
            self.seen[key] = val

    def _deps(self, reads, writes, dma):
        toks = []
        for b in reads:
            if b.w is not None:
                toks.append(b.w)
        for b in writes:
            if b.w is not None:
                toks.append(b.w)
            toks.extend(b.r.values())
        for t in toks:
            if dma or t[0] is not self or self.selfsync:
                self._wait(t)

    def _mark(self, tok, reads, writes):
        for b in reads:
            b.r[tok[0]] = tok
        for b in writes:
            b.w = tok
            b.r = {}

    def op(self, fn, reads=(), writes=()):
        if self.P.stopped:
            return None
        self._deps(reads, writes, False)
        ins = fn(self.eng)
        self.cnt += 1
        ins.then_inc(self.sem, 1)
        self._mark((self, self.sem, self.cnt), reads, writes)
        return ins

    def dma(self, out, in_, reads=(), writes=(), **kw):
        if self.P.stopped:
            return None
        if self.dsems is None:
            self.dsems = [self.P.newsem("d%d_%s" % (i, self.name)) for i in range(self.nslots)]
        j = self.dn
        slot = j % self.nslots
        key = (self, slot)
        prev = 16 * (j // self.nslots)
        if prev > 0:
            self._wait((key, self.dsems[slot], prev))
        self._deps(reads, writes, True)
        ins = self.eng.dma_start(out=out, in_=in_, **kw)
        ins.then_inc(self.dsems[slot], 16)
        self.dn += 1
        self._mark((key, self.dsems[slot], prev + 16), reads, writes)
        return ins


class Prog:
    def __init__(self):
        self.nc = bass.Bass("TRN2", target_bir_lowering=False)
        self.stack = contextlib.ExitStack()
        self._n = 0
        self.stopped = False
        self.cur = None
        self.ccsem = None
        self.ccn = 0
        self.ncores = NCORES
        self.bcreg = None
        nc = self.nc
        self.pe = Stream(self, "pe", nc.tensor)
        self.act = Stream(self, "act", nc.scalar)
        self.dve = Stream(self, "dve", nc.vector)
        self.pool = Stream(self, "pool", nc.gpsimd)
        self.sp = Stream(self, "sp", nc.sync)
        self.streams = [self.pe, self.act, self.dve, self.pool, self.sp]

    def newsem(self, name):
        return self.stack.enter_context(self.nc.semaphore(name))

    def inp(self, name, shape, dt=F32):
        return self.nc.dram_tensor(name, list(shape), dt, kind="ExternalInput").ap()

    def outp(self, name, shape, dt=F32):
        return self.nc.dram_tensor(name, list(shape), dt, kind="ExternalOutput").ap()

    def sb(self, shape, dt=F32, name=None, stack=None):
        self._n += 1
        name = "s%d_%s" % (self._n, name or "t")
        return (stack or self.cur or self.stack).enter_context(self.nc.sbuf_tensor(name, list(shape), dt))

    def ps(self, shape, dt=F32, name=None, stack=None):
        self._n += 1
        name = "p%d_%s" % (self._n, name or "t")
        return (stack or self.cur or self.stack).enter_context(self.nc.psum_tensor(name, list(shape), dt))

    def barrier(self):
        toks = []
        for s in self.streams:
            if s.cnt:
                toks.append((s, s.sem, s.cnt))
            if s.dsems is not None:
                for slot in range(s.nslots):
                    n = (s.dn - slot + s.nslots - 1) // s.nslots
                    if n > 0:
                        toks.append(((s, slot), s.dsems[slot], 16 * n))
        for s in self.streams:
            for t in toks:
                if t[0] is not s:
                    s._wait(t)

    def dram(self, name, shape, dt=F32):
        return self.nc.dram_tensor(name, list(shape), dt)

    def all_gather_pair(self, src, dst):
        if self.ccsem is None:
            self.ccsem = self.newsem("ccsem")
            self.ccdummy = self.sb([128, 1], F32, name="ccdummy", stack=self.stack)
            self.bccd = Buf()
        self.barrier()
        groups = [[2 * i, 2 * i + 1] for i in range(self.ncores // 2)]
        self.ccn += 1
        n = self.ccn

        def fn(e):
            e.collective_compute("AllGather", ALU.bypass, replica_groups=groups,
                                 ins=[src.ap().opt()], outs=[dst.ap().opt()]).then_inc(self.ccsem, 1)
            e.wait_ge(self.ccsem, n)
            return e.memset(self.ccdummy[:], 0.0)
        self.pool.op(fn, [], [self.bccd])
        self.barrier()

    def finish(self):
        self.barrier()
        self.stack.close()
        return self.nc


def bcast_rows(ap, nparts):
    n = ap.shape[-1]
    return bass.AP(ap.tensor, ap.offset, [[0, nparts], [1, n]])


class Ring:
    def __init__(self, P, n, shape, dt, stack, psum=False):
        alloc = P.ps if psum else P.sb
        self.t = [alloc(shape, dt, stack=stack) for _ in range(n)]
        self.b = [Buf() for _ in range(n)]
        self.i = -1

    def next(self):
        self.i = (self.i + 1) % len(self.t)
        return self.t[self.i], self.b[self.i]


def act_fn(out, in_, func, **kw):
    return lambda e: e.activation(out=out, in_=in_, func=func, **kw)


def mm_fn(out, lhsT, rhs, start, stop):
    return lambda e: e.matmul(out, lhsT, rhs, start=start, stop=stop)


def tt_fn(out, in0, in1, op):
    return lambda e: e.tensor_tensor(out=out, in0=in0, in1=in1, op=op)


def ts_fn(out, in0, s1, s2, op0, op1=None):
    if op1 is None:
        return lambda e: e.tensor_scalar(out=out, in0=in0, scalar1=s1, scalar2=None, op0=op0)
    return lambda e: e.tensor_scalar(out=out, in0=in0, scalar1=s1, scalar2=s2, op0=op0, op1=op1)


def stt_fn(out, in0, scalar, in1, op0, op1):
    return lambda e: e.scalar_tensor_tensor(out=out, in0=in0, scalar=scalar, in1=in1, op0=op0, op1=op1)


ATT_CFG = (1, 4, 16)


def vtile_list():
    tiles = []
    for ci, d in enumerate(ATT_CFG):
        nb = OWN // (128 * d)
        for c in range(d):
            for j in range(nb + 1):
                i0 = 0 if j == 0 else 128 * j - 64
                tiles.append((ci, d, c, j, c + d * i0))
    return tiles


def rms_rstd(P, x_ap, xb, n, ring_junk, ring_small, pbuf_extra=()):
    junk, bj = ring_junk.next()
    ss, bs = ring_small.next()
    P.act.op(act_fn(junk[:, 0:n], x_ap, AF.Square, accum_out=ss[:, 0:1]), [xb], [bj, bs])
    sd, bd = ring_small.next()
    P.act.op(act_fn(sd[:, 0:1], ss[:, 0:1], AF.Sqrt, scale=1.0 / n, bias=P.eps_t[:, 0:1]), [bs, P.beps], [bd])
    rs, br = ring_small.next()
    P.dve.op(lambda e: e.reciprocal(out=rs[:, 0:1], in_=sd[:, 0:1]), [bd], [br])
    return rs, br


def load_consts(P, cst_in):
    cst = P.sb([128, 6, 128], F32, name="cst")
    P.bcst = Buf("cst")
    P.sp.dma(out=cst[:], in_=cst_in, writes=[P.bcst])
    P.identf = cst[:, 0, :]
    P.ones512 = cst[:, 1, :]
    P.ones64 = cst[:, 2, :]
    P.cst = cst
    idb = P.sb([128, 128], BF16, name="identb")
    P.bidb = Buf("identb")
    P.dve.op(lambda e: e.tensor_copy(out=idb[:], in_=cst[:, 0, :]), [P.bcst], [P.bidb])
    P.identb = idb
    eps_t = P.sb([128, 1], F32, name="eps")
    P.beps = Buf("eps")
    P.dve.op(lambda e: e.memset(eps_t[:], EPS), [], [P.beps])
    P.eps_t = eps_t


def router_tile(P, x1t, bx1, gfb, bgfb, wr, bwr, brb, bbrb, rings, aff_out, affT_out, tt, hf_out=None):
    rs, br = rms_rstd(P, x1t[:], bx1, D, rings["junk"], rings["small"])
    hf, bhf = rings["hf"].next()
    P.dve.op(stt_fn(hf[:], x1t[:], rs[:, 0:1], gfb[:], ALU.mult, ALU.mult), [bx1, br, bgfb], [bhf])
    ptf, bptf = rings["ptf"].next()
    for k in range(8):
        P.pe.op(lambda e, k=k: e.transpose(ptf[:, k, :], hf[:, k * 128:(k + 1) * 128], P.identf),
                [bhf, P.bcst], [bptf])
    hfT, bhfT = rings["hfT"].next()
    P.act.op(act_fn(hfT[:], ptf[:], AF.Copy), [bptf], [bhfT])
    plog, bplog = rings["plog"].next()
    for k in range(8):
        P.pe.op(mm_fn(plog[:, 0:16], hfT[:, k, :], wr[:, k, :], k == 0, k == 7), [bhfT, bwr], [bplog])
    lg, blg = rings["sm16"].next()
    P.dve.op(tt_fn(lg[:], plog[:, 0:16], brb[:], ALU.add), [bplog, bbrb], [blg])
    mx, bmx = rings["small"].next()
    P.dve.op(lambda e: e.reduce_max(out=mx[:, 0:1], in_=lg[:], axis=AX.X, negate=True), [blg], [bmx])
    ex, bex = rings["sm16"].next()
    sm, bsm = rings["small"].next()
    P.act.op(act_fn(ex[:], lg[:], AF.Exp, bias=mx[:, 0:1], scale=1.0, accum_out=sm[:, 0:1]), [blg, bmx], [bex, bsm])
    rsm, brsm = rings["small"].next()
    P.dve.op(lambda e: e.reciprocal(out=rsm[:, 0:1], in_=sm[:, 0:1]), [bsm], [brsm])
    af, baf = rings["sm16"].next()
    P.dve.op(ts_fn(af[:], ex[:], rsm[:, 0:1], None, ALU.mult), [bex, brsm], [baf])
    P.sp.dma(out=aff_out[tt * 128:(tt + 1) * 128, :], in_=af[:], reads=[baf])
    pat, bpat = rings["pat"].next()
    P.pe.op(lambda e: e.transpose(pat[0:16, 0:128], af[:], P.identf), [baf, P.bcst], [bpat])
    aT, baT = rings["aT"].next()
    P.act.op(act_fn(aT[:], pat[0:16, 0:128], AF.Copy), [bpat], [baT])
    P.sp.dma(out=affT_out[:, tt * 128:(tt + 1) * 128], in_=aT[:], reads=[baT])
    return hf, bhf


def emit_phaseA(P, io):
    xw, g0, w_in, cvec, qk_g, w_out = io["xw"], io["g0"], io["w_in"], io["cvec"], io["qk_g"], io["w_out"]
    gf, w_r, b_r, eb = io["gf"], io["w_r"], io["b_r"], io["eb"]
    x1_out, aff_out, affT_out = io["x1_out"], io["aff_out"], io["affT_out"]
    dbg = None
    with contextlib.ExitStack() as ph:
        P.cur = ph
        _phaseA_body(P, xw, g0, w_in, cvec, qk_g, w_out, gf, w_r, b_r, eb, x1_out, aff_out, affT_out)
        P.barrier()
    P.cur = None


def _phaseA_body(P, xw, g0, w_in, cvec, qk_g, w_out, gf, w_r, b_r, eb, x1_out, aff_out, affT_out):
    w_in_v = w_in.rearrange("(k p) e -> p k e", p=128)
    w_out_v = w_out.rearrange("(k p) e -> p k e", p=128)
    w_r_v = w_r.rearrange("(k p) e -> p k e", p=128)

    hT = P.sb([128, 8, WIN], BF16, name="hT")
    bhT = [Buf("hT%d" % i) for i in range(WIN // 128)]
    mixT = P.sb([128, 8, OWN], BF16, name="mixT")
    bmix = [Buf("mix%d" % i) for i in range(8)]
    gb = P.sb([128, D], F32, name="gb")
    bgb = Buf("gb")
    P.sp.dma(out=gb[:], in_=bcast_rows(g0, 128), writes=[bgb])
    cv = P.sb([128, 4, 34], F32, name="cv")
    bcv = Buf("cv")
    P.sp.dma(out=cv[:], in_=cvec, writes=[bcv])
    qkg = P.sb([128, 2], F32, name="qkg")
    bqkg = Buf("qkg")
    P.sp.dma(out=qkg[:], in_=qk_g, writes=[bqkg])

    with contextlib.ExitStack() as st:
        xr = Ring(P, 4, [128, D], F32, st)
        junk = Ring(P, 2, [128, D], F32, st)
        small = Ring(P, 16, [128, 1], F32, st)
        hnr = Ring(P, 4, [128, D], BF16, st)
        ptr = Ring(P, 4, [128, 8, 128], BF16, st, psum=True)
        for tt in range(WIN // 128):
            xt, bx = xr.next()
            P.sp.dma(out=xt[:], in_=xw[tt * 128:(tt + 1) * 128, :], writes=[bx])
            rs, br = rms_rstd(P, xt[:], bx, D, junk, small)
            hn, bhn = hnr.next()
            P.dve.op(stt_fn(hn[:], xt[:], rs[:, 0:1], gb[:], ALU.mult, ALU.mult), [bx, br, bgb], [bhn])
            pt, bpt = ptr.next()
            for k in range(8):
                P.pe.op(lambda e, k=k: e.transpose(pt[:, k, :], hn[:, k * 128:(k + 1) * 128], P.identb[:]),
                        [bhn, P.bidb], [bpt])
            P.act.op(act_fn(hT[:, :, tt * 128:(tt + 1) * 128], pt[:], AF.Copy), [bpt], [bhT[tt]])
        P.barrier()

    def hT_bufs(t0, n):
        return bhT[t0 // 128:(t0 + n + 127) // 128]

    NCV = OWN + 128
    with contextlib.ExitStack() as st:
        hglu = P.sb([128, 4, 15 + NCV], BF16, stack=st)
        bhg = [Buf() for _ in range(4)]
        diag = P.sb([128, 4, 31, 128], BF16, stack=st)
        bdiag = [Buf() for _ in range(4)]
        wcv = Ring(P, 2, [128, 8, 256], BF16, st)
        pmr = Ring(P, 2, [128, 512], F32, st, psum=True)
        pgr = Ring(P, 2, [128, 512], F32, st, psum=True)
        pst = Ring(P, 2, [128, 512], F32, st, psum=True)
        sgr = Ring(P, 2, [128, 512], F32, st)
        vbuf = [Ring(P, 2, [128, 512], F32, st) for _ in range(4)]
        sqr = [Ring(P, 2, [128, 512], F32, st) for _ in range(4)]
        tmp = Ring(P, 10, [128, 512], F32, st)
        zr = Ring(P, 2, [128, 512], F32, st)
        for cc in range(4):
            P.dve.op(lambda e, cc=cc: e.memset(hglu[:, cc, 0:15], 0.0), [], [bhg[cc]])
            for k in range(31):
                P.pool.op(ts_fn(diag[:, cc, k, :], P.identf, cv[:, cc, k:k + 1], None, ALU.mult),
                          [P.bcst, bcv], [bdiag[cc]])
        ntiles = [(i * 512, 512) for i in range(4)] + [(2048, 128)]
        for cc in range(4):
            w, bw = wcv.next()
            P.pool.dma(out=w[:, :, 0:128], in_=w_in_v[:, :, cc * 128:(cc + 1) * 128], writes=[bw])
            P.pool.dma(out=w[:, :, 128:256], in_=w_in_v[:, :, 512 + cc * 128:512 + (cc + 1) * 128], writes=[bw])
            for (t0, n) in ntiles:
                pm, bpm = pmr.next()
                pg, bpg = pgr.next()
                for k in range(8):
                    P.pe.op(mm_fn(pm[:, 0:n], w[:, k, 0:128], hT[:, k, t0:t0 + n], k == 0, k == 7),
                            [bw] + hT_bufs(t0, n), [bpm])
                for k in range(8):
                    P.pe.op(mm_fn(pg[:, 0:n], w[:, k, 128:256], hT[:, k, t0:t0 + n], k == 0, k == 7),
                            [bw] + hT_bufs(t0, n), [bpg])
                sg, bsg = sgr.next()
                P.act.op(act_fn(sg[:, 0:n], pg[:, 0:n], AF.Sigmoid), [bpg], [bsg])
                P.dve.op(tt_fn(hglu[:, cc, 15 + t0:15 + t0 + n], pm[:, 0:n], sg[:, 0:n], ALU.mult),
                         [bpm, bsg], [bhg[cc]])
        for nt in range(4):
            t0 = nt * 512
            vb = []
            for cc in range(4):
                pm, bpm = pmr.next()
                for k in range(31):
                    P.pe.op(mm_fn(pm[:], diag[:, cc, k, :], hglu[:, cc, t0 + k:t0 + k + 512], k == 0, k == 30),
                            [bdiag[cc], bhg[cc]], [bpm])
                v, bv = vbuf[cc].next()
                sq, bsq = sqr[cc].next()
                P.act.op(act_fn(v[:], pm[:], AF.Identity, bias=cv[:, cc, 31:32], scale=1.0), [bpm, bcv], [bv])
                P.act.op(act_fn(sq[:], pm[:], AF.Square, bias=cv[:, cc, 31:32], scale=1.0), [bpm, bcv], [bsq])
                vb.append((v, bv, sq, bsq))
            pmean, bpmean = pst.next()
            pex2, bpex2 = pst.next()
            for cc in range(4):
                P.pe.op(mm_fn(pmean[:], P.ones512, vb[cc][0][:], cc == 0, cc == 3), [P.bcst, vb[cc][1]], [bpmean])
            for cc in range(4):
                P.pe.op(mm_fn(pex2[:], P.ones512, vb[cc][2][:], cc == 0, cc == 3), [P.bcst, vb[cc][3]], [bpex2])
            mean, bmean = tmp.next()
            P.act.op(act_fn(mean[:], pmean[:], AF.Copy), [bpmean], [bmean])
            m2, bm2 = tmp.next()
            P.dve.op(tt_fn(m2[:], mean[:], mean[:], ALU.mult), [bmean], [bm2])
            var, bvar = tmp.next()
            P.dve.op(tt_fn(var[:], pex2[:], m2[:], ALU.subtract), [bpex2, bm2], [bvar])
            sd, bsd = tmp.next()
            P.act.op(act_fn(sd[:], var[:], AF.Sqrt, bias=P.eps_t[:, 0:1], scale=1.0), [bvar, P.beps], [bsd])
            rstd, brstd = tmp.next()
            P.dve.op(lambda e: e.reciprocal(out=rstd[:], in_=sd[:]), [bsd], [brstd])
            for cc in range(4):
                v, bv, sq, bsq = vb[cc]
                z, bz = zr.next()
                P.dve.op(tt_fn(z[:], v[:], mean[:], ALU.subtract), [bv, bmean], [bz])
                P.dve.op(tt_fn(z[:], z[:], rstd[:], ALU.mult), [bz, brstd], [bz])
                P.act.op(act_fn(mixT[:, cc, t0:t0 + 512], z[:], AF.Silu, scale=cv[:, cc, 32:33],
                                bias=cv[:, cc, 33:34]), [bz, bcv], [bmix[cc]])
        P.barrier()

    tiles = vtile_list()
    NT = len(tiles)
    tindex = {(ci, c, j): i for i, (ci, d, c, j, s) in enumerate(tiles)}
    with contextlib.ExitStack() as st:
        wq = Ring(P, 2, [128, 8, 384], BF16, st)
        ebr = Ring(P, 1, [128, 6, 384], F32, st)
        qT = P.sb([128, OWN], BF16, stack=st)
        bqT = Buf()
        kT = P.sb([128, WIN], BF16, stack=st)
        bkT = Buf()
        vT = P.sb([128, WIN], BF16, stack=st)
        bvT = Buf()
        Vt = P.sb([128, NT, 256], BF16, stack=st)
        bVt = [Buf() for _ in range(NT)]
        bones = Buf()
        acc = P.sb([128, 2, OWN], F32, stack=st)
        bacc = [Buf(), Buf()]
        pmr = Ring(P, 2, [128, 512], F32, st, psum=True)
        pst = Ring(P, 1, [128, 512], F32, st, psum=True)
        psr = Ring(P, 2, [128, 512], F32, st, psum=True)
        ptvr = Ring(P, 1, [128, 1024], BF16, st, psum=True)
        por = Ring(P, 2, [128, 512], F32, st, psum=True)
        qfr = Ring(P, 2, [128, 512], F32, st)
        sqr = Ring(P, 2, [128, 512], F32, st)
        tmp = Ring(P, 4, [128, 512], F32, st)
        pex = Ring(P, 3, [128, 256], F32, st)
        pTr = Ring(P, 4, [128, 256], BF16, st)
        rzr = Ring(P, 1, [64, OWN], F32, st)
        sring = Ring.__new__(Ring)
        sring.t = psr.t + pmr.t
        sring.b = psr.b + pmr.b
        sring.i = -1
        Vt4 = Vt[:].rearrange("p t (s c) -> p t s c", s=4)
        P.pool.op(lambda e: e.memset(Vt4[:, :, 1, :], 1.0), [], [bones])
        P.pool.op(lambda e: e.memset(Vt4[:, :, 3, :], 1.0), [], [bones])
        ps_slot = [0]
        po_slot = [0]
        pv_slot = [0]
        psb = [[Buf(), Buf()], [Buf(), Buf()]]
        pob = [[Buf() for _ in range(4)] for _ in range(2)]
        pvb = [Buf() for _ in range(4)]
        for hp in range(4):
            w, bw = wq.next()
            for i, base in enumerate((1024, 1536, 2048)):
                P.pool.dma(out=w[:, :, i * 128:(i + 1) * 128],
                           in_=w_in_v[:, :, base + hp * 128:base + (hp + 1) * 128], writes=[bw])
            ebt, bebt = ebr.next()
            P.sp.dma(out=ebt[:], in_=eb[hp], writes=[bebt])
            for which, ntok, dst, bdst, gcol in ((0, OWN, qT, bqT, 0), (1, WIN, kT, bkT, 1)):
                for nt in range(ntok // 512):
                    t0 = nt * 512
                    pm, bpm = pmr.next()
                    for k in range(8):
                        P.pe.op(mm_fn(pm[:], w[:, k, which * 128:(which + 1) * 128], hT[:, k, t0:t0 + 512],
                                      k == 0, k == 7), [bw] + hT_bufs(t0, 512), [bpm])
                    qf, bqf = qfr.next()
                    sq, bsq = sqr.next()
                    P.act.op(act_fn(qf[:], pm[:], AF.Copy), [bpm], [bqf])
                    P.act.op(act_fn(sq[:], pm[:], AF.Square), [bpm], [bsq])
                    pms, bpms = pst.next()
                    P.pe.op(mm_fn(pms[:], P.ones64, sq[:], True, True), [P.bcst, bsq], [bpms])
                    sd, bsd = tmp.next()
                    P.act.op(act_fn(sd[:], pms[:], AF.Ln, bias=P.eps_t[:, 0:1], scale=1.0), [bpms, P.beps], [bsd])
                    rstd, brstd = tmp.next()
                    P.act.op(act_fn(rstd[:], sd[:], AF.Exp, scale=-0.5), [bsd], [brstd])
                    P.dve.op(tt_fn(qf[:], qf[:], rstd[:], ALU.mult), [bqf, brstd], [bqf])
                    P.dve.op(ts_fn(dst[:, t0:t0 + 512], qf[:], qkg[:, gcol:gcol + 1],
                                   0.125 if which == 0 else 1.0, ALU.mult, ALU.mult), [bqf, bqkg], [bdst])
            for nt in range(WIN // 512):
                t0 = nt * 512
                pm, bpm = pmr.next()
                for k in range(8):
                    P.pe.op(mm_fn(pm[:], w[:, k, 256:384], hT[:, k, t0:t0 + 512], k == 0, k == 7),
                            [bw] + hT_bufs(t0, 512), [bpm])
                P.act.op(act_fn(vT[:, t0:t0 + 512], pm[:], AF.Copy), [bpm], [bvT])
            for ti, (ci, d, c, j, s0) in enumerate(tiles):
                pv, bpv = ptvr.next()
                P.pe.op(lambda e, pv=pv, s0=s0, d=d: e.transpose(pv[:, 0:128], vT[:, s0:s0 + 127 * d + 1:d], P.identb[:]),
                        [bvT, P.bidb], [bpv])
                P.act.op(act_fn(Vt4[:, ti, 0:4:2, :], pv[:, 0:128].rearrange("p (s c) -> p s c", s=2), AF.Copy),
                         [bpv, bones], [bVt[ti]])
            for hh in range(2):
                r0 = 64 * hh
                for ci, d in enumerate(ATT_CFG):
                    nb = OWN // (128 * d)
                    for c in range(d):
                        pts = []
                        for j in range(nb + 1):
                            i0 = 0 if j == 0 else 128 * j - 64
                            ks = c + d * i0
                            jlo, jhi = max(j - 1, 0), min(j, nb - 1)
                            nq = 128 * (jhi - jlo + 1)
                            q0 = c + d * 128 * jlo
                            pst_, pbuf = sring.next()
                            sl = pst_[:, 0:nq]
                            P.pe.op(mm_fn(sl, kT[r0:r0 + 64, ks:ks + 127 * d + 1:d],
                                          qT[r0:r0 + 64, q0:q0 + (nq - 1) * d + 1:d], True, True), [bkT, bqT], [pbuf])
                            pe_, bpe = pex.next()
                            P.act.op(act_fn(pe_[:, 0:nq], sl, AF.Exp), [pbuf], [bpe])
                            if j == 0:
                                ebs = ebt[:, ci * 2 + hh, 256:384]
                            elif j == nb:
                                ebs = ebt[:, ci * 2 + hh, 0:128]
                            else:
                                ebs = ebt[:, ci * 2 + hh, 0:256]
                            pT, bpT = pTr.next()
                            P.dve.op(tt_fn(pT[:, 0:nq], pe_[:, 0:nq], ebs, ALU.mult), [bpe, bebt], [bpT])
                            pts.append((pT, bpT, nq))
                            if j >= 1:
                                jb = j - 1
                                pTa, bpTa, nqa = pts[jb]
                                a_off = 0 if jb == 0 else 128
                                po_, pobuf = por.next()
                                osl = po_[:, 0:128]
                                ta = tindex[(ci, c, jb)]
                                tb = tindex[(ci, c, j)]
                                P.pe.op(mm_fn(osl, Vt[:, ta, 128 * hh:128 * hh + 128], pTa[:, a_off:a_off + 128],
                                              True, False), [bVt[ta], bpTa], [pobuf])
                                P.pe.op(mm_fn(osl, Vt[:, tb, 128 * hh:128 * hh + 128], pT[:, 0:128],
                                              False, True), [bVt[tb], bpT], [pobuf])
                                a0 = c + d * 128 * jb
                                dsl = acc[:, hh, a0:a0 + 127 * d + 1:d]
                                if ci == 0:
                                    P.dve.op(lambda e, dsl=dsl, osl=osl: e.tensor_copy(out=dsl, in_=osl),
                                             [pobuf], [bacc[hh]])
                                else:
                                    P.dve.op(tt_fn(dsl, dsl, osl, ALU.add), [pobuf, bacc[hh]], [bacc[hh]])
                rz, brz = rzr.next()
                P.act.op(act_fn(rz[:], acc[64:128, hh, :], AF.Ln), [bacc[hh]], [brz])
                P.act.op(act_fn(rz[:], rz[:], AF.Exp, scale=-1.0), [brz], [brz])
                P.dve.op(tt_fn(mixT[r0:r0 + 64, 4 + hp, :], acc[0:64, hh, :], rz[:], ALU.mult),
                         [bacc[hh], brz], [bmix[4 + hp]])
        P.barrier()

    with contextlib.ExitStack() as st:
        wo = P.sb([128, 8, D], BF16, stack=st)
        bwo = Buf()
        for k in range(8):
            P.pool.dma(out=wo[:, k, :], in_=w_out_v[:, k, :], writes=[bwo])
        gfb = P.sb([128, D], F32, stack=st)
        bgfb = Buf()
        P.sp.dma(out=gfb[:], in_=bcast_rows(gf, 128), writes=[bgfb])
        wr = P.sb([128, 8, 16], F32, stack=st)
        bwr = Buf()
        P.sp.dma(out=wr[:], in_=w_r_v, writes=[bwr])
        brb = P.sb([128, 16], F32, stack=st)
        bbrb = Buf()
        P.sp.dma(out=brb[:], in_=bcast_rows(b_r, 128), writes=[bbrb])
        xr = Ring(P, 2, [128, D], F32, st)
        x1r = Ring(P, 2, [128, D], F32, st)
        pmr = Ring(P, 2, [128, 512], F32, st, psum=True)
        rings = {
            "junk": Ring(P, 1, [128, D], F32, st),
            "small": Ring(P, 12, [128, 1], F32, st),
            "hf": Ring(P, 2, [128, D], F32, st),
            "ptf": Ring(P, 1, [128, 8, 128], F32, st, psum=True),
            "hfT": Ring(P, 2, [128, 8, 128], F32, st),
            "plog": Ring(P, 2, [128, 512], F32, st, psum=True),
            "sm16": Ring(P, 6, [128, 16], F32, st),
            "pat": Ring(P, 1, [128, 512], F32, st, psum=True),
            "aT": Ring(P, 2, [16, 128], F32, st),
        }
        for tt in range(OWN // 128):
            xt, bx = xr.next()
            P.sp.dma(out=xt[:], in_=xw[tt * 128:(tt + 1) * 128, :], writes=[bx])
            x1t, bx1 = x1r.next()
            for half in range(2):
                pm, bpm = pmr.next()
                for k in range(8):
                    P.pe.op(mm_fn(pm[:], mixT[:, k, tt * 128:(tt + 1) * 128], wo[:, k, half * 512:(half + 1) * 512],
                                  k == 0, k == 7), [bmix[k], bwo], [bpm])
                P.dve.op(tt_fn(x1t[:, half * 512:(half + 1) * 512], pm[:], xt[:, half * 512:(half + 1) * 512],
                               ALU.add), [bpm, bx], [bx1])
            P.sp.dma(out=x1_out[tt * 128:(tt + 1) * 128, :], in_=x1t[:], reads=[bx1])
            router_tile(P, x1t, bx1, gfb, bgfb, wr, bwr, brb, bbrb, rings, aff_out, affT_out, tt)
    return


def host_consts():
    cst = np.zeros((128, 6, 128), np.float32)
    cst[:, 0, :] = np.eye(128, dtype=np.float32)
    cst[:, 1, :] = 1.0 / 512.0
    blk = np.zeros((128, 128), np.float32)
    blk[:64, :64] = 1.0 / 64.0
    blk[64:, 64:] = 1.0 / 64.0
    cst[:, 2, :] = blk
    cst[:, 3, :] = 1.0
    cst[:, 4, :] = np.triu(np.ones((128, 128), np.float32), 1)
    cst[:, 5, 0:16] = 512.0 * np.arange(16, dtype=np.float32)[None, :]
    return cst


def host_eb():
    p = np.arange(128)[:, None].astype(np.float64)
    f = np.arange(128)[None, :].astype(np.float64)
    eb = np.zeros((4, 128, 6, 384), np.float32)
    for head in range(8):
        slope = 2.0 ** (-(head + 1))
        hp, hh = head // 2, head % 2
        for ci, d in enumerate(ATT_CFG):
            B = np.where(p <= f, np.exp(-slope * d * np.abs(p + 64 - f)), 0.0)
            A = np.where(p >= f, np.exp(-slope * d * np.abs(p - 64 - f)), 0.0)
            A0 = np.where((p <= 63) & (p >= f - 64), np.exp(-slope * d * np.abs(p - f)), 0.0)
            eb[hp, :, ci * 2 + hh, 0:128] = B
            eb[hp, :, ci * 2 + hh, 128:256] = A
            eb[hp, :, ci * 2 + hh, 256:384] = A0
    return eb


def local_view(xb, h, n):
    if h == 0:
        return np.ascontiguousarray(xb[:n])
    return np.ascontiguousarray(xb[::-1][:n])


_CACHE = {}


def get_prog(name, builder):
    if name not in _CACHE:
        _CACHE[name] = builder()
    return _CACHE[name]


def run_phaseA(x, mix_norm_even, w_in, conv_w, conv_b, conv_ln_g, conv_ln_b, q_norm, k_norm, w_out,
               ffn_norm0, w_router0, b_router0):
    nc = get_prog("A", build_phaseA)
    cst = host_consts()
    eb = host_eb()
    in_maps = []
    for c in range(NCORES):
        b, h = c // 2, c % 2
        cw = conv_w[0] if h == 0 else conv_w[0][::-1]
        cvec = np.zeros((128, 4, 34), np.float32)
        cvec[:, :, 0:31] = cw.T.reshape(4, 128, 31).transpose(1, 0, 2)
        cvec[:, :, 31] = conv_b[0].reshape(4, 128).T
        cvec[:, :, 32] = conv_ln_g[0].reshape(4, 128).T
        cvec[:, :, 33] = conv_ln_b[0].reshape(4, 128).T
        qk = np.stack([np.tile(q_norm[0], 2), np.tile(k_norm[0], 2)], axis=1).astype(np.float32)
        in_maps.append({
            "xw": local_view(x[b], h, WIN),
            "g0": np.ascontiguousarray(mix_norm_even[0][None, :]),
            "w_in": np.ascontiguousarray(w_in[0]),
            "cvec": cvec,
            "qk_g": np.ascontiguousarray(qk),
            "w_out": np.ascontiguousarray(w_out[0]),
            "gf": np.ascontiguousarray(ffn_norm0[None, :]),
            "w_r": np.ascontiguousarray(w_router0),
            "b_r": np.ascontiguousarray(b_router0[None, :]),
            "eb": eb,
            "cst": cst,
        })
    res = run_bass_kernel_spmd(nc, in_maps, core_ids=list(range(NCORES)))
    return res.results


NEXP = 16
CAP = 512


def emit_phaseB(P, io):
    with contextlib.ExitStack() as ph:
        P.cur = ph
        _phaseB_body(P, io["x1"], io["affT_seq"], io["aff_own"], io["gf"], io["wg"], io["wu"], io["wd"],
                     io["x2_out"], io.get("gn"), io.get("hn_out"))
        P.barrier()
    P.cur = None


def _phaseB_body(P, x1, affT_seq, aff_own, gf, wg, wu, wd, x2_out, gn, hn_out):
    if gn is not None:
        gnb = P.sb([128, D], F32, name="gnb")
        bgnb = Buf()
        P.sp.dma(out=gnb[:], in_=bcast_rows(gn, 128), writes=[bgnb])
    gfb = P.sb([128, D], F32, name="gfb")
    bgfb = Buf()
    P.sp.dma(out=gfb[:], in_=bcast_rows(gf, 128), writes=[bgfb])
    gate = P.sb([128, OWN // 128, NEXP], F32, name="gate")
    bgate = Buf()
    pthr = P.ps([128, 512], F32, name="pthr")
    bpthr = Buf()

    with contextlib.ExitStack() as st:
        aT = P.sb([NEXP, SEQ], F32, stack=st)
        baT = Buf()
        P.sp.dma(out=aT[:].rearrange("e (r t) -> e r t", r=2), in_=affT_seq, writes=[baT])
        junk = P.sb([NEXP, SEQ], F32, stack=st)
        bjunk = Buf()
        lo = P.sb([NEXP, 1], F32, stack=st)
        blo = Buf()
        mid = P.sb([NEXP, 1], F32, stack=st)
        bmid = Buf()
        cnt = P.sb([NEXP, 1], F32, stack=st)
        bcnt = Buf()
        ge = P.sb([NEXP, 1], F32, stack=st)
        bge = Buf()
        P.dve.op(lambda e: e.memset(lo[:], 0.0), [], [blo])
        for it in range(36):
            wk = 2.0 ** (-(it + 1))
            P.dve.op(ts_fn(mid[:], lo[:], wk, None, ALU.add), [blo], [bmid])
            P.dve.op(lambda e: e.tensor_scalar(out=junk[:], in0=aT[:], scalar1=mid[:, 0:1], scalar2=0.0,
                                               op0=ALU.is_ge, op1=ALU.add, accum_out=cnt[:, 0:1]),
                     [baT, bmid], [bjunk, bcnt])
            P.dve.op(ts_fn(ge[:], cnt[:], CAP - 0.5, None, ALU.is_ge), [bcnt], [bge])
            P.dve.op(stt_fn(lo[:], ge[:], wk, lo[:], ALU.mult, ALU.add), [bge, blo], [blo])
        dthr = P.sb([NEXP, NEXP], F32, stack=st)
        bdthr = Buf()
        P.dve.op(ts_fn(dthr[:], P.cst[0:NEXP, 0, 0:NEXP], lo[:, 0:1], None, ALU.mult), [P.bcst, blo], [bdthr])
        P.pe.op(mm_fn(pthr[:, 0:NEXP], P.cst[0:NEXP, 3, :], dthr[:], True, True), [P.bcst, bdthr], [bpthr])
        thrb = P.sb([128, NEXP], F32, stack=st)
        bthrb = Buf()
        P.act.op(act_fn(thrb[:], pthr[:, 0:NEXP], AF.Copy), [bpthr], [bthrb])
        ao = P.sb([128, OWN // 128, NEXP], F32, stack=st)
        bao = Buf()
        P.sp.dma(out=ao[:], in_=aff_own.rearrange("(t p) e -> p t e", p=128), writes=[bao])
        msk = P.sb([128, OWN // 128, NEXP], F32, stack=st)
        bmsk = Buf()
        thr_bc = bass.AP(thrb.tensor if hasattr(thrb, "tensor") else thrb[:].tensor, thrb[:].offset,
                         [list(thrb[:].ap[0]), [0, OWN // 128], [1, NEXP]])
        P.dve.op(tt_fn(msk[:], ao[:], thr_bc, ALU.is_ge), [bao, bthrb], [bmsk])
        P.dve.op(tt_fn(gate[:], msk[:], ao[:], ALU.mult), [bmsk, bao], [bgate])
        P.barrier()

    HT = OWN // 2
    with contextlib.ExitStack() as st:
        acc = P.sb([128, HT // 128, D], F32, stack=st)
        bacc = [Buf() for _ in range(HT // 128)]
        hT = P.sb([128, 8, HT], BF16, stack=st)
        bhT = Buf()
        junk = Ring(P, 1, [128, D], F32, st)
        small = Ring(P, 8, [128, 1], F32, st)
        hnr = Ring(P, 2, [128, D], BF16, st)
        hor = Ring(P, 2, [128, D], F32, st)
        ptr = Ring(P, 1, [128, 8, 128], BF16, st, psum=True)
        wgr = Ring(P, 2, [128, 8, D], BF16, st)
        wur = Ring(P, 2, [128, 8, D], BF16, st)
        wdr = Ring(P, 2, [128, 8, D], BF16, st)
        atr = Ring(P, 2, [128, 8, 512], BF16, st)
        sgr = Ring(P, 2, [128, 512], F32, st)
        pgr = Ring(P, 2, [128, 512], F32, st, psum=True)
        pur = Ring(P, 2, [128, 512], F32, st, psum=True)
        pyr = Ring(P, 2, [128, 512], F32, st, psum=True)
        for half in range(2):
            for t in range(HT // 128):
                tg = half * (HT // 128) + t
                P.sp.dma(out=acc[:, t, :], in_=x1[tg * 128:(tg + 1) * 128, :], writes=[bacc[t]])
                rs, br = rms_rstd(P, acc[:, t, :], bacc[t], D, junk, small)
                hn, bhn = hnr.next()
                P.dve.op(stt_fn(hn[:], acc[:, t, :], rs[:, 0:1], gfb[:], ALU.mult, ALU.mult),
                         [bacc[t], br, bgfb], [bhn])
                pt, bpt = ptr.next()
                for k in range(8):
                    P.pe.op(lambda e, k=k, pt=pt, hn=hn: e.transpose(pt[:, k, :], hn[:, k * 128:(k + 1) * 128],
                                                                      P.identb[:]), [bhn, P.bidb], [bpt])
                P.act.op(act_fn(hT[:, :, t * 128:(t + 1) * 128], pt[:], AF.Copy), [bpt], [bhT])
            for e in range(NEXP):
                wts = []
                for src, ring in ((wg, wgr), (wu, wur), (wd, wdr)):
                    w, bw = ring.next()
                    sv = src[e].rearrange("(k p) f -> p k f", p=128)
                    P.pool.dma(out=w[:, 0:4, :], in_=sv[:, 0:4, :], writes=[bw])
                    P.pool.dma(out=w[:, 4:8, :], in_=sv[:, 4:8, :], writes=[bw])
                    wts.append((w, bw))
                (Wg, bWg), (Wu, bWu), (Wd, bWd) = wts
                for q in range(HT // 512):
                    AT, bAT = atr.next()
                    for fc in range(8):
                        pg, bpg = pgr.next()
                        pu, bpu = pur.next()
                        for k in range(8):
                            P.pe.op(mm_fn(pg[:], Wg[:, k, fc * 128:(fc + 1) * 128], hT[:, k, q * 512:(q + 1) * 512],
                                          k == 0, k == 7), [bWg, bhT], [bpg])
                        for k in range(8):
                            P.pe.op(mm_fn(pu[:], Wu[:, k, fc * 128:(fc + 1) * 128], hT[:, k, q * 512:(q + 1) * 512],
                                          k == 0, k == 7), [bWu, bhT], [bpu])
                        sg, bsg = sgr.next()
                        P.act.op(act_fn(sg[:], pg[:], AF.Silu), [bpg], [bsg])
                        P.dve.op(tt_fn(AT[:, fc, :], sg[:], pu[:], ALU.mult), [bsg, bpu], [bAT])
                    for tt in range(4):
                        t = q * 4 + tt
                        tg = half * (HT // 128) + t
                        for hc in range(2):
                            py, bpy = pyr.next()
                            for fc in range(8):
                                P.pe.op(mm_fn(py[:], AT[:, fc, tt * 128:(tt + 1) * 128],
                                              Wd[:, fc, hc * 512:(hc + 1) * 512], fc == 0, fc == 7), [bAT, bWd], [bpy])
                            asl = acc[:, t, hc * 512:(hc + 1) * 512]
                            P.dve.op(stt_fn(asl, py[:], gate[:, tg, e:e + 1], asl, ALU.mult, ALU.add),
                                     [bpy, bgate, bacc[t]], [bacc[t]])
            for t in range(HT // 128):
                tg = half * (HT // 128) + t
                P.sp.dma(out=x2_out[tg * 128:(tg + 1) * 128, :], in_=acc[:, t, :], reads=[bacc[t]])
                if gn is not None:
                    rs, br = rms_rstd(P, acc[:, t, :], bacc[t], D, junk, small)
                    ho, bho = hor.next()
                    P.dve.op(stt_fn(ho[:], acc[:, t, :], rs[:, 0:1], gnb[:], ALU.mult, ALU.mult),
                             [bacc[t], br, bgnb], [bho])
                    P.sp.dma(out=hn_out[tg * 128:(tg + 1) * 128, :], in_=ho[:], reads=[bho])


def run_phaseB(x1_cores, affT_cores, aff_cores, gf, wg, wu, wd, gn):
    nc = get_prog("B", build_phaseB)
    cst = host_consts()
    in_maps = []
    for c in range(NCORES):
        b = c // 2
        affT_seq = np.ascontiguousarray(np.concatenate([affT_cores[2 * b], affT_cores[2 * b + 1]], axis=1))
        in_maps.append({
            "x1": x1_cores[c], "affT_seq": affT_seq, "aff_own": aff_cores[c],
            "gf": np.ascontiguousarray(gf[None, :]), "wg": wg, "wu": wu, "wd": wd, "cst": cst,
            "gn": np.ascontiguousarray(gn[None, :]),
        })
    res = run_bass_kernel_spmd(nc, in_maps, core_ids=list(range(NCORES)))
    return [r["x2"] for r in res.results], [r["hn"] for r in res.results]


NG = 32
NK = SEQ // 8
NWIN = NK // 8
HALF_PI = 1.5707963267948966


class _Stop(Exception):
    pass


def build_phaseC(stop_after=99):
    P = Prog()
    try:
        _phaseC_body(P, stop_after)
    except _Stop:
        P.barrier()
    return P.finish()


def _phaseC_body(P, stop_after):
    lamr_i = P.inp("lamr", [128, NG])
    lami_i = P.inp("lami", [128, NG])
    ldt_i = P.inp("ldt", [128, NG])
    B_i = P.inp("Bri", [128, 2, NG, 16])
    C_i = P.inp("Cri", [128, 2, NG, 16])
    dcol_i = P.inp("dcol", [128, NG])
    Unat = P.inp("Unat", [NG, 128, NK])
    Uw = P.inp("Uw", [NWIN, 128, NG, 8])
    Uwr = P.inp("Uwr", [NWIN, 128, NG, 8])
    masks_i = P.inp("masks", [128, 2, 128])
    cst_in = P.inp("cst", [128, 6, 128])
    zp = P.outp("zp", [NG, 128, NK])
    load_consts(P, cst_in)
    dve, act, pe, pool, sp = P.dve, P.act, P.pe, P.pool, P.sp

    M = P.sb([128, NG, 128], BF16, name="M")
    bM = Buf()
    Wz = P.sb([128, NG, 4, 128], BF16, name="Wz")
    bWz = Buf()
    Rr = P.sb([128, NG, 128], BF16, name="Rr")
    Ri = P.sb([128, NG, 128], BF16, name="Ri")
    bR = Buf()
    A8 = P.sb([128, 2, 2, NG], F32, name="A8")
    bA8 = Buf()
    dcol = P.sb([128, NG], F32, name="dcol")
    bdcol = Buf()
    sp.dma(out=dcol[:], in_=dcol_i, writes=[bdcol])

    def small(st, name=None):
        return P.sb([128, NG], F32, stack=st), Buf()

    with contextlib.ExitStack() as st:
        lamr, blamr = small(st)
        lami, blami = small(st)
        ldt, bldt = small(st)
        sp.dma(out=lamr[:], in_=lamr_i, writes=[blamr])
        sp.dma(out=lami[:], in_=lami_i, writes=[blami])
        sp.dma(out=ldt[:], in_=ldt_i, writes=[bldt])
        Bt = P.sb([128, 2, NG, 16], F32, stack=st)
        bBt = Buf()
        Ct = P.sb([128, 2, NG, 16], F32, stack=st)
        bCt = Buf()
        sp.dma(out=Bt[:], in_=B_i, writes=[bBt])
        sp.dma(out=Ct[:], in_=C_i, writes=[bCt])
        mk = P.sb([128, 2, 128], F32, stack=st)
        bmk = Buf()
        sp.dma(out=mk[:], in_=masks_i, writes=[bmk])
        hpi = P.sb([128, 1], F32, stack=st)
        bhpi = Buf()
        dve.op(lambda e: e.memset(hpi[:], HALF_PI), [], [bhpi])

        def T2(a, ba, b, bb, op):
            o, bo = small(st)
            dve.op(tt_fn(o[:], a[:], b[:], op), [ba, bb], [bo])
            return o, bo

        dt, bdt = small(st)
        act.op(act_fn(dt[:], ldt[:], AF.Exp), [bldt], [bdt])
        a_, ba_ = T2(lamr, blamr, dt, bdt, ALU.mult)
        ang, bang = T2(lami, blami, dt, bdt, ALU.mult)
        mag, bmag = small(st)
        act.op(act_fn(mag[:], a_[:], AF.Exp, scale=1.0 / 16), [ba_], [bmag])
        s16, bs16 = small(st)
        act.op(act_fn(s16[:], ang[:], AF.Sin, scale=1.0 / 16), [bang], [bs16])
        c16, bc16 = small(st)
        act.op(act_fn(c16[:], ang[:], AF.Sin, scale=1.0 / 16, bias=hpi[:, 0:1]), [bang, bhpi], [bc16])
        re, bre = T2(mag, bmag, c16, bc16, ALU.mult)
        im, bim = T2(mag, bmag, s16, bs16, ALU.mult)
        for _ in range(4):
            r2, br2 = T2(re, bre, re, bre, ALU.mult)
            i2, bi2 = T2(im, bim, im, bim, ALU.mult)
            nre, bnre = T2(r2, br2, i2, bi2, ALU.subtract)
            nim, bnim = small(st)
            dve.op(stt_fn(nim[:], re[:], 2.0, im[:], ALU.mult, ALU.mult), [bre, bim], [bnim])
            re, bre, im, bim = nre, bnre, nim, bnim
        pw = P.sb([128, 9, 2, NG], F32, stack=st)
        bpw = Buf()
        ipw = P.sb([128, 9, 2, NG], F32, stack=st)
        bipw = Buf()
        dve.op(lambda e: e.memset(pw[:, 0, 0, :], 1.0), [], [bpw])
        dve.op(lambda e: e.memset(pw[:, 0, 1, :], 0.0), [], [bpw])
        dve.op(lambda e: e.memset(ipw[:, 0, 0, :], 1.0), [], [bipw])
        dve.op(lambda e: e.memset(ipw[:, 0, 1, :], 0.0), [], [bipw])
        dve.op(lambda e: e.tensor_copy(out=pw[:, 1, 0, :], in_=re[:]), [bre], [bpw])
        dve.op(lambda e: e.tensor_copy(out=pw[:, 1, 1, :], in_=im[:]), [bim], [bpw])
        t1, bt1 = small(st)
        t2, bt2 = small(st)
        for n in range(1, 8):
            dve.op(tt_fn(t1[:], pw[:, n, 0, :], re[:], ALU.mult), [bpw, bre], [bt1])
            dve.op(tt_fn(t2[:], pw[:, n, 1, :], im[:], ALU.mult), [bpw, bim], [bt2])
            dve.op(tt_fn(pw[:, n + 1, 0, :], t1[:], t2[:], ALU.subtract), [bt1, bt2], [bpw])
            dve.op(tt_fn(t1[:], pw[:, n, 0, :], im[:], ALU.mult), [bpw, bim], [bt1])
            dve.op(tt_fn(t2[:], pw[:, n, 1, :], re[:], ALU.mult), [bpw, bre], [bt2])
            dve.op(tt_fn(pw[:, n + 1, 1, :], t1[:], t2[:], ALU.add), [bt1, bt2], [bpw])
        en, ben = small(st)
        for n in range(1, 9):
            act.op(act_fn(en[:], a_[:], AF.Exp, scale=-2.0 * n), [ba_], [ben])
            dve.op(tt_fn(ipw[:, n, 0, :], pw[:, n, 0, :], en[:], ALU.mult), [bpw, ben], [bipw])
            dve.op(stt_fn(ipw[:, n, 1, :], pw[:, n, 1, :], -1.0, en[:], ALU.mult, ALU.mult), [bpw, ben], [bipw])
        dve.op(lambda e: e.tensor_copy(out=A8[:, 0, 0, :], in_=pw[:, 8, 0, :]), [bpw], [bA8])
        dve.op(lambda e: e.tensor_copy(out=A8[:, 0, 1, :], in_=pw[:, 8, 0, :]), [bpw], [bA8])
        dve.op(ts_fn(A8[:, 1, 0, :], pw[:, 8, 1, :], -1.0, None, ALU.mult), [bpw], [bA8])
        dve.op(lambda e: e.tensor_copy(out=A8[:, 1, 1, :], in_=pw[:, 8, 1, :]), [bpw], [bA8])
        lr2, blr2 = T2(lamr, blamr, lamr, blamr, ALU.mult)
        li2, bli2 = T2(lami, blami, lami, blami, ALU.mult)
        den, bden = T2(lr2, blr2, li2, bli2, ALU.add)
        rden, brden = small(st)
        dve.op(lambda e: e.reciprocal(out=rden[:], in_=den[:]), [bden], [brden])
        nr, bnr = small(st)
        dve.op(ts_fn(nr[:], re[:], -1.0, None, ALU.add), [bre], [bnr])
        u1, bu1 = T2(nr, bnr, lamr, blamr, ALU.mult)
        u2, bu2 = T2(im, bim, lami, blami, ALU.mult)
        u3, bu3 = T2(u1, bu1, u2, bu2, ALU.add)
        fre, bfre = T2(u3, bu3, rden, brden, ALU.mult)
        u4, bu4 = T2(im, bim, lamr, blamr, ALU.mult)
        u5, bu5 = T2(nr, bnr, lami, blami, ALU.mult)
        u6, bu6 = T2(u4, bu4, u5, bu5, ALU.subtract)
        fim, bfim = T2(u6, bu6, rden, brden, ALU.mult)

        def bc16_(t):
            a = t[:]
            return bass.AP(a.tensor, a.offset, [list(a.ap[0]), [1, NG], [0, 16]])

        big = lambda: (P.sb([128, NG, 16], F32, stack=st), Buf())
        bbr, bbbr = big()
        bbi, bbbi = big()
        g1, bg1 = big()
        g2, bg2 = big()
        dve.op(tt_fn(g1[:], Bt[:, 0, :, :], bc16_(fre), ALU.mult), [bBt, bfre], [bg1])
        dve.op(tt_fn(g2[:], Bt[:, 1, :, :], bc16_(fim), ALU.mult), [bBt, bfim], [bg2])
        dve.op(tt_fn(bbr[:], g1[:], g2[:], ALU.subtract), [bg1, bg2], [bbbr])
        dve.op(tt_fn(g1[:], Bt[:, 1, :, :], bc16_(fre), ALU.mult), [bBt, bfre], [bg1])
        dve.op(tt_fn(g2[:], Bt[:, 0, :, :], bc16_(fim), ALU.mult), [bBt, bfim], [bg2])
        dve.op(tt_fn(bbi[:], g1[:], g2[:], ALU.add), [bg1, bg2], [bbbi])

        def table(top, bot):
            t = P.sb([128, 8, 2, NG], F32, stack=st)
            bt = Buf()
            for i in range(8):
                (ta, tn), (ba, bn) = top[i], bot[i]
                pool.op(lambda e, i=i, ta=ta, tn=tn: e.tensor_copy(out=t[0:64, i, :, :], in_=ta[0:64, tn, :, :]),
                        [bpw, bipw], [bt])
                pool.op(lambda e, i=i, ba=ba, bn=bn: e.tensor_copy(out=t[64:128, i, :, :], in_=ba[64:128, bn, :, :]),
                        [bpw, bipw], [bt])
            return t, bt

        QX, bQX = table([(ipw, s) for s in range(8)], [(pw, s) for s in range(8)])
        PY, bPY = table([(pw, t) for t in range(8)], [(ipw, t) for t in range(8)])
        QW, bQW = table([(pw, 7 - s) for s in range(8)], [(pw, s) for s in range(8)])
        PR, bPR = table([(pw, j + 1) for j in range(8)], [(pw, 8 - j) for j in range(8)])

        def tb(t, i, ri):
            a = t[:, i, ri, :]
            return bass.AP(a.tensor, a.offset, [list(a.ap[0]), [1, NG], [0, 16]])

        def cprod(dst_re, dst_im, bdst, xr, xi, bx, tab, btab, neg_im=False, eng=None):
            eng = eng or dve
            bx = list(bx) if isinstance(bx, (list, tuple)) else [bx]
            for i in range(8):
                eng.op(tt_fn(g1[:], xr[:], tb(tab, i, 0), ALU.mult), bx + [btab], [bg1])
                eng.op(tt_fn(g2[:], xi[:], tb(tab, i, 1), ALU.mult), bx + [btab], [bg2])
                eng.op(tt_fn(dst_re[:, :, i, :], g1[:], g2[:], ALU.subtract), [bg1, bg2], [bdst])
                eng.op(tt_fn(g1[:], xr[:], tb(tab, i, 1), ALU.mult), bx + [btab], [bg1])
                eng.op(tt_fn(g2[:], xi[:], tb(tab, i, 0), ALU.mult), bx + [btab], [bg2])
                if neg_im:
                    eng.op(stt_fn(dst_im[:, :, i, :], g1[:], -1.0, g2[:], ALU.mult, ALU.subtract), [bg1, bg2], [bdst])
                else:
                    eng.op(tt_fn(dst_im[:, :, i, :], g1[:], g2[:], ALU.add), [bg1, bg2], [bdst])

        bbx = Buf()
        with contextlib.ExitStack() as st2:
            Xr = P.sb([128, NG, 8, 16], F32, stack=st2)
            Xi = P.sb([128, NG, 8, 16], F32, stack=st2)
            Yr = P.sb([128, NG, 8, 16], F32, stack=st2)
            Yn = P.sb([128, NG, 8, 16], F32, stack=st2)
            bX, bY = Buf(), Buf()
            bBB = Buf()
            cprod(Xr, Xi, bX, bbr, bbi, [bbbr, bbbi], QX, bQX)
            cprod(Yr, Yn, bY, Ct[:, 0, :, :], Ct[:, 1, :, :], bCt, PY, bPY, neg_im=True)
            psF = Ring(P, 2, [128, 512], F32, st2, psum=True)
            psB = Ring(P, 2, [128, 512], F32, st2, psum=True)
            tmr = Ring(P, 2, [128, 128], F32, st2)
            Xr3 = Xr[:].rearrange("p g s c -> p g (s c)")
            Xi3 = Xi[:].rearrange("p g s c -> p g (s c)")
            Yr3 = Yr[:].rearrange("p g s c -> p g (s c)")
            Yn3 = Yn[:].rearrange("p g s c -> p g (s c)")
            for g in range(NG):
                pf, bpf = psF.next()
                pb_, bpb = psB.next()
                pe.op(mm_fn(pf[:, 0:128], Xr3[0:64, g, :], Yr3[0:64, g, :], True, False), [bX, bY], [bpf])
                pe.op(mm_fn(pf[:, 0:128], Xi3[0:64, g, :], Yn3[0:64, g, :], False, True), [bX, bY], [bpf])
                pe.op(mm_fn(pb_[:, 0:128], Xr3[64:128, g, :], Yr3[64:128, g, :], True, False), [bX, bY], [bpb])
                pe.op(mm_fn(pb_[:, 0:128], Xi3[64:128, g, :], Yn3[64:128, g, :], False, True), [bX, bY], [bpb])
                tm, btm = tmr.next()
                dve.op(tt_fn(tm[:], pf[:, 0:128], mk[:, 0, :], ALU.mult), [bpf, bmk], [btm])
                tm2, btm2 = tmr.next()
                dve.op(tt_fn(tm2[:], pb_[:, 0:128], mk[:, 1, :], ALU.mult), [bpb, bmk], [btm2])
                dve.op(tt_fn(M[:, g, :], tm[:], tm2[:], ALU.add), [btm, btm2], [bM])
            P.barrier()
        if stop_after <= 1:
            P.stopped = True
        with contextlib.ExitStack() as st2:
            Wr = P.sb([128, NG, 8, 16], F32, stack=st2)
            Wi = P.sb([128, NG, 8, 16], F32, stack=st2)
            bW = Buf()
            cprod(Wr, Wi, bW, bbr, bbi, [bbbr, bbbi], QW, bQW)
            pool.op(lambda e: e.memset(Wz[:], 0.0), [], [bWz])
            ptw = Ring(P, 2, [128, 512], F32, st2, psum=True)
            W3 = (Wr[:].rearrange("p g s c -> p g (s c)"), Wi[:].rearrange("p g s c -> p g (s c)"))
            for g in range(NG):
                for ri in range(2):
                    pt, bpt = ptw.next()
                    pe.op(lambda e, pt=pt, g=g, ri=ri: e.transpose(pt[:, 0:128], W3[ri][:, g, :], P.identf),
                          [bW, P.bcst], [bpt])
                    act.op(act_fn(Wz[:, g, 2 * ri, 0:64], pt[:, 0:64], AF.Copy), [bpt], [bWz])
                    act.op(act_fn(Wz[:, g, 2 * ri + 1, 64:128], pt[:, 64:128], AF.Copy), [bpt], [bWz])
            P.barrier()
        if stop_after <= 2:
            P.stopped = True
        Rr4 = Rr[:].rearrange("p g (j c) -> p g j c", j=8)
        Ri4 = Ri[:].rearrange("p g (j c) -> p g j c", j=8)
        cprod(Rr4, Ri4, bR, Ct[:, 0, :, :], Ct[:, 1, :, :], bCt, PR, bPR, neg_im=True)
        P.barrier()

    with contextlib.ExitStack() as st:
        hist = P.sb([128, 2, NG, NK], BF16, stack=st)
        bhist = Buf()
        uwr = Ring(P, 3, [128, NG, 8], BF16, st)
        uwrr = Ring(P, 3, [128, NG, 8], BF16, st)
        pvr = Ring(P, 3, [128, 2, NG, 8], F32, st, psum=True)
        S4r = Ring(P, 4, [128, 3, NG], F32, st)
        p1 = P.sb([128, 2, NG], F32, stack=st)
        p2 = P.sb([128, 2, NG], F32, stack=st)
        bp1, bp2 = Buf(), Buf()
        S4, bS = S4r.next()
        dve.op(lambda e: e.memset(S4[:], 0.0), [], [bS])
        for w in range(NWIN):
            uw, buw = uwr.next()
            uwb, buwb = uwrr.next()
            pool.dma(out=uw[:], in_=Uw[w], writes=[buw])
            pool.dma(out=uwb[:], in_=Uwr[w], writes=[buwb])
            pv, bpv = pvr.next()
            for g in range(NG):
                for ri in range(2):
                    pe.op(mm_fn(pv[:, ri, g, :], Wz[:, g, 2 * ri, :], uw[:, g, :], True, False), [bWz, buw], [bpv])
                    pe.op(mm_fn(pv[:, ri, g, :], Wz[:, g, 2 * ri + 1, :], uwb[:, g, :], False, True), [bWz, buwb], [bpv])
            for jj in range(8):
                j = w * 8 + jj
                act.op(act_fn(hist[0:64, :, :, j], S4[0:64, 0:2, :], AF.Copy), [bS], [bhist])
                act.op(act_fn(hist[64:128, :, :, NK - 1 - j], S4[64:128, 0:2, :], AF.Copy), [bS], [bhist])
                Sn, bSn = S4r.next()
                dve.op(tt_fn(p1[:], A8[:, 0, :, :], S4[:, 0:2, :], ALU.mult), [bA8, bS], [bp1])
                dve.op(tt_fn(p2[:], A8[:, 1, :, :], S4[:, 1:3, :], ALU.mult), [bA8, bS], [bp2])
                dve.op(tt_fn(p1[:], p1[:], p2[:], ALU.add), [bp1, bp2], [bp1])
                dve.op(tt_fn(Sn[:, 0:2, :], p1[:], pv[:, :, :, jj], ALU.add), [bp1, bpv], [bSn])
                dve.op(lambda e, Sn=Sn: e.tensor_copy(out=Sn[:, 2, :], in_=Sn[:, 0, :]), [bSn], [bSn])
                S4, bS = Sn, bSn
        P.barrier()
        if stop_after <= 4:
            P.stopped = True
        ubr = Ring(P, 2, [128, NK], BF16, st)
        ufr = Ring(P, 2, [128, NK], F32, st)
        pyr = Ring(P, 2, [128, 512], F32, st, psum=True)
        yr = Ring(P, 2, [128, NK], F32, st)
        tr = Ring(P, 4, [128, NK], F32, st)
        for g in range(NG):
            ub, bub = ubr.next()
            uf, buf_ = ufr.next()
            pool.dma(out=ub[:], in_=Unat[g], writes=[bub])
            sp.dma(out=uf[:], in_=Unat[g], writes=[buf_])
            py, bpy = pyr.next()
            pe.op(mm_fn(py[:], M[:, g, :], ub[:], True, False), [bM, bub], [bpy])
            pe.op(mm_fn(py[:], Rr[:, g, :], hist[:, 0, g, :], False, False), [bR, bhist], [bpy])
            pe.op(mm_fn(py[:], Ri[:, g, :], hist[:, 1, g, :], False, True), [bR, bhist], [bpy])
            y, by = yr.next()
            dve.op(stt_fn(y[:], uf[:], dcol[:, g:g + 1], py[:], ALU.mult, ALU.add), [buf_, bdcol, bpy], [by])
            a1, ba1 = tr.next()
            dve.op(tt_fn(a1[:], y[:], y[:], ALU.mult), [by], [ba1])
            dve.op(ts_fn(a1[:], a1[:], 0.044715, 1.0, ALU.mult, ALU.add), [ba1], [ba1])
            dve.op(tt_fn(a1[:], a1[:], y[:], ALU.mult), [ba1, by], [ba1])
            a2, ba2 = tr.next()
            act.op(act_fn(a2[:], a1[:], AF.Sigmoid, scale=1.5957691216057308), [ba1], [ba2])
            dve.op(tt_fn(a2[:], a2[:], y[:], ALU.mult), [ba2, by], [ba2])
            sp.dma(out=zp[g], in_=a2[:], reads=[ba2])
    return


def host_masks():
    s = np.arange(128)[:, None] // 16
    t = np.arange(128)[None, :] // 16
    m = np.zeros((128, 2, 128), np.float32)
    m[:, 0, :] = (t >= s)
    m[:, 1, :] = (s >= t)
    return m


def phaseC_inputs(hn1_seq, gh, lam_re, lam_im, log_dt, b_re, b_im, c_re, c_im, d_skip):
    G0 = gh * NG
    sl = slice(G0, G0 + NG)

    def dpg(a):
        return np.ascontiguousarray(a.transpose(0, 2, 1).reshape(128, NG))

    lamr = dpg(lam_re[:, sl, :])
    lami = dpg(lam_im[:, sl, :])
    ldt = dpg(np.broadcast_to(log_dt[:, sl, None], (2, NG, 64)))
    Bri = np.stack([b_re[:, sl].transpose(0, 2, 1, 3).reshape(128, NG, 16),
                    b_im[:, sl].transpose(0, 2, 1, 3).reshape(128, NG, 16)], axis=1)
    Cri = np.stack([c_re[:, sl].transpose(0, 3, 1, 2).reshape(128, NG, 16),
                    c_im[:, sl].transpose(0, 3, 1, 2).reshape(128, NG, 16)], axis=1)
    dg = d_skip[G0 * 16:(G0 + NG) * 16].reshape(NG, 16)
    dcol = np.ascontiguousarray(np.broadcast_to(dg.T[None, :, :], (8, 16, NG)).reshape(128, NG))
    u = hn1_seq[:, G0 * 16:(G0 + NG) * 16].reshape(NK, 8, NG, 16)
    Unat = np.ascontiguousarray(u.transpose(2, 1, 3, 0).reshape(NG, 128, NK))
    Uw = np.ascontiguousarray(Unat.reshape(NG, 128, NWIN, 8).transpose(2, 1, 0, 3))
    Uwr = np.ascontiguousarray(Unat[:, :, ::-1].reshape(NG, 128, NWIN, 8).transpose(2, 1, 0, 3))
    return {"lamr": lamr, "lami": lami, "ldt": ldt, "Bri": np.ascontiguousarray(Bri),
            "Cri": np.ascontiguousarray(Cri), "dcol": dcol, "Unat": Unat, "Uw": Uw, "Uwr": Uwr,
            "masks": host_masks(), "cst": host_consts()}


def phaseC_unpack(zp):
    return np.ascontiguousarray(zp.reshape(NG, 8, 16, NK).transpose(3, 1, 0, 2).reshape(SEQ, NG * 16))


def emit_phaseD(P, io):
    with contextlib.ExitStack() as ph:
        P.cur = ph
        _phaseD_body(P, io["zs"], io["hn"], io["dvec"], io["x2"], io["w_glu"], io["gf"], io["w_r"], io["b_r"],
                     io["x3_out"], io["aff_out"], io["affT_out"])
        P.barrier()
    P.cur = None


def _phaseD_body(P, zt, hn_in, dvec, x2, w_glu, gf, w_r, b_r, x3_out, aff_out, affT_out):
    dvb = P.sb([128, D], F32, name="dvb")
    bdvb = Buf()
    P.sp.dma(out=dvb[:], in_=bcast_rows(dvec, 128), writes=[bdvb])
    st = P.cur
    wgl = P.sb([128, 8, 2 * D], BF16, name="wgl")
    bwgl = Buf()
    wv = w_glu.rearrange("(k p) e -> p k e", p=128)
    for k in range(8):
        P.pool.dma(out=wgl[:, k, :], in_=wv[:, k, :], writes=[bwgl])
    gfb = P.sb([128, D], F32, name="gfb")
    bgfb = Buf()
    P.sp.dma(out=gfb[:], in_=bcast_rows(gf, 128), writes=[bgfb])
    wr = P.sb([128, 8, 16], F32, name="wr")
    bwr = Buf()
    P.sp.dma(out=wr[:], in_=w_r.rearrange("(k p) e -> p k e", p=128), writes=[bwr])
    brb = P.sb([128, 16], F32, name="brb")
    bbrb = Buf()
    P.sp.dma(out=brb[:], in_=bcast_rows(b_r, 128), writes=[bbrb])
    zr = Ring(P, 2, [128, D], F32, st)
    zbr = Ring(P, 2, [128, D], BF16, st)
    hnr2 = Ring(P, 2, [128, D], F32, st)
    xr = Ring(P, 2, [128, D], F32, st)
    x3r = Ring(P, 2, [128, D], F32, st)
    ptr = Ring(P, 1, [128, 8, 128], BF16, st, psum=True)
    zTr = Ring(P, 2, [128, 8, 128], BF16, st)
    pvr = Ring(P, 1, [128, 512], F32, st, psum=True)
    pgr = Ring(P, 1, [128, 512], F32, st, psum=True)
    sgr = Ring(P, 2, [128, 512], F32, st)
    rings = {
        "junk": Ring(P, 1, [128, D], F32, st),
        "small": Ring(P, 12, [128, 1], F32, st),
        "hf": Ring(P, 2, [128, D], F32, st),
        "ptf": Ring(P, 1, [128, 8, 128], F32, st, psum=True),
        "hfT": Ring(P, 2, [128, 8, 128], F32, st),
        "plog": Ring(P, 1, [128, 512], F32, st, psum=True),
        "sm16": Ring(P, 6, [128, 16], F32, st),
        "pat": Ring(P, 1, [128, 512], F32, st, psum=True),
        "aT": Ring(P, 2, [16, 128], F32, st),
    }
    for tt in range(OWN // 128):
        z_, bz = zr.next()
        P.sp.dma(out=z_[:], in_=zt[tt * 128:(tt + 1) * 128, :], writes=[bz])
        xt, bx = xr.next()
        P.sp.dma(out=xt[:], in_=x2[tt * 128:(tt + 1) * 128, :], writes=[bx])
        hn_, bhn_ = hnr2.next()
        P.sp.dma(out=hn_[:], in_=hn_in[tt * 128:(tt + 1) * 128, :], writes=[bhn_])
        P.dve.op(tt_fn(hn_[:], hn_[:], dvb[:], ALU.mult), [bhn_, bdvb], [bhn_])
        P.dve.op(tt_fn(z_[:], z_[:], hn_[:], ALU.add), [bz, bhn_], [bz])
        P.dve.op(tt_fn(hn_[:], z_[:], z_[:], ALU.mult), [bz], [bhn_])
        P.dve.op(ts_fn(hn_[:], hn_[:], 0.044715, 1.0, ALU.mult, ALU.add), [bhn_], [bhn_])
        P.dve.op(tt_fn(hn_[:], hn_[:], z_[:], ALU.mult), [bhn_, bz], [bhn_])
        P.act.op(act_fn(hn_[:], hn_[:], AF.Sigmoid, scale=1.5957691216057308), [bhn_], [bhn_])
        zb, bzb = zbr.next()
        P.dve.op(tt_fn(zb[:], hn_[:], z_[:], ALU.mult), [bhn_, bz], [bzb])
        pt, bpt = ptr.next()
        for k in range(8):
            P.pe.op(lambda e, k=k, pt=pt, zb=zb: e.transpose(pt[:, k, :], zb[:, k * 128:(k + 1) * 128], P.identb[:]),
                    [bzb, P.bidb], [bpt])
        zT, bzT = zTr.next()
        P.act.op(act_fn(zT[:], pt[:], AF.Copy), [bpt], [bzT])
        x3t, bx3 = x3r.next()
        for half in range(2):
            pv, bpv = pvr.next()
            pg, bpg = pgr.next()
            for k in range(8):
                P.pe.op(mm_fn(pv[:], zT[:, k, :], wgl[:, k, half * 512:(half + 1) * 512], k == 0, k == 7),
                        [bzT, bwgl], [bpv])
            for k in range(8):
                P.pe.op(mm_fn(pg[:], zT[:, k, :], wgl[:, k, D + half * 512:D + (half + 1) * 512], k == 0, k == 7),
                        [bzT, bwgl], [bpg])
            sg, bsg = sgr.next()
            P.act.op(act_fn(sg[:], pg[:], AF.Sigmoid), [bpg], [bsg])
            P.dve.op(tt_fn(sg[:], sg[:], pv[:], ALU.mult), [bsg, bpv], [bsg])
            P.dve.op(tt_fn(x3t[:, half * 512:(half + 1) * 512], sg[:], xt[:, half * 512:(half + 1) * 512], ALU.add),
                     [bsg, bx], [bx3])
        P.sp.dma(out=x3_out[tt * 128:(tt + 1) * 128, :], in_=x3t[:], reads=[bx3])
        router_tile(P, x3t, bx3, gfb, bgfb, wr, bwr, brb, bbrb, rings, aff_out, affT_out, tt)
    return


def to_local(full_seq, h):
    return local_view(full_seq, h, OWN)


def from_local(parts):
    out = []
    for b in range(NCORES // 2):
        a0 = parts[2 * b]
        a1 = parts[2 * b + 1][::-1]
        out.append(np.concatenate([a0, a1], axis=0))
    return np.stack(out)


G32 = 32
NKL = OWN // 8
NWL = NKL // 8


def emit_phaseC2(P, io):
    with contextlib.ExitStack() as ph:
        P.cur = ph
        _phaseC2_body(P, io)
        P.barrier()
    P.cur = None


def _phaseC2_body(P, io):
    dve, act, pe, pool, sp = P.dve, P.act, P.pe, P.pool, P.sp
    s5p, s5B, s5C = io["s5p"], io["s5B"], io["s5C"]
    HN, ZS = io["hn"], io["zs_out"]
    SAo, SAall = io["sa_own"], io["sa_all"]

    MS, RS = io["ms"], io["rs"]
    bM = Buf()
    bR = Buf()
    U = P.sb([128, 64, NKL], BF16, name="U")
    bU = Buf()
    A8 = [P.sb([128, 2, 2, G32], F32, name="A8_%d" % d) for d in range(2)]
    bA8 = Buf()
    mk = P.sb([128, 2, 128], F32, name="mk")
    bmk = Buf()
    sp.dma(out=mk[:], in_=io["masks"], writes=[bmk])
    flg = P.sb([128, 2], F32, name="flg")
    bflg = Buf()
    sp.dma(out=flg[:], in_=io["flags"], writes=[bflg])
    hpi = P.sb([128, 1], F32, name="hpi")
    bhpi = Buf()
    dve.op(lambda e: e.memset(hpi[:], HALF_PI), [], [bhpi])
    g1 = P.sb([128, G32, 16], F32, name="g1")
    g2 = P.sb([128, G32, 16], F32, name="g2")
    bg1, bg2 = Buf(), Buf()
    pw_t = [P.sb([128, 9, 2, G32], F32, name="pw%d" % d) for d in range(2)]
    ipw_t = [P.sb([128, 9, 2, G32], F32, name="ipw%d" % d) for d in range(2)]
    bbr_t = [P.sb([128, G32, 16], F32, name="bbr%d" % d) for d in range(2)]
    bbi_t = [P.sb([128, G32, 16], F32, name="bbi%d" % d) for d in range(2)]
    with contextlib.ExitStack() as stp:
        M = P.sb([128, 64, 128], BF16, name="M", stack=stp)
        Rt = [[P.sb([128, G32, 128], BF16, name="R%d%d" % (d, r), stack=stp) for r in range(2)] for d in range(2)]
        par = P.sb([128, 2, 3, G32], F32, name="par", stack=stp)
        bpar = Buf()
        sp.dma(out=par[:], in_=s5p, writes=[bpar])
        Bt = P.sb([128, 2, 2, G32, 16], F32, name="Bt", stack=stp)
        bBt = Buf()
        sp.dma(out=Bt[:], in_=s5B, writes=[bBt])
        Ct = P.sb([128, 2, 2, G32, 16], F32, name="Ct", stack=stp)
        bCt = Buf()
        sp.dma(out=Ct[:], in_=s5C, writes=[bCt])

        def small():
            return P.sb([128, G32], F32, stack=stp), Buf()

        def T2(a, ba, b, bb, op):
            o, bo = small()
            dve.op(tt_fn(o[:], a[:], b[:], op), [ba, bb], [bo])
            return o, bo

        def bc(a):
            return bass.AP(a.tensor, a.offset, [list(a.ap[0]), [1, G32], [0, 16]])

        pws, ipws, bbs = [], [], []
        for d in range(2):
            lamr, lami, ldt = par[:, d, 0, :], par[:, d, 1, :], par[:, d, 2, :]
            dt, bdt = small()
            act.op(act_fn(dt[:], ldt, AF.Exp), [bpar], [bdt])
            a_, ba_ = small()
            dve.op(tt_fn(a_[:], lamr, dt[:], ALU.mult), [bpar, bdt], [ba_])
            ang, bang = small()
            dve.op(tt_fn(ang[:], lami, dt[:], ALU.mult), [bpar, bdt], [bang])
            mag, bmag = small()
            act.op(act_fn(mag[:], a_[:], AF.Exp, scale=1.0 / 16), [ba_], [bmag])
            s16, bs16 = small()
            act.op(act_fn(s16[:], ang[:], AF.Sin, scale=1.0 / 16), [bang], [bs16])
            c16, bc16 = small()
            act.op(act_fn(c16[:], ang[:], AF.Sin, scale=1.0 / 16, bias=hpi[:, 0:1]), [bang, bhpi], [bc16])
            re, bre = T2(mag, bmag, c16, bc16, ALU.mult)
            im, bim = T2(mag, bmag, s16, bs16, ALU.mult)
            for _ in range(4):
                r2, br2 = T2(re, bre, re, bre, ALU.mult)
                i2, bi2 = T2(im, bim, im, bim, ALU.mult)
                nre, bnre = T2(r2, br2, i2, bi2, ALU.subtract)
                nim, bnim = small()
                dve.op(stt_fn(nim[:], re[:], 2.0, im[:], ALU.mult, ALU.mult), [bre, bim], [bnim])
                re, bre, im, bim = nre, bnre, nim, bnim
            pw = pw_t[d]
            ipw = ipw_t[d]
            bpw, bipw = Buf(), Buf()
            dve.op(lambda e, pw=pw: e.memset(pw[:, 0, 0, :], 1.0), [], [bpw])
            dve.op(lambda e, pw=pw: e.memset(pw[:, 0, 1, :], 0.0), [], [bpw])
            dve.op(lambda e, ipw=ipw: e.memset(ipw[:, 0, 0, :], 1.0), [], [bipw])
            dve.op(lambda e, ipw=ipw: e.memset(ipw[:, 0, 1, :], 0.0), [], [bipw])
            dve.op(lambda e, pw=pw, re=re: e.tensor_copy(out=pw[:, 1, 0, :], in_=re[:]), [bre], [bpw])
            dve.op(lambda e, pw=pw, im=im: e.tensor_copy(out=pw[:, 1, 1, :], in_=im[:]), [bim], [bpw])
            t1, bt1 = small()
            t2, bt2 = small()
            for n in range(1, 8):
                dve.op(tt_fn(t1[:], pw[:, n, 0, :], re[:], ALU.mult), [bpw, bre], [bt1])
                dve.op(tt_fn(t2[:], pw[:, n, 1, :], im[:], ALU.mult), [bpw, bim], [bt2])
                dve.op(tt_fn(pw[:, n + 1, 0, :], t1[:], t2[:], ALU.subtract), [bt1, bt2], [bpw])
                dve.op(tt_fn(t1[:], pw[:, n, 0, :], im[:], ALU.mult), [bpw, bim], [bt1])
                dve.op(tt_fn(t2[:], pw[:, n, 1, :], re[:], ALU.mult), [bpw, bre], [bt2])
                dve.op(tt_fn(pw[:, n + 1, 1, :], t1[:], t2[:], ALU.add), [bt1, bt2], [bpw])
            en, ben = small()
            for n in range(1, 9):
                act.op(act_fn(en[:], a_[:], AF.Exp, scale=-2.0 * n), [ba_], [ben])
                dve.op(tt_fn(ipw[:, n, 0, :], pw[:, n, 0, :], en[:], ALU.mult), [bpw, ben], [bipw])
                dve.op(stt_fn(ipw[:, n, 1, :], pw[:, n, 1, :], -1.0, en[:], ALU.mult, ALU.mult), [bpw, ben], [bipw])
            a8 = A8[d]
            dve.op(lambda e, a8=a8, pw=pw: e.tensor_copy(out=a8[:, 0, 0, :], in_=pw[:, 8, 0, :]), [bpw], [bA8])
            dve.op(lambda e, a8=a8, pw=pw: e.tensor_copy(out=a8[:, 0, 1, :], in_=pw[:, 8, 0, :]), [bpw], [bA8])
            dve.op(ts_fn(a8[:, 1, 0, :], pw[:, 8, 1, :], -1.0, None, ALU.mult), [bpw], [bA8])
            dve.op(lambda e, a8=a8, pw=pw: e.tensor_copy(out=a8[:, 1, 1, :], in_=pw[:, 8, 1, :]), [bpw], [bA8])
            lr2, blr2 = small()
            dve.op(tt_fn(lr2[:], lamr, lamr, ALU.mult), [bpar], [blr2])
            li2, bli2 = small()
            dve.op(tt_fn(li2[:], lami, lami, ALU.mult), [bpar], [bli2])
            den, bden = T2(lr2, blr2, li2, bli2, ALU.add)
            rden, brden = small()
            dve.op(lambda e, rden=rden, den=den: e.reciprocal(out=rden[:], in_=den[:]), [bden], [brden])
            nr, bnr = small()
            dve.op(ts_fn(nr[:], re[:], -1.0, None, ALU.add), [bre], [bnr])
            u1, bu1 = small()
            dve.op(tt_fn(u1[:], nr[:], lamr, ALU.mult), [bnr, bpar], [bu1])
            u2, bu2 = small()
            dve.op(tt_fn(u2[:], im[:], lami, ALU.mult), [bim, bpar], [bu2])
            u3, bu3 = T2(u1, bu1, u2, bu2, ALU.add)
            fre, bfre = T2(u3, bu3, rden, brden, ALU.mult)
            u4, bu4 = small()
            dve.op(tt_fn(u4[:], im[:], lamr, ALU.mult), [bim, bpar], [bu4])
            u5, bu5 = small()
            dve.op(tt_fn(u5[:], nr[:], lami, ALU.mult), [bnr, bpar], [bu5])
            u6, bu6 = T2(u4, bu4, u5, bu5, ALU.subtract)
            fim, bfim = T2(u6, bu6, rden, brden, ALU.mult)
            bbr = bbr_t[d]
            bbi = bbi_t[d]
            bbb = Buf()
            dve.op(tt_fn(g1[:], Bt[:, d, 0, :, :], bc(fre[:]), ALU.mult), [bBt, bfre], [bg1])
            dve.op(tt_fn(g2[:], Bt[:, d, 1, :, :], bc(fim[:]), ALU.mult), [bBt, bfim], [bg2])
            dve.op(tt_fn(bbr[:], g1[:], g2[:], ALU.subtract), [bg1, bg2], [bbb])
            dve.op(tt_fn(g1[:], Bt[:, d, 1, :, :], bc(fre[:]), ALU.mult), [bBt, bfre], [bg1])
            dve.op(tt_fn(g2[:], Bt[:, d, 0, :, :], bc(fim[:]), ALU.mult), [bBt, bfim], [bg2])
            dve.op(tt_fn(bbi[:], g1[:], g2[:], ALU.add), [bg1, bg2], [bbb])
            pws.append((pw, bpw))
            ipws.append((ipw, bipw))
            bbs.append((bbr, bbi, bbb))

        def cprod(dst_re, dst_im, bdst, xr, xi, bx, tab, btab, idx, neg_im=False):
            bx = list(bx) if isinstance(bx, (list, tuple)) else [bx]
            for i in range(8):
                tr, ti = bc(tab[:, idx(i), 0, :]), bc(tab[:, idx(i), 1, :])
                dve.op(tt_fn(g1[:], xr, tr, ALU.mult), bx + [btab], [bg1])
                dve.op(tt_fn(g2[:], xi, ti, ALU.mult), bx + [btab], [bg2])
                dve.op(tt_fn(dst_re[:, :, i, :], g1[:], g2[:], ALU.subtract), [bg1, bg2], [bdst])
                dve.op(tt_fn(g1[:], xr, ti, ALU.mult), bx + [btab], [bg1])
                dve.op(tt_fn(g2[:], xi, tr, ALU.mult), bx + [btab], [bg2])
                if neg_im:
                    dve.op(stt_fn(dst_im[:, :, i, :], g1[:], -1.0, g2[:], ALU.mult, ALU.subtract), [bg1, bg2], [bdst])
                else:
                    dve.op(tt_fn(dst_im[:, :, i, :], g1[:], g2[:], ALU.add), [bg1, bg2], [bdst])

        v4 = lambda t: t[:].rearrange("p g (j c) -> p g j c", j=8)
        f3 = lambda t: t[:].rearrange("p g s c -> p g (s c)")

        with contextlib.ExitStack() as st2:
            XY = [[P.sb([128, G32, 8, 16], BF16, stack=st2) for _ in range(4)] for _ in range(2)]
            bXY = Buf()
            (pwA, bpwA), (ipwA, bipwA) = pws[0], ipws[0]
            (pwB, bpwB), (ipwB, bipwB) = pws[1], ipws[1]
            cprod(XY[0][0], XY[0][1], bXY, bbs[0][0][:], bbs[0][1][:], bbs[0][2], ipwA, bipwA, lambda s: s)
            cprod(XY[0][2], XY[0][3], bXY, Ct[:, 0, 0, :, :], Ct[:, 0, 1, :, :], bCt, pwA, bpwA, lambda t: t, neg_im=True)
            cprod(XY[1][0], XY[1][1], bXY, bbs[1][0][:], bbs[1][1][:], bbs[1][2], pwB, bpwB, lambda s: s)
            cprod(XY[1][2], XY[1][3], bXY, Ct[:, 1, 0, :, :], Ct[:, 1, 1, :, :], bCt, ipwB, bipwB, lambda t: t, neg_im=True)
            cprod(v4(Rt[0][0]), v4(Rt[0][1]), bR, Ct[:, 0, 0, :, :], Ct[:, 0, 1, :, :], bCt, pwA, bpwA,
                  lambda j: j + 1, neg_im=True)
            cprod(v4(Rt[1][0]), v4(Rt[1][1]), bR, Ct[:, 1, 0, :, :], Ct[:, 1, 1, :, :], bCt, pwB, bpwB,
                  lambda j: 8 - j, neg_im=True)
            pk = [[Ring(P, 1, [128, 512], F32, st2, psum=True) for _ in range(2)] for _ in range(2)]
            tmr = Ring(P, 4, [128, 128], F32, st2)
            for g in range(G32):
                pp = [[None, None], [None, None]]
                for d in range(2):
                    Xr, Xi, Yr, Yn = [f3(t) for t in XY[d]]
                    for hf in range(2):
                        r0 = 64 * hf
                        pt, bpt = pk[d][hf].next()
                        pe.op(mm_fn(pt[:, 0:128], Xr[r0:r0 + 64, g, :], Yr[r0:r0 + 64, g, :], True, False), [bXY], [bpt])
                        pe.op(mm_fn(pt[:, 0:128], Xi[r0:r0 + 64, g, :], Yn[r0:r0 + 64, g, :], False, True), [bXY], [bpt])
                        pp[d][hf] = (pt, bpt)
                for hf in range(2):
                    tm, btm = tmr.next()
                    dve.op(tt_fn(tm[:], pp[0][hf][0][:, 0:128], mk[:, 0, :], ALU.mult), [pp[0][hf][1], bmk], [btm])
                    tm2, btm2 = tmr.next()
                    dve.op(tt_fn(tm2[:], pp[1][hf][0][:, 0:128], mk[:, 1, :], ALU.mult), [pp[1][hf][1], bmk], [btm2])
                    dve.op(tt_fn(M[:, hf * G32 + g, :], tm[:], tm2[:], ALU.add), [btm, btm2], [bM])
            sp.dma(out=MS.ap(), in_=M[:].rearrange("p g c -> p (g c)"), reads=[bM])
            for d in range(2):
                for r in range(2):
                    sp.dma(out=RS.ap()[:, (2 * d + r) * G32 * 128:(2 * d + r + 1) * G32 * 128],
                           in_=Rt[d][r][:].rearrange("p g c -> p (g c)"), reads=[bR])
            P.barrier()

    with contextlib.ExitStack() as st2:
        Tb = Ring(P, 2, [128, 8, D], BF16, st2)
        Tb2 = Ring(P, 1, [128, 64, 128], BF16, st2)
        ptu = Ring(P, 2, [128, 8, 128], BF16, st2, psum=True)
        for kb in range(NKL // 128):
            tb, btb = Tb.next()
            src = HN[kb * 1024:(kb + 1) * 1024, :].rearrange("(p s) d -> p s d", s=8)
            pool.dma(out=tb[:], in_=src, writes=[btb])
            tb2, btb2 = Tb2.next()
            dve.op(lambda e, tb=tb, tb2=tb2: e.tensor_copy(
                out=tb2[:].rearrange("p g (s c) -> p s g c", s=8),
                in_=tb[:].rearrange("p s (g c) -> p s g c", c=16)), [btb], [btb2])
            for g0 in range(0, 64, 8):
                pt, bpt = ptu.next()
                for gi in range(8):
                    g = g0 + gi
                    pe.op(lambda e, pt=pt, gi=gi, g=g, tb2=tb2: e.transpose(pt[:, gi, :], tb2[:, g, :],
                                                                           P.identb[:]), [btb2, P.bidb], [bpt])
                act.op(act_fn(U[:, g0:g0 + 8, kb * 128:(kb + 1) * 128], pt[:], AF.Copy), [bpt], [bU])
        P.barrier()

    with contextlib.ExitStack() as sth:
        hist = [P.sb([128, 2, G32, NKL], BF16, stack=sth) for _ in range(2)]
        bhist = Buf()
        with contextlib.ExitStack() as st3:
            Wz = P.sb([128, G32, 4, 128], BF16, stack=st3)
            bWz = Buf()
            WT = [P.sb([128, G32, 8, 16], BF16, stack=st3) for _ in range(2)]
            bWT = Buf()
            ptw = Ring(P, 2, [128, 4, 128], BF16, st3, psum=True)
            pvr = Ring(P, 3, [128, 2, G32, 8], F32, st3, psum=True)
            S4r = Ring(P, 4, [128, 3, G32], F32, st3)
            p1 = P.sb([128, 2, G32], F32, stack=st3)
            p2 = P.sb([128, 2, G32], F32, stack=st3)
            bp1, bp2 = Buf(), Buf()
            gx = [P.sb([128, 2 * G32], F32, stack=st3) for _ in range(2)]
            bgx = Buf()
            for d in range(2):
                pw, bpw = pws[d]
                cprod(WT[0], WT[1], bWT, bbs[d][0][:], bbs[d][1][:], bbs[d][2], pw, bpw,
                      (lambda s: 7 - s) if d == 0 else (lambda s: s))
                pool.op(lambda e: e.memset(Wz[:], 0.0), [], [bWz])
                W3 = (f3(WT[0]), f3(WT[1]))
                for g in range(G32):
                    pt, bpt = ptw.next()
                    for ri in range(2):
                        pe.op(lambda e, pt=pt, g=g, ri=ri: e.transpose(pt[:, ri, :], W3[ri][:, g, :], P.identb[:]),
                              [bWT, P.bidb], [bpt])
                    act.op(act_fn(Wz[:, g, 0:4:2, 0:64], pt[:, 0:2, 0:64], AF.Copy), [bpt], [bWz])
                    act.op(act_fn(Wz[:, g, 1:4:2, 64:128], pt[:, 0:2, 64:128], AF.Copy), [bpt], [bWz])
                S4, bS = S4r.next()
                if d == 0:
                    dve.op(lambda e, S4=S4: e.memset(S4[:], 0.0), [], [bS])
                else:
                    sp.dma(out=gx[0][:], in_=SAall.ap()[0:128, :], writes=[bgx])
                    sp.dma(out=gx[1][:], in_=SAall.ap()[128:256, :], writes=[bgx])
                    dve.op(ts_fn(gx[0][:], gx[0][:], flg[:, 0:1], None, ALU.mult), [bgx, bflg], [bgx])
                    S2v = S4[:, 0:2, :].rearrange("p r g -> p (r g)")
                    dve.op(stt_fn(S2v, gx[1][:], flg[:, 1:2], gx[0][:], ALU.mult, ALU.add), [bgx, bflg], [bS])
                    dve.op(lambda e, S4=S4: e.tensor_copy(out=S4[:, 2, :], in_=S4[:, 0, :]), [bS], [bS])
                a8 = A8[d]
                for wi in range(NWL):
                    w = wi if d == 0 else NWL - 1 - wi
                    pv, bpv = pvr.next()
                    for g in range(G32):
                        for ri in range(2):
                            pe.op(mm_fn(pv[:, ri, g, :], Wz[:, g, 2 * ri, :], U[:, g, 8 * w:8 * w + 8], True, False),
                                  [bWz, bU], [bpv])
                            pe.op(mm_fn(pv[:, ri, g, :], Wz[:, g, 2 * ri + 1, :], U[:, G32 + g, 8 * w:8 * w + 8],
                                        False, True), [bWz, bU], [bpv])
                    dve.selfsync = SCAN_SELFSYNC
                    for ji in range(8):
                        jj = ji if d == 0 else 7 - ji
                        k = 8 * w + jj
                        act.op(act_fn(hist[d][:, :, :, k], S4[:, 0:2, :], AF.Copy), [bS], [bhist])
                        Sn, bSn = S4r.next()
                        dve.op(tt_fn(p1[:], a8[:, 0, :, :], S4[:, 0:2, :], ALU.mult), [bA8, bS], [bp1])
                        dve.op(tt_fn(p2[:], a8[:, 1, :, :], S4[:, 1:3, :], ALU.mult), [bA8, bS], [bp2])
                        dve.op(tt_fn(p1[:], p1[:], p2[:], ALU.add), [bp1, bp2], [bp1])
                        dve.op(tt_fn(Sn[:, 0:2, :], p1[:], pv[:, :, :, jj], ALU.add), [bp1, bpv], [bSn])
                        dve.op(lambda e, Sn=Sn: e.tensor_copy(out=Sn[:, 2, :], in_=Sn[:, 0, :]), [bSn], [bSn])
                        S4, bS = Sn, bSn
                    dve.selfsync = True
                if d == 0:
                    sp.dma(out=SAo.ap(), in_=S4[:, 0:2, :].rearrange("p r g -> p (r g)"), reads=[bS])
                    P.all_gather_pair(SAo, SAall)
            P.barrier()
        with contextlib.ExitStack() as st4:
            Z = P.sb([128, 8, D // 2], F32, stack=st4)
            bZ = Buf()
            M = P.sb([128, 64, 128], BF16, stack=st4)
            Rt = [[P.sb([128, G32, 128], BF16, stack=st4) for r in range(2)] for d in range(2)]
            bM, bR = Buf(), Buf()
            sp.dma(out=M[:].rearrange("p g c -> p (g c)"), in_=MS.ap(), writes=[bM])
            for d in range(2):
                for r in range(2):
                    sp.dma(out=Rt[d][r][:].rearrange("p g c -> p (g c)"),
                           in_=RS.ap()[:, (2 * d + r) * G32 * 128:(2 * d + r + 1) * G32 * 128], writes=[bR])
            p1r = Ring(P, 2, [128, 512], F32, st4, psum=True)
            p2r = [Ring(P, 1, [128, 512], F32, st4, psum=True) for _ in range(2)]
            pTr = Ring(P, 2, [128, 4, 128], F32, st4, psum=True)
            c1r = Ring(P, 2, [128, 512], F32, st4)
            ygr = Ring(P, 2, [128, 512], F32, st4)
            for kb in range(NKL // 128):
                ks = slice(kb * 128, (kb + 1) * 128)
                for hf in range(2):
                    r0 = 64 * hf
                    for q in range(G32 // 4):
                        P1, bP1 = p1r.next()
                        P2, bP2 = p2r[hf].next()
                        for gi in range(4):
                            g32 = 4 * q + gi
                            g = hf * G32 + g32
                            cs = slice(gi * 128, (gi + 1) * 128)
                            pe.op(mm_fn(P1[:, cs], M[:, g, :], U[:, g, ks], True, True), [bM, bU], [bP1])
                            seq = [(Rt[0][0], hist[0], 0), (Rt[0][1], hist[0], 1), (Rt[1][0], hist[1], 0),
                                   (Rt[1][1], hist[1], 1)]
                            for n, (Rm, hs, ri) in enumerate(seq):
                                pe.op(mm_fn(P2[:, cs], Rm[r0:r0 + 64, g32, :], hs[r0:r0 + 64, ri, g32, ks],
                                            n == 0, n == 3), [bR, bhist], [bP2])
                        c1, bc1 = c1r.next()
                        act.op(act_fn(c1[:], P1[:], AF.Copy), [bP1], [bc1])
                        yg, byg = ygr.next()
                        dve.op(tt_fn(yg[:], P2[:], c1[:], ALU.add), [bP2, bc1], [byg])
                        pT, bpT = pTr.next()
                        for gi in range(4):
                            pe.op(lambda e, pT=pT, gi=gi, yg=yg: e.transpose(pT[:, gi, :], yg[:, gi * 128:(gi + 1) * 128],
                                                                             P.identf), [byg, P.bcst], [bpT])
                        c0 = 16 * (4 * q)
                        zdst = Z[:, :, c0:c0 + 64].rearrange("p t (g c) -> p t g c", g=4)
                        zsrc = pT[:].rearrange("p g (t c) -> p t g c", t=8)
                        act.op(act_fn(zdst, zsrc, AF.Copy), [bpT], [bZ])
                    dst = ZS[kb * 1024:(kb + 1) * 1024, hf * 512:(hf + 1) * 512].rearrange("(p t) d -> p t d", t=8)
                    sp.dma(out=dst, in_=Z[:], reads=[bZ])


def build_fused(ncores=NCORES):
    P = Prog()
    P.ncores = ncores
    xw = P.inp("xw", [WIN, D])
    g0 = P.inp("g0", [1, D])
    w_in = P.inp("w_in", [D, 2560])
    cvec = P.inp("cvec", [128, 4, 34])
    qk_g = P.inp("qk_g", [128, 2])
    w_out = P.inp("w_out", [D, D])
    eb = P.inp("eb", [4, 128, 6, 384])
    cst_in = P.inp("cst", [128, 6, 128])
    gf = P.inp("gf", [2, D])
    w_r = P.inp("w_r", [2, D, 16])
    b_r = P.inp("b_r", [2, 16])
    wg = P.inp("wg", [2, NEXP, D, D])
    wu = P.inp("wu", [2, NEXP, D, D])
    wd = P.inp("wd", [2, NEXP, D, D])
    gn = P.inp("gn", [1, D])
    s5p = P.inp("s5p", [128, 2, 3, G32])
    s5B = P.inp("s5B", [128, 2, 2, G32, 16])
    s5C = P.inp("s5C", [128, 2, 2, G32, 16])
    dvec = P.inp("dvec", [1, D])
    masks = P.inp("masks", [128, 2, 128])
    flags = P.inp("flags", [128, 2])
    w_glu = P.inp("w_glu", [D, 2 * D])
    zeros_in = P.inp("zeros", [1024, D], BF16)
    out = P.outp("out", [OWN, D])
    X1 = P.dram("X1", [OWN, D])
    X2 = P.dram("X2", [OWN, D])
    X3 = P.dram("X3", [OWN, D])
    HN = P.dram("HN", [OWN, D])
    ZS = P.dram("ZS", [OWN, D])
    AFF = P.dram("AFF", [OWN, NEXP])
    ATo = P.dram("ATo", [NEXP, OWN])
    ATall = P.dram("ATall", [2 * NEXP, OWN])
    SAo = P.dram("SAo", [128, 2 * G32])
    SAall = P.dram("SAall", [256, 2 * G32])
    MS = P.dram("MS", [128, 64 * 128], BF16)
    RS = P.dram("RS", [128, 4 * G32 * 128], BF16)
    Xc = P.dram("Xc", [NEXP * CAP, D], BF16)
    Yc = P.dram("Yc", [NEXP * CAP, D], BF16)
    load_consts(P, cst_in)
    if SPARSE:
        for i in range(NEXP * CAP // 1024):
            P.sp.dma(out=Xc.ap()[i * 1024:(i + 1) * 1024, :], in_=zeros_in)
    emit_phaseA(P, dict(xw=xw, g0=g0, w_in=w_in, cvec=cvec, qk_g=qk_g, w_out=w_out, gf=gf[0:1, :], w_r=w_r[0],
                        b_r=b_r[0:1, :], eb=eb, x1_out=X1.ap(), aff_out=AFF.ap(), affT_out=ATo.ap()))
    P.all_gather_pair(ATo, ATall)
    at_view = ATall.ap().rearrange("(r e) t -> e r t", r=2)
    emitB = emit_phaseB_sparse if SPARSE else emit_phaseB
    emitB(P, dict(x1=X1.ap(), affT_seq=at_view, aff_own=AFF.ap(), gf=gf[0:1, :], wg=wg[0], wu=wu[0], wd=wd[0],
                  x2_out=X2.ap(), gn=gn, hn_out=HN.ap(), xc=Xc.ap(), yc=Yc.ap()))
    emit_phaseC2(P, dict(s5p=s5p, s5B=s5B, s5C=s5C, masks=masks, flags=flags, hn=HN.ap(), zs_out=ZS.ap(),
                         sa_own=SAo, sa_all=SAall, ms=MS, rs=RS))
    emit_phaseD(P, dict(zs=ZS.ap(), hn=HN.ap(), dvec=dvec, x2=X2.ap(), w_glu=w_glu, gf=gf[1:2, :], w_r=w_r[1],
                        b_r=b_r[1:2, :], x3_out=X3.ap(), aff_out=AFF.ap(), affT_out=ATo.ap()))
    P.all_gather_pair(ATo, ATall)
    emitB(P, dict(x1=X3.ap(), affT_seq=at_view, aff_own=AFF.ap(), gf=gf[1:2, :], wg=wg[1], wu=wu[1], wd=wd[1],
                  x2_out=out, xc=Xc.ap(), yc=Yc.ap()))
    return P.finish()


def fused_inputs(c, x, mix_norm_even, w_in, conv_w, conv_b, conv_ln_g, conv_ln_b, q_norm, k_norm,
                 w_out, mix_norm_odd, ssm_lam_re, ssm_lam_im, ssm_log_dt, ssm_b_re, ssm_b_im,
                 ssm_c_re, ssm_c_im, ssm_d, w_glu, ffn_norm, w_router, b_router,
                 w_e_gate, w_e_up, w_e_down, shared):
    b, h = c // 2, c % 2
    cw = conv_w[0] if h == 0 else conv_w[0][::-1]
    cvec = np.zeros((128, 4, 34), np.float32)
    cvec[:, :, 0:31] = cw.T.reshape(4, 128, 31).transpose(1, 0, 2)
    cvec[:, :, 31] = conv_b[0].reshape(4, 128).T
    cvec[:, :, 32] = conv_ln_g[0].reshape(4, 128).T
    cvec[:, :, 33] = conv_ln_b[0].reshape(4, 128).T
    qk = np.stack([np.tile(q_norm[0], 2), np.tile(k_norm[0], 2)], axis=1).astype(np.float32)
    order = [0, 1] if h == 0 else [1, 0]

    def gp(a):
        a = a[order]
        return a.reshape(2, 2, G32, 64).transpose(1, 3, 0, 2).reshape(128, 2, G32)

    ldt = np.broadcast_to(ssm_log_dt[0][:, :, None], (2, 64, 64))
    s5p = np.ascontiguousarray(np.stack([gp(ssm_lam_re[0]), gp(ssm_lam_im[0]), gp(ldt)], axis=2))

    def gB(a):
        a = a[order]
        return a.reshape(2, 2, G32, 64, 16).transpose(1, 3, 0, 2, 4).reshape(128, 2, G32, 16)

    def gC(a):
        a = a[order]
        return a.reshape(2, 2, G32, 16, 64).transpose(1, 4, 0, 2, 3).reshape(128, 2, G32, 16)

    s5B = np.ascontiguousarray(np.stack([gB(ssm_b_re[0]), gB(ssm_b_im[0])], axis=2))
    s5C = np.ascontiguousarray(np.stack([gC(ssm_c_re[0]), gC(ssm_c_im[0])], axis=2))
    flags = np.zeros((128, 2), np.float32)
    flags[:, 1 - h] = 1.0
    m = dict(shared)
    m.update({
        "xw": local_view(x[b], h, WIN), "cvec": cvec, "qk_g": np.ascontiguousarray(qk),
        "s5p": s5p, "s5B": s5B, "s5C": s5C, "flags": flags,
    })
    return m


def kernel(x, mix_norm_even, w_in, conv_w, conv_b, conv_ln_g, conv_ln_b, q_norm, k_norm,
           w_out, mix_norm_odd, ssm_lam_re, ssm_lam_im, ssm_log_dt, ssm_b_re, ssm_b_im,
           ssm_c_re, ssm_c_im, ssm_d, w_glu, ffn_norm, w_router, b_router,
           w_e_gate, w_e_up, w_e_down):
    f = lambda a: np.ascontiguousarray(np.asarray(a, dtype=np.float32))
    args = [f(a) for a in (x, mix_norm_even, w_in, conv_w, conv_b, conv_ln_g, conv_ln_b, q_norm, k_norm,
                           w_out, mix_norm_odd, ssm_lam_re, ssm_lam_im, ssm_log_dt, ssm_b_re, ssm_b_im,
                           ssm_c_re, ssm_c_im, ssm_d, w_glu, ffn_norm, w_router, b_router,
                           w_e_gate, w_e_up, w_e_down)]
    (x, mix_norm_even, w_in, conv_w, conv_b, conv_ln_g, conv_ln_b, q_norm, k_norm,
     w_out, mix_norm_odd, ssm_lam_re, ssm_lam_im, ssm_log_dt, ssm_b_re, ssm_b_im,
     ssm_c_re, ssm_c_im, ssm_d, w_glu, ffn_norm, w_router, b_router, w_e_gate, w_e_up, w_e_down) = args
    shared = {
        "g0": np.ascontiguousarray(mix_norm_even[0][None, :]), "w_in": w_in[0], "w_out": w_out[0],
        "eb": host_eb(), "cst": host_consts(), "gf": ffn_norm, "w_r": w_router, "b_r": b_router,
        "wg": w_e_gate, "wu": w_e_up, "wd": w_e_down, "gn": np.ascontiguousarray(mix_norm_odd[0][None, :]),
        "dvec": np.ascontiguousarray(ssm_d[0][None, :]), "masks": host_masks(), "w_glu": w_glu[0],
        "zeros": np.zeros((1024, D), ml_dtypes.bfloat16),
    }
    nc = get_prog("F", build_fused)
    in_maps = [fused_inputs(c, *args, shared) for c in range(NCORES)]
    res = run_bass_kernel_spmd(nc, in_maps, core_ids=list(range(NCORES)))
    return from_local([r["out"] for r in res.results]).astype(np.float32)


BIGIDX = float(1 << 20)
I32 = mybir.dt.int32


def emit_phaseB_sparse(P, io):
    with contextlib.ExitStack() as ph:
        P.cur = ph
        _phaseB_sparse_body(P, io["x1"], io["affT_seq"], io["aff_own"], io["gf"], io["wg"], io["wu"], io["wd"],
                            io["x2_out"], io.get("gn"), io.get("hn_out"), io["xc"], io["yc"])
        P.barrier()
    P.cur = None


def _phaseB_sparse_body(P, x1, affT_seq, aff_own, gf, wg, wu, wd, x2_out, gn, hn_out, Xc, Yc):
    dve, act, pe, pool, sp = P.dve, P.act, P.pe, P.pool, P.sp
    NT = OWN // 128
    if gn is not None:
        gnb = P.sb([128, D], F32, name="gnb")
        bgnb = Buf()
        sp.dma(out=gnb[:], in_=bcast_rows(gn, 128), writes=[bgnb])
    gfb = P.sb([128, D], F32, name="gfb")
    bgfb = Buf()
    sp.dma(out=gfb[:], in_=bcast_rows(gf, 128), writes=[bgfb])
    gate = P.sb([128, NT, NEXP], F32, name="gate")
    bgate = Buf()
    idx = P.sb([128, NT, NEXP], I32, name="idx")
    bidx = Buf()
    if P.bcreg is None:
        P.bcreg = P.nc.gpsimd.alloc_register("bcreg")
        P.nc.gpsimd.reg_mov(P.bcreg, NEXP * CAP - 1)

    with contextlib.ExitStack() as st:
        pthr = P.ps([128, 512], F32, stack=st)
        bpthr = Buf()
        aT = P.sb([NEXP, SEQ], F32, stack=st)
        baT = Buf()
        sp.dma(out=aT[:].rearrange("e (r t) -> e r t", r=2), in_=affT_seq, writes=[baT])
        junk = P.sb([NEXP, SEQ], F32, stack=st)
        bjunk = Buf()
        lo = P.sb([NEXP, 1], F32, stack=st)
        blo = Buf()
        mid = P.sb([NEXP, 1], F32, stack=st)
        bmid = Buf()
        cnt = P.sb([NEXP, 1], F32, stack=st)
        bcnt = Buf()
        ge = P.sb([NEXP, 1], F32, stack=st)
        bge = Buf()
        dve.op(lambda e: e.memset(lo[:], 0.0), [], [blo])
        for it in range(30):
            wk = 2.0 ** (-(it + 1))
            dve.op(ts_fn(mid[:], lo[:], wk, None, ALU.add), [blo], [bmid])
            dve.op(lambda e: e.tensor_scalar(out=junk[:], in0=aT[:], scalar1=mid[:, 0:1], scalar2=0.0,
                                             op0=ALU.is_ge, op1=ALU.add, accum_out=cnt[:, 0:1]),
                   [baT, bmid], [bjunk, bcnt])
            dve.op(ts_fn(ge[:], cnt[:], CAP - 0.5, None, ALU.is_ge), [bcnt], [bge])
            dve.op(stt_fn(lo[:], ge[:], wk, lo[:], ALU.mult, ALU.add), [bge, blo], [blo])
        dthr = P.sb([NEXP, NEXP], F32, stack=st)
        bdthr = Buf()
        dve.op(ts_fn(dthr[:], P.cst[0:NEXP, 0, 0:NEXP], lo[:, 0:1], None, ALU.mult), [P.bcst, blo], [bdthr])
        pe.op(mm_fn(pthr[:, 0:NEXP], P.cst[0:NEXP, 3, :], dthr[:], True, True), [P.bcst, bdthr], [bpthr])
        thrb = P.sb([128, NEXP], F32, stack=st)
        bthrb = Buf()
        act.op(act_fn(thrb[:], pthr[:, 0:NEXP], AF.Copy), [bpthr], [bthrb])
        ao = P.sb([128, NT, NEXP], F32, stack=st)
        bao = Buf()
        sp.dma(out=ao[:], in_=aff_own.rearrange("(t p) e -> p t e", p=128), writes=[bao])
        msk = P.sb([128, NT, NEXP], F32, stack=st)
        bmsk = Buf()

        def bct(t):
            a = t[:]
            return bass.AP(a.tensor, a.offset, [list(a.ap[0]), [0, NT], [1, NEXP]])

        dve.op(tt_fn(msk[:], ao[:], bct(thrb), ALU.is_ge), [bao, bthrb], [bmsk])
        ppos = P.ps([128, 512], F32, stack=st)
        bppos = Buf()
        pcnt = P.ps([128, 512], F32, stack=st)
        bpcnt = Buf()
        m2 = msk[:].rearrange("p t e -> p (t e)")
        pe.op(mm_fn(ppos[:, 0:NT * NEXP], P.cst[:, 4, :], m2, True, True), [P.bcst, bmsk], [bppos])
        pe.op(mm_fn(pcnt[:, 0:NT * NEXP], P.cst[:, 3, :], m2, True, True), [P.bcst, bmsk], [bpcnt])
        csb = P.sb([128, NT, NEXP], F32, stack=st)
        bcsb = Buf()
        act.op(act_fn(csb[:].rearrange("p t e -> p (t e)"), pcnt[:, 0:NT * NEXP], AF.Copy), [bpcnt], [bcsb])
        off = P.sb([128, NT, NEXP], F32, stack=st)
        boff = Buf()
        dve.op(lambda e: e.memset(off[:, 0, :], 0.0), [], [boff])
        for i in range(1, NT):
            dve.op(tt_fn(off[:, i, :], off[:, i - 1, :], csb[:, i - 1, :], ALU.add), [boff, bcsb], [boff])
        pos = P.sb([128, NT, NEXP], F32, stack=st)
        bpos = Buf()
        dve.op(tt_fn(pos[:].rearrange("p t e -> p (t e)"), ppos[:, 0:NT * NEXP],
                     off[:].rearrange("p t e -> p (t e)"), ALU.add), [bppos, boff], [bpos])
        ok = P.sb([128, NT, NEXP], F32, stack=st)
        bok = Buf()
        dve.op(ts_fn(ok[:], pos[:], CAP - 0.5, None, ALU.is_lt), [bpos], [bok])
        dve.op(tt_fn(msk[:], msk[:], ok[:], ALU.mult), [bmsk, bok], [bmsk])
        dve.op(tt_fn(gate[:], msk[:], ao[:], ALU.mult), [bmsk, bao], [bgate])
        ebase = P.cst[:, 5, 0:NEXP]
        eb_bc = bass.AP(ebase.tensor, ebase.offset, [list(ebase.ap[0]), [0, NT], [1, NEXP]])
        dve.op(tt_fn(pos[:], pos[:], eb_bc, ALU.add), [bpos, P.bcst], [bpos])
        dve.op(ts_fn(pos[:], pos[:], -BIGIDX, None, ALU.add), [bpos], [bpos])
        dve.op(tt_fn(pos[:], pos[:], msk[:], ALU.mult), [bpos, bmsk], [bpos])
        dve.op(ts_fn(pos[:], pos[:], BIGIDX, None, ALU.add), [bpos], [bpos])
        dve.op(lambda e: e.tensor_copy(out=idx[:], in_=pos[:]), [bpos], [bidx])
        P.barrier()

    def indirect(out, out_off, in_, in_off, reads, writes):
        s_ = pool
        if s_.dsems is None:
            s_.dsems = [P.newsem("d%d_%s" % (i, s_.name)) for i in range(s_.nslots)]
        j = s_.dn
        slot = j % s_.nslots
        key = (s_, slot)
        prev = 16 * (j // s_.nslots)
        if prev > 0:
            s_._wait((key, s_.dsems[slot], prev))
        s_._deps(reads, writes, True)
        ins = s_.eng.indirect_dma_start(out=out, out_offset=out_off, in_=in_, in_offset=in_off,
                                        bounds_check=P.bcreg, oob_is_err=False)
        ins.then_inc(s_.dsems[slot], 16)
        s_.dn += 1
        s_._mark((key, s_.dsems[slot], prev + 16), reads, writes)

    with contextlib.ExitStack() as st:
        acc = P.sb([128, NT, D], F32, stack=st)
        bacc = [Buf() for _ in range(NT)]
        bXc = [Buf() for _ in range(NEXP)]
        bYc = [Buf() for _ in range(NEXP)]
        small = Ring(P, 8, [128, 1], F32, st)
        with contextlib.ExitStack() as st1:
            junk = Ring(P, 1, [128, D], F32, st1)
            hnr = Ring(P, 3, [128, D], BF16, st1)
            for t in range(NT):
                sp.dma(out=acc[:, t, :], in_=x1[t * 128:(t + 1) * 128, :], writes=[bacc[t]])
                rs, br = rms_rstd(P, acc[:, t, :], bacc[t], D, junk, small)
                hn, bhn = hnr.next()
                dve.op(stt_fn(hn[:], acc[:, t, :], rs[:, 0:1], gfb[:], ALU.mult, ALU.mult), [bacc[t], br, bgfb], [bhn])
                for e in range(NEXP):
                    indirect(Xc[:, :], bass.IndirectOffsetOnAxis(ap=idx[:, t, e:e + 1], axis=0), hn[:, :], None,
                             [bhn, bidx], [bXc[e]])
            P.barrier()
        wgr = Ring(P, 2, [128, 8, D], BF16, st)
        wur = Ring(P, 2, [128, 8, D], BF16, st)
        wdr = Ring(P, 1, [128, 8, D], BF16, st)
        xsr = Ring(P, 1, [128, 4, D], BF16, st)
        xTr = Ring(P, 1, [128, 8, CAP], BF16, st)
        atr = Ring(P, 1, [128, 8, CAP], BF16, st)
        ysr = Ring(P, 1, [128, 4, D], BF16, st)
        sgr = Ring(P, 1, [128, 512], F32, st)
        ybr = Ring(P, 3, [128, D], BF16, st)
        ptr = Ring(P, 2, [128, 8, 128], BF16, st, psum=True)
        pgr = Ring(P, 2, [128, 512], F32, st, psum=True)
        pur = Ring(P, 2, [128, 512], F32, st, psum=True)
        pyr = Ring(P, 2, [128, 512], F32, st, psum=True)
        for yb_, byb_ in zip(ybr.t, ybr.b):
            dve.op(lambda e, yb_=yb_: e.memset(yb_[:], 0.0), [], [byb_])

        def load_w(e, which=((0, 1, 2))):
            wts = []
            for src, ring in [((wg, wgr), (wu, wur), (wd, wdr))[i_] for i_ in which]:
                w, bw = ring.next()
                sv = src[e].rearrange("(k p) f -> p k f", p=128)
                pool.dma(out=w[:, 0:4, :], in_=sv[:, 0:4, :], writes=[bw])
                pool.dma(out=w[:, 4:8, :], in_=sv[:, 4:8, :], writes=[bw])
                wts.append((w, bw))
            return wts

        def gather_back(e):
            for t in range(NT):
                yb, byb = ybr.next()
                indirect(yb[:, :], None, Yc[:, :], bass.IndirectOffsetOnAxis(ap=idx[:, t, e:e + 1], axis=0),
                         [bYc[e], bidx], [byb])
                dve.op(stt_fn(acc[:, t, :], yb[:], gate[:, t, e:e + 1], acc[:, t, :], ALU.mult, ALU.add),
                       [byb, bgate, bacc[t]], [bacc[t]])

        nxt = load_w(0, (0, 1))
        for e in range(NEXP):
            (Wg, bWg), (Wu, bWu) = nxt
            ((Wd, bWd),) = load_w(e, (2,))
            if e + 1 < NEXP:
                nxt = load_w(e + 1, (0, 1))
            xs, bxs = xsr.next()
            sp.dma(out=xs[:], in_=Xc[e * CAP:(e + 1) * CAP, :].rearrange("(s p) d -> p s d", p=128),
                   reads=[bXc[e]], writes=[bxs])
            xT, bxT = xTr.next()
            for sl in range(4):
                pt, bpt = ptr.next()
                for k in range(8):
                    pe.op(lambda en, pt=pt, k=k, xs=xs, sl=sl: en.transpose(pt[:, k, :], xs[:, sl, k * 128:(k + 1) * 128],
                                                                           P.identb[:]), [bxs, P.bidb], [bpt])
                act.op(act_fn(xT[:, :, sl * 128:(sl + 1) * 128], pt[:], AF.Copy), [bpt], [bxT])
            AT, bAT = atr.next()
            for fc in range(8):
                pg, bpg = pgr.next()
                pu, bpu = pur.next()
                for k in range(8):
                    pe.op(mm_fn(pg[:], Wg[:, k, fc * 128:(fc + 1) * 128], xT[:, k, :], k == 0, k == 7), [bWg, bxT], [bpg])
                for k in range(8):
                    pe.op(mm_fn(pu[:], Wu[:, k, fc * 128:(fc + 1) * 128], xT[:, k, :], k == 0, k == 7), [bWu, bxT], [bpu])
                sg, bsg = sgr.next()
                act.op(act_fn(sg[:], pg[:], AF.Silu), [bpg], [bsg])
                dve.op(tt_fn(AT[:, fc, :], sg[:], pu[:], ALU.mult), [bsg, bpu], [bAT])
            ys, bys = ysr.next()
            for sl in range(4):
                for hc in range(2):
                    py, bpy = pyr.next()
                    for fc in range(8):
                        pe.op(mm_fn(py[:], AT[:, fc, sl * 128:(sl + 1) * 128], Wd[:, fc, hc * 512:(hc + 1) * 512],
                                    fc == 0, fc == 7), [bAT, bWd], [bpy])
                    act.op(act_fn(ys[:, sl, hc * 512:(hc + 1) * 512], py[:], AF.Copy), [bpy], [bys])
            sp.dma(out=Yc[e * CAP:(e + 1) * CAP, :].rearrange("(s p) d -> p s d", p=128), in_=ys[:],
                   reads=[bys], writes=[bYc[e]])
            if e >= 1:
                gather_back(e - 1)
        gather_back(NEXP - 1)
        hor = Ring(P, 1, [128, D], F32, st)
        junk = hor
        for t in range(NT):
            sp.dma(out=x2_out[t * 128:(t + 1) * 128, :], in_=acc[:, t, :], reads=[bacc[t]])
            if gn is not None:
                rs, br = rms_rstd(P, acc[:, t, :], bacc[t], D, junk, small)
                ho, bho = hor.next()
                dve.op(stt_fn(ho[:], acc[:, t, :], rs[:, 0:1], gnb[:], ALU.mult, ALU.mult),
                       [bacc[t], br, bgnb], [bho])
                sp.dma(out=hn_out[t * 128:(t + 1) * 128, :], in_=ho[:], reads=[bho])
```

```python
import numpy as np
import ml_dtypes
import contextlib
import concourse.bass as bass
import concourse.mybir as mybir
from concourse.bass_utils import run_bass_kernel_spmd

F32 = mybir.dt.float32
BF16 = mybir.dt.bfloat16
ALU = mybir.AluOpType
AF = mybir.ActivationFunctionType
AX = mybir.AxisListType

NCORES = 8
D = 1024
SEQ = 4096
OWN = 2048
WIN = 3072
EPS = 1e-6
DEBUG = False
SPARSE = True
SCAN_SELFSYNC = False


class Buf:
    __slots__ = ("name", "w", "r")

    def __init__(self, name=""):
        self.name = name
        self.w = None
        self.r = {}


class Stream:
    def __init__(self, P, name, eng):
        self.P = P
        self.name = name
        self.eng = eng
        self.sem = P.newsem("c_" + name)
        self.cnt = 0
        self.seen = {}
        self.nslots = 8
        self.dsems = None
        self.dn = 0
        self.selfsync = name != "pe"

    def _wait(self, tok):
        key, sem, val = tok
        if self.seen.get(key, 0) < val:
            self.eng.wait_ge(sem, val)
            self.seen[key] = val

    def _deps(self, reads, writes, dma):
        toks = []
        for b in reads:
            if b.w is not None:
                toks.append(b.w)
        for b in writes:
            if b.w is not None:
                toks.append(b.w)
            toks.extend(b.r.values())
        for t in toks:
            if dma or t[0] is not self or self.selfsync:
                self._wait(t)

    def _mark(self, tok, reads, writes):
        for b in reads:
            b.r[tok[0]] = tok
        for b in writes:
            b.w = tok
            b.r = {}

    def op(self, fn, reads=(), writes=()):
        if self.P.stopped:
            return None
        self._deps(reads, writes, False)
        ins = fn(self.eng)
        self.cnt += 1
        ins.then_inc(self.sem, 1)
        self._mark((self, self.sem, self.cnt), reads, writes)
        return ins

    def dma(self, out, in_, reads=(), writes=(), **kw):
        if self.P.stopped:
            return None
        if self.dsems is None:
            self.dsems = [self.P.newsem("d%d_%s" % (i, self.name)) for i in range(self.nslots)]
        j = self.dn
        slot = j % self.nslots
        key = (self, slot)
        prev = 16 * (j // self.nslots)
        if prev > 0:
            self._wait((key, self.dsems[slot], prev))
        self._deps(reads, writes, True)
        ins = self.eng.dma_start(out=out, in_=in_, **kw)
        ins.then_inc(self.dsems[slot], 16)
        self.dn += 1
        self._mark((key, self.dsems[slot], prev + 16), reads, writes)
        return ins


class Prog:
    def __init__(self):
        self.nc = bass.Bass("TRN2", target_bir_lowering=False)
        self.stack = contextlib.ExitStack()
        self._n = 0
        self.stopped = False
        self.cur = None
        self.ccsem = None
        self.ccn = 0
        self.ncores = NCORES
        self.bcreg = None
        nc = self.nc
        self.pe = Stream(self, "pe", nc.tensor)
        self.act = Stream(self, "act", nc.scalar)
        self.dve = Stream(self, "dve", nc.vector)
        self.pool = Stream(self, "pool", nc.gpsimd)
        self.sp = Stream(self, "sp", nc.sync)
        self.streams = [self.pe, self.act, self.dve, self.pool, self.sp]

    def newsem(self, name):
        return self.stack.enter_context(self.nc.semaphore(name))

    def inp(self, name, shape, dt=F32):
        return self.nc.dram_tensor(name, list(shape), dt, kind="ExternalInput").ap()

    def outp(self, name, shape, dt=F32):
        return self.nc.dram_tensor(name, list(shape), dt, kind="ExternalOutput").ap()

    def sb(self, shape, dt=F32, name=None, stack=None):
        self._n += 1
        name = "s%d_%s" % (self._n, name or "t")
        return (stack or self.cur or self.stack).enter_context(self.nc.sbuf_tensor(name, list(shape), dt))

    def ps(self, shape, dt=F32, name=None, stack=None):
        self._n += 1
        name = "p%d_%s" % (self._n, name or "t")
        return (stack or self.cur or self.stack).enter_context(self.nc.psum_tensor(name, list(shape), dt))

    def barrier(self):
        toks = []
        for s in self.streams:
            if s.cnt:
                toks.append((s, s.sem, s.cnt))
            if s.dsems is not None:
                for slot in range(s.nslots):
                    n = (s.dn - slot + s.nslots - 1) // s.nslots
                    if n > 0:
                        toks.append(((s, slot), s.dsems[slot], 16 * n))
        for s in self.streams:
            for t in toks:
                if t[0] is not s:
                    s._wait(t)

    def dram(self, name, shape, dt=F32):
        return self.nc.dram_tensor(name, list(shape), dt)

    def all_gather_pair(self, src, dst):
        if self.ccsem is None:
            self.ccsem = self.newsem("ccsem")
            self.ccdummy = self.sb([128, 1], F32, name="ccdummy", stack=self.stack)
            self.bccd = Buf()
        self.barrier()
        groups = [[2 * i, 2 * i + 1] for i in range(self.ncores // 2)]
        self.ccn += 1
        n = self.ccn

        def fn(e):
            e.collective_compute("AllGather", ALU.bypass, replica_groups=groups,
                                 ins=[src.ap().opt()], outs=[dst.ap().opt()]).then_inc(self.ccsem, 1)
            e.wait_ge(self.ccsem, n)
            return e.memset(self.ccdummy[:], 0.0)
        self.pool.op(fn, [], [self.bccd])
        self.barrier()

    def finish(self):
        self.barrier()
        self.stack.close()
        return self.nc


def bcast_rows(ap, nparts):
    n = ap.shape[-1]
    return bass.AP(ap.tensor, ap.offset, [[0, nparts], [1, n]])


class Ring:
    def __init__(self, P, n, shape, dt, stack, psum=False):
        alloc = P.ps if psum else P.sb
        self.t = [alloc(shape, dt, stack=stack) for _ in range(n)]
        self.b = [Buf() for _ in range(n)]
        self.i = -1

    def next(self):
        self.i = (self.i + 1) % len(self.t)
        return self.t[self.i], self.b[self.i]


def act_fn(out, in_, func, **kw):
    return lambda e: e.activation(out=out, in_=in_, func=func, **kw)


def mm_fn(out, lhsT, rhs, start, stop):
    return lambda e: e.matmul(out, lhsT, rhs, start=start, stop=stop)


def tt_fn(out, in0, in1, op):
    return lambda e: e.tensor_tensor(out=out, in0=in0, in1=in1, op=op)


def ts_fn(out, in0, s1, s2, op0, op1=None):
    if op1 is None:
        return lambda e: e.tensor_scalar(out=out, in0=in0, scalar1=s1, scalar2=None, op0=op0)
    return lambda e: e.tensor_scalar(out=out, in0=in0, scalar1=s1, scalar2=s2, op0=op0, op1=op1)


def stt_fn(out, in0, scalar, in1, op0, op1):
    return lambda e: e.scalar_tensor_tensor(out=out, in0=in0, scalar=scalar, in1=in1, op0=op0, op1=op1)


ATT_CFG = (1, 4, 16)


def vtile_list():
    tiles = []
    for ci, d in enumerate(ATT_CFG):
        nb = OWN // (128 * d)
        for c in range(d):
            for j in range(nb + 1):
                i0 = 0 if j == 0 else 128 * j - 64
                tiles.append((ci, d, c, j, c + d * i0))
    return tiles


def rms_rstd(P, x_ap, xb, n, ring_junk, ring_small, pbuf_extra=()):
    junk, bj = ring_junk.next()
    ss, bs = ring_small.next()
    P.act.op(act_fn(junk[:, 0:n], x_ap, AF.Square, accum_out=ss[:, 0:1]), [xb], [bj, bs])
    sd, bd = ring_small.next()
    P.act.op(act_fn(sd[:, 0:1], ss[:, 0:1], AF.Sqrt, scale=1.0 / n, bias=P.eps_t[:, 0:1]), [bs, P.beps], [bd])
    rs, br = ring_small.next()
    P.dve.op(lambda e: e.reciprocal(out=rs[:, 0:1], in_=sd[:, 0:1]), [bd], [br])
    return rs, br


def load_consts(P, cst_in):
    cst = P.sb([128, 6, 128], F32, name="cst")
    P.bcst = Buf("cst")
    P.sp.dma(out=cst[:], in_=cst_in, writes=[P.bcst])
    P.identf = cst[:, 0, :]
    P.ones512 = cst[:, 1, :]
    P.ones64 = cst[:, 2, :]
    P.cst = cst
    idb = P.sb([128, 128], BF16, name="identb")
    P.bidb = Buf("identb")
    P.dve.op(lambda e: e.tensor_copy(out=idb[:], in_=cst[:, 0, :]), [P.bcst], [P.bidb])
    P.identb = idb
    eps_t = P.sb([128, 1], F32, name="eps")
    P.beps = Buf("eps")
    P.dve.op(lambda e: e.memset(eps_t[:], EPS), [], [P.beps])
    P.eps_t = eps_t


def router_tile(P, x1t, bx1, gfb, bgfb, wr, bwr, brb, bbrb, rings, aff_out, affT_out, tt, hf_out=None):
    rs, br = rms_rstd(P, x1t[:], bx1, D, rings["junk"], rings["small"])
    hf, bhf = rings["hf"].next()
    P.dve.op(stt_fn(hf[:], x1t[:], rs[:, 0:1], gfb[:], ALU.mult, ALU.mult), [bx1, br, bgfb], [bhf])
    ptf, bptf = rings["ptf"].next()
    for k in range(8):
        P.pe.op(lambda e, k=k: e.transpose(ptf[:, k, :], hf[:, k * 128:(k + 1) * 128], P.identf),
                [bhf, P.bcst], [bptf])
    hfT, bhfT = rings["hfT"].next()
    P.act.op(act_fn(hfT[:], ptf[:], AF.Copy), [bptf], [bhfT])
    plog, bplog = rings["plog"].next()
    for k in range(8):
        P.pe.op(mm_fn(plog[:, 0:16], hfT[:, k, :], wr[:, k, :], k == 0, k == 7), [bhfT, bwr], [bplog])
    lg, blg = rings["sm16"].next()
    P.dve.op(tt_fn(lg[:], plog[:, 0:16], brb[:], ALU.add), [bplog, bbrb], [blg])
    mx, bmx = rings["small"].next()
    P.dve.op(lambda e: e.reduce_max(out=mx[:, 0:1], in_=lg[:], axis=AX.X, negate=True), [blg], [bmx])
    ex, bex = rings["sm16"].next()
    sm, bsm = rings["small"].next()
    P.act.op(act_fn(ex[:], lg[:], AF.Exp, bias=mx[:, 0:1], scale=1.0, accum_out=sm[:, 0:1]), [blg, bmx], [bex, bsm])
    rsm, brsm = rings["small"].next()
    P.dve.op(lambda e: e.reciprocal(out=rsm[:, 0:1], in_=sm[:, 0:1]), [bsm], [brsm])
    af, baf = rings["sm16"].next()
    P.dve.op(ts_fn(af[:], ex[:], rsm[:, 0:1], None, ALU.mult), [bex, brsm], [baf])
    P.sp.dma(out=aff_out[tt * 128:(tt + 1) * 128, :], in_=af[:], reads=[baf])
    pat, bpat = rings["pat"].next()
    P.pe.op(lambda e: e.transpose(pat[0:16, 0:128], af[:], P.identf), [baf, P.bcst], [bpat])
    aT, baT = rings["aT"].next()
    P.act.op(act_fn(aT[:], pat[0:16, 0:128], AF.Copy), [bpat], [baT])
    P.sp.dma(out=affT_out[:, tt * 128:(tt + 1) * 128], in_=aT[:], reads=[baT])
    return hf, bhf


def emit_phaseA(P, io):
    xw, g0, w_in, cvec, qk_g, w_out = io["xw"], io["g0"], io["w_in"], io["cvec"], io["qk_g"], io["w_out"]
    gf, w_r, b_r, eb = io["gf"], io["w_r"], io["b_r"], io["eb"]
    x1_out, aff_out, affT_out = io["x1_out"], io["aff_out"], io["affT_out"]
    dbg = None
    with contextlib.ExitStack() as ph:
        P.cur = ph
        _phaseA_body(P, xw, g0, w_in, cvec, qk_g, w_out, gf, w_r, b_r, eb, x1_out, aff_out, affT_out)
        P.barrier()
    P.cur = None


def _phaseA_body(P, xw, g0, w_in, cvec, qk_g, w_out, gf, w_r, b_r, eb, x1_out, aff_out, affT_out):
    w_in_v = w_in.rearrange("(k p) e -> p k e", p=128)
    w_out_v = w_out.rearrange("(k p) e -> p k e", p=128)
    w_r_v = w_r.rearrange("(k p) e -> p k e", p=128)

    hT = P.sb([128, 8, WIN], BF16, name="hT")
    bhT = [Buf("hT%d" % i) for i in range(WIN // 128)]
    mixT = P.sb([128, 8, OWN], BF16, name="mixT")
    bmix = [Buf("mix%d" % i) for i in range(8)]
    gb = P.sb([128, D], F32, name="gb")
    bgb = Buf("gb")
    P.sp.dma(out=gb[:], in_=bcast_rows(g0, 128), writes=[bgb])
    cv = P.sb([128, 4, 34], F32, name="cv")
    bcv = Buf("cv")
    P.sp.dma(out=cv[:], in_=cvec, writes=[bcv])
    qkg = P.sb([128, 2], F32, name="qkg")
    bqkg = Buf("qkg")
    P.sp.dma(out=qkg[:], in_=qk_g, writes=[bqkg])

    with contextlib.ExitStack() as st:
        xr = Ring(P, 4, [128, D], F32, st)
        junk = Ring(P, 2, [128, D], F32, st)
        small = Ring(P, 16, [128, 1], F32, st)
        hnr = Ring(P, 4, [128, D], BF16, st)
        ptr = Ring(P, 4, [128, 8, 128], BF16, st, psum=True)
        for tt in range(WIN // 128):
            xt, bx = xr.next()
            P.sp.dma(out=xt[:], in_=xw[tt * 128:(tt + 1) * 128, :], writes=[bx])
            rs, br = rms_rstd(P, xt[:], bx, D, junk, small)
            hn, bhn = hnr.next()
            P.dve.op(stt_fn(hn[:], xt[:], rs[:, 0:1], gb[:], ALU.mult, ALU.mult), [bx, br, bgb], [bhn])
            pt, bpt = ptr.next()
            for k in range(8):
                P.pe.op(lambda e, k=k: e.transpose(pt[:, k, :], hn[:, k * 128:(k + 1) * 128], P.identb[:]),
                        [bhn, P.bidb], [bpt])
            P.act.op(act_fn(hT[:, :, tt * 128:(tt + 1) * 128], pt[:], AF.Copy), [bpt], [bhT[tt]])
        P.barrier()

    def hT_bufs(t0, n):
        return bhT[t0 // 128:(t0 + n + 127) // 128]

    NCV = OWN + 128
    with contextlib.ExitStack() as st:
        hglu = P.sb([128, 4, 15 + NCV], BF16, stack=st)
        bhg = [Buf() for _ in range(4)]
        diag = P.sb([128, 4, 31, 128], BF16, stack=st)
        bdiag = [Buf() for _ in range(4)]
        wcv = Ring(P, 2, [128, 8, 256], BF16, st)
        pmr = Ring(P, 2, [128, 512], F32, st, psum=True)
        pgr = Ring(P, 2, [128, 512], F32, st, psum=True)
        pst = Ring(P, 2, [128, 512], F32, st, psum=True)
        sgr = Ring(P, 2, [128, 512], F32, st)
        vbuf = [Ring(P, 2, [128, 512], F32, st) for _ in range(4)]
        sqr = [Ring(P, 2, [128, 512], F32, st) for _ in range(4)]
        tmp = Ring(P, 10, [128, 512], F32, st)
        zr = Ring(P, 2, [128, 512], F32, st)
        for cc in range(4):
            P.dve.op(lambda e, cc=cc: e.memset(hglu[:, cc, 0:15], 0.0), [], [bhg[cc]])
            for k in range(31):
                P.pool.op(ts_fn(diag[:, cc, k, :], P.identf, cv[:, cc, k:k + 1], None, ALU.mult),
                          [P.bcst, bcv], [bdiag[cc]])
        ntiles = [(i * 512, 512) for i in range(4)] + [(2048, 128)]
        for cc in range(4):
            w, bw = wcv.next()
            P.pool.dma(out=w[:, :, 0:128], in_=w_in_v[:, :, cc * 128:(cc + 1) * 128], writes=[bw])
            P.pool.dma(out=w[:, :, 128:256], in_=w_in_v[:, :, 512 + cc * 128:512 + (cc + 1) * 128], writes=[bw])
            for (t0, n) in ntiles:
                pm, bpm = pmr.next()
                pg, bpg = pgr.next()
                for k in range(8):
                    P.pe.op(mm_fn(pm[:, 0:n], w[:, k, 0:128], hT[:, k, t0:t0 + n], k == 0, k == 7),
                            [bw] + hT_bufs(t0, n), [bpm])
                for k in range(8):
                    P.pe.op(mm_fn(pg[:, 0:n], w[:, k, 128:256], hT[:, k, t0:t0 + n], k == 0, k == 7),
                            [bw] + hT_bufs(t0, n), [bpg])
                sg, bsg = sgr.next()
                P.act.op(act_fn(sg[:, 0:n], pg[:, 0:n], AF.Sigmoid), [bpg], [bsg])
                P.dve.op(tt_fn(hglu[:, cc, 15 + t0:15 + t0 + n], pm[:, 0:n], sg[:, 0:n], ALU.mult),
                         [bpm, bsg], [bhg[cc]])
        for nt in range(4):
            t0 = nt * 512
            vb = []
            for cc in range(4):
                pm, bpm = pmr.next()
                for k in range(31):
                    P.pe.op(mm_fn(pm[:], diag[:, cc, k, :], hglu[:, cc, t0 + k:t0 + k + 512], k == 0, k == 30),
                            [bdiag[cc], bhg[cc]], [bpm])
                v, bv = vbuf[cc].next()
                sq, bsq = sqr[cc].next()
                P.act.op(act_fn(v[:], pm[:], AF.Identity, bias=cv[:, cc, 31:32], scale=1.0), [bpm, bcv], [bv])
                P.act.op(act_fn(sq[:], pm[:], AF.Square, bias=cv[:, cc, 31:32], scale=1.0), [bpm, bcv], [bsq])
                vb.append((v, bv, sq, bsq))
            pmean, bpmean = pst.next()
            pex2, bpex2 = pst.next()
            for cc in range(4):
                P.pe.op(mm_fn(pmean[:], P.ones512, vb[cc][0][:], cc == 0, cc == 3), [P.bcst, vb[cc][1]], [bpmean])
            for cc in range(4):
                P.pe.op(mm_fn(pex2[:], P.ones512, vb[cc][2][:], cc == 0, cc == 3), [P.bcst, vb[cc][3]], [bpex2])
            mean, bmean = tmp.next()
            P.act.op(act_fn(mean[:], pmean[:], AF.Copy), [bpmean], [bmean])
            m2, bm2 = tmp.next()
            P.dve.op(tt_fn(m2[:], mean[:], mean[:], ALU.mult), [bmean], [bm2])
            var, bvar = tmp.next()
            P.dve.op(tt_fn(var[:], pex2[:], m2[:], ALU.subtract), [bpex2, bm2], [bvar])
            sd, bsd = tmp.next()
            P.act.op(act_fn(sd[:], var[:], AF.Sqrt, bias=P.eps_t[:, 0:1], scale=1.0), [bvar, P.beps], [bsd])
            rstd, brstd = tmp.next()
            P.dve.op(lambda e: e.reciprocal(out=rstd[:], in_=sd[:]), [bsd], [brstd])
            for cc in range(4):
                v, bv, sq, bsq = vb[cc]
                z, bz = zr.next()
                P.dve.op(tt_fn(z[:], v[:], mean[:], ALU.subtract), [bv, bmean], [bz])
                P.dve.op(tt_fn(z[:], z[:], rstd[:], ALU.mult), [bz, brstd], [bz])
                P.act.op(act_fn(mixT[:, cc, t0:t0 + 512], z[:], AF.Silu, scale=cv[:, cc, 32:33],
                                bias=cv[:, cc, 33:34]), [bz, bcv], [bmix[cc]])
        P.barrier()

    tiles = vtile_list()
    NT = len(tiles)
    tindex = {(ci, c, j): i for i, (ci, d, c, j, s) in enumerate(tiles)}
    with contextlib.ExitStack() as st:
        wq = Ring(P, 2, [128, 8, 384], BF16, st)
        ebr = Ring(P, 1, [128, 6, 384], F32, st)
        qT = P.sb([128, OWN], BF16, stack=st)
        bqT = Buf()
        kT = P.sb([128, WIN], BF16, stack=st)
        bkT = Buf()
        vT = P.sb([128, WIN], BF16, stack=st)
        bvT = Buf()
        Vt = P.sb([128, NT, 256], BF16, stack=st)
        bVt = [Buf() for _ in range(NT)]
        bones = Buf()
        acc = P.sb([128, 2, OWN], F32, stack=st)
        bacc = [Buf(), Buf()]
        pmr = Ring(P, 2, [128, 512], F32, st, psum=True)
        pst = Ring(P, 1, [128, 512], F32, st, psum=True)
        psr = Ring(P, 2, [128, 512], F32, st, psum=True)
        ptvr = Ring(P, 1, [128, 1024], BF16, st, psum=True)
        por = Ring(P, 2, [128, 512], F32, st, psum=True)
        qfr = Ring(P, 2, [128, 512], F32, st)
        sqr = Ring(P, 2, [128, 512], F32, st)
        tmp = Ring(P, 4, [128, 512], F32, st)
        pex = Ring(P, 3, [128, 256], F32, st)
        pTr = Ring(P, 4, [128, 256], BF16, st)
        rzr = Ring(P, 1, [64, OWN], F32, st)
        sring = Ring.__new__(Ring)
        sring.t = psr.t + pmr.t
        sring.b = psr.b + pmr.b
        sring.i = -1
        Vt4 = Vt[:].rearrange("p t (s c) -> p t s c", s=4)
        P.pool.op(lambda e: e.memset(Vt4[:, :, 1, :], 1.0), [], [bones])
        P.pool.op(lambda e: e.memset(Vt4[:, :, 3, :], 1.0), [], [bones])
        ps_slot = [0]
        po_slot = [0]
        pv_slot = [0]
        psb = [[Buf(), Buf()], [Buf(), Buf()]]
        pob = [[Buf() for _ in range(4)] for _ in range(2)]
        pvb = [Buf() for _ in range(4)]
        for hp in range(4):
            w, bw = wq.next()
            for i, base in enumerate((1024, 1536, 2048)):
                P.pool.dma(out=w[:, :, i * 128:(i + 1) * 128],
                           in_=w_in_v[:, :, base + hp * 128:base + (hp + 1) * 128], writes=[bw])
            ebt, bebt = ebr.next()
            P.sp.dma(out=ebt[:], in_=eb[hp], writes=[bebt])
            for which, ntok, dst, bdst, gcol in ((0, OWN, qT, bqT, 0), (1, WIN, kT, bkT, 1)):
                for nt in range(ntok // 512):
                    t0 = nt * 512
                    pm, bpm = pmr.next()
                    for k in range(8):
                        P.pe.op(mm_fn(pm[:], w[:, k, which * 128:(which + 1) * 128], hT[:, k, t0:t0 + 512],
                                      k == 0, k == 7), [bw] + hT_bufs(t0, 512), [bpm])
                    qf, bqf = qfr.next()
                    sq, bsq = sqr.next()
                    P.act.op(act_fn(qf[:], pm[:], AF.Copy), [bpm], [bqf])
                    P.act.op(act_fn(sq[:], pm[:], AF.Square), [bpm], [bsq])
                    pms, bpms = pst.next()
                    P.pe.op(mm_fn(pms[:], P.ones64, sq[:], True, True), [P.bcst, bsq], [bpms])
                    sd, bsd = tmp.next()
                    P.act.op(act_fn(sd[:], pms[:], AF.Ln, bias=P.eps_t[:, 0:1], scale=1.0), [bpms, P.beps], [bsd])
                    rstd, brstd = tmp.next()
                    P.act.op(act_fn(rstd[:], sd[:], AF.Exp, scale=-0.5), [bsd], [brstd])
                    P.dve.op(tt_fn(qf[:], qf[:], rstd[:], ALU.mult), [bqf, brstd], [bqf])
                    P.dve.op(ts_fn(dst[:, t0:t0 + 512], qf[:], qkg[:, gcol:gcol + 1],
                                   0.125 if which == 0 else 1.0, ALU.mult, ALU.mult), [bqf, bqkg], [bdst])
            for nt in range(WIN // 512):
                t0 = nt * 512
                pm, bpm = pmr.next()
                for k in range(8):
                    P.pe.op(mm_fn(pm[:], w[:, k, 256:384], hT[:, k, t0:t0 + 512], k == 0, k == 7),
                            [bw] + hT_bufs(t0, 512), [bpm])
                P.act.op(act_fn(vT[:, t0:t0 + 512], pm[:], AF.Copy), [bpm], [bvT])
            for ti, (ci, d, c, j, s0) in enumerate(tiles):
                pv, bpv = ptvr.next()
                P.pe.op(lambda e, pv=pv, s0=s0, d=d: e.transpose(pv[:, 0:128], vT[:, s0:s0 + 127 * d + 1:d], P.identb[:]),
                        [bvT, P.bidb], [bpv])
                P.act.op(act_fn(Vt4[:, ti, 0:4:2, :], pv[:, 0:128].rearrange("p (s c) -> p s c", s=2), AF.Copy),
                         [bpv, bones], [bVt[ti]])
            LA = 2
            for hh in range(2):
                r0 = 64 * hh
                items = []
                for ci, d in enumerate(ATT_CFG):
                    nb = OWN // (128 * d)
                    for c in range(d):
                        for j in range(nb + 1):
                            items.append((ci, d, c, j, nb))
                qk_out = {}

                def emit_qk(n):
                    ci, d, c, j, nb = items[n]
                    i0 = 0 if j == 0 else 128 * j - 64
                    ks = c + d * i0
                    jlo, jhi = max(j - 1, 0), min(j, nb - 1)
                    nq = 128 * (jhi - jlo + 1)
                    q0 = c + d * 128 * jlo
                    pst_, pbuf = sring.next()
                    sl = pst_[:, 0:nq]
                    P.pe.op(mm_fn(sl, kT[r0:r0 + 64, ks:ks + 127 * d + 1:d],
                                  qT[r0:r0 + 64, q0:q0 + (nq - 1) * d + 1:d], True, True), [bkT, bqT], [pbuf])
                    qk_out[n] = (sl, pbuf, nq)

                for n in range(min(LA, len(items))):
                    emit_qk(n)
                pts = {}
                for n, (ci, d, c, j, nb) in enumerate(items):
                    sl, pbuf, nq = qk_out.pop(n)
                    pe_, bpe = pex.next()
                    P.act.op(act_fn(pe_[:, 0:nq], sl, AF.Exp), [pbuf], [bpe])
                    if j == 0:
                        ebs = ebt[:, ci * 2 + hh, 256:384]
                    elif j == nb:
                        ebs = ebt[:, ci * 2 + hh, 0:128]
                    else:
                        ebs = ebt[:, ci * 2 + hh, 0:256]
                    pT, bpT = pTr.next()
                    P.dve.op(tt_fn(pT[:, 0:nq], pe_[:, 0:nq], ebs, ALU.mult), [bpe, bebt], [bpT])
                    pts[j] = (pT, bpT, nq)
                    if n + LA < len(items):
                        emit_qk(n + LA)
                    if j >= 1:
                        jb = j - 1
                        pTa, bpTa, nqa = pts[jb]
                        a_off = 0 if jb == 0 else 128
                        po_, pobuf = por.next()
                        osl = po_[:, 0:128]
                        ta = tindex[(ci, c, jb)]
                        tb = tindex[(ci, c, j)]
                        P.pe.op(mm_fn(osl, Vt[:, ta, 128 * hh:128 * hh + 128], pTa[:, a_off:a_off + 128],
                                      True, False), [bVt[ta], bpTa], [pobuf])
                        P.pe.op(mm_fn(osl, Vt[:, tb, 128 * hh:128 * hh + 128], pT[:, 0:128],
                                      False, True), [bVt[tb], bpT], [pobuf])
                        a0 = c + d * 128 * jb
                        dsl = acc[:, hh, a0:a0 + 127 * d + 1:d]
                        if ci == 0:
                            P.dve.op(lambda e, dsl=dsl, osl=osl: e.tensor_copy(out=dsl, in_=osl),
                                     [pobuf], [bacc[hh]])
                        else:
                            P.dve.op(tt_fn(dsl, dsl, osl, ALU.add), [pobuf, bacc[hh]], [bacc[hh]])
                rz, brz = rzr.next()
                P.act.op(act_fn(rz[:], acc[64:128, hh, :], AF.Ln), [bacc[hh]], [brz])
                P.act.op(act_fn(rz[:], rz[:], AF.Exp, scale=-1.0), [brz], [brz])
                P.dve.op(tt_fn(mixT[r0:r0 + 64, 4 + hp, :], acc[0:64, hh, :], rz[:], ALU.mult),
                         [bacc[hh], brz], [bmix[4 + hp]])
        P.barrier()

    with contextlib.ExitStack() as st:
        wo = P.sb([128, 8, D], BF16, stack=st)
        bwo = Buf()
        for k in range(8):
            P.pool.dma(out=wo[:, k, :], in_=w_out_v[:, k, :], writes=[bwo])
        gfb = P.sb([128, D], F32, stack=st)
        bgfb = Buf()
        P.sp.dma(out=gfb[:], in_=bcast_rows(gf, 128), writes=[bgfb])
        wr = P.sb([128, 8, 16], F32, stack=st)
        bwr = Buf()
        P.sp.dma(out=wr[:], in_=w_r_v, writes=[bwr])
        brb = P.sb([128, 16], F32, stack=st)
        bbrb = Buf()
        P.sp.dma(out=brb[:], in_=bcast_rows(b_r, 128), writes=[bbrb])
        xr = Ring(P, 2, [128, D], F32, st)
        x1r = Ring(P, 2, [128, D], F32, st)
        pmr = Ring(P, 2, [128, 512], F32, st, psum=True)
        rings = {
            "junk": Ring(P, 1, [128, D], F32, st),
            "small": Ring(P, 12, [128, 1], F32, st),
            "hf": Ring(P, 2, [128, D], F32, st),
            "ptf": Ring(P, 1, [128, 8, 128], F32, st, psum=True),
            "hfT": Ring(P, 2, [128, 8, 128], F32, st),
            "plog": Ring(P, 2, [128, 512], F32, st, psum=True),
            "sm16": Ring(P, 6, [128, 16], F32, st),
            "pat": Ring(P, 1, [128, 512], F32, st, psum=True),
            "aT": Ring(P, 2, [16, 128], F32, st),
        }
        for tt in range(OWN // 128):
            xt, bx = xr.next()
            P.sp.dma(out=xt[:], in_=xw[tt * 128:(tt + 1) * 128, :], writes=[bx])
            x1t, bx1 = x1r.next()
            for half in range(2):
                pm, bpm = pmr.next()
                for k in range(8):
                    P.pe.op(mm_fn(pm[:], mixT[:, k, tt * 128:(tt + 1) * 128], wo[:, k, half * 512:(half + 1) * 512],
                                  k == 0, k == 7), [bmix[k], bwo], [bpm])
                P.dve.op(tt_fn(x1t[:, half * 512:(half + 1) * 512], pm[:], xt[:, half * 512:(half + 1) * 512],
                               ALU.add), [bpm, bx], [bx1])
            P.sp.dma(out=x1_out[tt * 128:(tt + 1) * 128, :], in_=x1t[:], reads=[bx1])
            router_tile(P, x1t, bx1, gfb, bgfb, wr, bwr, brb, bbrb, rings, aff_out, affT_out, tt)
    return


def host_consts():
    cst = np.zeros((128, 6, 128), np.float32)
    cst[:, 0, :] = np.eye(128, dtype=np.float32)
    cst[:, 1, :] = 1.0 / 512.0
    blk = np.zeros((128, 128), np.float32)
    blk[:64, :64] = 1.0 / 64.0
    blk[64:, 64:] = 1.0 / 64.0
    cst[:, 2, :] = blk
    cst[:, 3, :] = 1.0
    cst[:, 4, :] = np.triu(np.ones((128, 128), np.float32), 1)
    cst[:, 5, 0:16] = 512.0 * np.arange(16, dtype=np.float32)[None, :]
    return cst


def host_eb():
    p = np.arange(128)[:, None].astype(np.float64)
    f = np.arange(128)[None, :].astype(np.float64)
    eb = np.zeros((4, 128, 6, 384), np.float32)
    for head in range(8):
        slope = 2.0 ** (-(head + 1))
        hp, hh = head // 2, head % 2
        for ci, d in enumerate(ATT_CFG):
            B = np.where(p <= f, np.exp(-slope * d * np.abs(p + 64 - f)), 0.0)
            A = np.where(p >= f, np.exp(-slope * d * np.abs(p - 64 - f)), 0.0)
            A0 = np.where((p <= 63) & (p >= f - 64), np.exp(-slope * d * np.abs(p - f)), 0.0)
            eb[hp, :, ci * 2 + hh, 0:128] = B
            eb[hp, :, ci * 2 + hh, 128:256] = A
            eb[hp, :, ci * 2 + hh, 256:384] = A0
    return eb


def local_view(xb, h, n):
    if h == 0:
        return np.ascontiguousarray(xb[:n])
    return np.ascontiguousarray(xb[::-1][:n])


_CACHE = {}


def get_prog(name, builder):
    if name not in _CACHE:
        _CACHE[name] = builder()
    return _CACHE[name]


def run_phaseA(x, mix_norm_even, w_in, conv_w, conv_b, conv_ln_g, conv_ln_b, q_norm, k_norm, w_out,
               ffn_norm0, w_router0, b_router0):
    nc = get_prog("A", build_phaseA)
    cst = host_consts()
    eb = host_eb()
    in_maps = []
    for c in range(NCORES):
        b, h = c // 2, c % 2
        cw = conv_w[0] if h == 0 else conv_w[0][::-1]
        cvec = np.zeros((128, 4, 34), np.float32)
        cvec[:, :, 0:31] = cw.T.reshape(4, 128, 31).transpose(1, 0, 2)
        cvec[:, :, 31] = conv_b[0].reshape(4, 128).T
        cvec[:, :, 32] = conv_ln_g[0].reshape(4, 128).T
        cvec[:, :, 33] = conv_ln_b[0].reshape(4, 128).T
        qk = np.stack([np.tile(q_norm[0], 2), np.tile(k_norm[0], 2)], axis=1).astype(np.float32)
        in_maps.append({
            "xw": local_view(x[b], h, WIN),
            "g0": np.ascontiguousarray(mix_norm_even[0][None, :]),
            "w_in": np.ascontiguousarray(w_in[0]),
            "cvec": cvec,
            "qk_g": np.ascontiguousarray(qk),
            "w_out": np.ascontiguousarray(w_out[0]),
            "gf": np.ascontiguousarray(ffn_norm0[None, :]),
            "w_r": np.ascontiguousarray(w_router0),
            "b_r": np.ascontiguousarray(b_router0[None, :]),
            "eb": eb,
            "cst": cst,
        })
    res = run_bass_kernel_spmd(nc, in_maps, core_ids=list(range(NCORES)))
    return res.results


NEXP = 16
CAP = 512


def emit_phaseB(P, io):
    with contextlib.ExitStack() as ph:
        P.cur = ph
        _phaseB_body(P, io["x1"], io["affT_seq"], io["aff_own"], io["gf"], io["wg"], io["wu"], io["wd"],
                     io["x2_out"], io.get("gn"), io.get("hn_out"))
        P.barrier()
    P.cur = None


def _phaseB_body(P, x1, affT_seq, aff_own, gf, wg, wu, wd, x2_out, gn, hn_out):
    if gn is not None:
        gnb = P.sb([128, D], F32, name="gnb")
        bgnb = Buf()
        P.sp.dma(out=gnb[:], in_=bcast_rows(gn, 128), writes=[bgnb])
    gfb = P.sb([128, D], F32, name="gfb")
    bgfb = Buf()
    P.sp.dma(out=gfb[:], in_=bcast_rows(gf, 128), writes=[bgfb])
    gate = P.sb([128, OWN // 128, NEXP], F32, name="gate")
    bgate = Buf()
    pthr = P.ps([128, 512], F32, name="pthr")
    bpthr = Buf()

    with contextlib.ExitStack() as st:
        aT = P.sb([NEXP, SEQ], F32, stack=st)
        baT = Buf()
        P.sp.dma(out=aT[:].rearrange("e (r t) -> e r t", r=2), in_=affT_seq, writes=[baT])
        junk = P.sb([NEXP, SEQ], F32, stack=st)
        bjunk = Buf()
        lo = P.sb([NEXP, 1], F32, stack=st)
        blo = Buf()
        mid = P.sb([NEXP, 1], F32, stack=st)
        bmid = Buf()
        cnt = P.sb([NEXP, 1], F32, stack=st)
        bcnt = Buf()
        ge = P.sb([NEXP, 1], F32, stack=st)
        bge = Buf()
        P.dve.op(lambda e: e.memset(lo[:], 0.0), [], [blo])
        for it in range(36):
            wk = 2.0 ** (-(it + 1))
            P.dve.op(ts_fn(mid[:], lo[:], wk, None, ALU.add), [blo], [bmid])
            P.dve.op(lambda e: e.tensor_scalar(out=junk[:], in0=aT[:], scalar1=mid[:, 0:1], scalar2=0.0,
                                               op0=ALU.is_ge, op1=ALU.add, accum_out=cnt[:, 0:1]),
                     [baT, bmid], [bjunk, bcnt])
            P.dve.op(ts_fn(ge[:], cnt[:], CAP - 0.5, None, ALU.is_ge), [bcnt], [bge])
            P.dve.op(stt_fn(lo[:], ge[:], wk, lo[:], ALU.mult, ALU.add), [bge, blo], [blo])
        dthr = P.sb([NEXP, NEXP], F32, stack=st)
        bdthr = Buf()
        P.dve.op(ts_fn(dthr[:], P.cst[0:NEXP, 0, 0:NEXP], lo[:, 0:1], None, ALU.mult), [P.bcst, blo], [bdthr])
        P.pe.op(mm_fn(pthr[:, 0:NEXP], P.cst[0:NEXP, 3, :], dthr[:], True, True), [P.bcst, bdthr], [bpthr])
        thrb = P.sb([128, NEXP], F32, stack=st)
        bthrb = Buf()
        P.act.op(act_fn(thrb[:], pthr[:, 0:NEXP], AF.Copy), [bpthr], [bthrb])
        ao = P.sb([128, OWN // 128, NEXP], F32, stack=st)
        bao = Buf()
        P.sp.dma(out=ao[:], in_=aff_own.rearrange("(t p) e -> p t e", p=128), writes=[bao])
        msk = P.sb([128, OWN // 128, NEXP], F32, stack=st)
        bmsk = Buf()
        thr_bc = bass.AP(thrb.tensor if hasattr(thrb, "tensor") else thrb[:].tensor, thrb[:].offset,
                         [list(thrb[:].ap[0]), [0, OWN // 128], [1, NEXP]])
        P.dve.op(tt_fn(msk[:], ao[:], thr_bc, ALU.is_ge), [bao, bthrb], [bmsk])
        P.dve.op(tt_fn(gate[:], msk[:], ao[:], ALU.mult), [bmsk, bao], [bgate])
        P.barrier()

    HT = OWN // 2
    with contextlib.ExitStack() as st:
        acc = P.sb([128, HT // 128, D], F32, stack=st)
        bacc = [Buf() for _ in range(HT // 128)]
        hT = P.sb([128, 8, HT], BF16, stack=st)
        bhT = Buf()
        junk = Ring(P, 1, [128, D], F32, st)
        small = Ring(P, 8, [128, 1], F32, st)
        hnr = Ring(P, 2, [128, D], BF16, st)
        hor = Ring(P, 2, [128, D], F32, st)
        ptr = Ring(P, 1, [128, 8, 128], BF16, st, psum=True)
        wgr = Ring(P, 2, [128, 8, D], BF16, st)
        wur = Ring(P, 2, [128, 8, D], BF16, st)
        wdr = Ring(P, 2, [128, 8, D], BF16, st)
        atr = Ring(P, 2, [128, 8, 512], BF16, st)
        sgr = Ring(P, 2, [128, 512], F32, st)
        pgr = Ring(P, 2, [128, 512], F32, st, psum=True)
        pur = Ring(P, 2, [128, 512], F32, st, psum=True)
        pyr = Ring(P, 2, [128, 512], F32, st, psum=True)
        for half in range(2):
            for t in range(HT // 128):
                tg = half * (HT // 128) + t
                P.sp.dma(out=acc[:, t, :], in_=x1[tg * 128:(tg + 1) * 128, :], writes=[bacc[t]])
                rs, br = rms_rstd(P, acc[:, t, :], bacc[t], D, junk, small)
                hn, bhn = hnr.next()
                P.dve.op(stt_fn(hn[:], acc[:, t, :], rs[:, 0:1], gfb[:], ALU.mult, ALU.mult),
                         [bacc[t], br, bgfb], [bhn])
                pt, bpt = ptr.next()
                for k in range(8):
                    P.pe.op(lambda e, k=k, pt=pt, hn=hn: e.transpose(pt[:, k, :], hn[:, k * 128:(k + 1) * 128],
                                                                      P.identb[:]), [bhn, P.bidb], [bpt])
                P.act.op(act_fn(hT[:, :, t * 128:(t + 1) * 128], pt[:], AF.Copy), [bpt], [bhT])
            for e in range(NEXP):
                wts = []
                for src, ring in ((wg, wgr), (wu, wur), (wd, wdr)):
                    w, bw = ring.next()
                    sv = src[e].rearrange("(k p) f -> p k f", p=128)
                    P.pool.dma(out=w[:, 0:4, :], in_=sv[:, 0:4, :], writes=[bw])
                    P.pool.dma(out=w[:, 4:8, :], in_=sv[:, 4:8, :], writes=[bw])
                    wts.append((w, bw))
                (Wg, bWg), (Wu, bWu), (Wd, bWd) = wts
                for q in range(HT // 512):
                    AT, bAT = atr.next()
                    for fc in range(8):
                        pg, bpg = pgr.next()
                        pu, bpu = pur.next()
                        for k in range(8):
                            P.pe.op(mm_fn(pg[:], Wg[:, k, fc * 128:(fc + 1) * 128], hT[:, k, q * 512:(q + 1) * 512],
                                          k == 0, k == 7), [bWg, bhT], [bpg])
                        for k in range(8):
                            P.pe.op(mm_fn(pu[:], Wu[:, k, fc * 128:(fc + 1) * 128], hT[:, k, q * 512:(q + 1) * 512],
                                          k == 0, k == 7), [bWu, bhT], [bpu])
                        sg, bsg = sgr.next()
                        P.act.op(act_fn(sg[:], pg[:], AF.Silu), [bpg], [bsg])
                        P.dve.op(tt_fn(AT[:, fc, :], sg[:], pu[:], ALU.mult), [bsg, bpu], [bAT])
                    for tt in range(4):
                        t = q * 4 + tt
                        tg = half * (HT // 128) + t
                        for hc in range(2):
                            py, bpy = pyr.next()
                            for fc in range(8):
                                P.pe.op(mm_fn(py[:], AT[:, fc, tt * 128:(tt + 1) * 128],
                                              Wd[:, fc, hc * 512:(hc + 1) * 512], fc == 0, fc == 7), [bAT, bWd], [bpy])
                            asl = acc[:, t, hc * 512:(hc + 1) * 512]
                            P.dve.op(stt_fn(asl, py[:], gate[:, tg, e:e + 1], asl, ALU.mult, ALU.add),
                                     [bpy, bgate, bacc[t]], [bacc[t]])
            for t in range(HT // 128):
                tg = half * (HT // 128) + t
                P.sp.dma(out=x2_out[tg * 128:(tg + 1) * 128, :], in_=acc[:, t, :], reads=[bacc[t]])
                if gn is not None:
                    rs, br = rms_rstd(P, acc[:, t, :], bacc[t], D, junk, small)
                    ho, bho = hor.next()
                    P.dve.op(stt_fn(ho[:], acc[:, t, :], rs[:, 0:1], gnb[:], ALU.mult, ALU.mult),
                             [bacc[t], br, bgnb], [bho])
                    P.sp.dma(out=hn_out[tg * 128:(tg + 1) * 128, :], in_=ho[:], reads=[bho])


def run_phaseB(x1_cores, affT_cores, aff_cores, gf, wg, wu, wd, gn):
    nc = get_prog("B", build_phaseB)
    cst = host_consts()
    in_maps = []
    for c in range(NCORES):
        b = c // 2
        affT_seq = np.ascontiguousarray(np.concatenate([affT_cores[2 * b], affT_cores[2 * b + 1]], axis=1))
        in_maps.append({
            "x1": x1_cores[c], "affT_seq": affT_seq, "aff_own": aff_cores[c],
            "gf": np.ascontiguousarray(gf[None, :]), "wg": wg, "wu": wu, "wd": wd, "cst": cst,
            "gn": np.ascontiguousarray(gn[None, :]),
        })
    res = run_bass_kernel_spmd(nc, in_maps, core_ids=list(range(NCORES)))
    return [r["x2"] for r in res.results], [r["hn"] for r in res.results]


NG = 32
NK = SEQ // 8
NWIN = NK // 8
HALF_PI = 1.5707963267948966


class _Stop(Exception):
    pass


def build_phaseC(stop_after=99):
    P = Prog()
    try:
        _phaseC_body(P, stop_after)
    except _Stop:
        P.barrier()
    return P.finish()


def _phaseC_body(P, stop_after):
    lamr_i = P.inp("lamr", [128, NG])
    lami_i = P.inp("lami", [128, NG])
    ldt_i = P.inp("ldt", [128, NG])
    B_i = P.inp("Bri", [128, 2, NG, 16])
    C_i = P.inp("Cri", [128, 2, NG, 16])
    dcol_i = P.inp("dcol", [128, NG])
    Unat = P.inp("Unat", [NG, 128, NK])
    Uw = P.inp("Uw", [NWIN, 128, NG, 8])
    Uwr = P.inp("Uwr", [NWIN, 128, NG, 8])
    masks_i = P.inp("masks", [128, 2, 128])
    cst_in = P.inp("cst", [128, 6, 128])
    zp = P.outp("zp", [NG, 128, NK])
    load_consts(P, cst_in)
    dve, act, pe, pool, sp = P.dve, P.act, P.pe, P.pool, P.sp

    M = P.sb([128, NG, 128], BF16, name="M")
    bM = Buf()
    Wz = P.sb([128, NG, 4, 128], BF16, name="Wz")
    bWz = Buf()
    Rr = P.sb([128, NG, 128], BF16, name="Rr")
    Ri = P.sb([128, NG, 128], BF16, name="Ri")
    bR = Buf()
    A8 = P.sb([128, 2, 2, NG], F32, name="A8")
    bA8 = Buf()
    dcol = P.sb([128, NG], F32, name="dcol")
    bdcol = Buf()
    sp.dma(out=dcol[:], in_=dcol_i, writes=[bdcol])

    def small(st, name=None):
        return P.sb([128, NG], F32, stack=st), Buf()

    with contextlib.ExitStack() as st:
        lamr, blamr = small(st)
        lami, blami = small(st)
        ldt, bldt = small(st)
        sp.dma(out=lamr[:], in_=lamr_i, writes=[blamr])
        sp.dma(out=lami[:], in_=lami_i, writes=[blami])
        sp.dma(out=ldt[:], in_=ldt_i, writes=[bldt])
        Bt = P.sb([128, 2, NG, 16], F32, stack=st)
        bBt = Buf()
        Ct = P.sb([128, 2, NG, 16], F32, stack=st)
        bCt = Buf()
        sp.dma(out=Bt[:], in_=B_i, writes=[bBt])
        sp.dma(out=Ct[:], in_=C_i, writes=[bCt])
        mk = P.sb([128, 2, 128], F32, stack=st)
        bmk = Buf()
        sp.dma(out=mk[:], in_=masks_i, writes=[bmk])
        hpi = P.sb([128, 1], F32, stack=st)
        bhpi = Buf()
        dve.op(lambda e: e.memset(hpi[:], HALF_PI), [], [bhpi])

        def T2(a, ba, b, bb, op):
            o, bo = small(st)
            dve.op(tt_fn(o[:], a[:], b[:], op), [ba, bb], [bo])
            return o, bo

        dt, bdt = small(st)
        act.op(act_fn(dt[:], ldt[:], AF.Exp), [bldt], [bdt])
        a_, ba_ = T2(lamr, blamr, dt, bdt, ALU.mult)
        ang, bang = T2(lami, blami, dt, bdt, ALU.mult)
        mag, bmag = small(st)
        act.op(act_fn(mag[:], a_[:], AF.Exp, scale=1.0 / 16), [ba_], [bmag])
        s16, bs16 = small(st)
        act.op(act_fn(s16[:], ang[:], AF.Sin, scale=1.0 / 16), [bang], [bs16])
        c16, bc16 = small(st)
        act.op(act_fn(c16[:], ang[:], AF.Sin, scale=1.0 / 16, bias=hpi[:, 0:1]), [bang, bhpi], [bc16])
        re, bre = T2(mag, bmag, c16, bc16, ALU.mult)
        im, bim = T2(mag, bmag, s16, bs16, ALU.mult)
        for _ in range(4):
            r2, br2 = T2(re, bre, re, bre, ALU.mult)
            i2, bi2 = T2(im, bim, im, bim, ALU.mult)
            nre, bnre = T2(r2, br2, i2, bi2, ALU.subtract)
            nim, bnim = small(st)
            dve.op(stt_fn(nim[:], re[:], 2.0, im[:], ALU.mult, ALU.mult), [bre, bim], [bnim])
            re, bre, im, bim = nre, bnre, nim, bnim
        pw = P.sb([128, 9, 2, NG], F32, stack=st)
        bpw = Buf()
        ipw = P.sb([128, 9, 2, NG], F32, stack=st)
        bipw = Buf()
        dve.op(lambda e: e.memset(pw[:, 0, 0, :], 1.0), [], [bpw])
        dve.op(lambda e: e.memset(pw[:, 0, 1, :], 0.0), [], [bpw])
        dve.op(lambda e: e.memset(ipw[:, 0, 0, :], 1.0), [], [bipw])
        dve.op(lambda e: e.memset(ipw[:, 0, 1, :], 0.0), [], [bipw])
        dve.op(lambda e: e.tensor_copy(out=pw[:, 1, 0, :], in_=re[:]), [bre], [bpw])
        dve.op(lambda e: e.tensor_copy(out=pw[:, 1, 1, :], in_=im[:]), [bim], [bpw])
        t1, bt1 = small(st)
        t2, bt2 = small(st)
        for n in range(1, 8):
            dve.op(tt_fn(t1[:], pw[:, n, 0, :], re[:], ALU.mult), [bpw, bre], [bt1])
            dve.op(tt_fn(t2[:], pw[:, n, 1, :], im[:], ALU.mult), [bpw, bim], [bt2])
            dve.op(tt_fn(pw[:, n + 1, 0, :], t1[:], t2[:], ALU.subtract), [bt1, bt2], [bpw])
            dve.op(tt_fn(t1[:], pw[:, n, 0, :], im[:], ALU.mult), [bpw, bim], [bt1])
            dve.op(tt_fn(t2[:], pw[:, n, 1, :], re[:], ALU.mult), [bpw, bre], [bt2])
            dve.op(tt_fn(pw[:, n + 1, 1, :], t1[:], t2[:], ALU.add), [bt1, bt2], [bpw])
        en, ben = small(st)
        for n in range(1, 9):
            act.op(act_fn(en[:], a_[:], AF.Exp, scale=-2.0 * n), [ba_], [ben])
            dve.op(tt_fn(ipw[:, n, 0, :], pw[:, n, 0, :], en[:], ALU.mult), [bpw, ben], [bipw])
            dve.op(stt_fn(ipw[:, n, 1, :], pw[:, n, 1, :], -1.0, en[:], ALU.mult, ALU.mult), [bpw, ben], [bipw])
        dve.op(lambda e: e.tensor_copy(out=A8[:, 0, 0, :], in_=pw[:, 8, 0, :]), [bpw], [bA8])
        dve.op(lambda e: e.tensor_copy(out=A8[:, 0, 1, :], in_=pw[:, 8, 0, :]), [bpw], [bA8])
        dve.op(ts_fn(A8[:, 1, 0, :], pw[:, 8, 1, :], -1.0, None, ALU.mult), [bpw], [bA8])
        dve.op(lambda e: e.tensor_copy(out=A8[:, 1, 1, :], in_=pw[:, 8, 1, :]), [bpw], [bA8])
        lr2, blr2 = T2(lamr, blamr, lamr, blamr, ALU.mult)
        li2, bli2 = T2(lami, blami, lami, blami, ALU.mult)
        den, bden = T2(lr2, blr2, li2, bli2, ALU.add)
        rden, brden = small(st)
        dve.op(lambda e: e.reciprocal(out=rden[:], in_=den[:]), [bden], [brden])
        nr, bnr = small(st)
        dve.op(ts_fn(nr[:], re[:], -1.0, None, ALU.add), [bre], [bnr])
        u1, bu1 = T2(nr, bnr, lamr, blamr, ALU.mult)
        u2, bu2 = T2(im, bim, lami, blami, ALU.mult)
        u3, bu3 = T2(u1, bu1, u2, bu2, ALU.add)
        fre, bfre = T2(u3, bu3, rden, brden, ALU.mult)
        u4, bu4 = T2(im, bim, lamr, blamr, ALU.mult)
        u5, bu5 = T2(nr, bnr, lami, blami, ALU.mult)
        u6, bu6 = T2(u4, bu4, u5, bu5, ALU.subtract)
        fim, bfim = T2(u6, bu6, rden, brden, ALU.mult)

        def bc16_(t):
            a = t[:]
            return bass.AP(a.tensor, a.offset, [list(a.ap[0]), [1, NG], [0, 16]])

        big = lambda: (P.sb([128, NG, 16], F32, stack=st), Buf())
        bbr, bbbr = big()
        bbi, bbbi = big()
        g1, bg1 = big()
        g2, bg2 = big()
        dve.op(tt_fn(g1[:], Bt[:, 0, :, :], bc16_(fre), ALU.mult), [bBt, bfre], [bg1])
        dve.op(tt_fn(g2[:], Bt[:, 1, :, :], bc16_(fim), ALU.mult), [bBt, bfim], [bg2])
        dve.op(tt_fn(bbr[:], g1[:], g2[:], ALU.subtract), [bg1, bg2], [bbbr])
        dve.op(tt_fn(g1[:], Bt[:, 1, :, :], bc16_(fre), ALU.mult), [bBt, bfre], [bg1])
        dve.op(tt_fn(g2[:], Bt[:, 0, :, :], bc16_(fim), ALU.mult), [bBt, bfim], [bg2])
        dve.op(tt_fn(bbi[:], g1[:], g2[:], ALU.add), [bg1, bg2], [bbbi])

        def table(top, bot):
            t = P.sb([128, 8, 2, NG], F32, stack=st)
            bt = Buf()
            for i in range(8):
                (ta, tn), (ba, bn) = top[i], bot[i]
                pool.op(lambda e, i=i, ta=ta, tn=tn: e.tensor_copy(out=t[0:64, i, :, :], in_=ta[0:64, tn, :, :]),
                        [bpw, bipw], [bt])
                pool.op(lambda e, i=i, ba=ba, bn=bn: e.tensor_copy(out=t[64:128, i, :, :], in_=ba[64:128, bn, :, :]),
                        [bpw, bipw], [bt])
            return t, bt

        QX, bQX = table([(ipw, s) for s in range(8)], [(pw, s) for s in range(8)])
        PY, bPY = table([(pw, t) for t in range(8)], [(ipw, t) for t in range(8)])
        QW, bQW = table([(pw, 7 - s) for s in range(8)], [(pw, s) for s in range(8)])
        PR, bPR = table([(pw, j + 1) for j in range(8)], [(pw, 8 - j) for j in range(8)])

        def tb(t, i, ri):
            a = t[:, i, ri, :]
            return bass.AP(a.tensor, a.offset, [list(a.ap[0]), [1, NG], [0, 16]])

        def cprod(dst_re, dst_im, bdst, xr, xi, bx, tab, btab, neg_im=False, eng=None):
            eng = eng or dve
            bx = list(bx) if isinstance(bx, (list, tuple)) else [bx]
            for i in range(8):
                eng.op(tt_fn(g1[:], xr[:], tb(tab, i, 0), ALU.mult), bx + [btab], [bg1])
                eng.op(tt_fn(g2[:], xi[:], tb(tab, i, 1), ALU.mult), bx + [btab], [bg2])
                eng.op(tt_fn(dst_re[:, :, i, :], g1[:], g2[:], ALU.subtract), [bg1, bg2], [bdst])
                eng.op(tt_fn(g1[:], xr[:], tb(tab, i, 1), ALU.mult), bx + [btab], [bg1])
                eng.op(tt_fn(g2[:], xi[:], tb(tab, i, 0), ALU.mult), bx + [btab], [bg2])
                if neg_im:
                    eng.op(stt_fn(dst_im[:, :, i, :], g1[:], -1.0, g2[:], ALU.mult, ALU.subtract), [bg1, bg2], [bdst])
                else:
                    eng.op(tt_fn(dst_im[:, :, i, :], g1[:], g2[:], ALU.add), [bg1, bg2], [bdst])

        bbx = Buf()
        with contextlib.ExitStack() as st2:
            Xr = P.sb([128, NG, 8, 16], F32, stack=st2)
            Xi = P.sb([128, NG, 8, 16], F32, stack=st2)
            Yr = P.sb([128, NG, 8, 16], F32, stack=st2)
            Yn = P.sb([128, NG, 8, 16], F32, stack=st2)
            bX, bY = Buf(), Buf()
            bBB = Buf()
            cprod(Xr, Xi, bX, bbr, bbi, [bbbr, bbbi], QX, bQX)
            cprod(Yr, Yn, bY, Ct[:, 0, :, :], Ct[:, 1, :, :], bCt, PY, bPY, neg_im=True)
            psF = Ring(P, 2, [128, 512], F32, st2, psum=True)
            psB = Ring(P, 2, [128, 512], F32, st2, psum=True)
            tmr = Ring(P, 2, [128, 128], F32, st2)
            Xr3 = Xr[:].rearrange("p g s c -> p g (s c)")
            Xi3 = Xi[:].rearrange("p g s c -> p g (s c)")
            Yr3 = Yr[:].rearrange("p g s c -> p g (s c)")
            Yn3 = Yn[:].rearrange("p g s c -> p g (s c)")
            for g in range(NG):
                pf, bpf = psF.next()
                pb_, bpb = psB.next()
                pe.op(mm_fn(pf[:, 0:128], Xr3[0:64, g, :], Yr3[0:64, g, :], True, False), [bX, bY], [bpf])
                pe.op(mm_fn(pf[:, 0:128], Xi3[0:64, g, :], Yn3[0:64, g, :], False, True), [bX, bY], [bpf])
                pe.op(mm_fn(pb_[:, 0:128], Xr3[64:128, g, :], Yr3[64:128, g, :], True, False), [bX, bY], [bpb])
                pe.op(mm_fn(pb_[:, 0:128], Xi3[64:128, g, :], Yn3[64:128, g, :], False, True), [bX, bY], [bpb])
                tm, btm = tmr.next()
                dve.op(tt_fn(tm[:], pf[:, 0:128], mk[:, 0, :], ALU.mult), [bpf, bmk], [btm])
                tm2, btm2 = tmr.next()
                dve.op(tt_fn(tm2[:], pb_[:, 0:128], mk[:, 1, :], ALU.mult), [bpb, bmk], [btm2])
                dve.op(tt_fn(M[:, g, :], tm[:], tm2[:], ALU.add), [btm, btm2], [bM])
            P.barrier()
        if stop_after <= 1:
            P.stopped = True
        with contextlib.ExitStack() as st2:
            Wr = P.sb([128, NG, 8, 16], F32, stack=st2)
            Wi = P.sb([128, NG, 8, 16], F32, stack=st2)
            bW = Buf()
            cprod(Wr, Wi, bW, bbr, bbi, [bbbr, bbbi], QW, bQW)
            pool.op(lambda e: e.memset(Wz[:], 0.0), [], [bWz])
            ptw = Ring(P, 2, [128, 512], F32, st2, psum=True)
            W3 = (Wr[:].rearrange("p g s c -> p g (s c)"), Wi[:].rearrange("p g s c -> p g (s c)"))
            for g in range(NG):
                for ri in range(2):
                    pt, bpt = ptw.next()
                    pe.op(lambda e, pt=pt, g=g, ri=ri: e.transpose(pt[:, 0:128], W3[ri][:, g, :], P.identf),
                          [bW, P.bcst], [bpt])
                    act.op(act_fn(Wz[:, g, 2 * ri, 0:64], pt[:, 0:64], AF.Copy), [bpt], [bWz])
                    act.op(act_fn(Wz[:, g, 2 * ri + 1, 64:128], pt[:, 64:128], AF.Copy), [bpt], [bWz])
            P.barrier()
        if stop_after <= 2:
            P.stopped = True
        Rr4 = Rr[:].rearrange("p g (j c) -> p g j c", j=8)
        Ri4 = Ri[:].rearrange("p g (j c) -> p g j c", j=8)
        cprod(Rr4, Ri4, bR, Ct[:, 0, :, :], Ct[:, 1, :, :], bCt, PR, bPR, neg_im=True)
        P.barrier()

    with contextlib.ExitStack() as st:
        hist = P.sb([128, 2, NG, NK], BF16, stack=st)
        bhist = Buf()
        uwr = Ring(P, 3, [128, NG, 8], BF16, st)
        uwrr = Ring(P, 3, [128, NG, 8], BF16, st)
        pvr = Ring(P, 3, [128, 2, NG, 8], F32, st, psum=True)
        S4r = Ring(P, 4, [128, 3, NG], F32, st)
        p1 = P.sb([128, 2, NG], F32, stack=st)
        p2 = P.sb([128, 2, NG], F32, stack=st)
        bp1, bp2 = Buf(), Buf()
        S4, bS = S4r.next()
        dve.op(lambda e: e.memset(S4[:], 0.0), [], [bS])
        for w in range(NWIN):
            uw, buw = uwr.next()
            uwb, buwb = uwrr.next()
            pool.dma(out=uw[:], in_=Uw[w], writes=[buw])
            pool.dma(out=uwb[:], in_=Uwr[w], writes=[buwb])
            pv, bpv = pvr.next()
            for g in range(NG):
                for ri in range(2):
                    pe.op(mm_fn(pv[:, ri, g, :], Wz[:, g, 2 * ri, :], uw[:, g, :], True, False), [bWz, buw], [bpv])
                    pe.op(mm_fn(pv[:, ri, g, :], Wz[:, g, 2 * ri + 1, :], uwb[:, g, :], False, True), [bWz, buwb], [bpv])
            for jj in range(8):
                j = w * 8 + jj
                act.op(act_fn(hist[0:64, :, :, j], S4[0:64, 0:2, :], AF.Copy), [bS], [bhist])
                act.op(act_fn(hist[64:128, :, :, NK - 1 - j], S4[64:128, 0:2, :], AF.Copy), [bS], [bhist])
                Sn, bSn = S4r.next()
                dve.op(tt_fn(p1[:], A8[:, 0, :, :], S4[:, 0:2, :], ALU.mult), [bA8, bS], [bp1])
                dve.op(tt_fn(p2[:], A8[:, 1, :, :], S4[:, 1:3, :], ALU.mult), [bA8, bS], [bp2])
                dve.op(tt_fn(p1[:], p1[:], p2[:], ALU.add), [bp1, bp2], [bp1])
                dve.op(tt_fn(Sn[:, 0:2, :], p1[:], pv[:, :, :, jj], ALU.add), [bp1, bpv], [bSn])
                dve.op(lambda e, Sn=Sn: e.tensor_copy(out=Sn[:, 2, :], in_=Sn[:, 0, :]), [bSn], [bSn])
                S4, bS = Sn, bSn
        P.barrier()
        if stop_after <= 4:
            P.stopped = True
        ubr = Ring(P, 2, [128, NK], BF16, st)
        ufr = Ring(P, 2, [128, NK], F32, st)
        pyr = Ring(P, 2, [128, 512], F32, st, psum=True)
        yr = Ring(P, 2, [128, NK], F32, st)
        tr = Ring(P, 4, [128, NK], F32, st)
        for g in range(NG):
            ub, bub = ubr.next()
            uf, buf_ = ufr.next()
            pool.dma(out=ub[:], in_=Unat[g], writes=[bub])
            sp.dma(out=uf[:], in_=Unat[g], writes=[buf_])
            py, bpy = pyr.next()
            pe.op(mm_fn(py[:], M[:, g, :], ub[:], True, False), [bM, bub], [bpy])
            pe.op(mm_fn(py[:], Rr[:, g, :], hist[:, 0, g, :], False, False), [bR, bhist], [bpy])
            pe.op(mm_fn(py[:], Ri[:, g, :], hist[:, 1, g, :], False, True), [bR, bhist], [bpy])
            y, by = yr.next()
            dve.op(stt_fn(y[:], uf[:], dcol[:, g:g + 1], py[:], ALU.mult, ALU.add), [buf_, bdcol, bpy], [by])
            a1, ba1 = tr.next()
            dve.op(tt_fn(a1[:], y[:], y[:], ALU.mult), [by], [ba1])
            dve.op(ts_fn(a1[:], a1[:], 0.044715, 1.0, ALU.mult, ALU.add), [ba1], [ba1])
            dve.op(tt_fn(a1[:], a1[:], y[:], ALU.mult), [ba1, by], [ba1])
            a2, ba2 = tr.next()
            act.op(act_fn(a2[:], a1[:], AF.Sigmoid, scale=1.5957691216057308), [ba1], [ba2])
            dve.op(tt_fn(a2[:], a2[:], y[:], ALU.mult), [ba2, by], [ba2])
            sp.dma(out=zp[g], in_=a2[:], reads=[ba2])
    return


def host_masks():
    s = np.arange(128)[:, None] // 16
    t = np.arange(128)[None, :] // 16
    m = np.zeros((128, 2, 128), np.float32)
    m[:, 0, :] = (t >= s)
    m[:, 1, :] = (s >= t)
    return m


def phaseC_inputs(hn1_seq, gh, lam_re, lam_im, log_dt, b_re, b_im, c_re, c_im, d_skip):
    G0 = gh * NG
    sl = slice(G0, G0 + NG)

    def dpg(a):
        return np.ascontiguousarray(a.transpose(0, 2, 1).reshape(128, NG))

    lamr = dpg(lam_re[:, sl, :])
    lami = dpg(lam_im[:, sl, :])
    ldt = dpg(np.broadcast_to(log_dt[:, sl, None], (2, NG, 64)))
    Bri = np.stack([b_re[:, sl].transpose(0, 2, 1, 3).reshape(128, NG, 16),
                    b_im[:, sl].transpose(0, 2, 1, 3).reshape(128, NG, 16)], axis=1)
    Cri = np.stack([c_re[:, sl].transpose(0, 3, 1, 2).reshape(128, NG, 16),
                    c_im[:, sl].transpose(0, 3, 1, 2).reshape(128, NG, 16)], axis=1)
    dg = d_skip[G0 * 16:(G0 + NG) * 16].reshape(NG, 16)
    dcol = np.ascontiguousarray(np.broadcast_to(dg.T[None, :, :], (8, 16, NG)).reshape(128, NG))
    u = hn1_seq[:, G0 * 16:(G0 + NG) * 16].reshape(NK, 8, NG, 16)
    Unat = np.ascontiguousarray(u.transpose(2, 1, 3, 0).reshape(NG, 128, NK))
    Uw = np.ascontiguousarray(Unat.reshape(NG, 128, NWIN, 8).transpose(2, 1, 0, 3))
    Uwr = np.ascontiguousarray(Unat[:, :, ::-1].reshape(NG, 128, NWIN, 8).transpose(2, 1, 0, 3))
    return {"lamr": lamr, "lami": lami, "ldt": ldt, "Bri": np.ascontiguousarray(Bri),
            "Cri": np.ascontiguousarray(Cri), "dcol": dcol, "Unat": Unat, "Uw": Uw, "Uwr": Uwr,
            "masks": host_masks(), "cst": host_consts()}


def phaseC_unpack(zp):
    return np.ascontiguousarray(zp.reshape(NG, 8, 16, NK).transpose(3, 1, 0, 2).reshape(SEQ, NG * 16))


def emit_phaseD(P, io):
    with contextlib.ExitStack() as ph:
        P.cur = ph
        _phaseD_body(P, io["zs"], io["hn"], io["dvec"], io["x2"], io["w_glu"], io["gf"], io["w_r"], io["b_r"],
                     io["x3_out"], io["aff_out"], io["affT_out"])
        P.barrier()
    P.cur = None


def _phaseD_body(P, zt, hn_in, dvec, x2, w_glu, gf, w_r, b_r, x3_out, aff_out, affT_out):
    dvb = P.sb([128, D], F32, name="dvb")
    bdvb = Buf()
    P.sp.dma(out=dvb[:], in_=bcast_rows(dvec, 128), writes=[bdvb])
    st = P.cur
    wgl = P.sb([128, 8, 2 * D], BF16, name="wgl")
    bwgl = Buf()
    wv = w_glu.rearrange("(k p) e -> p k e", p=128)
    for k in range(8):
        P.pool.dma(out=wgl[:, k, :], in_=wv[:, k, :], writes=[bwgl])
    gfb = P.sb([128, D], F32, name="gfb")
    bgfb = Buf()
    P.sp.dma(out=gfb[:], in_=bcast_rows(gf, 128), writes=[bgfb])
    wr = P.sb([128, 8, 16], F32, name="wr")
    bwr = Buf()
    P.sp.dma(out=wr[:], in_=w_r.rearrange("(k p) e -> p k e", p=128), writes=[bwr])
    brb = P.sb([128, 16], F32, name="brb")
    bbrb = Buf()
    P.sp.dma(out=brb[:], in_=bcast_rows(b_r, 128), writes=[bbrb])
    zr = Ring(P, 2, [128, D], F32, st)
    zbr = Ring(P, 2, [128, D], BF16, st)
    hnr2 = Ring(P, 2, [128, D], F32, st)
    xr = Ring(P, 2, [128, D], F32, st)
    x3r = Ring(P, 2, [128, D], F32, st)
    ptr = Ring(P, 1, [128, 8, 128], BF16, st, psum=True)
    zTr = Ring(P, 2, [128, 8, 128], BF16, st)
    pvr = Ring(P, 1, [128, 512], F32, st, psum=True)
    pgr = Ring(P, 1, [128, 512], F32, st, psum=True)
    sgr = Ring(P, 2, [128, 512], F32, st)
    rings = {
        "junk": Ring(P, 1, [128, D], F32, st),
        "small": Ring(P, 12, [128, 1], F32, st),
        "hf": Ring(P, 2, [128, D], F32, st),
        "ptf": Ring(P, 1, [128, 8, 128], F32, st, psum=True),
        "hfT": Ring(P, 2, [128, 8, 128], F32, st),
        "plog": Ring(P, 1, [128, 512], F32, st, psum=True),
        "sm16": Ring(P, 6, [128, 16], F32, st),
        "pat": Ring(P, 1, [128, 512], F32, st, psum=True),
        "aT": Ring(P, 2, [16, 128], F32, st),
    }
    for tt in range(OWN // 128):
        z_, bz = zr.next()
        P.sp.dma(out=z_[:], in_=zt[tt * 128:(tt + 1) * 128, :], writes=[bz])
        xt, bx = xr.next()
        P.sp.dma(out=xt[:], in_=x2[tt * 128:(tt + 1) * 128, :], writes=[bx])
        hn_, bhn_ = hnr2.next()
        P.sp.dma(out=hn_[:], in_=hn_in[tt * 128:(tt + 1) * 128, :], writes=[bhn_])
        P.dve.op(tt_fn(hn_[:], hn_[:], dvb[:], ALU.mult), [bhn_, bdvb], [bhn_])
        P.dve.op(tt_fn(z_[:], z_[:], hn_[:], ALU.add), [bz, bhn_], [bz])
        P.dve.op(tt_fn(hn_[:], z_[:], z_[:], ALU.mult), [bz], [bhn_])
        P.dve.op(ts_fn(hn_[:], hn_[:], 0.044715, 1.0, ALU.mult, ALU.add), [bhn_], [bhn_])
        P.dve.op(tt_fn(hn_[:], hn_[:], z_[:], ALU.mult), [bhn_, bz], [bhn_])
        P.act.op(act_fn(hn_[:], hn_[:], AF.Sigmoid, scale=1.5957691216057308), [bhn_], [bhn_])
        zb, bzb = zbr.next()
        P.dve.op(tt_fn(zb[:], hn_[:], z_[:], ALU.mult), [bhn_, bz], [bzb])
        pt, bpt = ptr.next()
        for k in range(8):
            P.pe.op(lambda e, k=k, pt=pt, zb=zb: e.transpose(pt[:, k, :], zb[:, k * 128:(k + 1) * 128], P.identb[:]),
                    [bzb, P.bidb], [bpt])
        zT, bzT = zTr.next()
        P.act.op(act_fn(zT[:], pt[:], AF.Copy), [bpt], [bzT])
        x3t, bx3 = x3r.next()
        for half in range(2):
            pv, bpv = pvr.next()
            pg, bpg = pgr.next()
            for k in range(8):
                P.pe.op(mm_fn(pv[:], zT[:, k, :], wgl[:, k, half * 512:(half + 1) * 512], k == 0, k == 7),
                        [bzT, bwgl], [bpv])
            for k in range(8):
                P.pe.op(mm_fn(pg[:], zT[:, k, :], wgl[:, k, D + half * 512:D + (half + 1) * 512], k == 0, k == 7),
                        [bzT, bwgl], [bpg])
            sg, bsg = sgr.next()
            P.act.op(act_fn(sg[:], pg[:], AF.Sigmoid), [bpg], [bsg])
            P.dve.op(tt_fn(sg[:], sg[:], pv[:], ALU.mult), [bsg, bpv], [bsg])
            P.dve.op(tt_fn(x3t[:, half * 512:(half + 1) * 512], sg[:], xt[:, half * 512:(half + 1) * 512], ALU.add),
                     [bsg, bx], [bx3])
        P.sp.dma(out=x3_out[tt * 128:(tt + 1) * 128, :], in_=x3t[:], reads=[bx3])
        router_tile(P, x3t, bx3, gfb, bgfb, wr, bwr, brb, bbrb, rings, aff_out, affT_out, tt)
    return


def to_local(full_seq, h):
    return local_view(full_seq, h, OWN)


def from_local(parts):
    out = []
    for b in range(NCORES // 2):
        a0 = parts[2 * b]
        a1 = parts[2 * b + 1][::-1]
        out.append(np.concatenate([a0, a1], axis=0))
    return np.stack(out)


G32 = 32
NKL = OWN // 8
NWL = NKL // 8


def emit_phaseC2(P, io):
    with contextlib.ExitStack() as ph:
        P.cur = ph
        _phaseC2_body(P, io)
        P.barrier()
    P.cur = None


def _phaseC2_body(P, io):
    dve, act, pe, pool, sp = P.dve, P.act, P.pe, P.pool, P.sp
    s5p, s5B, s5C = io["s5p"], io["s5B"], io["s5C"]
    HN, ZS = io["hn"], io["zs_out"]
    SAo, SAall = io["sa_own"], io["sa_all"]

    MS, RS = io["ms"], io["rs"]
    bM = Buf()
    bR = Buf()
    U = P.sb([128, 64, NKL], BF16, name="U")
    bU = Buf()
    A8 = [P.sb([128, 2, 2, G32], F32, name="A8_%d" % d) for d in range(2)]
    bA8 = Buf()
    mk = P.sb([128, 2, 128], F32, name="mk")
    bmk = Buf()
    sp.dma(out=mk[:], in_=io["masks"], writes=[bmk])
    flg = P.sb([128, 2], F32, name="flg")
    bflg = Buf()
    sp.dma(out=flg[:], in_=io["flags"], writes=[bflg])
    hpi = P.sb([128, 1], F32, name="hpi")
    bhpi = Buf()
    dve.op(lambda e: e.memset(hpi[:], HALF_PI), [], [bhpi])
    g1 = P.sb([128, G32, 16], F32, name="g1")
    g2 = P.sb([128, G32, 16], F32, name="g2")
    bg1, bg2 = Buf(), Buf()
    pw_t = [P.sb([128, 9, 2, G32], F32, name="pw%d" % d) for d in range(2)]
    ipw_t = [P.sb([128, 9, 2, G32], F32, name="ipw%d" % d) for d in range(2)]
    bbr_t = [P.sb([128, G32, 16], F32, name="bbr%d" % d) for d in range(2)]
    bbi_t = [P.sb([128, G32, 16], F32, name="bbi%d" % d) for d in range(2)]
    with contextlib.ExitStack() as stp:
        M = P.sb([128, 64, 128], BF16, name="M", stack=stp)
        Rt = [[P.sb([128, G32, 128], BF16, name="R%d%d" % (d, r), stack=stp) for r in range(2)] for d in range(2)]
        par = P.sb([128, 2, 3, G32], F32, name="par", stack=stp)
        bpar = Buf()
        sp.dma(out=par[:], in_=s5p, writes=[bpar])
        Bt = P.sb([128, 2, 2, G32, 16], F32, name="Bt", stack=stp)
        bBt = Buf()
        sp.dma(out=Bt[:], in_=s5B, writes=[bBt])
        Ct = P.sb([128, 2, 2, G32, 16], F32, name="Ct", stack=stp)
        bCt = Buf()
        sp.dma(out=Ct[:], in_=s5C, writes=[bCt])

        def small():
            return P.sb([128, G32], F32, stack=stp), Buf()

        def T2(a, ba, b, bb, op):
            o, bo = small()
            dve.op(tt_fn(o[:], a[:], b[:], op), [ba, bb], [bo])
            return o, bo

        def bc(a):
            return bass.AP(a.tensor, a.offset, [list(a.ap[0]), [1, G32], [0, 16]])

        pws, ipws, bbs = [], [], []
        for d in range(2):
            lamr, lami, ldt = par[:, d, 0, :], par[:, d, 1, :], par[:, d, 2, :]
            dt, bdt = small()
            act.op(act_fn(dt[:], ldt, AF.Exp), [bpar], [bdt])
            a_, ba_ = small()
            dve.op(tt_fn(a_[:], lamr, dt[:], ALU.mult), [bpar, bdt], [ba_])
            ang, bang = small()
            dve.op(tt_fn(ang[:], lami, dt[:], ALU.mult), [bpar, bdt], [bang])
            mag, bmag = small()
            act.op(act_fn(mag[:], a_[:], AF.Exp, scale=1.0 / 16), [ba_], [bmag])
            s16, bs16 = small()
            act.op(act_fn(s16[:], ang[:], AF.Sin, scale=1.0 / 16), [bang], [bs16])
            c16, bc16 = small()
            act.op(act_fn(c16[:], ang[:], AF.Sin, scale=1.0 / 16, bias=hpi[:, 0:1]), [bang, bhpi], [bc16])
            re, bre = T2(mag, bmag, c16, bc16, ALU.mult)
            im, bim = T2(mag, bmag, s16, bs16, ALU.mult)
            for _ in range(4):
                r2, br2 = T2(re, bre, re, bre, ALU.mult)
                i2, bi2 = T2(im, bim, im, bim, ALU.mult)
                nre, bnre = T2(r2, br2, i2, bi2, ALU.subtract)
                nim, bnim = small()
                dve.op(stt_fn(nim[:], re[:], 2.0, im[:], ALU.mult, ALU.mult), [bre, bim], [bnim])
                re, bre, im, bim = nre, bnre, nim, bnim
            pw = pw_t[d]
            ipw = ipw_t[d]
            bpw, bipw = Buf(), Buf()
            dve.op(lambda e, pw=pw: e.memset(pw[:, 0, 0, :], 1.0), [], [bpw])
            dve.op(lambda e, pw=pw: e.memset(pw[:, 0, 1, :], 0.0), [], [bpw])
            dve.op(lambda e, ipw=ipw: e.memset(ipw[:, 0, 0, :], 1.0), [], [bipw])
            dve.op(lambda e, ipw=ipw: e.memset(ipw[:, 0, 1, :], 0.0), [], [bipw])
            dve.op(lambda e, pw=pw, re=re: e.tensor_copy(out=pw[:, 1, 0, :], in_=re[:]), [bre], [bpw])
            dve.op(lambda e, pw=pw, im=im: e.tensor_copy(out=pw[:, 1, 1, :], in_=im[:]), [bim], [bpw])
            t1, bt1 = small()
            t2, bt2 = small()
            for n in range(1, 8):
                dve.op(tt_fn(t1[:], pw[:, n, 0, :], re[:], ALU.mult), [bpw, bre], [bt1])
                dve.op(tt_fn(t2[:], pw[:, n, 1, :], im[:], ALU.mult), [bpw, bim], [bt2])
                dve.op(tt_fn(pw[:, n + 1, 0, :], t1[:], t2[:], ALU.subtract), [bt1, bt2], [bpw])
                dve.op(tt_fn(t1[:], pw[:, n, 0, :], im[:], ALU.mult), [bpw, bim], [bt1])
                dve.op(tt_fn(t2[:], pw[:, n, 1, :], re[:], ALU.mult), [bpw, bre], [bt2])
                dve.op(tt_fn(pw[:, n + 1, 1, :], t1[:], t2[:], ALU.add), [bt1, bt2], [bpw])
            en, ben = small()
            for n in range(1, 9):
                act.op(act_fn(en[:], a_[:], AF.Exp, scale=-2.0 * n), [ba_], [ben])
                dve.op(tt_fn(ipw[:, n, 0, :], pw[:, n, 0, :], en[:], ALU.mult), [bpw, ben], [bipw])
                dve.op(stt_fn(ipw[:, n, 1, :], pw[:, n, 1, :], -1.0, en[:], ALU.mult, ALU.mult), [bpw, ben], [bipw])
            a8 = A8[d]
            dve.op(lambda e, a8=a8, pw=pw: e.tensor_copy(out=a8[:, 0, 0, :], in_=pw[:, 8, 0, :]), [bpw], [bA8])
            dve.op(lambda e, a8=a8, pw=pw: e.tensor_copy(out=a8[:, 0, 1, :], in_=pw[:, 8, 0, :]), [bpw], [bA8])
            dve.op(ts_fn(a8[:, 1, 0, :], pw[:, 8, 1, :], -1.0, None, ALU.mult), [bpw], [bA8])
            dve.op(lambda e, a8=a8, pw=pw: e.tensor_copy(out=a8[:, 1, 1, :], in_=pw[:, 8, 1, :]), [bpw], [bA8])
            lr2, blr2 = small()
            dve.op(tt_fn(lr2[:], lamr, lamr, ALU.mult), [bpar], [blr2])
            li2, bli2 = small()
            dve.op(tt_fn(li2[:], lami, lami, ALU.mult), [bpar], [bli2])
            den, bden = T2(lr2, blr2, li2, bli2, ALU.add)
            rden, brden = small()
            dve.op(lambda e, rden=rden, den=den: e.reciprocal(out=rden[:], in_=den[:]), [bden], [brden])
            nr, bnr = small()
            dve.op(ts_fn(nr[:], re[:], -1.0, None, ALU.add), [bre], [bnr])
            u1, bu1 = small()
            dve.op(tt_fn(u1[:], nr[:], lamr, ALU.mult), [bnr, bpar], [bu1])
            u2, bu2 = small()
            dve.op(tt_fn(u2[:], im[:], lami, ALU.mult), [bim, bpar], [bu2])
            u3, bu3 = T2(u1, bu1, u2, bu2, ALU.add)
            fre, bfre = T2(u3, bu3, rden, brden, ALU.mult)
            u4, bu4 = small()
            dve.op(tt_fn(u4[:], im[:], lamr, ALU.mult), [bim, bpar], [bu4])
            u5, bu5 = small()
            dve.op(tt_fn(u5[:], nr[:], lami, ALU.mult), [bnr, bpar], [bu5])
            u6, bu6 = T2(u4, bu4, u5, bu5, ALU.subtract)
            fim, bfim = T2(u6, bu6, rden, brden, ALU.mult)
            bbr = bbr_t[d]
            bbi = bbi_t[d]
            bbb = Buf()
            dve.op(tt_fn(g1[:], Bt[:, d, 0, :, :], bc(fre[:]), ALU.mult), [bBt, bfre], [bg1])
            dve.op(tt_fn(g2[:], Bt[:, d, 1, :, :], bc(fim[:]), ALU.mult), [bBt, bfim], [bg2])
            dve.op(tt_fn(bbr[:], g1[:], g2[:], ALU.subtract), [bg1, bg2], [bbb])
            dve.op(tt_fn(g1[:], Bt[:, d, 1, :, :], bc(fre[:]), ALU.mult), [bBt, bfre], [bg1])
            dve.op(tt_fn(g2[:], Bt[:, d, 0, :, :], bc(fim[:]), ALU.mult), [bBt, bfim], [bg2])
            dve.op(tt_fn(bbi[:], g1[:], g2[:], ALU.add), [bg1, bg2], [bbb])
            pws.append((pw, bpw))
            ipws.append((ipw, bipw))
            bbs.append((bbr, bbi, bbb))

        def cprod(dst_re, dst_im, bdst, xr, xi, bx, tab, btab, idx, neg_im=False):
            bx = list(bx) if isinstance(bx, (list, tuple)) else [bx]
            for i in range(8):
                tr, ti = bc(tab[:, idx(i), 0, :]), bc(tab[:, idx(i), 1, :])
                dve.op(tt_fn(g1[:], xr, tr, ALU.mult), bx + [btab], [bg1])
                dve.op(tt_fn(g2[:], xi, ti, ALU.mult), bx + [btab], [bg2])
                dve.op(tt_fn(dst_re[:, :, i, :], g1[:], g2[:], ALU.subtract), [bg1, bg2], [bdst])
                dve.op(tt_fn(g1[:], xr, ti, ALU.mult), bx + [btab], [bg1])
                dve.op(tt_fn(g2[:], xi, tr, ALU.mult), bx + [btab], [bg2])
                if neg_im:
                    dve.op(stt_fn(dst_im[:, :, i, :], g1[:], -1.0, g2[:], ALU.mult, ALU.subtract), [bg1, bg2], [bdst])
                else:
                    dve.op(tt_fn(dst_im[:, :, i, :], g1[:], g2[:], ALU.add), [bg1, bg2], [bdst])

        v4 = lambda t: t[:].rearrange("p g (j c) -> p g j c", j=8)
        f3 = lambda t: t[:].rearrange("p g s c -> p g (s c)")

        with contextlib.ExitStack() as st2:
            XY = [[P.sb([128, G32, 8, 16], BF16, stack=st2) for _ in range(4)] for _ in range(2)]
            bXY = Buf()
            (pwA, bpwA), (ipwA, bipwA) = pws[0], ipws[0]
            (pwB, bpwB), (ipwB, bipwB) = pws[1], ipws[1]
            cprod(XY[0][0], XY[0][1], bXY, bbs[0][0][:], bbs[0][1][:], bbs[0][2], ipwA, bipwA, lambda s: s)
            cprod(XY[0][2], XY[0][3], bXY, Ct[:, 0, 0, :, :], Ct[:, 0, 1, :, :], bCt, pwA, bpwA, lambda t: t, neg_im=True)
            cprod(XY[1][0], XY[1][1], bXY, bbs[1][0][:], bbs[1][1][:], bbs[1][2], pwB, bpwB, lambda s: s)
            cprod(XY[1][2], XY[1][3], bXY, Ct[:, 1, 0, :, :], Ct[:, 1, 1, :, :], bCt, ipwB, bipwB, lambda t: t, neg_im=True)
            cprod(v4(Rt[0][0]), v4(Rt[0][1]), bR, Ct[:, 0, 0, :, :], Ct[:, 0, 1, :, :], bCt, pwA, bpwA,
                  lambda j: j + 1, neg_im=True)
            cprod(v4(Rt[1][0]), v4(Rt[1][1]), bR, Ct[:, 1, 0, :, :], Ct[:, 1, 1, :, :], bCt, pwB, bpwB,
                  lambda j: 8 - j, neg_im=True)
            pk = [[Ring(P, 1, [128, 512], F32, st2, psum=True) for _ in range(2)] for _ in range(2)]
            tmr = Ring(P, 4, [128, 128], F32, st2)
            for g in range(G32):
                pp = [[None, None], [None, None]]
                for d in range(2):
                    Xr, Xi, Yr, Yn = [f3(t) for t in XY[d]]
                    for hf in range(2):
                        r0 = 64 * hf
                        pt, bpt = pk[d][hf].next()
                        pe.op(mm_fn(pt[:, 0:128], Xr[r0:r0 + 64, g, :], Yr[r0:r0 + 64, g, :], True, False), [bXY], [bpt])
                        pe.op(mm_fn(pt[:, 0:128], Xi[r0:r0 + 64, g, :], Yn[r0:r0 + 64, g, :], False, True), [bXY], [bpt])
                        pp[d][hf] = (pt, bpt)
                for hf in range(2):
                    tm, btm = tmr.next()
                    dve.op(tt_fn(tm[:], pp[0][hf][0][:, 0:128], mk[:, 0, :], ALU.mult), [pp[0][hf][1], bmk], [btm])
                    tm2, btm2 = tmr.next()
                    dve.op(tt_fn(tm2[:], pp[1][hf][0][:, 0:128], mk[:, 1, :], ALU.mult), [pp[1][hf][1], bmk], [btm2])
                    dve.op(tt_fn(M[:, hf * G32 + g, :], tm[:], tm2[:], ALU.add), [btm, btm2], [bM])
            sp.dma(out=MS.ap(), in_=M[:].rearrange("p g c -> p (g c)"), reads=[bM])
            for d in range(2):
                for r in range(2):
                    sp.dma(out=RS.ap()[:, (2 * d + r) * G32 * 128:(2 * d + r + 1) * G32 * 128],
                           in_=Rt[d][r][:].rearrange("p g c -> p (g c)"), reads=[bR])
            P.barrier()

    with contextlib.ExitStack() as st2:
        Tb = Ring(P, 2, [128, 8, D], BF16, st2)
        Tb2 = Ring(P, 1, [128, 64, 128], BF16, st2)
        ptu = Ring(P, 2, [128, 8, 128], BF16, st2, psum=True)
        for kb in range(NKL // 128):
            tb, btb = Tb.next()
            src = HN[kb * 1024:(kb + 1) * 1024, :].rearrange("(p s) d -> p s d", s=8)
            pool.dma(out=tb[:], in_=src, writes=[btb])
            tb2, btb2 = Tb2.next()
            dve.op(lambda e, tb=tb, tb2=tb2: e.tensor_copy(
                out=tb2[:].rearrange("p g (s c) -> p s g c", s=8),
                in_=tb[:].rearrange("p s (g c) -> p s g c", c=16)), [btb], [btb2])
            for g0 in range(0, 64, 8):
                pt, bpt = ptu.next()
                for gi in range(8):
                    g = g0 + gi
                    pe.op(lambda e, pt=pt, gi=gi, g=g, tb2=tb2: e.transpose(pt[:, gi, :], tb2[:, g, :],
                                                                           P.identb[:]), [btb2, P.bidb], [bpt])
                act.op(act_fn(U[:, g0:g0 + 8, kb * 128:(kb + 1) * 128], pt[:], AF.Copy), [bpt], [bU])
        P.barrier()

    with contextlib.ExitStack() as sth:
        hist = [P.sb([128, 2, G32, NKL], BF16, stack=sth) for _ in range(2)]
        bhist = Buf()
        with contextlib.ExitStack() as st3:
            Wz = P.sb([128, G32, 4, 128], BF16, stack=st3)
            bWz = Buf()
            WT = [P.sb([128, G32, 8, 16], BF16, stack=st3) for _ in range(2)]
            bWT = Buf()
            ptw = Ring(P, 2, [128, 4, 128], BF16, st3, psum=True)
            pvr = Ring(P, 3, [128, 2, G32, 8], F32, st3, psum=True)
            S4r = Ring(P, 4, [128, 3, G32], F32, st3)
            p1 = P.sb([128, 2, G32], F32, stack=st3)
            p2 = P.sb([128, 2, G32], F32, stack=st3)
            bp1, bp2 = Buf(), Buf()
            gx = [P.sb([128, 2 * G32], F32, stack=st3) for _ in range(2)]
            bgx = Buf()
            for d in range(2):
                pw, bpw = pws[d]
                cprod(WT[0], WT[1], bWT, bbs[d][0][:], bbs[d][1][:], bbs[d][2], pw, bpw,
                      (lambda s: 7 - s) if d == 0 else (lambda s: s))
                pool.op(lambda e: e.memset(Wz[:], 0.0), [], [bWz])
                W3 = (f3(WT[0]), f3(WT[1]))
                for g in range(G32):
                    pt, bpt = ptw.next()
                    for ri in range(2):
                        pe.op(lambda e, pt=pt, g=g, ri=ri: e.transpose(pt[:, ri, :], W3[ri][:, g, :], P.identb[:]),
                              [bWT, P.bidb], [bpt])
                    act.op(act_fn(Wz[:, g, 0:4:2, 0:64], pt[:, 0:2, 0:64], AF.Copy), [bpt], [bWz])
                    act.op(act_fn(Wz[:, g, 1:4:2, 64:128], pt[:, 0:2, 64:128], AF.Copy), [bpt], [bWz])
                S4, bS = S4r.next()
                if d == 0:
                    dve.op(lambda e, S4=S4: e.memset(S4[:], 0.0), [], [bS])
                else:
                    sp.dma(out=gx[0][:], in_=SAall.ap()[0:128, :], writes=[bgx])
                    sp.dma(out=gx[1][:], in_=SAall.ap()[128:256, :], writes=[bgx])
                    dve.op(ts_fn(gx[0][:], gx[0][:], flg[:, 0:1], None, ALU.mult), [bgx, bflg], [bgx])
                    S2v = S4[:, 0:2, :].rearrange("p r g -> p (r g)")
                    dve.op(stt_fn(S2v, gx[1][:], flg[:, 1:2], gx[0][:], ALU.mult, ALU.add), [bgx, bflg], [bS])
                    dve.op(lambda e, S4=S4: e.tensor_copy(out=S4[:, 2, :], in_=S4[:, 0, :]), [bS], [bS])
                a8 = A8[d]
                for wi in range(NWL):
                    w = wi if d == 0 else NWL - 1 - wi
                    pv, bpv = pvr.next()
                    for g in range(G32):
                        for ri in range(2):
                            pe.op(mm_fn(pv[:, ri, g, :], Wz[:, g, 2 * ri, :], U[:, g, 8 * w:8 * w + 8], True, False),
                                  [bWz, bU], [bpv])
                            pe.op(mm_fn(pv[:, ri, g, :], Wz[:, g, 2 * ri + 1, :], U[:, G32 + g, 8 * w:8 * w + 8],
                                        False, True), [bWz, bU], [bpv])
                    dve.selfsync = SCAN_SELFSYNC
                    for ji in range(8):
                        jj = ji if d == 0 else 7 - ji
                        k = 8 * w + jj
                        act.op(act_fn(hist[d][:, :, :, k], S4[:, 0:2, :], AF.Copy), [bS], [bhist])
                        Sn, bSn = S4r.next()
                        dve.op(tt_fn(p1[:], a8[:, 0, :, :], S4[:, 0:2, :], ALU.mult), [bA8, bS], [bp1])
                        dve.op(tt_fn(p2[:], a8[:, 1, :, :], S4[:, 1:3, :], ALU.mult), [bA8, bS], [bp2])
                        dve.op(tt_fn(p1[:], p1[:], p2[:], ALU.add), [bp1, bp2], [bp1])
                        dve.op(tt_fn(Sn[:, 0:2, :], p1[:], pv[:, :, :, jj], ALU.add), [bp1, bpv], [bSn])
                        dve.op(lambda e, Sn=Sn: e.tensor_copy(out=Sn[:, 2, :], in_=Sn[:, 0, :]), [bSn], [bSn])
                        S4, bS = Sn, bSn
                    dve.selfsync = True
                if d == 0:
                    sp.dma(out=SAo.ap(), in_=S4[:, 0:2, :].rearrange("p r g -> p (r g)"), reads=[bS])
                    P.all_gather_pair(SAo, SAall)
            P.barrier()
        with contextlib.ExitStack() as st4:
            Z = P.sb([128, 8, D // 2], F32, stack=st4)
            bZ = Buf()
            M = P.sb([128, 64, 128], BF16, stack=st4)
            Rt = [[P.sb([128, G32, 128], BF16, stack=st4) for r in range(2)] for d in range(2)]
            bM, bR = Buf(), Buf()
            sp.dma(out=M[:].rearrange("p g c -> p (g c)"), in_=MS.ap(), writes=[bM])
            for d in range(2):
                for r in range(2):
                    sp.dma(out=Rt[d][r][:].rearrange("p g c -> p (g c)"),
                           in_=RS.ap()[:, (2 * d + r) * G32 * 128:(2 * d + r + 1) * G32 * 128], writes=[bR])
            p1r = Ring(P, 2, [128, 512], F32, st4, psum=True)
            p2r = [Ring(P, 1, [128, 512], F32, st4, psum=True) for _ in range(2)]
            pTr = Ring(P, 2, [128, 4, 128], F32, st4, psum=True)
            c1r = Ring(P, 2, [128, 512], F32, st4)
            ygr = Ring(P, 2, [128, 512], F32, st4)
            for kb in range(NKL // 128):
                ks = slice(kb * 128, (kb + 1) * 128)
                for hf in range(2):
                    r0 = 64 * hf
                    for q in range(G32 // 4):
                        P1, bP1 = p1r.next()
                        P2, bP2 = p2r[hf].next()
                        for gi in range(4):
                            g32 = 4 * q + gi
                            g = hf * G32 + g32
                            cs = slice(gi * 128, (gi + 1) * 128)
                            pe.op(mm_fn(P1[:, cs], M[:, g, :], U[:, g, ks], True, True), [bM, bU], [bP1])
                            seq = [(Rt[0][0], hist[0], 0), (Rt[0][1], hist[0], 1), (Rt[1][0], hist[1], 0),
                                   (Rt[1][1], hist[1], 1)]
                            for n, (Rm, hs, ri) in enumerate(seq):
                                pe.op(mm_fn(P2[:, cs], Rm[r0:r0 + 64, g32, :], hs[r0:r0 + 64, ri, g32, ks],
                                            n == 0, n == 3), [bR, bhist], [bP2])
                        c1, bc1 = c1r.next()
                        act.op(act_fn(c1[:], P1[:], AF.Copy), [bP1], [bc1])
                        yg, byg = ygr.next()
                        dve.op(tt_fn(yg[:], P2[:], c1[:], ALU.add), [bP2, bc1], [byg])
                        pT, bpT = pTr.next()
                        for gi in range(4):
                            pe.op(lambda e, pT=pT, gi=gi, yg=yg: e.transpose(pT[:, gi, :], yg[:, gi * 128:(gi + 1) * 128],
                                                                             P.identf), [byg, P.bcst], [bpT])
                        c0 = 16 * (4 * q)
                        zdst = Z[:, :, c0:c0 + 64].rearrange("p t (g c) -> p t g c", g=4)
                        zsrc = pT[:].rearrange("p g (t c) -> p t g c", t=8)
                        act.op(act_fn(zdst, zsrc, AF.Copy), [bpT], [bZ])
                    dst = ZS[kb * 1024:(kb + 1) * 1024, hf * 512:(hf + 1) * 512].rearrange("(p t) d -> p t d", t=8)
                    sp.dma(out=dst, in_=Z[:], reads=[bZ])


def build_fused(ncores=NCORES):
    P = Prog()
    P.ncores = ncores
    xw = P.inp("xw", [WIN, D])
    g0 = P.inp("g0", [1, D])
    w_in = P.inp("w_in", [D, 2560])
    cvec = P.inp("cvec", [128, 4, 34])
    qk_g = P.inp("qk_g", [128, 2])
    w_out = P.inp("w_out", [D, D])
    eb = P.inp("eb", [4, 128, 6, 384])
    cst_in = P.inp("cst", [128, 6, 128])
    gf = P.inp("gf", [2, D])
    w_r = P.inp("w_r", [2, D, 16])
    b_r = P.inp("b_r", [2, 16])
    wg = P.inp("wg", [2, NEXP, D, D])
    wu = P.inp("wu", [2, NEXP, D, D])
    wd = P.inp("wd", [2, NEXP, D, D])
    gn = P.inp("gn", [1, D])
    s5p = P.inp("s5p", [128, 2, 3, G32])
    s5B = P.inp("s5B", [128, 2, 2, G32, 16])
    s5C = P.inp("s5C", [128, 2, 2, G32, 16])
    dvec = P.inp("dvec", [1, D])
    masks = P.inp("masks", [128, 2, 128])
    flags = P.inp("flags", [128, 2])
    w_glu = P.inp("w_glu", [D, 2 * D])
    zeros_in = P.inp("zeros", [1024, D], BF16)
    out = P.outp("out", [OWN, D])
    X1 = P.dram("X1", [OWN, D])
    X2 = P.dram("X2", [OWN, D])
    X3 = P.dram("X3", [OWN, D])
    HN = P.dram("HN", [OWN, D])
    ZS = P.dram("ZS", [OWN, D])
    AFF = P.dram("AFF", [OWN, NEXP])
    ATo = P.dram("ATo", [NEXP, OWN])
    ATall = P.dram("ATall", [2 * NEXP, OWN])
    SAo = P.dram("SAo", [128, 2 * G32])
    SAall = P.dram("SAall", [256, 2 * G32])
    MS = P.dram("MS", [128, 64 * 128], BF16)
    RS = P.dram("RS", [128, 4 * G32 * 128], BF16)
    Xc = P.dram("Xc", [NEXP * CAP, D], BF16)
    Yc = P.dram("Yc", [NEXP * CAP, D], BF16)
    load_consts(P, cst_in)
    if SPARSE:
        for i in range(NEXP * CAP // 1024):
            P.sp.dma(out=Xc.ap()[i * 1024:(i + 1) * 1024, :], in_=zeros_in)
    emit_phaseA(P, dict(xw=xw, g0=g0, w_in=w_in, cvec=cvec, qk_g=qk_g, w_out=w_out, gf=gf[0:1, :], w_r=w_r[0],
                        b_r=b_r[0:1, :], eb=eb, x1_out=X1.ap(), aff_out=AFF.ap(), affT_out=ATo.ap()))
    P.all_gather_pair(ATo, ATall)
    at_view = ATall.ap().rearrange("(r e) t -> e r t", r=2)
    emitB = emit_phaseB_sparse if SPARSE else emit_phaseB
    emitB(P, dict(x1=X1.ap(), affT_seq=at_view, aff_own=AFF.ap(), gf=gf[0:1, :], wg=wg[0], wu=wu[0], wd=wd[0],
                  x2_out=X2.ap(), gn=gn, hn_out=HN.ap(), xc=Xc.ap(), yc=Yc.ap()))
    emit_phaseC2(P, dict(s5p=s5p, s5B=s5B, s5C=s5C, masks=masks, flags=flags, hn=HN.ap(), zs_out=ZS.ap(),
                         sa_own=SAo, sa_all=SAall, ms=MS, rs=RS))
    emit_phaseD(P, dict(zs=ZS.ap(), hn=HN.ap(), dvec=dvec, x2=X2.ap(), w_glu=w_glu, gf=gf[1:2, :], w_r=w_r[1],
                        b_r=b_r[1:2, :], x3_out=X3.ap(), aff_out=AFF.ap(), affT_out=ATo.ap()))
    P.all_gather_pair(ATo, ATall)
    emitB(P, dict(x1=X3.ap(), affT_seq=at_view, aff_own=AFF.ap(), gf=gf[1:2, :], wg=wg[1], wu=wu[1], wd=wd[1],
                  x2_out=out, xc=Xc.ap(), yc=Yc.ap()))
    return P.finish()


def fused_inputs(c, x, mix_norm_even, w_in, conv_w, conv_b, conv_ln_g, conv_ln_b, q_norm, k_norm,
                 w_out, mix_norm_odd, ssm_lam_re, ssm_lam_im, ssm_log_dt, ssm_b_re, ssm_b_im,
                 ssm_c_re, ssm_c_im, ssm_d, w_glu, ffn_norm, w_router, b_router,
                 w_e_gate, w_e_up, w_e_down, shared):
    b, h = c // 2, c % 2
    cw = conv_w[0] if h == 0 else conv_w[0][::-1]
    cvec = np.zeros((128, 4, 34), np.float32)
    cvec[:, :, 0:31] = cw.T.reshape(4, 128, 31).transpose(1, 0, 2)
    cvec[:, :, 31] = conv_b[0].reshape(4, 128).T
    cvec[:, :, 32] = conv_ln_g[0].reshape(4, 128).T
    cvec[:, :, 33] = conv_ln_b[0].reshape(4, 128).T
    qk = np.stack([np.tile(q_norm[0], 2), np.tile(k_norm[0], 2)], axis=1).astype(np.float32)
    order = [0, 1] if h == 0 else [1, 0]

    def gp(a):
        a = a[order]
        return a.reshape(2, 2, G32, 64).transpose(1, 3, 0, 2).reshape(128, 2, G32)

    ldt = np.broadcast_to(ssm_log_dt[0][:, :, None], (2, 64, 64))
    s5p = np.ascontiguousarray(np.stack([gp(ssm_lam_re[0]), gp(ssm_lam_im[0]), gp(ldt)], axis=2))

    def gB(a):
        a = a[order]
        return a.reshape(2, 2, G32, 64, 16).transpose(1, 3, 0, 2, 4).reshape(128, 2, G32, 16)

    def gC(a):
        a = a[order]
        return a.reshape(2, 2, G32, 16, 64).transpose(1, 4, 0, 2, 3).reshape(128, 2, G32, 16)

    s5B = np.ascontiguousarray(np.stack([gB(ssm_b_re[0]), gB(ssm_b_im[0])], axis=2))
    s5C = np.ascontiguousarray(np.stack([gC(ssm_c_re[0]), gC(ssm_c_im[0])], axis=2))
    flags = np.zeros((128, 2), np.float32)
    flags[:, 1 - h] = 1.0
    m = dict(shared)
    m.update({
        "xw": local_view(x[b], h, WIN), "cvec": cvec, "qk_g": np.ascontiguousarray(qk),
        "s5p": s5p, "s5B": s5B, "s5C": s5C, "flags": flags,
    })
    return m


def kernel(x, mix_norm_even, w_in, conv_w, conv_b, conv_ln_g, conv_ln_b, q_norm, k_norm,
           w_out, mix_norm_odd, ssm_lam_re, ssm_lam_im, ssm_log_dt, ssm_b_re, ssm_b_im,
           ssm_c_re, ssm_c_im, ssm_d, w_glu, ffn_norm, w_router, b_router,
           w_e_gate, w_e_up, w_e_down):
    f = lambda a: np.ascontiguousarray(np.asarray(a, dtype=np.float32))
    args = [f(a) for a in (x, mix_norm_even, w_in, conv_w, conv_b, conv_ln_g, conv_ln_b, q_norm, k_norm,
                           w_out, mix_norm_odd, ssm_lam_re, ssm_lam_im, ssm_log_dt, ssm_b_re, ssm_b_im,
                           ssm_c_re, ssm_c_im, ssm_d, w_glu, ffn_norm, w_router, b_router,
                           w_e_gate, w_e_up, w_e_down)]
    (x, mix_norm_even, w_in, conv_w, conv_b, conv_ln_g, conv_ln_b, q_norm, k_norm,
     w_out, mix_norm_odd, ssm_lam_re, ssm_lam_im, ssm_log_dt, ssm_b_re, ssm_b_im,
     ssm_c_re, ssm_c_im, ssm_d, w_glu, ffn_norm, w_router, b_router, w_e_gate, w_e_up, w_e_down) = args
    shared = {
        "g0": np.ascontiguousarray(mix_norm_even[0][None, :]), "w_in": w_in[0], "w_out": w_out[0],
        "eb": host_eb(), "cst": host_consts(), "gf": ffn_norm, "w_r": w_router, "b_r": b_router,
        "wg": w_e_gate, "wu": w_e_up, "wd": w_e_down, "gn": np.ascontiguousarray(mix_norm_odd[0][None, :]),
        "dvec": np.ascontiguousarray(ssm_d[0][None, :]), "masks": host_masks(), "w_glu": w_glu[0],
        "zeros": np.zeros((1024, D), ml_dtypes.bfloat16),
    }
    nc = get_prog("F", build_fused)
    in_maps = [fused_inputs(c, *args, shared) for c in range(NCORES)]
    res = run_bass_kernel_spmd(nc, in_maps, core_ids=list(range(NCORES)))
    return from_local([r["out"] for r in res.results]).astype(np.float32)


BIGIDX = float(1 << 20)
I32 = mybir.dt.int32


def emit_phaseB_sparse(P, io):
    with contextlib.ExitStack() as ph:
        P.cur = ph
        _phaseB_sparse_body(P, io["x1"], io["affT_seq"], io["aff_own"], io["gf"], io["wg"], io["wu"], io["wd"],
                            io["x2_out"], io.get("gn"), io.get("hn_out"), io["xc"], io["yc"])
        P.barrier()
    P.cur = None


def _phaseB_sparse_body(P, x1, affT_seq, aff_own, gf, wg, wu, wd, x2_out, gn, hn_out, Xc, Yc):
    dve, act, pe, pool, sp = P.dve, P.act, P.pe, P.pool, P.sp
    NT = OWN // 128
    if gn is not None:
        gnb = P.sb([128, D], F32, name="gnb")
        bgnb = Buf()
        sp.dma(out=gnb[:], in_=bcast_rows(gn, 128), writes=[bgnb])
    gfb = P.sb([128, D], F32, name="gfb")
    bgfb = Buf()
    sp.dma(out=gfb[:], in_=bcast_rows(gf, 128), writes=[bgfb])
    gate = P.sb([128, NT, NEXP], F32, name="gate")
    bgate = Buf()
    idx = P.sb([128, NT, NEXP], I32, name="idx")
    bidx = Buf()
    if P.bcreg is None:
        P.bcreg = P.nc.gpsimd.alloc_register("bcreg")
        P.nc.gpsimd.reg_mov(P.bcreg, NEXP * CAP - 1)

    with contextlib.ExitStack() as st:
        pthr = P.ps([128, 512], F32, stack=st)
        bpthr = Buf()
        aT = P.sb([NEXP, SEQ], F32, stack=st)
        baT = Buf()
        sp.dma(out=aT[:].rearrange("e (r t) -> e r t", r=2), in_=affT_seq, writes=[baT])
        junk = P.sb([NEXP, SEQ], F32, stack=st)
        bjunk = Buf()
        lo = P.sb([NEXP, 1], F32, stack=st)
        blo = Buf()
        mid = P.sb([NEXP, 1], F32, stack=st)
        bmid = Buf()
        cnt = P.sb([NEXP, 1], F32, stack=st)
        bcnt = Buf()
        ge = P.sb([NEXP, 1], F32, stack=st)
        bge = Buf()
        dve.op(lambda e: e.memset(lo[:], 0.0), [], [blo])
        for it in range(30):
            wk = 2.0 ** (-(it + 1))
            dve.op(ts_fn(mid[:], lo[:], wk, None, ALU.add), [blo], [bmid])
            dve.op(lambda e: e.tensor_scalar(out=junk[:], in0=aT[:], scalar1=mid[:, 0:1], scalar2=0.0,
                                             op0=ALU.is_ge, op1=ALU.add, accum_out=cnt[:, 0:1]),
                   [baT, bmid], [bjunk, bcnt])
            dve.op(ts_fn(ge[:], cnt[:], CAP - 0.5, None, ALU.is_ge), [bcnt], [bge])
            dve.op(stt_fn(lo[:], ge[:], wk, lo[:], ALU.mult, ALU.add), [bge, blo], [blo])
        dthr = P.sb([NEXP, NEXP], F32, stack=st)
        bdthr = Buf()
        dve.op(ts_fn(dthr[:], P.cst[0:NEXP, 0, 0:NEXP], lo[:, 0:1], None, ALU.mult), [P.bcst, blo], [bdthr])
        pe.op(mm_fn(pthr[:, 0:NEXP], P.cst[0:NEXP, 3, :], dthr[:], True, True), [P.bcst, bdthr], [bpthr])
        thrb = P.sb([128, NEXP], F32, stack=st)
        bthrb = Buf()
        act.op(act_fn(thrb[:], pthr[:, 0:NEXP], AF.Copy), [bpthr], [bthrb])
        ao = P.sb([128, NT, NEXP], F32, stack=st)
        bao = Buf()
        sp.dma(out=ao[:], in_=aff_own.rearrange("(t p) e -> p t e", p=128), writes=[bao])
        msk = P.sb([128, NT, NEXP], F32, stack=st)
        bmsk = Buf()

        def bct(t):
            a = t[:]
            return bass.AP(a.tensor, a.offset, [list(a.ap[0]), [0, NT], [1, NEXP]])

        dve.op(tt_fn(msk[:], ao[:], bct(thrb), ALU.is_ge), [bao, bthrb], [bmsk])
        ppos = P.ps([128, 512], F32, stack=st)
        bppos = Buf()
        pcnt = P.ps([128, 512], F32, stack=st)
        bpcnt = Buf()
        m2 = msk[:].rearrange("p t e -> p (t e)")
        pe.op(mm_fn(ppos[:, 0:NT * NEXP], P.cst[:, 4, :], m2, True, True), [P.bcst, bmsk], [bppos])
        pe.op(mm_fn(pcnt[:, 0:NT * NEXP], P.cst[:, 3, :], m2, True, True), [P.bcst, bmsk], [bpcnt])
        csb = P.sb([128, NT, NEXP], F32, stack=st)
        bcsb = Buf()
        act.op(act_fn(csb[:].rearrange("p t e -> p (t e)"), pcnt[:, 0:NT * NEXP], AF.Copy), [bpcnt], [bcsb])
        off = P.sb([128, NT, NEXP], F32, stack=st)
        boff = Buf()
        dve.op(lambda e: e.memset(off[:, 0, :], 0.0), [], [boff])
        for i in range(1, NT):
            dve.op(tt_fn(off[:, i, :], off[:, i - 1, :], csb[:, i - 1, :], ALU.add), [boff, bcsb], [boff])
        pos = P.sb([128, NT, NEXP], F32, stack=st)
        bpos = Buf()
        dve.op(tt_fn(pos[:].rearrange("p t e -> p (t e)"), ppos[:, 0:NT * NEXP],
                     off[:].rearrange("p t e -> p (t e)"), ALU.add), [bppos, boff], [bpos])
        ok = P.sb([128, NT, NEXP], F32, stack=st)
        bok = Buf()
        dve.op(ts_fn(ok[:], pos[:], CAP - 0.5, None, ALU.is_lt), [bpos], [bok])
        dve.op(tt_fn(msk[:], msk[:], ok[:], ALU.mult), [bmsk, bok], [bmsk])
        dve.op(tt_fn(gate[:], msk[:], ao[:], ALU.mult), [bmsk, bao], [bgate])
        ebase = P.cst[:, 5, 0:NEXP]
        eb_bc = bass.AP(ebase.tensor, ebase.offset, [list(ebase.ap[0]), [0, NT], [1, NEXP]])
        dve.op(tt_fn(pos[:], pos[:], eb_bc, ALU.add), [bpos, P.bcst], [bpos])
        dve.op(ts_fn(pos[:], pos[:], -BIGIDX, None, ALU.add), [bpos], [bpos])
        dve.op(tt_fn(pos[:], pos[:], msk[:], ALU.mult), [bpos, bmsk], [bpos])
        dve.op(ts_fn(pos[:], pos[:], BIGIDX, None, ALU.add), [bpos], [bpos])
        dve.op(lambda e: e.tensor_copy(out=idx[:], in_=pos[:]), [bpos], [bidx])
        P.barrier()

    def indirect(out, out_off, in_, in_off, reads, writes):
        s_ = pool
        if s_.dsems is None:
            s_.dsems = [P.newsem("d%d_%s" % (i, s_.name)) for i in range(s_.nslots)]
        j = s_.dn
        slot = j % s_.nslots
        key = (s_, slot)
        prev = 16 * (j // s_.nslots)
        if prev > 0:
            s_._wait((key, s_.dsems[slot], prev))
        s_._deps(reads, writes, True)
        ins = s_.eng.indirect_dma_start(out=out, out_offset=out_off, in_=in_, in_offset=in_off,
                                        bounds_check=P.bcreg, oob_is_err=False)
        ins.then_inc(s_.dsems[slot], 16)
        s_.dn += 1
        s_._mark((key, s_.dsems[slot], prev + 16), reads, writes)

    with contextlib.ExitStack() as st:
        acc = P.sb([128, NT, D], F32, stack=st)
        bacc = [Buf() for _ in range(NT)]
        bXc = [Buf() for _ in range(NEXP)]
        bYc = [Buf() for _ in range(NEXP)]
        small = Ring(P, 8, [128, 1], F32, st)
        with contextlib.ExitStack() as st1:
            junk = Ring(P, 1, [128, D], F32, st1)
            hnr = Ring(P, 3, [128, D], BF16, st1)
            for t in range(NT):
                sp.dma(out=acc[:, t, :], in_=x1[t * 128:(t + 1) * 128, :], writes=[bacc[t]])
                rs, br = rms_rstd(P, acc[:, t, :], bacc[t], D, junk, small)
                hn, bhn = hnr.next()
                dve.op(stt_fn(hn[:], acc[:, t, :], rs[:, 0:1], gfb[:], ALU.mult, ALU.mult), [bacc[t], br, bgfb], [bhn])
                for e in range(NEXP):
                    indirect(Xc[:, :], bass.IndirectOffsetOnAxis(ap=idx[:, t, e:e + 1], axis=0), hn[:, :], None,
                             [bhn, bidx], [bXc[e]])
            P.barrier()
        wgr = Ring(P, 2, [128, 8, D], BF16, st)
        wur = Ring(P, 2, [128, 8, D], BF16, st)
        wdr = Ring(P, 1, [128, 8, D], BF16, st)
        xsr = Ring(P, 1, [128, 4, D], BF16, st)
        xTr = Ring(P, 1, [128, 8, CAP], BF16, st)
        atr = Ring(P, 1, [128, 8, CAP], BF16, st)
        ysr = Ring(P, 1, [128, 4, D], BF16, st)
        sgr = Ring(P, 1, [128, 512], F32, st)
        ybr = Ring(P, 3, [128, D], BF16, st)
        ptr = Ring(P, 2, [128, 8, 128], BF16, st, psum=True)
        pgr = Ring(P, 2, [128, 512], F32, st, psum=True)
        pur = Ring(P, 2, [128, 512], F32, st, psum=True)
        pyr = Ring(P, 2, [128, 512], F32, st, psum=True)
        for yb_, byb_ in zip(ybr.t, ybr.b):
            dve.op(lambda e, yb_=yb_: e.memset(yb_[:], 0.0), [], [byb_])

        def load_w(e, which=((0, 1, 2))):
            wts = []
            for src, ring in [((wg, wgr), (wu, wur), (wd, wdr))[i_] for i_ in which]:
                w, bw = ring.next()
                sv = src[e].rearrange("(k p) f -> p k f", p=128)
                pool.dma(out=w[:, 0:4, :], in_=sv[:, 0:4, :], writes=[bw])
                pool.dma(out=w[:, 4:8, :], in_=sv[:, 4:8, :], writes=[bw])
                wts.append((w, bw))
            return wts

        def gather_back(e):
            for t in range(NT):
                yb, byb = ybr.next()
                indirect(yb[:, :], None, Yc[:, :], bass.IndirectOffsetOnAxis(ap=idx[:, t, e:e + 1], axis=0),
                         [bYc[e], bidx], [byb])
                dve.op(stt_fn(acc[:, t, :], yb[:], gate[:, t, e:e + 1], acc[:, t, :], ALU.mult, ALU.add),
                       [byb, bgate, bacc[t]], [bacc[t]])

        nxt = load_w(0, (0, 1))
        for e in range(NEXP):
            (Wg, bWg), (Wu, bWu) = nxt
            ((Wd, bWd),) = load_w(e, (2,))
            if e + 1 < NEXP:
                nxt = load_w(e + 1, (0, 1))
            xs, bxs = xsr.next()
            sp.dma(out=xs[:], in_=Xc[e * CAP:(e + 1) * CAP, :].rearrange("(s p) d -> p s d", p=128),
                   reads=[bXc[e]], writes=[bxs])
            xT, bxT = xTr.next()
            for sl in range(4):
                pt, bpt = ptr.next()
                for k in range(8):
                    pe.op(lambda en, pt=pt, k=k, xs=xs, sl=sl: en.transpose(pt[:, k, :], xs[:, sl, k * 128:(k + 1) * 128],
                                                                           P.identb[:]), [bxs, P.bidb], [bpt])
                act.op(act_fn(xT[:, :, sl * 128:(sl + 1) * 128], pt[:], AF.Copy), [bpt], [bxT])
            AT, bAT = atr.next()
            for fc in range(8):
                pg, bpg = pgr.next()
                pu, bpu = pur.next()
                for k in range(8):
                    pe.op(mm_fn(pg[:], Wg[:, k, fc * 128:(fc + 1) * 128], xT[:, k, :], k == 0, k == 7), [bWg, bxT], [bpg])
                for k in range(8):
                    pe.op(mm_fn(pu[:], Wu[:, k, fc * 128:(fc + 1) * 128], xT[:, k, :], k == 0, k == 7), [bWu, bxT], [bpu])
                sg, bsg = sgr.next()
                act.op(act_fn(sg[:], pg[:], AF.Silu), [bpg], [bsg])
                dve.op(tt_fn(AT[:, fc, :], sg[:], pu[:], ALU.mult), [bsg, bpu], [bAT])
            ys, bys = ysr.next()
            for sl in range(4):
                for hc in range(2):
                    py, bpy = pyr.next()
                    for fc in range(8):
                        pe.op(mm_fn(py[:], AT[:, fc, sl * 128:(sl + 1) * 128], Wd[:, fc, hc * 512:(hc + 1) * 512],
                                    fc == 0, fc == 7), [bAT, bWd], [bpy])
                    act.op(act_fn(ys[:, sl, hc * 512:(hc + 1) * 512], py[:], AF.Copy), [bpy], [bys])
            sp.dma(out=Yc[e * CAP:(e + 1) * CAP, :].rearrange("(s p) d -> p s d", p=128), in_=ys[:],
                   reads=[bys], writes=[bYc[e]])
            if e >= 1:
                gather_back(e - 1)
        gather_back(NEXP - 1)
        hor = Ring(P, 1, [128, D], F32, st)
        junk = hor
        for t in range(NT):
            sp.dma(out=x2_out[t * 128:(t + 1) * 128, :], in_=acc[:, t, :], reads=[bacc[t]])
            if gn is not None:
                rs, br = rms_rstd(P, acc[:, t, :], bacc[t], D, junk, small)
                ho, bho = hor.next()
                dve.op(stt_fn(ho[:], acc[:, t, :], rs[:, 0:1], gnb[:], ALU.mult, ALU.mult),
                       [bacc[t], br, bgnb], [bho])
                sp.dma(out=hn_out[t * 128:(t + 1) * 128, :], in_=ho[:], reads=[bho])
```

```python
import numpy as np
import ml_dtypes
import contextlib
import concourse.bass as bass
import concourse.mybir as mybir
from concourse.bass_utils import run_bass_kernel_spmd

F32 = mybir.dt.float32
BF16 = mybir.dt.bfloat16
ALU = mybir.AluOpType
AF = mybir.ActivationFunctionType
AX = mybir.AxisListType

NCORES = 8
D = 1024
SEQ = 4096
OWN = 2048
WIN = 3072
EPS = 1e-6
DEBUG = False
SPARSE = True
SCAN_SELFSYNC = False


class Buf:
    __slots__ = ("name", "w", "r")

    def __init__(self, name=""):
        self.name = name
        self.w = None
        self.r = {}


class Stream:
    def __init__(self, P, name, eng):
        self.P = P
        self.name = name
        self.eng = eng
        self.sem = P.newsem("c_" + name)
        self.cnt = 0
        self.seen = {}
        self.nslots = 8
        self.dsems = None
        self.dn = 0
        self.selfsync = name != "pe"

    def _wait(self, tok):
        key, sem, val = tok
        if self.seen.get(key, 0) < val:
            self.eng.wait_ge(sem, val)
            self.seen[key] = val

    def _deps(self, reads, writes, dma):
        toks = []
        for b in reads:
            if b.w is not None:
                toks.append(b.w)
        for b in writes:
            if b.w is not None:
                toks.append(b.w)
            toks.extend(b.r.values())
        for t in toks:
            if dma or t[0] is not self or self.selfsync:
                self._wait(t)

    def _mark(self, tok, reads, writes):
        for b in reads:
            b.r[tok[0]] = tok
        for b in writes:
            b.w = tok
            b.r = {}

    def op(self, fn, reads=(), writes=()):
        if self.P.stopped:
            return None
        self._deps(reads, writes, False)
        ins = fn(self.eng)
        self.cnt += 1
        ins.then_inc(self.sem, 1)
        self._mark((self, self.sem, self.cnt), reads, writes)
        return ins

    def dma(self, out, in_, reads=(), writes=(), **kw):
        if self.P.stopped:
            return None
        if self.dsems is None:
            self.dsems = [self.P.newsem("d%d_%s" % (i, self.name)) for i in range(self.nslots)]
        j = self.dn
        slot = j % self.nslots
        key = (self, slot)
        prev = 16 * (j // self.nslots)
        if prev > 0:
            self._wait((key, self.dsems[slot], prev))
        self._deps(reads, writes, True)
        ins = self.eng.dma_start(out=out, in_=in_, **kw)
        ins.then_inc(self.dsems[slot], 16)
        self.dn += 1
        self._mark((key, self.dsems[slot], prev + 16), reads, writes)
        return ins


class Prog:
    def __init__(self):
        self.nc = bass.Bass("TRN2", target_bir_lowering=False)
        self.stack = contextlib.ExitStack()
        self._n = 0
        self.stopped = False
        self.cur = None
        self.ccsem = None
        self.ccn = 0
        self.ncores = NCORES
        self.bcreg = None
        nc = self.nc
        self.pe = Stream(self, "pe", nc.tensor)
        self.act = Stream(self, "act", nc.scalar)
        self.dve = Stream(self, "dve", nc.vector)
        self.pool = Stream(self, "pool", nc.gpsimd)
        self.sp = Stream(self, "sp", nc.sync)
        self.streams = [self.pe, self.act, self.dve, self.pool, self.sp]

    def newsem(self, name):
        return self.stack.enter_context(self.nc.semaphore(name))

    def inp(self, name, shape, dt=F32):
        return self.nc.dram_tensor(name, list(shape), dt, kind="ExternalInput").ap()

    def outp(self, name, shape, dt=F32):
        return self.nc.dram_tensor(name, list(shape), dt, kind="ExternalOutput").ap()

    def sb(self, shape, dt=F32, name=None, stack=None):
        self._n += 1
        name = "s%d_%s" % (self._n, name or "t")
        return (stack or self.cur or self.stack).enter_context(self.nc.sbuf_tensor(name, list(shape), dt))

    def ps(self, shape, dt=F32, name=None, stack=None):
        self._n += 1
        name = "p%d_%s" % (self._n, name or "t")
        return (stack or self.cur or self.stack).enter_context(self.nc.psum_tensor(name, list(shape), dt))

    def barrier(self):
        toks = []
        for s in self.streams:
            if s.cnt:
                toks.append((s, s.sem, s.cnt))
            if s.dsems is not None:
                for slot in range(s.nslots):
                    n = (s.dn - slot + s.nslots - 1) // s.nslots
                    if n > 0:
                        toks.append(((s, slot), s.dsems[slot], 16 * n))
        for s in self.streams:
            for t in toks:
                if t[0] is not s:
                    s._wait(t)

    def dram(self, name, shape, dt=F32):
        return self.nc.dram_tensor(name, list(shape), dt)

    def all_gather_pair(self, src, dst):
        if self.ccsem is None:
            self.ccsem = self.newsem("ccsem")
            self.ccdummy = self.sb([128, 1], F32, name="ccdummy", stack=self.stack)
            self.bccd = Buf()
        self.barrier()
        groups = [[2 * i, 2 * i + 1] for i in range(self.ncores // 2)]
        self.ccn += 1
        n = self.ccn

        def fn(e):
            e.collective_compute("AllGather", ALU.bypass, replica_groups=groups,
                                 ins=[src.ap().opt()], outs=[dst.ap().opt()]).then_inc(self.ccsem, 1)
            e.wait_ge(self.ccsem, n)
            return e.memset(self.ccdummy[:], 0.0)
        self.pool.op(fn, [], [self.bccd])
        self.barrier()

    def finish(self):
        self.barrier()
        self.stack.close()
        return self.nc


def bcast_rows(ap, nparts):
    n = ap.shape[-1]
    return bass.AP(ap.tensor, ap.offset, [[0, nparts], [1, n]])


class Ring:
    def __init__(self, P, n, shape, dt, stack, psum=False):
        alloc = P.ps if psum else P.sb
        self.t = [alloc(shape, dt, stack=stack) for _ in range(n)]
        self.b = [Buf() for _ in range(n)]
        self.i = -1

    def next(self):
        self.i = (self.i + 1) % len(self.t)
        return self.t[self.i], self.b[self.i]


def act_fn(out, in_, func, **kw):
    return lambda e: e.activation(out=out, in_=in_, func=func, **kw)


def mm_fn(out, lhsT, rhs, start, stop):
    return lambda e: e.matmul(out, lhsT, rhs, start=start, stop=stop)


def tt_fn(out, in0, in1, op):
    return lambda e: e.tensor_tensor(out=out, in0=in0, in1=in1, op=op)


def ts_fn(out, in0, s1, s2, op0, op1=None):
    if op1 is None:
        return lambda e: e.tensor_scalar(out=out, in0=in0, scalar1=s1, scalar2=None, op0=op0)
    return lambda e: e.tensor_scalar(out=out, in0=in0, scalar1=s1, scalar2=s2, op0=op0, op1=op1)


def stt_fn(out, in0, scalar, in1, op0, op1):
    return lambda e: e.scalar_tensor_tensor(out=out, in0=in0, scalar=scalar, in1=in1, op0=op0, op1=op1)


ATT_CFG = (1, 4, 16)


def vtile_list():
    tiles = []
    for ci, d in enumerate(ATT_CFG):
        nb = OWN // (128 * d)
        for c in range(d):
            for j in range(nb + 1):
                i0 = 0 if j == 0 else 128 * j - 64
                tiles.append((ci, d, c, j, c + d * i0))
    return tiles


def rms_rstd(P, x_ap, xb, n, ring_junk, ring_small, pbuf_extra=()):
    junk, bj = ring_junk.next()
    ss, bs = ring_small.next()
    P.act.op(act_fn(junk[:, 0:n], x_ap, AF.Square, accum_out=ss[:, 0:1]), [xb], [bj, bs])
    sd, bd = ring_small.next()
    P.act.op(act_fn(sd[:, 0:1], ss[:, 0:1], AF.Sqrt, scale=1.0 / n, bias=P.eps_t[:, 0:1]), [bs, P.beps], [bd])
    rs, br = ring_small.next()
    P.dve.op(lambda e: e.reciprocal(out=rs[:, 0:1], in_=sd[:, 0:1]), [bd], [br])
    return rs, br


def load_consts(P, cst_in):
    cst = P.sb([128, 6, 128], F32, name="cst")
    P.bcst = Buf("cst")
    P.sp.dma(out=cst[:], in_=cst_in, writes=[P.bcst])
    P.identf = cst[:, 0, :]
    P.ones512 = cst[:, 1, :]
    P.ones64 = cst[:, 2, :]
    P.cst = cst
    idb = P.sb([128, 128], BF16, name="identb")
    P.bidb = Buf("identb")
    P.dve.op(lambda e: e.tensor_copy(out=idb[:], in_=cst[:, 0, :]), [P.bcst], [P.bidb])
    P.identb = idb
    eps_t = P.sb([128, 1], F32, name="eps")
    P.beps = Buf("eps")
    P.dve.op(lambda e: e.memset(eps_t[:], EPS), [], [P.beps])
    P.eps_t = eps_t


def router_tile(P, x1t, bx1, gfb, bgfb, wr, bwr, brb, bbrb, rings, aff_out, affT_out, tt, hf_out=None):
    rs, br = rms_rstd(P, x1t[:], bx1, D, rings["junk"], rings["small"])
    hf, bhf = rings["hf"].next()
    P.dve.op(stt_fn(hf[:], x1t[:], rs[:, 0:1], gfb[:], ALU.mult, ALU.mult), [bx1, br, bgfb], [bhf])
    ptf, bptf = rings["ptf"].next()
    for k in range(8):
        P.pe.op(lambda e, k=k: e.transpose(ptf[:, k, :], hf[:, k * 128:(k + 1) * 128], P.identf),
                [bhf, P.bcst], [bptf])
    hfT, bhfT = rings["hfT"].next()
    P.act.op(act_fn(hfT[:], ptf[:], AF.Copy), [bptf], [bhfT])
    plog, bplog = rings["plog"].next()
    for k in range(8):
        P.pe.op(mm_fn(plog[:, 0:16], hfT[:, k, :], wr[:, k, :], k == 0, k == 7), [bhfT, bwr], [bplog])
    lg, blg = rings["sm16"].next()
    P.dve.op(tt_fn(lg[:], plog[:, 0:16], brb[:], ALU.add), [bplog, bbrb], [blg])
    mx, bmx = rings["small"].next()
    P.dve.op(lambda e: e.reduce_max(out=mx[:, 0:1], in_=lg[:], axis=AX.X, negate=True), [blg], [bmx])
    ex, bex = rings["sm16"].next()
    sm, bsm = rings["small"].next()
    P.act.op(act_fn(ex[:], lg[:], AF.Exp, bias=mx[:, 0:1], scale=1.0, accum_out=sm[:, 0:1]), [blg, bmx], [bex, bsm])
    rsm, brsm = rings["small"].next()
    P.dve.op(lambda e: e.reciprocal(out=rsm[:, 0:1], in_=sm[:, 0:1]), [bsm], [brsm])
    af, baf = rings["sm16"].next()
    P.dve.op(ts_fn(af[:], ex[:], rsm[:, 0:1], None, ALU.mult), [bex, brsm], [baf])
    P.sp.dma(out=aff_out[tt * 128:(tt + 1) * 128, :], in_=af[:], reads=[baf])
    pat, bpat = rings["pat"].next()
    P.pe.op(lambda e: e.transpose(pat[0:16, 0:128], af[:], P.identf), [baf, P.bcst], [bpat])
    aT, baT = rings["aT"].next()
    P.act.op(act_fn(aT[:], pat[0:16, 0:128], AF.Copy), [bpat], [baT])
    P.sp.dma(out=affT_out[:, tt * 128:(tt + 1) * 128], in_=aT[:], reads=[baT])
    return hf, bhf


def emit_phaseA(P, io):
    xw, g0, w_in, cvec, qk_g, w_out = io["xw"], io["g0"], io["w_in"], io["cvec"], io["qk_g"], io["w_out"]
    gf, w_r, b_r, eb = io["gf"], io["w_r"], io["b_r"], io["eb"]
    x1_out, aff_out, affT_out = io["x1_out"], io["aff_out"], io["affT_out"]
    dbg = None
    with contextlib.ExitStack() as ph:
        P.cur = ph
        _phaseA_body(P, xw, g0, w_in, cvec, qk_g, w_out, gf, w_r, b_r, eb, x1_out, aff_out, affT_out)
        P.barrier()
    P.cur = None


def _phaseA_body(P, xw, g0, w_in, cvec, qk_g, w_out, gf, w_r, b_r, eb, x1_out, aff_out, affT_out):
    w_in_v = w_in.rearrange("(k p) e -> p k e", p=128)
    w_out_v = w_out.rearrange("(k p) e -> p k e", p=128)
    w_r_v = w_r.rearrange("(k p) e -> p k e", p=128)

    hT = P.sb([128, 8, WIN], BF16, name="hT")
    bhT = [Buf("hT%d" % i) for i in range(WIN // 128)]
    mixT = P.sb([128, 8, OWN], BF16, name="mixT")
    bmix = [Buf("mix%d" % i) for i in range(8)]
    gb = P.sb([128, D], F32, name="gb")
    bgb = Buf("gb")
    P.sp.dma(out=gb[:], in_=bcast_rows(g0, 128), writes=[bgb])
    cv = P.sb([128, 4, 34], F32, name="cv")
    bcv = Buf("cv")
    P.sp.dma(out=cv[:], in_=cvec, writes=[bcv])
    qkg = P.sb([128, 2], F32, name="qkg")
    bqkg = Buf("qkg")
    P.sp.dma(out=qkg[:], in_=qk_g, writes=[bqkg])

    with contextlib.ExitStack() as st:
        xr = Ring(P, 4, [128, D], F32, st)
        junk = Ring(P, 2, [128, D], F32, st)
        small = Ring(P, 16, [128, 1], F32, st)
        hnr = Ring(P, 4, [128, D], BF16, st)
        ptr = Ring(P, 4, [128, 8, 128], BF16, st, psum=True)
        for tt in range(WIN // 128):
            xt, bx = xr.next()
            P.sp.dma(out=xt[:], in_=xw[tt * 128:(tt + 1) * 128, :], writes=[bx])
            rs, br = rms_rstd(P, xt[:], bx, D, junk, small)
            hn, bhn = hnr.next()
            P.dve.op(stt_fn(hn[:], xt[:], rs[:, 0:1], gb[:], ALU.mult, ALU.mult), [bx, br, bgb], [bhn])
            pt, bpt = ptr.next()
            for k in range(8):
                P.pe.op(lambda e, k=k: e.transpose(pt[:, k, :], hn[:, k * 128:(k + 1) * 128], P.identb[:]),
                        [bhn, P.bidb], [bpt])
            P.act.op(act_fn(hT[:, :, tt * 128:(tt + 1) * 128], pt[:], AF.Copy), [bpt], [bhT[tt]])
        P.barrier()
        if getattr(P, "after_a1", None) is not None:
            P.after_a1()
            P.after_a1 = None

    def hT_bufs(t0, n):
        return bhT[t0 // 128:(t0 + n + 127) // 128]

    NCV = OWN + 128
    with contextlib.ExitStack() as st:
        hglu = P.sb([128, 4, 15 + NCV], BF16, stack=st)
        bhg = [Buf() for _ in range(4)]
        diag = P.sb([128, 4, 31, 128], BF16, stack=st)
        bdiag = [Buf() for _ in range(4)]
        wcv = Ring(P, 2, [128, 8, 256], BF16, st)
        pmr = Ring(P, 2, [128, 512], F32, st, psum=True)
        pgr = Ring(P, 2, [128, 512], F32, st, psum=True)
        pst = Ring(P, 2, [128, 512], F32, st, psum=True)
        sgr = Ring(P, 2, [128, 512], F32, st)
        vbuf = [Ring(P, 2, [128, 512], F32, st) for _ in range(4)]
        sqr = [Ring(P, 2, [128, 512], F32, st) for _ in range(4)]
        tmp = Ring(P, 10, [128, 512], F32, st)
        zr = Ring(P, 2, [128, 512], F32, st)
        for cc in range(4):
            P.dve.op(lambda e, cc=cc: e.memset(hglu[:, cc, 0:15], 0.0), [], [bhg[cc]])
            for k in range(31):
                P.pool.op(ts_fn(diag[:, cc, k, :], P.identf, cv[:, cc, k:k + 1], None, ALU.mult),
                          [P.bcst, bcv], [bdiag[cc]])
        ntiles = [(i * 512, 512) for i in range(4)] + [(2048, 128)]
        for cc in range(4):
            w, bw = wcv.next()
            P.pool.dma(out=w[:, :, 0:128], in_=w_in_v[:, :, cc * 128:(cc + 1) * 128], writes=[bw])
            P.pool.dma(out=w[:, :, 128:256], in_=w_in_v[:, :, 512 + cc * 128:512 + (cc + 1) * 128], writes=[bw])
            for (t0, n) in ntiles:
                pm, bpm = pmr.next()
                pg, bpg = pgr.next()
                for k in range(8):
                    P.pe.op(mm_fn(pm[:, 0:n], w[:, k, 0:128], hT[:, k, t0:t0 + n], k == 0, k == 7),
                            [bw] + hT_bufs(t0, n), [bpm])
                for k in range(8):
                    P.pe.op(mm_fn(pg[:, 0:n], w[:, k, 128:256], hT[:, k, t0:t0 + n], k == 0, k == 7),
                            [bw] + hT_bufs(t0, n), [bpg])
                sg, bsg = sgr.next()
                P.act.op(act_fn(sg[:, 0:n], pg[:, 0:n], AF.Sigmoid), [bpg], [bsg])
                P.dve.op(tt_fn(hglu[:, cc, 15 + t0:15 + t0 + n], pm[:, 0:n], sg[:, 0:n], ALU.mult),
                         [bpm, bsg], [bhg[cc]])
        for nt in range(4):
            t0 = nt * 512
            vb = []
            for cc in range(4):
                pm, bpm = pmr.next()
                for k in range(31):
                    P.pe.op(mm_fn(pm[:], diag[:, cc, k, :], hglu[:, cc, t0 + k:t0 + k + 512], k == 0, k == 30),
                            [bdiag[cc], bhg[cc]], [bpm])
                v, bv = vbuf[cc].next()
                sq, bsq = sqr[cc].next()
                P.act.op(act_fn(v[:], pm[:], AF.Identity, bias=cv[:, cc, 31:32], scale=1.0), [bpm, bcv], [bv])
                P.act.op(act_fn(sq[:], pm[:], AF.Square, bias=cv[:, cc, 31:32], scale=1.0), [bpm, bcv], [bsq])
                vb.append((v, bv, sq, bsq))
            pmean, bpmean = pst.next()
            pex2, bpex2 = pst.next()
            for cc in range(4):
                P.pe.op(mm_fn(pmean[:], P.ones512, vb[cc][0][:], cc == 0, cc == 3), [P.bcst, vb[cc][1]], [bpmean])
            for cc in range(4):
                P.pe.op(mm_fn(pex2[:], P.ones512, vb[cc][2][:], cc == 0, cc == 3), [P.bcst, vb[cc][3]], [bpex2])
            mean, bmean = tmp.next()
            P.act.op(act_fn(mean[:], pmean[:], AF.Copy), [bpmean], [bmean])
            m2, bm2 = tmp.next()
            P.dve.op(tt_fn(m2[:], mean[:], mean[:], ALU.mult), [bmean], [bm2])
            var, bvar = tmp.next()
            P.dve.op(tt_fn(var[:], pex2[:], m2[:], ALU.subtract), [bpex2, bm2], [bvar])
            sd, bsd = tmp.next()
            P.act.op(act_fn(sd[:], var[:], AF.Sqrt, bias=P.eps_t[:, 0:1], scale=1.0), [bvar, P.beps], [bsd])
            rstd, brstd = tmp.next()
            P.dve.op(lambda e: e.reciprocal(out=rstd[:], in_=sd[:]), [bsd], [brstd])
            for cc in range(4):
                v, bv, sq, bsq = vb[cc]
                z, bz = zr.next()
                P.dve.op(tt_fn(z[:], v[:], mean[:], ALU.subtract), [bv, bmean], [bz])
                P.dve.op(tt_fn(z[:], z[:], rstd[:], ALU.mult), [bz, brstd], [bz])
                P.act.op(act_fn(mixT[:, cc, t0:t0 + 512], z[:], AF.Silu, scale=cv[:, cc, 32:33],
                                bias=cv[:, cc, 33:34]), [bz, bcv], [bmix[cc]])
        P.barrier()

    tiles = vtile_list()
    NT = len(tiles)
    tindex = {(ci, c, j): i for i, (ci, d, c, j, s) in enumerate(tiles)}
    with contextlib.ExitStack() as st:
        wq = Ring(P, 2, [128, 8, 384], BF16, st)
        ebr = Ring(P, 1, [128, 6, 384], F32, st)
        qT = P.sb([128, OWN], BF16, stack=st)
        bqT = Buf()
        kT = P.sb([128, WIN], BF16, stack=st)
        bkT = Buf()
        vT = P.sb([128, WIN], BF16, stack=st)
        bvT = Buf()
        Vt = P.sb([128, NT, 256], BF16, stack=st)
        bVt = [Buf() for _ in range(NT)]
        bones = Buf()
        acc = P.sb([128, 2, OWN], F32, stack=st)
        bacc = [Buf(), Buf()]
        pmr = Ring(P, 2, [128, 512], F32, st, psum=True)
        pst = Ring(P, 1, [128, 512], F32, st, psum=True)
        psr = Ring(P, 2, [128, 512], F32, st, psum=True)
        ptvr = Ring(P, 1, [128, 1024], BF16, st, psum=True)
        por = Ring(P, 2, [128, 512], F32, st, psum=True)
        qfr = Ring(P, 2, [128, 512], F32, st)
        sqr = Ring(P, 2, [128, 512], F32, st)
        tmp = Ring(P, 4, [128, 512], F32, st)
        pex = Ring(P, 3, [128, 256], F32, st)
        pTr = Ring(P, 4, [128, 256], BF16, st)
        rzr = Ring(P, 1, [64, OWN], F32, st)
        sring = Ring.__new__(Ring)
        sring.t = psr.t + pmr.t
        sring.b = psr.b + pmr.b
        sring.i = -1
        Vt4 = Vt[:].rearrange("p t (s c) -> p t s c", s=4)
        P.pool.op(lambda e: e.memset(Vt4[:, :, 1, :], 1.0), [], [bones])
        P.pool.op(lambda e: e.memset(Vt4[:, :, 3, :], 1.0), [], [bones])
        ps_slot = [0]
        po_slot = [0]
        pv_slot = [0]
        psb = [[Buf(), Buf()], [Buf(), Buf()]]
        pob = [[Buf() for _ in range(4)] for _ in range(2)]
        pvb = [Buf() for _ in range(4)]
        for hp in range(4):
            w, bw = wq.next()
            for i, base in enumerate((1024, 1536, 2048)):
                P.pool.dma(out=w[:, :, i * 128:(i + 1) * 128],
                           in_=w_in_v[:, :, base + hp * 128:base + (hp + 1) * 128], writes=[bw])
            ebt, bebt = ebr.next()
            P.sp.dma(out=ebt[:], in_=eb[hp], writes=[bebt])
            for which, ntok, dst, bdst, gcol in ((0, OWN, qT, bqT, 0), (1, WIN, kT, bkT, 1)):
                for nt in range(ntok // 512):
                    t0 = nt * 512
                    pm, bpm = pmr.next()
                    for k in range(8):
                        P.pe.op(mm_fn(pm[:], w[:, k, which * 128:(which + 1) * 128], hT[:, k, t0:t0 + 512],
                                      k == 0, k == 7), [bw] + hT_bufs(t0, 512), [bpm])
                    qf, bqf = qfr.next()
                    sq, bsq = sqr.next()
                    P.act.op(act_fn(qf[:], pm[:], AF.Copy), [bpm], [bqf])
                    P.act.op(act_fn(sq[:], pm[:], AF.Square), [bpm], [bsq])
                    pms, bpms = pst.next()
                    P.pe.op(mm_fn(pms[:], P.ones64, sq[:], True, True), [P.bcst, bsq], [bpms])
                    sd, bsd = tmp.next()
                    P.act.op(act_fn(sd[:], pms[:], AF.Ln, bias=P.eps_t[:, 0:1], scale=1.0), [bpms, P.beps], [bsd])
                    rstd, brstd = tmp.next()
                    P.act.op(act_fn(rstd[:], sd[:], AF.Exp, scale=-0.5), [bsd], [brstd])
                    P.dve.op(tt_fn(qf[:], qf[:], rstd[:], ALU.mult), [bqf, brstd], [bqf])
                    P.dve.op(ts_fn(dst[:, t0:t0 + 512], qf[:], qkg[:, gcol:gcol + 1],
                                   0.125 if which == 0 else 1.0, ALU.mult, ALU.mult), [bqf, bqkg], [bdst])
            for nt in range(WIN // 512):
                t0 = nt * 512
                pm, bpm = pmr.next()
                for k in range(8):
                    P.pe.op(mm_fn(pm[:], w[:, k, 256:384], hT[:, k, t0:t0 + 512], k == 0, k == 7),
                            [bw] + hT_bufs(t0, 512), [bpm])
                P.act.op(act_fn(vT[:, t0:t0 + 512], pm[:], AF.Copy), [bpm], [bvT])
            for ti, (ci, d, c, j, s0) in enumerate(tiles):
                pv, bpv = ptvr.next()
                P.pe.op(lambda e, pv=pv, s0=s0, d=d: e.transpose(pv[:, 0:128], vT[:, s0:s0 + 127 * d + 1:d], P.identb[:]),
                        [bvT, P.bidb], [bpv])
                P.act.op(act_fn(Vt4[:, ti, 0:4:2, :], pv[:, 0:128].rearrange("p (s c) -> p s c", s=2), AF.Copy),
                         [bpv, bones], [bVt[ti]])
            LA = 2
            for hh in range(2):
                r0 = 64 * hh
                items = []
                for ci, d in enumerate(ATT_CFG):
                    nb = OWN // (128 * d)
                    for c in range(d):
                        for j in range(nb + 1):
                            items.append((ci, d, c, j, nb))
                qk_out = {}

                def emit_qk(n):
                    ci, d, c, j, nb = items[n]
                    i0 = 0 if j == 0 else 128 * j - 64
                    ks = c + d * i0
                    jlo, jhi = max(j - 1, 0), min(j, nb - 1)
                    nq = 128 * (jhi - jlo + 1)
                    q0 = c + d * 128 * jlo
                    pst_, pbuf = sring.next()
                    sl = pst_[:, 0:nq]
                    P.pe.op(mm_fn(sl, kT[r0:r0 + 64, ks:ks + 127 * d + 1:d],
                                  qT[r0:r0 + 64, q0:q0 + (nq - 1) * d + 1:d], True, True), [bkT, bqT], [pbuf])
                    qk_out[n] = (sl, pbuf, nq)

                for n in range(min(LA, len(items))):
                    emit_qk(n)
                pts = {}
                for n, (ci, d, c, j, nb) in enumerate(items):
                    sl, pbuf, nq = qk_out.pop(n)
                    pe_, bpe = pex.next()
                    P.act.op(act_fn(pe_[:, 0:nq], sl, AF.Exp), [pbuf], [bpe])
                    if j == 0:
                        ebs = ebt[:, ci * 2 + hh, 256:384]
                    elif j == nb:
                        ebs = ebt[:, ci * 2 + hh, 0:128]
                    else:
                        ebs = ebt[:, ci * 2 + hh, 0:256]
                    pT, bpT = pTr.next()
                    P.dve.op(tt_fn(pT[:, 0:nq], pe_[:, 0:nq], ebs, ALU.mult), [bpe, bebt], [bpT])
                    pts[j] = (pT, bpT, nq)
                    if n + LA < len(items):
                        emit_qk(n + LA)
                    if j >= 1:
                        jb = j - 1
                        pTa, bpTa, nqa = pts[jb]
                        a_off = 0 if jb == 0 else 128
                        po_, pobuf = por.next()
                        osl = po_[:, 0:128]
                        ta = tindex[(ci, c, jb)]
                        tb = tindex[(ci, c, j)]
                        P.pe.op(mm_fn(osl, Vt[:, ta, 128 * hh:128 * hh + 128], pTa[:, a_off:a_off + 128],
                                      True, False), [bVt[ta], bpTa], [pobuf])
                        P.pe.op(mm_fn(osl, Vt[:, tb, 128 * hh:128 * hh + 128], pT[:, 0:128],
                                      False, True), [bVt[tb], bpT], [pobuf])
                        a0 = c + d * 128 * jb
                        dsl = acc[:, hh, a0:a0 + 127 * d + 1:d]
                        if ci == 0:
                            P.dve.op(lambda e, dsl=dsl, osl=osl: e.tensor_copy(out=dsl, in_=osl),
                                     [pobuf], [bacc[hh]])
                        else:
                            P.dve.op(tt_fn(dsl, dsl, osl, ALU.add), [pobuf, bacc[hh]], [bacc[hh]])
                rz, brz = rzr.next()
                P.act.op(act_fn(rz[:], acc[64:128, hh, :], AF.Ln), [bacc[hh]], [brz])
                P.act.op(act_fn(rz[:], rz[:], AF.Exp, scale=-1.0), [brz], [brz])
                P.dve.op(tt_fn(mixT[r0:r0 + 64, 4 + hp, :], acc[0:64, hh, :], rz[:], ALU.mult),
                         [bacc[hh], brz], [bmix[4 + hp]])
        P.barrier()

    with contextlib.ExitStack() as st:
        wo = P.sb([128, 8, D], BF16, stack=st)
        bwo = Buf()
        for k in range(8):
            P.pool.dma(out=wo[:, k, :], in_=w_out_v[:, k, :], writes=[bwo])
        gfb = P.sb([128, D], F32, stack=st)
        bgfb = Buf()
        P.sp.dma(out=gfb[:], in_=bcast_rows(gf, 128), writes=[bgfb])
        wr = P.sb([128, 8, 16], F32, stack=st)
        bwr = Buf()
        P.sp.dma(out=wr[:], in_=w_r_v, writes=[bwr])
        brb = P.sb([128, 16], F32, stack=st)
        bbrb = Buf()
        P.sp.dma(out=brb[:], in_=bcast_rows(b_r, 128), writes=[bbrb])
        xr = Ring(P, 2, [128, D], F32, st)
        x1r = Ring(P, 2, [128, D], F32, st)
        pmr = Ring(P, 2, [128, 512], F32, st, psum=True)
        rings = {
            "junk": Ring(P, 1, [128, D], F32, st),
            "small": Ring(P, 12, [128, 1], F32, st),
            "hf": Ring(P, 2, [128, D], F32, st),
            "ptf": Ring(P, 1, [128, 8, 128], F32, st, psum=True),
            "hfT": Ring(P, 2, [128, 8, 128], F32, st),
            "plog": Ring(P, 2, [128, 512], F32, st, psum=True),
            "sm16": Ring(P, 6, [128, 16], F32, st),
            "pat": Ring(P, 1, [128, 512], F32, st, psum=True),
            "aT": Ring(P, 2, [16, 128], F32, st),
        }
        for tt in range(OWN // 128):
            xt, bx = xr.next()
            P.sp.dma(out=xt[:], in_=xw[tt * 128:(tt + 1) * 128, :], writes=[bx])
            x1t, bx1 = x1r.next()
            for half in range(2):
                pm, bpm = pmr.next()
                for k in range(8):
                    P.pe.op(mm_fn(pm[:], mixT[:, k, tt * 128:(tt + 1) * 128], wo[:, k, half * 512:(half + 1) * 512],
                                  k == 0, k == 7), [bmix[k], bwo], [bpm])
                P.dve.op(tt_fn(x1t[:, half * 512:(half + 1) * 512], pm[:], xt[:, half * 512:(half + 1) * 512],
                               ALU.add), [bpm, bx], [bx1])
            P.sp.dma(out=x1_out[tt * 128:(tt + 1) * 128, :], in_=x1t[:], reads=[bx1])
            router_tile(P, x1t, bx1, gfb, bgfb, wr, bwr, brb, bbrb, rings, aff_out, affT_out, tt)
    return


def host_consts():
    cst = np.zeros((128, 6, 128), np.float32)
    cst[:, 0, :] = np.eye(128, dtype=np.float32)
    cst[:, 1, :] = 1.0 / 512.0
    blk = np.zeros((128, 128), np.float32)
    blk[:64, :64] = 1.0 / 64.0
    blk[64:, 64:] = 1.0 / 64.0
    cst[:, 2, :] = blk
    cst[:, 3, :] = 1.0
    cst[:, 4, :] = np.triu(np.ones((128, 128), np.float32), 1)
    cst[:, 5, 0:16] = 512.0 * np.arange(16, dtype=np.float32)[None, :]
    return cst


def host_eb():
    p = np.arange(128)[:, None].astype(np.float64)
    f = np.arange(128)[None, :].astype(np.float64)
    eb = np.zeros((4, 128, 6, 384), np.float32)
    for head in range(8):
        slope = 2.0 ** (-(head + 1))
        hp, hh = head // 2, head % 2
        for ci, d in enumerate(ATT_CFG):
            B = np.where(p <= f, np.exp(-slope * d * np.abs(p + 64 - f)), 0.0)
            A = np.where(p >= f, np.exp(-slope * d * np.abs(p - 64 - f)), 0.0)
            A0 = np.where((p <= 63) & (p >= f - 64), np.exp(-slope * d * np.abs(p - f)), 0.0)
            eb[hp, :, ci * 2 + hh, 0:128] = B
            eb[hp, :, ci * 2 + hh, 128:256] = A
            eb[hp, :, ci * 2 + hh, 256:384] = A0
    return eb


def local_view(xb, h, n):
    if h == 0:
        return np.ascontiguousarray(xb[:n])
    return np.ascontiguousarray(xb[::-1][:n])


_CACHE = {}


def get_prog(name, builder):
    if name not in _CACHE:
        _CACHE[name] = builder()
    return _CACHE[name]


def run_phaseA(x, mix_norm_even, w_in, conv_w, conv_b, conv_ln_g, conv_ln_b, q_norm, k_norm, w_out,
               ffn_norm0, w_router0, b_router0):
    nc = get_prog("A", build_phaseA)
    cst = host_consts()
    eb = host_eb()
    in_maps = []
    for c in range(NCORES):
        b, h = c // 2, c % 2
        cw = conv_w[0] if h == 0 else conv_w[0][::-1]
        cvec = np.zeros((128, 4, 34), np.float32)
        cvec[:, :, 0:31] = cw.T.reshape(4, 128, 31).transpose(1, 0, 2)
        cvec[:, :, 31] = conv_b[0].reshape(4, 128).T
        cvec[:, :, 32] = conv_ln_g[0].reshape(4, 128).T
        cvec[:, :, 33] = conv_ln_b[0].reshape(4, 128).T
        qk = np.stack([np.tile(q_norm[0], 2), np.tile(k_norm[0], 2)], axis=1).astype(np.float32)
        in_maps.append({
            "xw": local_view(x[b], h, WIN),
            "g0": np.ascontiguousarray(mix_norm_even[0][None, :]),
            "w_in": np.ascontiguousarray(w_in[0]),
            "cvec": cvec,
            "qk_g": np.ascontiguousarray(qk),
            "w_out": np.ascontiguousarray(w_out[0]),
            "gf": np.ascontiguousarray(ffn_norm0[None, :]),
            "w_r": np.ascontiguousarray(w_router0),
            "b_r": np.ascontiguousarray(b_router0[None, :]),
            "eb": eb,
            "cst": cst,
        })
    res = run_bass_kernel_spmd(nc, in_maps, core_ids=list(range(NCORES)))
    return res.results


NEXP = 16
CAP = 512


def emit_phaseB(P, io):
    with contextlib.ExitStack() as ph:
        P.cur = ph
        _phaseB_body(P, io["x1"], io["affT_seq"], io["aff_own"], io["gf"], io["wg"], io["wu"], io["wd"],
                     io["x2_out"], io.get("gn"), io.get("hn_out"))
        P.barrier()
    P.cur = None


def _phaseB_body(P, x1, affT_seq, aff_own, gf, wg, wu, wd, x2_out, gn, hn_out):
    if gn is not None:
        gnb = P.sb([128, D], F32, name="gnb")
        bgnb = Buf()
        P.sp.dma(out=gnb[:], in_=bcast_rows(gn, 128), writes=[bgnb])
    gfb = P.sb([128, D], F32, name="gfb")
    bgfb = Buf()
    P.sp.dma(out=gfb[:], in_=bcast_rows(gf, 128), writes=[bgfb])
    gate = P.sb([128, OWN // 128, NEXP], F32, name="gate")
    bgate = Buf()
    pthr = P.ps([128, 512], F32, name="pthr")
    bpthr = Buf()

    with contextlib.ExitStack() as st:
        aT = P.sb([NEXP, SEQ], F32, stack=st)
        baT = Buf()
        P.sp.dma(out=aT[:].rearrange("e (r t) -> e r t", r=2), in_=affT_seq, writes=[baT])
        junk = P.sb([NEXP, SEQ], F32, stack=st)
        bjunk = Buf()
        lo = P.sb([NEXP, 1], F32, stack=st)
        blo = Buf()
        mid = P.sb([NEXP, 1], F32, stack=st)
        bmid = Buf()
        cnt = P.sb([NEXP, 1], F32, stack=st)
        bcnt = Buf()
        ge = P.sb([NEXP, 1], F32, stack=st)
        bge = Buf()
        P.dve.op(lambda e: e.memset(lo[:], 0.0), [], [blo])
        for it in range(36):
            wk = 2.0 ** (-(it + 1))
            P.dve.op(ts_fn(mid[:], lo[:], wk, None, ALU.add), [blo], [bmid])
            P.dve.op(lambda e: e.tensor_scalar(out=junk[:], in0=aT[:], scalar1=mid[:, 0:1], scalar2=0.0,
                                               op0=ALU.is_ge, op1=ALU.add, accum_out=cnt[:, 0:1]),
                     [baT, bmid], [bjunk, bcnt])
            P.dve.op(ts_fn(ge[:], cnt[:], CAP - 0.5, None, ALU.is_ge), [bcnt], [bge])
            P.dve.op(stt_fn(lo[:], ge[:], wk, lo[:], ALU.mult, ALU.add), [bge, blo], [blo])
        dthr = P.sb([NEXP, NEXP], F32, stack=st)
        bdthr = Buf()
        P.dve.op(ts_fn(dthr[:], P.cst[0:NEXP, 0, 0:NEXP], lo[:, 0:1], None, ALU.mult), [P.bcst, blo], [bdthr])
        P.pe.op(mm_fn(pthr[:, 0:NEXP], P.cst[0:NEXP, 3, :], dthr[:], True, True), [P.bcst, bdthr], [bpthr])
        thrb = P.sb([128, NEXP], F32, stack=st)
        bthrb = Buf()
        P.act.op(act_fn(thrb[:], pthr[:, 0:NEXP], AF.Copy), [bpthr], [bthrb])
        ao = P.sb([128, OWN // 128, NEXP], F32, stack=st)
        bao = Buf()
        P.sp.dma(out=ao[:], in_=aff_own.rearrange("(t p) e -> p t e", p=128), writes=[bao])
        msk = P.sb([128, OWN // 128, NEXP], F32, stack=st)
        bmsk = Buf()
        thr_bc = bass.AP(thrb.tensor if hasattr(thrb, "tensor") else thrb[:].tensor, thrb[:].offset,
                         [list(thrb[:].ap[0]), [0, OWN // 128], [1, NEXP]])
        P.dve.op(tt_fn(msk[:], ao[:], thr_bc, ALU.is_ge), [bao, bthrb], [bmsk])
        P.dve.op(tt_fn(gate[:], msk[:], ao[:], ALU.mult), [bmsk, bao], [bgate])
        P.barrier()

    HT = OWN // 2
    with contextlib.ExitStack() as st:
        acc = P.sb([128, HT // 128, D], F32, stack=st)
        bacc = [Buf() for _ in range(HT // 128)]
        hT = P.sb([128, 8, HT], BF16, stack=st)
        bhT = Buf()
        junk = Ring(P, 1, [128, D], F32, st)
        small = Ring(P, 8, [128, 1], F32, st)
        hnr = Ring(P, 2, [128, D], BF16, st)
        hor = Ring(P, 2, [128, D], F32, st)
        ptr = Ring(P, 1, [128, 8, 128], BF16, st, psum=True)
        wgr = Ring(P, 2, [128, 8, D], BF16, st)
        wur = Ring(P, 2, [128, 8, D], BF16, st)
        wdr = Ring(P, 2, [128, 8, D], BF16, st)
        atr = Ring(P, 2, [128, 8, 512], BF16, st)
        sgr = Ring(P, 2, [128, 512], F32, st)
        pgr = Ring(P, 2, [128, 512], F32, st, psum=True)
        pur = Ring(P, 2, [128, 512], F32, st, psum=True)
        pyr = Ring(P, 2, [128, 512], F32, st, psum=True)
        for half in range(2):
            for t in range(HT // 128):
                tg = half * (HT // 128) + t
                P.sp.dma(out=acc[:, t, :], in_=x1[tg * 128:(tg + 1) * 128, :], writes=[bacc[t]])
                rs, br = rms_rstd(P, acc[:, t, :], bacc[t], D, junk, small)
                hn, bhn = hnr.next()
                P.dve.op(stt_fn(hn[:], acc[:, t, :], rs[:, 0:1], gfb[:], ALU.mult, ALU.mult),
                         [bacc[t], br, bgfb], [bhn])
                pt, bpt = ptr.next()
                for k in range(8):
                    P.pe.op(lambda e, k=k, pt=pt, hn=hn: e.transpose(pt[:, k, :], hn[:, k * 128:(k + 1) * 128],
                                                                      P.identb[:]), [bhn, P.bidb], [bpt])
                P.act.op(act_fn(hT[:, :, t * 128:(t + 1) * 128], pt[:], AF.Copy), [bpt], [bhT])
            for e in range(NEXP):
                wts = []
                for src, ring in ((wg, wgr), (wu, wur), (wd, wdr)):
                    w, bw = ring.next()
                    sv = src[e].rearrange("(k p) f -> p k f", p=128)
                    P.pool.dma(out=w[:, 0:4, :], in_=sv[:, 0:4, :], writes=[bw])
                    P.pool.dma(out=w[:, 4:8, :], in_=sv[:, 4:8, :], writes=[bw])
                    wts.append((w, bw))
                (Wg, bWg), (Wu, bWu), (Wd, bWd) = wts
                for q in range(HT // 512):
                    AT, bAT = atr.next()
                    for fc in range(8):
                        pg, bpg = pgr.next()
                        pu, bpu = pur.next()
                        for k in range(8):
                            P.pe.op(mm_fn(pg[:], Wg[:, k, fc * 128:(fc + 1) * 128], hT[:, k, q * 512:(q + 1) * 512],
                                          k == 0, k == 7), [bWg, bhT], [bpg])
                        for k in range(8):
                            P.pe.op(mm_fn(pu[:], Wu[:, k, fc * 128:(fc + 1) * 128], hT[:, k, q * 512:(q + 1) * 512],
                                          k == 0, k == 7), [bWu, bhT], [bpu])
                        sg, bsg = sgr.next()
                        P.act.op(act_fn(sg[:], pg[:], AF.Silu), [bpg], [bsg])
                        P.dve.op(tt_fn(AT[:, fc, :], sg[:], pu[:], ALU.mult), [bsg, bpu], [bAT])
                    for tt in range(4):
                        t = q * 4 + tt
                        tg = half * (HT // 128) + t
                        for hc in range(2):
                            py, bpy = pyr.next()
                            for fc in range(8):
                                P.pe.op(mm_fn(py[:], AT[:, fc, tt * 128:(tt + 1) * 128],
                                              Wd[:, fc, hc * 512:(hc + 1) * 512], fc == 0, fc == 7), [bAT, bWd], [bpy])
                            asl = acc[:, t, hc * 512:(hc + 1) * 512]
                            P.dve.op(stt_fn(asl, py[:], gate[:, tg, e:e + 1], asl, ALU.mult, ALU.add),
                                     [bpy, bgate, bacc[t]], [bacc[t]])
            for t in range(HT // 128):
                tg = half * (HT // 128) + t
                P.sp.dma(out=x2_out[tg * 128:(tg + 1) * 128, :], in_=acc[:, t, :], reads=[bacc[t]])
                if gn is not None:
                    rs, br = rms_rstd(P, acc[:, t, :], bacc[t], D, junk, small)
                    ho, bho = hor.next()
                    P.dve.op(stt_fn(ho[:], acc[:, t, :], rs[:, 0:1], gnb[:], ALU.mult, ALU.mult),
                             [bacc[t], br, bgnb], [bho])
                    P.sp.dma(out=hn_out[tg * 128:(tg + 1) * 128, :], in_=ho[:], reads=[bho])


def run_phaseB(x1_cores, affT_cores, aff_cores, gf, wg, wu, wd, gn):
    nc = get_prog("B", build_phaseB)
    cst = host_consts()
    in_maps = []
    for c in range(NCORES):
        b = c // 2
        affT_seq = np.ascontiguousarray(np.concatenate([affT_cores[2 * b], affT_cores[2 * b + 1]], axis=1))
        in_maps.append({
            "x1": x1_cores[c], "affT_seq": affT_seq, "aff_own": aff_cores[c],
            "gf": np.ascontiguousarray(gf[None, :]), "wg": wg, "wu": wu, "wd": wd, "cst": cst,
            "gn": np.ascontiguousarray(gn[None, :]),
        })
    res = run_bass_kernel_spmd(nc, in_maps, core_ids=list(range(NCORES)))
    return [r["x2"] for r in res.results], [r["hn"] for r in res.results]


NG = 32
NK = SEQ // 8
NWIN = NK // 8
HALF_PI = 1.5707963267948966


class _Stop(Exception):
    pass


def build_phaseC(stop_after=99):
    P = Prog()
    try:
        _phaseC_body(P, stop_after)
    except _Stop:
        P.barrier()
    return P.finish()


def _phaseC_body(P, stop_after):
    lamr_i = P.inp("lamr", [128, NG])
    lami_i = P.inp("lami", [128, NG])
    ldt_i = P.inp("ldt", [128, NG])
    B_i = P.inp("Bri", [128, 2, NG, 16])
    C_i = P.inp("Cri", [128, 2, NG, 16])
    dcol_i = P.inp("dcol", [128, NG])
    Unat = P.inp("Unat", [NG, 128, NK])
    Uw = P.inp("Uw", [NWIN, 128, NG, 8])
    Uwr = P.inp("Uwr", [NWIN, 128, NG, 8])
    masks_i = P.inp("masks", [128, 2, 128])
    cst_in = P.inp("cst", [128, 6, 128])
    zp = P.outp("zp", [NG, 128, NK])
    load_consts(P, cst_in)
    dve, act, pe, pool, sp = P.dve, P.act, P.pe, P.pool, P.sp

    M = P.sb([128, NG, 128], BF16, name="M")
    bM = Buf()
    Wz = P.sb([128, NG, 4, 128], BF16, name="Wz")
    bWz = Buf()
    Rr = P.sb([128, NG, 128], BF16, name="Rr")
    Ri = P.sb([128, NG, 128], BF16, name="Ri")
    bR = Buf()
    A8 = P.sb([128, 2, 2, NG], F32, name="A8")
    bA8 = Buf()
    dcol = P.sb([128, NG], F32, name="dcol")
    bdcol = Buf()
    sp.dma(out=dcol[:], in_=dcol_i, writes=[bdcol])

    def small(st, name=None):
        return P.sb([128, NG], F32, stack=st), Buf()

    with contextlib.ExitStack() as st:
        lamr, blamr = small(st)
        lami, blami = small(st)
        ldt, bldt = small(st)
        sp.dma(out=lamr[:], in_=lamr_i, writes=[blamr])
        sp.dma(out=lami[:], in_=lami_i, writes=[blami])
        sp.dma(out=ldt[:], in_=ldt_i, writes=[bldt])
        Bt = P.sb([128, 2, NG, 16], F32, stack=st)
        bBt = Buf()
        Ct = P.sb([128, 2, NG, 16], F32, stack=st)
        bCt = Buf()
        sp.dma(out=Bt[:], in_=B_i, writes=[bBt])
        sp.dma(out=Ct[:], in_=C_i, writes=[bCt])
        mk = P.sb([128, 2, 128], F32, stack=st)
        bmk = Buf()
        sp.dma(out=mk[:], in_=masks_i, writes=[bmk])
        hpi = P.sb([128, 1], F32, stack=st)
        bhpi = Buf()
        dve.op(lambda e: e.memset(hpi[:], HALF_PI), [], [bhpi])

        def T2(a, ba, b, bb, op):
            o, bo = small(st)
            dve.op(tt_fn(o[:], a[:], b[:], op), [ba, bb], [bo])
            return o, bo

        dt, bdt = small(st)
        act.op(act_fn(dt[:], ldt[:], AF.Exp), [bldt], [bdt])
        a_, ba_ = T2(lamr, blamr, dt, bdt, ALU.mult)
        ang, bang = T2(lami, blami, dt, bdt, ALU.mult)
        mag, bmag = small(st)
        act.op(act_fn(mag[:], a_[:], AF.Exp, scale=1.0 / 16), [ba_], [bmag])
        s16, bs16 = small(st)
        act.op(act_fn(s16[:], ang[:], AF.Sin, scale=1.0 / 16), [bang], [bs16])
        c16, bc16 = small(st)
        act.op(act_fn(c16[:], ang[:], AF.Sin, scale=1.0 / 16, bias=hpi[:, 0:1]), [bang, bhpi], [bc16])
        re, bre = T2(mag, bmag, c16, bc16, ALU.mult)
        im, bim = T2(mag, bmag, s16, bs16, ALU.mult)
        for _ in range(4):
            r2, br2 = T2(re, bre, re, bre, ALU.mult)
            i2, bi2 = T2(im, bim, im, bim, ALU.mult)
            nre, bnre = T2(r2, br2, i2, bi2, ALU.subtract)
            nim, bnim = small(st)
            dve.op(stt_fn(nim[:], re[:], 2.0, im[:], ALU.mult, ALU.mult), [bre, bim], [bnim])
            re, bre, im, bim = nre, bnre, nim, bnim
        pw = P.sb([128, 9, 2, NG], F32, stack=st)
        bpw = Buf()
        ipw = P.sb([128, 9, 2, NG], F32, stack=st)
        bipw = Buf()
        dve.op(lambda e: e.memset(pw[:, 0, 0, :], 1.0), [], [bpw])
        dve.op(lambda e: e.memset(pw[:, 0, 1, :], 0.0), [], [bpw])
        dve.op(lambda e: e.memset(ipw[:, 0, 0, :], 1.0), [], [bipw])
        dve.op(lambda e: e.memset(ipw[:, 0, 1, :], 0.0), [], [bipw])
        dve.op(lambda e: e.tensor_copy(out=pw[:, 1, 0, :], in_=re[:]), [bre], [bpw])
        dve.op(lambda e: e.tensor_copy(out=pw[:, 1, 1, :], in_=im[:]), [bim], [bpw])
        t1, bt1 = small(st)
        t2, bt2 = small(st)
        for n in range(1, 8):
            dve.op(tt_fn(t1[:], pw[:, n, 0, :], re[:], ALU.mult), [bpw, bre], [bt1])
            dve.op(tt_fn(t2[:], pw[:, n, 1, :], im[:], ALU.mult), [bpw, bim], [bt2])
            dve.op(tt_fn(pw[:, n + 1, 0, :], t1[:], t2[:], ALU.subtract), [bt1, bt2], [bpw])
            dve.op(tt_fn(t1[:], pw[:, n, 0, :], im[:], ALU.mult), [bpw, bim], [bt1])
            dve.op(tt_fn(t2[:], pw[:, n, 1, :], re[:], ALU.mult), [bpw, bre], [bt2])
            dve.op(tt_fn(pw[:, n + 1, 1, :], t1[:], t2[:], ALU.add), [bt1, bt2], [bpw])
        en, ben = small(st)
        for n in range(1, 9):
            act.op(act_fn(en[:], a_[:], AF.Exp, scale=-2.0 * n), [ba_], [ben])
            dve.op(tt_fn(ipw[:, n, 0, :], pw[:, n, 0, :], en[:], ALU.mult), [bpw, ben], [bipw])
            dve.op(stt_fn(ipw[:, n, 1, :], pw[:, n, 1, :], -1.0, en[:], ALU.mult, ALU.mult), [bpw, ben], [bipw])
        dve.op(lambda e: e.tensor_copy(out=A8[:, 0, 0, :], in_=pw[:, 8, 0, :]), [bpw], [bA8])
        dve.op(lambda e: e.tensor_copy(out=A8[:, 0, 1, :], in_=pw[:, 8, 0, :]), [bpw], [bA8])
        dve.op(ts_fn(A8[:, 1, 0, :], pw[:, 8, 1, :], -1.0, None, ALU.mult), [bpw], [bA8])
        dve.op(lambda e: e.tensor_copy(out=A8[:, 1, 1, :], in_=pw[:, 8, 1, :]), [bpw], [bA8])
        lr2, blr2 = T2(lamr, blamr, lamr, blamr, ALU.mult)
        li2, bli2 = T2(lami, blami, lami, blami, ALU.mult)
        den, bden = T2(lr2, blr2, li2, bli2, ALU.add)
        rden, brden = small(st)
        dve.op(lambda e: e.reciprocal(out=rden[:], in_=den[:]), [bden], [brden])
        nr, bnr = small(st)
        dve.op(ts_fn(nr[:], re[:], -1.0, None, ALU.add), [bre], [bnr])
        u1, bu1 = T2(nr, bnr, lamr, blamr, ALU.mult)
        u2, bu2 = T2(im, bim, lami, blami, ALU.mult)
        u3, bu3 = T2(u1, bu1, u2, bu2, ALU.add)
        fre, bfre = T2(u3, bu3, rden, brden, ALU.mult)
        u4, bu4 = T2(im, bim, lamr, blamr, ALU.mult)
        u5, bu5 = T2(nr, bnr, lami, blami, ALU.mult)
        u6, bu6 = T2(u4, bu4, u5, bu5, ALU.subtract)
        fim, bfim = T2(u6, bu6, rden, brden, ALU.mult)

        def bc16_(t):
            a = t[:]
            return bass.AP(a.tensor, a.offset, [list(a.ap[0]), [1, NG], [0, 16]])

        big = lambda: (P.sb([128, NG, 16], F32, stack=st), Buf())
        bbr, bbbr = big()
        bbi, bbbi = big()
        g1, bg1 = big()
        g2, bg2 = big()
        dve.op(tt_fn(g1[:], Bt[:, 0, :, :], bc16_(fre), ALU.mult), [bBt, bfre], [bg1])
        dve.op(tt_fn(g2[:], Bt[:, 1, :, :], bc16_(fim), ALU.mult), [bBt, bfim], [bg2])
        dve.op(tt_fn(bbr[:], g1[:], g2[:], ALU.subtract), [bg1, bg2], [bbbr])
        dve.op(tt_fn(g1[:], Bt[:, 1, :, :], bc16_(fre), ALU.mult), [bBt, bfre], [bg1])
        dve.op(tt_fn(g2[:], Bt[:, 0, :, :], bc16_(fim), ALU.mult), [bBt, bfim], [bg2])
        dve.op(tt_fn(bbi[:], g1[:], g2[:], ALU.add), [bg1, bg2], [bbbi])

        def table(top, bot):
            t = P.sb([128, 8, 2, NG], F32, stack=st)
            bt = Buf()
            for i in range(8):
                (ta, tn), (ba, bn) = top[i], bot[i]
                pool.op(lambda e, i=i, ta=ta, tn=tn: e.tensor_copy(out=t[0:64, i, :, :], in_=ta[0:64, tn, :, :]),
                        [bpw, bipw], [bt])
                pool.op(lambda e, i=i, ba=ba, bn=bn: e.tensor_copy(out=t[64:128, i, :, :], in_=ba[64:128, bn, :, :]),
                        [bpw, bipw], [bt])
            return t, bt

        QX, bQX = table([(ipw, s) for s in range(8)], [(pw, s) for s in range(8)])
        PY, bPY = table([(pw, t) for t in range(8)], [(ipw, t) for t in range(8)])
        QW, bQW = table([(pw, 7 - s) for s in range(8)], [(pw, s) for s in range(8)])
        PR, bPR = table([(pw, j + 1) for j in range(8)], [(pw, 8 - j) for j in range(8)])

        def tb(t, i, ri):
            a = t[:, i, ri, :]
            return bass.AP(a.tensor, a.offset, [list(a.ap[0]), [1, NG], [0, 16]])

        def cprod(dst_re, dst_im, bdst, xr, xi, bx, tab, btab, neg_im=False, eng=None):
            eng = eng or dve
            bx = list(bx) if isinstance(bx, (list, tuple)) else [bx]
            for i in range(8):
                eng.op(tt_fn(g1[:], xr[:], tb(tab, i, 0), ALU.mult), bx + [btab], [bg1])
                eng.op(tt_fn(g2[:], xi[:], tb(tab, i, 1), ALU.mult), bx + [btab], [bg2])
                eng.op(tt_fn(dst_re[:, :, i, :], g1[:], g2[:], ALU.subtract), [bg1, bg2], [bdst])
                eng.op(tt_fn(g1[:], xr[:], tb(tab, i, 1), ALU.mult), bx + [btab], [bg1])
                eng.op(tt_fn(g2[:], xi[:], tb(tab, i, 0), ALU.mult), bx + [btab], [bg2])
                if neg_im:
                    eng.op(stt_fn(dst_im[:, :, i, :], g1[:], -1.0, g2[:], ALU.mult, ALU.subtract), [bg1, bg2], [bdst])
                else:
                    eng.op(tt_fn(dst_im[:, :, i, :], g1[:], g2[:], ALU.add), [bg1, bg2], [bdst])

        bbx = Buf()
        with contextlib.ExitStack() as st2:
            Xr = P.sb([128, NG, 8, 16], F32, stack=st2)
            Xi = P.sb([128, NG, 8, 16], F32, stack=st2)
            Yr = P.sb([128, NG, 8, 16], F32, stack=st2)
            Yn = P.sb([128, NG, 8, 16], F32, stack=st2)
            bX, bY = Buf(), Buf()
            bBB = Buf()
            cprod(Xr, Xi, bX, bbr, bbi, [bbbr, bbbi], QX, bQX)
            cprod(Yr, Yn, bY, Ct[:, 0, :, :], Ct[:, 1, :, :], bCt, PY, bPY, neg_im=True)
            psF = Ring(P, 2, [128, 512], F32, st2, psum=True)
            psB = Ring(P, 2, [128, 512], F32, st2, psum=True)
            tmr = Ring(P, 2, [128, 128], F32, st2)
            Xr3 = Xr[:].rearrange("p g s c -> p g (s c)")
            Xi3 = Xi[:].rearrange("p g s c -> p g (s c)")
            Yr3 = Yr[:].rearrange("p g s c -> p g (s c)")
            Yn3 = Yn[:].rearrange("p g s c -> p g (s c)")
            for g in range(NG):
                pf, bpf = psF.next()
                pb_, bpb = psB.next()
                pe.op(mm_fn(pf[:, 0:128], Xr3[0:64, g, :], Yr3[0:64, g, :], True, False), [bX, bY], [bpf])
                pe.op(mm_fn(pf[:, 0:128], Xi3[0:64, g, :], Yn3[0:64, g, :], False, True), [bX, bY], [bpf])
                pe.op(mm_fn(pb_[:, 0:128], Xr3[64:128, g, :], Yr3[64:128, g, :], True, False), [bX, bY], [bpb])
                pe.op(mm_fn(pb_[:, 0:128], Xi3[64:128, g, :], Yn3[64:128, g, :], False, True), [bX, bY], [bpb])
                tm, btm = tmr.next()
                dve.op(tt_fn(tm[:], pf[:, 0:128], mk[:, 0, :], ALU.mult), [bpf, bmk], [btm])
                tm2, btm2 = tmr.next()
                dve.op(tt_fn(tm2[:], pb_[:, 0:128], mk[:, 1, :], ALU.mult), [bpb, bmk], [btm2])
                dve.op(tt_fn(M[:, g, :], tm[:], tm2[:], ALU.add), [btm, btm2], [bM])
            P.barrier()
        if stop_after <= 1:
            P.stopped = True
        with contextlib.ExitStack() as st2:
            Wr = P.sb([128, NG, 8, 16], F32, stack=st2)
            Wi = P.sb([128, NG, 8, 16], F32, stack=st2)
            bW = Buf()
            cprod(Wr, Wi, bW, bbr, bbi, [bbbr, bbbi], QW, bQW)
            pool.op(lambda e: e.memset(Wz[:], 0.0), [], [bWz])
            ptw = Ring(P, 2, [128, 512], F32, st2, psum=True)
            W3 = (Wr[:].rearrange("p g s c -> p g (s c)"), Wi[:].rearrange("p g s c -> p g (s c)"))
            for g in range(NG):
                for ri in range(2):
                    pt, bpt = ptw.next()
                    pe.op(lambda e, pt=pt, g=g, ri=ri: e.transpose(pt[:, 0:128], W3[ri][:, g, :], P.identf),
                          [bW, P.bcst], [bpt])
                    act.op(act_fn(Wz[:, g, 2 * ri, 0:64], pt[:, 0:64], AF.Copy), [bpt], [bWz])
                    act.op(act_fn(Wz[:, g, 2 * ri + 1, 64:128], pt[:, 64:128], AF.Copy), [bpt], [bWz])
            P.barrier()
        if stop_after <= 2:
            P.stopped = True
        Rr4 = Rr[:].rearrange("p g (j c) -> p g j c", j=8)
        Ri4 = Ri[:].rearrange("p g (j c) -> p g j c", j=8)
        cprod(Rr4, Ri4, bR, Ct[:, 0, :, :], Ct[:, 1, :, :], bCt, PR, bPR, neg_im=True)
        P.barrier()

    with contextlib.ExitStack() as st:
        hist = P.sb([128, 2, NG, NK], BF16, stack=st)
        bhist = Buf()
        uwr = Ring(P, 3, [128, NG, 8], BF16, st)
        uwrr = Ring(P, 3, [128, NG, 8], BF16, st)
        pvr = Ring(P, 3, [128, 2, NG, 8], F32, st, psum=True)
        S4r = Ring(P, 4, [128, 3, NG], F32, st)
        p1 = P.sb([128, 2, NG], F32, stack=st)
        p2 = P.sb([128, 2, NG], F32, stack=st)
        bp1, bp2 = Buf(), Buf()
        S4, bS = S4r.next()
        dve.op(lambda e: e.memset(S4[:], 0.0), [], [bS])
        for w in range(NWIN):
            uw, buw = uwr.next()
            uwb, buwb = uwrr.next()
            pool.dma(out=uw[:], in_=Uw[w], writes=[buw])
            pool.dma(out=uwb[:], in_=Uwr[w], writes=[buwb])
            pv, bpv = pvr.next()
            for g in range(NG):
                for ri in range(2):
                    pe.op(mm_fn(pv[:, ri, g, :], Wz[:, g, 2 * ri, :], uw[:, g, :], True, False), [bWz, buw], [bpv])
                    pe.op(mm_fn(pv[:, ri, g, :], Wz[:, g, 2 * ri + 1, :], uwb[:, g, :], False, True), [bWz, buwb], [bpv])
            for jj in range(8):
                j = w * 8 + jj
                act.op(act_fn(hist[0:64, :, :, j], S4[0:64, 0:2, :], AF.Copy), [bS], [bhist])
                act.op(act_fn(hist[64:128, :, :, NK - 1 - j], S4[64:128, 0:2, :], AF.Copy), [bS], [bhist])
                Sn, bSn = S4r.next()
                dve.op(tt_fn(p1[:], A8[:, 0, :, :], S4[:, 0:2, :], ALU.mult), [bA8, bS], [bp1])
                dve.op(tt_fn(p2[:], A8[:, 1, :, :], S4[:, 1:3, :], ALU.mult), [bA8, bS], [bp2])
                dve.op(tt_fn(p1[:], p1[:], p2[:], ALU.add), [bp1, bp2], [bp1])
                dve.op(tt_fn(Sn[:, 0:2, :], p1[:], pv[:, :, :, jj], ALU.add), [bp1, bpv], [bSn])
                dve.op(lambda e, Sn=Sn: e.tensor_copy(out=Sn[:, 2, :], in_=Sn[:, 0, :]), [bSn], [bSn])
                S4, bS = Sn, bSn
        P.barrier()
        if stop_after <= 4:
            P.stopped = True
        ubr = Ring(P, 2, [128, NK], BF16, st)
        ufr = Ring(P, 2, [128, NK], F32, st)
        pyr = Ring(P, 2, [128, 512], F32, st, psum=True)
        yr = Ring(P, 2, [128, NK], F32, st)
        tr = Ring(P, 4, [128, NK], F32, st)
        for g in range(NG):
            ub, bub = ubr.next()
            uf, buf_ = ufr.next()
            pool.dma(out=ub[:], in_=Unat[g], writes=[bub])
            sp.dma(out=uf[:], in_=Unat[g], writes=[buf_])
            py, bpy = pyr.next()
            pe.op(mm_fn(py[:], M[:, g, :], ub[:], True, False), [bM, bub], [bpy])
            pe.op(mm_fn(py[:], Rr[:, g, :], hist[:, 0, g, :], False, False), [bR, bhist], [bpy])
            pe.op(mm_fn(py[:], Ri[:, g, :], hist[:, 1, g, :], False, True), [bR, bhist], [bpy])
            y, by = yr.next()
            dve.op(stt_fn(y[:], uf[:], dcol[:, g:g + 1], py[:], ALU.mult, ALU.add), [buf_, bdcol, bpy], [by])
            a1, ba1 = tr.next()
            dve.op(tt_fn(a1[:], y[:], y[:], ALU.mult), [by], [ba1])
            dve.op(ts_fn(a1[:], a1[:], 0.044715, 1.0, ALU.mult, ALU.add), [ba1], [ba1])
            dve.op(tt_fn(a1[:], a1[:], y[:], ALU.mult), [ba1, by], [ba1])
            a2, ba2 = tr.next()
            act.op(act_fn(a2[:], a1[:], AF.Sigmoid, scale=1.5957691216057308), [ba1], [ba2])
            dve.op(tt_fn(a2[:], a2[:], y[:], ALU.mult), [ba2, by], [ba2])
            sp.dma(out=zp[g], in_=a2[:], reads=[ba2])
    return


def host_masks():
    s = np.arange(128)[:, None] // 16
    t = np.arange(128)[None, :] // 16
    m = np.zeros((128, 2, 128), np.float32)
    m[:, 0, :] = (t >= s)
    m[:, 1, :] = (s >= t)
    return m


def phaseC_inputs(hn1_seq, gh, lam_re, lam_im, log_dt, b_re, b_im, c_re, c_im, d_skip):
    G0 = gh * NG
    sl = slice(G0, G0 + NG)

    def dpg(a):
        return np.ascontiguousarray(a.transpose(0, 2, 1).reshape(128, NG))

    lamr = dpg(lam_re[:, sl, :])
    lami = dpg(lam_im[:, sl, :])
    ldt = dpg(np.broadcast_to(log_dt[:, sl, None], (2, NG, 64)))
    Bri = np.stack([b_re[:, sl].transpose(0, 2, 1, 3).reshape(128, NG, 16),
                    b_im[:, sl].transpose(0, 2, 1, 3).reshape(128, NG, 16)], axis=1)
    Cri = np.stack([c_re[:, sl].transpose(0, 3, 1, 2).reshape(128, NG, 16),
                    c_im[:, sl].transpose(0, 3, 1, 2).reshape(128, NG, 16)], axis=1)
    dg = d_skip[G0 * 16:(G0 + NG) * 16].reshape(NG, 16)
    dcol = np.ascontiguousarray(np.broadcast_to(dg.T[None, :, :], (8, 16, NG)).reshape(128, NG))
    u = hn1_seq[:, G0 * 16:(G0 + NG) * 16].reshape(NK, 8, NG, 16)
    Unat = np.ascontiguousarray(u.transpose(2, 1, 3, 0).reshape(NG, 128, NK))
    Uw = np.ascontiguousarray(Unat.reshape(NG, 128, NWIN, 8).transpose(2, 1, 0, 3))
    Uwr = np.ascontiguousarray(Unat[:, :, ::-1].reshape(NG, 128, NWIN, 8).transpose(2, 1, 0, 3))
    return {"lamr": lamr, "lami": lami, "ldt": ldt, "Bri": np.ascontiguousarray(Bri),
            "Cri": np.ascontiguousarray(Cri), "dcol": dcol, "Unat": Unat, "Uw": Uw, "Uwr": Uwr,
            "masks": host_masks(), "cst": host_consts()}


def phaseC_unpack(zp):
    return np.ascontiguousarray(zp.reshape(NG, 8, 16, NK).transpose(3, 1, 0, 2).reshape(SEQ, NG * 16))


def emit_phaseD(P, io):
    with contextlib.ExitStack() as ph:
        P.cur = ph
        _phaseD_body(P, io["zs"], io["hn"], io["dvec"], io["x2"], io["w_glu"], io["gf"], io["w_r"], io["b_r"],
                     io["x3_out"], io["aff_out"], io["affT_out"])
        P.barrier()
    P.cur = None


def _phaseD_body(P, zt, hn_in, dvec, x2, w_glu, gf, w_r, b_r, x3_out, aff_out, affT_out):
    dvb = P.sb([128, D], F32, name="dvb")
    bdvb = Buf()
    P.sp.dma(out=dvb[:], in_=bcast_rows(dvec, 128), writes=[bdvb])
    st = P.cur
    wgl = P.sb([128, 8, 2 * D], BF16, name="wgl")
    bwgl = Buf()
    wv = w_glu.rearrange("(k p) e -> p k e", p=128)
    for k in range(8):
        P.pool.dma(out=wgl[:, k, :], in_=wv[:, k, :], writes=[bwgl])
    gfb = P.sb([128, D], F32, name="gfb")
    bgfb = Buf()
    P.sp.dma(out=gfb[:], in_=bcast_rows(gf, 128), writes=[bgfb])
    wr = P.sb([128, 8, 16], F32, name="wr")
    bwr = Buf()
    P.sp.dma(out=wr[:], in_=w_r.rearrange("(k p) e -> p k e", p=128), writes=[bwr])
    brb = P.sb([128, 16], F32, name="brb")
    bbrb = Buf()
    P.sp.dma(out=brb[:], in_=bcast_rows(b_r, 128), writes=[bbrb])
    zr = Ring(P, 2, [128, D], F32, st)
    zbr = Ring(P, 2, [128, D], BF16, st)
    hnr2 = Ring(P, 2, [128, D], F32, st)
    xr = Ring(P, 2, [128, D], F32, st)
    x3r = Ring(P, 2, [128, D], F32, st)
    ptr = Ring(P, 1, [128, 8, 128], BF16, st, psum=True)
    zTr = Ring(P, 2, [128, 8, 128], BF16, st)
    pvr = Ring(P, 1, [128, 512], F32, st, psum=True)
    pgr = Ring(P, 1, [128, 512], F32, st, psum=True)
    sgr = Ring(P, 2, [128, 512], F32, st)
    rings = {
        "junk": Ring(P, 1, [128, D], F32, st),
        "small": Ring(P, 12, [128, 1], F32, st),
        "hf": Ring(P, 2, [128, D], F32, st),
        "ptf": Ring(P, 1, [128, 8, 128], F32, st, psum=True),
        "hfT": Ring(P, 2, [128, 8, 128], F32, st),
        "plog": Ring(P, 1, [128, 512], F32, st, psum=True),
        "sm16": Ring(P, 6, [128, 16], F32, st),
        "pat": Ring(P, 1, [128, 512], F32, st, psum=True),
        "aT": Ring(P, 2, [16, 128], F32, st),
    }
    for tt in range(OWN // 128):
        z_, bz = zr.next()
        P.sp.dma(out=z_[:], in_=zt[tt * 128:(tt + 1) * 128, :], writes=[bz])
        xt, bx = xr.next()
        P.sp.dma(out=xt[:], in_=x2[tt * 128:(tt + 1) * 128, :], writes=[bx])
        hn_, bhn_ = hnr2.next()
        P.sp.dma(out=hn_[:], in_=hn_in[tt * 128:(tt + 1) * 128, :], writes=[bhn_])
        P.dve.op(tt_fn(hn_[:], hn_[:], dvb[:], ALU.mult), [bhn_, bdvb], [bhn_])
        P.dve.op(tt_fn(z_[:], z_[:], hn_[:], ALU.add), [bz, bhn_], [bz])
        P.dve.op(tt_fn(hn_[:], z_[:], z_[:], ALU.mult), [bz], [bhn_])
        P.dve.op(ts_fn(hn_[:], hn_[:], 0.044715, 1.0, ALU.mult, ALU.add), [bhn_], [bhn_])
        P.dve.op(tt_fn(hn_[:], hn_[:], z_[:], ALU.mult), [bhn_, bz], [bhn_])
        P.act.op(act_fn(hn_[:], hn_[:], AF.Sigmoid, scale=1.5957691216057308), [bhn_], [bhn_])
        zb, bzb = zbr.next()
        P.dve.op(tt_fn(zb[:], hn_[:], z_[:], ALU.mult), [bhn_, bz], [bzb])
        pt, bpt = ptr.next()
        for k in range(8):
            P.pe.op(lambda e, k=k, pt=pt, zb=zb: e.transpose(pt[:, k, :], zb[:, k * 128:(k + 1) * 128], P.identb[:]),
                    [bzb, P.bidb], [bpt])
        zT, bzT = zTr.next()
        P.act.op(act_fn(zT[:], pt[:], AF.Copy), [bpt], [bzT])
        x3t, bx3 = x3r.next()
        for half in range(2):
            pv, bpv = pvr.next()
            pg, bpg = pgr.next()
            for k in range(8):
                P.pe.op(mm_fn(pv[:], zT[:, k, :], wgl[:, k, half * 512:(half + 1) * 512], k == 0, k == 7),
                        [bzT, bwgl], [bpv])
            for k in range(8):
                P.pe.op(mm_fn(pg[:], zT[:, k, :], wgl[:, k, D + half * 512:D + (half + 1) * 512], k == 0, k == 7),
                        [bzT, bwgl], [bpg])
            sg, bsg = sgr.next()
            P.act.op(act_fn(sg[:], pg[:], AF.Sigmoid), [bpg], [bsg])
            P.dve.op(tt_fn(sg[:], sg[:], pv[:], ALU.mult), [bsg, bpv], [bsg])
            P.dve.op(tt_fn(x3t[:, half * 512:(half + 1) * 512], sg[:], xt[:, half * 512:(half + 1) * 512], ALU.add),
                     [bsg, bx], [bx3])
        P.sp.dma(out=x3_out[tt * 128:(tt + 1) * 128, :], in_=x3t[:], reads=[bx3])
        router_tile(P, x3t, bx3, gfb, bgfb, wr, bwr, brb, bbrb, rings, aff_out, affT_out, tt)
    return


def to_local(full_seq, h):
    return local_view(full_seq, h, OWN)


def from_local(parts):
    out = []
    for b in range(NCORES // 2):
        a0 = parts[2 * b]
        a1 = parts[2 * b + 1][::-1]
        out.append(np.concatenate([a0, a1], axis=0))
    return np.stack(out)


G32 = 32
NKL = OWN // 8
NWL = NKL // 8


def emit_phaseC2(P, io):
    with contextlib.ExitStack() as ph:
        P.cur = ph
        _phaseC2_body(P, io)
        P.barrier()
    P.cur = None


def _phaseC2_body(P, io):
    dve, act, pe, pool, sp = P.dve, P.act, P.pe, P.pool, P.sp
    s5p, s5B, s5C = io["s5p"], io["s5B"], io["s5C"]
    HN, ZS = io["hn"], io["zs_out"]
    SAo, SAall = io["sa_own"], io["sa_all"]

    MS, RS = io["ms"], io["rs"]
    bM = Buf()
    bR = Buf()
    U = P.sb([128, 64, NKL], BF16, name="U")
    bU = Buf()
    A8 = [P.sb([128, 2, 2, G32], F32, name="A8_%d" % d) for d in range(2)]
    bA8 = Buf()
    mk = P.sb([128, 2, 128], F32, name="mk")
    bmk = Buf()
    sp.dma(out=mk[:], in_=io["masks"], writes=[bmk])
    flg = P.sb([128, 2], F32, name="flg")
    bflg = Buf()
    sp.dma(out=flg[:], in_=io["flags"], writes=[bflg])
    hpi = P.sb([128, 1], F32, name="hpi")
    bhpi = Buf()
    dve.op(lambda e: e.memset(hpi[:], HALF_PI), [], [bhpi])
    g1 = P.sb([128, G32, 16], F32, name="g1")
    g2 = P.sb([128, G32, 16], F32, name="g2")
    bg1, bg2 = Buf(), Buf()
    pw_t = [P.sb([128, 9, 2, G32], F32, name="pw%d" % d) for d in range(2)]
    ipw_t = [P.sb([128, 9, 2, G32], F32, name="ipw%d" % d) for d in range(2)]
    bbr_t = [P.sb([128, G32, 16], F32, name="bbr%d" % d) for d in range(2)]
    bbi_t = [P.sb([128, G32, 16], F32, name="bbi%d" % d) for d in range(2)]
    with contextlib.ExitStack() as stp:
        M = P.sb([128, 64, 128], BF16, name="M", stack=stp)
        Rt = [[P.sb([128, G32, 128], BF16, name="R%d%d" % (d, r), stack=stp) for r in range(2)] for d in range(2)]
        par = P.sb([128, 2, 3, G32], F32, name="par", stack=stp)
        bpar = Buf()
        sp.dma(out=par[:], in_=s5p, writes=[bpar])
        Bt = P.sb([128, 2, 2, G32, 16], F32, name="Bt", stack=stp)
        bBt = Buf()
        sp.dma(out=Bt[:], in_=s5B, writes=[bBt])
        Ct = P.sb([128, 2, 2, G32, 16], F32, name="Ct", stack=stp)
        bCt = Buf()
        sp.dma(out=Ct[:], in_=s5C, writes=[bCt])

        def small():
            return P.sb([128, G32], F32, stack=stp), Buf()

        def T2(a, ba, b, bb, op):
            o, bo = small()
            dve.op(tt_fn(o[:], a[:], b[:], op), [ba, bb], [bo])
            return o, bo

        def bc(a):
            return bass.AP(a.tensor, a.offset, [list(a.ap[0]), [1, G32], [0, 16]])

        pws, ipws, bbs = [], [], []
        for d in range(2):
            lamr, lami, ldt = par[:, d, 0, :], par[:, d, 1, :], par[:, d, 2, :]
            dt, bdt = small()
            act.op(act_fn(dt[:], ldt, AF.Exp), [bpar], [bdt])
            a_, ba_ = small()
            dve.op(tt_fn(a_[:], lamr, dt[:], ALU.mult), [bpar, bdt], [ba_])
            ang, bang = small()
            dve.op(tt_fn(ang[:], lami, dt[:], ALU.mult), [bpar, bdt], [bang])
            mag, bmag = small()
            act.op(act_fn(mag[:], a_[:], AF.Exp, scale=1.0 / 16), [ba_], [bmag])
            s16, bs16 = small()
            act.op(act_fn(s16[:], ang[:], AF.Sin, scale=1.0 / 16), [bang], [bs16])
            c16, bc16 = small()
            act.op(act_fn(c16[:], ang[:], AF.Sin, scale=1.0 / 16, bias=hpi[:, 0:1]), [bang, bhpi], [bc16])
            re, bre = T2(mag, bmag, c16, bc16, ALU.mult)
            im, bim = T2(mag, bmag, s16, bs16, ALU.mult)
            for _ in range(4):
                r2, br2 = T2(re, bre, re, bre, ALU.mult)
                i2, bi2 = T2(im, bim, im, bim, ALU.mult)
                nre, bnre = T2(r2, br2, i2, bi2, ALU.subtract)
                nim, bnim = small()
                dve.op(stt_fn(nim[:], re[:], 2.0, im[:], ALU.mult, ALU.mult), [bre, bim], [bnim])
                re, bre, im, bim = nre, bnre, nim, bnim
            pw = pw_t[d]
            ipw = ipw_t[d]
            bpw, bipw = Buf(), Buf()
            dve.op(lambda e, pw=pw: e.memset(pw[:, 0, 0, :], 1.0), [], [bpw])
            dve.op(lambda e, pw=pw: e.memset(pw[:, 0, 1, :], 0.0), [], [bpw])
            dve.op(lambda e, ipw=ipw: e.memset(ipw[:, 0, 0, :], 1.0), [], [bipw])
            dve.op(lambda e, ipw=ipw: e.memset(ipw[:, 0, 1, :], 0.0), [], [bipw])
            dve.op(lambda e, pw=pw, re=re: e.tensor_copy(out=pw[:, 1, 0, :], in_=re[:]), [bre], [bpw])
            dve.op(lambda e, pw=pw, im=im: e.tensor_copy(out=pw[:, 1, 1, :], in_=im[:]), [bim], [bpw])
            t1, bt1 = small()
            t2, bt2 = small()
            for n in range(1, 8):
                dve.op(tt_fn(t1[:], pw[:, n, 0, :], re[:], ALU.mult), [bpw, bre], [bt1])
                dve.op(tt_fn(t2[:], pw[:, n, 1, :], im[:], ALU.mult), [bpw, bim], [bt2])
                dve.op(tt_fn(pw[:, n + 1, 0, :], t1[:], t2[:], ALU.subtract), [bt1, bt2], [bpw])
                dve.op(tt_fn(t1[:], pw[:, n, 0, :], im[:], ALU.mult), [bpw, bim], [bt1])
                dve.op(tt_fn(t2[:], pw[:, n, 1, :], re[:], ALU.mult), [bpw, bre], [bt2])
                dve.op(tt_fn(pw[:, n + 1, 1, :], t1[:], t2[:], ALU.add), [bt1, bt2], [bpw])
            en, ben = small()
            for n in range(1, 9):
                act.op(act_fn(en[:], a_[:], AF.Exp, scale=-2.0 * n), [ba_], [ben])
                dve.op(tt_fn(ipw[:, n, 0, :], pw[:, n, 0, :], en[:], ALU.mult), [bpw, ben], [bipw])
                dve.op(stt_fn(ipw[:, n, 1, :], pw[:, n, 1, :], -1.0, en[:], ALU.mult, ALU.mult), [bpw, ben], [bipw])
            a8 = A8[d]
            dve.op(lambda e, a8=a8, pw=pw: e.tensor_copy(out=a8[:, 0, 0, :], in_=pw[:, 8, 0, :]), [bpw], [bA8])
            dve.op(lambda e, a8=a8, pw=pw: e.tensor_copy(out=a8[:, 0, 1, :], in_=pw[:, 8, 0, :]), [bpw], [bA8])
            dve.op(ts_fn(a8[:, 1, 0, :], pw[:, 8, 1, :], -1.0, None, ALU.mult), [bpw], [bA8])
            dve.op(lambda e, a8=a8, pw=pw: e.tensor_copy(out=a8[:, 1, 1, :], in_=pw[:, 8, 1, :]), [bpw], [bA8])
            lr2, blr2 = small()
            dve.op(tt_fn(lr2[:], lamr, lamr, ALU.mult), [bpar], [blr2])
            li2, bli2 = small()
            dve.op(tt_fn(li2[:], lami, lami, ALU.mult), [bpar], [bli2])
            den, bden = T2(lr2, blr2, li2, bli2, ALU.add)
            rden, brden = small()
            dve.op(lambda e, rden=rden, den=den: e.reciprocal(out=rden[:], in_=den[:]), [bden], [brden])
            nr, bnr = small()
            dve.op(ts_fn(nr[:], re[:], -1.0, None, ALU.add), [bre], [bnr])
            u1, bu1 = small()
            dve.op(tt_fn(u1[:], nr[:], lamr, ALU.mult), [bnr, bpar], [bu1])
            u2, bu2 = small()
            dve.op(tt_fn(u2[:], im[:], lami, ALU.mult), [bim, bpar], [bu2])
            u3, bu3 = T2(u1, bu1, u2, bu2, ALU.add)
            fre, bfre = T2(u3, bu3, rden, brden, ALU.mult)
            u4, bu4 = small()
            dve.op(tt_fn(u4[:], im[:], lamr, ALU.mult), [bim, bpar], [bu4])
            u5, bu5 = small()
            dve.op(tt_fn(u5[:], nr[:], lami, ALU.mult), [bnr, bpar], [bu5])
            u6, bu6 = T2(u4, bu4, u5, bu5, ALU.subtract)
            fim, bfim = T2(u6, bu6, rden, brden, ALU.mult)
            bbr = bbr_t[d]
            bbi = bbi_t[d]
            bbb = Buf()
            dve.op(tt_fn(g1[:], Bt[:, d, 0, :, :], bc(fre[:]), ALU.mult), [bBt, bfre], [bg1])
            dve.op(tt_fn(g2[:], Bt[:, d, 1, :, :], bc(fim[:]), ALU.mult), [bBt, bfim], [bg2])
            dve.op(tt_fn(bbr[:], g1[:], g2[:], ALU.subtract), [bg1, bg2], [bbb])
            dve.op(tt_fn(g1[:], Bt[:, d, 1, :, :], bc(fre[:]), ALU.mult), [bBt, bfre], [bg1])
            dve.op(tt_fn(g2[:], Bt[:, d, 0, :, :], bc(fim[:]), ALU.mult), [bBt, bfim], [bg2])
            dve.op(tt_fn(bbi[:], g1[:], g2[:], ALU.add), [bg1, bg2], [bbb])
            pws.append((pw, bpw))
            ipws.append((ipw, bipw))
            bbs.append((bbr, bbi, bbb))

        def cprod(dst_re, dst_im, bdst, xr, xi, bx, tab, btab, idx, neg_im=False):
            bx = list(bx) if isinstance(bx, (list, tuple)) else [bx]
            for i in range(8):
                tr, ti = bc(tab[:, idx(i), 0, :]), bc(tab[:, idx(i), 1, :])
                dve.op(tt_fn(g1[:], xr, tr, ALU.mult), bx + [btab], [bg1])
                dve.op(tt_fn(g2[:], xi, ti, ALU.mult), bx + [btab], [bg2])
                dve.op(tt_fn(dst_re[:, :, i, :], g1[:], g2[:], ALU.subtract), [bg1, bg2], [bdst])
                dve.op(tt_fn(g1[:], xr, ti, ALU.mult), bx + [btab], [bg1])
                dve.op(tt_fn(g2[:], xi, tr, ALU.mult), bx + [btab], [bg2])
                if neg_im:
                    dve.op(stt_fn(dst_im[:, :, i, :], g1[:], -1.0, g2[:], ALU.mult, ALU.subtract), [bg1, bg2], [bdst])
                else:
                    dve.op(tt_fn(dst_im[:, :, i, :], g1[:], g2[:], ALU.add), [bg1, bg2], [bdst])

        v4 = lambda t: t[:].rearrange("p g (j c) -> p g j c", j=8)
        f3 = lambda t: t[:].rearrange("p g s c -> p g (s c)")

        with contextlib.ExitStack() as st2:
            XY = [[P.sb([128, G32, 8, 16], BF16, stack=st2) for _ in range(4)] for _ in range(2)]
            bXY = Buf()
            (pwA, bpwA), (ipwA, bipwA) = pws[0], ipws[0]
            (pwB, bpwB), (ipwB, bipwB) = pws[1], ipws[1]
            cprod(XY[0][0], XY[0][1], bXY, bbs[0][0][:], bbs[0][1][:], bbs[0][2], ipwA, bipwA, lambda s: s)
            cprod(XY[0][2], XY[0][3], bXY, Ct[:, 0, 0, :, :], Ct[:, 0, 1, :, :], bCt, pwA, bpwA, lambda t: t, neg_im=True)
            cprod(XY[1][0], XY[1][1], bXY, bbs[1][0][:], bbs[1][1][:], bbs[1][2], pwB, bpwB, lambda s: s)
            cprod(XY[1][2], XY[1][3], bXY, Ct[:, 1, 0, :, :], Ct[:, 1, 1, :, :], bCt, ipwB, bipwB, lambda t: t, neg_im=True)
            cprod(v4(Rt[0][0]), v4(Rt[0][1]), bR, Ct[:, 0, 0, :, :], Ct[:, 0, 1, :, :], bCt, pwA, bpwA,
                  lambda j: j + 1, neg_im=True)
            cprod(v4(Rt[1][0]), v4(Rt[1][1]), bR, Ct[:, 1, 0, :, :], Ct[:, 1, 1, :, :], bCt, pwB, bpwB,
                  lambda j: 8 - j, neg_im=True)
            pk = [[Ring(P, 1, [128, 512], F32, st2, psum=True) for _ in range(2)] for _ in range(2)]
            tmr = Ring(P, 4, [128, 128], F32, st2)
            for g in range(G32):
                pp = [[None, None], [None, None]]
                for d in range(2):
                    Xr, Xi, Yr, Yn = [f3(t) for t in XY[d]]
                    for hf in range(2):
                        r0 = 64 * hf
                        pt, bpt = pk[d][hf].next()
                        pe.op(mm_fn(pt[:, 0:128], Xr[r0:r0 + 64, g, :], Yr[r0:r0 + 64, g, :], True, False), [bXY], [bpt])
                        pe.op(mm_fn(pt[:, 0:128], Xi[r0:r0 + 64, g, :], Yn[r0:r0 + 64, g, :], False, True), [bXY], [bpt])
                        pp[d][hf] = (pt, bpt)
                for hf in range(2):
                    tm, btm = tmr.next()
                    dve.op(tt_fn(tm[:], pp[0][hf][0][:, 0:128], mk[:, 0, :], ALU.mult), [pp[0][hf][1], bmk], [btm])
                    tm2, btm2 = tmr.next()
                    dve.op(tt_fn(tm2[:], pp[1][hf][0][:, 0:128], mk[:, 1, :], ALU.mult), [pp[1][hf][1], bmk], [btm2])
                    dve.op(tt_fn(M[:, hf * G32 + g, :], tm[:], tm2[:], ALU.add), [btm, btm2], [bM])
            sp.dma(out=MS.ap(), in_=M[:].rearrange("p g c -> p (g c)"), reads=[bM])
            for d in range(2):
                for r in range(2):
                    sp.dma(out=RS.ap()[:, (2 * d + r) * G32 * 128:(2 * d + r + 1) * G32 * 128],
                           in_=Rt[d][r][:].rearrange("p g c -> p (g c)"), reads=[bR])
            P.barrier()

    with contextlib.ExitStack() as st2:
        Tb = Ring(P, 2, [128, 8, D], BF16, st2)
        Tb2 = Ring(P, 1, [128, 64, 128], BF16, st2)
        ptu = Ring(P, 2, [128, 8, 128], BF16, st2, psum=True)
        for kb in range(NKL // 128):
            tb, btb = Tb.next()
            src = HN[kb * 1024:(kb + 1) * 1024, :].rearrange("(p s) d -> p s d", s=8)
            pool.dma(out=tb[:], in_=src, writes=[btb])
            tb2, btb2 = Tb2.next()
            dve.op(lambda e, tb=tb, tb2=tb2: e.tensor_copy(
                out=tb2[:].rearrange("p g (s c) -> p s g c", s=8),
                in_=tb[:].rearrange("p s (g c) -> p s g c", c=16)), [btb], [btb2])
            for g0 in range(0, 64, 8):
                pt, bpt = ptu.next()
                for gi in range(8):
                    g = g0 + gi
                    pe.op(lambda e, pt=pt, gi=gi, g=g, tb2=tb2: e.transpose(pt[:, gi, :], tb2[:, g, :],
                                                                           P.identb[:]), [btb2, P.bidb], [bpt])
                act.op(act_fn(U[:, g0:g0 + 8, kb * 128:(kb + 1) * 128], pt[:], AF.Copy), [bpt], [bU])
        P.barrier()

    with contextlib.ExitStack() as sth:
        hist = [P.sb([128, 2, G32, NKL], BF16, stack=sth) for _ in range(2)]
        bhist = Buf()
        with contextlib.ExitStack() as st3:
            Wz = P.sb([128, G32, 4, 128], BF16, stack=st3)
            bWz = Buf()
            WT = [P.sb([128, G32, 8, 16], BF16, stack=st3) for _ in range(2)]
            bWT = Buf()
            ptw = Ring(P, 2, [128, 4, 128], BF16, st3, psum=True)
            pvr = Ring(P, 3, [128, 2, G32, 8], F32, st3, psum=True)
            S4r = Ring(P, 4, [128, 3, G32], F32, st3)
            p1 = P.sb([128, 2, G32], F32, stack=st3)
            p2 = P.sb([128, 2, G32], F32, stack=st3)
            bp1, bp2 = Buf(), Buf()
            gx = [P.sb([128, 2 * G32], F32, stack=st3) for _ in range(2)]
            bgx = Buf()
            for d in range(2):
                pw, bpw = pws[d]
                cprod(WT[0], WT[1], bWT, bbs[d][0][:], bbs[d][1][:], bbs[d][2], pw, bpw,
                      (lambda s: 7 - s) if d == 0 else (lambda s: s))
                pool.op(lambda e: e.memset(Wz[:], 0.0), [], [bWz])
                W3 = (f3(WT[0]), f3(WT[1]))
                for g in range(G32):
                    pt, bpt = ptw.next()
                    for ri in range(2):
                        pe.op(lambda e, pt=pt, g=g, ri=ri: e.transpose(pt[:, ri, :], W3[ri][:, g, :], P.identb[:]),
                              [bWT, P.bidb], [bpt])
                    act.op(act_fn(Wz[:, g, 0:4:2, 0:64], pt[:, 0:2, 0:64], AF.Copy), [bpt], [bWz])
                    act.op(act_fn(Wz[:, g, 1:4:2, 64:128], pt[:, 0:2, 64:128], AF.Copy), [bpt], [bWz])
                S4, bS = S4r.next()
                if d == 0:
                    dve.op(lambda e, S4=S4: e.memset(S4[:], 0.0), [], [bS])
                else:
                    sp.dma(out=gx[0][:], in_=SAall.ap()[0:128, :], writes=[bgx])
                    sp.dma(out=gx[1][:], in_=SAall.ap()[128:256, :], writes=[bgx])
                    dve.op(ts_fn(gx[0][:], gx[0][:], flg[:, 0:1], None, ALU.mult), [bgx, bflg], [bgx])
                    S2v = S4[:, 0:2, :].rearrange("p r g -> p (r g)")
                    dve.op(stt_fn(S2v, gx[1][:], flg[:, 1:2], gx[0][:], ALU.mult, ALU.add), [bgx, bflg], [bS])
                    dve.op(lambda e, S4=S4: e.tensor_copy(out=S4[:, 2, :], in_=S4[:, 0, :]), [bS], [bS])
                a8 = A8[d]
                for wi in range(NWL):
                    w = wi if d == 0 else NWL - 1 - wi
                    pv, bpv = pvr.next()
                    for g in range(G32):
                        for ri in range(2):
                            pe.op(mm_fn(pv[:, ri, g, :], Wz[:, g, 2 * ri, :], U[:, g, 8 * w:8 * w + 8], True, False),
                                  [bWz, bU], [bpv])
                            pe.op(mm_fn(pv[:, ri, g, :], Wz[:, g, 2 * ri + 1, :], U[:, G32 + g, 8 * w:8 * w + 8],
                                        False, True), [bWz, bU], [bpv])
                    dve.selfsync = SCAN_SELFSYNC
                    for ji in range(8):
                        jj = ji if d == 0 else 7 - ji
                        k = 8 * w + jj
                        act.op(act_fn(hist[d][:, :, :, k], S4[:, 0:2, :], AF.Copy), [bS], [bhist])
                        Sn, bSn = S4r.next()
                        dve.op(tt_fn(p1[:], a8[:, 0, :, :], S4[:, 0:2, :], ALU.mult), [bA8, bS], [bp1])
                        dve.op(tt_fn(p2[:], a8[:, 1, :, :], S4[:, 1:3, :], ALU.mult), [bA8, bS], [bp2])
                        dve.op(tt_fn(p1[:], p1[:], p2[:], ALU.add), [bp1, bp2], [bp1])
                        dve.op(tt_fn(Sn[:, 0:2, :], p1[:], pv[:, :, :, jj], ALU.add), [bp1, bpv], [bSn])
                        dve.op(lambda e, Sn=Sn: e.tensor_copy(out=Sn[:, 2, :], in_=Sn[:, 0, :]), [bSn], [bSn])
                        S4, bS = Sn, bSn
                    dve.selfsync = True
                if d == 0:
                    sp.dma(out=SAo.ap(), in_=S4[:, 0:2, :].rearrange("p r g -> p (r g)"), reads=[bS])
                    P.all_gather_pair(SAo, SAall)
            P.barrier()
        with contextlib.ExitStack() as st4:
            Z = P.sb([128, 8, D // 2], F32, stack=st4)
            bZ = Buf()
            M = P.sb([128, 64, 128], BF16, stack=st4)
            Rt = [[P.sb([128, G32, 128], BF16, stack=st4) for r in range(2)] for d in range(2)]
            bM, bR = Buf(), Buf()
            sp.dma(out=M[:].rearrange("p g c -> p (g c)"), in_=MS.ap(), writes=[bM])
            for d in range(2):
                for r in range(2):
                    sp.dma(out=Rt[d][r][:].rearrange("p g c -> p (g c)"),
                           in_=RS.ap()[:, (2 * d + r) * G32 * 128:(2 * d + r + 1) * G32 * 128], writes=[bR])
            p1r = Ring(P, 2, [128, 512], F32, st4, psum=True)
            p2r = [Ring(P, 1, [128, 512], F32, st4, psum=True) for _ in range(2)]
            pTr = Ring(P, 2, [128, 4, 128], F32, st4, psum=True)
            c1r = Ring(P, 2, [128, 512], F32, st4)
            ygr = Ring(P, 2, [128, 512], F32, st4)
            for kb in range(NKL // 128):
                ks = slice(kb * 128, (kb + 1) * 128)
                for hf in range(2):
                    r0 = 64 * hf
                    for q in range(G32 // 4):
                        P1, bP1 = p1r.next()
                        P2, bP2 = p2r[hf].next()
                        for gi in range(4):
                            g32 = 4 * q + gi
                            g = hf * G32 + g32
                            cs = slice(gi * 128, (gi + 1) * 128)
                            pe.op(mm_fn(P1[:, cs], M[:, g, :], U[:, g, ks], True, True), [bM, bU], [bP1])
                            seq = [(Rt[0][0], hist[0], 0), (Rt[0][1], hist[0], 1), (Rt[1][0], hist[1], 0),
                                   (Rt[1][1], hist[1], 1)]
                            for n, (Rm, hs, ri) in enumerate(seq):
                                pe.op(mm_fn(P2[:, cs], Rm[r0:r0 + 64, g32, :], hs[r0:r0 + 64, ri, g32, ks],
                                            n == 0, n == 3), [bR, bhist], [bP2])
                        c1, bc1 = c1r.next()
                        act.op(act_fn(c1[:], P1[:], AF.Copy), [bP1], [bc1])
                        yg, byg = ygr.next()
                        dve.op(tt_fn(yg[:], P2[:], c1[:], ALU.add), [bP2, bc1], [byg])
                        pT, bpT = pTr.next()
                        for gi in range(4):
                            pe.op(lambda e, pT=pT, gi=gi, yg=yg: e.transpose(pT[:, gi, :], yg[:, gi * 128:(gi + 1) * 128],
                                                                             P.identf), [byg, P.bcst], [bpT])
                        c0 = 16 * (4 * q)
                        zdst = Z[:, :, c0:c0 + 64].rearrange("p t (g c) -> p t g c", g=4)
                        zsrc = pT[:].rearrange("p g (t c) -> p t g c", t=8)
                        act.op(act_fn(zdst, zsrc, AF.Copy), [bpT], [bZ])
                    dst = ZS[kb * 1024:(kb + 1) * 1024, hf * 512:(hf + 1) * 512].rearrange("(p t) d -> p t d", t=8)
                    sp.dma(out=dst, in_=Z[:], reads=[bZ])


def build_fused(ncores=NCORES):
    P = Prog()
    P.ncores = ncores
    xw = P.inp("xw", [WIN, D])
    g0 = P.inp("g0", [1, D])
    w_in = P.inp("w_in", [D, 2560])
    cvec = P.inp("cvec", [128, 4, 34])
    qk_g = P.inp("qk_g", [128, 2])
    w_out = P.inp("w_out", [D, D])
    eb = P.inp("eb", [4, 128, 6, 384])
    cst_in = P.inp("cst", [128, 6, 128])
    gf = P.inp("gf", [2, D])
    w_r = P.inp("w_r", [2, D, 16])
    b_r = P.inp("b_r", [2, 16])
    wg = P.inp("wg", [2, NEXP, D, D])
    wu = P.inp("wu", [2, NEXP, D, D])
    wd = P.inp("wd", [2, NEXP, D, D])
    gn = P.inp("gn", [1, D])
    s5p = P.inp("s5p", [128, 2, 3, G32])
    s5B = P.inp("s5B", [128, 2, 2, G32, 16])
    s5C = P.inp("s5C", [128, 2, 2, G32, 16])
    dvec = P.inp("dvec", [1, D])
    masks = P.inp("masks", [128, 2, 128])
    flags = P.inp("flags", [128, 2])
    w_glu = P.inp("w_glu", [D, 2 * D])
    zeros_in = P.inp("zeros", [1024, D], BF16)
    out = P.outp("out", [OWN, D])
    X1 = P.dram("X1", [OWN, D])
    X2 = P.dram("X2", [OWN, D])
    X3 = P.dram("X3", [OWN, D])
    HN = P.dram("HN", [OWN, D])
    ZS = P.dram("ZS", [OWN, D])
    AFF = P.dram("AFF", [OWN, NEXP])
    ATo = P.dram("ATo", [NEXP, OWN])
    ATall = P.dram("ATall", [2 * NEXP, OWN])
    SAo = P.dram("SAo", [128, 2 * G32])
    SAall = P.dram("SAall", [256, 2 * G32])
    MS = P.dram("MS", [128, 64 * 128], BF16)
    RS = P.dram("RS", [128, 4 * G32 * 128], BF16)
    Xc = P.dram("Xc", [NEXP * CAP, D], BF16)
    Yc = P.dram("Yc", [NEXP * CAP, D], BF16)
    load_consts(P, cst_in)
    def zero_fill():
        if SPARSE:
            for i in range(NEXP * CAP // 1024):
                P.sp.dma(out=Xc.ap()[i * 1024:(i + 1) * 1024, :], in_=zeros_in)
    P.after_a1 = zero_fill
    emit_phaseA(P, dict(xw=xw, g0=g0, w_in=w_in, cvec=cvec, qk_g=qk_g, w_out=w_out, gf=gf[0:1, :], w_r=w_r[0],
                        b_r=b_r[0:1, :], eb=eb, x1_out=X1.ap(), aff_out=AFF.ap(), affT_out=ATo.ap()))
    P.all_gather_pair(ATo, ATall)
    at_view = ATall.ap().rearrange("(r e) t -> e r t", r=2)
    emitB = emit_phaseB_sparse if SPARSE else emit_phaseB
    emitB(P, dict(x1=X1.ap(), affT_seq=at_view, aff_own=AFF.ap(), gf=gf[0:1, :], wg=wg[0], wu=wu[0], wd=wd[0],
                  x2_out=X2.ap(), gn=gn, hn_out=HN.ap(), xc=Xc.ap(), yc=Yc.ap()))
    emit_phaseC2(P, dict(s5p=s5p, s5B=s5B, s5C=s5C, masks=masks, flags=flags, hn=HN.ap(), zs_out=ZS.ap(),
                         sa_own=SAo, sa_all=SAall, ms=MS, rs=RS))
    emit_phaseD(P, dict(zs=ZS.ap(), hn=HN.ap(), dvec=dvec, x2=X2.ap(), w_glu=w_glu, gf=gf[1:2, :], w_r=w_r[1],
                        b_r=b_r[1:2, :], x3_out=X3.ap(), aff_out=AFF.ap(), affT_out=ATo.ap()))
    P.all_gather_pair(ATo, ATall)
    emitB(P, dict(x1=X3.ap(), affT_seq=at_view, aff_own=AFF.ap(), gf=gf[1:2, :], wg=wg[1], wu=wu[1], wd=wd[1],
                  x2_out=out, xc=Xc.ap(), yc=Yc.ap()))
    return P.finish()


def fused_inputs(c, x, mix_norm_even, w_in, conv_w, conv_b, conv_ln_g, conv_ln_b, q_norm, k_norm,
                 w_out, mix_norm_odd, ssm_lam_re, ssm_lam_im, ssm_log_dt, ssm_b_re, ssm_b_im,
                 ssm_c_re, ssm_c_im, ssm_d, w_glu, ffn_norm, w_router, b_router,
                 w_e_gate, w_e_up, w_e_down, shared):
    b, h = c // 2, c % 2
    cw = conv_w[0] if h == 0 else conv_w[0][::-1]
    cvec = np.zeros((128, 4, 34), np.float32)
    cvec[:, :, 0:31] = cw.T.reshape(4, 128, 31).transpose(1, 0, 2)
    cvec[:, :, 31] = conv_b[0].reshape(4, 128).T
    cvec[:, :, 32] = conv_ln_g[0].reshape(4, 128).T
    cvec[:, :, 33] = conv_ln_b[0].reshape(4, 128).T
    qk = np.stack([np.tile(q_norm[0], 2), np.tile(k_norm[0], 2)], axis=1).astype(np.float32)
    order = [0, 1] if h == 0 else [1, 0]

    def gp(a):
        a = a[order]
        return a.reshape(2, 2, G32, 64).transpose(1, 3, 0, 2).reshape(128, 2, G32)

    ldt = np.broadcast_to(ssm_log_dt[0][:, :, None], (2, 64, 64))
    s5p = np.ascontiguousarray(np.stack([gp(ssm_lam_re[0]), gp(ssm_lam_im[0]), gp(ldt)], axis=2))

    def gB(a):
        a = a[order]
        return a.reshape(2, 2, G32, 64, 16).transpose(1, 3, 0, 2, 4).reshape(128, 2, G32, 16)

    def gC(a):
        a = a[order]
        return a.reshape(2, 2, G32, 16, 64).transpose(1, 4, 0, 2, 3).reshape(128, 2, G32, 16)

    s5B = np.ascontiguousarray(np.stack([gB(ssm_b_re[0]), gB(ssm_b_im[0])], axis=2))
    s5C = np.ascontiguousarray(np.stack([gC(ssm_c_re[0]), gC(ssm_c_im[0])], axis=2))
    flags = np.zeros((128, 2), np.float32)
    flags[:, 1 - h] = 1.0
    m = dict(shared)
    m.update({
        "xw": local_view(x[b], h, WIN), "cvec": cvec, "qk_g": np.ascontiguousarray(qk),
        "s5p": s5p, "s5B": s5B, "s5C": s5C, "flags": flags,
    })
    return m


def kernel(x, mix_norm_even, w_in, conv_w, conv_b, conv_ln_g, conv_ln_b, q_norm, k_norm,
           w_out, mix_norm_odd, ssm_lam_re, ssm_lam_im, ssm_log_dt, ssm_b_re, ssm_b_im,
           ssm_c_re, ssm_c_im, ssm_d, w_glu, ffn_norm, w_router, b_router,
           w_e_gate, w_e_up, w_e_down):
    f = lambda a: np.ascontiguousarray(np.asarray(a, dtype=np.float32))
    args = [f(a) for a in (x, mix_norm_even, w_in, conv_w, conv_b, conv_ln_g, conv_ln_b, q_norm, k_norm,
                           w_out, mix_norm_odd, ssm_lam_re, ssm_lam_im, ssm_log_dt, ssm_b_re, ssm_b_im,
                           ssm_c_re, ssm_c_im, ssm_d, w_glu, ffn_norm, w_router, b_router,
                           w_e_gate, w_e_up, w_e_down)]
    (x, mix_norm_even, w_in, conv_w, conv_b, conv_ln_g, conv_ln_b, q_norm, k_norm,
     w_out, mix_norm_odd, ssm_lam_re, ssm_lam_im, ssm_log_dt, ssm_b_re, ssm_b_im,
     ssm_c_re, ssm_c_im, ssm_d, w_glu, ffn_norm, w_router, b_router, w_e_gate, w_e_up, w_e_down) = args
    shared = {
        "g0": np.ascontiguousarray(mix_norm_even[0][None, :]), "w_in": w_in[0], "w_out": w_out[0],
        "eb": host_eb(), "cst": host_consts(), "gf": ffn_norm, "w_r": w_router, "b_r": b_router,
        "wg": w_e_gate, "wu": w_e_up, "wd": w_e_down, "gn": np.ascontiguousarray(mix_norm_odd[0][None, :]),
        "dvec": np.ascontiguousarray(ssm_d[0][None, :]), "masks": host_masks(), "w_glu": w_glu[0],
        "zeros": np.zeros((1024, D), ml_dtypes.bfloat16),
    }
    nc = get_prog("F", build_fused)
    in_maps = [fused_inputs(c, *args, shared) for c in range(NCORES)]
    res = run_bass_kernel_spmd(nc, in_maps, core_ids=list(range(NCORES)))
    return from_local([r["out"] for r in res.results]).astype(np.float32)


BIGIDX = float(1 << 20)
I32 = mybir.dt.int32


def emit_phaseB_sparse(P, io):
    with contextlib.ExitStack() as ph:
        P.cur = ph
        _phaseB_sparse_body(P, io["x1"], io["affT_seq"], io["aff_own"], io["gf"], io["wg"], io["wu"], io["wd"],
                            io["x2_out"], io.get("gn"), io.get("hn_out"), io["xc"], io["yc"])
        P.barrier()
    P.cur = None


def _phaseB_sparse_body(P, x1, affT_seq, aff_own, gf, wg, wu, wd, x2_out, gn, hn_out, Xc, Yc):
    dve, act, pe, pool, sp = P.dve, P.act, P.pe, P.pool, P.sp
    NT = OWN // 128
    if gn is not None:
        gnb = P.sb([128, D], F32, name="gnb")
        bgnb = Buf()
        sp.dma(out=gnb[:], in_=bcast_rows(gn, 128), writes=[bgnb])
    gfb = P.sb([128, D], F32, name="gfb")
    bgfb = Buf()
    sp.dma(out=gfb[:], in_=bcast_rows(gf, 128), writes=[bgfb])
    gate = P.sb([128, NT, NEXP], F32, name="gate")
    bgate = Buf()
    idx = P.sb([128, NT, NEXP], I32, name="idx")
    bidx = Buf()
    if P.bcreg is None:
        P.bcreg = P.nc.gpsimd.alloc_register("bcreg")
        P.nc.gpsimd.reg_mov(P.bcreg, NEXP * CAP - 1)

    acc = P.sb([128, NT, D], F32, name="acc")
    bacc = [Buf() for _ in range(NT)]
    for t in range(NT):
        sp.dma(out=acc[:, t, :], in_=x1[t * 128:(t + 1) * 128, :], writes=[bacc[t]])
    wgr = Ring(P, 2, [128, 8, D], BF16, P.cur)
    wur = Ring(P, 2, [128, 8, D], BF16, P.cur)
    wdr = Ring(P, 1, [128, 8, D], BF16, P.cur)

    def load_w(e, which=((0, 1, 2))):
        wts = []
        for src, ring in [((wg, wgr), (wu, wur), (wd, wdr))[i_] for i_ in which]:
            w, bw = ring.next()
            sv = src[e].rearrange("(k p) f -> p k f", p=128)
            pool.dma(out=w[:, 0:4, :], in_=sv[:, 0:4, :], writes=[bw])
            pool.dma(out=w[:, 4:8, :], in_=sv[:, 4:8, :], writes=[bw])
            wts.append((w, bw))
        return wts

    nxt = load_w(0, (0, 1))
    wd0 = load_w(0, (2,))

    with contextlib.ExitStack() as st:
        pthr = P.ps([128, 512], F32, stack=st)
        bpthr = Buf()
        aT = P.sb([NEXP, SEQ], F32, stack=st)
        baT = Buf()
        sp.dma(out=aT[:].rearrange("e (r t) -> e r t", r=2), in_=affT_seq, writes=[baT])
        junk = P.sb([NEXP, SEQ], F32, stack=st)
        bjunk = Buf()
        lo = P.sb([NEXP, 1], F32, stack=st)
        blo = Buf()
        mid = P.sb([NEXP, 1], F32, stack=st)
        bmid = Buf()
        cnt = P.sb([NEXP, 1], F32, stack=st)
        bcnt = Buf()
        ge = P.sb([NEXP, 1], F32, stack=st)
        bge = Buf()
        dve.op(lambda e: e.memset(lo[:], 0.0), [], [blo])
        for it in range(30):
            wk = 2.0 ** (-(it + 1))
            dve.op(ts_fn(mid[:], lo[:], wk, None, ALU.add), [blo], [bmid])
            dve.op(lambda e: e.tensor_scalar(out=junk[:], in0=aT[:], scalar1=mid[:, 0:1], scalar2=0.0,
                                             op0=ALU.is_ge, op1=ALU.add, accum_out=cnt[:, 0:1]),
                   [baT, bmid], [bjunk, bcnt])
            dve.op(ts_fn(ge[:], cnt[:], CAP - 0.5, None, ALU.is_ge), [bcnt], [bge])
            dve.op(stt_fn(lo[:], ge[:], wk, lo[:], ALU.mult, ALU.add), [bge, blo], [blo])
        dthr = P.sb([NEXP, NEXP], F32, stack=st)
        bdthr = Buf()
        dve.op(ts_fn(dthr[:], P.cst[0:NEXP, 0, 0:NEXP], lo[:, 0:1], None, ALU.mult), [P.bcst, blo], [bdthr])
        pe.op(mm_fn(pthr[:, 0:NEXP], P.cst[0:NEXP, 3, :], dthr[:], True, True), [P.bcst, bdthr], [bpthr])
        thrb = P.sb([128, NEXP], F32, stack=st)
        bthrb = Buf()
        act.op(act_fn(thrb[:], pthr[:, 0:NEXP], AF.Copy), [bpthr], [bthrb])
        ao = P.sb([128, NT, NEXP], F32, stack=st)
        bao = Buf()
        sp.dma(out=ao[:], in_=aff_own.rearrange("(t p) e -> p t e", p=128), writes=[bao])
        msk = P.sb([128, NT, NEXP], F32, stack=st)
        bmsk = Buf()

        def bct(t):
            a = t[:]
            return bass.AP(a.tensor, a.offset, [list(a.ap[0]), [0, NT], [1, NEXP]])

        dve.op(tt_fn(msk[:], ao[:], bct(thrb), ALU.is_ge), [bao, bthrb], [bmsk])
        ppos = P.ps([128, 512], F32, stack=st)
        bppos = Buf()
        pcnt = P.ps([128, 512], F32, stack=st)
        bpcnt = Buf()
        m2 = msk[:].rearrange("p t e -> p (t e)")
        pe.op(mm_fn(ppos[:, 0:NT * NEXP], P.cst[:, 4, :], m2, True, True), [P.bcst, bmsk], [bppos])
        pe.op(mm_fn(pcnt[:, 0:NT * NEXP], P.cst[:, 3, :], m2, True, True), [P.bcst, bmsk], [bpcnt])
        csb = P.sb([128, NT, NEXP], F32, stack=st)
        bcsb = Buf()
        act.op(act_fn(csb[:].rearrange("p t e -> p (t e)"), pcnt[:, 0:NT * NEXP], AF.Copy), [bpcnt], [bcsb])
        off = P.sb([128, NT, NEXP], F32, stack=st)
        boff = Buf()
        dve.op(lambda e: e.memset(off[:, 0, :], 0.0), [], [boff])
        for i in range(1, NT):
            dve.op(tt_fn(off[:, i, :], off[:, i - 1, :], csb[:, i - 1, :], ALU.add), [boff, bcsb], [boff])
        pos = P.sb([128, NT, NEXP], F32, stack=st)
        bpos = Buf()
        dve.op(tt_fn(pos[:].rearrange("p t e -> p (t e)"), ppos[:, 0:NT * NEXP],
                     off[:].rearrange("p t e -> p (t e)"), ALU.add), [bppos, boff], [bpos])
        ok = P.sb([128, NT, NEXP], F32, stack=st)
        bok = Buf()
        dve.op(ts_fn(ok[:], pos[:], CAP - 0.5, None, ALU.is_lt), [bpos], [bok])
        dve.op(tt_fn(msk[:], msk[:], ok[:], ALU.mult), [bmsk, bok], [bmsk])
        dve.op(tt_fn(gate[:], msk[:], ao[:], ALU.mult), [bmsk, bao], [bgate])
        ebase = P.cst[:, 5, 0:NEXP]
        eb_bc = bass.AP(ebase.tensor, ebase.offset, [list(ebase.ap[0]), [0, NT], [1, NEXP]])
        dve.op(tt_fn(pos[:], pos[:], eb_bc, ALU.add), [bpos, P.bcst], [bpos])
        dve.op(ts_fn(pos[:], pos[:], -BIGIDX, None, ALU.add), [bpos], [bpos])
        dve.op(tt_fn(pos[:], pos[:], msk[:], ALU.mult), [bpos, bmsk], [bpos])
        dve.op(ts_fn(pos[:], pos[:], BIGIDX, None, ALU.add), [bpos], [bpos])
        dve.op(lambda e: e.tensor_copy(out=idx[:], in_=pos[:]), [bpos], [bidx])
        P.barrier()

    def indirect(out, out_off, in_, in_off, reads, writes):
        s_ = pool
        if s_.dsems is None:
            s_.dsems = [P.newsem("d%d_%s" % (i, s_.name)) for i in range(s_.nslots)]
        j = s_.dn
        slot = j % s_.nslots
        key = (s_, slot)
        prev = 16 * (j // s_.nslots)
        if prev > 0:
            s_._wait((key, s_.dsems[slot], prev))
        s_._deps(reads, writes, True)
        ins = s_.eng.indirect_dma_start(out=out, out_offset=out_off, in_=in_, in_offset=in_off,
                                        bounds_check=P.bcreg, oob_is_err=False)
        ins.then_inc(s_.dsems[slot], 16)
        s_.dn += 1
        s_._mark((key, s_.dsems[slot], prev + 16), reads, writes)

    with contextlib.ExitStack() as st:
        bXc = [Buf() for _ in range(NEXP)]
        bYc = [Buf() for _ in range(NEXP)]
        small = Ring(P, 8, [128, 1], F32, st)
        with contextlib.ExitStack() as st1:
            junk = Ring(P, 1, [128, D], F32, st1)
            hnr = Ring(P, 3, [128, D], BF16, st1)
            for t in range(NT):
                rs, br = rms_rstd(P, acc[:, t, :], bacc[t], D, junk, small)
                hn, bhn = hnr.next()
                dve.op(stt_fn(hn[:], acc[:, t, :], rs[:, 0:1], gfb[:], ALU.mult, ALU.mult), [bacc[t], br, bgfb], [bhn])
                for e in range(NEXP):
                    indirect(Xc[:, :], bass.IndirectOffsetOnAxis(ap=idx[:, t, e:e + 1], axis=0), hn[:, :], None,
                             [bhn, bidx], [bXc[e]])
            P.barrier()
        xsr = Ring(P, 1, [128, 4, D], BF16, st)
        xTr = Ring(P, 1, [128, 8, CAP], BF16, st)
        atr = Ring(P, 1, [128, 8, CAP], BF16, st)
        ysr = Ring(P, 1, [128, 4, D], BF16, st)
        sgr = Ring(P, 1, [128, 512], F32, st)
        ybr = Ring(P, 3, [128, D], BF16, st)
        ptr = Ring(P, 2, [128, 8, 128], BF16, st, psum=True)
        pgr = Ring(P, 2, [128, 512], F32, st, psum=True)
        pur = Ring(P, 2, [128, 512], F32, st, psum=True)
        pyr = Ring(P, 2, [128, 512], F32, st, psum=True)
        for yb_, byb_ in zip(ybr.t, ybr.b):
            dve.op(lambda e, yb_=yb_: e.memset(yb_[:], 0.0), [], [byb_])

        def gather_back(e):
            for t in range(NT):
                yb, byb = ybr.next()
                indirect(yb[:, :], None, Yc[:, :], bass.IndirectOffsetOnAxis(ap=idx[:, t, e:e + 1], axis=0),
                         [bYc[e], bidx], [byb])
                dve.op(stt_fn(acc[:, t, :], yb[:], gate[:, t, e:e + 1], acc[:, t, :], ALU.mult, ALU.add),
                       [byb, bgate, bacc[t]], [bacc[t]])

        for e in range(NEXP):
            (Wg, bWg), (Wu, bWu) = nxt
            ((Wd, bWd),) = wd0 if e == 0 else load_w(e, (2,))
            if e + 1 < NEXP:
                nxt = load_w(e + 1, (0, 1))
            xs, bxs = xsr.next()
            sp.dma(out=xs[:], in_=Xc[e * CAP:(e + 1) * CAP, :].rearrange("(s p) d -> p s d", p=128),
                   reads=[bXc[e]], writes=[bxs])
            xT, bxT = xTr.next()
            for sl in range(4):
                pt, bpt = ptr.next()
                for k in range(8):
                    pe.op(lambda en, pt=pt, k=k, xs=xs, sl=sl: en.transpose(pt[:, k, :], xs[:, sl, k * 128:(k + 1) * 128],
                                                                           P.identb[:]), [bxs, P.bidb], [bpt])
                act.op(act_fn(xT[:, :, sl * 128:(sl + 1) * 128], pt[:], AF.Copy), [bpt], [bxT])
            AT, bAT = atr.next()
            for fc in range(8):
                pg, bpg = pgr.next()
                pu, bpu = pur.next()
                for k in range(8):
                    pe.op(mm_fn(pg[:], Wg[:, k, fc * 128:(fc + 1) * 128], xT[:, k, :], k == 0, k == 7), [bWg, bxT], [bpg])
                for k in range(8):
                    pe.op(mm_fn(pu[:], Wu[:, k, fc * 128:(fc + 1) * 128], xT[:, k, :], k == 0, k == 7), [bWu, bxT], [bpu])
                sg, bsg = sgr.next()
                act.op(act_fn(sg[:], pg[:], AF.Silu), [bpg], [bsg])
                dve.op(tt_fn(AT[:, fc, :], sg[:], pu[:], ALU.mult), [bsg, bpu], [bAT])
            ys, bys = ysr.next()
            for sl in range(4):
                for hc in range(2):
                    py, bpy = pyr.next()
                    for fc in range(8):
                        pe.op(mm_fn(py[:], AT[:, fc, sl * 128:(sl + 1) * 128], Wd[:, fc, hc * 512:(hc + 1) * 512],
                                    fc == 0, fc == 7), [bAT, bWd], [bpy])
                    act.op(act_fn(ys[:, sl, hc * 512:(hc + 1) * 512], py[:], AF.Copy), [bpy], [bys])
            sp.dma(out=Yc[e * CAP:(e + 1) * CAP, :].rearrange("(s p) d -> p s d", p=128), in_=ys[:],
                   reads=[bys], writes=[bYc[e]])
            if e >= 1:
                gather_back(e - 1)
        gather_back(NEXP - 1)
        hor = Ring(P, 1, [128, D], F32, st)
        junk = hor
        for t in range(NT):
            sp.dma(out=x2_out[t * 128:(t + 1) * 128, :], in_=acc[:, t, :], reads=[bacc[t]])
            if gn is not None:
                rs, br = rms_rstd(P, acc[:, t, :], bacc[t], D, junk, small)
                ho, bho = hor.next()
                dve.op(stt_fn(ho[:], acc[:, t, :], rs[:, 0:1], gnb[:], ALU.mult, ALU.mult),
                       [bacc[t], br, bgnb], [bho])
                sp.dma(out=hn_out[t * 128:(t + 1) * 128, :], in_=ho[:], reads=[bho])
```
